# Optimizing a Trainium2 kernel written in Bass

```python
import math
import jax, jax.numpy as jnp
from jax import lax
import numpy as np

D_MODEL = 1024
BATCH = 4
SEQ = 8192
DEPTH = 1

EPS = 1e-6
ROPE_THETA = 10000.0
MOBA_HEADS = 8
MOBA_HEAD_DIM = 64
MOBA_BLOCK = 256
MOBA_TOPK = 3
MOBA_QBLOCK = 128
MOBA_W = MOBA_HEADS * MOBA_HEAD_DIM
SSD_D_INNER = D_MODEL
SSD_HEAD_DIM = 64
SSD_HEADS = SSD_D_INNER // SSD_HEAD_DIM
SSD_GROUPS = 4
SSD_STATE = 128
SSD_CONV = 4
SSD_CHUNK = 256
SSD_BC_W = SSD_GROUPS * SSD_STATE
SSD_XBC_W = SSD_D_INNER + 2 * SSD_BC_W
MEM_LEN = 256
MEM_HEADS = 4
MEM_HEAD_DIM = 128
MEM_W = MEM_HEADS * MEM_HEAD_DIM
N_BRANCH = 3
IN_SIZES = (MOBA_W, MOBA_W, MOBA_W, SSD_D_INNER, SSD_XBC_W, SSD_HEADS, MEM_W, N_BRANCH * D_MODEL)
IN_COLS = sum(IN_SIZES)
PAD_MULT = 256
MOE_GROUPS = 4
MOE_EXPERTS_PER_GROUP = 8
MOE_EXPERTS = MOE_GROUPS * MOE_EXPERTS_PER_GROUP
MOE_TOPK = 2
MOE_D_FF = 512
MOE_BLOCK = 128

kernel_name = 'hybrid_moba_ssd_memory_hmoe_layer'


def rmsnorm(u, g):
    uf = u.astype(jnp.float32)
    r = lax.rsqrt(jnp.mean(uf * uf, axis=-1, keepdims=True) + EPS)
    return (uf * r).astype(u.dtype) * g


def rope(u):
    s_len, d = u.shape[1], u.shape[-1]
    half = d // 2
    inv = ROPE_THETA ** (-jnp.arange(half, dtype=jnp.float32) / half)
    ang = jnp.arange(s_len, dtype=jnp.float32)[:, None] * inv[None, :]
    cos = jnp.cos(ang)[None, :, None, :]
    sin = jnp.sin(ang)[None, :, None, :]
    uf = u.astype(jnp.float32)
    u1, u2 = uf[..., :half], uf[..., half:]
    return jnp.concatenate([u1 * cos - u2 * sin, u2 * cos + u1 * sin], axis=-1).astype(u.dtype)


def moba_attention(q, k, v):
    bsz, s_len, nh, d = q.shape
    L = MOBA_BLOCK
    nb = s_len // L
    scale = d ** -0.5
    q = q.transpose(0, 2, 1, 3)
    kb = k.transpose(0, 2, 1, 3).reshape(bsz, nh, nb, L, d)
    vb = v.transpose(0, 2, 1, 3).reshape(bsz, nh, nb, L, d)
    k_mean = jnp.mean(kb.astype(jnp.float32), axis=3)
    gate = jnp.einsum('bhsd,bhnd->bhsn', q.astype(jnp.float32), k_mean)
    q_blk = jnp.arange(s_len) // L
    past = jnp.arange(nb)[None, :] < q_blk[:, None]
    gate = jnp.where(past[None, None], gate, -jnp.inf)
    n_sel = min(MOBA_TOPK, max(nb - 1, 1))
    g_val, g_idx = lax.top_k(gate, n_sel)
    g_valid = jnp.isfinite(g_val)
    b_ix = jnp.arange(bsz)[:, None, None]
    h_ix = jnp.arange(nh)[None, :, None]
    QB = MOBA_QBLOCK

    def one_block(i):
        t0 = i * QB
        qc = lax.dynamic_slice_in_dim(q, t0, QB, axis=2).astype(jnp.float32) * scale
        ic = lax.dynamic_slice_in_dim(g_idx, t0, QB, axis=2)
        vc = lax.dynamic_slice_in_dim(g_valid, t0, QB, axis=2)
        own = t0 // L
        k_own = lax.dynamic_index_in_dim(kb, own, axis=2, keepdims=False).astype(jnp.float32)
        v_own = lax.dynamic_index_in_dim(vb, own, axis=2, keepdims=False).astype(jnp.float32)
        s_own = jnp.einsum('bhqd,bhkd->bhqk', qc, k_own)
        causal = (own * L + jnp.arange(L))[None, :] <= (t0 + jnp.arange(QB))[:, None]
        s_own = jnp.where(causal[None, None], s_own, -jnp.inf)
        s_parts = []
        for n in range(n_sel):
            k_n = kb[b_ix, h_ix, ic[..., n]].astype(jnp.float32)
            s_n = jnp.einsum('bhqd,bhqkd->bhqk', qc, k_n)
            s_parts.append(jnp.where(vc[..., n, None], s_n, -jnp.inf))
        scores = jnp.concatenate(s_parts + [s_own], axis=-1)
        p = jax.nn.softmax(scores, axis=-1)
        out = jnp.einsum('bhqk,bhkd->bhqd', p[..., n_sel * L:], v_own)
        for n in range(n_sel):
            v_n = vb[b_ix, h_ix, ic[..., n]].astype(jnp.float32)
            out = out + jnp.einsum('bhqk,bhqkd->bhqd', p[..., n * L:(n + 1) * L], v_n)
        return out.astype(q.dtype)

    outs = lax.map(one_block, jnp.arange(s_len // QB))
    return outs.transpose(1, 0, 3, 2, 4).reshape(bsz, s_len, nh * d)


def causal_dwconv(u, w, b):
    kw = w.shape[0]
    out = lax.conv_general_dilated(u, w[:, None, :], window_strides=(1,), padding=[(kw - 1, 0)],
                                   dimension_numbers=('NWC', 'WIO', 'NWC'), feature_group_count=u.shape[-1])
    return out + b


def ssd_scan(x, dt, A, Bm, Cm):
    bsz, s_len, nh, hp = x.shape
    G, N = Bm.shape[2], Bm.shape[3]
    R = nh // G
    Q = SSD_CHUNK
    nc = s_len // Q
    x = x.astype(jnp.float32).reshape(bsz, nc, Q, G, R, hp)
    dt = dt.astype(jnp.float32).reshape(bsz, nc, Q, G, R)
    Bm = Bm.astype(jnp.float32).reshape(bsz, nc, Q, G, N)
    Cm = Cm.astype(jnp.float32).reshape(bsz, nc, Q, G, N)
    a_cum = jnp.cumsum(dt * A.reshape(G, R), axis=2)
    x_dt = x * dt[..., None]
    a_t = a_cum.transpose(0, 1, 3, 4, 2)
    seg = a_t[..., :, None] - a_t[..., None, :]
    tril = jnp.tril(jnp.ones((Q, Q), dtype=bool))
    Lmat = jnp.exp(jnp.where(tril, seg, -jnp.inf))
    cb = jnp.einsum('bcign,bcjgn->bcgij', Cm, Bm)
    y_diag = jnp.einsum('bcgrij,bcjgrp->bcigrp', cb[:, :, :, None] * Lmat, x_dt)
    decay = jnp.exp(a_cum[:, :, -1:] - a_cum)
    states = jnp.einsum('bcjgn,bcjgrp->bcgrpn', Bm, decay[..., None] * x_dt)
    chunk_decay = jnp.exp(a_cum[:, :, -1])

    def step(h, inp):
        st, dec = inp
        return h * dec[..., None, None] + st, h

    h0 = jnp.zeros((bsz, G, R, hp, N), jnp.float32)
    _, h_in = lax.scan(step, h0, (states.transpose(1, 0, 2, 3, 4, 5), chunk_decay.transpose(1, 0, 2, 3)))
    h_in = h_in.transpose(1, 0, 2, 3, 4, 5)
    y_off = jnp.einsum('bcign,bcgrpn->bcigrp', Cm, h_in) * jnp.exp(a_cum)[..., None]
    return (y_diag + y_off).reshape(bsz, s_len, nh * hp)


def memory_attention(qm, mem, g_mem, w_mem_kv, q_gain, k_gain):
    bsz, s_len, _ = qm.shape
    m_len = mem.shape[1]
    kv = rmsnorm(mem, g_mem) @ w_mem_kv
    km = rmsnorm(kv[..., :MEM_W].reshape(bsz, m_len, MEM_HEADS, MEM_HEAD_DIM), k_gain)
    vm = kv[..., MEM_W:].reshape(bsz, m_len, MEM_HEADS, MEM_HEAD_DIM)
    qh = rmsnorm(qm.reshape(bsz, s_len, MEM_HEADS, MEM_HEAD_DIM), q_gain)
    s = jnp.einsum('bshd,bmhd->bhsm', qh.astype(jnp.float32), km.astype(jnp.float32)) * (MEM_HEAD_DIM ** -0.5)
    p = jax.nn.softmax(s, axis=-1)
    o = jnp.einsum('bhsm,bmhd->bshd', p, vm.astype(jnp.float32))
    return o.reshape(bsz, s_len, MEM_W).astype(qm.dtype)


def hierarchical_moe(h2, w_router_group, w_router_expert, w_gate, w_up, w_down):
    n_tok, d = h2.shape
    grp_prob = jax.nn.softmax((h2 @ w_router_group).astype(jnp.float32), axis=-1)
    g_sel = jnp.argmax(grp_prob, axis=-1)
    p_g = jnp.max(grp_prob, axis=-1)
    e_logits = (h2 @ w_router_expert).astype(jnp.float32).reshape(n_tok, MOE_GROUPS, MOE_EXPERTS_PER_GROUP)
    e_logits = jnp.take_along_axis(e_logits, g_sel[:, None, None], axis=1)[:, 0]
    e_prob = jax.nn.softmax(e_logits, axis=-1)
    w_top, e_top = lax.top_k(e_prob, MOE_TOPK)
    w_top = w_top / jnp.sum(w_top, axis=-1, keepdims=True)
    comb = p_g[:, None] * w_top
    expert_id = g_sel[:, None] * MOE_EXPERTS_PER_GROUP + e_top
    A = n_tok * MOE_TOPK
    e_flat = expert_id.reshape(A).astype(jnp.int32)
    tok_flat = jnp.repeat(jnp.arange(n_tok, dtype=jnp.int32), MOE_TOPK)
    w_flat = comb.reshape(A)
    order = jnp.argsort(e_flat)
    e_s, tok_s, w_s = e_flat[order], tok_flat[order], w_flat[order]
    counts = jnp.bincount(e_flat, length=MOE_EXPERTS)
    padded = ((counts + MOE_BLOCK - 1) // MOE_BLOCK) * MOE_BLOCK
    pad_end = jnp.cumsum(padded)
    pad_start = pad_end - padded
    raw_start = jnp.cumsum(counts) - counts
    dest = pad_start[e_s] + jnp.arange(A, dtype=jnp.int32) - raw_start[e_s]
    n_blocks = (A + MOE_BLOCK - 1) // MOE_BLOCK + MOE_EXPERTS
    P = n_blocks * MOE_BLOCK
    buf_tok = jnp.zeros((P,), jnp.int32).at[dest].set(tok_s)
    buf_w = jnp.zeros((P,), h2.dtype).at[dest].set(w_s.astype(h2.dtype))
    blk_start = jnp.arange(n_blocks, dtype=jnp.int32) * MOE_BLOCK
    blk_expert = jnp.minimum(jnp.sum(pad_end[None, :] <= blk_start[:, None], axis=1), MOE_EXPERTS - 1)
    xb = h2[buf_tok].reshape(n_blocks, MOE_BLOCK, d)

    def expert_block(args):
        xi, e = args
        hid = jax.nn.silu(xi @ w_gate[e]) * (xi @ w_up[e])
        return hid @ w_down[e]

    yb = lax.map(expert_block, (xb, blk_expert)).reshape(P, d)
    return jnp.zeros((n_tok, d), h2.dtype).at[buf_tok].add(yb * buf_w[:, None])


def setup_inputs(seed: int = 0) -> dict:
    key = jax.random.key(seed)
    ks = jax.random.split(key, 32)
    f32 = jnp.float32

    def nrm(k, shape, fan_in):
        return jax.random.normal(k, shape, f32) * fan_in ** -0.5

    def gain(k, n):
        return 1.0 + 0.02 * jax.random.normal(k, (n,), f32)

    dt0 = jnp.exp(jax.random.uniform(ks[8], (SSD_HEADS,), f32) * (math.log(0.1) - math.log(0.001)) + math.log(0.001))
    return {
        'x': jax.random.normal(ks[0], (BATCH, SEQ, D_MODEL), f32),
        'mem': jax.random.normal(ks[1], (BATCH, MEM_LEN, D_MODEL), f32),
        'g_mix': gain(ks[2], D_MODEL),
        'w_in': nrm(ks[3], (D_MODEL, IN_COLS), D_MODEL),
        'moba_q_norm': gain(ks[4], MOBA_HEAD_DIM),
        'moba_k_norm': gain(ks[5], MOBA_HEAD_DIM),
        'conv_w': nrm(ks[6], (SSD_CONV, SSD_XBC_W), SSD_CONV),
        'conv_b': 0.01 * jax.random.normal(ks[7], (SSD_XBC_W,), f32),
        'dt_bias': dt0 + jnp.log(-jnp.expm1(-dt0)),
        'a_log': jnp.log(jax.random.uniform(ks[9], (SSD_HEADS,), f32, minval=1.0, maxval=16.0)),
        'd_skip': 1.0 + 0.1 * jax.random.normal(ks[10], (SSD_HEADS,), f32),
        'ssd_norm': gain(ks[11], SSD_D_INNER),
        'g_mem': gain(ks[12], D_MODEL),
        'w_mem_kv': nrm(ks[13], (D_MODEL, 2 * MEM_W), D_MODEL),
        'mem_q_norm': gain(ks[14], MEM_HEAD_DIM),
        'mem_k_norm': gain(ks[15], MEM_HEAD_DIM),
        'w_o_moba': nrm(ks[16], (MOBA_W, D_MODEL), MOBA_W),
        'w_o_ssd': nrm(ks[17], (SSD_D_INNER, D_MODEL), SSD_D_INNER),
        'w_o_mem': nrm(ks[18], (MEM_W, D_MODEL), MEM_W),
        'w_out': nrm(ks[19], (D_MODEL, D_MODEL), D_MODEL),
        'g_ffn': gain(ks[20], D_MODEL),
        'w_router_group': nrm(ks[21], (D_MODEL, MOE_GROUPS), D_MODEL),
        'w_router_expert': nrm(ks[22], (D_MODEL, MOE_EXPERTS), D_MODEL),
        'w_gate': nrm(ks[23], (MOE_EXPERTS, D_MODEL, MOE_D_FF), D_MODEL),
        'w_up': nrm(ks[24], (MOE_EXPERTS, D_MODEL, MOE_D_FF), D_MODEL),
        'w_down': nrm(ks[25], (MOE_EXPERTS, MOE_D_FF, D_MODEL), MOE_D_FF),
    }


def reference(x, mem, g_mix, w_in, moba_q_norm, moba_k_norm, conv_w, conv_b, dt_bias, a_log, d_skip,
              ssd_norm, g_mem, w_mem_kv, mem_q_norm, mem_k_norm, w_o_moba, w_o_ssd, w_o_mem, w_out,
              g_ffn, w_router_group, w_router_expert, w_gate, w_up, w_down):
    bsz, s_len, d = x.shape
    s_pad = ((s_len + PAD_MULT - 1) // PAD_MULT) * PAD_MULT
    for _layer in range(DEPTH):
        h = rmsnorm(x, g_mix)
        h = jnp.pad(h, ((0, 0), (0, s_pad - s_len), (0, 0)))
        proj = h @ w_in
        offs = np.cumsum((0,) + IN_SIZES)
        parts = [proj[..., int(offs[i]):int(offs[i + 1])] for i in range(len(IN_SIZES))]
        q_a, k_a, v_a, z, xbc, dt_raw, q_m, gate_logits = parts
        q_a = rope(rmsnorm(q_a.reshape(bsz, s_pad, MOBA_HEADS, MOBA_HEAD_DIM), moba_q_norm))
        k_a = rope(rmsnorm(k_a.reshape(bsz, s_pad, MOBA_HEADS, MOBA_HEAD_DIM), moba_k_norm))
        v_a = v_a.reshape(bsz, s_pad, MOBA_HEADS, MOBA_HEAD_DIM)
        o_a = moba_attention(q_a, k_a, v_a)
        xbc = jax.nn.silu(causal_dwconv(xbc, conv_w, conv_b))
        xs = xbc[..., :SSD_D_INNER].reshape(bsz, s_pad, SSD_HEADS, SSD_HEAD_DIM)
        Bm = xbc[..., SSD_D_INNER:SSD_D_INNER + SSD_BC_W].reshape(bsz, s_pad, SSD_GROUPS, SSD_STATE)
        Cm = xbc[..., SSD_D_INNER + SSD_BC_W:].reshape(bsz, s_pad, SSD_GROUPS, SSD_STATE)
        dt = jax.nn.softplus(dt_raw.astype(jnp.float32) + dt_bias.astype(jnp.float32))
        A = -jnp.exp(a_log.astype(jnp.float32))
        y = ssd_scan(xs, dt, A, Bm, Cm)
        y = y + (d_skip.astype(jnp.float32)[:, None] * xs.astype(jnp.float32)).reshape(bsz, s_pad, SSD_D_INNER)
        y = y * jax.nn.silu(z.astype(jnp.float32))
        y = rmsnorm(y.reshape(bsz, s_pad, SSD_GROUPS, SSD_D_INNER // SSD_GROUPS),
                    ssd_norm.astype(jnp.float32).reshape(SSD_GROUPS, SSD_D_INNER // SSD_GROUPS))
        o_s = y.reshape(bsz, s_pad, SSD_D_INNER).astype(h.dtype)
        o_m = memory_attention(q_m, mem, g_mem, w_mem_kv, mem_q_norm, mem_k_norm)
        gates = jax.nn.sigmoid(gate_logits.astype(jnp.float32)).astype(h.dtype).reshape(bsz, s_pad, N_BRANCH, d)
        merged = gates[:, :, 0] * (o_a @ w_o_moba) + gates[:, :, 1] * (o_s @ w_o_ssd) + gates[:, :, 2] * (o_m @ w_o_mem)
        x = x + (merged @ w_out)[:, :s_len]
        h2 = rmsnorm(x, g_ffn).reshape(bsz * s_len, d)
        x = x + hierarchical_moe(h2, w_router_group, w_router_expert, w_gate, w_up, w_down).reshape(bsz, s_len, d)
    return x
```

```python
import contextlib
import numpy as np
import ml_dtypes
import concourse.bass as bass
import concourse.mybir as mybir
from concourse.bass_utils import run_bass_kernel_spmd

F32 = mybir.dt.float32
BF16 = mybir.dt.bfloat16
I32 = mybir.dt.int32
U32 = mybir.dt.uint32
AF = mybir.ActivationFunctionType
ALU = mybir.AluOpType
AX = mybir.AxisListType

T = 4096
TP = 4096
NK = TP + T
D = 1024
EPS = 1e-6
IN_COLS = 8208
BIGG = 30000.0
MB = 1000.0
NEG = -30000.0
C_Z, C_X, C_DT = 1536, 2560, 4608
C_QM, C_G = 4624, 5136
CAP = 384
NE = 32


class Dep:
    __slots__ = ("w", "r")

    def __init__(self):
        self.w = None
        self.r = {}


class Op:
    __slots__ = ("eng", "fn", "deps", "is_dma", "sig", "sigval", "dsem", "dval", "prev")

    def __init__(self, eng, fn, is_dma):
        self.eng = eng
        self.fn = fn
        self.is_dma = is_dma
        self.deps = []
        self.sig = False
        self.sigval = 0
        self.dsem = None
        self.dval = 0
        self.prev = None


class Prog:
    ENGS = ("pe", "act", "dve", "pool", "sp")

    def __init__(self, nc, ndma_sems=12):
        self.nc = nc
        self.ops = {e: [] for e in self.ENGS}
        self.ndma = {e: 0 for e in self.ENGS}
        self.dma_last = {}
        self.ndma_sems = ndma_sems
        self.all_dma = []

    def _add(self, o, reads, writes):
        deps = {}
        for t in reads:
            if t.w is not None:
                deps[id(t.w)] = t.w
        for t in writes:
            if t.w is not None:
                deps[id(t.w)] = t.w
            for r in t.r.values():
                deps[id(r)] = r
        for t in reads:
            key = id(o) if o.is_dma else o.eng
            t.r[key] = o
        for t in writes:
            t.w = o
            t.r = {}
        dl = []
        for d in deps.values():
            if d is o:
                continue
            if (not d.is_dma) and (not o.is_dma) and d.eng == "pe" and o.eng == "pe":
                continue
            dl.append(d)
            if not d.is_dma:
                d.sig = True
        o.deps = dl
        self.ops[o.eng].append(o)
        return o

    def op(self, eng, fn, reads=(), writes=()):
        return self._add(Op(eng, fn, False), reads, writes)

    def dma(self, eng, fn, reads=(), writes=()):
        o = Op(eng, fn, True)
        n = self.ndma[eng]
        self.ndma[eng] += 1
        slot = (eng, n % self.ndma_sems)
        o.dsem = slot
        o.prev = self.dma_last.get(slot)
        o.dval = (o.prev.dval if o.prev else 0) + 16
        self.dma_last[slot] = o
        self.all_dma.append(o)
        return self._add(o, reads, writes)


    def barrier(self):
        lasts = []
        for e in self.ENGS:
            for o in reversed(self.ops[e]):
                if (not o.is_dma) and o.fn is not None:
                    o.sig = True
                    lasts.append(o)
                    break
        dmas = list(self.dma_last.values())
        for e in self.ENGS:
            o = Op(e, None, False)
            o.deps = [d for d in lasts if d.eng != e] + dmas
            self.ops[e].append(o)

    def emit(self):
        nc = self.nc
        import contextlib
        with contextlib.ExitStack() as st:
            esem = {e: st.enter_context(nc.semaphore("S_" + e)) for e in self.ENGS}
            dsem = {}
            for e in self.ENGS:
                if self.ndma[e]:
                    for i in range(min(self.ndma_sems, self.ndma[e])):
                        dsem[(e, i)] = st.enter_context(nc.semaphore("D_%s_%d" % (e, i)))
            for e in self.ENGS:
                c = 0
                for o in self.ops[e]:
                    if (not o.is_dma) and o.sig and o.fn is not None:
                        c += 1
                        o.sigval = c
            block = st.enter_context(nc.Block())
            handles = {"pe": block.tensor, "act": block.scalar, "dve": block.vector,
                       "pool": block.gpsimd, "sp": block.sync}

            def make(e):
                def body(eng):
                    known = {}

                    def wait(sem, key, val):
                        if known.get(key, 0) < val:
                            eng.wait_ge(sem, val)
                            known[key] = val
                    for o in self.ops[e]:
                        for d in o.deps:
                            if d.is_dma:
                                wait(dsem[d.dsem], d.dsem, d.dval)
                            else:
                                wait(esem[d.eng], d.eng, d.sigval)
                        if o.is_dma:
                            if o.prev is not None:
                                wait(dsem[o.dsem], o.dsem, o.prev.dval)
                            o.fn(eng).then_inc(dsem[o.dsem], 16)
                        elif o.fn is not None:
                            ins = o.fn(eng)
                            if o.sig:
                                ins.then_inc(esem[e], 1)
                    if e == "sp":
                        for slot, o in self.dma_last.items():
                            wait(dsem[slot], slot, o.dval)
                return body
            for e in self.ENGS:
                if self.ops[e] or e == "sp":
                    handles[e](make(e))


def phase_a(nc, P, st, xin, w_in, g_mix, qkn, cs, idb, d_idb, kT_d, v_d, qT_d):
    def sb(name, shape, dt):
        return st.enter_context(nc.sbuf_tensor("a_" + name, shape, dt))

    def ps(name, shape, dt=F32):
        return st.enter_context(nc.psum_tensor("a_" + name, shape, dt))
    wq = sb("wq", [128, 8, 1536], BF16); d_wq = Dep()
    gm = sb("gm", [128, D], F32); d_gm = Dep()
    gq = sb("gq", [128, 2, 512], F32); d_gq = Dep()
    NB = 2
    xt = [sb("xt%d" % i, [128, D], F32) for i in range(NB)]; d_xt = [Dep() for _ in range(NB)]
    cst = [sb("cst%d" % i, [128, 64], F32) for i in range(NB)]; d_cst = [Dep() for _ in range(NB)]
    junk = sb("junk", [128, D], BF16); d_junk = Dep()
    ssq = sb("ssq", [128, 1], F32); d_ssq = Dep()
    rstd = sb("rstd", [128, 1], F32); d_rstd = Dep()
    hb = sb("hb", [128, D], BF16); d_hb = Dep()
    hT = [sb("hT%d" % i, [128, 8, 128], BF16) for i in range(NB)]; d_hT = [Dep() for _ in range(NB)]
    pT = ps("pT", [128, 8, 128], BF16); d_pT = Dep()
    pq = [ps("pq%d" % i, [128, 512], F32) for i in range(3)]; d_pq = [Dep() for _ in range(3)]
    pkT = ps("pkT", [128, 4, 128], BF16); d_pkT = Dep()
    sq = sb("sq", [128, 512], F32); d_sq = Dep()
    hs = sb("hs", [128, 8], F32); d_hs = Dep()
    hr = sb("hr", [128, 8], F32); d_hr = Dep()
    qn = sb("qn", [128, 512], F32); d_qn = Dep()
    t1 = sb("t1", [128, 256], F32); d_t1 = Dep()
    t2 = sb("t2", [128, 256], F32); d_t2 = Dep()
    t3 = sb("t3", [128, 256], F32); d_t3 = Dep()
    t4 = sb("t4", [128, 256], F32); d_t4 = Dep()
    qr = sb("qr", [128, 512], BF16); d_qr = Dep()
    kTs = [sb("kTs%d" % i, [128, 4, 128], BF16) for i in range(NB)]; d_kTs = [Dep() for _ in range(NB)]
    vs = [sb("vs%d" % i, [128, 512], BF16) for i in range(NB)]; d_vs = [Dep() for _ in range(NB)]

    for k in range(8):
        P.dma("pool", lambda e, k=k: e.dma_start(out=wq[:, k, :], in_=w_in[k * 128:(k + 1) * 128, 0:1536]), writes=[d_wq])
    P.dma("sp", lambda e: e.dma_start(out=gm[:], in_=g_mix[0:1, :].partition_broadcast(128)), writes=[d_gm])
    for i in range(2):
        P.dma("sp", lambda e, i=i: e.dma_start(out=gq[:, i, :], in_=qkn[i:i + 1, :].partition_broadcast(128)), writes=[d_gq])
    P.op("dve", lambda e: e.tensor_scalar(out=gq[:, 0, :], in0=gq[:, 0, :], scalar1=0.125, scalar2=None, op0=ALU.mult),
         reads=[d_gq], writes=[d_gq])

    def qk_post(src_ps, d_src, which, b):
        P.op("act", lambda e: e.activation(out=sq[:], in_=src_ps[:], func=AF.Square), reads=[d_src], writes=[d_sq])
        P.op("dve", lambda e: e.tensor_reduce(out=hs[:], in_=sq[:].rearrange("p (h d) -> p h d", h=8), axis=AX.X, op=ALU.add),
             reads=[d_sq], writes=[d_hs])
        P.op("act", lambda e: e.activation(out=hr[:], in_=hs[:], func=AF.Sqrt, bias=EPS, scale=1.0 / 64), reads=[d_hs], writes=[d_hr])
        P.op("dve", lambda e: e.reciprocal(out=hr[:], in_=hr[:]), reads=[d_hr], writes=[d_hr])
        P.op("dve", lambda e: e.tensor_tensor(out=qn[:].rearrange("p (h d) -> p h d", h=8), in0=src_ps[:].rearrange("p (h d) -> p h d", h=8),
                                              in1=hr[:].unsqueeze(2).to_broadcast([128, 8, 64]), op=ALU.mult),
             reads=[d_src, d_hr], writes=[d_qn])
        P.op("pool", lambda e: e.tensor_tensor(out=qn[:], in0=qn[:], in1=gq[:, which, :], op=ALU.mult), reads=[d_qn, d_gq], writes=[d_qn])
        q3 = qn[:].rearrange("p (h d) -> p h d", h=8)
        q1 = q3[:, :, 0:32]
        q2 = q3[:, :, 32:64]
        cosb = cst[b][:, 0:32].unsqueeze(1).to_broadcast([128, 8, 32])
        sinb = cst[b][:, 32:64].unsqueeze(1).to_broadcast([128, 8, 32])
        v3 = lambda t: t[:].rearrange("p (h d) -> p h d", h=8)
        P.op("dve", lambda e: e.tensor_tensor(out=v3(t1), in0=q1, in1=cosb, op=ALU.mult), reads=[d_qn, d_cst[b]], writes=[d_t1])
        P.op("pool", lambda e: e.tensor_tensor(out=v3(t2), in0=q2, in1=sinb, op=ALU.mult), reads=[d_qn, d_cst[b]], writes=[d_t2])
        P.op("dve", lambda e: e.tensor_tensor(out=v3(t3), in0=q2, in1=cosb, op=ALU.mult), reads=[d_qn, d_cst[b]], writes=[d_t3])
        P.op("pool", lambda e: e.tensor_tensor(out=v3(t4), in0=q1, in1=sinb, op=ALU.mult), reads=[d_qn, d_cst[b]], writes=[d_t4])
        r3 = qr[:].rearrange("p (h d) -> p h d", h=8)
        P.op("dve", lambda e: e.tensor_tensor(out=r3[:, :, 0:32], in0=v3(t1), in1=v3(t2), op=ALU.subtract), reads=[d_t1, d_t2], writes=[d_qr])
        P.op("pool", lambda e: e.tensor_tensor(out=r3[:, :, 32:64], in0=v3(t3), in1=v3(t4), op=ALU.add), reads=[d_t3, d_t4], writes=[d_qr])

    def to_T_and_store(dst_dram, col0, b):
        for c in range(4):
            P.op("pe", lambda e, c=c: e.transpose(out=pkT[:, c, :], in_=qr[:, c * 128:(c + 1) * 128], identity=idb[:]),
                 reads=[d_qr, d_idb], writes=[d_pkT])
        P.op("act", lambda e: e.copy(out=kTs[b][:], in_=pkT[:]), reads=[d_pkT], writes=[d_kTs[b]])
        P.dma("sp", lambda e: e.dma_start(out=dst_dram[:, col0:col0 + 128].rearrange("(c p) t -> p c t", p=128), in_=kTs[b][:]),
              reads=[d_kTs[b]])

    for ti in range(64):
        b = ti % NB
        own = ti >= 32
        r0 = ti * 128
        P.dma("sp", lambda e, b=b, r0=r0: e.dma_start(out=xt[b][:], in_=xin[r0:r0 + 128, :]), writes=[d_xt[b]])
        P.dma("sp", lambda e, b=b, r0=r0: e.dma_start(out=cst[b][:], in_=cs[r0:r0 + 128, :]), writes=[d_cst[b]])
        P.op("act", lambda e, b=b: e.activation(out=junk[:], in_=xt[b][:], func=AF.Square, accum_out=ssq[:]), reads=[d_xt[b]], writes=[d_junk, d_ssq])
        P.op("act", lambda e: e.activation(out=rstd[:], in_=ssq[:], func=AF.Sqrt, bias=EPS, scale=1.0 / D), reads=[d_ssq], writes=[d_rstd])
        P.op("dve", lambda e: e.reciprocal(out=rstd[:], in_=rstd[:]), reads=[d_rstd], writes=[d_rstd])
        P.op("dve", lambda e, b=b: e.scalar_tensor_tensor(out=hb[:], in0=xt[b][:], scalar=rstd[:, 0:1], in1=gm[:], op0=ALU.mult, op1=ALU.mult),
             reads=[d_xt[b], d_rstd, d_gm], writes=[d_hb])
        for k in range(8):
            P.op("pe", lambda e, k=k: e.transpose(out=pT[:, k, :], in_=hb[:, k * 128:(k + 1) * 128], identity=idb[:]), reads=[d_hb, d_idb], writes=[d_pT])
        P.op("act", lambda e, b=b: e.copy(out=hT[b][:], in_=pT[:]), reads=[d_pT], writes=[d_hT[b]])
        groups = [0, 1, 2] if own else [1, 2]
        for g in groups:
            for k in range(8):
                P.op("pe", lambda e, g=g, k=k, b=b: e.matmul(pq[g][:], lhsT=hT[b][:, k, :], rhs=wq[:, k, g * 512:(g + 1) * 512], start=(k == 0), stop=(k == 7)),
                     reads=[d_hT[b], d_wq], writes=[d_pq[g]])
        P.op("act", lambda e, b=b: e.copy(out=vs[b][:], in_=pq[2][:]), reads=[d_pq[2]], writes=[d_vs[b]])
        P.dma("sp", lambda e, b=b, r0=r0: e.dma_start(out=v_d[r0:r0 + 128, :], in_=vs[b][:]), reads=[d_vs[b]])
        qk_post(pq[1], d_pq[1], 1, b)
        to_T_and_store(kT_d, r0, b)
        if own:
            qk_post(pq[0], d_pq[0], 0, b)
            to_T_and_store(qT_d, r0 - TP, b)


def phase_b(nc, P, st, kT_d, v_d, qT_d, e_d, vq_d, nb_d, oq_d, tri_d, idb, d_idb, oa, d_oa, heads=range(8), nblk=16):
    def sb(name, shape, dt):
        return st.enter_context(nc.sbuf_tensor("b_" + name, shape, dt))

    def ps(name, shape, dt=F32):
        return st.enter_context(nc.psum_tensor(name, shape, dt))
    NB = 2
    kaug = [sb("kaug%d" % i, [96, NK], BF16) for i in range(NB)]; d_kaug = [Dep() for _ in range(NB)]
    vaug = [sb("vaug%d" % i, [128, 64, 65], BF16) for i in range(NB)]; d_vaug = [Dep() for _ in range(NB)]
    qaug = [sb("qaug%d" % i, [96, T], BF16) for i in range(NB)]; d_qaug = [Dep() for _ in range(NB)]
    d_qmb = [Dep() for _ in range(NB)]
    vq = sb("vq_s", [128, 1024], F32); d_c = Dep()
    nb = sb("nb_s", [128, 1024], F32)
    oq = sb("oq_s", [128, 1024], F32)
    tri = sb("tri_s", [128, 128], BF16)
    ksum = sb("ksum", [64, 32], F32); d_ksum = Dep()
    kmean = sb("kmean", [64, 32], BF16); d_kmean = Dep()
    g1 = sb("g1", [128, 512], F32); d_g1 = Dep()
    top8 = sb("top8", [128, 16, 8], F32); d_top8 = Dep()
    sel = sb("sel", [128, 512], F32); d_sel = Dep()
    mbp = sb("mbp", [128, 16, 96], BF16); d_mbp = Dep()
    NPT = 3
    pt = [sb("pt%d" % i, [128, 256], BF16) for i in range(NPT)]; d_pt = [Dep() for _ in range(NPT)]
    rc = sb("rc", [128, 1], F32); d_rc = Dep()
    pg = ps("pg", [128, 512], F32); d_pg = Dep()
    pmT = ps("pmT", [96, 8, 128], BF16); d_pmT = Dep()
    psS = [ps("psS%d" % i, [128, 256], F32) for i in range(NPT)]; d_psS = [Dep() for _ in range(NPT)]
    po = [ps("po%d" % i, [128, 65], F32) for i in range(2)]; d_po = [Dep() for _ in range(2)]

    for i in range(NB):
        P.dma("sp", lambda e, i=i: e.dma_start(out=kaug[i][64:96, :], in_=e_d[:, :]), writes=[d_kaug[i]])
        P.op("pool", lambda e, i=i: e.memset(vaug[i][:], 1.0), writes=[d_vaug[i]])
    P.dma("sp", lambda e: e.dma_start(out=vq[:], in_=vq_d[0:1, :].partition_broadcast(128)), writes=[d_c])
    P.dma("sp", lambda e: e.dma_start(out=nb[:], in_=nb_d[0:1, :].partition_broadcast(128)), writes=[d_c])
    P.dma("sp", lambda e: e.dma_start(out=oq[:], in_=oq_d[0:1, :].partition_broadcast(128)), writes=[d_c])
    P.dma("pool", lambda e: e.dma_start(out=tri[:], in_=tri_d[:, :]), writes=[d_c])
    P.op("pool", lambda e: e.memset(mbp[:], 0.0), writes=[d_mbp])

    ipt = 0
    for ih, h in enumerate(heads):
        b = ih % NB
        P.dma("sp", lambda e, b=b, h=h: e.dma_start(out=kaug[b][0:64, :], in_=kT_d[h * 64:(h + 1) * 64, :]), writes=[d_kaug[b]])
        P.dma("sp", lambda e, b=b, h=h: e.dma_start(out=vaug[b][:, :, 0:64],
                                                    in_=v_d[:, h * 64:(h + 1) * 64].rearrange("(t p) d -> p t d", p=128)),
              writes=[d_vaug[b]])
        P.dma("sp", lambda e, b=b, h=h: e.dma_start(out=qaug[b][0:64, :], in_=qT_d[h * 64:(h + 1) * 64, :]), writes=[d_qaug[b]])
        P.op("dve", lambda e, b=b: e.tensor_reduce(out=ksum[:], in_=kaug[b][0:64, :].rearrange("p (b k) -> p b k", k=256),
                                                   axis=AX.X, op=ALU.add), reads=[d_kaug[b]], writes=[d_ksum])
        P.op("dve", lambda e: e.tensor_scalar(out=kmean[:], in0=ksum[:], scalar1=1.0 / 256, scalar2=None, op0=ALU.mult),
             reads=[d_ksum], writes=[d_kmean])
        for half in range(2):
            for j in range(16):
                qt = half * 16 + j
                P.op("pe", lambda e, b=b, j=j, qt=qt: e.matmul(pg[:, j * 32:(j + 1) * 32], lhsT=qaug[b][0:64, qt * 128:(qt + 1) * 128],
                                                              rhs=kmean[:, :], start=True, stop=True),
                     reads=[d_qaug[b], d_kmean], writes=[d_pg])
            cs_ = slice(half * 512, (half + 1) * 512)
            P.op("dve", lambda e, cs_=cs_: e.tensor_tensor(out=g1[:], in0=pg[:], in1=vq[:, cs_], op=ALU.mult),
                 reads=[d_pg, d_c], writes=[d_g1])
            P.op("dve", lambda e, cs_=cs_: e.tensor_tensor(out=g1[:], in0=g1[:], in1=nb[:, cs_], op=ALU.add),
                 reads=[d_g1, d_c], writes=[d_g1])
            for j in range(16):
                P.op("dve", lambda e, j=j: e.max(out=top8[:, j, :], in_=g1[:, j * 32:(j + 1) * 32]), reads=[d_g1], writes=[d_top8])
            P.op("dve", lambda e: e.tensor_tensor(out=sel[:].rearrange("p (j b) -> p j b", b=32),
                                                  in0=g1[:].rearrange("p (j b) -> p j b", b=32),
                                                  in1=top8[:, :, 2:3].to_broadcast([128, 16, 32]), op=ALU.is_ge),
                 reads=[d_g1, d_top8], writes=[d_sel])
            P.op("dve", lambda e, cs_=cs_: e.tensor_tensor(out=sel[:], in0=sel[:], in1=vq[:, cs_], op=ALU.mult),
                 reads=[d_sel, d_c], writes=[d_sel])
            P.op("dve", lambda e, cs_=cs_: e.tensor_tensor(out=sel[:], in0=sel[:], in1=oq[:, cs_], op=ALU.add),
                 reads=[d_sel, d_c], writes=[d_sel])
            P.op("dve", lambda e: e.tensor_scalar(out=mbp[:, :, 64:96], in0=sel[:].rearrange("p (j b) -> p j b", b=32),
                                                  scalar1=1.0, scalar2=MB, op0=ALU.subtract, op1=ALU.mult),
                 reads=[d_sel], writes=[d_mbp])
            for grp in range(2):
                for j in range(8):
                    P.op("pe", lambda e, grp=grp, j=j: e.transpose(out=pmT[:, j, :], in_=mbp[:, grp * 8 + j, :], identity=idb[:]),
                         reads=[d_mbp, d_idb], writes=[d_pmT])
                c0 = (half * 16 + grp * 8) * 128
                P.op("act", lambda e, b=b, c0=c0: e.copy(out=qaug[b][64:96, c0:c0 + 1024], in_=pmT[64:96, :, :]),
                     reads=[d_pmT], writes=[d_qmb[b]])
        for jb in range(nblk):
            ktiles = list(range(32)) + list(range(32, 32 + 2 * jb)) + [32 + 2 * jb, 32 + 2 * jb + 1]
            nk = len(ktiles)
            for ik, kt in enumerate(ktiles):
                isA = ik == nk - 2
                isB = ik == nk - 1
                s = ipt % NPT
                ipt += 1
                q0 = jb * 256 + (128 if isB else 0)
                nq = 128 if isB else 256
                P.op("pe", lambda e, b=b, s=s, kt=kt, q0=q0, nq=nq: e.matmul(psS[s][:, 0:nq], lhsT=kaug[b][0:96, kt * 128:(kt + 1) * 128],
                                                                           rhs=qaug[b][0:96, q0:q0 + nq], start=True, stop=True),
                     reads=[d_kaug[b], d_qaug[b], d_qmb[b]], writes=[d_psS[s]])
                P.op("act", lambda e, s=s, nq=nq: e.activation(out=pt[s][:, 0:nq], in_=psS[s][:, 0:nq], func=AF.Exp),
                     reads=[d_psS[s]], writes=[d_pt[s]])
                if isA or isB:
                    P.op("pool", lambda e, s=s: e.tensor_tensor(out=pt[s][:, 0:128], in0=pt[s][:, 0:128], in1=tri[:], op=ALU.mult),
                         reads=[d_pt[s], d_c], writes=[d_pt[s]])
                qis = [1] if isB else [0, 1]
                for qi in qis:
                    col0 = 0 if isB else qi * 128
                    last = (isA and qi == 0) or isB
                    P.op("pe", lambda e, b=b, s=s, kt=kt, qi=qi, col0=col0, ik=ik, last=last: e.matmul(
                        po[qi][:, :], lhsT=pt[s][:, col0:col0 + 128], rhs=vaug[b][:, kt, :], start=(ik == 0), stop=last),
                        reads=[d_pt[s], d_vaug[b]], writes=[d_po[qi]])
            for qi in range(2):
                qt = jb * 2 + qi
                P.op("dve", lambda e, qi=qi: e.reciprocal(out=rc[:], in_=po[qi][:, 64:65]), reads=[d_po[qi]], writes=[d_rc])
                P.op("dve", lambda e, qi=qi, qt=qt, h=h: e.tensor_scalar(out=oa[:, qt, h * 64:(h + 1) * 64], in0=po[qi][:, 0:64],
                                                                        scalar1=rc[:, 0:1], scalar2=None, op0=ALU.mult),
                     reads=[d_po[qi], d_rc], writes=[d_oa])


def host_consts(half):
    pv = 1.0 if half == 1 else 0.0
    V = np.zeros((32, 32), np.float32); O = np.zeros((32, 32), np.float32)
    for qt in range(32):
        jb = qt // 2
        V[qt, :16] = pv
        V[qt, 16:16 + jb] = 1.0
        O[qt, 16 + jb] = 1.0
    NBm = (V - 1.0) * BIGG
    E = np.zeros((32, NK), np.float32)
    for b in range(32):
        E[b, b * 256:(b + 1) * 256] = 1.0
    tri = (np.arange(128)[:, None] <= np.arange(128)[None, :]).astype(np.float32)
    return dict(vq=V.reshape(1, 1024), nb=NBm.reshape(1, 1024), oq=O.reshape(1, 1024),
                e_d=E.astype(ml_dtypes.bfloat16), tri=tri.astype(ml_dtypes.bfloat16))


def phase_c(nc, P, st, xin, w_in, g_mix, cwl_d, cbl_d, dtb_d, alog_d, dsk_d, ssdn_d, tri2_d, sel_d, tb_d, pflag_d,
            idb, d_idb, os_d, d_osd, chunks=range(32)):
    def sb(name, shape, dt):
        return st.enter_context(nc.sbuf_tensor("c_" + name, shape, dt))

    def ps(name, shape, dt=F32):
        return st.enter_context(nc.psum_tensor(name, shape, dt))
    wz = sb("wz", [128, 8, 1024], BF16); d_w = Dep()
    wx = sb("wx", [128, 8, 2048], BF16)
    wdt = sb("wdt", [128, 8, 16], BF16)
    gm = sb("gmc", [128, D], F32); d_c = Dep()
    cw = sb("cw", [128, 16, 4], F32)
    cb = sb("cb", [128, 16], F32)
    dtb = sb("dtb", [128, 2, 16], F32)
    Aneg = sb("Aneg", [128, 2, 16], F32); d_A = Dep()
    dsk = sb("dsk", [128, 16], F32)
    ssdn = sb("ssdn", [128, D], F32)
    tri2 = sb("tri2", [128, 2, 256], F32)
    onesf = sb("onesf", [128, 128], F32)
    sel = sb("selc", [16, 16, 128], F32)
    tb = sb("tbc", [128, 384], F32)
    pflag = sb("pflag", [128, 1], F32)
    xt = sb("xtc", [128, 2, D], F32); d_xt = Dep()
    junk = sb("junkc", [128, D], BF16); d_junk = Dep()
    ssq = sb("ssqc", [128, 2], F32); d_ssq = Dep()
    rstd = sb("rstdc", [128, 2], F32); d_rstd = Dep()
    hb = sb("hbc", [128, 2, D], BF16); d_hb = Dep()
    hT = sb("hTc", [128, 8, 256], BF16); d_hT = Dep()
    xraw = sb("xraw", [128, 16, 259], F32); d_xraw = Dep(); d_halo = Dep()
    acc = sb("acc", [128, 8, 256], F32); d_acc = [Dep() for _ in range(8)]
    xc = sb("xc", [128, 16, 256], BF16); d_xc = [Dep() for _ in range(16)]
    xtok = sb("xtok", [128, 2, 1024], BF16); d_xtok = Dep()
    btok = sb("btok", [128, 2, 512], BF16); d_btok = Dep()
    dtr = sb("dtr", [128, 2, 16], F32); d_dtr = Dep()
    dt = sb("dt", [128, 2, 16], F32); d_dt = Dep()
    aa = sb("aa", [128, 2, 16], F32); d_aa = Dep()
    acum = sb("acum", [128, 2, 16], F32); d_acum = Dep()
    nacum = sb("nacum", [128, 2, 16], F32); d_nacum = Dep()
    acumT = sb("acumT", [16, 256], F32); d_acumT = Dep()
    tot = sb("tot", [128, 16], F32); d_tot = Dep()
    cdec = sb("cdec", [128, 16], F32); d_cdec = Dep()
    dte = sb("dte", [128, 2, 16], F32); d_dte = Dep()
    eac = sb("eac", [128, 2, 16], F32); d_eac = Dep()
    w2 = sb("w2", [128, 2, 16], F32); d_w2 = Dep()
    xdt = sb("xdt", [128, 2, 1024], BF16); d_xdt = Dep()
    xdd = sb("xdd", [128, 2, 1024], BF16); d_xdd = Dep()
    zs = sb("zs", [128, 2, 1024], BF16); d_zs = Dep()
    cbT = sb("cbT", [128, 4, 384], F32); d_cbT = [Dep() for _ in range(4)]
    arg = sb("arg", [128, 384], F32); d_arg = Dep()
    Lt = sb("Lt", [128, 384], F32); d_Lt = Dep()
    Mt = [sb("Mt%d" % i, [128, 384], BF16) for i in range(2)]; d_Mt = [Dep() for _ in range(2)]
    stf = sb("stf", [128, 4, 256], F32); d_stf = Dep()
    stT = sb("stT", [128, 4, 256], BF16); d_stT = Dep()
    yt = sb("yt", [128, 256], F32); d_yt = Dep()
    y2 = sb("y2", [128, 256], F32); d_y2 = Dep()
    gss = sb("gss", [128, 1], F32); d_gss = Dep()
    grs = sb("grs", [128, 1], F32); d_grs = Dep()
    osb = sb("osb", [128, 2, 1024], BF16); d_osb = Dep()

    B = [ps("bk%d" % i, [128, 512], F32) for i in range(7)]
    d_B = [Dep() for _ in range(7)]
    pT = ps("pTc", [128, 8, 128], BF16); d_pT = Dep()

    for k in range(8):
        rows = slice(k * 128, (k + 1) * 128)
        P.dma("pool", lambda e, k=k, rows=rows: e.dma_start(out=wz[:, k, :], in_=w_in[rows, C_Z:C_Z + 1024]), writes=[d_w])
        P.dma("pool", lambda e, k=k, rows=rows: e.dma_start(out=wx[:, k, :], in_=w_in[rows, C_X:C_X + 2048]), writes=[d_w])
        P.dma("pool", lambda e, k=k, rows=rows: e.dma_start(out=wdt[:, k, :], in_=w_in[rows, C_DT:C_DT + 16]), writes=[d_w])
    P.dma("sp", lambda e: e.dma_start(out=gm[:], in_=g_mix[0:1, :].partition_broadcast(128)), writes=[d_c])
    P.dma("sp", lambda e: e.dma_start(out=cw[:], in_=cwl_d[:, :, :]), writes=[d_c])
    P.dma("sp", lambda e: e.dma_start(out=cb[:], in_=cbl_d[:, :]), writes=[d_c])
    for i in range(2):
        P.dma("sp", lambda e, i=i: e.dma_start(out=dtb[:, i, :], in_=dtb_d[0:1, :].partition_broadcast(128)), writes=[d_c])
        P.dma("sp", lambda e, i=i: e.dma_start(out=Aneg[:, i, :], in_=alog_d[0:1, :].partition_broadcast(128)), writes=[d_A])
    P.dma("sp", lambda e: e.dma_start(out=dsk[:], in_=dsk_d[0:1, :].partition_broadcast(128)), writes=[d_c])
    P.dma("sp", lambda e: e.dma_start(out=ssdn[:], in_=ssdn_d[0:1, :].partition_broadcast(128)), writes=[d_c])
    P.dma("sp", lambda e: e.dma_start(out=tri2[:], in_=tri2_d[:, :, :]), writes=[d_c])
    P.dma("sp", lambda e: e.dma_start(out=sel[:], in_=sel_d[:, :, :]), writes=[d_c])
    P.dma("sp", lambda e: e.dma_start(out=tb[:], in_=tb_d[:, :]), writes=[d_c])
    P.dma("sp", lambda e: e.dma_start(out=pflag[:], in_=pflag_d[0:1, :].partition_broadcast(128)), writes=[d_c])
    P.op("pool", lambda e: e.memset(onesf[:], 1.0), writes=[d_c])
    P.op("act", lambda e: e.activation(out=Aneg[:], in_=Aneg[:], func=AF.Exp), reads=[d_A], writes=[d_A])
    P.op("dve", lambda e: e.tensor_scalar(out=Aneg[:], in0=Aneg[:], scalar1=-1.0, scalar2=None, op0=ALU.mult), reads=[d_A], writes=[d_A])
    P.op("pool", lambda e: e.memset(xraw[:], 0.0), writes=[d_xraw, d_halo])
    P.op("pool", lambda e: e.memset(stf[:], 0.0), writes=[d_stf])
    P.op("pool", lambda e: e.memset(stT[:], 0.0), writes=[d_stT])

    h3 = lambda ap: ap.rearrange("p (h d) -> p h d", d=64)

    for c in chunks:
        own = c >= 16
        r0 = c * 256
        P.dma("sp", lambda e, r0=r0: e.dma_start(out=xt[:], in_=xin[r0:r0 + 256, :].rearrange("(t p) d -> p t d", p=128)), writes=[d_xt])
        for it in range(2):
            P.op("act", lambda e, it=it: e.activation(out=junk[:], in_=xt[:, it, :], func=AF.Square, accum_out=ssq[:, it:it + 1]),
                 reads=[d_xt], writes=[d_junk, d_ssq])
        P.op("act", lambda e: e.activation(out=rstd[:], in_=ssq[:], func=AF.Sqrt, bias=EPS, scale=1.0 / D), reads=[d_ssq], writes=[d_rstd])
        P.op("dve", lambda e: e.reciprocal(out=rstd[:], in_=rstd[:]), reads=[d_rstd], writes=[d_rstd])
        for it in range(2):
            P.op("dve", lambda e, it=it: e.scalar_tensor_tensor(out=hb[:, it, :], in0=xt[:, it, :], scalar=rstd[:, it:it + 1], in1=gm[:],
                                                                                    op0=ALU.mult, op1=ALU.mult),
                 reads=[d_xt, d_rstd, d_c], writes=[d_hb])
        for it in range(2):
            for k in range(8):
                P.op("pe", lambda e, it=it, k=k: e.transpose(out=pT[:, k, :], in_=hb[:, it, k * 128:(k + 1) * 128], identity=idb[:]),
                     reads=[d_hb, d_idb], writes=[d_pT])
            P.op("act", lambda e, it=it: e.copy(out=hT[:, :, it * 128:(it + 1) * 128], in_=pT[:]), reads=[d_pT], writes=[d_hT])
        for it in range(2):
            for k in range(8):
                P.op("pe", lambda e, it=it, k=k: e.matmul(B[3][:, it * 16:(it + 1) * 16], lhsT=hT[:, k, it * 128:(it + 1) * 128], rhs=wdt[:, k, :],
                                                         start=(k == 0), stop=(k == 7)), reads=[d_hT, d_w], writes=[d_B[3]])
        P.op("dve", lambda e: e.tensor_tensor(out=dtr[:].rearrange("p a b -> p (a b)"), in0=B[3][:, 0:32], in1=dtb[:].rearrange("p a b -> p (a b)"), op=ALU.add),
             reads=[d_B[3], d_c], writes=[d_dtr])
        P.op("act", lambda e: e.activation(out=dtr[:], in_=dtr[:], func=AF.Exp), reads=[d_dtr], writes=[d_dtr])
        P.op("act", lambda e: e.activation(out=dt[:], in_=dtr[:], func=AF.Ln, bias=1.0), reads=[d_dtr], writes=[d_dt])
        P.op("dve", lambda e: e.tensor_tensor(out=aa[:], in0=dt[:], in1=Aneg[:], op=ALU.mult), reads=[d_dt, d_A], writes=[d_aa])
        for it in range(2):
            for jt in range(it + 1):
                P.op("pe", lambda e, it=it, jt=jt: e.matmul(B[3][:, 32 + it * 16:32 + (it + 1) * 16], lhsT=tri2[:, jt, it * 128:(it + 1) * 128],
                                                           rhs=aa[:, jt, :], start=(jt == 0), stop=(jt == it)),
                     reads=[d_aa, d_c], writes=[d_B[3]])
        for jt in range(2):
            P.op("pe", lambda e, jt=jt: e.matmul(B[3][:, 64:80], lhsT=onesf[:], rhs=aa[:, jt, :], start=(jt == 0), stop=(jt == 1)),
                 reads=[d_aa, d_c], writes=[d_B[3]])
        for jt in range(2):
            P.op("pe", lambda e, jt=jt: e.matmul(B[3][0:16, 128:384], lhsT=aa[:, jt, :], rhs=tri2[:, jt, :], start=(jt == 0), stop=(jt == 1)),
                 reads=[d_aa, d_c], writes=[d_B[3]])
        P.op("act", lambda e: e.copy(out=acum[:].rearrange("p a b -> p (a b)"), in_=B[3][:, 32:64]), reads=[d_B[3]], writes=[d_acum])
        P.op("dve", lambda e: e.tensor_scalar(out=nacum[:].rearrange("p a b -> p (a b)"), in0=B[3][:, 32:64], scalar1=-1.0, scalar2=None, op0=ALU.mult),
             reads=[d_B[3]], writes=[d_nacum])
        P.op("act", lambda e: e.copy(out=tot[:], in_=B[3][:, 64:80]), reads=[d_B[3]], writes=[d_tot])
        P.op("act", lambda e: e.copy(out=acumT[:], in_=B[3][0:16, 128:384]), reads=[d_B[3]], writes=[d_acumT])
        P.op("act", lambda e: e.activation(out=cdec[:], in_=tot[:], func=AF.Exp), reads=[d_tot], writes=[d_cdec])
        P.op("dve", lambda e: e.tensor_tensor(out=dte[:], in0=nacum[:], in1=tot[:].unsqueeze(1).to_broadcast([128, 2, 16]), op=ALU.add),
             reads=[d_nacum, d_tot], writes=[d_dte])
        P.op("act", lambda e: e.activation(out=dte[:], in_=dte[:], func=AF.Exp), reads=[d_dte], writes=[d_dte])
        P.op("act", lambda e: e.activation(out=eac[:], in_=acum[:], func=AF.Exp), reads=[d_acum], writes=[d_eac])
        P.op("dve", lambda e: e.tensor_tensor(out=w2[:], in0=dt[:], in1=dte[:], op=ALU.mult), reads=[d_dt, d_dte], writes=[d_w2])
        for fc in range(16):
            hv = fc % 2
            for k in range(8):
                P.op("pe", lambda e, fc=fc, k=k, hv=hv: e.matmul(B[1][:, hv * 256:(hv + 1) * 256], lhsT=wx[:, k, fc * 128:(fc + 1) * 128], rhs=hT[:, k, :],
                                                               start=(k == 0), stop=(k == 7)), reads=[d_hT, d_w], writes=[d_B[1]])
            P.op("act", lambda e, fc=fc, hv=hv: e.copy(out=xraw[:, fc, 3:259], in_=B[1][:, hv * 256:(hv + 1) * 256]),
                 reads=[d_B[1]], writes=[d_xraw])
        for half in range(2):
            for f8 in range(8):
                fc = half * 8 + f8
                eng = "dve"
                P.op(eng, lambda e, fc=fc, f8=f8: e.tensor_scalar(out=acc[:, f8, :], in0=xraw[:, fc, 0:256], scalar1=cw[:, fc, 0:1], scalar2=cb[:, fc:fc + 1],
                                                                 op0=ALU.mult, op1=ALU.add), reads=[d_xraw, d_halo, d_c], writes=[d_acc[f8]])
                for kk in range(1, 4):
                    P.op(eng, lambda e, fc=fc, f8=f8, kk=kk: e.scalar_tensor_tensor(out=acc[:, f8, :], in0=xraw[:, fc, kk:kk + 256], scalar=cw[:, fc, kk:kk + 1],
                                                                                   in1=acc[:, f8, :], op0=ALU.mult, op1=ALU.add),
                         reads=[d_xraw, d_halo, d_c, d_acc[f8]], writes=[d_acc[f8]])
                P.op("act", lambda e, fc=fc, f8=f8: e.activation(out=xc[:, fc, :], in_=acc[:, f8, :], func=AF.Silu),
                     reads=[d_acc[f8]], writes=[d_xc[fc]])
        P.op("pool", lambda e: e.tensor_copy(out=xraw[:, :, 0:3], in_=xraw[:, :, 256:259]), reads=[d_xraw], writes=[d_halo])
        for it in range(2):
            for fc in range(8):
                P.op("pe", lambda e, it=it, fc=fc: e.transpose(out=pT[:, fc, :], in_=xc[:, fc, it * 128:(it + 1) * 128], identity=idb[:]),
                     reads=[d_xc[fc], d_idb], writes=[d_pT])
            P.op("act", lambda e, it=it: e.copy(out=xtok[:, it, :], in_=pT[:]), reads=[d_pT], writes=[d_xtok])
            for g in range(4):
                P.op("pe", lambda e, it=it, g=g: e.transpose(out=pT[:, g, :], in_=xc[:, 8 + g, it * 128:(it + 1) * 128], identity=idb[:]),
                     reads=[d_xc[8 + g], d_idb], writes=[d_pT])
            P.op("act", lambda e, it=it: e.copy(out=btok[:, it, :], in_=pT[:, 0:4, :]), reads=[d_pT], writes=[d_btok])
        for it in range(2):
            if own:
                P.op("dve", lambda e, it=it: e.tensor_tensor(out=h3(xdt[:, it, :]), in0=h3(xtok[:, it, :]),
                                                             in1=dt[:, it, :].unsqueeze(2).to_broadcast([128, 16, 64]), op=ALU.mult),
                     reads=[d_xtok, d_dt], writes=[d_xdt])
            P.op("pool", lambda e, it=it: e.tensor_tensor(out=h3(xdd[:, it, :]), in0=h3(xtok[:, it, :]),
                                                          in1=w2[:, it, :].unsqueeze(2).to_broadcast([128, 16, 64]), op=ALU.mult),
                 reads=[d_xtok, d_w2], writes=[d_xdd])
        if own:
            for it in range(2):
                for hv in range(2):
                    for k in range(8):
                        P.op("pe", lambda e, it=it, hv=hv, k=k: e.matmul(B[2][:, :], lhsT=hT[:, k, it * 128:(it + 1) * 128], rhs=wz[:, k, hv * 512:(hv + 1) * 512],
                                                                       start=(k == 0), stop=(k == 7)), reads=[d_hT, d_w], writes=[d_B[2]])
                    P.op("act", lambda e, it=it, hv=hv: e.activation(out=zs[:, it, hv * 512:(hv + 1) * 512], in_=B[2][:, :], func=AF.Silu),
                         reads=[d_B[2]], writes=[d_zs])
            for g in range(4):
                P.op("pe", lambda e, g=g: e.matmul(B[4][:, 0:256], lhsT=xc[:, 8 + g, 0:128], rhs=xc[:, 12 + g, :], start=True, stop=True),
                     reads=[d_xc[8 + g], d_xc[12 + g]], writes=[d_B[4]])
                P.op("pe", lambda e, g=g: e.matmul(B[4][:, 256:384], lhsT=xc[:, 8 + g, 128:256], rhs=xc[:, 12 + g, 128:256], start=True, stop=True),
                     reads=[d_xc[8 + g], d_xc[12 + g]], writes=[d_B[4]])
                P.op("act", lambda e, g=g: e.copy(out=cbT[:, g, :], in_=B[4][:, 0:384]), reads=[d_B[4]], writes=[d_cbT[g]])
            for g in range(4):
                for r in range(4):
                    h = g * 4 + r
                    mi = h % 2
                    P.op("pe", lambda e, h=h: e.matmul(B[5][:, 0:256], lhsT=sel[:, h, :], rhs=acumT[:, :], start=True, stop=True),
                         reads=[d_c, d_acumT], writes=[d_B[5]])
                    P.op("dve", lambda e, h=h: e.scalar_tensor_tensor(out=arg[:, 0:256], in0=B[5][:, 0:256], scalar=nacum[:, 0, h:h + 1], in1=tb[:, 0:256],
                                                                      op0=ALU.add, op1=ALU.add), reads=[d_B[5], d_nacum, d_c], writes=[d_arg])
                    P.op("dve", lambda e, h=h: e.scalar_tensor_tensor(out=arg[:, 256:384], in0=B[5][:, 128:256], scalar=nacum[:, 1, h:h + 1], in1=tb[:, 256:384],
                                                                      op0=ALU.add, op1=ALU.add), reads=[d_B[5], d_nacum, d_c], writes=[d_arg])
                    P.op("act", lambda e: e.activation(out=Lt[:], in_=arg[:], func=AF.Exp), reads=[d_arg], writes=[d_Lt])
                    P.op("pool", lambda e, g=g, mi=mi: e.tensor_tensor(out=Mt[mi][:], in0=Lt[:], in1=cbT[:, g, :], op=ALU.mult),
                         reads=[d_Lt, d_cbT[g]], writes=[d_Mt[mi]])
                    cs_ = slice(r * 64, (r + 1) * 64)
                    hs_ = slice(h * 64, (h + 1) * 64)
                    P.op("pe", lambda e, mi=mi, cs_=cs_, hs_=hs_: e.matmul(B[6][:, 0:256][:, cs_], lhsT=Mt[mi][:, 0:128], rhs=xdt[:, 0, hs_], start=True, stop=True),
                         reads=[d_Mt[mi], d_xdt], writes=[d_B[6]])
                    P.op("pe", lambda e, mi=mi, cs_=cs_, hs_=hs_: e.matmul(B[6][:, 256:512][:, cs_], lhsT=Mt[mi][:, 128:256], rhs=xdt[:, 0, hs_], start=True, stop=False),
                         reads=[d_Mt[mi], d_xdt], writes=[d_B[6]])
                    P.op("pe", lambda e, mi=mi, cs_=cs_, hs_=hs_: e.matmul(B[6][:, 256:512][:, cs_], lhsT=Mt[mi][:, 256:384], rhs=xdt[:, 1, hs_], start=False, stop=True),
                         reads=[d_Mt[mi], d_xdt], writes=[d_B[6]])
                gs_ = slice(g * 256, (g + 1) * 256)
                for it in range(2):
                    P.op("pe", lambda e, g=g, it=it: e.matmul(B[0][:, it * 256:(it + 1) * 256], lhsT=xc[:, 12 + g, it * 128:(it + 1) * 128], rhs=stT[:, g, :],
                                                             start=True, stop=True), reads=[d_xc[12 + g], d_stT], writes=[d_B[0]])
                for it in range(2):
                    v4 = lambda ap: ap.rearrange("p (r d) -> p r d", d=64)
                    P.op("dve", lambda e, g=g, it=it: e.tensor_tensor(out=v4(yt[:]), in0=v4(B[0][:, it * 256:(it + 1) * 256]),
                                                                      in1=eac[:, it, g * 4:(g + 1) * 4].unsqueeze(2).to_broadcast([128, 4, 64]), op=ALU.mult),
                         reads=[d_B[0], d_eac], writes=[d_yt])
                    P.op("dve", lambda e, it=it: e.tensor_tensor(out=yt[:], in0=yt[:], in1=B[6][:, it * 256:(it + 1) * 256], op=ALU.add),
                         reads=[d_yt, d_B[6]], writes=[d_yt])
                    P.op("pool", lambda e, g=g, it=it, gs_=gs_: e.tensor_tensor(out=v4(y2[:]), in0=v4(xtok[:, it, gs_]),
                                                                               in1=dsk[:, g * 4:(g + 1) * 4].unsqueeze(2).to_broadcast([128, 4, 64]), op=ALU.mult),
                         reads=[d_xtok, d_c], writes=[d_y2])
                    P.op("dve", lambda e: e.tensor_tensor(out=yt[:], in0=yt[:], in1=y2[:], op=ALU.add), reads=[d_yt, d_y2], writes=[d_yt])
                    P.op("dve", lambda e, it=it, gs_=gs_: e.tensor_tensor(out=yt[:], in0=yt[:], in1=zs[:, it, gs_], op=ALU.mult), reads=[d_yt, d_zs], writes=[d_yt])
                    P.op("act", lambda e: e.activation(out=y2[:], in_=yt[:], func=AF.Square, accum_out=gss[:]), reads=[d_yt, d_y2], writes=[d_y2, d_gss])
                    P.op("act", lambda e: e.activation(out=grs[:], in_=gss[:], func=AF.Sqrt, bias=EPS, scale=1.0 / 256), reads=[d_gss], writes=[d_grs])
                    P.op("dve", lambda e: e.reciprocal(out=grs[:], in_=grs[:]), reads=[d_grs], writes=[d_grs])
                    P.op("dve", lambda e, it=it, gs_=gs_: e.scalar_tensor_tensor(out=osb[:, it, gs_], in0=yt[:], scalar=grs[:, 0:1], in1=ssdn[:, gs_],
                                                                                op0=ALU.mult, op1=ALU.mult), reads=[d_yt, d_grs, d_c], writes=[d_osb])
            P.dma("sp", lambda e, r0=r0: e.dma_start(out=os_d[r0 - TP:r0 - TP + 256, :].rearrange("(t p) c -> p t c", p=128), in_=osb[:]),
                  reads=[d_osb], writes=[d_osd])
        for g in range(4):
            bank = B[2] if g < 2 else B[1]
            dbank = d_B[2] if g < 2 else d_B[1]
            cs_ = slice((g % 2) * 256, (g % 2 + 1) * 256)
            for jt in range(2):
                P.op("pe", lambda e, g=g, jt=jt, bank=bank, cs_=cs_: e.matmul(bank[:, cs_], lhsT=btok[:, jt, g * 128:(g + 1) * 128], rhs=xdd[:, jt, g * 256:(g + 1) * 256],
                                                                            start=(jt == 0), stop=(jt == 1)), reads=[d_btok, d_xdd], writes=[dbank])
        v16 = lambda ap: ap.rearrange("p (h d) -> p h d", d=64)
        P.op("dve", lambda e: e.tensor_tensor(out=v16(stf[:].rearrange("p g c -> p (g c)")), in0=v16(stf[:].rearrange("p g c -> p (g c)")),
                                              in1=cdec[:].unsqueeze(2).to_broadcast([128, 16, 64]), op=ALU.mult), reads=[d_stf, d_cdec], writes=[d_stf])
        P.op("dve", lambda e: e.tensor_tensor(out=stf[:, 0:2, :].rearrange("p g c -> p (g c)"), in0=stf[:, 0:2, :].rearrange("p g c -> p (g c)"), in1=B[2][:, :], op=ALU.add),
             reads=[d_stf, d_B[2]], writes=[d_stf])
        P.op("dve", lambda e: e.tensor_tensor(out=stf[:, 2:4, :].rearrange("p g c -> p (g c)"), in0=stf[:, 2:4, :].rearrange("p g c -> p (g c)"), in1=B[1][:, :], op=ALU.add),
             reads=[d_stf, d_B[1]], writes=[d_stf])
        if c == 15:
            P.op("dve", lambda e: e.tensor_scalar(out=stf[:], in0=stf[:], scalar1=pflag[:, 0:1], scalar2=None, op0=ALU.mult), reads=[d_stf, d_c], writes=[d_stf])
        P.op("act", lambda e: e.copy(out=stT[:], in_=stf[:]), reads=[d_stf], writes=[d_stT])


def host_consts_c(z, half):
    cwl = np.ascontiguousarray(z['conv_w'].reshape(4, 16, 128).transpose(2, 1, 0)).astype(np.float32)
    cbl = np.ascontiguousarray(z['conv_b'].reshape(16, 128).T).astype(np.float32)
    tri = (np.arange(128)[:, None] <= np.arange(128)[None, :]).astype(np.float32)
    tri2 = np.zeros((128, 2, 256), np.float32)
    tri2[:, 0, 0:128] = tri; tri2[:, 0, 128:256] = 1.0; tri2[:, 1, 128:256] = tri
    sel = np.zeros((16, 16, 128), np.float32)
    for h in range(16):
        sel[h, h, :] = 1.0
    tbias = np.where(tri > 0, 0.0, NEG).astype(np.float32)
    tb = np.zeros((128, 384), np.float32)
    tb[:, 0:128] = tbias; tb[:, 256:384] = tbias
    return dict(cwl=cwl, cbl=cbl, dtb=z['dt_bias'][None, :], alog=z['a_log'][None, :], dsk=z['d_skip'][None, :], ssdn=z['ssd_norm'][None, :],
                tri2=tri2, sel_d=sel, tb_d=tb, pflag=np.array([[1.0 if half == 1 else 0.0]], np.float32))


def phase_def(nc, P, xown, w_in, g_mix, mem_d, gmem_d, wmemkv, mqn_d, mkn_d, woa_d, wos_d, wom_d, wout_d, gffn_d, wr_d,
              wgate_d, wup_d, wdown_d, tri_d, ecap_d, oa_d, d_oad, os_d, d_osd, x1_d, xbuf, ybuf, out_d, idb, d_idb, idf, d_idf,
              ntiles=32, experts=range(32), stage=3):
    d_x1d = Dep(); d_xbuf = Dep(); d_ybuf = Dep(); d_out = Dep()
    with contextlib.ExitStack() as stp:
        def sbp(name, shape, dt):
            return stp.enter_context(nc.sbuf_tensor("d_" + name, shape, dt))
        combs = sbp("combs", [128, 32, 2], F32); d_combs = Dep()
        idxs = sbp("idxs", [128, 32, 2], I32); d_idxs = Dep()

        with contextlib.ExitStack() as st:
            def sb(name, shape, dt):
                return st.enter_context(nc.sbuf_tensor("d_" + name, shape, dt))

            def ps(name, shape, dt=F32):
                return st.enter_context(nc.psum_tensor(name, shape, dt))
            d_w = Dep(); d_c = Dep()
            gm = sb("gmd", [128, D], F32)
            gf = sb("gfd", [128, D], F32)
            mqn = sb("mqn", [128, 512], F32)
            mkn = sb("mkn", [128, 512], F32)
            tri = sb("trid", [128, 128], F32)
            onesf = sb("onesfd", [128, 128], F32)
            onesb = sb("onesbd", [128, 128], BF16)
            ecap = sb("ecap", [128, 32], F32)
            base = sb("base", [128, 32], F32); d_base = Dep()
            kmT = sb("kmT", [128, 4, 256], BF16); d_kmT = Dep()
            vm = sb("vm", [128, 2, 512], BF16); d_vm = Dep()

            xt = sb("xtd", [128, D], F32); d_xt = Dep()
            junk = sb("junkd", [128, D], BF16); d_junk = Dep()
            ssq = sb("ssqd", [128, 1], F32); d_ssq = Dep()
            rstd = sb("rstdd", [128, 1], F32); d_rstd = Dep()
            hb = sb("hbd", [128, D], BF16); d_hb = Dep()
            hT = sb("hTd", [128, 8, 128], BF16); d_hT = Dep()
            sq = sb("sqd", [128, 512], F32); d_sq = Dep()
            hs = sb("hsd", [128, 4], F32); d_hs = Dep()
            hr = sb("hrd", [128, 4], F32); d_hr = Dep()
            qmn = sb("qmn", [128, 512], F32); d_qmn = Dep()
            qmb = sb("qmb", [128, 512], BF16); d_qmb = Dep()
            qmT = sb("qmT", [128, 4, 128], BF16); d_qmT = Dep()
            pm = sb("pm", [128, 4, 2, 128], BF16); d_pm = Dep()
            rcp = sb("rcp", [128, 512], F32); d_rcp = Dep()
            omT = sb("omT", [128, 4, 128], BF16); d_omT = Dep()
            gs = sb("gs", [128, 3072], F32); d_gs = Dep()
            oat = sb("oat", [128, 512], BF16); d_oat = Dep()
            ost = sb("ost", [128, 1024], BF16); d_ost = Dep()
            oaT = sb("oaT", [128, 4, 128], BF16); d_oaT = Dep()
            osT = sb("osT", [128, 8, 128], BF16); d_osT = Dep()
            mg = sb("mg", [128, 512], F32); d_mg = Dep()
            tt = sb("tt", [128, 512], F32); d_tt = Dep()
            mgb = sb("mgb", [128, D], BF16); d_mgb = Dep()
            mgT = sb("mgT", [128, 8, 128], BF16); d_mgT = Dep()
            x1 = sb("x1", [128, D], F32); d_x1 = Dep()
            h2f = sb("h2f", [128, D], F32); d_h2f = Dep()
            h2b = [sb("h2b%d" % i, [128, D], BF16) for i in range(2)]; d_h2b = [Dep() for _ in range(2)]
            h2T = sb("h2T", [128, 8, 128], F32); d_h2T = Dep()
            lg = sb("lg", [128, 36], F32); d_lg = Dep()
            sm = sb("sm", [128, 64], F32); d_sm = Dep()
            goh = sb("goh", [128, 4], F32); d_goh = Dep()
            lsel = sb("lsel", [128, 32], F32); d_lsel = Dep()
            les = sb("les", [128, 8], F32); d_les = Dep()
            t8 = sb("t8", [128, 8], F32); d_t8 = Dep()
            oh = sb("oh", [128, 64], F32); d_oh = Dep()
            ohs = sb("ohs", [128, 32], F32); d_ohs = Dep()
            posf = sb("posf", [128, 32], F32); d_posf = Dep()
            idf2 = sb("idf2", [128, 2], F32); d_idf2 = Dep()

            pT = ps("pTd", [128, 8, 128], BF16); d_pT = Dep()
            pF = ps("pFd", [128, 512], F32); d_pF = Dep()
            pG = [ps("pGd%d" % i, [128, 512], F32) for i in range(3)]; d_pG = [Dep() for _ in range(3)]
            pms = [ps("pmsd%d" % i, [128, 512], F32) for i in range(2)]; d_pms = [Dep() for _ in range(2)]
            pmo = ps("pmod", [128, 512], F32); d_pmo = Dep()

            P.dma("sp", lambda e: e.dma_start(out=gm[:], in_=g_mix[0:1, :].partition_broadcast(128)), writes=[d_c])
            P.dma("sp", lambda e: e.dma_start(out=gf[:], in_=gffn_d[0:1, :].partition_broadcast(128)), writes=[d_c])
            P.dma("sp", lambda e: e.dma_start(out=mqn[:], in_=mqn_d[0:1, :].partition_broadcast(128)), writes=[d_c])
            P.dma("sp", lambda e: e.dma_start(out=mkn[:], in_=mkn_d[0:1, :].partition_broadcast(128)), writes=[d_c])
            P.dma("sp", lambda e: e.dma_start(out=tri[:], in_=tri_d[:, :]), writes=[d_c])
            P.dma("sp", lambda e: e.dma_start(out=ecap[:], in_=ecap_d[0:1, :].partition_broadcast(128)), writes=[d_c])
            P.op("pool", lambda e: e.memset(onesf[:], 1.0), writes=[d_c])
            P.op("pool", lambda e: e.memset(onesb[:], 1.0), writes=[d_c])
            P.op("pool", lambda e: e.memset(base[:], 0.0), writes=[d_base])

            def head_norm(src_ps, d_src, gain, nh, dst, d_dst):
                hd = 512 // nh
                v = lambda ap: ap.rearrange("p (h d) -> p h d", h=nh)
                P.op("act", lambda e: e.activation(out=sq[:], in_=src_ps, func=AF.Square), reads=[d_src], writes=[d_sq])
                P.op("dve", lambda e: e.tensor_reduce(out=hs[:, 0:nh], in_=v(sq[:]), axis=AX.X, op=ALU.add), reads=[d_sq], writes=[d_hs])
                P.op("act", lambda e: e.activation(out=hr[:, 0:nh], in_=hs[:, 0:nh], func=AF.Sqrt, bias=EPS, scale=1.0 / hd), reads=[d_hs], writes=[d_hr])
                P.op("dve", lambda e: e.reciprocal(out=hr[:, 0:nh], in_=hr[:, 0:nh]), reads=[d_hr], writes=[d_hr])
                P.op("dve", lambda e: e.tensor_tensor(out=v(qmn[:]), in0=v(src_ps), in1=hr[:, 0:nh].unsqueeze(2).to_broadcast([128, nh, hd]), op=ALU.mult),
                     reads=[d_src, d_hr], writes=[d_qmn])
                P.op("pool", lambda e: e.tensor_tensor(out=dst, in0=qmn[:], in1=gain[:], op=ALU.mult), reads=[d_qmn, d_c], writes=[d_dst])

            with contextlib.ExitStack() as stm:
                wkv = stm.enter_context(nc.sbuf_tensor("s_wkv", [128, 8, 1024], BF16)); d_wkv = Dep()
                mt_ = stm.enter_context(nc.sbuf_tensor("s_memt", [128, 2, D], F32)); d_mt = Dep()
                gme = stm.enter_context(nc.sbuf_tensor("s_gme", [128, D], F32)); d_gme = Dep()
                mhb = stm.enter_context(nc.sbuf_tensor("s_mhb", [128, 2, D], BF16)); d_mhb = Dep()
                mT = stm.enter_context(nc.sbuf_tensor("s_mT", [128, 8, 256], BF16)); d_mT = Dep()
                ss2 = stm.enter_context(nc.sbuf_tensor("s_ss2", [128, 2], F32)); d_ss2 = Dep()
                for k in range(8):
                    P.dma("pool", lambda e, k=k: e.dma_start(out=wkv[:, k, :], in_=wmemkv[k * 128:(k + 1) * 128, :]), writes=[d_wkv])
                P.dma("sp", lambda e: e.dma_start(out=mt_[:], in_=mem_d[:, :].rearrange("(t p) d -> p t d", p=128)), writes=[d_mt])
                P.dma("sp", lambda e: e.dma_start(out=gme[:], in_=gmem_d[0:1, :].partition_broadcast(128)), writes=[d_gme])
                for it in range(2):
                    P.op("act", lambda e, it=it: e.activation(out=junk[:], in_=mt_[:, it, :], func=AF.Square, accum_out=ss2[:, it:it + 1]),
                         reads=[d_mt], writes=[d_junk, d_ss2])
                P.op("act", lambda e: e.activation(out=ss2[:], in_=ss2[:], func=AF.Sqrt, bias=EPS, scale=1.0 / D), reads=[d_ss2], writes=[d_ss2])
                P.op("dve", lambda e: e.reciprocal(out=ss2[:], in_=ss2[:]), reads=[d_ss2], writes=[d_ss2])
                for it in range(2):
                    P.op("dve", lambda e, it=it: e.scalar_tensor_tensor(out=mhb[:, it, :], in0=mt_[:, it, :], scalar=ss2[:, it:it + 1], in1=gme[:],
                                                                        op0=ALU.mult, op1=ALU.mult), reads=[d_mt, d_ss2, d_gme], writes=[d_mhb])
                    for k in range(8):
                        P.op("pe", lambda e, it=it, k=k: e.transpose(out=pT[:, k, :], in_=mhb[:, it, k * 128:(k + 1) * 128], identity=idb[:]),
                             reads=[d_mhb, d_idb], writes=[d_pT])
                    P.op("act", lambda e, it=it: e.copy(out=mT[:, :, it * 128:(it + 1) * 128], in_=pT[:]), reads=[d_pT], writes=[d_mT])
                for it in range(2):
                    for hv in range(2):
                        for k in range(8):
                            P.op("pe", lambda e, it=it, hv=hv, k=k: e.matmul(pG[hv][:, :], lhsT=mT[:, k, it * 128:(it + 1) * 128], rhs=wkv[:, k, hv * 512:(hv + 1) * 512],
                                                                           start=(k == 0), stop=(k == 7)), reads=[d_mT, d_wkv], writes=[d_pG[hv]])
                    head_norm(pG[0][:, :], d_pG[0], mkn, 4, qmb[:], d_qmb)
                    for h in range(4):
                        P.op("pe", lambda e, h=h: e.transpose(out=pT[:, h, :], in_=qmb[:, h * 128:(h + 1) * 128], identity=idb[:]),
                             reads=[d_qmb, d_idb], writes=[d_pT])
                    P.op("act", lambda e, it=it: e.copy(out=kmT[:, :, it * 128:(it + 1) * 128], in_=pT[:, 0:4, :]), reads=[d_pT], writes=[d_kmT])
                    P.op("act", lambda e, it=it: e.copy(out=vm[:, it, :], in_=pG[1][:, :]), reads=[d_pG[1]], writes=[d_vm])

            wqm = sb("wqm", [128, 8, 512], BF16)
            wg = sb("wg", [128, 8, 3072], BF16)
            woa = sb("woa", [128, 4, 1024], BF16)
            wos = sb("wos", [128, 8, 1024], BF16)
            wom = sb("wom", [128, 4, 1024], BF16)
            wout = sb("wout", [128, 8, 1024], BF16)
            wr = sb("wr", [128, 8, 36], F32)
            for k in range(8):
                rows = slice(k * 128, (k + 1) * 128)
                P.dma("pool", lambda e, k=k, rows=rows: e.dma_start(out=wqm[:, k, :], in_=w_in[rows, C_QM:C_QM + 512]), writes=[d_w])
                for j in range(2):
                    P.dma("pool", lambda e, k=k, rows=rows, j=j: e.dma_start(out=wg[:, k, j * 1536:(j + 1) * 1536], in_=w_in[rows, C_G + j * 1536:C_G + (j + 1) * 1536]), writes=[d_w])
                P.dma("pool", lambda e, k=k, rows=rows: e.dma_start(out=wos[:, k, :], in_=wos_d[rows, :]), writes=[d_w])
                P.dma("pool", lambda e, k=k, rows=rows: e.dma_start(out=wout[:, k, :], in_=wout_d[rows, :]), writes=[d_w])
                P.dma("sp", lambda e, k=k, rows=rows: e.dma_start(out=wr[:, k, :], in_=wr_d[rows, :]), writes=[d_w])
            for k in range(4):
                rows = slice(k * 128, (k + 1) * 128)
                P.dma("pool", lambda e, k=k, rows=rows: e.dma_start(out=woa[:, k, :], in_=woa_d[rows, :]), writes=[d_w])
                P.dma("pool", lambda e, k=k, rows=rows: e.dma_start(out=wom[:, k, :], in_=wom_d[rows, :]), writes=[d_w])
            for ti in range(ntiles):
                r0 = ti * 128
                hbuf = ti % 2
                P.dma("sp", lambda e, r0=r0: e.dma_start(out=xt[:], in_=xown[r0:r0 + 128, :]), writes=[d_xt])
                P.dma("sp", lambda e, r0=r0: e.dma_start(out=oat[:], in_=oa_d[r0:r0 + 128, :]), reads=[d_oad], writes=[d_oat])
                P.dma("sp", lambda e, r0=r0: e.dma_start(out=ost[:], in_=os_d[r0:r0 + 128, :]), reads=[d_osd], writes=[d_ost])
                P.op("act", lambda e: e.activation(out=junk[:], in_=xt[:], func=AF.Square, accum_out=ssq[:]), reads=[d_xt], writes=[d_junk, d_ssq])
                P.op("act", lambda e: e.activation(out=rstd[:], in_=ssq[:], func=AF.Sqrt, bias=EPS, scale=1.0 / D), reads=[d_ssq], writes=[d_rstd])
                P.op("dve", lambda e: e.reciprocal(out=rstd[:], in_=rstd[:]), reads=[d_rstd], writes=[d_rstd])
                P.op("dve", lambda e: e.scalar_tensor_tensor(out=hb[:], in0=xt[:], scalar=rstd[:, 0:1], in1=gm[:], op0=ALU.mult, op1=ALU.mult),
                     reads=[d_xt, d_rstd, d_c], writes=[d_hb])
                for k in range(8):
                    P.op("pe", lambda e, k=k: e.transpose(out=pT[:, k, :], in_=hb[:, k * 128:(k + 1) * 128], identity=idb[:]), reads=[d_hb, d_idb], writes=[d_pT])
                P.op("act", lambda e: e.copy(out=hT[:], in_=pT[:]), reads=[d_pT], writes=[d_hT])
                for k in range(8):
                    P.op("pe", lambda e, k=k: e.matmul(pG[0][:, :], lhsT=hT[:, k, :], rhs=wqm[:, k, :], start=(k == 0), stop=(k == 7)),
                         reads=[d_hT, d_w], writes=[d_pG[0]])
                head_norm(pG[0][:, :], d_pG[0], mqn, 4, qmb[:], d_qmb)
                for h in range(4):
                    P.op("pe", lambda e, h=h: e.transpose(out=pT[:, h, :], in_=qmb[:, h * 128:(h + 1) * 128], identity=idb[:]), reads=[d_qmb, d_idb], writes=[d_pT])
                P.op("act", lambda e: e.copy(out=qmT[:], in_=pT[:, 0:4, :]), reads=[d_pT], writes=[d_qmT])
                for h in range(4):
                    for mt in range(2):
                        bank = pms[h // 2]
                        c0 = ((h % 2) * 2 + mt) * 128
                        P.op("pe", lambda e, h=h, mt=mt, bank=bank, c0=c0: e.matmul(bank[:, c0:c0 + 128], lhsT=kmT[:, h, mt * 128:(mt + 1) * 128], rhs=qmT[:, h, :],
                                                                                  start=True, stop=True), reads=[d_kmT, d_qmT], writes=[d_pms[h // 2]])
                for hp in range(2):
                    P.op("act", lambda e, hp=hp: e.activation(out=pm[:, hp * 2:(hp + 1) * 2, :, :].rearrange("p a b c -> p (a b c)"), in_=pms[hp][:, :], func=AF.Exp,
                                                              scale=float(128 ** -0.5)), reads=[d_pms[hp]], writes=[d_pm])
                for h in range(4):
                    for mt in range(2):
                        P.op("pe", lambda e, h=h, mt=mt: e.matmul(pmo[:, h * 128:(h + 1) * 128], lhsT=vm[:, mt, h * 128:(h + 1) * 128], rhs=pm[:, h, mt, :],
                                                                 start=(mt == 0), stop=(mt == 1)), reads=[d_vm, d_pm], writes=[d_pmo])
                for mt in range(2):
                    P.op("pe", lambda e, mt=mt: e.matmul(pG[1][:, :].rearrange("p (h t) -> p h t", h=4), lhsT=onesb[:], rhs=pm[:, :, mt, :],
                                                        start=(mt == 0), stop=(mt == 1)), reads=[d_c, d_pm], writes=[d_pG[1]])
                P.op("dve", lambda e: e.reciprocal(out=rcp[:], in_=pG[1][:, :]), reads=[d_pG[1]], writes=[d_rcp])
                P.op("dve", lambda e: e.tensor_tensor(out=omT[:].rearrange("p h t -> p (h t)"), in0=pmo[:, :], in1=rcp[:], op=ALU.mult),
                     reads=[d_pmo, d_rcp], writes=[d_omT])
                for g6 in range(6):
                    bk = g6 % 3
                    for k in range(8):
                        P.op("pe", lambda e, g6=g6, k=k, bk=bk: e.matmul(pG[bk][:, :], lhsT=hT[:, k, :], rhs=wg[:, k, g6 * 512:(g6 + 1) * 512], start=(k == 0), stop=(k == 7)),
                             reads=[d_hT, d_w], writes=[d_pG[bk]])
                    P.op("act", lambda e, g6=g6, bk=bk: e.activation(out=gs[:, g6 * 512:(g6 + 1) * 512], in_=pG[bk][:, :], func=AF.Sigmoid),
                         reads=[d_pG[bk]], writes=[d_gs])
                for k in range(4):
                    P.op("pe", lambda e, k=k: e.transpose(out=pT[:, k, :], in_=oat[:, k * 128:(k + 1) * 128], identity=idb[:]), reads=[d_oat, d_idb], writes=[d_pT])
                P.op("act", lambda e: e.copy(out=oaT[:], in_=pT[:, 0:4, :]), reads=[d_pT], writes=[d_oaT])
                for k in range(8):
                    P.op("pe", lambda e, k=k: e.transpose(out=pT[:, k, :], in_=ost[:, k * 128:(k + 1) * 128], identity=idb[:]), reads=[d_ost, d_idb], writes=[d_pT])
                P.op("act", lambda e: e.copy(out=osT[:], in_=pT[:]), reads=[d_pT], writes=[d_osT])
                for hv in range(2):
                    cs_ = slice(hv * 512, (hv + 1) * 512)
                    for k in range(4):
                        P.op("pe", lambda e, k=k, cs_=cs_: e.matmul(pG[0][:, :], lhsT=oaT[:, k, :], rhs=woa[:, k, cs_], start=(k == 0), stop=(k == 3)),
                             reads=[d_oaT, d_w], writes=[d_pG[0]])
                    for k in range(8):
                        P.op("pe", lambda e, k=k, cs_=cs_: e.matmul(pG[1][:, :], lhsT=osT[:, k, :], rhs=wos[:, k, cs_], start=(k == 0), stop=(k == 7)),
                             reads=[d_osT, d_w], writes=[d_pG[1]])
                    for k in range(4):
                        P.op("pe", lambda e, k=k, cs_=cs_: e.matmul(pG[2][:, :], lhsT=omT[:, k, :], rhs=wom[:, k, cs_], start=(k == 0), stop=(k == 3)),
                             reads=[d_omT, d_w], writes=[d_pG[2]])
                    P.op("dve", lambda e, hv=hv: e.tensor_tensor(out=mg[:], in0=pG[0][:, :], in1=gs[:, hv * 512:(hv + 1) * 512], op=ALU.mult),
                         reads=[d_pG[0], d_gs], writes=[d_mg])
                    P.op("dve", lambda e, hv=hv: e.tensor_tensor(out=tt[:], in0=pG[1][:, :], in1=gs[:, 1024 + hv * 512:1024 + (hv + 1) * 512], op=ALU.mult),
                         reads=[d_pG[1], d_gs], writes=[d_tt])
                    P.op("pool", lambda e: e.tensor_tensor(out=mg[:], in0=mg[:], in1=tt[:], op=ALU.add), reads=[d_mg, d_tt], writes=[d_mg])
                    P.op("dve", lambda e, hv=hv: e.tensor_tensor(out=tt[:], in0=pG[2][:, :], in1=gs[:, 2048 + hv * 512:2048 + (hv + 1) * 512], op=ALU.mult),
                         reads=[d_pG[2], d_gs], writes=[d_tt])
                    P.op("pool", lambda e, cs_=cs_: e.tensor_tensor(out=mgb[:, cs_], in0=mg[:], in1=tt[:], op=ALU.add), reads=[d_mg, d_tt], writes=[d_mgb])
                for k in range(8):
                    P.op("pe", lambda e, k=k: e.transpose(out=pT[:, k, :], in_=mgb[:, k * 128:(k + 1) * 128], identity=idb[:]), reads=[d_mgb, d_idb], writes=[d_pT])
                P.op("act", lambda e: e.copy(out=mgT[:], in_=pT[:]), reads=[d_pT], writes=[d_mgT])
                for hv in range(2):
                    cs_ = slice(hv * 512, (hv + 1) * 512)
                    for k in range(8):
                        P.op("pe", lambda e, k=k, cs_=cs_, hv=hv: e.matmul(pG[hv][:, :], lhsT=mgT[:, k, :], rhs=wout[:, k, cs_], start=(k == 0), stop=(k == 7)),
                             reads=[d_mgT, d_w], writes=[d_pG[hv]])
                    P.op("dve", lambda e, cs_=cs_, hv=hv: e.tensor_tensor(out=x1[:, cs_], in0=pG[hv][:, :], in1=xt[:, cs_], op=ALU.add),
                         reads=[d_pG[hv], d_xt], writes=[d_x1])
                P.dma("sp", lambda e, r0=r0: e.dma_start(out=x1_d[r0:r0 + 128, :], in_=x1[:]), reads=[d_x1], writes=[d_x1d])
                if stage < 2:
                    continue
                P.op("act", lambda e: e.activation(out=junk[:], in_=x1[:], func=AF.Square, accum_out=ssq[:]), reads=[d_x1], writes=[d_junk, d_ssq])
                P.op("act", lambda e: e.activation(out=rstd[:], in_=ssq[:], func=AF.Sqrt, bias=EPS, scale=1.0 / D), reads=[d_ssq], writes=[d_rstd])
                P.op("dve", lambda e: e.reciprocal(out=rstd[:], in_=rstd[:]), reads=[d_rstd], writes=[d_rstd])
                P.op("dve", lambda e: e.scalar_tensor_tensor(out=h2f[:], in0=x1[:], scalar=rstd[:, 0:1], in1=gf[:], op0=ALU.mult, op1=ALU.mult),
                     reads=[d_x1, d_rstd, d_c], writes=[d_h2f])
                P.op("pool", lambda e, hbuf=hbuf: e.tensor_copy(out=h2b[hbuf][:], in_=h2f[:]), reads=[d_h2f], writes=[d_h2b[hbuf]])
                for half in range(2):
                    for k4 in range(4):
                        k = half * 4 + k4
                        P.op("pe", lambda e, k=k, k4=k4: e.transpose(out=pF[:, k4 * 128:(k4 + 1) * 128], in_=h2f[:, k * 128:(k + 1) * 128], identity=idf[:]),
                             reads=[d_h2f, d_idf], writes=[d_pF])
                    P.op("act", lambda e, half=half: e.copy(out=h2T[:, half * 4:(half + 1) * 4, :].rearrange("p a b -> p (a b)"), in_=pF[:, :]), reads=[d_pF], writes=[d_h2T])
                for k in range(8):
                    P.op("pe", lambda e, k=k: e.matmul(pF[:, 0:36], lhsT=h2T[:, k, :], rhs=wr[:, k, :], start=(k == 0), stop=(k == 7)), reads=[d_h2T, d_w], writes=[d_pF])
                P.op("act", lambda e: e.copy(out=lg[:], in_=pF[:, 0:36]), reads=[d_pF], writes=[d_lg])
                P.op("dve", lambda e: e.tensor_reduce(out=sm[:, 0:1], in_=lg[:, 0:4], axis=AX.X, op=ALU.max), reads=[d_lg], writes=[d_sm])
                P.op("dve", lambda e: e.tensor_scalar(out=sm[:, 1:2], in0=sm[:, 0:1], scalar1=-1.0, scalar2=None, op0=ALU.mult), reads=[d_sm], writes=[d_sm])
                P.op("act", lambda e: e.activation(out=sm[:, 8:12], in_=lg[:, 0:4], func=AF.Exp, bias=sm[:, 1:2], accum_out=sm[:, 2:3]), reads=[d_lg, d_sm], writes=[d_sm])
                P.op("dve", lambda e: e.reciprocal(out=sm[:, 3:4], in_=sm[:, 2:3]), reads=[d_sm], writes=[d_sm])
                P.op("dve", lambda e: e.tensor_scalar(out=goh[:], in0=lg[:, 0:4], scalar1=sm[:, 0:1], scalar2=None, op0=ALU.is_ge), reads=[d_lg, d_sm], writes=[d_goh])
                P.op("dve", lambda e: e.tensor_tensor(out=lsel[:].rearrange("p (g e) -> p g e", g=4), in0=lg[:, 4:36].rearrange("p (g e) -> p g e", g=4),
                                                      in1=goh[:].unsqueeze(2).to_broadcast([128, 4, 8]), op=ALU.mult), reads=[d_lg, d_goh], writes=[d_lsel])
                P.op("dve", lambda e: e.tensor_reduce(out=les[:], in_=lsel[:].rearrange("p (g e) -> p e g", g=4), axis=AX.X, op=ALU.add), reads=[d_lsel], writes=[d_les])
                P.op("dve", lambda e: e.max(out=t8[:], in_=les[:]), reads=[d_les], writes=[d_t8])
                P.op("dve", lambda e: e.tensor_tensor(out=sm[:, 4:5], in0=t8[:, 1:2], in1=t8[:, 0:1], op=ALU.subtract), reads=[d_t8, d_sm], writes=[d_sm])
                P.op("act", lambda e: e.activation(out=sm[:, 5:6], in_=sm[:, 4:5], func=AF.Exp), reads=[d_sm], writes=[d_sm])
                P.op("dve", lambda e: e.tensor_scalar(out=sm[:, 6:7], in0=sm[:, 5:6], scalar1=1.0, scalar2=None, op0=ALU.add), reads=[d_sm], writes=[d_sm])
                P.op("dve", lambda e: e.reciprocal(out=sm[:, 6:7], in_=sm[:, 6:7]), reads=[d_sm], writes=[d_sm])
                P.op("dve", lambda e: e.tensor_tensor(out=sm[:, 7:8], in0=sm[:, 5:6], in1=sm[:, 6:7], op=ALU.mult), reads=[d_sm], writes=[d_sm])
                P.op("dve", lambda e, ti=ti: e.tensor_scalar(out=combs[:, ti, :], in0=sm[:, 6:8], scalar1=sm[:, 3:4], scalar2=None, op0=ALU.mult), reads=[d_sm], writes=[d_combs])
                for j in range(2):
                    P.op("dve", lambda e, j=j: e.tensor_scalar(out=oh[:, j * 32:(j + 1) * 32], in0=lg[:, 4:36], scalar1=t8[:, j:j + 1], scalar2=None, op0=ALU.is_equal),
                         reads=[d_lg, d_t8], writes=[d_oh])
                    P.op("dve", lambda e, j=j: e.tensor_tensor(out=oh[:, j * 32:(j + 1) * 32].rearrange("p (g e) -> p g e", g=4), in0=oh[:, j * 32:(j + 1) * 32].rearrange("p (g e) -> p g e", g=4),
                                                               in1=goh[:].unsqueeze(2).to_broadcast([128, 4, 8]), op=ALU.mult), reads=[d_oh, d_goh], writes=[d_oh])
                P.op("dve", lambda e: e.tensor_tensor(out=ohs[:], in0=oh[:, 0:32], in1=oh[:, 32:64], op=ALU.add), reads=[d_oh], writes=[d_ohs])
                P.op("pe", lambda e: e.matmul(pF[:, 64:96], lhsT=tri[:], rhs=ohs[:], start=True, stop=True), reads=[d_c, d_ohs], writes=[d_pF])
                P.op("pe", lambda e: e.matmul(pF[:, 128:160], lhsT=onesf[:], rhs=ohs[:], start=True, stop=True), reads=[d_c, d_ohs], writes=[d_pF])
                P.op("dve", lambda e: e.tensor_tensor(out=posf[:], in0=pF[:, 64:96], in1=ohs[:], op=ALU.subtract), reads=[d_pF, d_ohs], writes=[d_posf])
                P.op("dve", lambda e: e.tensor_tensor(out=posf[:], in0=posf[:], in1=base[:], op=ALU.add), reads=[d_posf, d_base], writes=[d_posf])
                P.op("dve", lambda e: e.tensor_tensor(out=posf[:], in0=posf[:], in1=ecap[:], op=ALU.add), reads=[d_posf, d_c], writes=[d_posf])
                P.op("dve", lambda e: e.tensor_tensor(out=base[:], in0=base[:], in1=pF[:, 128:160], op=ALU.add), reads=[d_base, d_pF, d_posf], writes=[d_base])
                for j in range(2):
                    P.op("dve", lambda e, j=j: e.tensor_tensor(out=oh[:, j * 32:(j + 1) * 32], in0=oh[:, j * 32:(j + 1) * 32], in1=posf[:], op=ALU.mult), reads=[d_oh, d_posf], writes=[d_oh])
                    P.op("dve", lambda e, j=j: e.tensor_reduce(out=idf2[:, j:j + 1], in_=oh[:, j * 32:(j + 1) * 32], axis=AX.X, op=ALU.add), reads=[d_oh], writes=[d_idf2])
                P.op("dve", lambda e, ti=ti: e.tensor_copy(out=idxs[:, ti, :], in_=idf2[:]), reads=[d_idf2], writes=[d_idxs])
                for j in range(2):
                    P.dma("pool", lambda e, ti=ti, j=j, hbuf=hbuf: e.indirect_dma_start(out=xbuf[:, :], out_offset=bass.IndirectOffsetOnAxis(ap=idxs[:, ti, j:j + 1], axis=0),
                                                                                      in_=h2b[hbuf][:], in_offset=None),
                          reads=[d_h2b[hbuf], d_idxs], writes=[d_xbuf])

        if stage < 3:
            return
        if hasattr(P, 'barrier'):
            P.barrier()
        with contextlib.ExitStack() as st:
            def sb(name, shape, dt):
                return st.enter_context(nc.sbuf_tensor("d_" + name, shape, dt))

            def ps(name, shape, dt=F32):
                return st.enter_context(nc.psum_tensor(name, shape, dt))
            NW = 2
            wge = [sb("wge%d" % i, [128, 8, 512], BF16) for i in range(NW)]; d_wge = [Dep() for _ in range(NW)]
            wue = [sb("wue%d" % i, [128, 8, 512], BF16) for i in range(NW)]; d_wue = [Dep() for _ in range(NW)]
            wde = [sb("wde%d" % i, [128, 4, 1024], BF16) for i in range(NW)]; d_wde = [Dep() for _ in range(NW)]
            xe = [sb("xe%d" % i, [128, 3, D], BF16) for i in range(NW)]; d_xe = [Dep() for _ in range(NW)]
            xeT = sb("xeT", [128, 8, CAP], BF16); d_xeT = Dep()
            sg = sb("sg", [128, CAP], F32); d_sg = Dep()
            hTe = sb("hTe", [128, 4, CAP], BF16); d_hTe = Dep()
            ye = [sb("ye%d" % i, [128, 3, D], F32) for i in range(NW)]; d_ye = [Dep() for _ in range(NW)]
            pT = ps("pTe", [128, 8, 128], BF16); d_pT = Dep()
            pg_ = [ps("pge%d" % i, [128, 512], F32) for i in range(2)]; d_pg = [Dep() for _ in range(2)]
            pu_ = [ps("pue%d" % i, [128, 512], F32) for i in range(2)]; d_pu = [Dep() for _ in range(2)]
            py_ = [ps("pye%d" % i, [128, 512], F32) for i in range(2)]; d_py = [Dep() for _ in range(2)]
            for ie, ex in enumerate(experts):
                b = ie % NW
                P.dma("pool", lambda e, b=b, ex=ex: e.dma_start(out=wge[b][:], in_=wgate_d[ex].rearrange("(k p) f -> p k f", p=128)), writes=[d_wge[b]])
                P.dma("pool", lambda e, b=b, ex=ex: e.dma_start(out=wue[b][:], in_=wup_d[ex].rearrange("(k p) f -> p k f", p=128)), writes=[d_wue[b]])
                P.dma("pool", lambda e, b=b, ex=ex: e.dma_start(out=wde[b][:], in_=wdown_d[ex].rearrange("(k p) f -> p k f", p=128)), writes=[d_wde[b]])
                P.dma("sp", lambda e, b=b, ex=ex: e.dma_start(out=xe[b][:], in_=xbuf[ex * CAP:(ex + 1) * CAP, :].rearrange("(s p) d -> p s d", p=128)),
                      reads=[d_xbuf], writes=[d_xe[b]])
                for s in range(3):
                    for k in range(8):
                        P.op("pe", lambda e, b=b, s=s, k=k: e.transpose(out=pT[:, k, :], in_=xe[b][:, s, k * 128:(k + 1) * 128], identity=idb[:]),
                             reads=[d_xe[b], d_idb], writes=[d_pT])
                    P.op("act", lambda e, s=s: e.copy(out=xeT[:, :, s * 128:(s + 1) * 128], in_=pT[:]), reads=[d_pT], writes=[d_xeT])
                for ft in range(4):
                    pb = ft % 2
                    for k in range(8):
                        P.op("pe", lambda e, b=b, ft=ft, k=k, pb=pb: e.matmul(pg_[pb][:, 0:CAP], lhsT=wge[b][:, k, ft * 128:(ft + 1) * 128], rhs=xeT[:, k, :],
                                                                            start=(k == 0), stop=(k == 7)), reads=[d_wge[b], d_xeT], writes=[d_pg[pb]])
                    for k in range(8):
                        P.op("pe", lambda e, b=b, ft=ft, k=k, pb=pb: e.matmul(pu_[pb][:, 0:CAP], lhsT=wue[b][:, k, ft * 128:(ft + 1) * 128], rhs=xeT[:, k, :],
                                                                            start=(k == 0), stop=(k == 7)), reads=[d_wue[b], d_xeT], writes=[d_pu[pb]])
                    P.op("act", lambda e, pb=pb: e.activation(out=sg[:], in_=pg_[pb][:, 0:CAP], func=AF.Silu), reads=[d_pg[pb]], writes=[d_sg])
                    P.op("dve", lambda e, ft=ft, pb=pb: e.tensor_tensor(out=hTe[:, ft, :], in0=sg[:], in1=pu_[pb][:, 0:CAP], op=ALU.mult),
                         reads=[d_sg, d_pu[pb]], writes=[d_hTe])
                for s in range(3):
                    for hv in range(2):
                        for ft in range(4):
                            P.op("pe", lambda e, b=b, s=s, hv=hv, ft=ft: e.matmul(py_[hv][:, :], lhsT=hTe[:, ft, s * 128:(s + 1) * 128], rhs=wde[b][:, ft, hv * 512:(hv + 1) * 512],
                                                                                start=(ft == 0), stop=(ft == 3)), reads=[d_hTe, d_wde[b]], writes=[d_py[hv]])
                        P.op("act" if hv == 0 else "dve", (lambda e, b=b, s=s, hv=hv: e.copy(out=ye[b][:, s, hv * 512:(hv + 1) * 512], in_=py_[hv][:, :])) if hv == 0 else
                             (lambda e, b=b, s=s, hv=hv: e.tensor_copy(out=ye[b][:, s, hv * 512:(hv + 1) * 512], in_=py_[hv][:, :])),
                             reads=[d_py[hv]], writes=[d_ye[b]])
                P.dma("sp", lambda e, b=b, ex=ex: e.dma_start(out=ybuf[ex * CAP:(ex + 1) * CAP, :].rearrange("(s p) d -> p s d", p=128), in_=ye[b][:]),
                      reads=[d_ye[b]], writes=[d_ybuf])

        if hasattr(P, 'barrier'):
            P.barrier()
        with contextlib.ExitStack() as st:
            def sb(name, shape, dt):
                return st.enter_context(nc.sbuf_tensor("d_" + name, shape, dt))
            NF = 2
            x1t = [sb("x1t%d" % i, [128, D], F32) for i in range(NF)]; d_x1t = [Dep() for _ in range(NF)]
            y1 = [sb("y1t%d" % i, [128, D], F32) for i in range(NF)]; d_y1 = [Dep() for _ in range(NF)]
            y2 = [sb("y2t%d" % i, [128, D], F32) for i in range(NF)]; d_y2 = [Dep() for _ in range(NF)]
            ot = [sb("ot%d" % i, [128, D], F32) for i in range(NF)]; d_ot = [Dep() for _ in range(NF)]
            for ti in range(ntiles):
                b = ti % NF
                r0 = ti * 128
                P.dma("sp", lambda e, b=b, r0=r0: e.dma_start(out=x1t[b][:], in_=x1_d[r0:r0 + 128, :]), reads=[d_x1d], writes=[d_x1t[b]])
                P.dma("pool", lambda e, b=b, ti=ti: e.indirect_dma_start(out=y1[b][:], out_offset=None, in_=ybuf[:, :],
                                                                        in_offset=bass.IndirectOffsetOnAxis(ap=idxs[:, ti, 0:1], axis=0)),
                      reads=[d_ybuf, d_idxs], writes=[d_y1[b]])
                P.dma("pool", lambda e, b=b, ti=ti: e.indirect_dma_start(out=y2[b][:], out_offset=None, in_=ybuf[:, :],
                                                                        in_offset=bass.IndirectOffsetOnAxis(ap=idxs[:, ti, 1:2], axis=0)),
                      reads=[d_ybuf, d_idxs], writes=[d_y2[b]])
                P.op("dve", lambda e, b=b, ti=ti: e.scalar_tensor_tensor(out=ot[b][:], in0=y1[b][:], scalar=combs[:, ti, 0:1], in1=x1t[b][:], op0=ALU.mult, op1=ALU.add),
                     reads=[d_y1[b], d_combs, d_x1t[b]], writes=[d_ot[b]])
                P.op("dve", lambda e, b=b, ti=ti: e.scalar_tensor_tensor(out=ot[b][:], in0=y2[b][:], scalar=combs[:, ti, 1:2], in1=ot[b][:], op0=ALU.mult, op1=ALU.add),
                     reads=[d_y2[b], d_combs, d_ot[b]], writes=[d_ot[b]])
                P.dma("sp", lambda e, b=b, r0=r0: e.dma_start(out=out_d[r0:r0 + 128, :], in_=ot[b][:]), reads=[d_ot[b]], writes=[d_out])


def host_consts_d(z):
    tri = (np.arange(128)[:, None] <= np.arange(128)[None, :]).astype(np.float32)
    return dict(mqn=np.tile(z['mem_q_norm'], 4)[None, :], mkn=np.tile(z['mem_k_norm'], 4)[None, :],
                w_r=np.ascontiguousarray(np.concatenate([z['w_router_group'], z['w_router_expert']], axis=1)),
                trif=tri, ecap=(np.arange(32, dtype=np.float32) * CAP)[None, :], ident=np.eye(128, dtype=np.float32))


def build_full():
    nc = bass.Bass("TRN2", target_bir_lowering=False)
    P = Prog(nc)

    def di(name, shape, dt=F32):
        return nc.dram_tensor(name, shape, dt, kind="ExternalInput")
    xin = di("xin", [TP + T, D]); w_in = di("w_in", [D, IN_COLS]); g_mix = di("g_mix", [1, D])
    qkn = di("qkn", [2, 512]); cs = di("cs", [TP + T, 64]); ident_d = di("ident", [128, 128])
    e_d = di("e_d", [32, NK], BF16); vq_d = di("vq", [1, 1024]); nb_d = di("nb", [1, 1024]); oq_d = di("oq", [1, 1024])
    tri_d = di("tri", [128, 128], BF16)
    cwl_d = di("cwl", [128, 16, 4]); cbl_d = di("cbl", [128, 16]); dtb_d = di("dtb", [1, 16]); alog_d = di("alog", [1, 16])
    dsk_d = di("dsk", [1, 16]); ssdn_d = di("ssdn", [1, D]); tri2_d = di("tri2", [128, 2, 256]); sel_d = di("sel_d", [16, 16, 128])
    tb_d = di("tb_d", [128, 384]); pflag_d = di("pflag", [1, 1])
    mem_d = di("mem", [256, D]); gmem_d = di("g_mem", [1, D]); wmemkv = di("w_mem_kv", [D, 1024])
    mqn_d = di("mqn", [1, 512]); mkn_d = di("mkn", [1, 512])
    woa_d = di("w_o_moba", [512, D]); wos_d = di("w_o_ssd", [D, D]); wom_d = di("w_o_mem", [512, D]); wout_d = di("w_out", [D, D])
    gffn_d = di("g_ffn", [1, D]); wr_d = di("w_r", [D, 36])
    wgate_d = di("w_gate", [NE, D, 512]); wup_d = di("w_up", [NE, D, 512]); wdown_d = di("w_down", [NE, 512, D])
    trif_d = di("trif", [128, 128]); ecap_d = di("ecap", [1, 32])
    out_d = nc.dram_tensor("out", [T, D], F32, kind="ExternalOutput")
    kT_d = nc.dram_tensor("kT_d", [512, NK], BF16)
    v_d = nc.dram_tensor("v_d", [NK, 512], BF16)
    qT_d = nc.dram_tensor("qT_d", [512, T], BF16)
    oa_d = nc.dram_tensor("oa_d", [T, 512], BF16)
    os_d = nc.dram_tensor("os_d", [T, D], BF16)
    x1_d = nc.dram_tensor("x1_d", [T, D], F32)
    xbuf = nc.dram_tensor("xbuf", [NE * CAP, D], BF16)
    ybuf = nc.dram_tensor("ybuf", [NE * CAP, D], F32)

    with contextlib.ExitStack() as st0:
        idf = st0.enter_context(nc.sbuf_tensor("idf", [128, 128], F32)); d_idf = Dep()
        idb = st0.enter_context(nc.sbuf_tensor("idb", [128, 128], BF16)); d_idb = Dep()
        P.dma("sp", lambda e: e.dma_start(out=idf[:], in_=ident_d[:, :]), writes=[d_idf])
        P.op("dve", lambda e: e.tensor_copy(out=idb[:], in_=idf[:]), reads=[d_idf], writes=[d_idb])
        with contextlib.ExitStack() as st:
            phase_a(nc, P, st, xin, w_in, g_mix, qkn, cs, idb, d_idb, kT_d, v_d, qT_d)
        P.barrier()
        with contextlib.ExitStack() as st:
            oa = st.enter_context(nc.sbuf_tensor("s_oa", [128, 32, 512], BF16)); d_oa = Dep()
            phase_b(nc, P, st, kT_d, v_d, qT_d, e_d, vq_d, nb_d, oq_d, tri_d, idb, d_idb, oa, d_oa)
            P.dma("sp", lambda e: e.dma_start(out=oa_d[:, :].rearrange("(t p) c -> p t c", p=128), in_=oa[:]), reads=[d_oa])
        P.barrier()
        with contextlib.ExitStack() as st:
            phase_c(nc, P, st, xin, w_in, g_mix, cwl_d, cbl_d, dtb_d, alog_d, dsk_d, ssdn_d, tri2_d, sel_d, tb_d, pflag_d,
                    idb, d_idb, os_d, Dep())
        P.barrier()
        phase_def(nc, P, xin[TP:TP + T, :], w_in, g_mix, mem_d, gmem_d, wmemkv, mqn_d, mkn_d, woa_d, wos_d, wom_d, wout_d, gffn_d, wr_d,
                  wgate_d, wup_d, wdown_d, trif_d, ecap_d, oa_d, Dep(), os_d, Dep(), x1_d, xbuf, ybuf, out_d, idb, d_idb, idf, d_idf)
        P.emit()
    return nc


_NC_CACHE = {}


def kernel(x, mem, g_mix, w_in, moba_q_norm, moba_k_norm, conv_w, conv_b, dt_bias, a_log, d_skip, ssd_norm, g_mem, w_mem_kv,
           mem_q_norm, mem_k_norm, w_o_moba, w_o_ssd, w_o_mem, w_out, g_ffn, w_router_group, w_router_expert, w_gate, w_up, w_down):
    f32 = np.float32
    A = lambda a: np.ascontiguousarray(np.asarray(a, dtype=f32))
    x = A(x); mem = A(mem)
    z = dict(conv_w=A(conv_w), conv_b=A(conv_b), dt_bias=A(dt_bias), a_log=A(a_log), d_skip=A(d_skip), ssd_norm=A(ssd_norm),
             mem_q_norm=A(mem_q_norm), mem_k_norm=A(mem_k_norm), w_router_group=A(w_router_group), w_router_expert=A(w_router_expert))
    if "nc" not in _NC_CACHE:
        _NC_CACHE["nc"] = build_full()
    nc = _NC_CACHE["nc"]
    pos = np.arange(2 * T, dtype=f32)
    inv = (10000.0 ** (-np.arange(32, dtype=f32) / 32)).astype(f32)
    ang = pos[:, None] * inv[None, :]
    cs_full = np.concatenate([np.cos(ang), np.sin(ang)], axis=1).astype(f32)
    qkn = np.stack([np.tile(A(moba_q_norm), 8), np.tile(A(moba_k_norm), 8)]).astype(f32)
    shared = dict(w_in=A(w_in), g_mix=A(g_mix)[None, :], qkn=qkn, ident=np.eye(128, dtype=f32),
                  g_mem=A(g_mem)[None, :], w_mem_kv=A(w_mem_kv), w_o_moba=A(w_o_moba), w_o_ssd=A(w_o_ssd), w_o_mem=A(w_o_mem),
                  w_out=A(w_out), g_ffn=A(g_ffn)[None, :], w_gate=A(w_gate), w_up=A(w_up), w_down=A(w_down))
    shared.update(host_consts_d(z))
    in_maps = []
    for c in range(8):
        bb, hh = c // 2, c % 2
        if hh == 0:
            xin = np.concatenate([np.zeros((TP, D), f32), x[bb, :T]], 0)
            csc = np.concatenate([cs_full[:TP], cs_full[:T]], 0)
        else:
            xin = x[bb]
            csc = cs_full
        m = dict(shared)
        m.update(xin=np.ascontiguousarray(xin), cs=np.ascontiguousarray(csc), mem=mem[bb])
        m.update(host_consts(hh))
        m.update(host_consts_c(z, hh))
        in_maps.append(m)
    res = run_bass_kernel_spmd(nc, in_maps, core_ids=list(range(8)))
    out = np.empty((4, 2 * T, D), f32)
    for c in range(8):
        bb, hh = c // 2, c % 2
        out[bb, hh * T:(hh + 1) * T] = np.asarray(res.results[c]["out"], dtype=f32)
    return out
```

```python
import contextlib
import numpy as np
import ml_dtypes
import concourse.bass as bass
import concourse.mybir as mybir
from concourse.bass_utils import run_bass_kernel_spmd

F32 = mybir.dt.float32
BF16 = mybir.dt.bfloat16
I32 = mybir.dt.int32
U32 = mybir.dt.uint32
AF = mybir.ActivationFunctionType
ALU = mybir.AluOpType
AX = mybir.AxisListType

T = 4096
TP = 4096
NK = TP + T
D = 1024
EPS = 1e-6
IN_COLS = 8208
BIGG = 30000.0
MB = 1000.0
NEG = -30000.0
C_Z, C_X, C_DT = 1536, 2560, 4608
C_QM, C_G = 4624, 5136
CAP = 384
NE = 32


class Dep:
    __slots__ = ("w", "r")

    def __init__(self):
        self.w = None
        self.r = {}


class Op:
    __slots__ = ("eng", "fn", "deps", "is_dma", "sig", "sigval", "dsem", "dval", "prev")

    def __init__(self, eng, fn, is_dma):
        self.eng = eng
        self.fn = fn
        self.is_dma = is_dma
        self.deps = []
        self.sig = False
        self.sigval = 0
        self.dsem = None
        self.dval = 0
        self.prev = None


class Prog:
    ENGS = ("pe", "act", "dve", "pool", "sp")

    def __init__(self, nc, ndma_sems=12):
        self.nc = nc
        self.ops = {e: [] for e in self.ENGS}
        self.ndma = {e: 0 for e in self.ENGS}
        self.dma_last = {}
        self.ndma_sems = ndma_sems
        self.all_dma = []

    def _add(self, o, reads, writes):
        deps = {}
        for t in reads:
            if t.w is not None:
                deps[id(t.w)] = t.w
        for t in writes:
            if t.w is not None:
                deps[id(t.w)] = t.w
            for r in t.r.values():
                deps[id(r)] = r
        for t in reads:
            key = id(o) if o.is_dma else o.eng
            t.r[key] = o
        for t in writes:
            t.w = o
            t.r = {}
        dl = []
        for d in deps.values():
            if d is o:
                continue
            if (not d.is_dma) and (not o.is_dma) and d.eng == "pe" and o.eng == "pe":
                continue
            dl.append(d)
            if not d.is_dma:
                d.sig = True
        o.deps = dl
        self.ops[o.eng].append(o)
        return o

    def op(self, eng, fn, reads=(), writes=()):
        return self._add(Op(eng, fn, False), reads, writes)

    def dma(self, eng, fn, reads=(), writes=()):
        o = Op(eng, fn, True)
        n = self.ndma[eng]
        self.ndma[eng] += 1
        slot = (eng, n % self.ndma_sems)
        o.dsem = slot
        o.prev = self.dma_last.get(slot)
        o.dval = (o.prev.dval if o.prev else 0) + 16
        self.dma_last[slot] = o
        self.all_dma.append(o)
        return self._add(o, reads, writes)


    def barrier(self):
        lasts = []
        for e in self.ENGS:
            for o in reversed(self.ops[e]):
                if (not o.is_dma) and o.fn is not None:
                    o.sig = True
                    lasts.append(o)
                    break
        dmas = list(self.dma_last.values())
        for e in self.ENGS:
            o = Op(e, None, False)
            o.deps = [d for d in lasts if d.eng != e] + dmas
            self.ops[e].append(o)

    def emit(self):
        nc = self.nc
        import contextlib
        with contextlib.ExitStack() as st:
            esem = {e: st.enter_context(nc.semaphore("S_" + e)) for e in self.ENGS}
            dsem = {}
            for e in self.ENGS:
                if self.ndma[e]:
                    for i in range(min(self.ndma_sems, self.ndma[e])):
                        dsem[(e, i)] = st.enter_context(nc.semaphore("D_%s_%d" % (e, i)))
            for e in self.ENGS:
                c = 0
                for o in self.ops[e]:
                    if (not o.is_dma) and o.sig and o.fn is not None:
                        c += 1
                        o.sigval = c
            block = st.enter_context(nc.Block())
            handles = {"pe": block.tensor, "act": block.scalar, "dve": block.vector,
                       "pool": block.gpsimd, "sp": block.sync}

            def make(e):
                def body(eng):
                    known = {}

                    def wait(sem, key, val):
                        if known.get(key, 0) < val:
                            eng.wait_ge(sem, val)
                            known[key] = val
                    for o in self.ops[e]:
                        for d in o.deps:
                            if d.is_dma:
                                wait(dsem[d.dsem], d.dsem, d.dval)
                            else:
                                wait(esem[d.eng], d.eng, d.sigval)
                        if o.is_dma:
                            if o.prev is not None:
                                wait(dsem[o.dsem], o.dsem, o.prev.dval)
                            o.fn(eng).then_inc(dsem[o.dsem], 16)
                        elif o.fn is not None:
                            ins = o.fn(eng)
                            if o.sig:
                                ins.then_inc(esem[e], 1)
                    if e == "sp":
                        for slot, o in self.dma_last.items():
                            wait(dsem[slot], slot, o.dval)
                return body
            for e in self.ENGS:
                if self.ops[e] or e == "sp":
                    handles[e](make(e))


def phase_a(nc, P, st, xin, w_in, g_mix, qkn, cs, idb, d_idb, kT_d, v_d, qT_d):
    def sb(name, shape, dt):
        return st.enter_context(nc.sbuf_tensor("a_" + name, shape, dt))

    def ps(name, shape, dt=F32):
        return st.enter_context(nc.psum_tensor("a_" + name, shape, dt))
    wq = sb("wq", [128, 8, 1536], BF16); d_wq = Dep()
    gm = sb("gm", [128, D], F32); d_gm = Dep()
    gq = sb("gq", [128, 2, 512], F32); d_gq = Dep()
    NB = 2
    xt = [sb("xt%d" % i, [128, D], F32) for i in range(NB)]; d_xt = [Dep() for _ in range(NB)]
    cst = [sb("cst%d" % i, [128, 64], F32) for i in range(NB)]; d_cst = [Dep() for _ in range(NB)]
    junk = sb("junk", [128, D], BF16); d_junk = Dep()
    ssq = sb("ssq", [128, 1], F32); d_ssq = Dep()
    rstd = sb("rstd", [128, 1], F32); d_rstd = Dep()
    hb = sb("hb", [128, D], BF16); d_hb = Dep()
    hT = [sb("hT%d" % i, [128, 8, 128], BF16) for i in range(NB)]; d_hT = [Dep() for _ in range(NB)]
    pT = ps("pT", [128, 8, 128], BF16); d_pT = Dep()
    pq = [ps("pq%d" % i, [128, 512], F32) for i in range(3)]; d_pq = [Dep() for _ in range(3)]
    pkT = ps("pkT", [128, 4, 128], BF16); d_pkT = Dep()
    sq = sb("sq", [128, 512], F32); d_sq = Dep()
    hs = sb("hs", [128, 8], F32); d_hs = Dep()
    hr = sb("hr", [128, 8], F32); d_hr = Dep()
    qn = sb("qn", [128, 512], F32); d_qn = Dep()
    t1 = sb("t1", [128, 256], F32); d_t1 = Dep()
    t2 = sb("t2", [128, 256], F32); d_t2 = Dep()
    t3 = sb("t3", [128, 256], F32); d_t3 = Dep()
    t4 = sb("t4", [128, 256], F32); d_t4 = Dep()
    qr = sb("qr", [128, 512], BF16); d_qr = Dep()
    kTs = [sb("kTs%d" % i, [128, 4, 128], BF16) for i in range(NB)]; d_kTs = [Dep() for _ in range(NB)]
    vs = [sb("vs%d" % i, [128, 512], BF16) for i in range(NB)]; d_vs = [Dep() for _ in range(NB)]

    for k in range(8):
        P.dma("pool", lambda e, k=k: e.dma_start(out=wq[:, k, :], in_=w_in[k * 128:(k + 1) * 128, 0:1536]), writes=[d_wq])
    P.dma("sp", lambda e: e.dma_start(out=gm[:], in_=g_mix[0:1, :].partition_broadcast(128)), writes=[d_gm])
    for i in range(2):
        P.dma("sp", lambda e, i=i: e.dma_start(out=gq[:, i, :], in_=qkn[i:i + 1, :].partition_broadcast(128)), writes=[d_gq])
    P.op("dve", lambda e: e.tensor_scalar(out=gq[:, 0, :], in0=gq[:, 0, :], scalar1=0.125, scalar2=None, op0=ALU.mult),
         reads=[d_gq], writes=[d_gq])

    def qk_post(src_ps, d_src, which, b):
        P.op("act", lambda e: e.activation(out=sq[:], in_=src_ps[:], func=AF.Square), reads=[d_src], writes=[d_sq])
        P.op("dve", lambda e: e.tensor_reduce(out=hs[:], in_=sq[:].rearrange("p (h d) -> p h d", h=8), axis=AX.X, op=ALU.add),
             reads=[d_sq], writes=[d_hs])
        P.op("act", lambda e: e.activation(out=hr[:], in_=hs[:], func=AF.Sqrt, bias=EPS, scale=1.0 / 64), reads=[d_hs], writes=[d_hr])
        P.op("dve", lambda e: e.reciprocal(out=hr[:], in_=hr[:]), reads=[d_hr], writes=[d_hr])
        P.op("dve", lambda e: e.tensor_tensor(out=qn[:].rearrange("p (h d) -> p h d", h=8), in0=src_ps[:].rearrange("p (h d) -> p h d", h=8),
                                              in1=hr[:].unsqueeze(2).to_broadcast([128, 8, 64]), op=ALU.mult),
             reads=[d_src, d_hr], writes=[d_qn])
        P.op("pool", lambda e: e.tensor_tensor(out=qn[:], in0=qn[:], in1=gq[:, which, :], op=ALU.mult), reads=[d_qn, d_gq], writes=[d_qn])
        q3 = qn[:].rearrange("p (h d) -> p h d", h=8)
        q1 = q3[:, :, 0:32]
        q2 = q3[:, :, 32:64]
        cosb = cst[b][:, 0:32].unsqueeze(1).to_broadcast([128, 8, 32])
        sinb = cst[b][:, 32:64].unsqueeze(1).to_broadcast([128, 8, 32])
        v3 = lambda t: t[:].rearrange("p (h d) -> p h d", h=8)
        P.op("dve", lambda e: e.tensor_tensor(out=v3(t1), in0=q1, in1=cosb, op=ALU.mult), reads=[d_qn, d_cst[b]], writes=[d_t1])
        P.op("pool", lambda e: e.tensor_tensor(out=v3(t2), in0=q2, in1=sinb, op=ALU.mult), reads=[d_qn, d_cst[b]], writes=[d_t2])
        P.op("dve", lambda e: e.tensor_tensor(out=v3(t3), in0=q2, in1=cosb, op=ALU.mult), reads=[d_qn, d_cst[b]], writes=[d_t3])
        P.op("pool", lambda e: e.tensor_tensor(out=v3(t4), in0=q1, in1=sinb, op=ALU.mult), reads=[d_qn, d_cst[b]], writes=[d_t4])
        r3 = qr[:].rearrange("p (h d) -> p h d", h=8)
        P.op("dve", lambda e: e.tensor_tensor(out=r3[:, :, 0:32], in0=v3(t1), in1=v3(t2), op=ALU.subtract), reads=[d_t1, d_t2], writes=[d_qr])
        P.op("pool", lambda e: e.tensor_tensor(out=r3[:, :, 32:64], in0=v3(t3), in1=v3(t4), op=ALU.add), reads=[d_t3, d_t4], writes=[d_qr])

    def to_T_and_store(dst_dram, col0, b):
        for c in range(4):
            P.op("pe", lambda e, c=c: e.transpose(out=pkT[:, c, :], in_=qr[:, c * 128:(c + 1) * 128], identity=idb[:]),
                 reads=[d_qr, d_idb], writes=[d_pkT])
        P.op("act", lambda e: e.copy(out=kTs[b][:], in_=pkT[:]), reads=[d_pkT], writes=[d_kTs[b]])
        P.dma("sp", lambda e: e.dma_start(out=dst_dram[:, col0:col0 + 128].rearrange("(c p) t -> p c t", p=128), in_=kTs[b][:]),
              reads=[d_kTs[b]])

    for ti in range(64):
        b = ti % NB
        own = ti >= 32
        r0 = ti * 128
        P.dma("sp", lambda e, b=b, r0=r0: e.dma_start(out=xt[b][:], in_=xin[r0:r0 + 128, :]), writes=[d_xt[b]])
        P.dma("sp", lambda e, b=b, r0=r0: e.dma_start(out=cst[b][:], in_=cs[r0:r0 + 128, :]), writes=[d_cst[b]])
        P.op("act", lambda e, b=b: e.activation(out=junk[:], in_=xt[b][:], func=AF.Square, accum_out=ssq[:]), reads=[d_xt[b]], writes=[d_junk, d_ssq])
        P.op("act", lambda e: e.activation(out=rstd[:], in_=ssq[:], func=AF.Sqrt, bias=EPS, scale=1.0 / D), reads=[d_ssq], writes=[d_rstd])
        P.op("dve", lambda e: e.reciprocal(out=rstd[:], in_=rstd[:]), reads=[d_rstd], writes=[d_rstd])
        P.op("dve", lambda e, b=b: e.scalar_tensor_tensor(out=hb[:], in0=xt[b][:], scalar=rstd[:, 0:1], in1=gm[:], op0=ALU.mult, op1=ALU.mult),
             reads=[d_xt[b], d_rstd, d_gm], writes=[d_hb])
        for k in range(8):
            P.op("pe", lambda e, k=k: e.transpose(out=pT[:, k, :], in_=hb[:, k * 128:(k + 1) * 128], identity=idb[:]), reads=[d_hb, d_idb], writes=[d_pT])
        P.op("act", lambda e, b=b: e.copy(out=hT[b][:], in_=pT[:]), reads=[d_pT], writes=[d_hT[b]])
        groups = [0, 1, 2] if own else [1, 2]
        for g in groups:
            for k in range(8):
                P.op("pe", lambda e, g=g, k=k, b=b: e.matmul(pq[g][:], lhsT=hT[b][:, k, :], rhs=wq[:, k, g * 512:(g + 1) * 512], start=(k == 0), stop=(k == 7)),
                     reads=[d_hT[b], d_wq], writes=[d_pq[g]])
        P.op("act", lambda e, b=b: e.copy(out=vs[b][:], in_=pq[2][:]), reads=[d_pq[2]], writes=[d_vs[b]])
        P.dma("sp", lambda e, b=b, r0=r0: e.dma_start(out=v_d[r0:r0 + 128, :], in_=vs[b][:]), reads=[d_vs[b]])
        qk_post(pq[1], d_pq[1], 1, b)
        to_T_and_store(kT_d, r0, b)
        if own:
            qk_post(pq[0], d_pq[0], 0, b)
            to_T_and_store(qT_d, r0 - TP, b)


def phase_b(nc, P, st, kT_d, v_d, qT_d, e_d, vq_d, nb_d, oq_d, tri_d, idb, d_idb, oa, d_oa, heads=range(8), nblk=16):
    def sb(name, shape, dt):
        return st.enter_context(nc.sbuf_tensor("b_" + name, shape, dt))

    def ps(name, shape, dt=F32):
        return st.enter_context(nc.psum_tensor(name, shape, dt))
    NB = 2
    kaug = [sb("kaug%d" % i, [96, NK], BF16) for i in range(NB)]; d_kaug = [Dep() for _ in range(NB)]
    vaug = [sb("vaug%d" % i, [128, 64, 65], BF16) for i in range(NB)]; d_vaug = [Dep() for _ in range(NB)]
    qaug = [sb("qaug%d" % i, [96, T], BF16) for i in range(NB)]; d_qaug = [Dep() for _ in range(NB)]
    d_qmb = [Dep() for _ in range(NB)]
    vq = sb("vq_s", [128, 1024], F32); d_c = Dep()
    nb = sb("nb_s", [128, 1024], F32)
    oq = sb("oq_s", [128, 1024], F32)
    tri = sb("tri_s", [128, 128], BF16)
    ksum = sb("ksum", [64, 32], F32); d_ksum = Dep()
    kmean = sb("kmean", [64, 32], BF16); d_kmean = Dep()
    g1 = sb("g1", [128, 512], F32); d_g1 = Dep()
    top8 = sb("top8", [128, 16, 8], F32); d_top8 = Dep()
    sel = sb("sel", [128, 512], F32); d_sel = Dep()
    mbp = sb("mbp", [128, 16, 96], BF16); d_mbp = Dep()
    NPT = 3
    pt = [sb("pt%d" % i, [128, 512], BF16) for i in range(NPT)]; d_pt = [Dep() for _ in range(NPT)]
    rc = sb("rc", [128, 2], F32); d_rc = [Dep(), Dep()]
    pg = ps("pg", [128, 512], F32); d_pg = Dep()
    pmT = ps("pmT", [96, 8, 128], BF16); d_pmT = Dep()
    psS = [ps("psS%d" % i, [128, 512], F32) for i in range(NPT)]; d_psS = [Dep() for _ in range(NPT)]
    po = [ps("po%d" % i, [128, 65], F32) for i in range(2)]; d_po = [Dep() for _ in range(2)]

    for i in range(NB):
        P.dma("sp", lambda e, i=i: e.dma_start(out=kaug[i][64:96, :], in_=e_d[:, :]), writes=[d_kaug[i]])
        P.op("pool", lambda e, i=i: e.memset(vaug[i][:], 1.0), writes=[d_vaug[i]])
    P.dma("sp", lambda e: e.dma_start(out=vq[:], in_=vq_d[0:1, :].partition_broadcast(128)), writes=[d_c])
    P.dma("sp", lambda e: e.dma_start(out=nb[:], in_=nb_d[0:1, :].partition_broadcast(128)), writes=[d_c])
    P.dma("sp", lambda e: e.dma_start(out=oq[:], in_=oq_d[0:1, :].partition_broadcast(128)), writes=[d_c])
    P.dma("pool", lambda e: e.dma_start(out=tri[:], in_=tri_d[:, :]), writes=[d_c])
    P.op("pool", lambda e: e.memset(mbp[:], 0.0), writes=[d_mbp])

    for ih, h in enumerate(heads):
        b = ih % NB
        P.dma("sp", lambda e, b=b, h=h: e.dma_start(out=kaug[b][0:64, :], in_=kT_d[h * 64:(h + 1) * 64, :]), writes=[d_kaug[b]])
        P.dma("sp", lambda e, b=b, h=h: e.dma_start(out=vaug[b][:, :, 0:64],
                                                    in_=v_d[:, h * 64:(h + 1) * 64].rearrange("(t p) d -> p t d", p=128)),
              writes=[d_vaug[b]])
        P.dma("sp", lambda e, b=b, h=h: e.dma_start(out=qaug[b][0:64, :], in_=qT_d[h * 64:(h + 1) * 64, :]), writes=[d_qaug[b]])
        P.op("dve", lambda e, b=b: e.tensor_reduce(out=ksum[:], in_=kaug[b][0:64, :].rearrange("p (b k) -> p b k", k=256),
                                                   axis=AX.X, op=ALU.add), reads=[d_kaug[b]], writes=[d_ksum])
        P.op("dve", lambda e: e.tensor_scalar(out=kmean[:], in0=ksum[:], scalar1=1.0 / 256, scalar2=None, op0=ALU.mult),
             reads=[d_ksum], writes=[d_kmean])
        for half in range(2):
            for j in range(16):
                qt = half * 16 + j
                P.op("pe", lambda e, b=b, j=j, qt=qt: e.matmul(pg[:, j * 32:(j + 1) * 32], lhsT=qaug[b][0:64, qt * 128:(qt + 1) * 128],
                                                              rhs=kmean[:, :], start=True, stop=True),
                     reads=[d_qaug[b], d_kmean], writes=[d_pg])
            cs_ = slice(half * 512, (half + 1) * 512)
            P.op("dve", lambda e, cs_=cs_: e.tensor_tensor(out=g1[:], in0=pg[:], in1=vq[:, cs_], op=ALU.mult),
                 reads=[d_pg, d_c], writes=[d_g1])
            P.op("dve", lambda e, cs_=cs_: e.tensor_tensor(out=g1[:], in0=g1[:], in1=nb[:, cs_], op=ALU.add),
                 reads=[d_g1, d_c], writes=[d_g1])
            for j in range(16):
                P.op("dve", lambda e, j=j: e.max(out=top8[:, j, :], in_=g1[:, j * 32:(j + 1) * 32]), reads=[d_g1], writes=[d_top8])
            P.op("dve", lambda e: e.tensor_tensor(out=sel[:].rearrange("p (j b) -> p j b", b=32),
                                                  in0=g1[:].rearrange("p (j b) -> p j b", b=32),
                                                  in1=top8[:, :, 2:3].to_broadcast([128, 16, 32]), op=ALU.is_ge),
                 reads=[d_g1, d_top8], writes=[d_sel])
            P.op("dve", lambda e, cs_=cs_: e.tensor_tensor(out=sel[:], in0=sel[:], in1=vq[:, cs_], op=ALU.mult),
                 reads=[d_sel, d_c], writes=[d_sel])
            P.op("dve", lambda e, cs_=cs_: e.tensor_tensor(out=sel[:], in0=sel[:], in1=oq[:, cs_], op=ALU.add),
                 reads=[d_sel, d_c], writes=[d_sel])
            P.op("dve", lambda e: e.tensor_scalar(out=mbp[:, :, 64:96], in0=sel[:].rearrange("p (j b) -> p j b", b=32),
                                                  scalar1=1.0, scalar2=MB, op0=ALU.subtract, op1=ALU.mult),
                 reads=[d_sel], writes=[d_mbp])
            for grp in range(2):
                for j in range(8):
                    P.op("pe", lambda e, grp=grp, j=j: e.transpose(out=pmT[:, j, :], in_=mbp[:, grp * 8 + j, :], identity=idb[:]),
                         reads=[d_mbp, d_idb], writes=[d_pmT])
                c0 = (half * 16 + grp * 8) * 128
                P.op("act", lambda e, b=b, c0=c0: e.copy(out=qaug[b][64:96, c0:c0 + 1024], in_=pmT[64:96, :, :]),
                     reads=[d_pmT], writes=[d_qmb[b]])
        units = []
        for jb in range(nblk):
            ncommon = 32 + 2 * jb
            for u in range(ncommon // 2):
                units.append((jb, [2 * u, 2 * u + 1], False, u == 0))
            units.append((jb, [32 + 2 * jb, 32 + 2 * jb + 1], True, False))

        def emit_S(u, s, b=b):
            jb, kts, diag, first = u
            for j, kt in enumerate(kts):
                if diag and j == 1:
                    q0, nq, c0 = jb * 256 + 128, 128, 256
                else:
                    q0, nq, c0 = jb * 256, 256, j * 256
                P.op("pe", lambda e, s=s, kt=kt, q0=q0, nq=nq, c0=c0: e.matmul(psS[s][:, c0:c0 + nq], lhsT=kaug[b][0:96, kt * 128:(kt + 1) * 128],
                                                                             rhs=qaug[b][0:96, q0:q0 + nq], start=True, stop=True),
                     reads=[d_kaug[b], d_qaug[b], d_qmb[b]], writes=[d_psS[s]])

        def emit_rest(u, s, b=b, h=h):
            jb, kts, diag, first = u
            ncols = 384 if diag else 512
            P.op("act", lambda e, s=s, ncols=ncols: e.activation(out=pt[s][:, 0:ncols], in_=psS[s][:, 0:ncols], func=AF.Exp),
                 reads=[d_psS[s]], writes=[d_pt[s]])
            if diag:
                P.op("pool", lambda e, s=s: e.tensor_tensor(out=pt[s][:, 0:128], in0=pt[s][:, 0:128], in1=tri[:], op=ALU.mult),
                     reads=[d_pt[s], d_c], writes=[d_pt[s]])
                P.op("pool", lambda e, s=s: e.tensor_tensor(out=pt[s][:, 256:384], in0=pt[s][:, 256:384], in1=tri[:], op=ALU.mult),
                     reads=[d_pt[s], d_c], writes=[d_pt[s]])
            for j, kt in enumerate(kts):
                if diag and j == 1:
                    pv = [(1, 256, True)]
                elif diag:
                    pv = [(0, 0, True), (1, 128, False)]
                else:
                    pv = [(0, j * 256, False), (1, j * 256 + 128, False)]
                st_ = first and j == 0
                for qi, col0, last in pv:
                    P.op("pe", lambda e, s=s, kt=kt, qi=qi, col0=col0, st_=st_, last=last: e.matmul(
                        po[qi][:, :], lhsT=pt[s][:, col0:col0 + 128], rhs=vaug[b][:, kt, :], start=st_, stop=last),
                        reads=[d_pt[s], d_vaug[b]], writes=[d_po[qi]])
            if diag:
                for qi in range(2):
                    qt = jb * 2 + qi
                    P.op("dve", lambda e, qi=qi: e.reciprocal(out=rc[:, qi:qi + 1], in_=po[qi][:, 64:65]), reads=[d_po[qi]], writes=[d_rc[qi]])
                    P.op("dve", lambda e, qi=qi, qt=qt: e.tensor_scalar(out=oa[:, qt, h * 64:(h + 1) * 64], in0=po[qi][:, 0:64],
                                                                       scalar1=rc[:, qi:qi + 1], scalar2=None, op0=ALU.mult),
                         reads=[d_po[qi], d_rc[qi]], writes=[d_oa])
        SK = 2
        n = len(units)
        for i in range(n + SK):
            if i < n:
                emit_S(units[i], i % NPT)
            if i >= SK:
                emit_rest(units[i - SK], (i - SK) % NPT)


def host_consts(half):
    pv = 1.0 if half == 1 else 0.0
    V = np.zeros((32, 32), np.float32); O = np.zeros((32, 32), np.float32)
    for qt in range(32):
        jb = qt // 2
        V[qt, :16] = pv
        V[qt, 16:16 + jb] = 1.0
        O[qt, 16 + jb] = 1.0
    NBm = (V - 1.0) * BIGG
    E = np.zeros((32, NK), np.float32)
    for b in range(32):
        E[b, b * 256:(b + 1) * 256] = 1.0
    tri = (np.arange(128)[:, None] <= np.arange(128)[None, :]).astype(np.float32)
    return dict(vq=V.reshape(1, 1024), nb=NBm.reshape(1, 1024), oq=O.reshape(1, 1024),
                e_d=E.astype(ml_dtypes.bfloat16), tri=tri.astype(ml_dtypes.bfloat16))


def phase_c(nc, P, st, xin, w_in, g_mix, cwl_d, cbl_d, dtb_d, alog_d, dsk_d, ssdn_d, tri2_d, sel_d, tb_d, pflag_d,
            idb, d_idb, os_d, d_osd, chunks=range(32)):
    def sb(name, shape, dt):
        return st.enter_context(nc.sbuf_tensor("c_" + name, shape, dt))

    def ps(name, shape, dt=F32):
        return st.enter_context(nc.psum_tensor(name, shape, dt))
    wz = sb("wz", [128, 8, 1024], BF16); d_w = Dep()
    wx = sb("wx", [128, 8, 2048], BF16)
    wdt = sb("wdt", [128, 8, 16], BF16)
    gm = sb("gmc", [128, D], F32); d_c = Dep()
    cw = sb("cw", [128, 16, 4], F32)
    cb = sb("cb", [128, 16], F32)
    dtb = sb("dtb", [128, 2, 16], F32)
    Aneg = sb("Aneg", [128, 2, 16], F32); d_A = Dep()
    dsk = sb("dsk", [128, 16], F32)
    ssdn = sb("ssdn", [128, D], F32)
    tri2 = sb("tri2", [128, 2, 256], F32)
    onesf = sb("onesf", [128, 128], F32)
    sel = sb("selc", [16, 16, 128], F32)
    tb = sb("tbc", [128, 384], F32)
    pflag = sb("pflag", [128, 1], F32)
    xt = sb("xtc", [128, 2, D], F32); d_xt = Dep()
    junk = sb("junkc", [128, D], BF16); d_junk = Dep()
    ssq = sb("ssqc", [128, 2], F32); d_ssq = Dep()
    rstd = sb("rstdc", [128, 2], F32); d_rstd = Dep()
    hb = sb("hbc", [128, 2, D], BF16); d_hb = Dep()
    hT = sb("hTc", [128, 8, 256], BF16); d_hT = Dep()
    xraw = sb("xraw", [128, 16, 259], F32); d_xraw = Dep(); d_halo = Dep()
    acc = sb("acc", [128, 8, 256], F32); d_acc = [Dep() for _ in range(8)]
    xc = sb("xc", [128, 16, 256], BF16); d_xc = [Dep() for _ in range(16)]
    xtok = sb("xtok", [128, 2, 1024], BF16); d_xtok = Dep()
    btok = sb("btok", [128, 2, 512], BF16); d_btok = Dep()
    dtr = sb("dtr", [128, 2, 16], F32); d_dtr = Dep()
    dt = sb("dt", [128, 2, 16], F32); d_dt = Dep()
    aa = sb("aa", [128, 2, 16], F32); d_aa = Dep()
    acum = sb("acum", [128, 2, 16], F32); d_acum = Dep()
    nacum = sb("nacum", [128, 2, 16], F32); d_nacum = Dep()
    acumT = sb("acumT", [16, 256], F32); d_acumT = Dep()
    tot = sb("tot", [128, 16], F32); d_tot = Dep()
    cdec = sb("cdec", [128, 16], F32); d_cdec = Dep()
    dte = sb("dte", [128, 2, 16], F32); d_dte = Dep()
    eac = sb("eac", [128, 2, 16], F32); d_eac = Dep()
    w2 = sb("w2", [128, 2, 16], F32); d_w2 = Dep()
    xdt = sb("xdt", [128, 2, 1024], BF16); d_xdt = Dep()
    xdd = sb("xdd", [128, 2, 1024], BF16); d_xdd = Dep()
    zs = sb("zs", [128, 2, 1024], BF16); d_zs = Dep()
    cbT = sb("cbT", [128, 4, 384], F32); d_cbT = [Dep() for _ in range(4)]
    arg = sb("arg", [128, 384], F32); d_arg = Dep()
    Lt = sb("Lt", [128, 384], F32); d_Lt = Dep()
    Mt = [sb("Mt%d" % i, [128, 384], BF16) for i in range(2)]; d_Mt = [Dep() for _ in range(2)]
    stf = sb("stf", [128, 4, 256], F32); d_stf = Dep()
    stT = sb("stT", [128, 4, 256], BF16); d_stT = Dep()
    yt = sb("yt", [128, 256], F32); d_yt = Dep()
    y2 = sb("y2", [128, 256], F32); d_y2 = Dep()
    gss = sb("gss", [128, 1], F32); d_gss = Dep()
    grs = sb("grs", [128, 1], F32); d_grs = Dep()
    osb = sb("osb", [128, 2, 1024], BF16); d_osb = Dep()

    B = [ps("bk%d" % i, [128, 512], F32) for i in range(7)]
    d_B = [Dep() for _ in range(7)]
    pT = ps("pTc", [128, 8, 128], BF16); d_pT = Dep()

    for k in range(8):
        rows = slice(k * 128, (k + 1) * 128)
        P.dma("pool", lambda e, k=k, rows=rows: e.dma_start(out=wz[:, k, :], in_=w_in[rows, C_Z:C_Z + 1024]), writes=[d_w])
        P.dma("pool", lambda e, k=k, rows=rows: e.dma_start(out=wx[:, k, :], in_=w_in[rows, C_X:C_X + 2048]), writes=[d_w])
        P.dma("pool", lambda e, k=k, rows=rows: e.dma_start(out=wdt[:, k, :], in_=w_in[rows, C_DT:C_DT + 16]), writes=[d_w])
    P.dma("sp", lambda e: e.dma_start(out=gm[:], in_=g_mix[0:1, :].partition_broadcast(128)), writes=[d_c])
    P.dma("sp", lambda e: e.dma_start(out=cw[:], in_=cwl_d[:, :, :]), writes=[d_c])
    P.dma("sp", lambda e: e.dma_start(out=cb[:], in_=cbl_d[:, :]), writes=[d_c])
    for i in range(2):
        P.dma("sp", lambda e, i=i: e.dma_start(out=dtb[:, i, :], in_=dtb_d[0:1, :].partition_broadcast(128)), writes=[d_c])
        P.dma("sp", lambda e, i=i: e.dma_start(out=Aneg[:, i, :], in_=alog_d[0:1, :].partition_broadcast(128)), writes=[d_A])
    P.dma("sp", lambda e: e.dma_start(out=dsk[:], in_=dsk_d[0:1, :].partition_broadcast(128)), writes=[d_c])
    P.dma("sp", lambda e: e.dma_start(out=ssdn[:], in_=ssdn_d[0:1, :].partition_broadcast(128)), writes=[d_c])
    P.dma("sp", lambda e: e.dma_start(out=tri2[:], in_=tri2_d[:, :, :]), writes=[d_c])
    P.dma("sp", lambda e: e.dma_start(out=sel[:], in_=sel_d[:, :, :]), writes=[d_c])
    P.dma("sp", lambda e: e.dma_start(out=tb[:], in_=tb_d[:, :]), writes=[d_c])
    P.dma("sp", lambda e: e.dma_start(out=pflag[:], in_=pflag_d[0:1, :].partition_broadcast(128)), writes=[d_c])
    P.op("pool", lambda e: e.memset(onesf[:], 1.0), writes=[d_c])
    P.op("act", lambda e: e.activation(out=Aneg[:], in_=Aneg[:], func=AF.Exp), reads=[d_A], writes=[d_A])
    P.op("dve", lambda e: e.tensor_scalar(out=Aneg[:], in0=Aneg[:], scalar1=-1.0, scalar2=None, op0=ALU.mult), reads=[d_A], writes=[d_A])
    P.op("pool", lambda e: e.memset(xraw[:], 0.0), writes=[d_xraw, d_halo])
    P.op("pool", lambda e: e.memset(stf[:], 0.0), writes=[d_stf])
    P.op("pool", lambda e: e.memset(stT[:], 0.0), writes=[d_stT])

    h3 = lambda ap: ap.rearrange("p (h d) -> p h d", d=64)

    for c in chunks:
        own = c >= 16
        r0 = c * 256
        P.dma("sp", lambda e, r0=r0: e.dma_start(out=xt[:], in_=xin[r0:r0 + 256, :].rearrange("(t p) d -> p t d", p=128)), writes=[d_xt])
        for it in range(2):
            P.op("act", lambda e, it=it: e.activation(out=junk[:], in_=xt[:, it, :], func=AF.Square, accum_out=ssq[:, it:it + 1]),
                 reads=[d_xt], writes=[d_junk, d_ssq])
        P.op("act", lambda e: e.activation(out=rstd[:], in_=ssq[:], func=AF.Sqrt, bias=EPS, scale=1.0 / D), reads=[d_ssq], writes=[d_rstd])
        P.op("dve", lambda e: e.reciprocal(out=rstd[:], in_=rstd[:]), reads=[d_rstd], writes=[d_rstd])
        for it in range(2):
            P.op("dve", lambda e, it=it: e.scalar_tensor_tensor(out=hb[:, it, :], in0=xt[:, it, :], scalar=rstd[:, it:it + 1], in1=gm[:],
                                                                                    op0=ALU.mult, op1=ALU.mult),
                 reads=[d_xt, d_rstd, d_c], writes=[d_hb])
        for it in range(2):
            for k in range(8):
                P.op("pe", lambda e, it=it, k=k: e.transpose(out=pT[:, k, :], in_=hb[:, it, k * 128:(k + 1) * 128], identity=idb[:]),
                     reads=[d_hb, d_idb], writes=[d_pT])
            P.op("act", lambda e, it=it: e.copy(out=hT[:, :, it * 128:(it + 1) * 128], in_=pT[:]), reads=[d_pT], writes=[d_hT])
        for it in range(2):
            for k in range(8):
                P.op("pe", lambda e, it=it, k=k: e.matmul(B[3][:, it * 16:(it + 1) * 16], lhsT=hT[:, k, it * 128:(it + 1) * 128], rhs=wdt[:, k, :],
                                                         start=(k == 0), stop=(k == 7)), reads=[d_hT, d_w], writes=[d_B[3]])
        P.op("dve", lambda e: e.tensor_tensor(out=dtr[:].rearrange("p a b -> p (a b)"), in0=B[3][:, 0:32], in1=dtb[:].rearrange("p a b -> p (a b)"), op=ALU.add),
             reads=[d_B[3], d_c], writes=[d_dtr])
        P.op("act", lambda e: e.activation(out=dtr[:], in_=dtr[:], func=AF.Exp), reads=[d_dtr], writes=[d_dtr])
        P.op("act", lambda e: e.activation(out=dt[:], in_=dtr[:], func=AF.Ln, bias=1.0), reads=[d_dtr], writes=[d_dt])
        P.op("dve", lambda e: e.tensor_tensor(out=aa[:], in0=dt[:], in1=Aneg[:], op=ALU.mult), reads=[d_dt, d_A], writes=[d_aa])
        for it in range(2):
            for jt in range(it + 1):
                P.op("pe", lambda e, it=it, jt=jt: e.matmul(B[3][:, 32 + it * 16:32 + (it + 1) * 16], lhsT=tri2[:, jt, it * 128:(it + 1) * 128],
                                                           rhs=aa[:, jt, :], start=(jt == 0), stop=(jt == it)),
                     reads=[d_aa, d_c], writes=[d_B[3]])
        for jt in range(2):
            P.op("pe", lambda e, jt=jt: e.matmul(B[3][:, 64:80], lhsT=onesf[:], rhs=aa[:, jt, :], start=(jt == 0), stop=(jt == 1)),
                 reads=[d_aa, d_c], writes=[d_B[3]])
        for jt in range(2):
            P.op("pe", lambda e, jt=jt: e.matmul(B[3][0:16, 128:384], lhsT=aa[:, jt, :], rhs=tri2[:, jt, :], start=(jt == 0), stop=(jt == 1)),
                 reads=[d_aa, d_c], writes=[d_B[3]])
        P.op("act", lambda e: e.copy(out=acum[:].rearrange("p a b -> p (a b)"), in_=B[3][:, 32:64]), reads=[d_B[3]], writes=[d_acum])
        P.op("dve", lambda e: e.tensor_scalar(out=nacum[:].rearrange("p a b -> p (a b)"), in0=B[3][:, 32:64], scalar1=-1.0, scalar2=None, op0=ALU.mult),
             reads=[d_B[3]], writes=[d_nacum])
        P.op("act", lambda e: e.copy(out=tot[:], in_=B[3][:, 64:80]), reads=[d_B[3]], writes=[d_tot])
        P.op("act", lambda e: e.copy(out=acumT[:], in_=B[3][0:16, 128:384]), reads=[d_B[3]], writes=[d_acumT])
        P.op("act", lambda e: e.activation(out=cdec[:], in_=tot[:], func=AF.Exp), reads=[d_tot], writes=[d_cdec])
        P.op("dve", lambda e: e.tensor_tensor(out=dte[:], in0=nacum[:], in1=tot[:].unsqueeze(1).to_broadcast([128, 2, 16]), op=ALU.add),
             reads=[d_nacum, d_tot], writes=[d_dte])
        P.op("act", lambda e: e.activation(out=dte[:], in_=dte[:], func=AF.Exp), reads=[d_dte], writes=[d_dte])
        P.op("act", lambda e: e.activation(out=eac[:], in_=acum[:], func=AF.Exp), reads=[d_acum], writes=[d_eac])
        P.op("dve", lambda e: e.tensor_tensor(out=w2[:], in0=dt[:], in1=dte[:], op=ALU.mult), reads=[d_dt, d_dte], writes=[d_w2])
        for fc in range(16):
            hv = fc % 2
            for k in range(8):
                P.op("pe", lambda e, fc=fc, k=k, hv=hv: e.matmul(B[1][:, hv * 256:(hv + 1) * 256], lhsT=wx[:, k, fc * 128:(fc + 1) * 128], rhs=hT[:, k, :],
                                                               start=(k == 0), stop=(k == 7)), reads=[d_hT, d_w], writes=[d_B[1]])
            P.op("act", lambda e, fc=fc, hv=hv: e.copy(out=xraw[:, fc, 3:259], in_=B[1][:, hv * 256:(hv + 1) * 256]),
                 reads=[d_B[1]], writes=[d_xraw])
        for half in range(2):
            for f8 in range(8):
                fc = half * 8 + f8
                eng = "dve"
                P.op(eng, lambda e, fc=fc, f8=f8: e.tensor_scalar(out=acc[:, f8, :], in0=xraw[:, fc, 0:256], scalar1=cw[:, fc, 0:1], scalar2=cb[:, fc:fc + 1],
                                                                 op0=ALU.mult, op1=ALU.add), reads=[d_xraw, d_halo, d_c], writes=[d_acc[f8]])
                for kk in range(1, 4):
                    P.op(eng, lambda e, fc=fc, f8=f8, kk=kk: e.scalar_tensor_tensor(out=acc[:, f8, :], in0=xraw[:, fc, kk:kk + 256], scalar=cw[:, fc, kk:kk + 1],
                                                                                   in1=acc[:, f8, :], op0=ALU.mult, op1=ALU.add),
                         reads=[d_xraw, d_halo, d_c, d_acc[f8]], writes=[d_acc[f8]])
                P.op("act", lambda e, fc=fc, f8=f8: e.activation(out=xc[:, fc, :], in_=acc[:, f8, :], func=AF.Silu),
                     reads=[d_acc[f8]], writes=[d_xc[fc]])
        P.op("pool", lambda e: e.tensor_copy(out=xraw[:, :, 0:3], in_=xraw[:, :, 256:259]), reads=[d_xraw], writes=[d_halo])
        for it in range(2):
            for fc in range(8):
                P.op("pe", lambda e, it=it, fc=fc: e.transpose(out=pT[:, fc, :], in_=xc[:, fc, it * 128:(it + 1) * 128], identity=idb[:]),
                     reads=[d_xc[fc], d_idb], writes=[d_pT])
            P.op("act", lambda e, it=it: e.copy(out=xtok[:, it, :], in_=pT[:]), reads=[d_pT], writes=[d_xtok])
            for g in range(4):
                P.op("pe", lambda e, it=it, g=g: e.transpose(out=pT[:, g, :], in_=xc[:, 8 + g, it * 128:(it + 1) * 128], identity=idb[:]),
                     reads=[d_xc[8 + g], d_idb], writes=[d_pT])
            P.op("act", lambda e, it=it: e.copy(out=btok[:, it, :], in_=pT[:, 0:4, :]), reads=[d_pT], writes=[d_btok])
        for it in range(2):
            if own:
                P.op("dve", lambda e, it=it: e.tensor_tensor(out=h3(xdt[:, it, :]), in0=h3(xtok[:, it, :]),
                                                             in1=dt[:, it, :].unsqueeze(2).to_broadcast([128, 16, 64]), op=ALU.mult),
                     reads=[d_xtok, d_dt], writes=[d_xdt])
            P.op("pool", lambda e, it=it: e.tensor_tensor(out=h3(xdd[:, it, :]), in0=h3(xtok[:, it, :]),
                                                          in1=w2[:, it, :].unsqueeze(2).to_broadcast([128, 16, 64]), op=ALU.mult),
                 reads=[d_xtok, d_w2], writes=[d_xdd])
        if own:
            for it in range(2):
                for hv in range(2):
                    for k in range(8):
                        P.op("pe", lambda e, it=it, hv=hv, k=k: e.matmul(B[2][:, :], lhsT=hT[:, k, it * 128:(it + 1) * 128], rhs=wz[:, k, hv * 512:(hv + 1) * 512],
                                                                       start=(k == 0), stop=(k == 7)), reads=[d_hT, d_w], writes=[d_B[2]])
                    P.op("act", lambda e, it=it, hv=hv: e.activation(out=zs[:, it, hv * 512:(hv + 1) * 512], in_=B[2][:, :], func=AF.Silu),
                         reads=[d_B[2]], writes=[d_zs])
            for g in range(4):
                P.op("pe", lambda e, g=g: e.matmul(B[4][:, 0:256], lhsT=xc[:, 8 + g, 0:128], rhs=xc[:, 12 + g, :], start=True, stop=True),
                     reads=[d_xc[8 + g], d_xc[12 + g]], writes=[d_B[4]])
                P.op("pe", lambda e, g=g: e.matmul(B[4][:, 256:384], lhsT=xc[:, 8 + g, 128:256], rhs=xc[:, 12 + g, 128:256], start=True, stop=True),
                     reads=[d_xc[8 + g], d_xc[12 + g]], writes=[d_B[4]])
                P.op("act", lambda e, g=g: e.copy(out=cbT[:, g, :], in_=B[4][:, 0:384]), reads=[d_B[4]], writes=[d_cbT[g]])
            for g in range(4):
                for r in range(4):
                    h = g * 4 + r
                    mi = h % 2
                    P.op("pe", lambda e, h=h: e.matmul(B[5][:, 0:256], lhsT=sel[:, h, :], rhs=acumT[:, :], start=True, stop=True),
                         reads=[d_c, d_acumT], writes=[d_B[5]])
                    P.op("dve", lambda e, h=h: e.scalar_tensor_tensor(out=arg[:, 0:256], in0=B[5][:, 0:256], scalar=nacum[:, 0, h:h + 1], in1=tb[:, 0:256],
                                                                      op0=ALU.add, op1=ALU.add), reads=[d_B[5], d_nacum, d_c], writes=[d_arg])
                    P.op("dve", lambda e, h=h: e.scalar_tensor_tensor(out=arg[:, 256:384], in0=B[5][:, 128:256], scalar=nacum[:, 1, h:h + 1], in1=tb[:, 256:384],
                                                                      op0=ALU.add, op1=ALU.add), reads=[d_B[5], d_nacum, d_c], writes=[d_arg])
                    P.op("act", lambda e: e.activation(out=Lt[:], in_=arg[:], func=AF.Exp), reads=[d_arg], writes=[d_Lt])
                    P.op("pool", lambda e, g=g, mi=mi: e.tensor_tensor(out=Mt[mi][:], in0=Lt[:], in1=cbT[:, g, :], op=ALU.mult),
                         reads=[d_Lt, d_cbT[g]], writes=[d_Mt[mi]])
                    cs_ = slice(r * 64, (r + 1) * 64)
                    hs_ = slice(h * 64, (h + 1) * 64)
                    P.op("pe", lambda e, mi=mi, cs_=cs_, hs_=hs_: e.matmul(B[6][:, 0:256][:, cs_], lhsT=Mt[mi][:, 0:128], rhs=xdt[:, 0, hs_], start=True, stop=True),
                         reads=[d_Mt[mi], d_xdt], writes=[d_B[6]])
                    P.op("pe", lambda e, mi=mi, cs_=cs_, hs_=hs_: e.matmul(B[6][:, 256:512][:, cs_], lhsT=Mt[mi][:, 128:256], rhs=xdt[:, 0, hs_], start=True, stop=False),
                         reads=[d_Mt[mi], d_xdt], writes=[d_B[6]])
                    P.op("pe", lambda e, mi=mi, cs_=cs_, hs_=hs_: e.matmul(B[6][:, 256:512][:, cs_], lhsT=Mt[mi][:, 256:384], rhs=xdt[:, 1, hs_], start=False, stop=True),
                         reads=[d_Mt[mi], d_xdt], writes=[d_B[6]])
                gs_ = slice(g * 256, (g + 1) * 256)
                for it in range(2):
                    P.op("pe", lambda e, g=g, it=it: e.matmul(B[0][:, it * 256:(it + 1) * 256], lhsT=xc[:, 12 + g, it * 128:(it + 1) * 128], rhs=stT[:, g, :],
                                                             start=True, stop=True), reads=[d_xc[12 + g], d_stT], writes=[d_B[0]])
                for it in range(2):
                    v4 = lambda ap: ap.rearrange("p (r d) -> p r d", d=64)
                    P.op("dve", lambda e, g=g, it=it: e.tensor_tensor(out=v4(yt[:]), in0=v4(B[0][:, it * 256:(it + 1) * 256]),
                                                                      in1=eac[:, it, g * 4:(g + 1) * 4].unsqueeze(2).to_broadcast([128, 4, 64]), op=ALU.mult),
                         reads=[d_B[0], d_eac], writes=[d_yt])
                    P.op("dve", lambda e, it=it: e.tensor_tensor(out=yt[:], in0=yt[:], in1=B[6][:, it * 256:(it + 1) * 256], op=ALU.add),
                         reads=[d_yt, d_B[6]], writes=[d_yt])
                    P.op("pool", lambda e, g=g, it=it, gs_=gs_: e.tensor_tensor(out=v4(y2[:]), in0=v4(xtok[:, it, gs_]),
                                                                               in1=dsk[:, g * 4:(g + 1) * 4].unsqueeze(2).to_broadcast([128, 4, 64]), op=ALU.mult),
                         reads=[d_xtok, d_c], writes=[d_y2])
                    P.op("dve", lambda e: e.tensor_tensor(out=yt[:], in0=yt[:], in1=y2[:], op=ALU.add), reads=[d_yt, d_y2], writes=[d_yt])
                    P.op("dve", lambda e, it=it, gs_=gs_: e.tensor_tensor(out=yt[:], in0=yt[:], in1=zs[:, it, gs_], op=ALU.mult), reads=[d_yt, d_zs], writes=[d_yt])
                    P.op("act", lambda e: e.activation(out=y2[:], in_=yt[:], func=AF.Square, accum_out=gss[:]), reads=[d_yt, d_y2], writes=[d_y2, d_gss])
                    P.op("act", lambda e: e.activation(out=grs[:], in_=gss[:], func=AF.Sqrt, bias=EPS, scale=1.0 / 256), reads=[d_gss], writes=[d_grs])
                    P.op("dve", lambda e: e.reciprocal(out=grs[:], in_=grs[:]), reads=[d_grs], writes=[d_grs])
                    P.op("dve", lambda e, it=it, gs_=gs_: e.scalar_tensor_tensor(out=osb[:, it, gs_], in0=yt[:], scalar=grs[:, 0:1], in1=ssdn[:, gs_],
                                                                                op0=ALU.mult, op1=ALU.mult), reads=[d_yt, d_grs, d_c], writes=[d_osb])
            P.dma("sp", lambda e, r0=r0: e.dma_start(out=os_d[r0 - TP:r0 - TP + 256, :].rearrange("(t p) c -> p t c", p=128), in_=osb[:]),
                  reads=[d_osb], writes=[d_osd])
        for g in range(4):
            bank = B[2] if g < 2 else B[1]
            dbank = d_B[2] if g < 2 else d_B[1]
            cs_ = slice((g % 2) * 256, (g % 2 + 1) * 256)
            for jt in range(2):
                P.op("pe", lambda e, g=g, jt=jt, bank=bank, cs_=cs_: e.matmul(bank[:, cs_], lhsT=btok[:, jt, g * 128:(g + 1) * 128], rhs=xdd[:, jt, g * 256:(g + 1) * 256],
                                                                            start=(jt == 0), stop=(jt == 1)), reads=[d_btok, d_xdd], writes=[dbank])
        v16 = lambda ap: ap.rearrange("p (h d) -> p h d", d=64)
        P.op("dve", lambda e: e.tensor_tensor(out=v16(stf[:].rearrange("p g c -> p (g c)")), in0=v16(stf[:].rearrange("p g c -> p (g c)")),
                                              in1=cdec[:].unsqueeze(2).to_broadcast([128, 16, 64]), op=ALU.mult), reads=[d_stf, d_cdec], writes=[d_stf])
        P.op("dve", lambda e: e.tensor_tensor(out=stf[:, 0:2, :].rearrange("p g c -> p (g c)"), in0=stf[:, 0:2, :].rearrange("p g c -> p (g c)"), in1=B[2][:, :], op=ALU.add),
             reads=[d_stf, d_B[2]], writes=[d_stf])
        P.op("dve", lambda e: e.tensor_tensor(out=stf[:, 2:4, :].rearrange("p g c -> p (g c)"), in0=stf[:, 2:4, :].rearrange("p g c -> p (g c)"), in1=B[1][:, :], op=ALU.add),
             reads=[d_stf, d_B[1]], writes=[d_stf])
        if c == 15:
            P.op("dve", lambda e: e.tensor_scalar(out=stf[:], in0=stf[:], scalar1=pflag[:, 0:1], scalar2=None, op0=ALU.mult), reads=[d_stf, d_c], writes=[d_stf])
        P.op("act", lambda e: e.copy(out=stT[:], in_=stf[:]), reads=[d_stf], writes=[d_stT])


def host_consts_c(z, half):
    cwl = np.ascontiguousarray(z['conv_w'].reshape(4, 16, 128).transpose(2, 1, 0)).astype(np.float32)
    cbl = np.ascontiguousarray(z['conv_b'].reshape(16, 128).T).astype(np.float32)
    tri = (np.arange(128)[:, None] <= np.arange(128)[None, :]).astype(np.float32)
    tri2 = np.zeros((128, 2, 256), np.float32)
    tri2[:, 0, 0:128] = tri; tri2[:, 0, 128:256] = 1.0; tri2[:, 1, 128:256] = tri
    sel = np.zeros((16, 16, 128), np.float32)
    for h in range(16):
        sel[h, h, :] = 1.0
    tbias = np.where(tri > 0, 0.0, NEG).astype(np.float32)
    tb = np.zeros((128, 384), np.float32)
    tb[:, 0:128] = tbias; tb[:, 256:384] = tbias
    return dict(cwl=cwl, cbl=cbl, dtb=z['dt_bias'][None, :], alog=z['a_log'][None, :], dsk=z['d_skip'][None, :], ssdn=z['ssd_norm'][None, :],
                tri2=tri2, sel_d=sel, tb_d=tb, pflag=np.array([[1.0 if half == 1 else 0.0]], np.float32))


def phase_def(nc, P, xown, w_in, g_mix, mem_d, gmem_d, wmemkv, mqn_d, mkn_d, woa_d, wos_d, wom_d, wout_d, gffn_d, wr_d,
              wgate_d, wup_d, wdown_d, tri_d, ecap_d, oa_d, d_oad, os_d, d_osd, x1_d, xbuf, ybuf, out_d, idb, d_idb, idf, d_idf,
              ntiles=32, experts=range(32), stage=3):
    d_x1d = Dep(); d_xbuf = Dep(); d_ybuf = Dep(); d_out = Dep()
    with contextlib.ExitStack() as stp:
        def sbp(name, shape, dt):
            return stp.enter_context(nc.sbuf_tensor("d_" + name, shape, dt))
        combs = sbp("combs", [128, 32, 2], F32); d_combs = Dep()
        idxs = sbp("idxs", [128, 32, 2], I32); d_idxs = Dep()

        with contextlib.ExitStack() as st:
            def sb(name, shape, dt):
                return st.enter_context(nc.sbuf_tensor("d_" + name, shape, dt))

            def ps(name, shape, dt=F32):
                return st.enter_context(nc.psum_tensor(name, shape, dt))
            d_w = Dep(); d_c = Dep()
            gm = sb("gmd", [128, D], F32)
            gf = sb("gfd", [128, D], F32)
            mqn = sb("mqn", [128, 512], F32)
            mkn = sb("mkn", [128, 512], F32)
            tri = sb("trid", [128, 128], F32)
            onesf = sb("onesfd", [128, 128], F32)
            onesb = sb("onesbd", [128, 128], BF16)
            ecap = sb("ecap", [128, 32], F32)
            base = sb("base", [128, 32], F32); d_base = Dep()
            kmT = sb("kmT", [128, 4, 256], BF16); d_kmT = Dep()
            vm = sb("vm", [128, 2, 512], BF16); d_vm = Dep()

            xt = sb("xtd", [128, D], F32); d_xt = Dep()
            junk = sb("junkd", [128, D], BF16); d_junk = Dep()
            ssq = sb("ssqd", [128, 1], F32); d_ssq = Dep()
            rstd = sb("rstdd", [128, 1], F32); d_rstd = Dep()
            hb = sb("hbd", [128, D], BF16); d_hb = Dep()
            hT = sb("hTd", [128, 8, 128], BF16); d_hT = Dep()
            sq = sb("sqd", [128, 512], F32); d_sq = Dep()
            hs = sb("hsd", [128, 4], F32); d_hs = Dep()
            hr = sb("hrd", [128, 4], F32); d_hr = Dep()
            qmn = sb("qmn", [128, 512], F32); d_qmn = Dep()
            qmb = sb("qmb", [128, 512], BF16); d_qmb = Dep()
            qmT = sb("qmT", [128, 4, 128], BF16); d_qmT = Dep()
            pm = sb("pm", [128, 4, 2, 128], BF16); d_pm = Dep()
            rcp = sb("rcp", [128, 512], F32); d_rcp = Dep()
            omT = sb("omT", [128, 4, 128], BF16); d_omT = Dep()
            gs = sb("gs", [128, 3072], F32); d_gs = Dep()
            oat = sb("oat", [128, 512], BF16); d_oat = Dep()
            ost = sb("ost", [128, 1024], BF16); d_ost = Dep()
            oaT = sb("oaT", [128, 4, 128], BF16); d_oaT = Dep()
            osT = sb("osT", [128, 8, 128], BF16); d_osT = Dep()
            mg = sb("mg", [128, 512], F32); d_mg = Dep()
            tt = sb("tt", [128, 512], F32); d_tt = Dep()
            mgb = sb("mgb", [128, D], BF16); d_mgb = Dep()
            mgT = sb("mgT", [128, 8, 128], BF16); d_mgT = Dep()
            x1 = sb("x1", [128, D], F32); d_x1 = Dep()
            h2f = sb("h2f", [128, D], F32); d_h2f = Dep()
            h2b = [sb("h2b%d" % i, [128, D], BF16) for i in range(2)]; d_h2b = [Dep() for _ in range(2)]
            h2T = sb("h2T", [128, 8, 128], F32); d_h2T = Dep()
            lg = sb("lg", [128, 36], F32); d_lg = Dep()
            sm = sb("sm", [128, 64], F32); d_sm = Dep()
            goh = sb("goh", [128, 4], F32); d_goh = Dep()
            lsel = sb("lsel", [128, 32], F32); d_lsel = Dep()
            les = sb("les", [128, 8], F32); d_les = Dep()
            t8 = sb("t8", [128, 8], F32); d_t8 = Dep()
            oh = sb("oh", [128, 64], F32); d_oh = Dep()
            ohs = sb("ohs", [128, 32], F32); d_ohs = Dep()
            posf = sb("posf", [128, 32], F32); d_posf = Dep()
            idf2 = sb("idf2", [128, 2], F32); d_idf2 = Dep()

            pT = ps("pTd", [128, 8, 128], BF16); d_pT = Dep()
            pF = ps("pFd", [128, 512], F32); d_pF = Dep()
            pG = [ps("pGd%d" % i, [128, 512], F32) for i in range(3)]; d_pG = [Dep() for _ in range(3)]
            pms = [ps("pmsd%d" % i, [128, 512], F32) for i in range(2)]; d_pms = [Dep() for _ in range(2)]
            pmo = ps("pmod", [128, 512], F32); d_pmo = Dep()

            P.dma("sp", lambda e: e.dma_start(out=gm[:], in_=g_mix[0:1, :].partition_broadcast(128)), writes=[d_c])
            P.dma("sp", lambda e: e.dma_start(out=gf[:], in_=gffn_d[0:1, :].partition_broadcast(128)), writes=[d_c])
            P.dma("sp", lambda e: e.dma_start(out=mqn[:], in_=mqn_d[0:1, :].partition_broadcast(128)), writes=[d_c])
            P.dma("sp", lambda e: e.dma_start(out=mkn[:], in_=mkn_d[0:1, :].partition_broadcast(128)), writes=[d_c])
            P.dma("sp", lambda e: e.dma_start(out=tri[:], in_=tri_d[:, :]), writes=[d_c])
            P.dma("sp", lambda e: e.dma_start(out=ecap[:], in_=ecap_d[0:1, :].partition_broadcast(128)), writes=[d_c])
            P.op("pool", lambda e: e.memset(onesf[:], 1.0), writes=[d_c])
            P.op("pool", lambda e: e.memset(onesb[:], 1.0), writes=[d_c])
            P.op("pool", lambda e: e.memset(base[:], 0.0), writes=[d_base])

            def head_norm(src_ps, d_src, gain, nh, dst, d_dst):
                hd = 512 // nh
                v = lambda ap: ap.rearrange("p (h d) -> p h d", h=nh)
                P.op("act", lambda e: e.activation(out=sq[:], in_=src_ps, func=AF.Square), reads=[d_src], writes=[d_sq])
                P.op("dve", lambda e: e.tensor_reduce(out=hs[:, 0:nh], in_=v(sq[:]), axis=AX.X, op=ALU.add), reads=[d_sq], writes=[d_hs])
                P.op("act", lambda e: e.activation(out=hr[:, 0:nh], in_=hs[:, 0:nh], func=AF.Sqrt, bias=EPS, scale=1.0 / hd), reads=[d_hs], writes=[d_hr])
                P.op("dve", lambda e: e.reciprocal(out=hr[:, 0:nh], in_=hr[:, 0:nh]), reads=[d_hr], writes=[d_hr])
                P.op("dve", lambda e: e.tensor_tensor(out=v(qmn[:]), in0=v(src_ps), in1=hr[:, 0:nh].unsqueeze(2).to_broadcast([128, nh, hd]), op=ALU.mult),
                     reads=[d_src, d_hr], writes=[d_qmn])
                P.op("pool", lambda e: e.tensor_tensor(out=dst, in0=qmn[:], in1=gain[:], op=ALU.mult), reads=[d_qmn, d_c], writes=[d_dst])

            with contextlib.ExitStack() as stm:
                wkv = stm.enter_context(nc.sbuf_tensor("s_wkv", [128, 8, 1024], BF16)); d_wkv = Dep()
                mt_ = stm.enter_context(nc.sbuf_tensor("s_memt", [128, 2, D], F32)); d_mt = Dep()
                gme = stm.enter_context(nc.sbuf_tensor("s_gme", [128, D], F32)); d_gme = Dep()
                mhb = stm.enter_context(nc.sbuf_tensor("s_mhb", [128, 2, D], BF16)); d_mhb = Dep()
                mT = stm.enter_context(nc.sbuf_tensor("s_mT", [128, 8, 256], BF16)); d_mT = Dep()
                ss2 = stm.enter_context(nc.sbuf_tensor("s_ss2", [128, 2], F32)); d_ss2 = Dep()
                for k in range(8):
                    P.dma("pool", lambda e, k=k: e.dma_start(out=wkv[:, k, :], in_=wmemkv[k * 128:(k + 1) * 128, :]), writes=[d_wkv])
                P.dma("sp", lambda e: e.dma_start(out=mt_[:], in_=mem_d[:, :].rearrange("(t p) d -> p t d", p=128)), writes=[d_mt])
                P.dma("sp", lambda e: e.dma_start(out=gme[:], in_=gmem_d[0:1, :].partition_broadcast(128)), writes=[d_gme])
                for it in range(2):
                    P.op("act", lambda e, it=it: e.activation(out=junk[:], in_=mt_[:, it, :], func=AF.Square, accum_out=ss2[:, it:it + 1]),
                         reads=[d_mt], writes=[d_junk, d_ss2])
                P.op("act", lambda e: e.activation(out=ss2[:], in_=ss2[:], func=AF.Sqrt, bias=EPS, scale=1.0 / D), reads=[d_ss2], writes=[d_ss2])
                P.op("dve", lambda e: e.reciprocal(out=ss2[:], in_=ss2[:]), reads=[d_ss2], writes=[d_ss2])
                for it in range(2):
                    P.op("dve", lambda e, it=it: e.scalar_tensor_tensor(out=mhb[:, it, :], in0=mt_[:, it, :], scalar=ss2[:, it:it + 1], in1=gme[:],
                                                                        op0=ALU.mult, op1=ALU.mult), reads=[d_mt, d_ss2, d_gme], writes=[d_mhb])
                    for k in range(8):
                        P.op("pe", lambda e, it=it, k=k: e.transpose(out=pT[:, k, :], in_=mhb[:, it, k * 128:(k + 1) * 128], identity=idb[:]),
                             reads=[d_mhb, d_idb], writes=[d_pT])
                    P.op("act", lambda e, it=it: e.copy(out=mT[:, :, it * 128:(it + 1) * 128], in_=pT[:]), reads=[d_pT], writes=[d_mT])
                for it in range(2):
                    for hv in range(2):
                        for k in range(8):
                            P.op("pe", lambda e, it=it, hv=hv, k=k: e.matmul(pG[hv][:, :], lhsT=mT[:, k, it * 128:(it + 1) * 128], rhs=wkv[:, k, hv * 512:(hv + 1) * 512],
                                                                           start=(k == 0), stop=(k == 7)), reads=[d_mT, d_wkv], writes=[d_pG[hv]])
                    head_norm(pG[0][:, :], d_pG[0], mkn, 4, qmb[:], d_qmb)
                    for h in range(4):
                        P.op("pe", lambda e, h=h: e.transpose(out=pT[:, h, :], in_=qmb[:, h * 128:(h + 1) * 128], identity=idb[:]),
                             reads=[d_qmb, d_idb], writes=[d_pT])
                    P.op("act", lambda e, it=it: e.copy(out=kmT[:, :, it * 128:(it + 1) * 128], in_=pT[:, 0:4, :]), reads=[d_pT], writes=[d_kmT])
                    P.op("act", lambda e, it=it: e.copy(out=vm[:, it, :], in_=pG[1][:, :]), reads=[d_pG[1]], writes=[d_vm])

            wqm = sb("wqm", [128, 8, 512], BF16)
            wg = sb("wg", [128, 8, 3072], BF16)
            woa = sb("woa", [128, 4, 1024], BF16)
            wos = sb("wos", [128, 8, 1024], BF16)
            wom = sb("wom", [128, 4, 1024], BF16)
            wout = sb("wout", [128, 8, 1024], BF16)
            wr = sb("wr", [128, 8, 36], F32)
            for k in range(8):
                rows = slice(k * 128, (k + 1) * 128)
                P.dma("pool", lambda e, k=k, rows=rows: e.dma_start(out=wqm[:, k, :], in_=w_in[rows, C_QM:C_QM + 512]), writes=[d_w])
                for j in range(2):
                    P.dma("pool", lambda e, k=k, rows=rows, j=j: e.dma_start(out=wg[:, k, j * 1536:(j + 1) * 1536], in_=w_in[rows, C_G + j * 1536:C_G + (j + 1) * 1536]), writes=[d_w])
                P.dma("pool", lambda e, k=k, rows=rows: e.dma_start(out=wos[:, k, :], in_=wos_d[rows, :]), writes=[d_w])
                P.dma("pool", lambda e, k=k, rows=rows: e.dma_start(out=wout[:, k, :], in_=wout_d[rows, :]), writes=[d_w])
                P.dma("sp", lambda e, k=k, rows=rows: e.dma_start(out=wr[:, k, :], in_=wr_d[rows, :]), writes=[d_w])
            for k in range(4):
                rows = slice(k * 128, (k + 1) * 128)
                P.dma("pool", lambda e, k=k, rows=rows: e.dma_start(out=woa[:, k, :], in_=woa_d[rows, :]), writes=[d_w])
                P.dma("pool", lambda e, k=k, rows=rows: e.dma_start(out=wom[:, k, :], in_=wom_d[rows, :]), writes=[d_w])
            for ti in range(ntiles):
                r0 = ti * 128
                hbuf = ti % 2
                P.dma("sp", lambda e, r0=r0: e.dma_start(out=xt[:], in_=xown[r0:r0 + 128, :]), writes=[d_xt])
                P.dma("sp", lambda e, r0=r0: e.dma_start(out=oat[:], in_=oa_d[r0:r0 + 128, :]), reads=[d_oad], writes=[d_oat])
                P.dma("sp", lambda e, r0=r0: e.dma_start(out=ost[:], in_=os_d[r0:r0 + 128, :]), reads=[d_osd], writes=[d_ost])
                P.op("act", lambda e: e.activation(out=junk[:], in_=xt[:], func=AF.Square, accum_out=ssq[:]), reads=[d_xt], writes=[d_junk, d_ssq])
                P.op("act", lambda e: e.activation(out=rstd[:], in_=ssq[:], func=AF.Sqrt, bias=EPS, scale=1.0 / D), reads=[d_ssq], writes=[d_rstd])
                P.op("dve", lambda e: e.reciprocal(out=rstd[:], in_=rstd[:]), reads=[d_rstd], writes=[d_rstd])
                P.op("dve", lambda e: e.scalar_tensor_tensor(out=hb[:], in0=xt[:], scalar=rstd[:, 0:1], in1=gm[:], op0=ALU.mult, op1=ALU.mult),
                     reads=[d_xt, d_rstd, d_c], writes=[d_hb])
                for k in range(8):
                    P.op("pe", lambda e, k=k: e.transpose(out=pT[:, k, :], in_=hb[:, k * 128:(k + 1) * 128], identity=idb[:]), reads=[d_hb, d_idb], writes=[d_pT])
                P.op("act", lambda e: e.copy(out=hT[:], in_=pT[:]), reads=[d_pT], writes=[d_hT])
                for k in range(8):
                    P.op("pe", lambda e, k=k: e.matmul(pG[0][:, :], lhsT=hT[:, k, :], rhs=wqm[:, k, :], start=(k == 0), stop=(k == 7)),
                         reads=[d_hT, d_w], writes=[d_pG[0]])
                head_norm(pG[0][:, :], d_pG[0], mqn, 4, qmb[:], d_qmb)
                for h in range(4):
                    P.op("pe", lambda e, h=h: e.transpose(out=pT[:, h, :], in_=qmb[:, h * 128:(h + 1) * 128], identity=idb[:]), reads=[d_qmb, d_idb], writes=[d_pT])
                P.op("act", lambda e: e.copy(out=qmT[:], in_=pT[:, 0:4, :]), reads=[d_pT], writes=[d_qmT])
                for h in range(4):
                    for mt in range(2):
                        bank = pms[h // 2]
                        c0 = ((h % 2) * 2 + mt) * 128
                        P.op("pe", lambda e, h=h, mt=mt, bank=bank, c0=c0: e.matmul(bank[:, c0:c0 + 128], lhsT=kmT[:, h, mt * 128:(mt + 1) * 128], rhs=qmT[:, h, :],
                                                                                  start=True, stop=True), reads=[d_kmT, d_qmT], writes=[d_pms[h // 2]])
                for hp in range(2):
                    P.op("act", lambda e, hp=hp: e.activation(out=pm[:, hp * 2:(hp + 1) * 2, :, :].rearrange("p a b c -> p (a b c)"), in_=pms[hp][:, :], func=AF.Exp,
                                                              scale=float(128 ** -0.5)), reads=[d_pms[hp]], writes=[d_pm])
                for h in range(4):
                    for mt in range(2):
                        P.op("pe", lambda e, h=h, mt=mt: e.matmul(pmo[:, h * 128:(h + 1) * 128], lhsT=vm[:, mt, h * 128:(h + 1) * 128], rhs=pm[:, h, mt, :],
                                                                 start=(mt == 0), stop=(mt == 1)), reads=[d_vm, d_pm], writes=[d_pmo])
                for mt in range(2):
                    P.op("pe", lambda e, mt=mt: e.matmul(pG[1][:, :].rearrange("p (h t) -> p h t", h=4), lhsT=onesb[:], rhs=pm[:, :, mt, :],
                                                        start=(mt == 0), stop=(mt == 1)), reads=[d_c, d_pm], writes=[d_pG[1]])
                P.op("dve", lambda e: e.reciprocal(out=rcp[:], in_=pG[1][:, :]), reads=[d_pG[1]], writes=[d_rcp])
                P.op("dve", lambda e: e.tensor_tensor(out=omT[:].rearrange("p h t -> p (h t)"), in0=pmo[:, :], in1=rcp[:], op=ALU.mult),
                     reads=[d_pmo, d_rcp], writes=[d_omT])
                for g6 in range(6):
                    bk = g6 % 3
                    for k in range(8):
                        P.op("pe", lambda e, g6=g6, k=k, bk=bk: e.matmul(pG[bk][:, :], lhsT=hT[:, k, :], rhs=wg[:, k, g6 * 512:(g6 + 1) * 512], start=(k == 0), stop=(k == 7)),
                             reads=[d_hT, d_w], writes=[d_pG[bk]])
                    P.op("act", lambda e, g6=g6, bk=bk: e.activation(out=gs[:, g6 * 512:(g6 + 1) * 512], in_=pG[bk][:, :], func=AF.Sigmoid),
                         reads=[d_pG[bk]], writes=[d_gs])
                for k in range(4):
                    P.op("pe", lambda e, k=k: e.transpose(out=pT[:, k, :], in_=oat[:, k * 128:(k + 1) * 128], identity=idb[:]), reads=[d_oat, d_idb], writes=[d_pT])
                P.op("act", lambda e: e.copy(out=oaT[:], in_=pT[:, 0:4, :]), reads=[d_pT], writes=[d_oaT])
                for k in range(8):
                    P.op("pe", lambda e, k=k: e.transpose(out=pT[:, k, :], in_=ost[:, k * 128:(k + 1) * 128], identity=idb[:]), reads=[d_ost, d_idb], writes=[d_pT])
                P.op("act", lambda e: e.copy(out=osT[:], in_=pT[:]), reads=[d_pT], writes=[d_osT])
                for hv in range(2):
                    cs_ = slice(hv * 512, (hv + 1) * 512)
                    for k in range(4):
                        P.op("pe", lambda e, k=k, cs_=cs_: e.matmul(pG[0][:, :], lhsT=oaT[:, k, :], rhs=woa[:, k, cs_], start=(k == 0), stop=(k == 3)),
                             reads=[d_oaT, d_w], writes=[d_pG[0]])
                    for k in range(8):
                        P.op("pe", lambda e, k=k, cs_=cs_: e.matmul(pG[1][:, :], lhsT=osT[:, k, :], rhs=wos[:, k, cs_], start=(k == 0), stop=(k == 7)),
                             reads=[d_osT, d_w], writes=[d_pG[1]])
                    for k in range(4):
                        P.op("pe", lambda e, k=k, cs_=cs_: e.matmul(pG[2][:, :], lhsT=omT[:, k, :], rhs=wom[:, k, cs_], start=(k == 0), stop=(k == 3)),
                             reads=[d_omT, d_w], writes=[d_pG[2]])
                    P.op("dve", lambda e, hv=hv: e.tensor_tensor(out=mg[:], in0=pG[0][:, :], in1=gs[:, hv * 512:(hv + 1) * 512], op=ALU.mult),
                         reads=[d_pG[0], d_gs], writes=[d_mg])
                    P.op("dve", lambda e, hv=hv: e.tensor_tensor(out=tt[:], in0=pG[1][:, :], in1=gs[:, 1024 + hv * 512:1024 + (hv + 1) * 512], op=ALU.mult),
                         reads=[d_pG[1], d_gs], writes=[d_tt])
                    P.op("pool", lambda e: e.tensor_tensor(out=mg[:], in0=mg[:], in1=tt[:], op=ALU.add), reads=[d_mg, d_tt], writes=[d_mg])
                    P.op("dve", lambda e, hv=hv: e.tensor_tensor(out=tt[:], in0=pG[2][:, :], in1=gs[:, 2048 + hv * 512:2048 + (hv + 1) * 512], op=ALU.mult),
                         reads=[d_pG[2], d_gs], writes=[d_tt])
                    P.op("pool", lambda e, cs_=cs_: e.tensor_tensor(out=mgb[:, cs_], in0=mg[:], in1=tt[:], op=ALU.add), reads=[d_mg, d_tt], writes=[d_mgb])
                for k in range(8):
                    P.op("pe", lambda e, k=k: e.transpose(out=pT[:, k, :], in_=mgb[:, k * 128:(k + 1) * 128], identity=idb[:]), reads=[d_mgb, d_idb], writes=[d_pT])
                P.op("act", lambda e: e.copy(out=mgT[:], in_=pT[:]), reads=[d_pT], writes=[d_mgT])
                for hv in range(2):
                    cs_ = slice(hv * 512, (hv + 1) * 512)
                    for k in range(8):
                        P.op("pe", lambda e, k=k, cs_=cs_, hv=hv: e.matmul(pG[hv][:, :], lhsT=mgT[:, k, :], rhs=wout[:, k, cs_], start=(k == 0), stop=(k == 7)),
                             reads=[d_mgT, d_w], writes=[d_pG[hv]])
                    P.op("dve", lambda e, cs_=cs_, hv=hv: e.tensor_tensor(out=x1[:, cs_], in0=pG[hv][:, :], in1=xt[:, cs_], op=ALU.add),
                         reads=[d_pG[hv], d_xt], writes=[d_x1])
                P.dma("sp", lambda e, r0=r0: e.dma_start(out=x1_d[r0:r0 + 128, :], in_=x1[:]), reads=[d_x1], writes=[d_x1d])
                if stage < 2:
                    continue
                P.op("act", lambda e: e.activation(out=junk[:], in_=x1[:], func=AF.Square, accum_out=ssq[:]), reads=[d_x1], writes=[d_junk, d_ssq])
                P.op("act", lambda e: e.activation(out=rstd[:], in_=ssq[:], func=AF.Sqrt, bias=EPS, scale=1.0 / D), reads=[d_ssq], writes=[d_rstd])
                P.op("dve", lambda e: e.reciprocal(out=rstd[:], in_=rstd[:]), reads=[d_rstd], writes=[d_rstd])
                P.op("dve", lambda e: e.scalar_tensor_tensor(out=h2f[:], in0=x1[:], scalar=rstd[:, 0:1], in1=gf[:], op0=ALU.mult, op1=ALU.mult),
                     reads=[d_x1, d_rstd, d_c], writes=[d_h2f])
                P.op("pool", lambda e, hbuf=hbuf: e.tensor_copy(out=h2b[hbuf][:], in_=h2f[:]), reads=[d_h2f], writes=[d_h2b[hbuf]])
                for half in range(2):
                    for k4 in range(4):
                        k = half * 4 + k4
                        P.op("pe", lambda e, k=k, k4=k4: e.transpose(out=pF[:, k4 * 128:(k4 + 1) * 128], in_=h2f[:, k * 128:(k + 1) * 128], identity=idf[:]),
                             reads=[d_h2f, d_idf], writes=[d_pF])
                    P.op("act", lambda e, half=half: e.copy(out=h2T[:, half * 4:(half + 1) * 4, :].rearrange("p a b -> p (a b)"), in_=pF[:, :]), reads=[d_pF], writes=[d_h2T])
                for k in range(8):
                    P.op("pe", lambda e, k=k: e.matmul(pF[:, 0:36], lhsT=h2T[:, k, :], rhs=wr[:, k, :], start=(k == 0), stop=(k == 7)), reads=[d_h2T, d_w], writes=[d_pF])
                P.op("act", lambda e: e.copy(out=lg[:], in_=pF[:, 0:36]), reads=[d_pF], writes=[d_lg])
                P.op("dve", lambda e: e.tensor_reduce(out=sm[:, 0:1], in_=lg[:, 0:4], axis=AX.X, op=ALU.max), reads=[d_lg], writes=[d_sm])
                P.op("dve", lambda e: e.tensor_scalar(out=sm[:, 1:2], in0=sm[:, 0:1], scalar1=-1.0, scalar2=None, op0=ALU.mult), reads=[d_sm], writes=[d_sm])
                P.op("act", lambda e: e.activation(out=sm[:, 8:12], in_=lg[:, 0:4], func=AF.Exp, bias=sm[:, 1:2], accum_out=sm[:, 2:3]), reads=[d_lg, d_sm], writes=[d_sm])
                P.op("dve", lambda e: e.reciprocal(out=sm[:, 3:4], in_=sm[:, 2:3]), reads=[d_sm], writes=[d_sm])
                P.op("dve", lambda e: e.tensor_scalar(out=goh[:], in0=lg[:, 0:4], scalar1=sm[:, 0:1], scalar2=None, op0=ALU.is_ge), reads=[d_lg, d_sm], writes=[d_goh])
                P.op("dve", lambda e: e.tensor_tensor(out=lsel[:].rearrange("p (g e) -> p g e", g=4), in0=lg[:, 4:36].rearrange("p (g e) -> p g e", g=4),
                                                      in1=goh[:].unsqueeze(2).to_broadcast([128, 4, 8]), op=ALU.mult), reads=[d_lg, d_goh], writes=[d_lsel])
                P.op("dve", lambda e: e.tensor_reduce(out=les[:], in_=lsel[:].rearrange("p (g e) -> p e g", g=4), axis=AX.X, op=ALU.add), reads=[d_lsel], writes=[d_les])
                P.op("dve", lambda e: e.max(out=t8[:], in_=les[:]), reads=[d_les], writes=[d_t8])
                P.op("dve", lambda e: e.tensor_tensor(out=sm[:, 4:5], in0=t8[:, 1:2], in1=t8[:, 0:1], op=ALU.subtract), reads=[d_t8, d_sm], writes=[d_sm])
                P.op("act", lambda e: e.activation(out=sm[:, 5:6], in_=sm[:, 4:5], func=AF.Exp), reads=[d_sm], writes=[d_sm])
                P.op("dve", lambda e: e.tensor_scalar(out=sm[:, 6:7], in0=sm[:, 5:6], scalar1=1.0, scalar2=None, op0=ALU.add), reads=[d_sm], writes=[d_sm])
                P.op("dve", lambda e: e.reciprocal(out=sm[:, 6:7], in_=sm[:, 6:7]), reads=[d_sm], writes=[d_sm])
                P.op("dve", lambda e: e.tensor_tensor(out=sm[:, 7:8], in0=sm[:, 5:6], in1=sm[:, 6:7], op=ALU.mult), reads=[d_sm], writes=[d_sm])
                P.op("dve", lambda e, ti=ti: e.tensor_scalar(out=combs[:, ti, :], in0=sm[:, 6:8], scalar1=sm[:, 3:4], scalar2=None, op0=ALU.mult), reads=[d_sm], writes=[d_combs])
                for j in range(2):
                    P.op("dve", lambda e, j=j: e.tensor_scalar(out=oh[:, j * 32:(j + 1) * 32], in0=lg[:, 4:36], scalar1=t8[:, j:j + 1], scalar2=None, op0=ALU.is_equal),
                         reads=[d_lg, d_t8], writes=[d_oh])
                    P.op("dve", lambda e, j=j: e.tensor_tensor(out=oh[:, j * 32:(j + 1) * 32].rearrange("p (g e) -> p g e", g=4), in0=oh[:, j * 32:(j + 1) * 32].rearrange("p (g e) -> p g e", g=4),
                                                               in1=goh[:].unsqueeze(2).to_broadcast([128, 4, 8]), op=ALU.mult), reads=[d_oh, d_goh], writes=[d_oh])
                P.op("dve", lambda e: e.tensor_tensor(out=ohs[:], in0=oh[:, 0:32], in1=oh[:, 32:64], op=ALU.add), reads=[d_oh], writes=[d_ohs])
                P.op("pe", lambda e: e.matmul(pF[:, 64:96], lhsT=tri[:], rhs=ohs[:], start=True, stop=True), reads=[d_c, d_ohs], writes=[d_pF])
                P.op("pe", lambda e: e.matmul(pF[:, 128:160], lhsT=onesf[:], rhs=ohs[:], start=True, stop=True), reads=[d_c, d_ohs], writes=[d_pF])
                P.op("dve", lambda e: e.tensor_tensor(out=posf[:], in0=pF[:, 64:96], in1=ohs[:], op=ALU.subtract), reads=[d_pF, d_ohs], writes=[d_posf])
                P.op("dve", lambda e: e.tensor_tensor(out=posf[:], in0=posf[:], in1=base[:], op=ALU.add), reads=[d_posf, d_base], writes=[d_posf])
                P.op("dve", lambda e: e.tensor_tensor(out=posf[:], in0=posf[:], in1=ecap[:], op=ALU.add), reads=[d_posf, d_c], writes=[d_posf])
                P.op("dve", lambda e: e.tensor_tensor(out=base[:], in0=base[:], in1=pF[:, 128:160], op=ALU.add), reads=[d_base, d_pF, d_posf], writes=[d_base])
                for j in range(2):
                    P.op("dve", lambda e, j=j: e.tensor_tensor(out=oh[:, j * 32:(j + 1) * 32], in0=oh[:, j * 32:(j + 1) * 32], in1=posf[:], op=ALU.mult), reads=[d_oh, d_posf], writes=[d_oh])
                    P.op("dve", lambda e, j=j: e.tensor_reduce(out=idf2[:, j:j + 1], in_=oh[:, j * 32:(j + 1) * 32], axis=AX.X, op=ALU.add), reads=[d_oh], writes=[d_idf2])
                P.op("dve", lambda e, ti=ti: e.tensor_copy(out=idxs[:, ti, :], in_=idf2[:]), reads=[d_idf2], writes=[d_idxs])
                for j in range(2):
                    P.dma("pool", lambda e, ti=ti, j=j, hbuf=hbuf: e.indirect_dma_start(out=xbuf[:, :], out_offset=bass.IndirectOffsetOnAxis(ap=idxs[:, ti, j:j + 1], axis=0),
                                                                                      in_=h2b[hbuf][:], in_offset=None),
                          reads=[d_h2b[hbuf], d_idxs], writes=[d_xbuf])

        if stage < 3:
            return
        if hasattr(P, 'barrier'):
            P.barrier()
        with contextlib.ExitStack() as st:
            def sb(name, shape, dt):
                return st.enter_context(nc.sbuf_tensor("d_" + name, shape, dt))

            def ps(name, shape, dt=F32):
                return st.enter_context(nc.psum_tensor(name, shape, dt))
            NW = 2
            wge = [sb("wge%d" % i, [128, 8, 512], BF16) for i in range(NW)]; d_wge = [Dep() for _ in range(NW)]
            wue = [sb("wue%d" % i, [128, 8, 512], BF16) for i in range(NW)]; d_wue = [Dep() for _ in range(NW)]
            wde = [sb("wde%d" % i, [128, 4, 1024], BF16) for i in range(NW)]; d_wde = [Dep() for _ in range(NW)]
            xe = [sb("xe%d" % i, [128, 3, D], BF16) for i in range(NW)]; d_xe = [Dep() for _ in range(NW)]
            xeT = sb("xeT", [128, 8, CAP], BF16); d_xeT = Dep()
            sg = sb("sg", [128, CAP], F32); d_sg = Dep()
            hTe = sb("hTe", [128, 4, CAP], BF16); d_hTe = Dep()
            ye = [sb("ye%d" % i, [128, 3, D], F32) for i in range(NW)]; d_ye = [Dep() for _ in range(NW)]
            pT = ps("pTe", [128, 8, 128], BF16); d_pT = Dep()
            pg_ = [ps("pge%d" % i, [128, 512], F32) for i in range(2)]; d_pg = [Dep() for _ in range(2)]
            pu_ = [ps("pue%d" % i, [128, 512], F32) for i in range(2)]; d_pu = [Dep() for _ in range(2)]
            py_ = [ps("pye%d" % i, [128, 512], F32) for i in range(2)]; d_py = [Dep() for _ in range(2)]
            for ie, ex in enumerate(experts):
                b = ie % NW
                P.dma("pool", lambda e, b=b, ex=ex: e.dma_start(out=wge[b][:], in_=wgate_d[ex].rearrange("(k p) f -> p k f", p=128)), writes=[d_wge[b]])
                P.dma("pool", lambda e, b=b, ex=ex: e.dma_start(out=wue[b][:], in_=wup_d[ex].rearrange("(k p) f -> p k f", p=128)), writes=[d_wue[b]])
                P.dma("pool", lambda e, b=b, ex=ex: e.dma_start(out=wde[b][:], in_=wdown_d[ex].rearrange("(k p) f -> p k f", p=128)), writes=[d_wde[b]])
                P.dma("sp", lambda e, b=b, ex=ex: e.dma_start(out=xe[b][:], in_=xbuf[ex * CAP:(ex + 1) * CAP, :].rearrange("(s p) d -> p s d", p=128)),
                      reads=[d_xbuf], writes=[d_xe[b]])
                for s in range(3):
                    for k in range(8):
                        P.op("pe", lambda e, b=b, s=s, k=k: e.transpose(out=pT[:, k, :], in_=xe[b][:, s, k * 128:(k + 1) * 128], identity=idb[:]),
                             reads=[d_xe[b], d_idb], writes=[d_pT])
                    P.op("act", lambda e, s=s: e.copy(out=xeT[:, :, s * 128:(s + 1) * 128], in_=pT[:]), reads=[d_pT], writes=[d_xeT])
                for ft in range(4):
                    pb = ft % 2
                    for k in range(8):
                        P.op("pe", lambda e, b=b, ft=ft, k=k, pb=pb: e.matmul(pg_[pb][:, 0:CAP], lhsT=wge[b][:, k, ft * 128:(ft + 1) * 128], rhs=xeT[:, k, :],
                                                                            start=(k == 0), stop=(k == 7)), reads=[d_wge[b], d_xeT], writes=[d_pg[pb]])
                    for k in range(8):
                        P.op("pe", lambda e, b=b, ft=ft, k=k, pb=pb: e.matmul(pu_[pb][:, 0:CAP], lhsT=wue[b][:, k, ft * 128:(ft + 1) * 128], rhs=xeT[:, k, :],
                                                                            start=(k == 0), stop=(k == 7)), reads=[d_wue[b], d_xeT], writes=[d_pu[pb]])
                    P.op("act", lambda e, pb=pb: e.activation(out=sg[:], in_=pg_[pb][:, 0:CAP], func=AF.Silu), reads=[d_pg[pb]], writes=[d_sg])
                    P.op("dve", lambda e, ft=ft, pb=pb: e.tensor_tensor(out=hTe[:, ft, :], in0=sg[:], in1=pu_[pb][:, 0:CAP], op=ALU.mult),
                         reads=[d_sg, d_pu[pb]], writes=[d_hTe])
                for s in range(3):
                    for hv in range(2):
                        for ft in range(4):
                            P.op("pe", lambda e, b=b, s=s, hv=hv, ft=ft: e.matmul(py_[hv][:, :], lhsT=hTe[:, ft, s * 128:(s + 1) * 128], rhs=wde[b][:, ft, hv * 512:(hv + 1) * 512],
                                                                                start=(ft == 0), stop=(ft == 3)), reads=[d_hTe, d_wde[b]], writes=[d_py[hv]])
                        P.op("act" if hv == 0 else "dve", (lambda e, b=b, s=s, hv=hv: e.copy(out=ye[b][:, s, hv * 512:(hv + 1) * 512], in_=py_[hv][:, :])) if hv == 0 else
                             (lambda e, b=b, s=s, hv=hv: e.tensor_copy(out=ye[b][:, s, hv * 512:(hv + 1) * 512], in_=py_[hv][:, :])),
                             reads=[d_py[hv]], writes=[d_ye[b]])
                P.dma("sp", lambda e, b=b, ex=ex: e.dma_start(out=ybuf[ex * CAP:(ex + 1) * CAP, :].rearrange("(s p) d -> p s d", p=128), in_=ye[b][:]),
                      reads=[d_ye[b]], writes=[d_ybuf])

        if hasattr(P, 'barrier'):
            P.barrier()
        with contextlib.ExitStack() as st:
            def sb(name, shape, dt):
                return st.enter_context(nc.sbuf_tensor("d_" + name, shape, dt))
            NF = 2
            x1t = [sb("x1t%d" % i, [128, D], F32) for i in range(NF)]; d_x1t = [Dep() for _ in range(NF)]
            y1 = [sb("y1t%d" % i, [128, D], F32) for i in range(NF)]; d_y1 = [Dep() for _ in range(NF)]
            y2 = [sb("y2t%d" % i, [128, D], F32) for i in range(NF)]; d_y2 = [Dep() for _ in range(NF)]
            ot = [sb("ot%d" % i, [128, D], F32) for i in range(NF)]; d_ot = [Dep() for _ in range(NF)]
            for ti in range(ntiles):
                b = ti % NF
                r0 = ti * 128
                P.dma("sp", lambda e, b=b, r0=r0: e.dma_start(out=x1t[b][:], in_=x1_d[r0:r0 + 128, :]), reads=[d_x1d], writes=[d_x1t[b]])
                P.dma("pool", lambda e, b=b, ti=ti: e.indirect_dma_start(out=y1[b][:], out_offset=None, in_=ybuf[:, :],
                                                                        in_offset=bass.IndirectOffsetOnAxis(ap=idxs[:, ti, 0:1], axis=0)),
                      reads=[d_ybuf, d_idxs], writes=[d_y1[b]])
                P.dma("pool", lambda e, b=b, ti=ti: e.indirect_dma_start(out=y2[b][:], out_offset=None, in_=ybuf[:, :],
                                                                        in_offset=bass.IndirectOffsetOnAxis(ap=idxs[:, ti, 1:2], axis=0)),
                      reads=[d_ybuf, d_idxs], writes=[d_y2[b]])
                P.op("dve", lambda e, b=b, ti=ti: e.scalar_tensor_tensor(out=ot[b][:], in0=y1[b][:], scalar=combs[:, ti, 0:1], in1=x1t[b][:], op0=ALU.mult, op1=ALU.add),
                     reads=[d_y1[b], d_combs, d_x1t[b]], writes=[d_ot[b]])
                P.op("dve", lambda e, b=b, ti=ti: e.scalar_tensor_tensor(out=ot[b][:], in0=y2[b][:], scalar=combs[:, ti, 1:2], in1=ot[b][:], op0=ALU.mult, op1=ALU.add),
                     reads=[d_y2[b], d_combs, d_ot[b]], writes=[d_ot[b]])
                P.dma("sp", lambda e, b=b, r0=r0: e.dma_start(out=out_d[r0:r0 + 128, :], in_=ot[b][:]), reads=[d_ot[b]], writes=[d_out])


def host_consts_d(z):
    tri = (np.arange(128)[:, None] <= np.arange(128)[None, :]).astype(np.float32)
    return dict(mqn=np.tile(z['mem_q_norm'], 4)[None, :], mkn=np.tile(z['mem_k_norm'], 4)[None, :],
                w_r=np.ascontiguousarray(np.concatenate([z['w_router_group'], z['w_router_expert']], axis=1)),
                trif=tri, ecap=(np.arange(32, dtype=np.float32) * CAP)[None, :], ident=np.eye(128, dtype=np.float32))


def build_full():
    nc = bass.Bass("TRN2", target_bir_lowering=False)
    P = Prog(nc)

    def di(name, shape, dt=F32):
        return nc.dram_tensor(name, shape, dt, kind="ExternalInput")
    xin = di("xin", [TP + T, D]); w_in = di("w_in", [D, IN_COLS]); g_mix = di("g_mix", [1, D])
    qkn = di("qkn", [2, 512]); cs = di("cs", [TP + T, 64]); ident_d = di("ident", [128, 128])
    e_d = di("e_d", [32, NK], BF16); vq_d = di("vq", [1, 1024]); nb_d = di("nb", [1, 1024]); oq_d = di("oq", [1, 1024])
    tri_d = di("tri", [128, 128], BF16)
    cwl_d = di("cwl", [128, 16, 4]); cbl_d = di("cbl", [128, 16]); dtb_d = di("dtb", [1, 16]); alog_d = di("alog", [1, 16])
    dsk_d = di("dsk", [1, 16]); ssdn_d = di("ssdn", [1, D]); tri2_d = di("tri2", [128, 2, 256]); sel_d = di("sel_d", [16, 16, 128])
    tb_d = di("tb_d", [128, 384]); pflag_d = di("pflag", [1, 1])
    mem_d = di("mem", [256, D]); gmem_d = di("g_mem", [1, D]); wmemkv = di("w_mem_kv", [D, 1024])
    mqn_d = di("mqn", [1, 512]); mkn_d = di("mkn", [1, 512])
    woa_d = di("w_o_moba", [512, D]); wos_d = di("w_o_ssd", [D, D]); wom_d = di("w_o_mem", [512, D]); wout_d = di("w_out", [D, D])
    gffn_d = di("g_ffn", [1, D]); wr_d = di("w_r", [D, 36])
    wgate_d = di("w_gate", [NE, D, 512]); wup_d = di("w_up", [NE, D, 512]); wdown_d = di("w_down", [NE, 512, D])
    trif_d = di("trif", [128, 128]); ecap_d = di("ecap", [1, 32])
    out_d = nc.dram_tensor("out", [T, D], F32, kind="ExternalOutput")
    kT_d = nc.dram_tensor("kT_d", [512, NK], BF16)
    v_d = nc.dram_tensor("v_d", [NK, 512], BF16)
    qT_d = nc.dram_tensor("qT_d", [512, T], BF16)
    oa_d = nc.dram_tensor("oa_d", [T, 512], BF16)
    os_d = nc.dram_tensor("os_d", [T, D], BF16)
    x1_d = nc.dram_tensor("x1_d", [T, D], F32)
    xbuf = nc.dram_tensor("xbuf", [NE * CAP, D], BF16)
    ybuf = nc.dram_tensor("ybuf", [NE * CAP, D], F32)

    with contextlib.ExitStack() as st0:
        idf = st0.enter_context(nc.sbuf_tensor("idf", [128, 128], F32)); d_idf = Dep()
        idb = st0.enter_context(nc.sbuf_tensor("idb", [128, 128], BF16)); d_idb = Dep()
        P.dma("sp", lambda e: e.dma_start(out=idf[:], in_=ident_d[:, :]), writes=[d_idf])
        P.op("dve", lambda e: e.tensor_copy(out=idb[:], in_=idf[:]), reads=[d_idf], writes=[d_idb])
        with contextlib.ExitStack() as st:
            phase_a(nc, P, st, xin, w_in, g_mix, qkn, cs, idb, d_idb, kT_d, v_d, qT_d)
        P.barrier()
        with contextlib.ExitStack() as st:
            oa = st.enter_context(nc.sbuf_tensor("s_oa", [128, 32, 512], BF16)); d_oa = Dep()
            phase_b(nc, P, st, kT_d, v_d, qT_d, e_d, vq_d, nb_d, oq_d, tri_d, idb, d_idb, oa, d_oa)
            P.dma("sp", lambda e: e.dma_start(out=oa_d[:, :].rearrange("(t p) c -> p t c", p=128), in_=oa[:]), reads=[d_oa])
        P.barrier()
        with contextlib.ExitStack() as st:
            phase_c(nc, P, st, xin, w_in, g_mix, cwl_d, cbl_d, dtb_d, alog_d, dsk_d, ssdn_d, tri2_d, sel_d, tb_d, pflag_d,
                    idb, d_idb, os_d, Dep())
        P.barrier()
        phase_def(nc, P, xin[TP:TP + T, :], w_in, g_mix, mem_d, gmem_d, wmemkv, mqn_d, mkn_d, woa_d, wos_d, wom_d, wout_d, gffn_d, wr_d,
                  wgate_d, wup_d, wdown_d, trif_d, ecap_d, oa_d, Dep(), os_d, Dep(), x1_d, xbuf, ybuf, out_d, idb, d_idb, idf, d_idf)
        P.emit()
    return nc


_NC_CACHE = {}


def kernel(x, mem, g_mix, w_in, moba_q_norm, moba_k_norm, conv_w, conv_b, dt_bias, a_log, d_skip, ssd_norm, g_mem, w_mem_kv,
           mem_q_norm, mem_k_norm, w_o_moba, w_o_ssd, w_o_mem, w_out, g_ffn, w_router_group, w_router_expert, w_gate, w_up, w_down):
    f32 = np.float32
    A = lambda a: np.ascontiguousarray(np.asarray(a, dtype=f32))
    x = A(x); mem = A(mem)
    z = dict(conv_w=A(conv_w), conv_b=A(conv_b), dt_bias=A(dt_bias), a_log=A(a_log), d_skip=A(d_skip), ssd_norm=A(ssd_norm),
             mem_q_norm=A(mem_q_norm), mem_k_norm=A(mem_k_norm), w_router_group=A(w_router_group), w_router_expert=A(w_router_expert))
    if "nc" not in _NC_CACHE:
        _NC_CACHE["nc"] = build_full()
    nc = _NC_CACHE["nc"]
    pos = np.arange(2 * T, dtype=f32)
    inv = (10000.0 ** (-np.arange(32, dtype=f32) / 32)).astype(f32)
    ang = pos[:, None] * inv[None, :]
    cs_full = np.concatenate([np.cos(ang), np.sin(ang)], axis=1).astype(f32)
    qkn = np.stack([np.tile(A(moba_q_norm), 8), np.tile(A(moba_k_norm), 8)]).astype(f32)
    shared = dict(w_in=A(w_in), g_mix=A(g_mix)[None, :], qkn=qkn, ident=np.eye(128, dtype=f32),
                  g_mem=A(g_mem)[None, :], w_mem_kv=A(w_mem_kv), w_o_moba=A(w_o_moba), w_o_ssd=A(w_o_ssd), w_o_mem=A(w_o_mem),
                  w_out=A(w_out), g_ffn=A(g_ffn)[None, :], w_gate=A(w_gate), w_up=A(w_up), w_down=A(w_down))
    shared.update(host_consts_d(z))
    in_maps = []
    for c in range(8):
        bb, hh = c // 2, c % 2
        if hh == 0:
            xin = np.concatenate([np.zeros((TP, D), f32), x[bb, :T]], 0)
            csc = np.concatenate([cs_full[:TP], cs_full[:T]], 0)
        else:
            xin = x[bb]
            csc = cs_full
        m = dict(shared)
        m.update(xin=np.ascontiguousarray(xin), cs=np.ascontiguousarray(csc), mem=mem[bb])
        m.update(host_consts(hh))
        m.update(host_consts_c(z, hh))
        in_maps.append(m)
    res = run_bass_kernel_spmd(nc, in_maps, core_ids=list(range(8)))
    out = np.empty((4, 2 * T, D), f32)
    for c in range(8):
        bb, hh = c // 2, c % 2
        out[bb, hh * T:(hh + 1) * T] = np.asarray(res.results[c]["out"], dtype=f32)
    return out
```

```python
import contextlib
import numpy as np
import ml_dtypes
import concourse.bass as bass
import concourse.mybir as mybir
from concourse.bass_utils import run_bass_kernel_spmd

F32 = mybir.dt.float32
BF16 = mybir.dt.bfloat16
I32 = mybir.dt.int32
U32 = mybir.dt.uint32
AF = mybir.ActivationFunctionType
ALU = mybir.AluOpType
AX = mybir.AxisListType

T = 4096
TP = 4096
NK = TP + T
D = 1024
EPS = 1e-6
IN_COLS = 8208
BIGG = 30000.0
MB = 1000.0
NEG = -30000.0
C_Z, C_X, C_DT = 1536, 2560, 4608
C_QM, C_G = 4624, 5136
CAP = 384
NE = 32


class Dep:
    __slots__ = ("w", "r")

    def __init__(self):
        self.w = None
        self.r = {}


class Op:
    __slots__ = ("eng", "fn", "deps", "is_dma", "sig", "sigval", "dsem", "dval", "prev")

    def __init__(self, eng, fn, is_dma):
        self.eng = eng
        self.fn = fn
        self.is_dma = is_dma
        self.deps = []
        self.sig = False
        self.sigval = 0
        self.dsem = None
        self.dval = 0
        self.prev = None


class Prog:
    ENGS = ("pe", "act", "dve", "pool", "sp")

    def __init__(self, nc, ndma_sems=12):
        self.nc = nc
        self.ops = {e: [] for e in self.ENGS}
        self.ndma = {e: 0 for e in self.ENGS}
        self.dma_last = {}
        self.ndma_sems = ndma_sems
        self.all_dma = []

    def _add(self, o, reads, writes):
        deps = {}
        for t in reads:
            if t.w is not None:
                deps[id(t.w)] = t.w
        for t in writes:
            if t.w is not None:
                deps[id(t.w)] = t.w
            for r in t.r.values():
                deps[id(r)] = r
        for t in reads:
            key = id(o) if o.is_dma else o.eng
            t.r[key] = o
        for t in writes:
            t.w = o
            t.r = {}
        dl = []
        for d in deps.values():
            if d is o:
                continue
            if (not d.is_dma) and (not o.is_dma) and d.eng == "pe" and o.eng == "pe":
                continue
            dl.append(d)
            if not d.is_dma:
                d.sig = True
        o.deps = dl
        self.ops[o.eng].append(o)
        return o

    def op(self, eng, fn, reads=(), writes=()):
        return self._add(Op(eng, fn, False), reads, writes)

    def dma(self, eng, fn, reads=(), writes=()):
        o = Op(eng, fn, True)
        n = self.ndma[eng]
        self.ndma[eng] += 1
        slot = (eng, n % self.ndma_sems)
        o.dsem = slot
        o.prev = self.dma_last.get(slot)
        o.dval = (o.prev.dval if o.prev else 0) + 16
        self.dma_last[slot] = o
        self.all_dma.append(o)
        return self._add(o, reads, writes)


    def barrier(self):
        lasts = []
        for e in self.ENGS:
            for o in reversed(self.ops[e]):
                if (not o.is_dma) and o.fn is not None:
                    o.sig = True
                    lasts.append(o)
                    break
        dmas = list(self.dma_last.values())
        for e in self.ENGS:
            o = Op(e, None, False)
            o.deps = [d for d in lasts if d.eng != e] + dmas
            self.ops[e].append(o)

    def emit(self):
        nc = self.nc
        import contextlib
        with contextlib.ExitStack() as st:
            esem = {e: st.enter_context(nc.semaphore("S_" + e)) for e in self.ENGS}
            dsem = {}
            for e in self.ENGS:
                if self.ndma[e]:
                    for i in range(min(self.ndma_sems, self.ndma[e])):
                        dsem[(e, i)] = st.enter_context(nc.semaphore("D_%s_%d" % (e, i)))
            for e in self.ENGS:
                c = 0
                for o in self.ops[e]:
                    if (not o.is_dma) and o.sig and o.fn is not None:
                        c += 1
                        o.sigval = c
            block = st.enter_context(nc.Block())
            handles = {"pe": block.tensor, "act": block.scalar, "dve": block.vector,
                       "pool": block.gpsimd, "sp": block.sync}

            def make(e):
                def body(eng):
                    known = {}

                    def wait(sem, key, val):
                        if known.get(key, 0) < val:
                            eng.wait_ge(sem, val)
                            known[key] = val
                    for o in self.ops[e]:
                        for d in o.deps:
                            if d.is_dma:
                                wait(dsem[d.dsem], d.dsem, d.dval)
                            else:
                                wait(esem[d.eng], d.eng, d.sigval)
                        if o.is_dma:
                            if o.prev is not None:
                                wait(dsem[o.dsem], o.dsem, o.prev.dval)
                            o.fn(eng).then_inc(dsem[o.dsem], 16)
                        elif o.fn is not None:
                            ins = o.fn(eng)
                            if o.sig:
                                ins.then_inc(esem[e], 1)
                    if e == "sp":
                        for slot, o in self.dma_last.items():
                            wait(dsem[slot], slot, o.dval)
                return body
            for e in self.ENGS:
                if self.ops[e] or e == "sp":
                    handles[e](make(e))


def phase_a(nc, P, st, xin, w_in, g_mix, qkn, cs, idb, d_idb, kT_d, v_d, qT_d):
    def sb(name, shape, dt):
        return st.enter_context(nc.sbuf_tensor("a_" + name, shape, dt))

    def ps(name, shape, dt=F32):
        return st.enter_context(nc.psum_tensor("a_" + name, shape, dt))
    wq = sb("wq", [128, 8, 1536], BF16); d_wq = Dep()
    gm = sb("gm", [128, D], F32); d_gm = Dep()
    gq = sb("gq", [128, 2, 512], F32); d_gq = Dep()
    NB = 2
    xt = [sb("xt%d" % i, [128, D], F32) for i in range(NB)]; d_xt = [Dep() for _ in range(NB)]
    cst = [sb("cst%d" % i, [128, 64], F32) for i in range(NB)]; d_cst = [Dep() for _ in range(NB)]
    junk = sb("junk", [128, D], BF16); d_junk = Dep()
    ssq = sb("ssq", [128, 1], F32); d_ssq = Dep()
    rstd = sb("rstd", [128, 1], F32); d_rstd = Dep()
    hb = sb("hb", [128, D], BF16); d_hb = Dep()
    hT = [sb("hT%d" % i, [128, 8, 128], BF16) for i in range(NB)]; d_hT = [Dep() for _ in range(NB)]
    pT = ps("pT", [128, 8, 128], BF16); d_pT = Dep()
    pq = [[ps("pq%d_%d" % (j, i), [128, 512], F32) for i in range(3)] for j in range(2)]; d_pq = [[Dep() for _ in range(3)] for _ in range(2)]
    pkT = ps("pkT", [128, 4, 128], BF16); d_pkT = Dep()
    sq = sb("sq", [128, 512], F32); d_sq = Dep()
    hs = sb("hs", [128, 8], F32); d_hs = Dep()
    hr = sb("hr", [128, 8], F32); d_hr = Dep()
    qn = sb("qn", [128, 512], F32); d_qn = Dep()
    t1 = sb("t1", [128, 256], F32); d_t1 = Dep()
    t2 = sb("t2", [128, 256], F32); d_t2 = Dep()
    t3 = sb("t3", [128, 256], F32); d_t3 = Dep()
    t4 = sb("t4", [128, 256], F32); d_t4 = Dep()
    qr = sb("qr", [128, 512], BF16); d_qr = Dep()
    kTs = [sb("kTs%d" % i, [128, 4, 128], BF16) for i in range(NB)]; d_kTs = [Dep() for _ in range(NB)]
    vs = [sb("vs%d" % i, [128, 512], BF16) for i in range(NB)]; d_vs = [Dep() for _ in range(NB)]

    for k in range(8):
        P.dma("pool", lambda e, k=k: e.dma_start(out=wq[:, k, :], in_=w_in[k * 128:(k + 1) * 128, 0:1536]), writes=[d_wq])
    P.dma("sp", lambda e: e.dma_start(out=gm[:], in_=g_mix[0:1, :].partition_broadcast(128)), writes=[d_gm])
    for i in range(2):
        P.dma("sp", lambda e, i=i: e.dma_start(out=gq[:, i, :], in_=qkn[i:i + 1, :].partition_broadcast(128)), writes=[d_gq])
    P.op("dve", lambda e: e.tensor_scalar(out=gq[:, 0, :], in0=gq[:, 0, :], scalar1=0.125, scalar2=None, op0=ALU.mult),
         reads=[d_gq], writes=[d_gq])

    def qk_post(src_ps, d_src, which, b):
        P.op("act", lambda e: e.activation(out=sq[:], in_=src_ps[:], func=AF.Square), reads=[d_src], writes=[d_sq])
        P.op("dve", lambda e: e.tensor_reduce(out=hs[:], in_=sq[:].rearrange("p (h d) -> p h d", h=8), axis=AX.X, op=ALU.add),
             reads=[d_sq], writes=[d_hs])
        P.op("act", lambda e: e.activation(out=hr[:], in_=hs[:], func=AF.Sqrt, bias=EPS, scale=1.0 / 64), reads=[d_hs], writes=[d_hr])
        P.op("dve", lambda e: e.reciprocal(out=hr[:], in_=hr[:]), reads=[d_hr], writes=[d_hr])
        P.op("dve", lambda e: e.tensor_tensor(out=qn[:].rearrange("p (h d) -> p h d", h=8), in0=src_ps[:].rearrange("p (h d) -> p h d", h=8),
                                              in1=hr[:].unsqueeze(2).to_broadcast([128, 8, 64]), op=ALU.mult),
             reads=[d_src, d_hr], writes=[d_qn])
        P.op("pool", lambda e: e.tensor_tensor(out=qn[:], in0=qn[:], in1=gq[:, which, :], op=ALU.mult), reads=[d_qn, d_gq], writes=[d_qn])
        q3 = qn[:].rearrange("p (h d) -> p h d", h=8)
        q1 = q3[:, :, 0:32]
        q2 = q3[:, :, 32:64]
        cosb = cst[b][:, 0:32].unsqueeze(1).to_broadcast([128, 8, 32])
        sinb = cst[b][:, 32:64].unsqueeze(1).to_broadcast([128, 8, 32])
        v3 = lambda t: t[:].rearrange("p (h d) -> p h d", h=8)
        P.op("dve", lambda e: e.tensor_tensor(out=v3(t1), in0=q1, in1=cosb, op=ALU.mult), reads=[d_qn, d_cst[b]], writes=[d_t1])
        P.op("pool", lambda e: e.tensor_tensor(out=v3(t2), in0=q2, in1=sinb, op=ALU.mult), reads=[d_qn, d_cst[b]], writes=[d_t2])
        P.op("dve", lambda e: e.tensor_tensor(out=v3(t3), in0=q2, in1=cosb, op=ALU.mult), reads=[d_qn, d_cst[b]], writes=[d_t3])
        P.op("pool", lambda e: e.tensor_tensor(out=v3(t4), in0=q1, in1=sinb, op=ALU.mult), reads=[d_qn, d_cst[b]], writes=[d_t4])
        r3 = qr[:].rearrange("p (h d) -> p h d", h=8)
        P.op("dve", lambda e: e.tensor_tensor(out=r3[:, :, 0:32], in0=v3(t1), in1=v3(t2), op=ALU.subtract), reads=[d_t1, d_t2], writes=[d_qr])
        P.op("pool", lambda e: e.tensor_tensor(out=r3[:, :, 32:64], in0=v3(t3), in1=v3(t4), op=ALU.add), reads=[d_t3, d_t4], writes=[d_qr])

    def to_T_and_store(dst_dram, col0, b):
        for c in range(4):
            P.op("pe", lambda e, c=c: e.transpose(out=pkT[:, c, :], in_=qr[:, c * 128:(c + 1) * 128], identity=idb[:]),
                 reads=[d_qr, d_idb], writes=[d_pkT])
        P.op("act", lambda e: e.copy(out=kTs[b][:], in_=pkT[:]), reads=[d_pkT], writes=[d_kTs[b]])
        P.dma("sp", lambda e: e.dma_start(out=dst_dram[:, col0:col0 + 128].rearrange("(c p) t -> p c t", p=128), in_=kTs[b][:]),
              reads=[d_kTs[b]])

    def front(ti):
        b = ti % NB
        own = ti >= 32
        r0 = ti * 128
        P.dma("sp", lambda e: e.dma_start(out=xt[b][:], in_=xin[r0:r0 + 128, :]), writes=[d_xt[b]])
        P.dma("sp", lambda e: e.dma_start(out=cst[b][:], in_=cs[r0:r0 + 128, :]), writes=[d_cst[b]])
        P.op("act", lambda e: e.activation(out=junk[:], in_=xt[b][:], func=AF.Square, accum_out=ssq[:]), reads=[d_xt[b]], writes=[d_junk, d_ssq])
        P.op("act", lambda e: e.activation(out=rstd[:], in_=ssq[:], func=AF.Sqrt, bias=EPS, scale=1.0 / D), reads=[d_ssq], writes=[d_rstd])
        P.op("dve", lambda e: e.reciprocal(out=rstd[:], in_=rstd[:]), reads=[d_rstd], writes=[d_rstd])
        P.op("dve", lambda e: e.scalar_tensor_tensor(out=hb[:], in0=xt[b][:], scalar=rstd[:, 0:1], in1=gm[:], op0=ALU.mult, op1=ALU.mult),
             reads=[d_xt[b], d_rstd, d_gm], writes=[d_hb])
        for k in range(8):
            P.op("pe", lambda e, k=k: e.transpose(out=pT[:, k, :], in_=hb[:, k * 128:(k + 1) * 128], identity=idb[:]), reads=[d_hb, d_idb], writes=[d_pT])
        P.op("act", lambda e: e.copy(out=hT[b][:], in_=pT[:]), reads=[d_pT], writes=[d_hT[b]])
        groups = [0, 1, 2] if own else [1, 2]
        for g in groups:
            for k in range(8):
                P.op("pe", lambda e, g=g, k=k: e.matmul(pq[b][g][:], lhsT=hT[b][:, k, :], rhs=wq[:, k, g * 512:(g + 1) * 512], start=(k == 0), stop=(k == 7)),
                     reads=[d_hT[b], d_wq], writes=[d_pq[b][g]])
        P.op("act", lambda e: e.copy(out=vs[b][:], in_=pq[b][2][:]), reads=[d_pq[b][2]], writes=[d_vs[b]])
        P.dma("sp", lambda e: e.dma_start(out=v_d[r0:r0 + 128, :], in_=vs[b][:]), reads=[d_vs[b]])

    def post(ti):
        b = ti % NB
        own = ti >= 32
        r0 = ti * 128
        qk_post(pq[b][1], d_pq[b][1], 1, b)
        to_T_and_store(kT_d, r0, b)
        if own:
            qk_post(pq[b][0], d_pq[b][0], 0, b)
            to_T_and_store(qT_d, r0 - TP, b)

    front(0)
    for ti in range(64):
        if ti + 1 < 64:
            front(ti + 1)
        post(ti)


def phase_b(nc, P, st, kT_d, v_d, qT_d, e_d, vq_d, nb_d, oq_d, tri_d, idb, d_idb, oa, d_oa, heads=range(8), nblk=16):
    def sb(name, shape, dt):
        return st.enter_context(nc.sbuf_tensor("b_" + name, shape, dt))

    def ps(name, shape, dt=F32):
        return st.enter_context(nc.psum_tensor(name, shape, dt))
    NB = 2
    kaug = [sb("kaug%d" % i, [96, NK], BF16) for i in range(NB)]; d_kaug = [Dep() for _ in range(NB)]
    vaug = [sb("vaug%d" % i, [128, 64, 65], BF16) for i in range(NB)]; d_vaug = [Dep() for _ in range(NB)]
    qaug = [sb("qaug%d" % i, [96, T], BF16) for i in range(NB)]; d_qaug = [Dep() for _ in range(NB)]
    d_qmb = [Dep() for _ in range(NB)]
    vq = sb("vq_s", [128, 1024], F32); d_c = Dep()
    nb = sb("nb_s", [128, 1024], F32)
    oq = sb("oq_s", [128, 1024], F32)
    tri = sb("tri_s", [128, 128], BF16)
    ksum = sb("ksum", [64, 32], F32); d_ksum = Dep()
    kmean = sb("kmean", [64, 32], BF16); d_kmean = Dep()
    g1 = sb("g1", [128, 512], F32); d_g1 = Dep()
    top8 = sb("top8", [128, 16, 8], F32); d_top8 = Dep()
    sel = sb("sel", [128, 512], F32); d_sel = Dep()
    mbp = sb("mbp", [128, 16, 96], BF16); d_mbp = Dep()
    NPT = 3
    pt = [sb("pt%d" % i, [128, 512], BF16) for i in range(NPT)]; d_pt = [Dep() for _ in range(NPT)]
    rc = sb("rc", [128, 2], F32); d_rc = [Dep(), Dep()]
    pg = ps("pg", [128, 512], F32); d_pg = Dep()
    pmT = ps("pmT", [96, 8, 128], BF16); d_pmT = Dep()
    psS = [ps("psS%d" % i, [128, 512], F32) for i in range(NPT)]; d_psS = [Dep() for _ in range(NPT)]
    po = [ps("po%d" % i, [128, 65], F32) for i in range(2)]; d_po = [Dep() for _ in range(2)]

    for i in range(NB):
        P.dma("sp", lambda e, i=i: e.dma_start(out=kaug[i][64:96, :], in_=e_d[:, :]), writes=[d_kaug[i]])
        P.op("pool", lambda e, i=i: e.memset(vaug[i][:], 1.0), writes=[d_vaug[i]])
    P.dma("sp", lambda e: e.dma_start(out=vq[:], in_=vq_d[0:1, :].partition_broadcast(128)), writes=[d_c])
    P.dma("sp", lambda e: e.dma_start(out=nb[:], in_=nb_d[0:1, :].partition_broadcast(128)), writes=[d_c])
    P.dma("sp", lambda e: e.dma_start(out=oq[:], in_=oq_d[0:1, :].partition_broadcast(128)), writes=[d_c])
    P.dma("pool", lambda e: e.dma_start(out=tri[:], in_=tri_d[:, :]), writes=[d_c])
    P.op("pool", lambda e: e.memset(mbp[:], 0.0), writes=[d_mbp])

    for ih, h in enumerate(heads):
        b = ih % NB
        P.dma("sp", lambda e, b=b, h=h: e.dma_start(out=kaug[b][0:64, :], in_=kT_d[h * 64:(h + 1) * 64, :]), writes=[d_kaug[b]])
        P.dma("sp", lambda e, b=b, h=h: e.dma_start(out=vaug[b][:, :, 0:64],
                                                    in_=v_d[:, h * 64:(h + 1) * 64].rearrange("(t p) d -> p t d", p=128)),
              writes=[d_vaug[b]])
        P.dma("sp", lambda e, b=b, h=h: e.dma_start(out=qaug[b][0:64, :], in_=qT_d[h * 64:(h + 1) * 64, :]), writes=[d_qaug[b]])
        P.op("dve", lambda e, b=b: e.tensor_reduce(out=ksum[:], in_=kaug[b][0:64, :].rearrange("p (b k) -> p b k", k=256),
                                                   axis=AX.X, op=ALU.add), reads=[d_kaug[b]], writes=[d_ksum])
        P.op("dve", lambda e: e.tensor_scalar(out=kmean[:], in0=ksum[:], scalar1=1.0 / 256, scalar2=None, op0=ALU.mult),
             reads=[d_ksum], writes=[d_kmean])
        for half in range(2):
            for j in range(16):
                qt = half * 16 + j
                P.op("pe", lambda e, b=b, j=j, qt=qt: e.matmul(pg[:, j * 32:(j + 1) * 32], lhsT=qaug[b][0:64, qt * 128:(qt + 1) * 128],
                                                              rhs=kmean[:, :], start=True, stop=True),
                     reads=[d_qaug[b], d_kmean], writes=[d_pg])
            cs_ = slice(half * 512, (half + 1) * 512)
            P.op("dve", lambda e, cs_=cs_: e.tensor_tensor(out=g1[:], in0=pg[:], in1=vq[:, cs_], op=ALU.mult),
                 reads=[d_pg, d_c], writes=[d_g1])
            P.op("dve", lambda e, cs_=cs_: e.tensor_tensor(out=g1[:], in0=g1[:], in1=nb[:, cs_], op=ALU.add),
                 reads=[d_g1, d_c], writes=[d_g1])
            for j in range(16):
                P.op("dve", lambda e, j=j: e.max(out=top8[:, j, :], in_=g1[:, j * 32:(j + 1) * 32]), reads=[d_g1], writes=[d_top8])
            P.op("dve", lambda e: e.tensor_tensor(out=sel[:].rearrange("p (j b) -> p j b", b=32),
                                                  in0=g1[:].rearrange("p (j b) -> p j b", b=32),
                                                  in1=top8[:, :, 2:3].to_broadcast([128, 16, 32]), op=ALU.is_ge),
                 reads=[d_g1, d_top8], writes=[d_sel])
            P.op("dve", lambda e, cs_=cs_: e.tensor_tensor(out=sel[:], in0=sel[:], in1=vq[:, cs_], op=ALU.mult),
                 reads=[d_sel, d_c], writes=[d_sel])
            P.op("dve", lambda e, cs_=cs_: e.tensor_tensor(out=sel[:], in0=sel[:], in1=oq[:, cs_], op=ALU.add),
                 reads=[d_sel, d_c], writes=[d_sel])
            P.op("dve", lambda e: e.tensor_scalar(out=mbp[:, :, 64:96], in0=sel[:].rearrange("p (j b) -> p j b", b=32),
                                                  scalar1=1.0, scalar2=MB, op0=ALU.subtract, op1=ALU.mult),
                 reads=[d_sel], writes=[d_mbp])
            for grp in range(2):
                for j in range(8):
                    P.op("pe", lambda e, grp=grp, j=j: e.transpose(out=pmT[:, j, :], in_=mbp[:, grp * 8 + j, :], identity=idb[:]),
                         reads=[d_mbp, d_idb], writes=[d_pmT])
                c0 = (half * 16 + grp * 8) * 128
                P.op("act", lambda e, b=b, c0=c0: e.copy(out=qaug[b][64:96, c0:c0 + 1024], in_=pmT[64:96, :, :]),
                     reads=[d_pmT], writes=[d_qmb[b]])
        units = []
        for jb in range(nblk):
            ncommon = 32 + 2 * jb
            for u in range(ncommon // 2):
                units.append((jb, [2 * u, 2 * u + 1], False, u == 0))
            units.append((jb, [32 + 2 * jb, 32 + 2 * jb + 1], True, False))

        def emit_S(u, s, b=b):
            jb, kts, diag, first = u
            for j, kt in enumerate(kts):
                if diag and j == 1:
                    q0, nq, c0 = jb * 256 + 128, 128, 256
                else:
                    q0, nq, c0 = jb * 256, 256, j * 256
                P.op("pe", lambda e, s=s, kt=kt, q0=q0, nq=nq, c0=c0: e.matmul(psS[s][:, c0:c0 + nq], lhsT=kaug[b][0:96, kt * 128:(kt + 1) * 128],
                                                                             rhs=qaug[b][0:96, q0:q0 + nq], start=True, stop=True),
                     reads=[d_kaug[b], d_qaug[b], d_qmb[b]], writes=[d_psS[s]])

        def emit_rest(u, s, b=b, h=h):
            jb, kts, diag, first = u
            ncols = 384 if diag else 512
            P.op("act", lambda e, s=s, ncols=ncols: e.activation(out=pt[s][:, 0:ncols], in_=psS[s][:, 0:ncols], func=AF.Exp),
                 reads=[d_psS[s]], writes=[d_pt[s]])
            if diag:
                P.op("pool", lambda e, s=s: e.tensor_tensor(out=pt[s][:, 0:128], in0=pt[s][:, 0:128], in1=tri[:], op=ALU.mult),
                     reads=[d_pt[s], d_c], writes=[d_pt[s]])
                P.op("pool", lambda e, s=s: e.tensor_tensor(out=pt[s][:, 256:384], in0=pt[s][:, 256:384], in1=tri[:], op=ALU.mult),
                     reads=[d_pt[s], d_c], writes=[d_pt[s]])
            for j, kt in enumerate(kts):
                if diag and j == 1:
                    pv = [(1, 256, True)]
                elif diag:
                    pv = [(0, 0, True), (1, 128, False)]
                else:
                    pv = [(0, j * 256, False), (1, j * 256 + 128, False)]
                st_ = first and j == 0
                for qi, col0, last in pv:
                    P.op("pe", lambda e, s=s, kt=kt, qi=qi, col0=col0, st_=st_, last=last: e.matmul(
                        po[qi][:, :], lhsT=pt[s][:, col0:col0 + 128], rhs=vaug[b][:, kt, :], start=st_, stop=last),
                        reads=[d_pt[s], d_vaug[b]], writes=[d_po[qi]])
            if diag:
                for qi in range(2):
                    qt = jb * 2 + qi
                    P.op("dve", lambda e, qi=qi: e.reciprocal(out=rc[:, qi:qi + 1], in_=po[qi][:, 64:65]), reads=[d_po[qi]], writes=[d_rc[qi]])
                    P.op("dve", lambda e, qi=qi, qt=qt: e.tensor_scalar(out=oa[:, qt, h * 64:(h + 1) * 64], in0=po[qi][:, 0:64],
                                                                       scalar1=rc[:, qi:qi + 1], scalar2=None, op0=ALU.mult),
                         reads=[d_po[qi], d_rc[qi]], writes=[d_oa])
        SK = 2
        n = len(units)
        for i in range(n + SK):
            if i < n:
                emit_S(units[i], i % NPT)
            if i >= SK:
                emit_rest(units[i - SK], (i - SK) % NPT)


def host_consts(half):
    pv = 1.0 if half == 1 else 0.0
    V = np.zeros((32, 32), np.float32); O = np.zeros((32, 32), np.float32)
    for qt in range(32):
        jb = qt // 2
        V[qt, :16] = pv
        V[qt, 16:16 + jb] = 1.0
        O[qt, 16 + jb] = 1.0
    NBm = (V - 1.0) * BIGG
    E = np.zeros((32, NK), np.float32)
    for b in range(32):
        E[b, b * 256:(b + 1) * 256] = 1.0
    tri = (np.arange(128)[:, None] <= np.arange(128)[None, :]).astype(np.float32)
    return dict(vq=V.reshape(1, 1024), nb=NBm.reshape(1, 1024), oq=O.reshape(1, 1024),
                e_d=E.astype(ml_dtypes.bfloat16), tri=tri.astype(ml_dtypes.bfloat16))


def phase_c(nc, P, st, xin, w_in, g_mix, cwl_d, cbl_d, dtb_d, alog_d, dsk_d, ssdn_d, tri2_d, sel_d, tb_d, pflag_d,
            idb, d_idb, os_d, d_osd, chunks=range(32)):
    def sb(name, shape, dt):
        return st.enter_context(nc.sbuf_tensor("c_" + name, shape, dt))

    def ps(name, shape, dt=F32):
        return st.enter_context(nc.psum_tensor(name, shape, dt))
    wz = sb("wz", [128, 8, 1024], BF16); d_w = Dep()
    wx = sb("wx", [128, 8, 2048], BF16)
    wdt = sb("wdt", [128, 8, 16], BF16)
    gm = sb("gmc", [128, D], F32); d_c = Dep()
    cw = sb("cw", [128, 16, 4], F32)
    cb = sb("cb", [128, 16], F32)
    dtb = sb("dtb", [128, 2, 16], F32)
    Aneg = sb("Aneg", [128, 2, 16], F32); d_A = Dep()
    dsk = sb("dsk", [128, 16], F32)
    ssdn = sb("ssdn", [128, D], F32)
    tri2 = sb("tri2", [128, 2, 256], F32)
    onesf = sb("onesf", [128, 128], F32)
    sel = sb("selc", [16, 16, 128], F32)
    tb = sb("tbc", [128, 384], F32)
    pflag = sb("pflag", [128, 1], F32)
    xt = sb("xtc", [128, 2, D], F32); d_xt = Dep()
    junk = sb("junkc", [128, D], BF16); d_junk = Dep()
    ssq = sb("ssqc", [128, 2], F32); d_ssq = Dep()
    rstd = sb("rstdc", [128, 2], F32); d_rstd = Dep()
    hb = sb("hbc", [128, 2, D], BF16); d_hb = Dep()
    hT = sb("hTc", [128, 8, 256], BF16); d_hT = Dep()
    xraw = sb("xraw", [128, 16, 259], F32); d_xraw = Dep(); d_halo = Dep()
    acc = sb("acc", [128, 8, 256], F32); d_acc = [Dep() for _ in range(8)]
    xc = sb("xc", [128, 16, 256], BF16); d_xc = [Dep() for _ in range(16)]
    xtok = sb("xtok", [128, 2, 1024], BF16); d_xtok = Dep()
    btok = sb("btok", [128, 2, 512], BF16); d_btok = Dep()
    dtr = sb("dtr", [128, 2, 16], F32); d_dtr = Dep()
    dt = sb("dt", [128, 2, 16], F32); d_dt = Dep()
    aa = sb("aa", [128, 2, 16], F32); d_aa = Dep()
    acum = sb("acum", [128, 2, 16], F32); d_acum = Dep()
    nacum = sb("nacum", [128, 2, 16], F32); d_nacum = Dep()
    acumT = sb("acumT", [16, 256], F32); d_acumT = Dep()
    tot = sb("tot", [128, 16], F32); d_tot = Dep()
    cdec = sb("cdec", [128, 16], F32); d_cdec = Dep()
    dte = sb("dte", [128, 2, 16], F32); d_dte = Dep()
    eac = sb("eac", [128, 2, 16], F32); d_eac = Dep()
    w2 = sb("w2", [128, 2, 16], F32); d_w2 = Dep()
    xdt = sb("xdt", [128, 2, 1024], BF16); d_xdt = Dep()
    xdd = sb("xdd", [128, 2, 1024], BF16); d_xdd = Dep()
    zs = sb("zs", [128, 2, 1024], BF16); d_zs = Dep()
    cbT = sb("cbT", [128, 4, 384], F32); d_cbT = [Dep() for _ in range(4)]
    arg = sb("arg", [128, 384], F32); d_arg = Dep()
    Lt = sb("Lt", [128, 384], F32); d_Lt = Dep()
    Mt = [sb("Mt%d" % i, [128, 384], BF16) for i in range(2)]; d_Mt = [Dep() for _ in range(2)]
    stf = sb("stf", [128, 4, 256], F32); d_stf = Dep()
    stT = sb("stT", [128, 4, 256], BF16); d_stT = Dep()
    yt = sb("yt", [128, 256], F32); d_yt = Dep()
    y2 = sb("y2", [128, 256], F32); d_y2 = Dep()
    gss = sb("gss", [128, 1], F32); d_gss = Dep()
    grs = sb("grs", [128, 1], F32); d_grs = Dep()
    osb = sb("osb", [128, 2, 1024], BF16); d_osb = Dep()

    B = [ps("bk%d" % i, [128, 512], F32) for i in range(7)]
    d_B = [Dep() for _ in range(7)]
    pT = ps("pTc", [128, 8, 128], BF16); d_pT = Dep()

    for k in range(8):
        rows = slice(k * 128, (k + 1) * 128)
        P.dma("pool", lambda e, k=k, rows=rows: e.dma_start(out=wz[:, k, :], in_=w_in[rows, C_Z:C_Z + 1024]), writes=[d_w])
        P.dma("pool", lambda e, k=k, rows=rows: e.dma_start(out=wx[:, k, :], in_=w_in[rows, C_X:C_X + 2048]), writes=[d_w])
        P.dma("pool", lambda e, k=k, rows=rows: e.dma_start(out=wdt[:, k, :], in_=w_in[rows, C_DT:C_DT + 16]), writes=[d_w])
    P.dma("sp", lambda e: e.dma_start(out=gm[:], in_=g_mix[0:1, :].partition_broadcast(128)), writes=[d_c])
    P.dma("sp", lambda e: e.dma_start(out=cw[:], in_=cwl_d[:, :, :]), writes=[d_c])
    P.dma("sp", lambda e: e.dma_start(out=cb[:], in_=cbl_d[:, :]), writes=[d_c])
    for i in range(2):
        P.dma("sp", lambda e, i=i: e.dma_start(out=dtb[:, i, :], in_=dtb_d[0:1, :].partition_broadcast(128)), writes=[d_c])
        P.dma("sp", lambda e, i=i: e.dma_start(out=Aneg[:, i, :], in_=alog_d[0:1, :].partition_broadcast(128)), writes=[d_A])
    P.dma("sp", lambda e: e.dma_start(out=dsk[:], in_=dsk_d[0:1, :].partition_broadcast(128)), writes=[d_c])
    P.dma("sp", lambda e: e.dma_start(out=ssdn[:], in_=ssdn_d[0:1, :].partition_broadcast(128)), writes=[d_c])
    P.dma("sp", lambda e: e.dma_start(out=tri2[:], in_=tri2_d[:, :, :]), writes=[d_c])
    P.dma("sp", lambda e: e.dma_start(out=sel[:], in_=sel_d[:, :, :]), writes=[d_c])
    P.dma("sp", lambda e: e.dma_start(out=tb[:], in_=tb_d[:, :]), writes=[d_c])
    P.dma("sp", lambda e: e.dma_start(out=pflag[:], in_=pflag_d[0:1, :].partition_broadcast(128)), writes=[d_c])
    P.op("pool", lambda e: e.memset(onesf[:], 1.0), writes=[d_c])
    P.op("act", lambda e: e.activation(out=Aneg[:], in_=Aneg[:], func=AF.Exp), reads=[d_A], writes=[d_A])
    P.op("dve", lambda e: e.tensor_scalar(out=Aneg[:], in0=Aneg[:], scalar1=-1.0, scalar2=None, op0=ALU.mult), reads=[d_A], writes=[d_A])
    P.op("pool", lambda e: e.memset(xraw[:], 0.0), writes=[d_xraw, d_halo])
    P.op("pool", lambda e: e.memset(stf[:], 0.0), writes=[d_stf])
    P.op("pool", lambda e: e.memset(stT[:], 0.0), writes=[d_stT])

    h3 = lambda ap: ap.rearrange("p (h d) -> p h d", d=64)

    for c in chunks:
        own = c >= 16
        r0 = c * 256
        P.dma("sp", lambda e, r0=r0: e.dma_start(out=xt[:], in_=xin[r0:r0 + 256, :].rearrange("(t p) d -> p t d", p=128)), writes=[d_xt])
        for it in range(2):
            P.op("act", lambda e, it=it: e.activation(out=junk[:], in_=xt[:, it, :], func=AF.Square, accum_out=ssq[:, it:it + 1]),
                 reads=[d_xt], writes=[d_junk, d_ssq])
        P.op("act", lambda e: e.activation(out=rstd[:], in_=ssq[:], func=AF.Sqrt, bias=EPS, scale=1.0 / D), reads=[d_ssq], writes=[d_rstd])
        P.op("dve", lambda e: e.reciprocal(out=rstd[:], in_=rstd[:]), reads=[d_rstd], writes=[d_rstd])
        for it in range(2):
            P.op("dve", lambda e, it=it: e.scalar_tensor_tensor(out=hb[:, it, :], in0=xt[:, it, :], scalar=rstd[:, it:it + 1], in1=gm[:],
                                                                                    op0=ALU.mult, op1=ALU.mult),
                 reads=[d_xt, d_rstd, d_c], writes=[d_hb])
        for it in range(2):
            for k in range(8):
                P.op("pe", lambda e, it=it, k=k: e.transpose(out=pT[:, k, :], in_=hb[:, it, k * 128:(k + 1) * 128], identity=idb[:]),
                     reads=[d_hb, d_idb], writes=[d_pT])
            P.op("act", lambda e, it=it: e.copy(out=hT[:, :, it * 128:(it + 1) * 128], in_=pT[:]), reads=[d_pT], writes=[d_hT])
        for it in range(2):
            for k in range(8):
                P.op("pe", lambda e, it=it, k=k: e.matmul(B[3][:, it * 16:(it + 1) * 16], lhsT=hT[:, k, it * 128:(it + 1) * 128], rhs=wdt[:, k, :],
                                                         start=(k == 0), stop=(k == 7)), reads=[d_hT, d_w], writes=[d_B[3]])
        P.op("dve", lambda e: e.tensor_tensor(out=dtr[:].rearrange("p a b -> p (a b)"), in0=B[3][:, 0:32], in1=dtb[:].rearrange("p a b -> p (a b)"), op=ALU.add),
             reads=[d_B[3], d_c], writes=[d_dtr])
        P.op("act", lambda e: e.activation(out=dtr[:], in_=dtr[:], func=AF.Exp), reads=[d_dtr], writes=[d_dtr])
        P.op("act", lambda e: e.activation(out=dt[:], in_=dtr[:], func=AF.Ln, bias=1.0), reads=[d_dtr], writes=[d_dt])
        P.op("dve", lambda e: e.tensor_tensor(out=aa[:], in0=dt[:], in1=Aneg[:], op=ALU.mult), reads=[d_dt, d_A], writes=[d_aa])
        for it in range(2):
            for jt in range(it + 1):
                P.op("pe", lambda e, it=it, jt=jt: e.matmul(B[3][:, 32 + it * 16:32 + (it + 1) * 16], lhsT=tri2[:, jt, it * 128:(it + 1) * 128],
                                                           rhs=aa[:, jt, :], start=(jt == 0), stop=(jt == it)),
                     reads=[d_aa, d_c], writes=[d_B[3]])
        for jt in range(2):
            P.op("pe", lambda e, jt=jt: e.matmul(B[3][:, 64:80], lhsT=onesf[:], rhs=aa[:, jt, :], start=(jt == 0), stop=(jt == 1)),
                 reads=[d_aa, d_c], writes=[d_B[3]])
        for jt in range(2):
            P.op("pe", lambda e, jt=jt: e.matmul(B[3][0:16, 128:384], lhsT=aa[:, jt, :], rhs=tri2[:, jt, :], start=(jt == 0), stop=(jt == 1)),
                 reads=[d_aa, d_c], writes=[d_B[3]])
        P.op("act", lambda e: e.copy(out=acum[:].rearrange("p a b -> p (a b)"), in_=B[3][:, 32:64]), reads=[d_B[3]], writes=[d_acum])
        P.op("dve", lambda e: e.tensor_scalar(out=nacum[:].rearrange("p a b -> p (a b)"), in0=B[3][:, 32:64], scalar1=-1.0, scalar2=None, op0=ALU.mult),
             reads=[d_B[3]], writes=[d_nacum])
        P.op("act", lambda e: e.copy(out=tot[:], in_=B[3][:, 64:80]), reads=[d_B[3]], writes=[d_tot])
        P.op("act", lambda e: e.copy(out=acumT[:], in_=B[3][0:16, 128:384]), reads=[d_B[3]], writes=[d_acumT])
        P.op("act", lambda e: e.activation(out=cdec[:], in_=tot[:], func=AF.Exp), reads=[d_tot], writes=[d_cdec])
        P.op("dve", lambda e: e.tensor_tensor(out=dte[:], in0=nacum[:], in1=tot[:].unsqueeze(1).to_broadcast([128, 2, 16]), op=ALU.add),
             reads=[d_nacum, d_tot], writes=[d_dte])
        P.op("act", lambda e: e.activation(out=dte[:], in_=dte[:], func=AF.Exp), reads=[d_dte], writes=[d_dte])
        P.op("act", lambda e: e.activation(out=eac[:], in_=acum[:], func=AF.Exp), reads=[d_acum], writes=[d_eac])
        P.op("dve", lambda e: e.tensor_tensor(out=w2[:], in0=dt[:], in1=dte[:], op=ALU.mult), reads=[d_dt, d_dte], writes=[d_w2])
        for fc in range(16):
            hv = fc % 2
            for k in range(8):
                P.op("pe", lambda e, fc=fc, k=k, hv=hv: e.matmul(B[1][:, hv * 256:(hv + 1) * 256], lhsT=wx[:, k, fc * 128:(fc + 1) * 128], rhs=hT[:, k, :],
                                                               start=(k == 0), stop=(k == 7)), reads=[d_hT, d_w], writes=[d_B[1]])
            P.op("act", lambda e, fc=fc, hv=hv: e.copy(out=xraw[:, fc, 3:259], in_=B[1][:, hv * 256:(hv + 1) * 256]),
                 reads=[d_B[1]], writes=[d_xraw])
        for half in range(2):
            for f8 in range(8):
                fc = half * 8 + f8
                eng = "dve"
                P.op(eng, lambda e, fc=fc, f8=f8: e.tensor_scalar(out=acc[:, f8, :], in0=xraw[:, fc, 0:256], scalar1=cw[:, fc, 0:1], scalar2=cb[:, fc:fc + 1],
                                                                 op0=ALU.mult, op1=ALU.add), reads=[d_xraw, d_halo, d_c], writes=[d_acc[f8]])
                for kk in range(1, 4):
                    P.op(eng, lambda e, fc=fc, f8=f8, kk=kk: e.scalar_tensor_tensor(out=acc[:, f8, :], in0=xraw[:, fc, kk:kk + 256], scalar=cw[:, fc, kk:kk + 1],
                                                                                   in1=acc[:, f8, :], op0=ALU.mult, op1=ALU.add),
                         reads=[d_xraw, d_halo, d_c, d_acc[f8]], writes=[d_acc[f8]])
                P.op("act", lambda e, fc=fc, f8=f8: e.activation(out=xc[:, fc, :], in_=acc[:, f8, :], func=AF.Silu),
                     reads=[d_acc[f8]], writes=[d_xc[fc]])
        P.op("pool", lambda e: e.tensor_copy(out=xraw[:, :, 0:3], in_=xraw[:, :, 256:259]), reads=[d_xraw], writes=[d_halo])
        for it in range(2):
            for fc in range(8):
                P.op("pe", lambda e, it=it, fc=fc: e.transpose(out=pT[:, fc, :], in_=xc[:, fc, it * 128:(it + 1) * 128], identity=idb[:]),
                     reads=[d_xc[fc], d_idb], writes=[d_pT])
            P.op("act", lambda e, it=it: e.copy(out=xtok[:, it, :], in_=pT[:]), reads=[d_pT], writes=[d_xtok])
            for g in range(4):
                P.op("pe", lambda e, it=it, g=g: e.transpose(out=pT[:, g, :], in_=xc[:, 8 + g, it * 128:(it + 1) * 128], identity=idb[:]),
                     reads=[d_xc[8 + g], d_idb], writes=[d_pT])
            P.op("act", lambda e, it=it: e.copy(out=btok[:, it, :], in_=pT[:, 0:4, :]), reads=[d_pT], writes=[d_btok])
        for it in range(2):
            if own:
                P.op("dve", lambda e, it=it: e.tensor_tensor(out=h3(xdt[:, it, :]), in0=h3(xtok[:, it, :]),
                                                             in1=dt[:, it, :].unsqueeze(2).to_broadcast([128, 16, 64]), op=ALU.mult),
                     reads=[d_xtok, d_dt], writes=[d_xdt])
            P.op("pool", lambda e, it=it: e.tensor_tensor(out=h3(xdd[:, it, :]), in0=h3(xtok[:, it, :]),
                                                          in1=w2[:, it, :].unsqueeze(2).to_broadcast([128, 16, 64]), op=ALU.mult),
                 reads=[d_xtok, d_w2], writes=[d_xdd])
        if own:
            for it in range(2):
                for hv in range(2):
                    for k in range(8):
                        P.op("pe", lambda e, it=it, hv=hv, k=k: e.matmul(B[2][:, :], lhsT=hT[:, k, it * 128:(it + 1) * 128], rhs=wz[:, k, hv * 512:(hv + 1) * 512],
                                                                       start=(k == 0), stop=(k == 7)), reads=[d_hT, d_w], writes=[d_B[2]])
                    P.op("act", lambda e, it=it, hv=hv: e.activation(out=zs[:, it, hv * 512:(hv + 1) * 512], in_=B[2][:, :], func=AF.Silu),
                         reads=[d_B[2]], writes=[d_zs])
            for g in range(4):
                P.op("pe", lambda e, g=g: e.matmul(B[4][:, 0:256], lhsT=xc[:, 8 + g, 0:128], rhs=xc[:, 12 + g, :], start=True, stop=True),
                     reads=[d_xc[8 + g], d_xc[12 + g]], writes=[d_B[4]])
                P.op("pe", lambda e, g=g: e.matmul(B[4][:, 256:384], lhsT=xc[:, 8 + g, 128:256], rhs=xc[:, 12 + g, 128:256], start=True, stop=True),
                     reads=[d_xc[8 + g], d_xc[12 + g]], writes=[d_B[4]])
                P.op("act", lambda e, g=g: e.copy(out=cbT[:, g, :], in_=B[4][:, 0:384]), reads=[d_B[4]], writes=[d_cbT[g]])
            for g in range(4):
                for r in range(4):
                    h = g * 4 + r
                    mi = h % 2
                    P.op("pe", lambda e, h=h: e.matmul(B[5][:, 0:256], lhsT=sel[:, h, :], rhs=acumT[:, :], start=True, stop=True),
                         reads=[d_c, d_acumT], writes=[d_B[5]])
                    P.op("dve", lambda e, h=h: e.scalar_tensor_tensor(out=arg[:, 0:256], in0=B[5][:, 0:256], scalar=nacum[:, 0, h:h + 1], in1=tb[:, 0:256],
                                                                      op0=ALU.add, op1=ALU.add), reads=[d_B[5], d_nacum, d_c], writes=[d_arg])
                    P.op("dve", lambda e, h=h: e.scalar_tensor_tensor(out=arg[:, 256:384], in0=B[5][:, 128:256], scalar=nacum[:, 1, h:h + 1], in1=tb[:, 256:384],
                                                                      op0=ALU.add, op1=ALU.add), reads=[d_B[5], d_nacum, d_c], writes=[d_arg])
                    P.op("act", lambda e: e.activation(out=Lt[:], in_=arg[:], func=AF.Exp), reads=[d_arg], writes=[d_Lt])
                    P.op("pool", lambda e, g=g, mi=mi: e.tensor_tensor(out=Mt[mi][:], in0=Lt[:], in1=cbT[:, g, :], op=ALU.mult),
                         reads=[d_Lt, d_cbT[g]], writes=[d_Mt[mi]])
                    cs_ = slice(r * 64, (r + 1) * 64)
                    hs_ = slice(h * 64, (h + 1) * 64)
                    P.op("pe", lambda e, mi=mi, cs_=cs_, hs_=hs_: e.matmul(B[6][:, 0:256][:, cs_], lhsT=Mt[mi][:, 0:128], rhs=xdt[:, 0, hs_], start=True, stop=True),
                         reads=[d_Mt[mi], d_xdt], writes=[d_B[6]])
                    P.op("pe", lambda e, mi=mi, cs_=cs_, hs_=hs_: e.matmul(B[6][:, 256:512][:, cs_], lhsT=Mt[mi][:, 128:256], rhs=xdt[:, 0, hs_], start=True, stop=False),
                         reads=[d_Mt[mi], d_xdt], writes=[d_B[6]])
                    P.op("pe", lambda e, mi=mi, cs_=cs_, hs_=hs_: e.matmul(B[6][:, 256:512][:, cs_], lhsT=Mt[mi][:, 256:384], rhs=xdt[:, 1, hs_], start=False, stop=True),
                         reads=[d_Mt[mi], d_xdt], writes=[d_B[6]])
                gs_ = slice(g * 256, (g + 1) * 256)
                for it in range(2):
                    P.op("pe", lambda e, g=g, it=it: e.matmul(B[0][:, it * 256:(it + 1) * 256], lhsT=xc[:, 12 + g, it * 128:(it + 1) * 128], rhs=stT[:, g, :],
                                                             start=True, stop=True), reads=[d_xc[12 + g], d_stT], writes=[d_B[0]])
                for it in range(2):
                    v4 = lambda ap: ap.rearrange("p (r d) -> p r d", d=64)
                    P.op("dve", lambda e, g=g, it=it: e.tensor_tensor(out=v4(yt[:]), in0=v4(B[0][:, it * 256:(it + 1) * 256]),
                                                                      in1=eac[:, it, g * 4:(g + 1) * 4].unsqueeze(2).to_broadcast([128, 4, 64]), op=ALU.mult),
                         reads=[d_B[0], d_eac], writes=[d_yt])
                    P.op("dve", lambda e, it=it: e.tensor_tensor(out=yt[:], in0=yt[:], in1=B[6][:, it * 256:(it + 1) * 256], op=ALU.add),
                         reads=[d_yt, d_B[6]], writes=[d_yt])
                    P.op("pool", lambda e, g=g, it=it, gs_=gs_: e.tensor_tensor(out=v4(y2[:]), in0=v4(xtok[:, it, gs_]),
                                                                               in1=dsk[:, g * 4:(g + 1) * 4].unsqueeze(2).to_broadcast([128, 4, 64]), op=ALU.mult),
                         reads=[d_xtok, d_c], writes=[d_y2])
                    P.op("dve", lambda e: e.tensor_tensor(out=yt[:], in0=yt[:], in1=y2[:], op=ALU.add), reads=[d_yt, d_y2], writes=[d_yt])
                    P.op("dve", lambda e, it=it, gs_=gs_: e.tensor_tensor(out=yt[:], in0=yt[:], in1=zs[:, it, gs_], op=ALU.mult), reads=[d_yt, d_zs], writes=[d_yt])
                    P.op("act", lambda e: e.activation(out=y2[:], in_=yt[:], func=AF.Square, accum_out=gss[:]), reads=[d_yt, d_y2], writes=[d_y2, d_gss])
                    P.op("act", lambda e: e.activation(out=grs[:], in_=gss[:], func=AF.Sqrt, bias=EPS, scale=1.0 / 256), reads=[d_gss], writes=[d_grs])
                    P.op("dve", lambda e: e.reciprocal(out=grs[:], in_=grs[:]), reads=[d_grs], writes=[d_grs])
                    P.op("dve", lambda e, it=it, gs_=gs_: e.scalar_tensor_tensor(out=osb[:, it, gs_], in0=yt[:], scalar=grs[:, 0:1], in1=ssdn[:, gs_],
                                                                                op0=ALU.mult, op1=ALU.mult), reads=[d_yt, d_grs, d_c], writes=[d_osb])
            P.dma("sp", lambda e, r0=r0: e.dma_start(out=os_d[r0 - TP:r0 - TP + 256, :].rearrange("(t p) c -> p t c", p=128), in_=osb[:]),
                  reads=[d_osb], writes=[d_osd])
        for g in range(4):
            bank = B[2] if g < 2 else B[1]
            dbank = d_B[2] if g < 2 else d_B[1]
            cs_ = slice((g % 2) * 256, (g % 2 + 1) * 256)
            for jt in range(2):
                P.op("pe", lambda e, g=g, jt=jt, bank=bank, cs_=cs_: e.matmul(bank[:, cs_], lhsT=btok[:, jt, g * 128:(g + 1) * 128], rhs=xdd[:, jt, g * 256:(g + 1) * 256],
                                                                            start=(jt == 0), stop=(jt == 1)), reads=[d_btok, d_xdd], writes=[dbank])
        v16 = lambda ap: ap.rearrange("p (h d) -> p h d", d=64)
        P.op("dve", lambda e: e.tensor_tensor(out=v16(stf[:].rearrange("p g c -> p (g c)")), in0=v16(stf[:].rearrange("p g c -> p (g c)")),
                                              in1=cdec[:].unsqueeze(2).to_broadcast([128, 16, 64]), op=ALU.mult), reads=[d_stf, d_cdec], writes=[d_stf])
        P.op("dve", lambda e: e.tensor_tensor(out=stf[:, 0:2, :].rearrange("p g c -> p (g c)"), in0=stf[:, 0:2, :].rearrange("p g c -> p (g c)"), in1=B[2][:, :], op=ALU.add),
             reads=[d_stf, d_B[2]], writes=[d_stf])
        P.op("dve", lambda e: e.tensor_tensor(out=stf[:, 2:4, :].rearrange("p g c -> p (g c)"), in0=stf[:, 2:4, :].rearrange("p g c -> p (g c)"), in1=B[1][:, :], op=ALU.add),
             reads=[d_stf, d_B[1]], writes=[d_stf])
        if c == 15:
            P.op("dve", lambda e: e.tensor_scalar(out=stf[:], in0=stf[:], scalar1=pflag[:, 0:1], scalar2=None, op0=ALU.mult), reads=[d_stf, d_c], writes=[d_stf])
        P.op("act", lambda e: e.copy(out=stT[:], in_=stf[:]), reads=[d_stf], writes=[d_stT])


def host_consts_c(z, half):
    cwl = np.ascontiguousarray(z['conv_w'].reshape(4, 16, 128).transpose(2, 1, 0)).astype(np.float32)
    cbl = np.ascontiguousarray(z['conv_b'].reshape(16, 128).T).astype(np.float32)
    tri = (np.arange(128)[:, None] <= np.arange(128)[None, :]).astype(np.float32)
    tri2 = np.zeros((128, 2, 256), np.float32)
    tri2[:, 0, 0:128] = tri; tri2[:, 0, 128:256] = 1.0; tri2[:, 1, 128:256] = tri
    sel = np.zeros((16, 16, 128), np.float32)
    for h in range(16):
        sel[h, h, :] = 1.0
    tbias = np.where(tri > 0, 0.0, NEG).astype(np.float32)
    tb = np.zeros((128, 384), np.float32)
    tb[:, 0:128] = tbias; tb[:, 256:384] = tbias
    return dict(cwl=cwl, cbl=cbl, dtb=z['dt_bias'][None, :], alog=z['a_log'][None, :], dsk=z['d_skip'][None, :], ssdn=z['ssd_norm'][None, :],
                tri2=tri2, sel_d=sel, tb_d=tb, pflag=np.array([[1.0 if half == 1 else 0.0]], np.float32))


def phase_def(nc, P, xown, w_in, g_mix, mem_d, gmem_d, wmemkv, mqn_d, mkn_d, woa_d, wos_d, wom_d, wout_d, gffn_d, wr_d,
              wgate_d, wup_d, wdown_d, tri_d, ecap_d, oa_d, d_oad, os_d, d_osd, x1_d, xbuf, ybuf, out_d, idb, d_idb, idf, d_idf,
              ntiles=32, experts=range(32), stage=3):
    d_x1d = Dep(); d_xbuf = Dep(); d_ybuf = Dep(); d_out = Dep()
    with contextlib.ExitStack() as stp:
        def sbp(name, shape, dt):
            return stp.enter_context(nc.sbuf_tensor("d_" + name, shape, dt))
        combs = sbp("combs", [128, 32, 2], F32); d_combs = Dep()
        idxs = sbp("idxs", [128, 32, 2], I32); d_idxs = Dep()

        with contextlib.ExitStack() as st:
            def sb(name, shape, dt):
                return st.enter_context(nc.sbuf_tensor("d_" + name, shape, dt))

            def ps(name, shape, dt=F32):
                return st.enter_context(nc.psum_tensor(name, shape, dt))
            d_w = Dep(); d_c = Dep()
            gm = sb("gmd", [128, D], F32)
            gf = sb("gfd", [128, D], F32)
            mqn = sb("mqn", [128, 512], F32)
            mkn = sb("mkn", [128, 512], F32)
            tri = sb("trid", [128, 128], F32)
            onesf = sb("onesfd", [128, 128], F32)
            onesb = sb("onesbd", [128, 128], BF16)
            ecap = sb("ecap", [128, 32], F32)
            base = sb("base", [128, 32], F32); d_base = Dep()
            kmT = sb("kmT", [128, 4, 256], BF16); d_kmT = Dep()
            vm = sb("vm", [128, 2, 512], BF16); d_vm = Dep()

            xt = sb("xtd", [128, D], F32); d_xt = Dep()
            junk = sb("junkd", [128, D], BF16); d_junk = Dep()
            ssq = sb("ssqd", [128, 1], F32); d_ssq = Dep()
            rstd = sb("rstdd", [128, 1], F32); d_rstd = Dep()
            hb = sb("hbd", [128, D], BF16); d_hb = Dep()
            hT = sb("hTd", [128, 8, 128], BF16); d_hT = Dep()
            sq = sb("sqd", [128, 512], F32); d_sq = Dep()
            hs = sb("hsd", [128, 4], F32); d_hs = Dep()
            hr = sb("hrd", [128, 4], F32); d_hr = Dep()
            qmn = sb("qmn", [128, 512], F32); d_qmn = Dep()
            qmb = sb("qmb", [128, 512], BF16); d_qmb = Dep()
            qmT = sb("qmT", [128, 4, 128], BF16); d_qmT = Dep()
            pm = sb("pm", [128, 4, 2, 128], BF16); d_pm = Dep()
            rcp = sb("rcp", [128, 512], F32); d_rcp = Dep()
            omT = sb("omT", [128, 4, 128], BF16); d_omT = Dep()
            gs = sb("gs", [128, 3072], F32); d_gs = Dep()
            oat = sb("oat", [128, 512], BF16); d_oat = Dep()
            ost = sb("ost", [128, 1024], BF16); d_ost = Dep()
            oaT = sb("oaT", [128, 4, 128], BF16); d_oaT = Dep()
            osT = sb("osT", [128, 8, 128], BF16); d_osT = Dep()
            mg = sb("mg", [128, 512], F32); d_mg = Dep()
            tt = sb("tt", [128, 512], F32); d_tt = Dep()
            mgb = sb("mgb", [128, D], BF16); d_mgb = Dep()
            mgT = sb("mgT", [128, 8, 128], BF16); d_mgT = Dep()
            x1 = sb("x1", [128, D], F32); d_x1 = Dep()
            h2f = sb("h2f", [128, D], F32); d_h2f = Dep()
            h2b = [sb("h2b%d" % i, [128, D], BF16) for i in range(2)]; d_h2b = [Dep() for _ in range(2)]
            h2T = sb("h2T", [128, 8, 128], F32); d_h2T = Dep()
            lg = sb("lg", [128, 36], F32); d_lg = Dep()
            sm = sb("sm", [128, 64], F32); d_sm = Dep()
            goh = sb("goh", [128, 4], F32); d_goh = Dep()
            lsel = sb("lsel", [128, 32], F32); d_lsel = Dep()
            les = sb("les", [128, 8], F32); d_les = Dep()
            t8 = sb("t8", [128, 8], F32); d_t8 = Dep()
            oh = sb("oh", [128, 64], F32); d_oh = Dep()
            ohs = sb("ohs", [128, 32], F32); d_ohs = Dep()
            posf = sb("posf", [128, 32], F32); d_posf = Dep()
            idf2 = sb("idf2", [128, 2], F32); d_idf2 = Dep()

            pT = ps("pTd", [128, 8, 128], BF16); d_pT = Dep()
            pF = ps("pFd", [128, 512], F32); d_pF = Dep()
            pG = [ps("pGd%d" % i, [128, 512], F32) for i in range(3)]; d_pG = [Dep() for _ in range(3)]
            pms = [ps("pmsd%d" % i, [128, 512], F32) for i in range(2)]; d_pms = [Dep() for _ in range(2)]
            pmo = ps("pmod", [128, 512], F32); d_pmo = Dep()

            P.dma("sp", lambda e: e.dma_start(out=gm[:], in_=g_mix[0:1, :].partition_broadcast(128)), writes=[d_c])
            P.dma("sp", lambda e: e.dma_start(out=gf[:], in_=gffn_d[0:1, :].partition_broadcast(128)), writes=[d_c])
            P.dma("sp", lambda e: e.dma_start(out=mqn[:], in_=mqn_d[0:1, :].partition_broadcast(128)), writes=[d_c])
            P.dma("sp", lambda e: e.dma_start(out=mkn[:], in_=mkn_d[0:1, :].partition_broadcast(128)), writes=[d_c])
            P.dma("sp", lambda e: e.dma_start(out=tri[:], in_=tri_d[:, :]), writes=[d_c])
            P.dma("sp", lambda e: e.dma_start(out=ecap[:], in_=ecap_d[0:1, :].partition_broadcast(128)), writes=[d_c])
            P.op("pool", lambda e: e.memset(onesf[:], 1.0), writes=[d_c])
            P.op("pool", lambda e: e.memset(onesb[:], 1.0), writes=[d_c])
            P.op("pool", lambda e: e.memset(base[:], 0.0), writes=[d_base])

            def head_norm(src_ps, d_src, gain, nh, dst, d_dst):
                hd = 512 // nh
                v = lambda ap: ap.rearrange("p (h d) -> p h d", h=nh)
                P.op("act", lambda e: e.activation(out=sq[:], in_=src_ps, func=AF.Square), reads=[d_src], writes=[d_sq])
                P.op("dve", lambda e: e.tensor_reduce(out=hs[:, 0:nh], in_=v(sq[:]), axis=AX.X, op=ALU.add), reads=[d_sq], writes=[d_hs])
                P.op("act", lambda e: e.activation(out=hr[:, 0:nh], in_=hs[:, 0:nh], func=AF.Sqrt, bias=EPS, scale=1.0 / hd), reads=[d_hs], writes=[d_hr])
                P.op("dve", lambda e: e.reciprocal(out=hr[:, 0:nh], in_=hr[:, 0:nh]), reads=[d_hr], writes=[d_hr])
                P.op("dve", lambda e: e.tensor_tensor(out=v(qmn[:]), in0=v(src_ps), in1=hr[:, 0:nh].unsqueeze(2).to_broadcast([128, nh, hd]), op=ALU.mult),
                     reads=[d_src, d_hr], writes=[d_qmn])
                P.op("pool", lambda e: e.tensor_tensor(out=dst, in0=qmn[:], in1=gain[:], op=ALU.mult), reads=[d_qmn, d_c], writes=[d_dst])

            with contextlib.ExitStack() as stm:
                wkv = stm.enter_context(nc.sbuf_tensor("s_wkv", [128, 8, 1024], BF16)); d_wkv = Dep()
                mt_ = stm.enter_context(nc.sbuf_tensor("s_memt", [128, 2, D], F32)); d_mt = Dep()
                gme = stm.enter_context(nc.sbuf_tensor("s_gme", [128, D], F32)); d_gme = Dep()
                mhb = stm.enter_context(nc.sbuf_tensor("s_mhb", [128, 2, D], BF16)); d_mhb = Dep()
                mT = stm.enter_context(nc.sbuf_tensor("s_mT", [128, 8, 256], BF16)); d_mT = Dep()
                ss2 = stm.enter_context(nc.sbuf_tensor("s_ss2", [128, 2], F32)); d_ss2 = Dep()
                for k in range(8):
                    P.dma("pool", lambda e, k=k: e.dma_start(out=wkv[:, k, :], in_=wmemkv[k * 128:(k + 1) * 128, :]), writes=[d_wkv])
                P.dma("sp", lambda e: e.dma_start(out=mt_[:], in_=mem_d[:, :].rearrange("(t p) d -> p t d", p=128)), writes=[d_mt])
                P.dma("sp", lambda e: e.dma_start(out=gme[:], in_=gmem_d[0:1, :].partition_broadcast(128)), writes=[d_gme])
                for it in range(2):
                    P.op("act", lambda e, it=it: e.activation(out=junk[:], in_=mt_[:, it, :], func=AF.Square, accum_out=ss2[:, it:it + 1]),
                         reads=[d_mt], writes=[d_junk, d_ss2])
                P.op("act", lambda e: e.activation(out=ss2[:], in_=ss2[:], func=AF.Sqrt, bias=EPS, scale=1.0 / D), reads=[d_ss2], writes=[d_ss2])
                P.op("dve", lambda e: e.reciprocal(out=ss2[:], in_=ss2[:]), reads=[d_ss2], writes=[d_ss2])
                for it in range(2):
                    P.op("dve", lambda e, it=it: e.scalar_tensor_tensor(out=mhb[:, it, :], in0=mt_[:, it, :], scalar=ss2[:, it:it + 1], in1=gme[:],
                                                                        op0=ALU.mult, op1=ALU.mult), reads=[d_mt, d_ss2, d_gme], writes=[d_mhb])
                    for k in range(8):
                        P.op("pe", lambda e, it=it, k=k: e.transpose(out=pT[:, k, :], in_=mhb[:, it, k * 128:(k + 1) * 128], identity=idb[:]),
                             reads=[d_mhb, d_idb], writes=[d_pT])
                    P.op("act", lambda e, it=it: e.copy(out=mT[:, :, it * 128:(it + 1) * 128], in_=pT[:]), reads=[d_pT], writes=[d_mT])
                for it in range(2):
                    for hv in range(2):
                        for k in range(8):
                            P.op("pe", lambda e, it=it, hv=hv, k=k: e.matmul(pG[hv][:, :], lhsT=mT[:, k, it * 128:(it + 1) * 128], rhs=wkv[:, k, hv * 512:(hv + 1) * 512],
                                                                           start=(k == 0), stop=(k == 7)), reads=[d_mT, d_wkv], writes=[d_pG[hv]])
                    head_norm(pG[0][:, :], d_pG[0], mkn, 4, qmb[:], d_qmb)
                    for h in range(4):
                        P.op("pe", lambda e, h=h: e.transpose(out=pT[:, h, :], in_=qmb[:, h * 128:(h + 1) * 128], identity=idb[:]),
                             reads=[d_qmb, d_idb], writes=[d_pT])
                    P.op("act", lambda e, it=it: e.copy(out=kmT[:, :, it * 128:(it + 1) * 128], in_=pT[:, 0:4, :]), reads=[d_pT], writes=[d_kmT])
                    P.op("act", lambda e, it=it: e.copy(out=vm[:, it, :], in_=pG[1][:, :]), reads=[d_pG[1]], writes=[d_vm])

            wqm = sb("wqm", [128, 8, 512], BF16)
            wg = sb("wg", [128, 8, 3072], BF16)
            woa = sb("woa", [128, 4, 1024], BF16)
            wos = sb("wos", [128, 8, 1024], BF16)
            wom = sb("wom", [128, 4, 1024], BF16)
            wout = sb("wout", [128, 8, 1024], BF16)
            wr = sb("wr", [128, 8, 36], F32)
            for k in range(8):
                rows = slice(k * 128, (k + 1) * 128)
                P.dma("pool", lambda e, k=k, rows=rows: e.dma_start(out=wqm[:, k, :], in_=w_in[rows, C_QM:C_QM + 512]), writes=[d_w])
                for j in range(2):
                    P.dma("pool", lambda e, k=k, rows=rows, j=j: e.dma_start(out=wg[:, k, j * 1536:(j + 1) * 1536], in_=w_in[rows, C_G + j * 1536:C_G + (j + 1) * 1536]), writes=[d_w])
                P.dma("pool", lambda e, k=k, rows=rows: e.dma_start(out=wos[:, k, :], in_=wos_d[rows, :]), writes=[d_w])
                P.dma("pool", lambda e, k=k, rows=rows: e.dma_start(out=wout[:, k, :], in_=wout_d[rows, :]), writes=[d_w])
                P.dma("sp", lambda e, k=k, rows=rows: e.dma_start(out=wr[:, k, :], in_=wr_d[rows, :]), writes=[d_w])
            for k in range(4):
                rows = slice(k * 128, (k + 1) * 128)
                P.dma("pool", lambda e, k=k, rows=rows: e.dma_start(out=woa[:, k, :], in_=woa_d[rows, :]), writes=[d_w])
                P.dma("pool", lambda e, k=k, rows=rows: e.dma_start(out=wom[:, k, :], in_=wom_d[rows, :]), writes=[d_w])
            for ti in range(ntiles):
                r0 = ti * 128
                hbuf = ti % 2
                P.dma("sp", lambda e, r0=r0: e.dma_start(out=xt[:], in_=xown[r0:r0 + 128, :]), writes=[d_xt])
                P.dma("sp", lambda e, r0=r0: e.dma_start(out=oat[:], in_=oa_d[r0:r0 + 128, :]), reads=[d_oad], writes=[d_oat])
                P.dma("sp", lambda e, r0=r0: e.dma_start(out=ost[:], in_=os_d[r0:r0 + 128, :]), reads=[d_osd], writes=[d_ost])
                P.op("act", lambda e: e.activation(out=junk[:], in_=xt[:], func=AF.Square, accum_out=ssq[:]), reads=[d_xt], writes=[d_junk, d_ssq])
                P.op("act", lambda e: e.activation(out=rstd[:], in_=ssq[:], func=AF.Sqrt, bias=EPS, scale=1.0 / D), reads=[d_ssq], writes=[d_rstd])
                P.op("dve", lambda e: e.reciprocal(out=rstd[:], in_=rstd[:]), reads=[d_rstd], writes=[d_rstd])
                P.op("dve", lambda e: e.scalar_tensor_tensor(out=hb[:], in0=xt[:], scalar=rstd[:, 0:1], in1=gm[:], op0=ALU.mult, op1=ALU.mult),
                     reads=[d_xt, d_rstd, d_c], writes=[d_hb])
                for k in range(8):
                    P.op("pe", lambda e, k=k: e.transpose(out=pT[:, k, :], in_=hb[:, k * 128:(k + 1) * 128], identity=idb[:]), reads=[d_hb, d_idb], writes=[d_pT])
                P.op("act", lambda e: e.copy(out=hT[:], in_=pT[:]), reads=[d_pT], writes=[d_hT])
                for k in range(8):
                    P.op("pe", lambda e, k=k: e.matmul(pG[0][:, :], lhsT=hT[:, k, :], rhs=wqm[:, k, :], start=(k == 0), stop=(k == 7)),
                         reads=[d_hT, d_w], writes=[d_pG[0]])
                head_norm(pG[0][:, :], d_pG[0], mqn, 4, qmb[:], d_qmb)
                for h in range(4):
                    P.op("pe", lambda e, h=h: e.transpose(out=pT[:, h, :], in_=qmb[:, h * 128:(h + 1) * 128], identity=idb[:]), reads=[d_qmb, d_idb], writes=[d_pT])
                P.op("act", lambda e: e.copy(out=qmT[:], in_=pT[:, 0:4, :]), reads=[d_pT], writes=[d_qmT])
                for h in range(4):
                    for mt in range(2):
                        bank = pms[h // 2]
                        c0 = ((h % 2) * 2 + mt) * 128
                        P.op("pe", lambda e, h=h, mt=mt, bank=bank, c0=c0: e.matmul(bank[:, c0:c0 + 128], lhsT=kmT[:, h, mt * 128:(mt + 1) * 128], rhs=qmT[:, h, :],
                                                                                  start=True, stop=True), reads=[d_kmT, d_qmT], writes=[d_pms[h // 2]])
                for hp in range(2):
                    P.op("act", lambda e, hp=hp: e.activation(out=pm[:, hp * 2:(hp + 1) * 2, :, :].rearrange("p a b c -> p (a b c)"), in_=pms[hp][:, :], func=AF.Exp,
                                                              scale=float(128 ** -0.5)), reads=[d_pms[hp]], writes=[d_pm])
                for h in range(4):
                    for mt in range(2):
                        P.op("pe", lambda e, h=h, mt=mt: e.matmul(pmo[:, h * 128:(h + 1) * 128], lhsT=vm[:, mt, h * 128:(h + 1) * 128], rhs=pm[:, h, mt, :],
                                                                 start=(mt == 0), stop=(mt == 1)), reads=[d_vm, d_pm], writes=[d_pmo])
                for mt in range(2):
                    P.op("pe", lambda e, mt=mt: e.matmul(pG[1][:, :].rearrange("p (h t) -> p h t", h=4), lhsT=onesb[:], rhs=pm[:, :, mt, :],
                                                        start=(mt == 0), stop=(mt == 1)), reads=[d_c, d_pm], writes=[d_pG[1]])
                P.op("dve", lambda e: e.reciprocal(out=rcp[:], in_=pG[1][:, :]), reads=[d_pG[1]], writes=[d_rcp])
                P.op("dve", lambda e: e.tensor_tensor(out=omT[:].rearrange("p h t -> p (h t)"), in0=pmo[:, :], in1=rcp[:], op=ALU.mult),
                     reads=[d_pmo, d_rcp], writes=[d_omT])
                for g6 in range(6):
                    bk = g6 % 3
                    for k in range(8):
                        P.op("pe", lambda e, g6=g6, k=k, bk=bk: e.matmul(pG[bk][:, :], lhsT=hT[:, k, :], rhs=wg[:, k, g6 * 512:(g6 + 1) * 512], start=(k == 0), stop=(k == 7)),
                             reads=[d_hT, d_w], writes=[d_pG[bk]])
                    P.op("act", lambda e, g6=g6, bk=bk: e.activation(out=gs[:, g6 * 512:(g6 + 1) * 512], in_=pG[bk][:, :], func=AF.Sigmoid),
                         reads=[d_pG[bk]], writes=[d_gs])
                for k in range(4):
                    P.op("pe", lambda e, k=k: e.transpose(out=pT[:, k, :], in_=oat[:, k * 128:(k + 1) * 128], identity=idb[:]), reads=[d_oat, d_idb], writes=[d_pT])
                P.op("act", lambda e: e.copy(out=oaT[:], in_=pT[:, 0:4, :]), reads=[d_pT], writes=[d_oaT])
                for k in range(8):
                    P.op("pe", lambda e, k=k: e.transpose(out=pT[:, k, :], in_=ost[:, k * 128:(k + 1) * 128], identity=idb[:]), reads=[d_ost, d_idb], writes=[d_pT])
                P.op("act", lambda e: e.copy(out=osT[:], in_=pT[:]), reads=[d_pT], writes=[d_osT])
                for hv in range(2):
                    cs_ = slice(hv * 512, (hv + 1) * 512)
                    for k in range(4):
                        P.op("pe", lambda e, k=k, cs_=cs_: e.matmul(pG[0][:, :], lhsT=oaT[:, k, :], rhs=woa[:, k, cs_], start=(k == 0), stop=(k == 3)),
                             reads=[d_oaT, d_w], writes=[d_pG[0]])
                    for k in range(8):
                        P.op("pe", lambda e, k=k, cs_=cs_: e.matmul(pG[1][:, :], lhsT=osT[:, k, :], rhs=wos[:, k, cs_], start=(k == 0), stop=(k == 7)),
                             reads=[d_osT, d_w], writes=[d_pG[1]])
                    for k in range(4):
                        P.op("pe", lambda e, k=k, cs_=cs_: e.matmul(pG[2][:, :], lhsT=omT[:, k, :], rhs=wom[:, k, cs_], start=(k == 0), stop=(k == 3)),
                             reads=[d_omT, d_w], writes=[d_pG[2]])
                    P.op("dve", lambda e, hv=hv: e.tensor_tensor(out=mg[:], in0=pG[0][:, :], in1=gs[:, hv * 512:(hv + 1) * 512], op=ALU.mult),
                         reads=[d_pG[0], d_gs], writes=[d_mg])
                    P.op("dve", lambda e, hv=hv: e.tensor_tensor(out=tt[:], in0=pG[1][:, :], in1=gs[:, 1024 + hv * 512:1024 + (hv + 1) * 512], op=ALU.mult),
                         reads=[d_pG[1], d_gs], writes=[d_tt])
                    P.op("pool", lambda e: e.tensor_tensor(out=mg[:], in0=mg[:], in1=tt[:], op=ALU.add), reads=[d_mg, d_tt], writes=[d_mg])
                    P.op("dve", lambda e, hv=hv: e.tensor_tensor(out=tt[:], in0=pG[2][:, :], in1=gs[:, 2048 + hv * 512:2048 + (hv + 1) * 512], op=ALU.mult),
                         reads=[d_pG[2], d_gs], writes=[d_tt])
                    P.op("pool", lambda e, cs_=cs_: e.tensor_tensor(out=mgb[:, cs_], in0=mg[:], in1=tt[:], op=ALU.add), reads=[d_mg, d_tt], writes=[d_mgb])
                for k in range(8):
                    P.op("pe", lambda e, k=k: e.transpose(out=pT[:, k, :], in_=mgb[:, k * 128:(k + 1) * 128], identity=idb[:]), reads=[d_mgb, d_idb], writes=[d_pT])
                P.op("act", lambda e: e.copy(out=mgT[:], in_=pT[:]), reads=[d_pT], writes=[d_mgT])
                for hv in range(2):
                    cs_ = slice(hv * 512, (hv + 1) * 512)
                    for k in range(8):
                        P.op("pe", lambda e, k=k, cs_=cs_, hv=hv: e.matmul(pG[hv][:, :], lhsT=mgT[:, k, :], rhs=wout[:, k, cs_], start=(k == 0), stop=(k == 7)),
                             reads=[d_mgT, d_w], writes=[d_pG[hv]])
                    P.op("dve", lambda e, cs_=cs_, hv=hv: e.tensor_tensor(out=x1[:, cs_], in0=pG[hv][:, :], in1=xt[:, cs_], op=ALU.add),
                         reads=[d_pG[hv], d_xt], writes=[d_x1])
                P.dma("sp", lambda e, r0=r0: e.dma_start(out=x1_d[r0:r0 + 128, :], in_=x1[:]), reads=[d_x1], writes=[d_x1d])
                if stage < 2:
                    continue
                P.op("act", lambda e: e.activation(out=junk[:], in_=x1[:], func=AF.Square, accum_out=ssq[:]), reads=[d_x1], writes=[d_junk, d_ssq])
                P.op("act", lambda e: e.activation(out=rstd[:], in_=ssq[:], func=AF.Sqrt, bias=EPS, scale=1.0 / D), reads=[d_ssq], writes=[d_rstd])
                P.op("dve", lambda e: e.reciprocal(out=rstd[:], in_=rstd[:]), reads=[d_rstd], writes=[d_rstd])
                P.op("dve", lambda e: e.scalar_tensor_tensor(out=h2f[:], in0=x1[:], scalar=rstd[:, 0:1], in1=gf[:], op0=ALU.mult, op1=ALU.mult),
                     reads=[d_x1, d_rstd, d_c], writes=[d_h2f])
                P.op("pool", lambda e, hbuf=hbuf: e.tensor_copy(out=h2b[hbuf][:], in_=h2f[:]), reads=[d_h2f], writes=[d_h2b[hbuf]])
                for half in range(2):
                    for k4 in range(4):
                        k = half * 4 + k4
                        P.op("pe", lambda e, k=k, k4=k4: e.transpose(out=pF[:, k4 * 128:(k4 + 1) * 128], in_=h2f[:, k * 128:(k + 1) * 128], identity=idf[:]),
                             reads=[d_h2f, d_idf], writes=[d_pF])
                    P.op("act", lambda e, half=half: e.copy(out=h2T[:, half * 4:(half + 1) * 4, :].rearrange("p a b -> p (a b)"), in_=pF[:, :]), reads=[d_pF], writes=[d_h2T])
                for k in range(8):
                    P.op("pe", lambda e, k=k: e.matmul(pF[:, 0:36], lhsT=h2T[:, k, :], rhs=wr[:, k, :], start=(k == 0), stop=(k == 7)), reads=[d_h2T, d_w], writes=[d_pF])
                P.op("act", lambda e: e.copy(out=lg[:], in_=pF[:, 0:36]), reads=[d_pF], writes=[d_lg])
                P.op("dve", lambda e: e.tensor_reduce(out=sm[:, 0:1], in_=lg[:, 0:4], axis=AX.X, op=ALU.max), reads=[d_lg], writes=[d_sm])
                P.op("dve", lambda e: e.tensor_scalar(out=sm[:, 1:2], in0=sm[:, 0:1], scalar1=-1.0, scalar2=None, op0=ALU.mult), reads=[d_sm], writes=[d_sm])
                P.op("act", lambda e: e.activation(out=sm[:, 8:12], in_=lg[:, 0:4], func=AF.Exp, bias=sm[:, 1:2], accum_out=sm[:, 2:3]), reads=[d_lg, d_sm], writes=[d_sm])
                P.op("dve", lambda e: e.reciprocal(out=sm[:, 3:4], in_=sm[:, 2:3]), reads=[d_sm], writes=[d_sm])
                P.op("dve", lambda e: e.tensor_scalar(out=goh[:], in0=lg[:, 0:4], scalar1=sm[:, 0:1], scalar2=None, op0=ALU.is_ge), reads=[d_lg, d_sm], writes=[d_goh])
                P.op("dve", lambda e: e.tensor_tensor(out=lsel[:].rearrange("p (g e) -> p g e", g=4), in0=lg[:, 4:36].rearrange("p (g e) -> p g e", g=4),
                                                      in1=goh[:].unsqueeze(2).to_broadcast([128, 4, 8]), op=ALU.mult), reads=[d_lg, d_goh], writes=[d_lsel])
                P.op("dve", lambda e: e.tensor_reduce(out=les[:], in_=lsel[:].rearrange("p (g e) -> p e g", g=4), axis=AX.X, op=ALU.add), reads=[d_lsel], writes=[d_les])
                P.op("dve", lambda e: e.max(out=t8[:], in_=les[:]), reads=[d_les], writes=[d_t8])
                P.op("dve", lambda e: e.tensor_tensor(out=sm[:, 4:5], in0=t8[:, 1:2], in1=t8[:, 0:1], op=ALU.subtract), reads=[d_t8, d_sm], writes=[d_sm])
                P.op("act", lambda e: e.activation(out=sm[:, 5:6], in_=sm[:, 4:5], func=AF.Exp), reads=[d_sm], writes=[d_sm])
                P.op("dve", lambda e: e.tensor_scalar(out=sm[:, 6:7], in0=sm[:, 5:6], scalar1=1.0, scalar2=None, op0=ALU.add), reads=[d_sm], writes=[d_sm])
                P.op("dve", lambda e: e.reciprocal(out=sm[:, 6:7], in_=sm[:, 6:7]), reads=[d_sm], writes=[d_sm])
                P.op("dve", lambda e: e.tensor_tensor(out=sm[:, 7:8], in0=sm[:, 5:6], in1=sm[:, 6:7], op=ALU.mult), reads=[d_sm], writes=[d_sm])
                P.op("dve", lambda e, ti=ti: e.tensor_scalar(out=combs[:, ti, :], in0=sm[:, 6:8], scalar1=sm[:, 3:4], scalar2=None, op0=ALU.mult), reads=[d_sm], writes=[d_combs])
                for j in range(2):
                    P.op("dve", lambda e, j=j: e.tensor_scalar(out=oh[:, j * 32:(j + 1) * 32], in0=lg[:, 4:36], scalar1=t8[:, j:j + 1], scalar2=None, op0=ALU.is_equal),
                         reads=[d_lg, d_t8], writes=[d_oh])
                    P.op("dve", lambda e, j=j: e.tensor_tensor(out=oh[:, j * 32:(j + 1) * 32].rearrange("p (g e) -> p g e", g=4), in0=oh[:, j * 32:(j + 1) * 32].rearrange("p (g e) -> p g e", g=4),
                                                               in1=goh[:].unsqueeze(2).to_broadcast([128, 4, 8]), op=ALU.mult), reads=[d_oh, d_goh], writes=[d_oh])
                P.op("dve", lambda e: e.tensor_tensor(out=ohs[:], in0=oh[:, 0:32], in1=oh[:, 32:64], op=ALU.add), reads=[d_oh], writes=[d_ohs])
                P.op("pe", lambda e: e.matmul(pF[:, 64:96], lhsT=tri[:], rhs=ohs[:], start=True, stop=True), reads=[d_c, d_ohs], writes=[d_pF])
                P.op("pe", lambda e: e.matmul(pF[:, 128:160], lhsT=onesf[:], rhs=ohs[:], start=True, stop=True), reads=[d_c, d_ohs], writes=[d_pF])
                P.op("dve", lambda e: e.tensor_tensor(out=posf[:], in0=pF[:, 64:96], in1=ohs[:], op=ALU.subtract), reads=[d_pF, d_ohs], writes=[d_posf])
                P.op("dve", lambda e: e.tensor_tensor(out=posf[:], in0=posf[:], in1=base[:], op=ALU.add), reads=[d_posf, d_base], writes=[d_posf])
                P.op("dve", lambda e: e.tensor_tensor(out=posf[:], in0=posf[:], in1=ecap[:], op=ALU.add), reads=[d_posf, d_c], writes=[d_posf])
                P.op("dve", lambda e: e.tensor_tensor(out=base[:], in0=base[:], in1=pF[:, 128:160], op=ALU.add), reads=[d_base, d_pF, d_posf], writes=[d_base])
                for j in range(2):
                    P.op("dve", lambda e, j=j: e.tensor_tensor(out=oh[:, j * 32:(j + 1) * 32], in0=oh[:, j * 32:(j + 1) * 32], in1=posf[:], op=ALU.mult), reads=[d_oh, d_posf], writes=[d_oh])
                    P.op("dve", lambda e, j=j: e.tensor_reduce(out=idf2[:, j:j + 1], in_=oh[:, j * 32:(j + 1) * 32], axis=AX.X, op=ALU.add), reads=[d_oh], writes=[d_idf2])
                P.op("dve", lambda e, ti=ti: e.tensor_copy(out=idxs[:, ti, :], in_=idf2[:]), reads=[d_idf2], writes=[d_idxs])
                for j in range(2):
                    P.dma("pool", lambda e, ti=ti, j=j, hbuf=hbuf: e.indirect_dma_start(out=xbuf[:, :], out_offset=bass.IndirectOffsetOnAxis(ap=idxs[:, ti, j:j + 1], axis=0),
                                                                                      in_=h2b[hbuf][:], in_offset=None),
                          reads=[d_h2b[hbuf], d_idxs], writes=[d_xbuf])

        if stage < 3:
            return
        if hasattr(P, 'barrier'):
            P.barrier()
        with contextlib.ExitStack() as st:
            def sb(name, shape, dt):
                return st.enter_context(nc.sbuf_tensor("d_" + name, shape, dt))

            def ps(name, shape, dt=F32):
                return st.enter_context(nc.psum_tensor(name, shape, dt))
            NW = 2
            wge = [sb("wge%d" % i, [128, 8, 512], BF16) for i in range(NW)]; d_wge = [Dep() for _ in range(NW)]
            wue = [sb("wue%d" % i, [128, 8, 512], BF16) for i in range(NW)]; d_wue = [Dep() for _ in range(NW)]
            wde = [sb("wde%d" % i, [128, 4, 1024], BF16) for i in range(NW)]; d_wde = [Dep() for _ in range(NW)]
            xe = [sb("xe%d" % i, [128, 3, D], BF16) for i in range(NW)]; d_xe = [Dep() for _ in range(NW)]
            xeT = sb("xeT", [128, 8, CAP], BF16); d_xeT = Dep()
            sg = sb("sg", [128, CAP], F32); d_sg = Dep()
            hTe = sb("hTe", [128, 4, CAP], BF16); d_hTe = Dep()
            ye = [sb("ye%d" % i, [128, 3, D], F32) for i in range(NW)]; d_ye = [Dep() for _ in range(NW)]
            pT = ps("pTe", [128, 8, 128], BF16); d_pT = Dep()
            pg_ = [ps("pge%d" % i, [128, 512], F32) for i in range(2)]; d_pg = [Dep() for _ in range(2)]
            pu_ = [ps("pue%d" % i, [128, 512], F32) for i in range(2)]; d_pu = [Dep() for _ in range(2)]
            py_ = [ps("pye%d" % i, [128, 512], F32) for i in range(2)]; d_py = [Dep() for _ in range(2)]
            for ie, ex in enumerate(experts):
                b = ie % NW
                P.dma("pool", lambda e, b=b, ex=ex: e.dma_start(out=wge[b][:], in_=wgate_d[ex].rearrange("(k p) f -> p k f", p=128)), writes=[d_wge[b]])
                P.dma("pool", lambda e, b=b, ex=ex: e.dma_start(out=wue[b][:], in_=wup_d[ex].rearrange("(k p) f -> p k f", p=128)), writes=[d_wue[b]])
                P.dma("pool", lambda e, b=b, ex=ex: e.dma_start(out=wde[b][:], in_=wdown_d[ex].rearrange("(k p) f -> p k f", p=128)), writes=[d_wde[b]])
                P.dma("sp", lambda e, b=b, ex=ex: e.dma_start(out=xe[b][:], in_=xbuf[ex * CAP:(ex + 1) * CAP, :].rearrange("(s p) d -> p s d", p=128)),
                      reads=[d_xbuf], writes=[d_xe[b]])
                for s in range(3):
                    for k in range(8):
                        P.op("pe", lambda e, b=b, s=s, k=k: e.transpose(out=pT[:, k, :], in_=xe[b][:, s, k * 128:(k + 1) * 128], identity=idb[:]),
                             reads=[d_xe[b], d_idb], writes=[d_pT])
                    P.op("act", lambda e, s=s: e.copy(out=xeT[:, :, s * 128:(s + 1) * 128], in_=pT[:]), reads=[d_pT], writes=[d_xeT])
                for ft in range(4):
                    pb = ft % 2
                    for k in range(8):
                        P.op("pe", lambda e, b=b, ft=ft, k=k, pb=pb: e.matmul(pg_[pb][:, 0:CAP], lhsT=wge[b][:, k, ft * 128:(ft + 1) * 128], rhs=xeT[:, k, :],
                                                                            start=(k == 0), stop=(k == 7)), reads=[d_wge[b], d_xeT], writes=[d_pg[pb]])
                    for k in range(8):
                        P.op("pe", lambda e, b=b, ft=ft, k=k, pb=pb: e.matmul(pu_[pb][:, 0:CAP], lhsT=wue[b][:, k, ft * 128:(ft + 1) * 128], rhs=xeT[:, k, :],
                                                                            start=(k == 0), stop=(k == 7)), reads=[d_wue[b], d_xeT], writes=[d_pu[pb]])
                    P.op("act", lambda e, pb=pb: e.activation(out=sg[:], in_=pg_[pb][:, 0:CAP], func=AF.Silu), reads=[d_pg[pb]], writes=[d_sg])
                    P.op("dve", lambda e, ft=ft, pb=pb: e.tensor_tensor(out=hTe[:, ft, :], in0=sg[:], in1=pu_[pb][:, 0:CAP], op=ALU.mult),
                         reads=[d_sg, d_pu[pb]], writes=[d_hTe])
                for s in range(3):
                    for hv in range(2):
                        for ft in range(4):
                            P.op("pe", lambda e, b=b, s=s, hv=hv, ft=ft: e.matmul(py_[hv][:, :], lhsT=hTe[:, ft, s * 128:(s + 1) * 128], rhs=wde[b][:, ft, hv * 512:(hv + 1) * 512],
                                                                                start=(ft == 0), stop=(ft == 3)), reads=[d_hTe, d_wde[b]], writes=[d_py[hv]])
                        P.op("act" if hv == 0 else "dve", (lambda e, b=b, s=s, hv=hv: e.copy(out=ye[b][:, s, hv * 512:(hv + 1) * 512], in_=py_[hv][:, :])) if hv == 0 else
                             (lambda e, b=b, s=s, hv=hv: e.tensor_copy(out=ye[b][:, s, hv * 512:(hv + 1) * 512], in_=py_[hv][:, :])),
                             reads=[d_py[hv]], writes=[d_ye[b]])
                P.dma("sp", lambda e, b=b, ex=ex: e.dma_start(out=ybuf[ex * CAP:(ex + 1) * CAP, :].rearrange("(s p) d -> p s d", p=128), in_=ye[b][:]),
                      reads=[d_ye[b]], writes=[d_ybuf])

        if hasattr(P, 'barrier'):
            P.barrier()
        with contextlib.ExitStack() as st:
            def sb(name, shape, dt):
                return st.enter_context(nc.sbuf_tensor("d_" + name, shape, dt))
            NF = 2
            x1t = [sb("x1t%d" % i, [128, D], F32) for i in range(NF)]; d_x1t = [Dep() for _ in range(NF)]
            y1 = [sb("y1t%d" % i, [128, D], F32) for i in range(NF)]; d_y1 = [Dep() for _ in range(NF)]
            y2 = [sb("y2t%d" % i, [128, D], F32) for i in range(NF)]; d_y2 = [Dep() for _ in range(NF)]
            ot = [sb("ot%d" % i, [128, D], F32) for i in range(NF)]; d_ot = [Dep() for _ in range(NF)]
            for ti in range(ntiles):
                b = ti % NF
                r0 = ti * 128
                P.dma("sp", lambda e, b=b, r0=r0: e.dma_start(out=x1t[b][:], in_=x1_d[r0:r0 + 128, :]), reads=[d_x1d], writes=[d_x1t[b]])
                P.dma("pool", lambda e, b=b, ti=ti: e.indirect_dma_start(out=y1[b][:], out_offset=None, in_=ybuf[:, :],
                                                                        in_offset=bass.IndirectOffsetOnAxis(ap=idxs[:, ti, 0:1], axis=0)),
                      reads=[d_ybuf, d_idxs], writes=[d_y1[b]])
                P.dma("pool", lambda e, b=b, ti=ti: e.indirect_dma_start(out=y2[b][:], out_offset=None, in_=ybuf[:, :],
                                                                        in_offset=bass.IndirectOffsetOnAxis(ap=idxs[:, ti, 1:2], axis=0)),
                      reads=[d_ybuf, d_idxs], writes=[d_y2[b]])
                P.op("dve", lambda e, b=b, ti=ti: e.scalar_tensor_tensor(out=ot[b][:], in0=y1[b][:], scalar=combs[:, ti, 0:1], in1=x1t[b][:], op0=ALU.mult, op1=ALU.add),
                     reads=[d_y1[b], d_combs, d_x1t[b]], writes=[d_ot[b]])
                P.op("dve", lambda e, b=b, ti=ti: e.scalar_tensor_tensor(out=ot[b][:], in0=y2[b][:], scalar=combs[:, ti, 1:2], in1=ot[b][:], op0=ALU.mult, op1=ALU.add),
                     reads=[d_y2[b], d_combs, d_ot[b]], writes=[d_ot[b]])
                P.dma("sp", lambda e, b=b, r0=r0: e.dma_start(out=out_d[r0:r0 + 128, :], in_=ot[b][:]), reads=[d_ot[b]], writes=[d_out])


def host_consts_d(z):
    tri = (np.arange(128)[:, None] <= np.arange(128)[None, :]).astype(np.float32)
    return dict(mqn=np.tile(z['mem_q_norm'], 4)[None, :], mkn=np.tile(z['mem_k_norm'], 4)[None, :],
                w_r=np.ascontiguousarray(np.concatenate([z['w_router_group'], z['w_router_expert']], axis=1)),
                trif=tri, ecap=(np.arange(32, dtype=np.float32) * CAP)[None, :], ident=np.eye(128, dtype=np.float32))


def build_full():
    nc = bass.Bass("TRN2", target_bir_lowering=False)
    P = Prog(nc)

    def di(name, shape, dt=F32):
        return nc.dram_tensor(name, shape, dt, kind="ExternalInput")
    xin = di("xin", [TP + T, D]); w_in = di("w_in", [D, IN_COLS]); g_mix = di("g_mix", [1, D])
    qkn = di("qkn", [2, 512]); cs = di("cs", [TP + T, 64]); ident_d = di("ident", [128, 128])
    e_d = di("e_d", [32, NK], BF16); vq_d = di("vq", [1, 1024]); nb_d = di("nb", [1, 1024]); oq_d = di("oq", [1, 1024])
    tri_d = di("tri", [128, 128], BF16)
    cwl_d = di("cwl", [128, 16, 4]); cbl_d = di("cbl", [128, 16]); dtb_d = di("dtb", [1, 16]); alog_d = di("alog", [1, 16])
    dsk_d = di("dsk", [1, 16]); ssdn_d = di("ssdn", [1, D]); tri2_d = di("tri2", [128, 2, 256]); sel_d = di("sel_d", [16, 16, 128])
    tb_d = di("tb_d", [128, 384]); pflag_d = di("pflag", [1, 1])
    mem_d = di("mem", [256, D]); gmem_d = di("g_mem", [1, D]); wmemkv = di("w_mem_kv", [D, 1024])
    mqn_d = di("mqn", [1, 512]); mkn_d = di("mkn", [1, 512])
    woa_d = di("w_o_moba", [512, D]); wos_d = di("w_o_ssd", [D, D]); wom_d = di("w_o_mem", [512, D]); wout_d = di("w_out", [D, D])
    gffn_d = di("g_ffn", [1, D]); wr_d = di("w_r", [D, 36])
    wgate_d = di("w_gate", [NE, D, 512]); wup_d = di("w_up", [NE, D, 512]); wdown_d = di("w_down", [NE, 512, D])
    trif_d = di("trif", [128, 128]); ecap_d = di("ecap", [1, 32])
    out_d = nc.dram_tensor("out", [T, D], F32, kind="ExternalOutput")
    kT_d = nc.dram_tensor("kT_d", [512, NK], BF16)
    v_d = nc.dram_tensor("v_d", [NK, 512], BF16)
    qT_d = nc.dram_tensor("qT_d", [512, T], BF16)
    oa_d = nc.dram_tensor("oa_d", [T, 512], BF16)
    os_d = nc.dram_tensor("os_d", [T, D], BF16)
    x1_d = nc.dram_tensor("x1_d", [T, D], F32)
    xbuf = nc.dram_tensor("xbuf", [NE * CAP, D], BF16)
    ybuf = nc.dram_tensor("ybuf", [NE * CAP, D], F32)

    with contextlib.ExitStack() as st0:
        idf = st0.enter_context(nc.sbuf_tensor("idf", [128, 128], F32)); d_idf = Dep()
        idb = st0.enter_context(nc.sbuf_tensor("idb", [128, 128], BF16)); d_idb = Dep()
        P.dma("sp", lambda e: e.dma_start(out=idf[:], in_=ident_d[:, :]), writes=[d_idf])
        P.op("dve", lambda e: e.tensor_copy(out=idb[:], in_=idf[:]), reads=[d_idf], writes=[d_idb])
        with contextlib.ExitStack() as st:
            phase_a(nc, P, st, xin, w_in, g_mix, qkn, cs, idb, d_idb, kT_d, v_d, qT_d)
        P.barrier()
        with contextlib.ExitStack() as st:
            oa = st.enter_context(nc.sbuf_tensor("s_oa", [128, 32, 512], BF16)); d_oa = Dep()
            phase_b(nc, P, st, kT_d, v_d, qT_d, e_d, vq_d, nb_d, oq_d, tri_d, idb, d_idb, oa, d_oa)
            P.dma("sp", lambda e: e.dma_start(out=oa_d[:, :].rearrange("(t p) c -> p t c", p=128), in_=oa[:]), reads=[d_oa])
        P.barrier()
        with contextlib.ExitStack() as st:
            phase_c(nc, P, st, xin, w_in, g_mix, cwl_d, cbl_d, dtb_d, alog_d, dsk_d, ssdn_d, tri2_d, sel_d, tb_d, pflag_d,
                    idb, d_idb, os_d, Dep())
        P.barrier()
        phase_def(nc, P, xin[TP:TP + T, :], w_in, g_mix, mem_d, gmem_d, wmemkv, mqn_d, mkn_d, woa_d, wos_d, wom_d, wout_d, gffn_d, wr_d,
                  wgate_d, wup_d, wdown_d, trif_d, ecap_d, oa_d, Dep(), os_d, Dep(), x1_d, xbuf, ybuf, out_d, idb, d_idb, idf, d_idf)
        P.emit()
    return nc


_NC_CACHE = {}


def kernel(x, mem, g_mix, w_in, moba_q_norm, moba_k_norm, conv_w, conv_b, dt_bias, a_log, d_skip, ssd_norm, g_mem, w_mem_kv,
           mem_q_norm, mem_k_norm, w_o_moba, w_o_ssd, w_o_mem, w_out, g_ffn, w_router_group, w_router_expert, w_gate, w_up, w_down):
    f32 = np.float32
    A = lambda a: np.ascontiguousarray(np.asarray(a, dtype=f32))
    x = A(x); mem = A(mem)
    z = dict(conv_w=A(conv_w), conv_b=A(conv_b), dt_bias=A(dt_bias), a_log=A(a_log), d_skip=A(d_skip), ssd_norm=A(ssd_norm),
             mem_q_norm=A(mem_q_norm), mem_k_norm=A(mem_k_norm), w_router_group=A(w_router_group), w_router_expert=A(w_router_expert))
    if "nc" not in _NC_CACHE:
        _NC_CACHE["nc"] = build_full()
    nc = _NC_CACHE["nc"]
    pos = np.arange(2 * T, dtype=f32)
    inv = (10000.0 ** (-np.arange(32, dtype=f32) / 32)).astype(f32)
    ang = pos[:, None] * inv[None, :]
    cs_full = np.concatenate([np.cos(ang), np.sin(ang)], axis=1).astype(f32)
    qkn = np.stack([np.tile(A(moba_q_norm), 8), np.tile(A(moba_k_norm), 8)]).astype(f32)
    shared = dict(w_in=A(w_in), g_mix=A(g_mix)[None, :], qkn=qkn, ident=np.eye(128, dtype=f32),
                  g_mem=A(g_mem)[None, :], w_mem_kv=A(w_mem_kv), w_o_moba=A(w_o_moba), w_o_ssd=A(w_o_ssd), w_o_mem=A(w_o_mem),
                  w_out=A(w_out), g_ffn=A(g_ffn)[None, :], w_gate=A(w_gate), w_up=A(w_up), w_down=A(w_down))
    shared.update(host_consts_d(z))
    in_maps = []
    for c in range(8):
        bb, hh = c // 2, c % 2
        if hh == 0:
            xin = np.concatenate([np.zeros((TP, D), f32), x[bb, :T]], 0)
            csc = np.concatenate([cs_full[:TP], cs_full[:T]], 0)
        else:
            xin = x[bb]
            csc = cs_full
        m = dict(shared)
        m.update(xin=np.ascontiguousarray(xin), cs=np.ascontiguousarray(csc), mem=mem[bb])
        m.update(host_consts(hh))
        m.update(host_consts_c(z, hh))
        in_maps.append(m)
    res = run_bass_kernel_spmd(nc, in_maps, core_ids=list(range(8)))
    out = np.empty((4, 2 * T, D), f32)
    for c in range(8):
        bb, hh = c // 2, c % 2
        out[bb, hh * T:(hh + 1) * T] = np.asarray(res.results[c]["out"], dtype=f32)
    return out
```

```python
import contextlib
import numpy as np
import ml_dtypes
import concourse.bass as bass
import concourse.mybir as mybir
from concourse.bass_utils import run_bass_kernel_spmd

F32 = mybir.dt.float32
BF16 = mybir.dt.bfloat16
I32 = mybir.dt.int32
U32 = mybir.dt.uint32
AF = mybir.ActivationFunctionType
ALU = mybir.AluOpType
AX = mybir.AxisListType

T = 4096
TP = 4096
NK = TP + T
D = 1024
EPS = 1e-6
IN_COLS = 8208
BIGG = 30000.0
MB = 1000.0
NEG = -30000.0
C_Z, C_X, C_DT = 1536, 2560, 4608
C_QM, C_G = 4624, 5136
CAP = 384
NE = 32


class Dep:
    __slots__ = ("w", "r")

    def __init__(self):
        self.w = None
        self.r = {}


class Op:
    __slots__ = ("eng", "fn", "deps", "is_dma", "sig", "sigval", "dsem", "dval", "prev")

    def __init__(self, eng, fn, is_dma):
        self.eng = eng
        self.fn = fn
        self.is_dma = is_dma
        self.deps = []
        self.sig = False
        self.sigval = 0
        self.dsem = None
        self.dval = 0
        self.prev = None


class Prog:
    ENGS = ("pe", "act", "dve", "pool", "sp")

    def __init__(self, nc, ndma_sems=12):
        self.nc = nc
        self.ops = {e: [] for e in self.ENGS}
        self.ndma = {e: 0 for e in self.ENGS}
        self.dma_last = {}
        self.ndma_sems = ndma_sems
        self.all_dma = []

    def _add(self, o, reads, writes):
        deps = {}
        for t in reads:
            if t.w is not None:
                deps[id(t.w)] = t.w
        for t in writes:
            if t.w is not None:
                deps[id(t.w)] = t.w
            for r in t.r.values():
                deps[id(r)] = r
        for t in reads:
            key = id(o) if o.is_dma else o.eng
            t.r[key] = o
        for t in writes:
            t.w = o
            t.r = {}
        dl = []
        for d in deps.values():
            if d is o:
                continue
            if (not d.is_dma) and (not o.is_dma) and d.eng == "pe" and o.eng == "pe":
                continue
            dl.append(d)
            if not d.is_dma:
                d.sig = True
        o.deps = dl
        self.ops[o.eng].append(o)
        return o

    def op(self, eng, fn, reads=(), writes=()):
        return self._add(Op(eng, fn, False), reads, writes)

    def dma(self, eng, fn, reads=(), writes=()):
        o = Op(eng, fn, True)
        n = self.ndma[eng]
        self.ndma[eng] += 1
        slot = (eng, n % self.ndma_sems)
        o.dsem = slot
        o.prev = self.dma_last.get(slot)
        o.dval = (o.prev.dval if o.prev else 0) + 16
        self.dma_last[slot] = o
        self.all_dma.append(o)
        return self._add(o, reads, writes)


    def barrier(self):
        lasts = []
        for e in self.ENGS:
            for o in reversed(self.ops[e]):
                if (not o.is_dma) and o.fn is not None:
                    o.sig = True
                    lasts.append(o)
                    break
        dmas = list(self.dma_last.values())
        for e in self.ENGS:
            o = Op(e, None, False)
            o.deps = [d for d in lasts if d.eng != e] + dmas
            self.ops[e].append(o)

    def emit(self):
        nc = self.nc
        import contextlib
        with contextlib.ExitStack() as st:
            esem = {e: st.enter_context(nc.semaphore("S_" + e)) for e in self.ENGS}
            dsem = {}
            for e in self.ENGS:
                if self.ndma[e]:
                    for i in range(min(self.ndma_sems, self.ndma[e])):
                        dsem[(e, i)] = st.enter_context(nc.semaphore("D_%s_%d" % (e, i)))
            for e in self.ENGS:
                c = 0
                for o in self.ops[e]:
                    if (not o.is_dma) and o.sig and o.fn is not None:
                        c += 1
                        o.sigval = c
            block = st.enter_context(nc.Block())
            handles = {"pe": block.tensor, "act": block.scalar, "dve": block.vector,
                       "pool": block.gpsimd, "sp": block.sync}

            def make(e):
                def body(eng):
                    known = {}

                    def wait(sem, key, val):
                        if known.get(key, 0) < val:
                            eng.wait_ge(sem, val)
                            known[key] = val
                    for o in self.ops[e]:
                        for d in o.deps:
                            if d.is_dma:
                                wait(dsem[d.dsem], d.dsem, d.dval)
                            else:
                                wait(esem[d.eng], d.eng, d.sigval)
                        if o.is_dma:
                            if o.prev is not None:
                                wait(dsem[o.dsem], o.dsem, o.prev.dval)
                            o.fn(eng).then_inc(dsem[o.dsem], 16)
                        elif o.fn is not None:
                            ins = o.fn(eng)
                            if o.sig:
                                ins.then_inc(esem[e], 1)
                    if e == "sp":
                        for slot, o in self.dma_last.items():
                            wait(dsem[slot], slot, o.dval)
                return body
            for e in self.ENGS:
                if self.ops[e] or e == "sp":
                    handles[e](make(e))


def phase_a(nc, P, st, xin, w_in, g_mix, qkn, cs, idb, d_idb, kT_d, v_d, qT_d):
    def sb(name, shape, dt):
        return st.enter_context(nc.sbuf_tensor("a_" + name, shape, dt))

    def ps(name, shape, dt=F32):
        return st.enter_context(nc.psum_tensor("a_" + name, shape, dt))
    wq = sb("wq", [128, 8, 1536], BF16); d_wq = Dep()
    gm = sb("gm", [128, D], F32); d_gm = Dep()
    gq = sb("gq", [128, 2, 512], F32); d_gq = Dep()
    NB = 2
    xt = [sb("xt%d" % i, [128, D], F32) for i in range(NB)]; d_xt = [Dep() for _ in range(NB)]
    cst = [sb("cst%d" % i, [128, 64], F32) for i in range(NB)]; d_cst = [Dep() for _ in range(NB)]
    junk = sb("junk", [128, D], BF16); d_junk = Dep()
    ssq = sb("ssq", [128, 1], F32); d_ssq = Dep()
    rstd = sb("rstd", [128, 1], F32); d_rstd = Dep()
    hb = sb("hb", [128, D], BF16); d_hb = Dep()
    hT = [sb("hT%d" % i, [128, 8, 128], BF16) for i in range(NB)]; d_hT = [Dep() for _ in range(NB)]
    pT = ps("pT", [128, 8, 128], BF16); d_pT = Dep()
    pq = [[ps("pq%d_%d" % (j, i), [128, 512], F32) for i in range(3)] for j in range(2)]; d_pq = [[Dep() for _ in range(3)] for _ in range(2)]
    pkT = ps("pkT", [128, 4, 128], BF16); d_pkT = Dep()
    sq = sb("sq", [128, 512], F32); d_sq = Dep()
    hs = sb("hs", [128, 8], F32); d_hs = Dep()
    hr = sb("hr", [128, 8], F32); d_hr = Dep()
    qn = sb("qn", [128, 512], F32); d_qn = Dep()
    t1 = sb("t1", [128, 256], F32); d_t1 = Dep()
    t2 = sb("t2", [128, 256], F32); d_t2 = Dep()
    t3 = sb("t3", [128, 256], F32); d_t3 = Dep()
    t4 = sb("t4", [128, 256], F32); d_t4 = Dep()
    qr = sb("qr", [128, 512], BF16); d_qr = Dep()
    kTs = [sb("kTs%d" % i, [128, 4, 128], BF16) for i in range(NB)]; d_kTs = [Dep() for _ in range(NB)]
    vs = [sb("vs%d" % i, [128, 512], BF16) for i in range(NB)]; d_vs = [Dep() for _ in range(NB)]

    for k in range(8):
        P.dma("pool", lambda e, k=k: e.dma_start(out=wq[:, k, :], in_=w_in[k * 128:(k + 1) * 128, 0:1536]), writes=[d_wq])
    P.dma("sp", lambda e: e.dma_start(out=gm[:], in_=g_mix[0:1, :].partition_broadcast(128)), writes=[d_gm])
    for i in range(2):
        P.dma("sp", lambda e, i=i: e.dma_start(out=gq[:, i, :], in_=qkn[i:i + 1, :].partition_broadcast(128)), writes=[d_gq])
    P.op("dve", lambda e: e.tensor_scalar(out=gq[:, 0, :], in0=gq[:, 0, :], scalar1=0.125, scalar2=None, op0=ALU.mult),
         reads=[d_gq], writes=[d_gq])

    def qk_post(src_ps, d_src, which, b):
        P.op("act", lambda e: e.activation(out=sq[:], in_=src_ps[:], func=AF.Square), reads=[d_src], writes=[d_sq])
        P.op("dve", lambda e: e.tensor_reduce(out=hs[:], in_=sq[:].rearrange("p (h d) -> p h d", h=8), axis=AX.X, op=ALU.add),
             reads=[d_sq], writes=[d_hs])
        P.op("act", lambda e: e.activation(out=hr[:], in_=hs[:], func=AF.Sqrt, bias=EPS, scale=1.0 / 64), reads=[d_hs], writes=[d_hr])
        P.op("dve", lambda e: e.reciprocal(out=hr[:], in_=hr[:]), reads=[d_hr], writes=[d_hr])
        P.op("dve", lambda e: e.tensor_tensor(out=qn[:].rearrange("p (h d) -> p h d", h=8), in0=src_ps[:].rearrange("p (h d) -> p h d", h=8),
                                              in1=hr[:].unsqueeze(2).to_broadcast([128, 8, 64]), op=ALU.mult),
             reads=[d_src, d_hr], writes=[d_qn])
        P.op("pool", lambda e: e.tensor_tensor(out=qn[:], in0=qn[:], in1=gq[:, which, :], op=ALU.mult), reads=[d_qn, d_gq], writes=[d_qn])
        q3 = qn[:].rearrange("p (h d) -> p h d", h=8)
        q1 = q3[:, :, 0:32]
        q2 = q3[:, :, 32:64]
        cosb = cst[b][:, 0:32].unsqueeze(1).to_broadcast([128, 8, 32])
        sinb = cst[b][:, 32:64].unsqueeze(1).to_broadcast([128, 8, 32])
        v3 = lambda t: t[:].rearrange("p (h d) -> p h d", h=8)
        P.op("dve", lambda e: e.tensor_tensor(out=v3(t1), in0=q1, in1=cosb, op=ALU.mult), reads=[d_qn, d_cst[b]], writes=[d_t1])
        P.op("pool", lambda e: e.tensor_tensor(out=v3(t2), in0=q2, in1=sinb, op=ALU.mult), reads=[d_qn, d_cst[b]], writes=[d_t2])
        P.op("dve", lambda e: e.tensor_tensor(out=v3(t3), in0=q2, in1=cosb, op=ALU.mult), reads=[d_qn, d_cst[b]], writes=[d_t3])
        P.op("pool", lambda e: e.tensor_tensor(out=v3(t4), in0=q1, in1=sinb, op=ALU.mult), reads=[d_qn, d_cst[b]], writes=[d_t4])
        r3 = qr[:].rearrange("p (h d) -> p h d", h=8)
        P.op("dve", lambda e: e.tensor_tensor(out=r3[:, :, 0:32], in0=v3(t1), in1=v3(t2), op=ALU.subtract), reads=[d_t1, d_t2], writes=[d_qr])
        P.op("pool", lambda e: e.tensor_tensor(out=r3[:, :, 32:64], in0=v3(t3), in1=v3(t4), op=ALU.add), reads=[d_t3, d_t4], writes=[d_qr])

    def to_T_and_store(dst_dram, col0, b):
        for c in range(4):
            P.op("pe", lambda e, c=c: e.transpose(out=pkT[:, c, :], in_=qr[:, c * 128:(c + 1) * 128], identity=idb[:]),
                 reads=[d_qr, d_idb], writes=[d_pkT])
        P.op("act", lambda e: e.copy(out=kTs[b][:], in_=pkT[:]), reads=[d_pkT], writes=[d_kTs[b]])
        P.dma("sp", lambda e: e.dma_start(out=dst_dram[:, col0:col0 + 128].rearrange("(c p) t -> p c t", p=128), in_=kTs[b][:]),
              reads=[d_kTs[b]])

    def front(ti):
        b = ti % NB
        own = ti >= 32
        r0 = ti * 128
        P.dma("sp", lambda e: e.dma_start(out=xt[b][:], in_=xin[r0:r0 + 128, :]), writes=[d_xt[b]])
        P.dma("sp", lambda e: e.dma_start(out=cst[b][:], in_=cs[r0:r0 + 128, :]), writes=[d_cst[b]])
        P.op("act", lambda e: e.activation(out=junk[:], in_=xt[b][:], func=AF.Square, accum_out=ssq[:]), reads=[d_xt[b]], writes=[d_junk, d_ssq])
        P.op("act", lambda e: e.activation(out=rstd[:], in_=ssq[:], func=AF.Sqrt, bias=EPS, scale=1.0 / D), reads=[d_ssq], writes=[d_rstd])
        P.op("dve", lambda e: e.reciprocal(out=rstd[:], in_=rstd[:]), reads=[d_rstd], writes=[d_rstd])
        P.op("dve", lambda e: e.scalar_tensor_tensor(out=hb[:], in0=xt[b][:], scalar=rstd[:, 0:1], in1=gm[:], op0=ALU.mult, op1=ALU.mult),
             reads=[d_xt[b], d_rstd, d_gm], writes=[d_hb])
        for k in range(8):
            P.op("pe", lambda e, k=k: e.transpose(out=pT[:, k, :], in_=hb[:, k * 128:(k + 1) * 128], identity=idb[:]), reads=[d_hb, d_idb], writes=[d_pT])
        P.op("act", lambda e: e.copy(out=hT[b][:], in_=pT[:]), reads=[d_pT], writes=[d_hT[b]])
        groups = [0, 1, 2] if own else [1, 2]
        for g in groups:
            for k in range(8):
                P.op("pe", lambda e, g=g, k=k: e.matmul(pq[b][g][:], lhsT=hT[b][:, k, :], rhs=wq[:, k, g * 512:(g + 1) * 512], start=(k == 0), stop=(k == 7)),
                     reads=[d_hT[b], d_wq], writes=[d_pq[b][g]])
        P.op("act", lambda e: e.copy(out=vs[b][:], in_=pq[b][2][:]), reads=[d_pq[b][2]], writes=[d_vs[b]])
        P.dma("sp", lambda e: e.dma_start(out=v_d[r0:r0 + 128, :], in_=vs[b][:]), reads=[d_vs[b]])

    def post(ti):
        b = ti % NB
        own = ti >= 32
        r0 = ti * 128
        qk_post(pq[b][1], d_pq[b][1], 1, b)
        to_T_and_store(kT_d, r0, b)
        if own:
            qk_post(pq[b][0], d_pq[b][0], 0, b)
            to_T_and_store(qT_d, r0 - TP, b)

    front(0)
    for ti in range(64):
        if ti + 1 < 64:
            front(ti + 1)
        post(ti)


def phase_b(nc, P, st, kT_d, v_d, qT_d, e_d, vq_d, nb_d, oq_d, tri_d, idb, d_idb, oa, d_oa, heads=range(8), nblk=16):
    def sb(name, shape, dt):
        return st.enter_context(nc.sbuf_tensor("b_" + name, shape, dt))

    def ps(name, shape, dt=F32):
        return st.enter_context(nc.psum_tensor(name, shape, dt))
    NB = 2
    kaug = [sb("kaug%d" % i, [96, NK], BF16) for i in range(NB)]; d_kaug = [Dep() for _ in range(NB)]
    vaug = [sb("vaug%d" % i, [128, 64, 65], BF16) for i in range(NB)]; d_vaug = [Dep() for _ in range(NB)]
    qaug = [sb("qaug%d" % i, [96, T], BF16) for i in range(NB)]; d_qaug = [Dep() for _ in range(NB)]
    d_qmb = [Dep() for _ in range(NB)]
    vq = sb("vq_s", [128, 1024], F32); d_c = Dep()
    nb = sb("nb_s", [128, 1024], F32)
    oq = sb("oq_s", [128, 1024], F32)
    tri = sb("tri_s", [128, 128], BF16)
    ksum = sb("ksum", [64, 32], F32); d_ksum = Dep()
    kmean = sb("kmean", [64, 32], BF16); d_kmean = Dep()
    g1 = sb("g1", [128, 512], F32); d_g1 = Dep()
    top8 = sb("top8", [128, 16, 8], F32); d_top8 = Dep()
    sel = sb("sel", [128, 512], F32); d_sel = Dep()
    mbp = sb("mbp", [128, 16, 96], BF16); d_mbp = Dep()
    NPT = 3
    pt = [sb("pt%d" % i, [128, 512], BF16) for i in range(NPT)]; d_pt = [Dep() for _ in range(NPT)]
    rc = sb("rc", [128, 2], F32); d_rc = [Dep(), Dep()]
    pg = ps("pg", [128, 512], F32); d_pg = Dep()
    pmT = ps("pmT", [96, 8, 128], BF16); d_pmT = Dep()
    psS = [ps("psS%d" % i, [128, 512], F32) for i in range(NPT)]; d_psS = [Dep() for _ in range(NPT)]
    po = [ps("po%d" % i, [128, 65], F32) for i in range(2)]; d_po = [Dep() for _ in range(2)]

    for i in range(NB):
        P.dma("sp", lambda e, i=i: e.dma_start(out=kaug[i][64:96, :], in_=e_d[:, :]), writes=[d_kaug[i]])
        P.op("pool", lambda e, i=i: e.memset(vaug[i][:], 1.0), writes=[d_vaug[i]])
    P.dma("sp", lambda e: e.dma_start(out=vq[:], in_=vq_d[0:1, :].partition_broadcast(128)), writes=[d_c])
    P.dma("sp", lambda e: e.dma_start(out=nb[:], in_=nb_d[0:1, :].partition_broadcast(128)), writes=[d_c])
    P.dma("sp", lambda e: e.dma_start(out=oq[:], in_=oq_d[0:1, :].partition_broadcast(128)), writes=[d_c])
    P.dma("pool", lambda e: e.dma_start(out=tri[:], in_=tri_d[:, :]), writes=[d_c])
    P.op("pool", lambda e: e.memset(mbp[:], 0.0), writes=[d_mbp])

    for ih, h in enumerate(heads):
        b = ih % NB
        P.dma("sp", lambda e, b=b, h=h: e.dma_start(out=kaug[b][0:64, :], in_=kT_d[h * 64:(h + 1) * 64, :]), writes=[d_kaug[b]])
        P.dma("sp", lambda e, b=b, h=h: e.dma_start(out=vaug[b][:, :, 0:64],
                                                    in_=v_d[:, h * 64:(h + 1) * 64].rearrange("(t p) d -> p t d", p=128)),
              writes=[d_vaug[b]])
        P.dma("sp", lambda e, b=b, h=h: e.dma_start(out=qaug[b][0:64, :], in_=qT_d[h * 64:(h + 1) * 64, :]), writes=[d_qaug[b]])
        P.op("dve", lambda e, b=b: e.tensor_reduce(out=ksum[:], in_=kaug[b][0:64, :].rearrange("p (b k) -> p b k", k=256),
                                                   axis=AX.X, op=ALU.add), reads=[d_kaug[b]], writes=[d_ksum])
        P.op("dve", lambda e: e.tensor_scalar(out=kmean[:], in0=ksum[:], scalar1=1.0 / 256, scalar2=None, op0=ALU.mult),
             reads=[d_ksum], writes=[d_kmean])
        for half in range(2):
            for j in range(16):
                qt = half * 16 + j
                P.op("pe", lambda e, b=b, j=j, qt=qt: e.matmul(pg[:, j * 32:(j + 1) * 32], lhsT=qaug[b][0:64, qt * 128:(qt + 1) * 128],
                                                              rhs=kmean[:, :], start=True, stop=True),
                     reads=[d_qaug[b], d_kmean], writes=[d_pg])
            cs_ = slice(half * 512, (half + 1) * 512)
            P.op("dve", lambda e, cs_=cs_: e.tensor_tensor(out=g1[:], in0=pg[:], in1=vq[:, cs_], op=ALU.mult),
                 reads=[d_pg, d_c], writes=[d_g1])
            P.op("dve", lambda e, cs_=cs_: e.tensor_tensor(out=g1[:], in0=g1[:], in1=nb[:, cs_], op=ALU.add),
                 reads=[d_g1, d_c], writes=[d_g1])
            for j in range(16):
                P.op("dve", lambda e, j=j: e.max(out=top8[:, j, :], in_=g1[:, j * 32:(j + 1) * 32]), reads=[d_g1], writes=[d_top8])
            P.op("dve", lambda e: e.tensor_tensor(out=sel[:].rearrange("p (j b) -> p j b", b=32),
                                                  in0=g1[:].rearrange("p (j b) -> p j b", b=32),
                                                  in1=top8[:, :, 2:3].to_broadcast([128, 16, 32]), op=ALU.is_ge),
                 reads=[d_g1, d_top8], writes=[d_sel])
            P.op("dve", lambda e, cs_=cs_: e.tensor_tensor(out=sel[:], in0=sel[:], in1=vq[:, cs_], op=ALU.mult),
                 reads=[d_sel, d_c], writes=[d_sel])
            P.op("dve", lambda e, cs_=cs_: e.tensor_tensor(out=sel[:], in0=sel[:], in1=oq[:, cs_], op=ALU.add),
                 reads=[d_sel, d_c], writes=[d_sel])
            P.op("dve", lambda e: e.tensor_scalar(out=mbp[:, :, 64:96], in0=sel[:].rearrange("p (j b) -> p j b", b=32),
                                                  scalar1=1.0, scalar2=MB, op0=ALU.subtract, op1=ALU.mult),
                 reads=[d_sel], writes=[d_mbp])
            for grp in range(2):
                for j in range(8):
                    P.op("pe", lambda e, grp=grp, j=j: e.transpose(out=pmT[:, j, :], in_=mbp[:, grp * 8 + j, :], identity=idb[:]),
                         reads=[d_mbp, d_idb], writes=[d_pmT])
                c0 = (half * 16 + grp * 8) * 128
                P.op("act", lambda e, b=b, c0=c0: e.copy(out=qaug[b][64:96, c0:c0 + 1024], in_=pmT[64:96, :, :]),
                     reads=[d_pmT], writes=[d_qmb[b]])
        units = []
        for jb in range(nblk):
            ncommon = 32 + 2 * jb
            for u in range(ncommon // 2):
                units.append((jb, [2 * u, 2 * u + 1], False, u == 0))
            units.append((jb, [32 + 2 * jb, 32 + 2 * jb + 1], True, False))

        def emit_S(u, s, b=b):
            jb, kts, diag, first = u
            for j, kt in enumerate(kts):
                if diag and j == 1:
                    q0, nq, c0 = jb * 256 + 128, 128, 256
                else:
                    q0, nq, c0 = jb * 256, 256, j * 256
                P.op("pe", lambda e, s=s, kt=kt, q0=q0, nq=nq, c0=c0: e.matmul(psS[s][:, c0:c0 + nq], lhsT=kaug[b][0:96, kt * 128:(kt + 1) * 128],
                                                                             rhs=qaug[b][0:96, q0:q0 + nq], start=True, stop=True),
                     reads=[d_kaug[b], d_qaug[b], d_qmb[b]], writes=[d_psS[s]])

        def emit_rest(u, s, b=b, h=h):
            jb, kts, diag, first = u
            ncols = 384 if diag else 512
            P.op("act", lambda e, s=s, ncols=ncols: e.activation(out=pt[s][:, 0:ncols], in_=psS[s][:, 0:ncols], func=AF.Exp),
                 reads=[d_psS[s]], writes=[d_pt[s]])
            if diag:
                P.op("pool", lambda e, s=s: e.tensor_tensor(out=pt[s][:, 0:128], in0=pt[s][:, 0:128], in1=tri[:], op=ALU.mult),
                     reads=[d_pt[s], d_c], writes=[d_pt[s]])
                P.op("pool", lambda e, s=s: e.tensor_tensor(out=pt[s][:, 256:384], in0=pt[s][:, 256:384], in1=tri[:], op=ALU.mult),
                     reads=[d_pt[s], d_c], writes=[d_pt[s]])
            for j, kt in enumerate(kts):
                if diag and j == 1:
                    pv = [(1, 256, True)]
                elif diag:
                    pv = [(0, 0, True), (1, 128, False)]
                else:
                    pv = [(0, j * 256, False), (1, j * 256 + 128, False)]
                st_ = first and j == 0
                for qi, col0, last in pv:
                    P.op("pe", lambda e, s=s, kt=kt, qi=qi, col0=col0, st_=st_, last=last: e.matmul(
                        po[qi][:, :], lhsT=pt[s][:, col0:col0 + 128], rhs=vaug[b][:, kt, :], start=st_, stop=last),
                        reads=[d_pt[s], d_vaug[b]], writes=[d_po[qi]])
            if diag:
                for qi in range(2):
                    qt = jb * 2 + qi
                    P.op("dve", lambda e, qi=qi: e.reciprocal(out=rc[:, qi:qi + 1], in_=po[qi][:, 64:65]), reads=[d_po[qi]], writes=[d_rc[qi]])
                    P.op("dve", lambda e, qi=qi, qt=qt: e.tensor_scalar(out=oa[:, qt, h * 64:(h + 1) * 64], in0=po[qi][:, 0:64],
                                                                       scalar1=rc[:, qi:qi + 1], scalar2=None, op0=ALU.mult),
                         reads=[d_po[qi], d_rc[qi]], writes=[d_oa])
        SK = 2
        n = len(units)
        for i in range(n + SK):
            if i < n:
                emit_S(units[i], i % NPT)
            if i >= SK:
                emit_rest(units[i - SK], (i - SK) % NPT)


def host_consts(half):
    pv = 1.0 if half == 1 else 0.0
    V = np.zeros((32, 32), np.float32); O = np.zeros((32, 32), np.float32)
    for qt in range(32):
        jb = qt // 2
        V[qt, :16] = pv
        V[qt, 16:16 + jb] = 1.0
        O[qt, 16 + jb] = 1.0
    NBm = (V - 1.0) * BIGG
    E = np.zeros((32, NK), np.float32)
    for b in range(32):
        E[b, b * 256:(b + 1) * 256] = 1.0
    tri = (np.arange(128)[:, None] <= np.arange(128)[None, :]).astype(np.float32)
    return dict(vq=V.reshape(1, 1024), nb=NBm.reshape(1, 1024), oq=O.reshape(1, 1024),
                e_d=E.astype(ml_dtypes.bfloat16), tri=tri.astype(ml_dtypes.bfloat16))


def phase_c(nc, P, st, xin, w_in, g_mix, cwl_d, cbl_d, dtb_d, alog_d, dsk_d, ssdn_d, tri2_d, sel_d, tb_d, pflag_d,
            idb, d_idb, os_d, d_osd, chunks=range(32)):
    def sb(name, shape, dt):
        return st.enter_context(nc.sbuf_tensor("c_" + name, shape, dt))

    def ps(name, shape, dt=F32):
        return st.enter_context(nc.psum_tensor(name, shape, dt))
    wz = sb("wz", [128, 8, 1024], BF16); d_w = Dep()
    wx = sb("wx", [128, 8, 2048], BF16)
    wdt = sb("wdt", [128, 8, 16], BF16)
    gm = sb("gmc", [128, D], F32); d_c = Dep()
    cw = sb("cw", [128, 16, 4], F32)
    cb = sb("cb", [128, 16], F32)
    dtb = sb("dtb", [128, 2, 16], F32)
    Aneg = sb("Aneg", [128, 2, 16], F32); d_A = Dep()
    dsk = sb("dsk", [128, 16], F32)
    ssdn = sb("ssdn", [128, D], F32)
    tri2 = sb("tri2", [128, 2, 256], F32)
    onesf = sb("onesf", [128, 128], F32)
    sel = sb("selc", [16, 16, 128], F32)
    tb = sb("tbc", [128, 384], F32)
    pflag = sb("pflag", [128, 1], F32)
    xt = sb("xtc", [128, 2, D], F32); d_xt = Dep()
    junk = sb("junkc", [128, D], BF16); d_junk = Dep()
    ssq = sb("ssqc", [128, 2], F32); d_ssq = Dep()
    rstd = sb("rstdc", [128, 2], F32); d_rstd = Dep()
    hb = sb("hbc", [128, 2, D], BF16); d_hb = Dep()
    hT = sb("hTc", [128, 8, 256], BF16); d_hT = Dep()
    xraw = sb("xraw", [128, 16, 259], F32); d_xraw = Dep(); d_halo = Dep()
    acc = sb("acc", [128, 8, 256], F32); d_acc = [Dep() for _ in range(8)]
    xc = sb("xc", [128, 16, 256], BF16); d_xc = [Dep() for _ in range(16)]
    xtok = sb("xtok", [128, 2, 1024], BF16); d_xtok = Dep()
    btok = sb("btok", [128, 2, 512], BF16); d_btok = Dep()
    dtr = sb("dtr", [128, 2, 16], F32); d_dtr = Dep()
    dt = sb("dt", [128, 2, 16], F32); d_dt = Dep()
    aa = sb("aa", [128, 2, 16], F32); d_aa = Dep()
    acum = sb("acum", [128, 2, 16], F32); d_acum = Dep()
    nacum = sb("nacum", [128, 2, 16], F32); d_nacum = Dep()
    acumT = sb("acumT", [16, 256], F32); d_acumT = Dep()
    tot = sb("tot", [128, 16], F32); d_tot = Dep()
    cdec = sb("cdec", [128, 16], F32); d_cdec = Dep()
    dte = sb("dte", [128, 2, 16], F32); d_dte = Dep()
    eac = sb("eac", [128, 2, 16], F32); d_eac = Dep()
    w2 = sb("w2", [128, 2, 16], F32); d_w2 = Dep()
    xdt = sb("xdt", [128, 2, 1024], BF16); d_xdt = Dep()
    xdd = sb("xdd", [128, 2, 1024], BF16); d_xdd = Dep()
    zs = sb("zs", [128, 2, 1024], BF16); d_zs = Dep()
    cbT = sb("cbT", [128, 4, 384], F32); d_cbT = [Dep() for _ in range(4)]
    arg = [sb("arg%d" % i, [128, 384], F32) for i in range(2)]; d_arg = [Dep(), Dep()]
    Lt = [sb("Lt%d" % i, [128, 384], F32) for i in range(2)]; d_Lt = [Dep(), Dep()]
    Mt = [sb("Mt%d" % i, [128, 384], BF16) for i in range(2)]; d_Mt = [Dep() for _ in range(2)]
    stf = sb("stf", [128, 4, 256], F32); d_stf = Dep()
    stT = sb("stT", [128, 4, 256], BF16); d_stT = Dep()
    yt = [sb("yt%d" % i, [128, 256], F32) for i in range(2)]; d_yt = [Dep(), Dep()]
    y2 = [sb("y2%d" % i, [128, 256], F32) for i in range(2)]; d_y2 = [Dep(), Dep()]
    gss = [sb("gss%d" % i, [128, 1], F32) for i in range(2)]; d_gss = [Dep(), Dep()]
    grs = [sb("grs%d" % i, [128, 1], F32) for i in range(2)]; d_grs = [Dep(), Dep()]
    osb = sb("osb", [128, 2, 1024], BF16); d_osb = Dep()

    B = [ps("bk%d" % i, [128, 512], F32) for i in range(7)]
    d_B = [Dep() for _ in range(7)]
    d_B1h = [Dep(), Dep()]
    d_B5h = [Dep(), Dep()]
    pT = ps("pTc", [128, 8, 128], BF16); d_pT = Dep()

    for k in range(8):
        rows = slice(k * 128, (k + 1) * 128)
        P.dma("pool", lambda e, k=k, rows=rows: e.dma_start(out=wz[:, k, :], in_=w_in[rows, C_Z:C_Z + 1024]), writes=[d_w])
        P.dma("pool", lambda e, k=k, rows=rows: e.dma_start(out=wx[:, k, :], in_=w_in[rows, C_X:C_X + 2048]), writes=[d_w])
        P.dma("pool", lambda e, k=k, rows=rows: e.dma_start(out=wdt[:, k, :], in_=w_in[rows, C_DT:C_DT + 16]), writes=[d_w])
    P.dma("sp", lambda e: e.dma_start(out=gm[:], in_=g_mix[0:1, :].partition_broadcast(128)), writes=[d_c])
    P.dma("sp", lambda e: e.dma_start(out=cw[:], in_=cwl_d[:, :, :]), writes=[d_c])
    P.dma("sp", lambda e: e.dma_start(out=cb[:], in_=cbl_d[:, :]), writes=[d_c])
    for i in range(2):
        P.dma("sp", lambda e, i=i: e.dma_start(out=dtb[:, i, :], in_=dtb_d[0:1, :].partition_broadcast(128)), writes=[d_c])
        P.dma("sp", lambda e, i=i: e.dma_start(out=Aneg[:, i, :], in_=alog_d[0:1, :].partition_broadcast(128)), writes=[d_A])
    P.dma("sp", lambda e: e.dma_start(out=dsk[:], in_=dsk_d[0:1, :].partition_broadcast(128)), writes=[d_c])
    P.dma("sp", lambda e: e.dma_start(out=ssdn[:], in_=ssdn_d[0:1, :].partition_broadcast(128)), writes=[d_c])
    P.dma("sp", lambda e: e.dma_start(out=tri2[:], in_=tri2_d[:, :, :]), writes=[d_c])
    P.dma("sp", lambda e: e.dma_start(out=sel[:], in_=sel_d[:, :, :]), writes=[d_c])
    P.dma("sp", lambda e: e.dma_start(out=tb[:], in_=tb_d[:, :]), writes=[d_c])
    P.dma("sp", lambda e: e.dma_start(out=pflag[:], in_=pflag_d[0:1, :].partition_broadcast(128)), writes=[d_c])
    P.op("pool", lambda e: e.memset(onesf[:], 1.0), writes=[d_c])
    P.op("act", lambda e: e.activation(out=Aneg[:], in_=Aneg[:], func=AF.Exp), reads=[d_A], writes=[d_A])
    P.op("dve", lambda e: e.tensor_scalar(out=Aneg[:], in0=Aneg[:], scalar1=-1.0, scalar2=None, op0=ALU.mult), reads=[d_A], writes=[d_A])
    P.op("pool", lambda e: e.memset(xraw[:], 0.0), writes=[d_xraw, d_halo])
    P.op("pool", lambda e: e.memset(stf[:], 0.0), writes=[d_stf])
    P.op("pool", lambda e: e.memset(stT[:], 0.0), writes=[d_stT])

    h3 = lambda ap: ap.rearrange("p (h d) -> p h d", d=64)

    for c in chunks:
        own = c >= 16
        r0 = c * 256
        P.dma("sp", lambda e, r0=r0: e.dma_start(out=xt[:], in_=xin[r0:r0 + 256, :].rearrange("(t p) d -> p t d", p=128)), writes=[d_xt])
        for it in range(2):
            P.op("act", lambda e, it=it: e.activation(out=junk[:], in_=xt[:, it, :], func=AF.Square, accum_out=ssq[:, it:it + 1]),
                 reads=[d_xt], writes=[d_junk, d_ssq])
        P.op("act", lambda e: e.activation(out=rstd[:], in_=ssq[:], func=AF.Sqrt, bias=EPS, scale=1.0 / D), reads=[d_ssq], writes=[d_rstd])
        P.op("dve", lambda e: e.reciprocal(out=rstd[:], in_=rstd[:]), reads=[d_rstd], writes=[d_rstd])
        for it in range(2):
            P.op("dve", lambda e, it=it: e.scalar_tensor_tensor(out=hb[:, it, :], in0=xt[:, it, :], scalar=rstd[:, it:it + 1], in1=gm[:],
                                                                                    op0=ALU.mult, op1=ALU.mult),
                 reads=[d_xt, d_rstd, d_c], writes=[d_hb])
        for it in range(2):
            for k in range(8):
                P.op("pe", lambda e, it=it, k=k: e.transpose(out=pT[:, k, :], in_=hb[:, it, k * 128:(k + 1) * 128], identity=idb[:]),
                     reads=[d_hb, d_idb], writes=[d_pT])
            P.op("act", lambda e, it=it: e.copy(out=hT[:, :, it * 128:(it + 1) * 128], in_=pT[:]), reads=[d_pT], writes=[d_hT])
        for it in range(2):
            for k in range(8):
                P.op("pe", lambda e, it=it, k=k: e.matmul(B[3][:, it * 16:(it + 1) * 16], lhsT=hT[:, k, it * 128:(it + 1) * 128], rhs=wdt[:, k, :],
                                                         start=(k == 0), stop=(k == 7)), reads=[d_hT, d_w], writes=[d_B[3]])
        P.op("dve", lambda e: e.tensor_tensor(out=dtr[:].rearrange("p a b -> p (a b)"), in0=B[3][:, 0:32], in1=dtb[:].rearrange("p a b -> p (a b)"), op=ALU.add),
             reads=[d_B[3], d_c], writes=[d_dtr])
        P.op("act", lambda e: e.activation(out=dtr[:], in_=dtr[:], func=AF.Exp), reads=[d_dtr], writes=[d_dtr])
        P.op("act", lambda e: e.activation(out=dt[:], in_=dtr[:], func=AF.Ln, bias=1.0), reads=[d_dtr], writes=[d_dt])
        P.op("dve", lambda e: e.tensor_tensor(out=aa[:], in0=dt[:], in1=Aneg[:], op=ALU.mult), reads=[d_dt, d_A], writes=[d_aa])
        for it in range(2):
            for jt in range(it + 1):
                P.op("pe", lambda e, it=it, jt=jt: e.matmul(B[3][:, 32 + it * 16:32 + (it + 1) * 16], lhsT=tri2[:, jt, it * 128:(it + 1) * 128],
                                                           rhs=aa[:, jt, :], start=(jt == 0), stop=(jt == it)),
                     reads=[d_aa, d_c], writes=[d_B[3]])
        for jt in range(2):
            P.op("pe", lambda e, jt=jt: e.matmul(B[3][:, 64:80], lhsT=onesf[:], rhs=aa[:, jt, :], start=(jt == 0), stop=(jt == 1)),
                 reads=[d_aa, d_c], writes=[d_B[3]])
        for jt in range(2):
            P.op("pe", lambda e, jt=jt: e.matmul(B[3][0:16, 128:384], lhsT=aa[:, jt, :], rhs=tri2[:, jt, :], start=(jt == 0), stop=(jt == 1)),
                 reads=[d_aa, d_c], writes=[d_B[3]])
        P.op("act", lambda e: e.copy(out=acum[:].rearrange("p a b -> p (a b)"), in_=B[3][:, 32:64]), reads=[d_B[3]], writes=[d_acum])
        P.op("dve", lambda e: e.tensor_scalar(out=nacum[:].rearrange("p a b -> p (a b)"), in0=B[3][:, 32:64], scalar1=-1.0, scalar2=None, op0=ALU.mult),
             reads=[d_B[3]], writes=[d_nacum])
        P.op("act", lambda e: e.copy(out=tot[:], in_=B[3][:, 64:80]), reads=[d_B[3]], writes=[d_tot])
        P.op("act", lambda e: e.copy(out=acumT[:], in_=B[3][0:16, 128:384]), reads=[d_B[3]], writes=[d_acumT])
        P.op("act", lambda e: e.activation(out=cdec[:], in_=tot[:], func=AF.Exp), reads=[d_tot], writes=[d_cdec])
        P.op("dve", lambda e: e.tensor_tensor(out=dte[:], in0=nacum[:], in1=tot[:].unsqueeze(1).to_broadcast([128, 2, 16]), op=ALU.add),
             reads=[d_nacum, d_tot], writes=[d_dte])
        P.op("act", lambda e: e.activation(out=dte[:], in_=dte[:], func=AF.Exp), reads=[d_dte], writes=[d_dte])
        P.op("act", lambda e: e.activation(out=eac[:], in_=acum[:], func=AF.Exp), reads=[d_acum], writes=[d_eac])
        P.op("dve", lambda e: e.tensor_tensor(out=w2[:], in0=dt[:], in1=dte[:], op=ALU.mult), reads=[d_dt, d_dte], writes=[d_w2])
        for fc in range(16):
            bi = 1 if fc % 2 == 0 else 4
            for k in range(8):
                P.op("pe", lambda e, fc=fc, k=k, bi=bi: e.matmul(B[bi][:, 0:256], lhsT=wx[:, k, fc * 128:(fc + 1) * 128], rhs=hT[:, k, :],
                                                               start=(k == 0), stop=(k == 7)), reads=[d_hT, d_w], writes=[d_B[bi]])
            P.op("act", lambda e, fc=fc, bi=bi: e.copy(out=xraw[:, fc, 3:259], in_=B[bi][:, 0:256]),
                 reads=[d_B[bi]], writes=[d_xraw])
        for half in range(2):
            for f8 in range(8):
                fc = half * 8 + f8
                P.op("dve", lambda e, fc=fc, f8=f8: e.tensor_scalar(out=acc[:, f8, :], in0=xraw[:, fc, 0:256], scalar1=cw[:, fc, 0:1], scalar2=cb[:, fc:fc + 1],
                                                                   op0=ALU.mult, op1=ALU.add), reads=[d_xraw, d_halo, d_c], writes=[d_acc[f8]])
            for kk in range(1, 4):
                for f8 in range(8):
                    fc = half * 8 + f8
                    P.op("dve", lambda e, fc=fc, f8=f8, kk=kk: e.scalar_tensor_tensor(out=acc[:, f8, :], in0=xraw[:, fc, kk:kk + 256], scalar=cw[:, fc, kk:kk + 1],
                                                                                     in1=acc[:, f8, :], op0=ALU.mult, op1=ALU.add),
                         reads=[d_xraw, d_halo, d_c, d_acc[f8]], writes=[d_acc[f8]])
            for f8 in range(8):
                fc = half * 8 + f8
                P.op("act", lambda e, fc=fc, f8=f8: e.activation(out=xc[:, fc, :], in_=acc[:, f8, :], func=AF.Silu),
                     reads=[d_acc[f8]], writes=[d_xc[fc]])
        P.op("pool", lambda e: e.tensor_copy(out=xraw[:, :, 0:3], in_=xraw[:, :, 256:259]), reads=[d_xraw], writes=[d_halo])
        for it in range(2):
            for fc in range(8):
                P.op("pe", lambda e, it=it, fc=fc: e.transpose(out=pT[:, fc, :], in_=xc[:, fc, it * 128:(it + 1) * 128], identity=idb[:]),
                     reads=[d_xc[fc], d_idb], writes=[d_pT])
            P.op("act", lambda e, it=it: e.copy(out=xtok[:, it, :], in_=pT[:]), reads=[d_pT], writes=[d_xtok])
            for g in range(4):
                P.op("pe", lambda e, it=it, g=g: e.transpose(out=pT[:, g, :], in_=xc[:, 8 + g, it * 128:(it + 1) * 128], identity=idb[:]),
                     reads=[d_xc[8 + g], d_idb], writes=[d_pT])
            P.op("act", lambda e, it=it: e.copy(out=btok[:, it, :], in_=pT[:, 0:4, :]), reads=[d_pT], writes=[d_btok])
        for it in range(2):
            if own:
                P.op("dve", lambda e, it=it: e.tensor_tensor(out=h3(xdt[:, it, :]), in0=h3(xtok[:, it, :]),
                                                             in1=dt[:, it, :].unsqueeze(2).to_broadcast([128, 16, 64]), op=ALU.mult),
                     reads=[d_xtok, d_dt], writes=[d_xdt])
            P.op("pool", lambda e, it=it: e.tensor_tensor(out=h3(xdd[:, it, :]), in0=h3(xtok[:, it, :]),
                                                          in1=w2[:, it, :].unsqueeze(2).to_broadcast([128, 16, 64]), op=ALU.mult),
                 reads=[d_xtok, d_w2], writes=[d_xdd])
        if own:
            for it in range(2):
                for hv in range(2):
                    zb = 2 if hv == 0 else 6
                    for k in range(8):
                        P.op("pe", lambda e, it=it, hv=hv, k=k, zb=zb: e.matmul(B[zb][:, :], lhsT=hT[:, k, it * 128:(it + 1) * 128], rhs=wz[:, k, hv * 512:(hv + 1) * 512],
                                                                              start=(k == 0), stop=(k == 7)), reads=[d_hT, d_w], writes=[d_B[zb]])
                    P.op("act", lambda e, it=it, hv=hv, zb=zb: e.activation(out=zs[:, it, hv * 512:(hv + 1) * 512], in_=B[zb][:, :], func=AF.Silu),
                         reads=[d_B[zb]], writes=[d_zs])
            for g in range(4):
                P.op("pe", lambda e, g=g: e.matmul(B[4][:, 0:256], lhsT=xc[:, 8 + g, 0:128], rhs=xc[:, 12 + g, :], start=True, stop=True),
                     reads=[d_xc[8 + g], d_xc[12 + g]], writes=[d_B[4]])
                P.op("pe", lambda e, g=g: e.matmul(B[4][:, 256:384], lhsT=xc[:, 8 + g, 128:256], rhs=xc[:, 12 + g, 128:256], start=True, stop=True),
                     reads=[d_xc[8 + g], d_xc[12 + g]], writes=[d_B[4]])
                P.op("act", lambda e, g=g: e.copy(out=cbT[:, g, :], in_=B[4][:, 0:384]), reads=[d_B[4]], writes=[d_cbT[g]])
            for g in range(4):
                def stA(h, g=g):
                    mi = h % 2
                    bk = 5 if mi == 0 else 4
                    P.op("pe", lambda e: e.matmul(B[bk][:, 0:256], lhsT=sel[:, h, :], rhs=acumT[:, :], start=True, stop=True),
                         reads=[d_c, d_acumT], writes=[d_B[bk]])
                    P.op("dve", lambda e: e.scalar_tensor_tensor(out=arg[mi][:, 0:256], in0=B[bk][:, 0:256], scalar=nacum[:, 0, h:h + 1], in1=tb[:, 0:256],
                                                                 op0=ALU.add, op1=ALU.add), reads=[d_B[bk], d_nacum, d_c], writes=[d_arg[mi]])
                    P.op("dve", lambda e: e.scalar_tensor_tensor(out=arg[mi][:, 256:384], in0=B[bk][:, 128:256], scalar=nacum[:, 1, h:h + 1], in1=tb[:, 256:384],
                                                                 op0=ALU.add, op1=ALU.add), reads=[d_B[bk], d_nacum, d_c], writes=[d_arg[mi]])
                    P.op("act", lambda e: e.activation(out=Lt[mi][:], in_=arg[mi][:], func=AF.Exp), reads=[d_arg[mi]], writes=[d_Lt[mi]])
                    P.op("pool", lambda e: e.tensor_tensor(out=Mt[mi][:], in0=Lt[mi][:], in1=cbT[:, g, :], op=ALU.mult),
                         reads=[d_Lt[mi], d_cbT[g]], writes=[d_Mt[mi]])

                def stB(h, g=g):
                    mi = h % 2
                    r = h % 4
                    cs_ = slice(r * 64, (r + 1) * 64)
                    hs_ = slice(h * 64, (h + 1) * 64)
                    P.op("pe", lambda e: e.matmul(B[6][:, 0:256][:, cs_], lhsT=Mt[mi][:, 0:128], rhs=xdt[:, 0, hs_], start=True, stop=True),
                         reads=[d_Mt[mi], d_xdt], writes=[d_B[6]])
                    P.op("pe", lambda e: e.matmul(B[6][:, 256:512][:, cs_], lhsT=Mt[mi][:, 128:256], rhs=xdt[:, 0, hs_], start=True, stop=False),
                         reads=[d_Mt[mi], d_xdt], writes=[d_B[6]])
                    P.op("pe", lambda e: e.matmul(B[6][:, 256:512][:, cs_], lhsT=Mt[mi][:, 256:384], rhs=xdt[:, 1, hs_], start=False, stop=True),
                         reads=[d_Mt[mi], d_xdt], writes=[d_B[6]])
                hs4 = [g * 4 + r for r in range(4)]
                stA(hs4[0]); stA(hs4[1]); stB(hs4[0]); stA(hs4[2]); stB(hs4[1]); stA(hs4[3]); stB(hs4[2]); stB(hs4[3])
                gs_ = slice(g * 256, (g + 1) * 256)
                for it in range(2):
                    P.op("pe", lambda e, g=g, it=it: e.matmul(B[0][:, it * 256:(it + 1) * 256], lhsT=xc[:, 12 + g, it * 128:(it + 1) * 128], rhs=stT[:, g, :],
                                                             start=True, stop=True), reads=[d_xc[12 + g], d_stT], writes=[d_B[0]])
                v4 = lambda ap: ap.rearrange("p (r d) -> p r d", d=64)

                def ystep(step, it, g=g, gs_=gs_):
                    if step == 0:
                        P.op("dve", lambda e: e.tensor_tensor(out=v4(yt[it][:]), in0=v4(B[0][:, it * 256:(it + 1) * 256]),
                                                              in1=eac[:, it, g * 4:(g + 1) * 4].unsqueeze(2).to_broadcast([128, 4, 64]), op=ALU.mult),
                             reads=[d_B[0], d_eac], writes=[d_yt[it]])
                    elif step == 1:
                        P.op("dve", lambda e: e.tensor_tensor(out=yt[it][:], in0=yt[it][:], in1=B[6][:, it * 256:(it + 1) * 256], op=ALU.add),
                             reads=[d_yt[it], d_B[6]], writes=[d_yt[it]])
                    elif step == 2:
                        P.op("pool", lambda e: e.tensor_tensor(out=v4(y2[it][:]), in0=v4(xtok[:, it, gs_]),
                                                               in1=dsk[:, g * 4:(g + 1) * 4].unsqueeze(2).to_broadcast([128, 4, 64]), op=ALU.mult),
                             reads=[d_xtok, d_c], writes=[d_y2[it]])
                    elif step == 3:
                        P.op("dve", lambda e: e.tensor_tensor(out=yt[it][:], in0=yt[it][:], in1=y2[it][:], op=ALU.add), reads=[d_yt[it], d_y2[it]], writes=[d_yt[it]])
                    elif step == 4:
                        P.op("dve", lambda e: e.tensor_tensor(out=yt[it][:], in0=yt[it][:], in1=zs[:, it, gs_], op=ALU.mult), reads=[d_yt[it], d_zs], writes=[d_yt[it]])
                    elif step == 5:
                        P.op("act", lambda e: e.activation(out=y2[it][:], in_=yt[it][:], func=AF.Square, accum_out=gss[it][:]), reads=[d_yt[it], d_y2[it]], writes=[d_y2[it], d_gss[it]])
                    elif step == 6:
                        P.op("act", lambda e: e.activation(out=grs[it][:], in_=gss[it][:], func=AF.Sqrt, bias=EPS, scale=1.0 / 256), reads=[d_gss[it]], writes=[d_grs[it]])
                    elif step == 7:
                        P.op("dve", lambda e: e.reciprocal(out=grs[it][:], in_=grs[it][:]), reads=[d_grs[it]], writes=[d_grs[it]])
                    elif step == 8:
                        P.op("dve", lambda e: e.scalar_tensor_tensor(out=osb[:, it, gs_], in0=yt[it][:], scalar=grs[it][:, 0:1], in1=ssdn[:, gs_],
                                                                     op0=ALU.mult, op1=ALU.mult), reads=[d_yt[it], d_grs[it], d_c], writes=[d_osb])
                for step in range(9):
                    for it in range(2):
                        ystep(step, it)
            P.dma("sp", lambda e, r0=r0: e.dma_start(out=os_d[r0 - TP:r0 - TP + 256, :].rearrange("(t p) c -> p t c", p=128), in_=osb[:]),
                  reads=[d_osb], writes=[d_osd])
        for g in range(4):
            bank = B[2] if g < 2 else B[1]
            dbank = d_B[2] if g < 2 else d_B[1]
            cs_ = slice((g % 2) * 256, (g % 2 + 1) * 256)
            for jt in range(2):
                P.op("pe", lambda e, g=g, jt=jt, bank=bank, cs_=cs_: e.matmul(bank[:, cs_], lhsT=btok[:, jt, g * 128:(g + 1) * 128], rhs=xdd[:, jt, g * 256:(g + 1) * 256],
                                                                            start=(jt == 0), stop=(jt == 1)), reads=[d_btok, d_xdd], writes=[dbank])
        v16 = lambda ap: ap.rearrange("p (h d) -> p h d", d=64)
        P.op("dve", lambda e: e.tensor_tensor(out=v16(stf[:].rearrange("p g c -> p (g c)")), in0=v16(stf[:].rearrange("p g c -> p (g c)")),
                                              in1=cdec[:].unsqueeze(2).to_broadcast([128, 16, 64]), op=ALU.mult), reads=[d_stf, d_cdec], writes=[d_stf])
        P.op("dve", lambda e: e.tensor_tensor(out=stf[:, 0:2, :].rearrange("p g c -> p (g c)"), in0=stf[:, 0:2, :].rearrange("p g c -> p (g c)"), in1=B[2][:, :], op=ALU.add),
             reads=[d_stf, d_B[2]], writes=[d_stf])
        P.op("dve", lambda e: e.tensor_tensor(out=stf[:, 2:4, :].rearrange("p g c -> p (g c)"), in0=stf[:, 2:4, :].rearrange("p g c -> p (g c)"), in1=B[1][:, :], op=ALU.add),
             reads=[d_stf, d_B[1]], writes=[d_stf])
        if c == 15:
            P.op("dve", lambda e: e.tensor_scalar(out=stf[:], in0=stf[:], scalar1=pflag[:, 0:1], scalar2=None, op0=ALU.mult), reads=[d_stf, d_c], writes=[d_stf])
        P.op("act", lambda e: e.copy(out=stT[:], in_=stf[:]), reads=[d_stf], writes=[d_stT])


def host_consts_c(z, half):
    cwl = np.ascontiguousarray(z['conv_w'].reshape(4, 16, 128).transpose(2, 1, 0)).astype(np.float32)
    cbl = np.ascontiguousarray(z['conv_b'].reshape(16, 128).T).astype(np.float32)
    tri = (np.arange(128)[:, None] <= np.arange(128)[None, :]).astype(np.float32)
    tri2 = np.zeros((128, 2, 256), np.float32)
    tri2[:, 0, 0:128] = tri; tri2[:, 0, 128:256] = 1.0; tri2[:, 1, 128:256] = tri
    sel = np.zeros((16, 16, 128), np.float32)
    for h in range(16):
        sel[h, h, :] = 1.0
    tbias = np.where(tri > 0, 0.0, NEG).astype(np.float32)
    tb = np.zeros((128, 384), np.float32)
    tb[:, 0:128] = tbias; tb[:, 256:384] = tbias
    return dict(cwl=cwl, cbl=cbl, dtb=z['dt_bias'][None, :], alog=z['a_log'][None, :], dsk=z['d_skip'][None, :], ssdn=z['ssd_norm'][None, :],
                tri2=tri2, sel_d=sel, tb_d=tb, pflag=np.array([[1.0 if half == 1 else 0.0]], np.float32))


def phase_def(nc, P, xown, w_in, g_mix, mem_d, gmem_d, wmemkv, mqn_d, mkn_d, woa_d, wos_d, wom_d, wout_d, gffn_d, wr_d,
              wgate_d, wup_d, wdown_d, tri_d, ecap_d, oa_d, d_oad, os_d, d_osd, x1_d, xbuf, ybuf, out_d, idb, d_idb, idf, d_idf,
              ntiles=32, experts=range(32), stage=3):
    d_x1d = Dep(); d_xbuf = Dep(); d_ybuf = Dep(); d_out = Dep()
    with contextlib.ExitStack() as stp:
        def sbp(name, shape, dt):
            return stp.enter_context(nc.sbuf_tensor("d_" + name, shape, dt))
        combs = sbp("combs", [128, 32, 2], F32); d_combs = Dep()
        idxs = sbp("idxs", [128, 32, 2], I32); d_idxs = Dep()

        with contextlib.ExitStack() as st:
            def sb(name, shape, dt):
                return st.enter_context(nc.sbuf_tensor("d_" + name, shape, dt))

            def ps(name, shape, dt=F32):
                return st.enter_context(nc.psum_tensor(name, shape, dt))
            d_w = Dep(); d_c = Dep()
            gm = sb("gmd", [128, D], F32)
            gf = sb("gfd", [128, D], F32)
            mqn = sb("mqn", [128, 512], F32)
            mkn = sb("mkn", [128, 512], F32)
            tri = sb("trid", [128, 128], F32)
            onesf = sb("onesfd", [128, 128], F32)
            onesb = sb("onesbd", [128, 128], BF16)
            ecap = sb("ecap", [128, 32], F32)
            base = sb("base", [128, 32], F32); d_base = Dep()
            kmT = sb("kmT", [128, 4, 256], BF16); d_kmT = Dep()
            vm = sb("vm", [128, 2, 512], BF16); d_vm = Dep()

            xt = sb("xtd", [128, D], F32); d_xt = Dep()
            junk = sb("junkd", [128, D], BF16); d_junk = Dep()
            ssq = sb("ssqd", [128, 1], F32); d_ssq = Dep()
            rstd = sb("rstdd", [128, 1], F32); d_rstd = Dep()
            hb = sb("hbd", [128, D], BF16); d_hb = Dep()
            hT = sb("hTd", [128, 8, 128], BF16); d_hT = Dep()
            sq = sb("sqd", [128, 512], F32); d_sq = Dep()
            hs = sb("hsd", [128, 4], F32); d_hs = Dep()
            hr = sb("hrd", [128, 4], F32); d_hr = Dep()
            qmn = sb("qmn", [128, 512], F32); d_qmn = Dep()
            qmb = sb("qmb", [128, 512], BF16); d_qmb = Dep()
            qmT = sb("qmT", [128, 4, 128], BF16); d_qmT = Dep()
            pm = sb("pm", [128, 4, 2, 128], BF16); d_pm = Dep()
            rcp = sb("rcp", [128, 512], F32); d_rcp = Dep()
            omT = sb("omT", [128, 4, 128], BF16); d_omT = Dep()
            gs = sb("gs", [128, 3072], F32); d_gs = Dep()
            oat = sb("oat", [128, 512], BF16); d_oat = Dep()
            ost = sb("ost", [128, 1024], BF16); d_ost = Dep()
            oaT = sb("oaT", [128, 4, 128], BF16); d_oaT = Dep()
            osT = sb("osT", [128, 8, 128], BF16); d_osT = Dep()
            mg = sb("mg", [128, 512], F32); d_mg = Dep()
            tt = sb("tt", [128, 512], F32); d_tt = Dep()
            mgb = sb("mgb", [128, D], BF16); d_mgb = Dep()
            mgT = sb("mgT", [128, 8, 128], BF16); d_mgT = Dep()
            x1 = sb("x1", [128, D], F32); d_x1 = Dep()
            h2f = sb("h2f", [128, D], F32); d_h2f = Dep()
            h2b = [sb("h2b%d" % i, [128, D], BF16) for i in range(2)]; d_h2b = [Dep() for _ in range(2)]
            h2T = sb("h2T", [128, 8, 128], F32); d_h2T = Dep()
            lg = sb("lg", [128, 36], F32); d_lg = Dep()
            sm = sb("sm", [128, 64], F32); d_sm = Dep()
            goh = sb("goh", [128, 4], F32); d_goh = Dep()
            lsel = sb("lsel", [128, 32], F32); d_lsel = Dep()
            les = sb("les", [128, 8], F32); d_les = Dep()
            t8 = sb("t8", [128, 8], F32); d_t8 = Dep()
            oh = sb("oh", [128, 64], F32); d_oh = Dep()
            ohs = sb("ohs", [128, 32], F32); d_ohs = Dep()
            posf = sb("posf", [128, 32], F32); d_posf = Dep()
            idf2 = sb("idf2", [128, 2], F32); d_idf2 = Dep()

            pT = ps("pTd", [128, 8, 128], BF16); d_pT = Dep()
            pF = ps("pFd", [128, 512], F32); d_pF = Dep()
            pG = [ps("pGd%d" % i, [128, 512], F32) for i in range(3)]; d_pG = [Dep() for _ in range(3)]
            pms = [ps("pmsd%d" % i, [128, 512], F32) for i in range(2)]; d_pms = [Dep() for _ in range(2)]
            pmo = ps("pmod", [128, 512], F32); d_pmo = Dep()

            P.dma("sp", lambda e: e.dma_start(out=gm[:], in_=g_mix[0:1, :].partition_broadcast(128)), writes=[d_c])
            P.dma("sp", lambda e: e.dma_start(out=gf[:], in_=gffn_d[0:1, :].partition_broadcast(128)), writes=[d_c])
            P.dma("sp", lambda e: e.dma_start(out=mqn[:], in_=mqn_d[0:1, :].partition_broadcast(128)), writes=[d_c])
            P.dma("sp", lambda e: e.dma_start(out=mkn[:], in_=mkn_d[0:1, :].partition_broadcast(128)), writes=[d_c])
            P.dma("sp", lambda e: e.dma_start(out=tri[:], in_=tri_d[:, :]), writes=[d_c])
            P.dma("sp", lambda e: e.dma_start(out=ecap[:], in_=ecap_d[0:1, :].partition_broadcast(128)), writes=[d_c])
            P.op("pool", lambda e: e.memset(onesf[:], 1.0), writes=[d_c])
            P.op("pool", lambda e: e.memset(onesb[:], 1.0), writes=[d_c])
            P.op("pool", lambda e: e.memset(base[:], 0.0), writes=[d_base])

            def head_norm(src_ps, d_src, gain, nh, dst, d_dst):
                hd = 512 // nh
                v = lambda ap: ap.rearrange("p (h d) -> p h d", h=nh)
                P.op("act", lambda e: e.activation(out=sq[:], in_=src_ps, func=AF.Square), reads=[d_src], writes=[d_sq])
                P.op("dve", lambda e: e.tensor_reduce(out=hs[:, 0:nh], in_=v(sq[:]), axis=AX.X, op=ALU.add), reads=[d_sq], writes=[d_hs])
                P.op("act", lambda e: e.activation(out=hr[:, 0:nh], in_=hs[:, 0:nh], func=AF.Sqrt, bias=EPS, scale=1.0 / hd), reads=[d_hs], writes=[d_hr])
                P.op("dve", lambda e: e.reciprocal(out=hr[:, 0:nh], in_=hr[:, 0:nh]), reads=[d_hr], writes=[d_hr])
                P.op("dve", lambda e: e.tensor_tensor(out=v(qmn[:]), in0=v(src_ps), in1=hr[:, 0:nh].unsqueeze(2).to_broadcast([128, nh, hd]), op=ALU.mult),
                     reads=[d_src, d_hr], writes=[d_qmn])
                P.op("pool", lambda e: e.tensor_tensor(out=dst, in0=qmn[:], in1=gain[:], op=ALU.mult), reads=[d_qmn, d_c], writes=[d_dst])

            with contextlib.ExitStack() as stm:
                wkv = stm.enter_context(nc.sbuf_tensor("s_wkv", [128, 8, 1024], BF16)); d_wkv = Dep()
                mt_ = stm.enter_context(nc.sbuf_tensor("s_memt", [128, 2, D], F32)); d_mt = Dep()
                gme = stm.enter_context(nc.sbuf_tensor("s_gme", [128, D], F32)); d_gme = Dep()
                mhb = stm.enter_context(nc.sbuf_tensor("s_mhb", [128, 2, D], BF16)); d_mhb = Dep()
                mT = stm.enter_context(nc.sbuf_tensor("s_mT", [128, 8, 256], BF16)); d_mT = Dep()
                ss2 = stm.enter_context(nc.sbuf_tensor("s_ss2", [128, 2], F32)); d_ss2 = Dep()
                for k in range(8):
                    P.dma("pool", lambda e, k=k: e.dma_start(out=wkv[:, k, :], in_=wmemkv[k * 128:(k + 1) * 128, :]), writes=[d_wkv])
                P.dma("sp", lambda e: e.dma_start(out=mt_[:], in_=mem_d[:, :].rearrange("(t p) d -> p t d", p=128)), writes=[d_mt])
                P.dma("sp", lambda e: e.dma_start(out=gme[:], in_=gmem_d[0:1, :].partition_broadcast(128)), writes=[d_gme])
                for it in range(2):
                    P.op("act", lambda e, it=it: e.activation(out=junk[:], in_=mt_[:, it, :], func=AF.Square, accum_out=ss2[:, it:it + 1]),
                         reads=[d_mt], writes=[d_junk, d_ss2])
                P.op("act", lambda e: e.activation(out=ss2[:], in_=ss2[:], func=AF.Sqrt, bias=EPS, scale=1.0 / D), reads=[d_ss2], writes=[d_ss2])
                P.op("dve", lambda e: e.reciprocal(out=ss2[:], in_=ss2[:]), reads=[d_ss2], writes=[d_ss2])
                for it in range(2):
                    P.op("dve", lambda e, it=it: e.scalar_tensor_tensor(out=mhb[:, it, :], in0=mt_[:, it, :], scalar=ss2[:, it:it + 1], in1=gme[:],
                                                                        op0=ALU.mult, op1=ALU.mult), reads=[d_mt, d_ss2, d_gme], writes=[d_mhb])
                    for k in range(8):
                        P.op("pe", lambda e, it=it, k=k: e.transpose(out=pT[:, k, :], in_=mhb[:, it, k * 128:(k + 1) * 128], identity=idb[:]),
                             reads=[d_mhb, d_idb], writes=[d_pT])
                    P.op("act", lambda e, it=it: e.copy(out=mT[:, :, it * 128:(it + 1) * 128], in_=pT[:]), reads=[d_pT], writes=[d_mT])
                for it in range(2):
                    for hv in range(2):
                        for k in range(8):
                            P.op("pe", lambda e, it=it, hv=hv, k=k: e.matmul(pG[hv][:, :], lhsT=mT[:, k, it * 128:(it + 1) * 128], rhs=wkv[:, k, hv * 512:(hv + 1) * 512],
                                                                           start=(k == 0), stop=(k == 7)), reads=[d_mT, d_wkv], writes=[d_pG[hv]])
                    head_norm(pG[0][:, :], d_pG[0], mkn, 4, qmb[:], d_qmb)
                    for h in range(4):
                        P.op("pe", lambda e, h=h: e.transpose(out=pT[:, h, :], in_=qmb[:, h * 128:(h + 1) * 128], identity=idb[:]),
                             reads=[d_qmb, d_idb], writes=[d_pT])
                    P.op("act", lambda e, it=it: e.copy(out=kmT[:, :, it * 128:(it + 1) * 128], in_=pT[:, 0:4, :]), reads=[d_pT], writes=[d_kmT])
                    P.op("act", lambda e, it=it: e.copy(out=vm[:, it, :], in_=pG[1][:, :]), reads=[d_pG[1]], writes=[d_vm])

            wqm = sb("wqm", [128, 8, 512], BF16)
            wg = sb("wg", [128, 8, 3072], BF16)
            woa = sb("woa", [128, 4, 1024], BF16)
            wos = sb("wos", [128, 8, 1024], BF16)
            wom = sb("wom", [128, 4, 1024], BF16)
            wout = sb("wout", [128, 8, 1024], BF16)
            wr = sb("wr", [128, 8, 36], F32)
            for k in range(8):
                rows = slice(k * 128, (k + 1) * 128)
                P.dma("pool", lambda e, k=k, rows=rows: e.dma_start(out=wqm[:, k, :], in_=w_in[rows, C_QM:C_QM + 512]), writes=[d_w])
                for j in range(2):
                    P.dma("pool", lambda e, k=k, rows=rows, j=j: e.dma_start(out=wg[:, k, j * 1536:(j + 1) * 1536], in_=w_in[rows, C_G + j * 1536:C_G + (j + 1) * 1536]), writes=[d_w])
                P.dma("pool", lambda e, k=k, rows=rows: e.dma_start(out=wos[:, k, :], in_=wos_d[rows, :]), writes=[d_w])
                P.dma("pool", lambda e, k=k, rows=rows: e.dma_start(out=wout[:, k, :], in_=wout_d[rows, :]), writes=[d_w])
                P.dma("sp", lambda e, k=k, rows=rows: e.dma_start(out=wr[:, k, :], in_=wr_d[rows, :]), writes=[d_w])
            for k in range(4):
                rows = slice(k * 128, (k + 1) * 128)
                P.dma("pool", lambda e, k=k, rows=rows: e.dma_start(out=woa[:, k, :], in_=woa_d[rows, :]), writes=[d_w])
                P.dma("pool", lambda e, k=k, rows=rows: e.dma_start(out=wom[:, k, :], in_=wom_d[rows, :]), writes=[d_w])
            for ti in range(ntiles):
                r0 = ti * 128
                hbuf = ti % 2
                P.dma("sp", lambda e, r0=r0: e.dma_start(out=xt[:], in_=xown[r0:r0 + 128, :]), writes=[d_xt])
                P.dma("sp", lambda e, r0=r0: e.dma_start(out=oat[:], in_=oa_d[r0:r0 + 128, :]), reads=[d_oad], writes=[d_oat])
                P.dma("sp", lambda e, r0=r0: e.dma_start(out=ost[:], in_=os_d[r0:r0 + 128, :]), reads=[d_osd], writes=[d_ost])
                P.op("act", lambda e: e.activation(out=junk[:], in_=xt[:], func=AF.Square, accum_out=ssq[:]), reads=[d_xt], writes=[d_junk, d_ssq])
                P.op("act", lambda e: e.activation(out=rstd[:], in_=ssq[:], func=AF.Sqrt, bias=EPS, scale=1.0 / D), reads=[d_ssq], writes=[d_rstd])
                P.op("dve", lambda e: e.reciprocal(out=rstd[:], in_=rstd[:]), reads=[d_rstd], writes=[d_rstd])
                P.op("dve", lambda e: e.scalar_tensor_tensor(out=hb[:], in0=xt[:], scalar=rstd[:, 0:1], in1=gm[:], op0=ALU.mult, op1=ALU.mult),
                     reads=[d_xt, d_rstd, d_c], writes=[d_hb])
                for k in range(8):
                    P.op("pe", lambda e, k=k: e.transpose(out=pT[:, k, :], in_=hb[:, k * 128:(k + 1) * 128], identity=idb[:]), reads=[d_hb, d_idb], writes=[d_pT])
                P.op("act", lambda e: e.copy(out=hT[:], in_=pT[:]), reads=[d_pT], writes=[d_hT])
                for k in range(8):
                    P.op("pe", lambda e, k=k: e.matmul(pG[0][:, :], lhsT=hT[:, k, :], rhs=wqm[:, k, :], start=(k == 0), stop=(k == 7)),
                         reads=[d_hT, d_w], writes=[d_pG[0]])
                head_norm(pG[0][:, :], d_pG[0], mqn, 4, qmb[:], d_qmb)
                for h in range(4):
                    P.op("pe", lambda e, h=h: e.transpose(out=pT[:, h, :], in_=qmb[:, h * 128:(h + 1) * 128], identity=idb[:]), reads=[d_qmb, d_idb], writes=[d_pT])
                P.op("act", lambda e: e.copy(out=qmT[:], in_=pT[:, 0:4, :]), reads=[d_pT], writes=[d_qmT])
                for h in range(4):
                    for mt in range(2):
                        bank = pms[h // 2]
                        c0 = ((h % 2) * 2 + mt) * 128
                        P.op("pe", lambda e, h=h, mt=mt, bank=bank, c0=c0: e.matmul(bank[:, c0:c0 + 128], lhsT=kmT[:, h, mt * 128:(mt + 1) * 128], rhs=qmT[:, h, :],
                                                                                  start=True, stop=True), reads=[d_kmT, d_qmT], writes=[d_pms[h // 2]])
                for hp in range(2):
                    P.op("act", lambda e, hp=hp: e.activation(out=pm[:, hp * 2:(hp + 1) * 2, :, :].rearrange("p a b c -> p (a b c)"), in_=pms[hp][:, :], func=AF.Exp,
                                                              scale=float(128 ** -0.5)), reads=[d_pms[hp]], writes=[d_pm])
                for h in range(4):
                    for mt in range(2):
                        P.op("pe", lambda e, h=h, mt=mt: e.matmul(pmo[:, h * 128:(h + 1) * 128], lhsT=vm[:, mt, h * 128:(h + 1) * 128], rhs=pm[:, h, mt, :],
                                                                 start=(mt == 0), stop=(mt == 1)), reads=[d_vm, d_pm], writes=[d_pmo])
                for mt in range(2):
                    P.op("pe", lambda e, mt=mt: e.matmul(pG[1][:, :].rearrange("p (h t) -> p h t", h=4), lhsT=onesb[:], rhs=pm[:, :, mt, :],
                                                        start=(mt == 0), stop=(mt == 1)), reads=[d_c, d_pm], writes=[d_pG[1]])
                P.op("dve", lambda e: e.reciprocal(out=rcp[:], in_=pG[1][:, :]), reads=[d_pG[1]], writes=[d_rcp])
                P.op("dve", lambda e: e.tensor_tensor(out=omT[:].rearrange("p h t -> p (h t)"), in0=pmo[:, :], in1=rcp[:], op=ALU.mult),
                     reads=[d_pmo, d_rcp], writes=[d_omT])
                for g6 in range(6):
                    bk = g6 % 3
                    for k in range(8):
                        P.op("pe", lambda e, g6=g6, k=k, bk=bk: e.matmul(pG[bk][:, :], lhsT=hT[:, k, :], rhs=wg[:, k, g6 * 512:(g6 + 1) * 512], start=(k == 0), stop=(k == 7)),
                             reads=[d_hT, d_w], writes=[d_pG[bk]])
                    P.op("act", lambda e, g6=g6, bk=bk: e.activation(out=gs[:, g6 * 512:(g6 + 1) * 512], in_=pG[bk][:, :], func=AF.Sigmoid),
                         reads=[d_pG[bk]], writes=[d_gs])
                for k in range(4):
                    P.op("pe", lambda e, k=k: e.transpose(out=pT[:, k, :], in_=oat[:, k * 128:(k + 1) * 128], identity=idb[:]), reads=[d_oat, d_idb], writes=[d_pT])
                P.op("act", lambda e: e.copy(out=oaT[:], in_=pT[:, 0:4, :]), reads=[d_pT], writes=[d_oaT])
                for k in range(8):
                    P.op("pe", lambda e, k=k: e.transpose(out=pT[:, k, :], in_=ost[:, k * 128:(k + 1) * 128], identity=idb[:]), reads=[d_ost, d_idb], writes=[d_pT])
                P.op("act", lambda e: e.copy(out=osT[:], in_=pT[:]), reads=[d_pT], writes=[d_osT])
                for hv in range(2):
                    cs_ = slice(hv * 512, (hv + 1) * 512)
                    for k in range(4):
                        P.op("pe", lambda e, k=k, cs_=cs_: e.matmul(pG[0][:, :], lhsT=oaT[:, k, :], rhs=woa[:, k, cs_], start=(k == 0), stop=(k == 3)),
                             reads=[d_oaT, d_w], writes=[d_pG[0]])
                    for k in range(8):
                        P.op("pe", lambda e, k=k, cs_=cs_: e.matmul(pG[1][:, :], lhsT=osT[:, k, :], rhs=wos[:, k, cs_], start=(k == 0), stop=(k == 7)),
                             reads=[d_osT, d_w], writes=[d_pG[1]])
                    for k in range(4):
                        P.op("pe", lambda e, k=k, cs_=cs_: e.matmul(pG[2][:, :], lhsT=omT[:, k, :], rhs=wom[:, k, cs_], start=(k == 0), stop=(k == 3)),
                             reads=[d_omT, d_w], writes=[d_pG[2]])
                    P.op("dve", lambda e, hv=hv: e.tensor_tensor(out=mg[:], in0=pG[0][:, :], in1=gs[:, hv * 512:(hv + 1) * 512], op=ALU.mult),
                         reads=[d_pG[0], d_gs], writes=[d_mg])
                    P.op("dve", lambda e, hv=hv: e.tensor_tensor(out=tt[:], in0=pG[1][:, :], in1=gs[:, 1024 + hv * 512:1024 + (hv + 1) * 512], op=ALU.mult),
                         reads=[d_pG[1], d_gs], writes=[d_tt])
                    P.op("pool", lambda e: e.tensor_tensor(out=mg[:], in0=mg[:], in1=tt[:], op=ALU.add), reads=[d_mg, d_tt], writes=[d_mg])
                    P.op("dve", lambda e, hv=hv: e.tensor_tensor(out=tt[:], in0=pG[2][:, :], in1=gs[:, 2048 + hv * 512:2048 + (hv + 1) * 512], op=ALU.mult),
                         reads=[d_pG[2], d_gs], writes=[d_tt])
                    P.op("pool", lambda e, cs_=cs_: e.tensor_tensor(out=mgb[:, cs_], in0=mg[:], in1=tt[:], op=ALU.add), reads=[d_mg, d_tt], writes=[d_mgb])
                for k in range(8):
                    P.op("pe", lambda e, k=k: e.transpose(out=pT[:, k, :], in_=mgb[:, k * 128:(k + 1) * 128], identity=idb[:]), reads=[d_mgb, d_idb], writes=[d_pT])
                P.op("act", lambda e: e.copy(out=mgT[:], in_=pT[:]), reads=[d_pT], writes=[d_mgT])
                for hv in range(2):
                    cs_ = slice(hv * 512, (hv + 1) * 512)
                    for k in range(8):
                        P.op("pe", lambda e, k=k, cs_=cs_, hv=hv: e.matmul(pG[hv][:, :], lhsT=mgT[:, k, :], rhs=wout[:, k, cs_], start=(k == 0), stop=(k == 7)),
                             reads=[d_mgT, d_w], writes=[d_pG[hv]])
                    P.op("dve", lambda e, cs_=cs_, hv=hv: e.tensor_tensor(out=x1[:, cs_], in0=pG[hv][:, :], in1=xt[:, cs_], op=ALU.add),
                         reads=[d_pG[hv], d_xt], writes=[d_x1])
                P.dma("sp", lambda e, r0=r0: e.dma_start(out=x1_d[r0:r0 + 128, :], in_=x1[:]), reads=[d_x1], writes=[d_x1d])
                if stage < 2:
                    continue
                P.op("act", lambda e: e.activation(out=junk[:], in_=x1[:], func=AF.Square, accum_out=ssq[:]), reads=[d_x1], writes=[d_junk, d_ssq])
                P.op("act", lambda e: e.activation(out=rstd[:], in_=ssq[:], func=AF.Sqrt, bias=EPS, scale=1.0 / D), reads=[d_ssq], writes=[d_rstd])
                P.op("dve", lambda e: e.reciprocal(out=rstd[:], in_=rstd[:]), reads=[d_rstd], writes=[d_rstd])
                P.op("dve", lambda e: e.scalar_tensor_tensor(out=h2f[:], in0=x1[:], scalar=rstd[:, 0:1], in1=gf[:], op0=ALU.mult, op1=ALU.mult),
                     reads=[d_x1, d_rstd, d_c], writes=[d_h2f])
                P.op("pool", lambda e, hbuf=hbuf: e.tensor_copy(out=h2b[hbuf][:], in_=h2f[:]), reads=[d_h2f], writes=[d_h2b[hbuf]])
                for half in range(2):
                    for k4 in range(4):
                        k = half * 4 + k4
                        P.op("pe", lambda e, k=k, k4=k4: e.transpose(out=pF[:, k4 * 128:(k4 + 1) * 128], in_=h2f[:, k * 128:(k + 1) * 128], identity=idf[:]),
                             reads=[d_h2f, d_idf], writes=[d_pF])
                    P.op("act", lambda e, half=half: e.copy(out=h2T[:, half * 4:(half + 1) * 4, :].rearrange("p a b -> p (a b)"), in_=pF[:, :]), reads=[d_pF], writes=[d_h2T])
                for k in range(8):
                    P.op("pe", lambda e, k=k: e.matmul(pF[:, 0:36], lhsT=h2T[:, k, :], rhs=wr[:, k, :], start=(k == 0), stop=(k == 7)), reads=[d_h2T, d_w], writes=[d_pF])
                P.op("act", lambda e: e.copy(out=lg[:], in_=pF[:, 0:36]), reads=[d_pF], writes=[d_lg])
                P.op("dve", lambda e: e.tensor_reduce(out=sm[:, 0:1], in_=lg[:, 0:4], axis=AX.X, op=ALU.max), reads=[d_lg], writes=[d_sm])
                P.op("dve", lambda e: e.tensor_scalar(out=sm[:, 1:2], in0=sm[:, 0:1], scalar1=-1.0, scalar2=None, op0=ALU.mult), reads=[d_sm], writes=[d_sm])
                P.op("act", lambda e: e.activation(out=sm[:, 8:12], in_=lg[:, 0:4], func=AF.Exp, bias=sm[:, 1:2], accum_out=sm[:, 2:3]), reads=[d_lg, d_sm], writes=[d_sm])
                P.op("dve", lambda e: e.reciprocal(out=sm[:, 3:4], in_=sm[:, 2:3]), reads=[d_sm], writes=[d_sm])
                P.op("dve", lambda e: e.tensor_scalar(out=goh[:], in0=lg[:, 0:4], scalar1=sm[:, 0:1], scalar2=None, op0=ALU.is_ge), reads=[d_lg, d_sm], writes=[d_goh])
                P.op("dve", lambda e: e.tensor_tensor(out=lsel[:].rearrange("p (g e) -> p g e", g=4), in0=lg[:, 4:36].rearrange("p (g e) -> p g e", g=4),
                                                      in1=goh[:].unsqueeze(2).to_broadcast([128, 4, 8]), op=ALU.mult), reads=[d_lg, d_goh], writes=[d_lsel])
                P.op("dve", lambda e: e.tensor_reduce(out=les[:], in_=lsel[:].rearrange("p (g e) -> p e g", g=4), axis=AX.X, op=ALU.add), reads=[d_lsel], writes=[d_les])
                P.op("dve", lambda e: e.max(out=t8[:], in_=les[:]), reads=[d_les], writes=[d_t8])
                P.op("dve", lambda e: e.tensor_tensor(out=sm[:, 4:5], in0=t8[:, 1:2], in1=t8[:, 0:1], op=ALU.subtract), reads=[d_t8, d_sm], writes=[d_sm])
                P.op("act", lambda e: e.activation(out=sm[:, 5:6], in_=sm[:, 4:5], func=AF.Exp), reads=[d_sm], writes=[d_sm])
                P.op("dve", lambda e: e.tensor_scalar(out=sm[:, 6:7], in0=sm[:, 5:6], scalar1=1.0, scalar2=None, op0=ALU.add), reads=[d_sm], writes=[d_sm])
                P.op("dve", lambda e: e.reciprocal(out=sm[:, 6:7], in_=sm[:, 6:7]), reads=[d_sm], writes=[d_sm])
                P.op("dve", lambda e: e.tensor_tensor(out=sm[:, 7:8], in0=sm[:, 5:6], in1=sm[:, 6:7], op=ALU.mult), reads=[d_sm], writes=[d_sm])
                P.op("dve", lambda e, ti=ti: e.tensor_scalar(out=combs[:, ti, :], in0=sm[:, 6:8], scalar1=sm[:, 3:4], scalar2=None, op0=ALU.mult), reads=[d_sm], writes=[d_combs])
                for j in range(2):
                    P.op("dve", lambda e, j=j: e.tensor_scalar(out=oh[:, j * 32:(j + 1) * 32], in0=lg[:, 4:36], scalar1=t8[:, j:j + 1], scalar2=None, op0=ALU.is_equal),
                         reads=[d_lg, d_t8], writes=[d_oh])
                    P.op("dve", lambda e, j=j: e.tensor_tensor(out=oh[:, j * 32:(j + 1) * 32].rearrange("p (g e) -> p g e", g=4), in0=oh[:, j * 32:(j + 1) * 32].rearrange("p (g e) -> p g e", g=4),
                                                               in1=goh[:].unsqueeze(2).to_broadcast([128, 4, 8]), op=ALU.mult), reads=[d_oh, d_goh], writes=[d_oh])
                P.op("dve", lambda e: e.tensor_tensor(out=ohs[:], in0=oh[:, 0:32], in1=oh[:, 32:64], op=ALU.add), reads=[d_oh], writes=[d_ohs])
                P.op("pe", lambda e: e.matmul(pF[:, 64:96], lhsT=tri[:], rhs=ohs[:], start=True, stop=True), reads=[d_c, d_ohs], writes=[d_pF])
                P.op("pe", lambda e: e.matmul(pF[:, 128:160], lhsT=onesf[:], rhs=ohs[:], start=True, stop=True), reads=[d_c, d_ohs], writes=[d_pF])
                P.op("dve", lambda e: e.tensor_tensor(out=posf[:], in0=pF[:, 64:96], in1=ohs[:], op=ALU.subtract), reads=[d_pF, d_ohs], writes=[d_posf])
                P.op("dve", lambda e: e.tensor_tensor(out=posf[:], in0=posf[:], in1=base[:], op=ALU.add), reads=[d_posf, d_base], writes=[d_posf])
                P.op("dve", lambda e: e.tensor_tensor(out=posf[:], in0=posf[:], in1=ecap[:], op=ALU.add), reads=[d_posf, d_c], writes=[d_posf])
                P.op("dve", lambda e: e.tensor_tensor(out=base[:], in0=base[:], in1=pF[:, 128:160], op=ALU.add), reads=[d_base, d_pF, d_posf], writes=[d_base])
                for j in range(2):
                    P.op("dve", lambda e, j=j: e.tensor_tensor(out=oh[:, j * 32:(j + 1) * 32], in0=oh[:, j * 32:(j + 1) * 32], in1=posf[:], op=ALU.mult), reads=[d_oh, d_posf], writes=[d_oh])
                    P.op("dve", lambda e, j=j: e.tensor_reduce(out=idf2[:, j:j + 1], in_=oh[:, j * 32:(j + 1) * 32], axis=AX.X, op=ALU.add), reads=[d_oh], writes=[d_idf2])
                P.op("dve", lambda e, ti=ti: e.tensor_copy(out=idxs[:, ti, :], in_=idf2[:]), reads=[d_idf2], writes=[d_idxs])
                for j in range(2):
                    P.dma("pool", lambda e, ti=ti, j=j, hbuf=hbuf: e.indirect_dma_start(out=xbuf[:, :], out_offset=bass.IndirectOffsetOnAxis(ap=idxs[:, ti, j:j + 1], axis=0),
                                                                                      in_=h2b[hbuf][:], in_offset=None),
                          reads=[d_h2b[hbuf], d_idxs], writes=[d_xbuf])

        if stage < 3:
            return
        if hasattr(P, 'barrier'):
            P.barrier()
        with contextlib.ExitStack() as st:
            def sb(name, shape, dt):
                return st.enter_context(nc.sbuf_tensor("d_" + name, shape, dt))

            def ps(name, shape, dt=F32):
                return st.enter_context(nc.psum_tensor(name, shape, dt))
            NW = 2
            wge = [sb("wge%d" % i, [128, 8, 512], BF16) for i in range(NW)]; d_wge = [Dep() for _ in range(NW)]
            wue = [sb("wue%d" % i, [128, 8, 512], BF16) for i in range(NW)]; d_wue = [Dep() for _ in range(NW)]
            wde = [sb("wde%d" % i, [128, 4, 1024], BF16) for i in range(NW)]; d_wde = [Dep() for _ in range(NW)]
            xe = [sb("xe%d" % i, [128, 3, D], BF16) for i in range(NW)]; d_xe = [Dep() for _ in range(NW)]
            xeT = sb("xeT", [128, 8, CAP], BF16); d_xeT = Dep()
            sg = sb("sg", [128, CAP], F32); d_sg = Dep()
            hTe = sb("hTe", [128, 4, CAP], BF16); d_hTe = Dep()
            ye = [sb("ye%d" % i, [128, 3, D], F32) for i in range(NW)]; d_ye = [Dep() for _ in range(NW)]
            pT = ps("pTe", [128, 8, 128], BF16); d_pT = Dep()
            pg_ = [ps("pge%d" % i, [128, 512], F32) for i in range(2)]; d_pg = [Dep() for _ in range(2)]
            pu_ = [ps("pue%d" % i, [128, 512], F32) for i in range(2)]; d_pu = [Dep() for _ in range(2)]
            py_ = [ps("pye%d" % i, [128, 512], F32) for i in range(2)]; d_py = [Dep() for _ in range(2)]
            for ie, ex in enumerate(experts):
                b = ie % NW
                P.dma("pool", lambda e, b=b, ex=ex: e.dma_start(out=wge[b][:], in_=wgate_d[ex].rearrange("(k p) f -> p k f", p=128)), writes=[d_wge[b]])
                P.dma("pool", lambda e, b=b, ex=ex: e.dma_start(out=wue[b][:], in_=wup_d[ex].rearrange("(k p) f -> p k f", p=128)), writes=[d_wue[b]])
                P.dma("pool", lambda e, b=b, ex=ex: e.dma_start(out=wde[b][:], in_=wdown_d[ex].rearrange("(k p) f -> p k f", p=128)), writes=[d_wde[b]])
                P.dma("sp", lambda e, b=b, ex=ex: e.dma_start(out=xe[b][:], in_=xbuf[ex * CAP:(ex + 1) * CAP, :].rearrange("(s p) d -> p s d", p=128)),
                      reads=[d_xbuf], writes=[d_xe[b]])
                for s in range(3):
                    for k in range(8):
                        P.op("pe", lambda e, b=b, s=s, k=k: e.transpose(out=pT[:, k, :], in_=xe[b][:, s, k * 128:(k + 1) * 128], identity=idb[:]),
                             reads=[d_xe[b], d_idb], writes=[d_pT])
                    P.op("act", lambda e, s=s: e.copy(out=xeT[:, :, s * 128:(s + 1) * 128], in_=pT[:]), reads=[d_pT], writes=[d_xeT])
                for ft in range(4):
                    pb = ft % 2
                    for k in range(8):
                        P.op("pe", lambda e, b=b, ft=ft, k=k, pb=pb: e.matmul(pg_[pb][:, 0:CAP], lhsT=wge[b][:, k, ft * 128:(ft + 1) * 128], rhs=xeT[:, k, :],
                                                                            start=(k == 0), stop=(k == 7)), reads=[d_wge[b], d_xeT], writes=[d_pg[pb]])
                    for k in range(8):
                        P.op("pe", lambda e, b=b, ft=ft, k=k, pb=pb: e.matmul(pu_[pb][:, 0:CAP], lhsT=wue[b][:, k, ft * 128:(ft + 1) * 128], rhs=xeT[:, k, :],
                                                                            start=(k == 0), stop=(k == 7)), reads=[d_wue[b], d_xeT], writes=[d_pu[pb]])
                    P.op("act", lambda e, pb=pb: e.activation(out=sg[:], in_=pg_[pb][:, 0:CAP], func=AF.Silu), reads=[d_pg[pb]], writes=[d_sg])
                    P.op("dve", lambda e, ft=ft, pb=pb: e.tensor_tensor(out=hTe[:, ft, :], in0=sg[:], in1=pu_[pb][:, 0:CAP], op=ALU.mult),
                         reads=[d_sg, d_pu[pb]], writes=[d_hTe])
                for s in range(3):
                    for hv in range(2):
                        for ft in range(4):
                            P.op("pe", lambda e, b=b, s=s, hv=hv, ft=ft: e.matmul(py_[hv][:, :], lhsT=hTe[:, ft, s * 128:(s + 1) * 128], rhs=wde[b][:, ft, hv * 512:(hv + 1) * 512],
                                                                                start=(ft == 0), stop=(ft == 3)), reads=[d_hTe, d_wde[b]], writes=[d_py[hv]])
                        P.op("act" if hv == 0 else "dve", (lambda e, b=b, s=s, hv=hv: e.copy(out=ye[b][:, s, hv * 512:(hv + 1) * 512], in_=py_[hv][:, :])) if hv == 0 else
                             (lambda e, b=b, s=s, hv=hv: e.tensor_copy(out=ye[b][:, s, hv * 512:(hv + 1) * 512], in_=py_[hv][:, :])),
                             reads=[d_py[hv]], writes=[d_ye[b]])
                P.dma("sp", lambda e, b=b, ex=ex: e.dma_start(out=ybuf[ex * CAP:(ex + 1) * CAP, :].rearrange("(s p) d -> p s d", p=128), in_=ye[b][:]),
                      reads=[d_ye[b]], writes=[d_ybuf])

        if hasattr(P, 'barrier'):
            P.barrier()
        with contextlib.ExitStack() as st:
            def sb(name, shape, dt):
                return st.enter_context(nc.sbuf_tensor("d_" + name, shape, dt))
            NF = 2
            x1t = [sb("x1t%d" % i, [128, D], F32) for i in range(NF)]; d_x1t = [Dep() for _ in range(NF)]
            y1 = [sb("y1t%d" % i, [128, D], F32) for i in range(NF)]; d_y1 = [Dep() for _ in range(NF)]
            y2 = [sb("y2t%d" % i, [128, D], F32) for i in range(NF)]; d_y2 = [Dep() for _ in range(NF)]
            ot = [sb("ot%d" % i, [128, D], F32) for i in range(NF)]; d_ot = [Dep() for _ in range(NF)]
            for ti in range(ntiles):
                b = ti % NF
                r0 = ti * 128
                P.dma("sp", lambda e, b=b, r0=r0: e.dma_start(out=x1t[b][:], in_=x1_d[r0:r0 + 128, :]), reads=[d_x1d], writes=[d_x1t[b]])
                P.dma("pool", lambda e, b=b, ti=ti: e.indirect_dma_start(out=y1[b][:], out_offset=None, in_=ybuf[:, :],
                                                                        in_offset=bass.IndirectOffsetOnAxis(ap=idxs[:, ti, 0:1], axis=0)),
                      reads=[d_ybuf, d_idxs], writes=[d_y1[b]])
                P.dma("pool", lambda e, b=b, ti=ti: e.indirect_dma_start(out=y2[b][:], out_offset=None, in_=ybuf[:, :],
                                                                        in_offset=bass.IndirectOffsetOnAxis(ap=idxs[:, ti, 1:2], axis=0)),
                      reads=[d_ybuf, d_idxs], writes=[d_y2[b]])
                P.op("dve", lambda e, b=b, ti=ti: e.scalar_tensor_tensor(out=ot[b][:], in0=y1[b][:], scalar=combs[:, ti, 0:1], in1=x1t[b][:], op0=ALU.mult, op1=ALU.add),
                     reads=[d_y1[b], d_combs, d_x1t[b]], writes=[d_ot[b]])
                P.op("dve", lambda e, b=b, ti=ti: e.scalar_tensor_tensor(out=ot[b][:], in0=y2[b][:], scalar=combs[:, ti, 1:2], in1=ot[b][:], op0=ALU.mult, op1=ALU.add),
                     reads=[d_y2[b], d_combs, d_ot[b]], writes=[d_ot[b]])
                P.dma("sp", lambda e, b=b, r0=r0: e.dma_start(out=out_d[r0:r0 + 128, :], in_=ot[b][:]), reads=[d_ot[b]], writes=[d_out])


def host_consts_d(z):
    tri = (np.arange(128)[:, None] <= np.arange(128)[None, :]).astype(np.float32)
    return dict(mqn=np.tile(z['mem_q_norm'], 4)[None, :], mkn=np.tile(z['mem_k_norm'], 4)[None, :],
                w_r=np.ascontiguousarray(np.concatenate([z['w_router_group'], z['w_router_expert']], axis=1)),
                trif=tri, ecap=(np.arange(32, dtype=np.float32) * CAP)[None, :], ident=np.eye(128, dtype=np.float32))


def build_full():
    nc = bass.Bass("TRN2", target_bir_lowering=False)
    P = Prog(nc)

    def di(name, shape, dt=F32):
        return nc.dram_tensor(name, shape, dt, kind="ExternalInput")
    xin = di("xin", [TP + T, D]); w_in = di("w_in", [D, IN_COLS]); g_mix = di("g_mix", [1, D])
    qkn = di("qkn", [2, 512]); cs = di("cs", [TP + T, 64]); ident_d = di("ident", [128, 128])
    e_d = di("e_d", [32, NK], BF16); vq_d = di("vq", [1, 1024]); nb_d = di("nb", [1, 1024]); oq_d = di("oq", [1, 1024])
    tri_d = di("tri", [128, 128], BF16)
    cwl_d = di("cwl", [128, 16, 4]); cbl_d = di("cbl", [128, 16]); dtb_d = di("dtb", [1, 16]); alog_d = di("alog", [1, 16])
    dsk_d = di("dsk", [1, 16]); ssdn_d = di("ssdn", [1, D]); tri2_d = di("tri2", [128, 2, 256]); sel_d = di("sel_d", [16, 16, 128])
    tb_d = di("tb_d", [128, 384]); pflag_d = di("pflag", [1, 1])
    mem_d = di("mem", [256, D]); gmem_d = di("g_mem", [1, D]); wmemkv = di("w_mem_kv", [D, 1024])
    mqn_d = di("mqn", [1, 512]); mkn_d = di("mkn", [1, 512])
    woa_d = di("w_o_moba", [512, D]); wos_d = di("w_o_ssd", [D, D]); wom_d = di("w_o_mem", [512, D]); wout_d = di("w_out", [D, D])
    gffn_d = di("g_ffn", [1, D]); wr_d = di("w_r", [D, 36])
    wgate_d = di("w_gate", [NE, D, 512]); wup_d = di("w_up", [NE, D, 512]); wdown_d = di("w_down", [NE, 512, D])
    trif_d = di("trif", [128, 128]); ecap_d = di("ecap", [1, 32])
    out_d = nc.dram_tensor("out", [T, D], F32, kind="ExternalOutput")
    kT_d = nc.dram_tensor("kT_d", [512, NK], BF16)
    v_d = nc.dram_tensor("v_d", [NK, 512], BF16)
    qT_d = nc.dram_tensor("qT_d", [512, T], BF16)
    oa_d = nc.dram_tensor("oa_d", [T, 512], BF16)
    os_d = nc.dram_tensor("os_d", [T, D], BF16)
    x1_d = nc.dram_tensor("x1_d", [T, D], F32)
    xbuf = nc.dram_tensor("xbuf", [NE * CAP, D], BF16)
    ybuf = nc.dram_tensor("ybuf", [NE * CAP, D], F32)

    with contextlib.ExitStack() as st0:
        idf = st0.enter_context(nc.sbuf_tensor("idf", [128, 128], F32)); d_idf = Dep()
        idb = st0.enter_context(nc.sbuf_tensor("idb", [128, 128], BF16)); d_idb = Dep()
        P.dma("sp", lambda e: e.dma_start(out=idf[:], in_=ident_d[:, :]), writes=[d_idf])
        P.op("dve", lambda e: e.tensor_copy(out=idb[:], in_=idf[:]), reads=[d_idf], writes=[d_idb])
        with contextlib.ExitStack() as st:
            phase_a(nc, P, st, xin, w_in, g_mix, qkn, cs, idb, d_idb, kT_d, v_d, qT_d)
        P.barrier()
        with contextlib.ExitStack() as st:
            oa = st.enter_context(nc.sbuf_tensor("s_oa", [128, 32, 512], BF16)); d_oa = Dep()
            phase_b(nc, P, st, kT_d, v_d, qT_d, e_d, vq_d, nb_d, oq_d, tri_d, idb, d_idb, oa, d_oa)
            P.dma("sp", lambda e: e.dma_start(out=oa_d[:, :].rearrange("(t p) c -> p t c", p=128), in_=oa[:]), reads=[d_oa])
        P.barrier()
        with contextlib.ExitStack() as st:
            phase_c(nc, P, st, xin, w_in, g_mix, cwl_d, cbl_d, dtb_d, alog_d, dsk_d, ssdn_d, tri2_d, sel_d, tb_d, pflag_d,
                    idb, d_idb, os_d, Dep())
        P.barrier()
        phase_def(nc, P, xin[TP:TP + T, :], w_in, g_mix, mem_d, gmem_d, wmemkv, mqn_d, mkn_d, woa_d, wos_d, wom_d, wout_d, gffn_d, wr_d,
                  wgate_d, wup_d, wdown_d, trif_d, ecap_d, oa_d, Dep(), os_d, Dep(), x1_d, xbuf, ybuf, out_d, idb, d_idb, idf, d_idf)
        P.emit()
    return nc


_NC_CACHE = {}


def kernel(x, mem, g_mix, w_in, moba_q_norm, moba_k_norm, conv_w, conv_b, dt_bias, a_log, d_skip, ssd_norm, g_mem, w_mem_kv,
           mem_q_norm, mem_k_norm, w_o_moba, w_o_ssd, w_o_mem, w_out, g_ffn, w_router_group, w_router_expert, w_gate, w_up, w_down):
    f32 = np.float32
    A = lambda a: np.ascontiguousarray(np.asarray(a, dtype=f32))
    x = A(x); mem = A(mem)
    z = dict(conv_w=A(conv_w), conv_b=A(conv_b), dt_bias=A(dt_bias), a_log=A(a_log), d_skip=A(d_skip), ssd_norm=A(ssd_norm),
             mem_q_norm=A(mem_q_norm), mem_k_norm=A(mem_k_norm), w_router_group=A(w_router_group), w_router_expert=A(w_router_expert))
    if "nc" not in _NC_CACHE:
        _NC_CACHE["nc"] = build_full()
    nc = _NC_CACHE["nc"]
    pos = np.arange(2 * T, dtype=f32)
    inv = (10000.0 ** (-np.arange(32, dtype=f32) / 32)).astype(f32)
    ang = pos[:, None] * inv[None, :]
    cs_full = np.concatenate([np.cos(ang), np.sin(ang)], axis=1).astype(f32)
    qkn = np.stack([np.tile(A(moba_q_norm), 8), np.tile(A(moba_k_norm), 8)]).astype(f32)
    shared = dict(w_in=A(w_in), g_mix=A(g_mix)[None, :], qkn=qkn, ident=np.eye(128, dtype=f32),
                  g_mem=A(g_mem)[None, :], w_mem_kv=A(w_mem_kv), w_o_moba=A(w_o_moba), w_o_ssd=A(w_o_ssd), w_o_mem=A(w_o_mem),
                  w_out=A(w_out), g_ffn=A(g_ffn)[None, :], w_gate=A(w_gate), w_up=A(w_up), w_down=A(w_down))
    shared.update(host_consts_d(z))
    in_maps = []
    for c in range(8):
        bb, hh = c // 2, c % 2
        if hh == 0:
            xin = np.concatenate([np.zeros((TP, D), f32), x[bb, :T]], 0)
            csc = np.concatenate([cs_full[:TP], cs_full[:T]], 0)
        else:
            xin = x[bb]
            csc = cs_full
        m = dict(shared)
        m.update(xin=np.ascontiguousarray(xin), cs=np.ascontiguousarray(csc), mem=mem[bb])
        m.update(host_consts(hh))
        m.update(host_consts_c(z, hh))
        in_maps.append(m)
    res = run_bass_kernel_spmd(nc, in_maps, core_ids=list(range(8)))
    out = np.empty((4, 2 * T, D), f32)
    for c in range(8):
        bb, hh = c // 2, c % 2
        out[bb, hh * T:(hh + 1) * T] = np.asarray(res.results[c]["out"], dtype=f32)
    return out
```

```python
import contextlib
import numpy as np
import ml_dtypes
import concourse.bass as bass
import concourse.mybir as mybir
from concourse.bass_utils import run_bass_kernel_spmd

F32 = mybir.dt.float32
BF16 = mybir.dt.bfloat16
I32 = mybir.dt.int32
U32 = mybir.dt.uint32
AF = mybir.ActivationFunctionType
ALU = mybir.AluOpType
AX = mybir.AxisListType

T = 4096
TP = 4096
NK = TP + T
D = 1024
EPS = 1e-6
IN_COLS = 8208
BIGG = 30000.0
MB = 1000.0
NEG = -30000.0
C_Z, C_X, C_DT = 1536, 2560, 4608
C_QM, C_G = 4624, 5136
CAP = 384
NE = 32


class Dep:
    __slots__ = ("w", "r")

    def __init__(self):
        self.w = None
        self.r = {}


class Op:
    __slots__ = ("eng", "fn", "deps", "is_dma", "sig", "sigval", "dsem", "dval", "prev")

    def __init__(self, eng, fn, is_dma):
        self.eng = eng
        self.fn = fn
        self.is_dma = is_dma
        self.deps = []
        self.sig = False
        self.sigval = 0
        self.dsem = None
        self.dval = 0
        self.prev = None


class Prog:
    ENGS = ("pe", "act", "dve", "pool", "sp")

    def __init__(self, nc, ndma_sems=12):
        self.nc = nc
        self.ops = {e: [] for e in self.ENGS}
        self.ndma = {e: 0 for e in self.ENGS}
        self.dma_last = {}
        self.ndma_sems = ndma_sems
        self.all_dma = []

    def _add(self, o, reads, writes):
        deps = {}
        for t in reads:
            if t.w is not None:
                deps[id(t.w)] = t.w
        for t in writes:
            if t.w is not None:
                deps[id(t.w)] = t.w
            for r in t.r.values():
                deps[id(r)] = r
        for t in reads:
            key = id(o) if o.is_dma else o.eng
            t.r[key] = o
        for t in writes:
            t.w = o
            t.r = {}
        dl = []
        for d in deps.values():
            if d is o:
                continue
            if (not d.is_dma) and (not o.is_dma) and d.eng == "pe" and o.eng == "pe":
                continue
            dl.append(d)
            if not d.is_dma:
                d.sig = True
        o.deps = dl
        self.ops[o.eng].append(o)
        return o

    def op(self, eng, fn, reads=(), writes=()):
        return self._add(Op(eng, fn, False), reads, writes)

    def dma(self, eng, fn, reads=(), writes=()):
        o = Op(eng, fn, True)
        n = self.ndma[eng]
        self.ndma[eng] += 1
        slot = (eng, n % self.ndma_sems)
        o.dsem = slot
        o.prev = self.dma_last.get(slot)
        o.dval = (o.prev.dval if o.prev else 0) + 16
        self.dma_last[slot] = o
        self.all_dma.append(o)
        return self._add(o, reads, writes)


    def barrier(self):
        lasts = []
        for e in self.ENGS:
            for o in reversed(self.ops[e]):
                if (not o.is_dma) and o.fn is not None:
                    o.sig = True
                    lasts.append(o)
                    break
        dmas = list(self.dma_last.values())
        for e in self.ENGS:
            o = Op(e, None, False)
            o.deps = [d for d in lasts if d.eng != e] + dmas
            self.ops[e].append(o)

    def emit(self):
        nc = self.nc
        import contextlib
        with contextlib.ExitStack() as st:
            esem = {e: st.enter_context(nc.semaphore("S_" + e)) for e in self.ENGS}
            dsem = {}
            for e in self.ENGS:
                if self.ndma[e]:
                    for i in range(min(self.ndma_sems, self.ndma[e])):
                        dsem[(e, i)] = st.enter_context(nc.semaphore("D_%s_%d" % (e, i)))
            for e in self.ENGS:
                c = 0
                for o in self.ops[e]:
                    if (not o.is_dma) and o.sig and o.fn is not None:
                        c += 1
                        o.sigval = c
            block = st.enter_context(nc.Block())
            handles = {"pe": block.tensor, "act": block.scalar, "dve": block.vector,
                       "pool": block.gpsimd, "sp": block.sync}

            def make(e):
                def body(eng):
                    known = {}

                    def wait(sem, key, val):
                        if known.get(key, 0) < val:
                            eng.wait_ge(sem, val)
                            known[key] = val
                    for o in self.ops[e]:
                        for d in o.deps:
                            if d.is_dma:
                                wait(dsem[d.dsem], d.dsem, d.dval)
                            else:
                                wait(esem[d.eng], d.eng, d.sigval)
                        if o.is_dma:
                            if o.prev is not None:
                                wait(dsem[o.dsem], o.dsem, o.prev.dval)
                            o.fn(eng).then_inc(dsem[o.dsem], 16)
                        elif o.fn is not None:
                            ins = o.fn(eng)
                            if o.sig:
                                ins.then_inc(esem[e], 1)
                    if e == "sp":
                        for slot, o in self.dma_last.items():
                            wait(dsem[slot], slot, o.dval)
                return body
            for e in self.ENGS:
                if self.ops[e] or e == "sp":
                    handles[e](make(e))


def phase_a(nc, P, st, xin, w_in, g_mix, qkn, cs, idb, d_idb, kT_d, v_d, qT_d):
    def sb(name, shape, dt):
        return st.enter_context(nc.sbuf_tensor("a_" + name, shape, dt))

    def ps(name, shape, dt=F32):
        return st.enter_context(nc.psum_tensor("a_" + name, shape, dt))
    wq = sb("wq", [128, 8, 1536], BF16); d_wq = Dep()
    gm = sb("gm", [128, D], F32); d_gm = Dep()
    gq = sb("gq", [128, 2, 512], F32); d_gq = Dep()
    NB = 2
    xt = [sb("xt%d" % i, [128, D], F32) for i in range(NB)]; d_xt = [Dep() for _ in range(NB)]
    cst = [sb("cst%d" % i, [128, 64], F32) for i in range(NB)]; d_cst = [Dep() for _ in range(NB)]
    junk = sb("junk", [128, D], BF16); d_junk = Dep()
    ssq = sb("ssq", [128, 1], F32); d_ssq = Dep()
    rstd = sb("rstd", [128, 1], F32); d_rstd = Dep()
    hb = sb("hb", [128, D], BF16); d_hb = Dep()
    hT = [sb("hT%d" % i, [128, 8, 128], BF16) for i in range(NB)]; d_hT = [Dep() for _ in range(NB)]
    pT = ps("pT", [128, 8, 128], BF16); d_pT = Dep()
    pq = [[ps("pq%d_%d" % (j, i), [128, 512], F32) for i in range(3)] for j in range(2)]; d_pq = [[Dep() for _ in range(3)] for _ in range(2)]
    pkT = ps("pkT", [128, 4, 128], BF16); d_pkT = Dep()
    sq = sb("sq", [128, 512], F32); d_sq = Dep()
    hs = sb("hs", [128, 8], F32); d_hs = Dep()
    hr = sb("hr", [128, 8], F32); d_hr = Dep()
    qn = sb("qn", [128, 512], F32); d_qn = Dep()
    t1 = sb("t1", [128, 256], F32); d_t1 = Dep()
    t2 = sb("t2", [128, 256], F32); d_t2 = Dep()
    t3 = sb("t3", [128, 256], F32); d_t3 = Dep()
    t4 = sb("t4", [128, 256], F32); d_t4 = Dep()
    qr = sb("qr", [128, 512], BF16); d_qr = Dep()
    kTs = [sb("kTs%d" % i, [128, 4, 128], BF16) for i in range(NB)]; d_kTs = [Dep() for _ in range(NB)]
    vs = [sb("vs%d" % i, [128, 512], BF16) for i in range(NB)]; d_vs = [Dep() for _ in range(NB)]

    for k in range(8):
        P.dma("pool", lambda e, k=k: e.dma_start(out=wq[:, k, :], in_=w_in[k * 128:(k + 1) * 128, 0:1536]), writes=[d_wq])
    P.dma("sp", lambda e: e.dma_start(out=gm[:], in_=g_mix[0:1, :].partition_broadcast(128)), writes=[d_gm])
    for i in range(2):
        P.dma("sp", lambda e, i=i: e.dma_start(out=gq[:, i, :], in_=qkn[i:i + 1, :].partition_broadcast(128)), writes=[d_gq])
    P.op("dve", lambda e: e.tensor_scalar(out=gq[:, 0, :], in0=gq[:, 0, :], scalar1=0.125, scalar2=None, op0=ALU.mult),
         reads=[d_gq], writes=[d_gq])

    def qk_post(src_ps, d_src, which, b):
        P.op("act", lambda e: e.activation(out=sq[:], in_=src_ps[:], func=AF.Square), reads=[d_src], writes=[d_sq])
        P.op("dve", lambda e: e.tensor_reduce(out=hs[:], in_=sq[:].rearrange("p (h d) -> p h d", h=8), axis=AX.X, op=ALU.add),
             reads=[d_sq], writes=[d_hs])
        P.op("act", lambda e: e.activation(out=hr[:], in_=hs[:], func=AF.Sqrt, bias=EPS, scale=1.0 / 64), reads=[d_hs], writes=[d_hr])
        P.op("dve", lambda e: e.reciprocal(out=hr[:], in_=hr[:]), reads=[d_hr], writes=[d_hr])
        P.op("dve", lambda e: e.tensor_tensor(out=qn[:].rearrange("p (h d) -> p h d", h=8), in0=src_ps[:].rearrange("p (h d) -> p h d", h=8),
                                              in1=hr[:].unsqueeze(2).to_broadcast([128, 8, 64]), op=ALU.mult),
             reads=[d_src, d_hr], writes=[d_qn])
        P.op("pool", lambda e: e.tensor_tensor(out=qn[:], in0=qn[:], in1=gq[:, which, :], op=ALU.mult), reads=[d_qn, d_gq], writes=[d_qn])
        q3 = qn[:].rearrange("p (h d) -> p h d", h=8)
        q1 = q3[:, :, 0:32]
        q2 = q3[:, :, 32:64]
        cosb = cst[b][:, 0:32].unsqueeze(1).to_broadcast([128, 8, 32])
        sinb = cst[b][:, 32:64].unsqueeze(1).to_broadcast([128, 8, 32])
        v3 = lambda t: t[:].rearrange("p (h d) -> p h d", h=8)
        P.op("dve", lambda e: e.tensor_tensor(out=v3(t1), in0=q1, in1=cosb, op=ALU.mult), reads=[d_qn, d_cst[b]], writes=[d_t1])
        P.op("pool", lambda e: e.tensor_tensor(out=v3(t2), in0=q2, in1=sinb, op=ALU.mult), reads=[d_qn, d_cst[b]], writes=[d_t2])
        P.op("dve", lambda e: e.tensor_tensor(out=v3(t3), in0=q2, in1=cosb, op=ALU.mult), reads=[d_qn, d_cst[b]], writes=[d_t3])
        P.op("pool", lambda e: e.tensor_tensor(out=v3(t4), in0=q1, in1=sinb, op=ALU.mult), reads=[d_qn, d_cst[b]], writes=[d_t4])
        r3 = qr[:].rearrange("p (h d) -> p h d", h=8)
        P.op("dve", lambda e: e.tensor_tensor(out=r3[:, :, 0:32], in0=v3(t1), in1=v3(t2), op=ALU.subtract), reads=[d_t1, d_t2], writes=[d_qr])
        P.op("pool", lambda e: e.tensor_tensor(out=r3[:, :, 32:64], in0=v3(t3), in1=v3(t4), op=ALU.add), reads=[d_t3, d_t4], writes=[d_qr])

    def to_T_and_store(dst_dram, col0, b):
        for c in range(4):
            P.op("pe", lambda e, c=c: e.transpose(out=pkT[:, c, :], in_=qr[:, c * 128:(c + 1) * 128], identity=idb[:]),
                 reads=[d_qr, d_idb], writes=[d_pkT])
        P.op("act", lambda e: e.copy(out=kTs[b][:], in_=pkT[:]), reads=[d_pkT], writes=[d_kTs[b]])
        P.dma("sp", lambda e: e.dma_start(out=dst_dram[:, col0:col0 + 128].rearrange("(c p) t -> p c t", p=128), in_=kTs[b][:]),
              reads=[d_kTs[b]])

    def front(ti):
        b = ti % NB
        own = ti >= 32
        r0 = ti * 128
        P.dma("sp", lambda e: e.dma_start(out=xt[b][:], in_=xin[r0:r0 + 128, :]), writes=[d_xt[b]])
        P.dma("sp", lambda e: e.dma_start(out=cst[b][:], in_=cs[r0:r0 + 128, :]), writes=[d_cst[b]])
        P.op("act", lambda e: e.activation(out=junk[:], in_=xt[b][:], func=AF.Square, accum_out=ssq[:]), reads=[d_xt[b]], writes=[d_junk, d_ssq])
        P.op("act", lambda e: e.activation(out=rstd[:], in_=ssq[:], func=AF.Sqrt, bias=EPS, scale=1.0 / D), reads=[d_ssq], writes=[d_rstd])
        P.op("dve", lambda e: e.reciprocal(out=rstd[:], in_=rstd[:]), reads=[d_rstd], writes=[d_rstd])
        P.op("dve", lambda e: e.scalar_tensor_tensor(out=hb[:], in0=xt[b][:], scalar=rstd[:, 0:1], in1=gm[:], op0=ALU.mult, op1=ALU.mult),
             reads=[d_xt[b], d_rstd, d_gm], writes=[d_hb])
        for k in range(8):
            P.op("pe", lambda e, k=k: e.transpose(out=pT[:, k, :], in_=hb[:, k * 128:(k + 1) * 128], identity=idb[:]), reads=[d_hb, d_idb], writes=[d_pT])
        P.op("act", lambda e: e.copy(out=hT[b][:], in_=pT[:]), reads=[d_pT], writes=[d_hT[b]])
        groups = [0, 1, 2] if own else [1, 2]
        for g in groups:
            for k in range(8):
                P.op("pe", lambda e, g=g, k=k: e.matmul(pq[b][g][:], lhsT=hT[b][:, k, :], rhs=wq[:, k, g * 512:(g + 1) * 512], start=(k == 0), stop=(k == 7)),
                     reads=[d_hT[b], d_wq], writes=[d_pq[b][g]])

    def post(ti):
        b = ti % NB
        own = ti >= 32
        r0 = ti * 128
        qk_post(pq[b][1], d_pq[b][1], 1, b)
        to_T_and_store(kT_d, r0, b)
        if own:
            qk_post(pq[b][0], d_pq[b][0], 0, b)
            to_T_and_store(qT_d, r0 - TP, b)
        P.op("act", lambda e: e.copy(out=vs[b][:], in_=pq[b][2][:]), reads=[d_pq[b][2]], writes=[d_vs[b]])
        P.dma("sp", lambda e: e.dma_start(out=v_d[r0:r0 + 128, :], in_=vs[b][:]), reads=[d_vs[b]])

    front(0)
    for ti in range(64):
        if ti + 1 < 64:
            front(ti + 1)
        post(ti)


def phase_b(nc, P, st, kT_d, v_d, qT_d, e_d, vq_d, nb_d, oq_d, tri_d, idb, d_idb, oa, d_oa, heads=range(8), nblk=16):
    def sb(name, shape, dt):
        return st.enter_context(nc.sbuf_tensor("b_" + name, shape, dt))

    def ps(name, shape, dt=F32):
        return st.enter_context(nc.psum_tensor(name, shape, dt))
    NB = 2
    kaug = [sb("kaug%d" % i, [96, NK], BF16) for i in range(NB)]; d_kaug = [Dep() for _ in range(NB)]
    vaug = [sb("vaug%d" % i, [128, 64, 65], BF16) for i in range(NB)]; d_vaug = [Dep() for _ in range(NB)]
    qaug = [sb("qaug%d" % i, [96, T], BF16) for i in range(NB)]; d_qaug = [Dep() for _ in range(NB)]
    d_qmb = [Dep() for _ in range(NB)]
    vq = sb("vq_s", [128, 1024], F32); d_c = Dep()
    nb = sb("nb_s", [128, 1024], F32)
    oq = sb("oq_s", [128, 1024], F32)
    tri = sb("tri_s", [128, 128], BF16)
    ksum = sb("ksum", [64, 32], F32); d_ksum = Dep()
    kmean = sb("kmean", [64, 32], BF16); d_kmean = Dep()
    g1 = sb("g1", [128, 512], F32); d_g1 = Dep()
    top8 = sb("top8", [128, 16, 8], F32); d_top8 = Dep()
    sel = sb("sel", [128, 512], F32); d_sel = Dep()
    mbp = sb("mbp", [128, 16, 96], BF16); d_mbp = Dep()
    NPT = 3
    pt = [sb("pt%d" % i, [128, 512], BF16) for i in range(NPT)]; d_pt = [Dep() for _ in range(NPT)]
    rc = sb("rc", [128, 2], F32); d_rc = [Dep(), Dep()]
    pg = ps("pg", [128, 512], F32); d_pg = Dep()
    pmT = ps("pmT", [96, 8, 128], BF16); d_pmT = Dep()
    psS = [ps("psS%d" % i, [128, 512], F32) for i in range(NPT)]; d_psS = [Dep() for _ in range(NPT)]
    po = [ps("po%d" % i, [128, 65], F32) for i in range(2)]; d_po = [Dep() for _ in range(2)]

    for i in range(NB):
        P.dma("sp", lambda e, i=i: e.dma_start(out=kaug[i][64:96, :], in_=e_d[:, :]), writes=[d_kaug[i]])
        P.op("pool", lambda e, i=i: e.memset(vaug[i][:], 1.0), writes=[d_vaug[i]])
    P.dma("sp", lambda e: e.dma_start(out=vq[:], in_=vq_d[0:1, :].partition_broadcast(128)), writes=[d_c])
    P.dma("sp", lambda e: e.dma_start(out=nb[:], in_=nb_d[0:1, :].partition_broadcast(128)), writes=[d_c])
    P.dma("sp", lambda e: e.dma_start(out=oq[:], in_=oq_d[0:1, :].partition_broadcast(128)), writes=[d_c])
    P.dma("pool", lambda e: e.dma_start(out=tri[:], in_=tri_d[:, :]), writes=[d_c])
    P.op("pool", lambda e: e.memset(mbp[:], 0.0), writes=[d_mbp])

    for ih, h in enumerate(heads):
        b = ih % NB
        P.dma("sp", lambda e, b=b, h=h: e.dma_start(out=kaug[b][0:64, :], in_=kT_d[h * 64:(h + 1) * 64, :]), writes=[d_kaug[b]])
        P.dma("sp", lambda e, b=b, h=h: e.dma_start(out=vaug[b][:, :, 0:64],
                                                    in_=v_d[:, h * 64:(h + 1) * 64].rearrange("(t p) d -> p t d", p=128)),
              writes=[d_vaug[b]])
        P.dma("sp", lambda e, b=b, h=h: e.dma_start(out=qaug[b][0:64, :], in_=qT_d[h * 64:(h + 1) * 64, :]), writes=[d_qaug[b]])
        P.op("dve", lambda e, b=b: e.tensor_reduce(out=ksum[:], in_=kaug[b][0:64, :].rearrange("p (b k) -> p b k", k=256),
                                                   axis=AX.X, op=ALU.add), reads=[d_kaug[b]], writes=[d_ksum])
        P.op("dve", lambda e: e.tensor_scalar(out=kmean[:], in0=ksum[:], scalar1=1.0 / 256, scalar2=None, op0=ALU.mult),
             reads=[d_ksum], writes=[d_kmean])
        for half in range(2):
            for j in range(16):
                qt = half * 16 + j
                P.op("pe", lambda e, b=b, j=j, qt=qt: e.matmul(pg[:, j * 32:(j + 1) * 32], lhsT=qaug[b][0:64, qt * 128:(qt + 1) * 128],
                                                              rhs=kmean[:, :], start=True, stop=True),
                     reads=[d_qaug[b], d_kmean], writes=[d_pg])
            cs_ = slice(half * 512, (half + 1) * 512)
            P.op("dve", lambda e, cs_=cs_: e.tensor_tensor(out=g1[:], in0=pg[:], in1=vq[:, cs_], op=ALU.mult),
                 reads=[d_pg, d_c], writes=[d_g1])
            P.op("dve", lambda e, cs_=cs_: e.tensor_tensor(out=g1[:], in0=g1[:], in1=nb[:, cs_], op=ALU.add),
                 reads=[d_g1, d_c], writes=[d_g1])
            for j in range(16):
                P.op("dve", lambda e, j=j: e.max(out=top8[:, j, :], in_=g1[:, j * 32:(j + 1) * 32]), reads=[d_g1], writes=[d_top8])
            P.op("dve", lambda e: e.tensor_tensor(out=sel[:].rearrange("p (j b) -> p j b", b=32),
                                                  in0=g1[:].rearrange("p (j b) -> p j b", b=32),
                                                  in1=top8[:, :, 2:3].to_broadcast([128, 16, 32]), op=ALU.is_ge),
                 reads=[d_g1, d_top8], writes=[d_sel])
            P.op("dve", lambda e, cs_=cs_: e.tensor_tensor(out=sel[:], in0=sel[:], in1=vq[:, cs_], op=ALU.mult),
                 reads=[d_sel, d_c], writes=[d_sel])
            P.op("dve", lambda e, cs_=cs_: e.tensor_tensor(out=sel[:], in0=sel[:], in1=oq[:, cs_], op=ALU.add),
                 reads=[d_sel, d_c], writes=[d_sel])
            P.op("dve", lambda e: e.tensor_scalar(out=mbp[:, :, 64:96], in0=sel[:].rearrange("p (j b) -> p j b", b=32),
                                                  scalar1=1.0, scalar2=MB, op0=ALU.subtract, op1=ALU.mult),
                 reads=[d_sel], writes=[d_mbp])
            for grp in range(2):
                for j in range(8):
                    P.op("pe", lambda e, grp=grp, j=j: e.transpose(out=pmT[:, j, :], in_=mbp[:, grp * 8 + j, :], identity=idb[:]),
                         reads=[d_mbp, d_idb], writes=[d_pmT])
                c0 = (half * 16 + grp * 8) * 128
                P.op("act", lambda e, b=b, c0=c0: e.copy(out=qaug[b][64:96, c0:c0 + 1024], in_=pmT[64:96, :, :]),
                     reads=[d_pmT], writes=[d_qmb[b]])
        units = []
        for jb in range(nblk):
            ncommon = 32 + 2 * jb
            for u in range(ncommon // 2):
                units.append((jb, [2 * u, 2 * u + 1], False, u == 0))
            units.append((jb, [32 + 2 * jb, 32 + 2 * jb + 1], True, False))

        def emit_S(u, s, b=b):
            jb, kts, diag, first = u
            for j, kt in enumerate(kts):
                if diag and j == 1:
                    q0, nq, c0 = jb * 256 + 128, 128, 256
                else:
                    q0, nq, c0 = jb * 256, 256, j * 256
                P.op("pe", lambda e, s=s, kt=kt, q0=q0, nq=nq, c0=c0: e.matmul(psS[s][:, c0:c0 + nq], lhsT=kaug[b][0:96, kt * 128:(kt + 1) * 128],
                                                                             rhs=qaug[b][0:96, q0:q0 + nq], start=True, stop=True),
                     reads=[d_kaug[b], d_qaug[b], d_qmb[b]], writes=[d_psS[s]])

        def emit_rest(u, s, b=b, h=h):
            jb, kts, diag, first = u
            ncols = 384 if diag else 512
            P.op("act", lambda e, s=s, ncols=ncols: e.activation(out=pt[s][:, 0:ncols], in_=psS[s][:, 0:ncols], func=AF.Exp),
                 reads=[d_psS[s]], writes=[d_pt[s]])
            if diag:
                P.op("pool", lambda e, s=s: e.tensor_tensor(out=pt[s][:, 0:128], in0=pt[s][:, 0:128], in1=tri[:], op=ALU.mult),
                     reads=[d_pt[s], d_c], writes=[d_pt[s]])
                P.op("pool", lambda e, s=s: e.tensor_tensor(out=pt[s][:, 256:384], in0=pt[s][:, 256:384], in1=tri[:], op=ALU.mult),
                     reads=[d_pt[s], d_c], writes=[d_pt[s]])
            for j, kt in enumerate(kts):
                if diag and j == 1:
                    pv = [(1, 256, True)]
                elif diag:
                    pv = [(0, 0, True), (1, 128, False)]
                else:
                    pv = [(0, j * 256, False), (1, j * 256 + 128, False)]
                st_ = first and j == 0
                for qi, col0, last in pv:
                    P.op("pe", lambda e, s=s, kt=kt, qi=qi, col0=col0, st_=st_, last=last: e.matmul(
                        po[qi][:, :], lhsT=pt[s][:, col0:col0 + 128], rhs=vaug[b][:, kt, :], start=st_, stop=last),
                        reads=[d_pt[s], d_vaug[b]], writes=[d_po[qi]])
            if diag:
                for qi in range(2):
                    qt = jb * 2 + qi
                    P.op("dve", lambda e, qi=qi: e.reciprocal(out=rc[:, qi:qi + 1], in_=po[qi][:, 64:65]), reads=[d_po[qi]], writes=[d_rc[qi]])
                    P.op("dve", lambda e, qi=qi, qt=qt: e.tensor_scalar(out=oa[:, qt, h * 64:(h + 1) * 64], in0=po[qi][:, 0:64],
                                                                       scalar1=rc[:, qi:qi + 1], scalar2=None, op0=ALU.mult),
                         reads=[d_po[qi], d_rc[qi]], writes=[d_oa])
        SK = 2
        n = len(units)
        for i in range(n + SK):
            if i < n:
                emit_S(units[i], i % NPT)
            if i >= SK:
                emit_rest(units[i - SK], (i - SK) % NPT)


def host_consts(half):
    pv = 1.0 if half == 1 else 0.0
    V = np.zeros((32, 32), np.float32); O = np.zeros((32, 32), np.float32)
    for qt in range(32):
        jb = qt // 2
        V[qt, :16] = pv
        V[qt, 16:16 + jb] = 1.0
        O[qt, 16 + jb] = 1.0
    NBm = (V - 1.0) * BIGG
    E = np.zeros((32, NK), np.float32)
    for b in range(32):
        E[b, b * 256:(b + 1) * 256] = 1.0
    tri = (np.arange(128)[:, None] <= np.arange(128)[None, :]).astype(np.float32)
    return dict(vq=V.reshape(1, 1024), nb=NBm.reshape(1, 1024), oq=O.reshape(1, 1024),
                e_d=E.astype(ml_dtypes.bfloat16), tri=tri.astype(ml_dtypes.bfloat16))


def phase_c(nc, P, st, xin, w_in, g_mix, cwl_d, cbl_d, dtb_d, alog_d, dsk_d, ssdn_d, tri2_d, sel_d, tb_d, pflag_d,
            idb, d_idb, os_d, d_osd, chunks=range(32)):
    def sb(name, shape, dt):
        return st.enter_context(nc.sbuf_tensor("c_" + name, shape, dt))

    def ps(name, shape, dt=F32):
        return st.enter_context(nc.psum_tensor(name, shape, dt))
    wz = sb("wz", [128, 8, 1024], BF16); d_w = Dep()
    wx = sb("wx", [128, 8, 2048], BF16)
    wdt = sb("wdt", [128, 8, 16], BF16)
    gm = sb("gmc", [128, D], F32); d_c = Dep()
    cw = sb("cw", [128, 16, 4], F32)
    cb = sb("cb", [128, 16], F32)
    dtb = sb("dtb", [128, 2, 16], F32)
    Aneg = sb("Aneg", [128, 2, 16], F32); d_A = Dep()
    dsk = sb("dsk", [128, 16], F32)
    ssdn = sb("ssdn", [128, D], F32)
    tri2 = sb("tri2", [128, 2, 256], F32)
    onesf = sb("onesf", [128, 128], F32)
    sel = sb("selc", [16, 16, 128], F32)
    tb = sb("tbc", [128, 384], F32)
    pflag = sb("pflag", [128, 1], F32)
    xt = sb("xtc", [128, 2, D], F32); d_xt = Dep()
    junk = sb("junkc", [128, D], BF16); d_junk = Dep()
    ssq = sb("ssqc", [128, 2], F32); d_ssq = Dep()
    rstd = sb("rstdc", [128, 2], F32); d_rstd = Dep()
    hb = sb("hbc", [128, 2, D], BF16); d_hb = Dep()
    hT = sb("hTc", [128, 8, 256], BF16); d_hT = Dep()
    xraw = sb("xraw", [128, 16, 259], F32); d_xraw = Dep(); d_halo = Dep()
    acc = sb("acc", [128, 8, 256], F32); d_acc = [Dep() for _ in range(8)]
    xc = sb("xc", [128, 16, 256], BF16); d_xc = [Dep() for _ in range(16)]
    xtok = sb("xtok", [128, 2, 1024], BF16); d_xtok = Dep()
    btok = sb("btok", [128, 2, 512], BF16); d_btok = Dep()
    dtr = sb("dtr", [128, 2, 16], F32); d_dtr = Dep()
    dt = sb("dt", [128, 2, 16], F32); d_dt = Dep()
    aa = sb("aa", [128, 2, 16], F32); d_aa = Dep()
    acum = sb("acum", [128, 2, 16], F32); d_acum = Dep()
    nacum = sb("nacum", [128, 2, 16], F32); d_nacum = Dep()
    acumT = sb("acumT", [16, 256], F32); d_acumT = Dep()
    tot = sb("tot", [128, 16], F32); d_tot = Dep()
    cdec = sb("cdec", [128, 16], F32); d_cdec = Dep()
    dte = sb("dte", [128, 2, 16], F32); d_dte = Dep()
    eac = sb("eac", [128, 2, 16], F32); d_eac = Dep()
    w2 = sb("w2", [128, 2, 16], F32); d_w2 = Dep()
    xdt = sb("xdt", [128, 2, 1024], BF16); d_xdt = Dep()
    xdd = sb("xdd", [128, 2, 1024], BF16); d_xdd = Dep()
    zs = sb("zs", [128, 2, 1024], BF16); d_zs = Dep()
    cbT = sb("cbT", [128, 4, 384], F32); d_cbT = [Dep() for _ in range(4)]
    arg = [sb("arg%d" % i, [128, 384], F32) for i in range(2)]; d_arg = [Dep(), Dep()]
    Lt = [sb("Lt%d" % i, [128, 384], F32) for i in range(2)]; d_Lt = [Dep(), Dep()]
    Mt = [sb("Mt%d" % i, [128, 384], BF16) for i in range(2)]; d_Mt = [Dep() for _ in range(2)]
    stf = sb("stf", [128, 4, 256], F32); d_stf = Dep()
    stT = sb("stT", [128, 4, 256], BF16); d_stT = Dep()
    yt = [sb("yt%d" % i, [128, 256], F32) for i in range(2)]; d_yt = [Dep(), Dep()]
    y2 = [sb("y2%d" % i, [128, 256], F32) for i in range(2)]; d_y2 = [Dep(), Dep()]
    gss = [sb("gss%d" % i, [128, 1], F32) for i in range(2)]; d_gss = [Dep(), Dep()]
    grs = [sb("grs%d" % i, [128, 1], F32) for i in range(2)]; d_grs = [Dep(), Dep()]
    osb = sb("osb", [128, 2, 1024], BF16); d_osb = Dep()

    B = [ps("bk%d" % i, [128, 512], F32) for i in range(7)]
    d_B = [Dep() for _ in range(7)]
    d_B1h = [Dep(), Dep()]
    d_B5h = [Dep(), Dep()]
    pT = ps("pTc", [128, 8, 128], BF16); d_pT = Dep()

    for k in range(8):
        rows = slice(k * 128, (k + 1) * 128)
        P.dma("pool", lambda e, k=k, rows=rows: e.dma_start(out=wz[:, k, :], in_=w_in[rows, C_Z:C_Z + 1024]), writes=[d_w])
        P.dma("pool", lambda e, k=k, rows=rows: e.dma_start(out=wx[:, k, :], in_=w_in[rows, C_X:C_X + 2048]), writes=[d_w])
        P.dma("pool", lambda e, k=k, rows=rows: e.dma_start(out=wdt[:, k, :], in_=w_in[rows, C_DT:C_DT + 16]), writes=[d_w])
    P.dma("sp", lambda e: e.dma_start(out=gm[:], in_=g_mix[0:1, :].partition_broadcast(128)), writes=[d_c])
    P.dma("sp", lambda e: e.dma_start(out=cw[:], in_=cwl_d[:, :, :]), writes=[d_c])
    P.dma("sp", lambda e: e.dma_start(out=cb[:], in_=cbl_d[:, :]), writes=[d_c])
    for i in range(2):
        P.dma("sp", lambda e, i=i: e.dma_start(out=dtb[:, i, :], in_=dtb_d[0:1, :].partition_broadcast(128)), writes=[d_c])
        P.dma("sp", lambda e, i=i: e.dma_start(out=Aneg[:, i, :], in_=alog_d[0:1, :].partition_broadcast(128)), writes=[d_A])
    P.dma("sp", lambda e: e.dma_start(out=dsk[:], in_=dsk_d[0:1, :].partition_broadcast(128)), writes=[d_c])
    P.dma("sp", lambda e: e.dma_start(out=ssdn[:], in_=ssdn_d[0:1, :].partition_broadcast(128)), writes=[d_c])
    P.dma("sp", lambda e: e.dma_start(out=tri2[:], in_=tri2_d[:, :, :]), writes=[d_c])
    P.dma("sp", lambda e: e.dma_start(out=sel[:], in_=sel_d[:, :, :]), writes=[d_c])
    P.dma("sp", lambda e: e.dma_start(out=tb[:], in_=tb_d[:, :]), writes=[d_c])
    P.dma("sp", lambda e: e.dma_start(out=pflag[:], in_=pflag_d[0:1, :].partition_broadcast(128)), writes=[d_c])
    P.op("pool", lambda e: e.memset(onesf[:], 1.0), writes=[d_c])
    P.op("act", lambda e: e.activation(out=Aneg[:], in_=Aneg[:], func=AF.Exp), reads=[d_A], writes=[d_A])
    P.op("dve", lambda e: e.tensor_scalar(out=Aneg[:], in0=Aneg[:], scalar1=-1.0, scalar2=None, op0=ALU.mult), reads=[d_A], writes=[d_A])
    P.op("pool", lambda e: e.memset(xraw[:], 0.0), writes=[d_xraw, d_halo])
    P.op("pool", lambda e: e.memset(stf[:], 0.0), writes=[d_stf])
    P.op("pool", lambda e: e.memset(stT[:], 0.0), writes=[d_stT])

    h3 = lambda ap: ap.rearrange("p (h d) -> p h d", d=64)

    for c in chunks:
        own = c >= 16
        r0 = c * 256
        P.dma("sp", lambda e, r0=r0: e.dma_start(out=xt[:], in_=xin[r0:r0 + 256, :].rearrange("(t p) d -> p t d", p=128)), writes=[d_xt])
        for it in range(2):
            P.op("act", lambda e, it=it: e.activation(out=junk[:], in_=xt[:, it, :], func=AF.Square, accum_out=ssq[:, it:it + 1]),
                 reads=[d_xt], writes=[d_junk, d_ssq])
        P.op("act", lambda e: e.activation(out=rstd[:], in_=ssq[:], func=AF.Sqrt, bias=EPS, scale=1.0 / D), reads=[d_ssq], writes=[d_rstd])
        P.op("dve", lambda e: e.reciprocal(out=rstd[:], in_=rstd[:]), reads=[d_rstd], writes=[d_rstd])
        for it in range(2):
            P.op("dve", lambda e, it=it: e.scalar_tensor_tensor(out=hb[:, it, :], in0=xt[:, it, :], scalar=rstd[:, it:it + 1], in1=gm[:],
                                                                                    op0=ALU.mult, op1=ALU.mult),
                 reads=[d_xt, d_rstd, d_c], writes=[d_hb])
        for it in range(2):
            for k in range(8):
                P.op("pe", lambda e, it=it, k=k: e.transpose(out=pT[:, k, :], in_=hb[:, it, k * 128:(k + 1) * 128], identity=idb[:]),
                     reads=[d_hb, d_idb], writes=[d_pT])
            P.op("act", lambda e, it=it: e.copy(out=hT[:, :, it * 128:(it + 1) * 128], in_=pT[:]), reads=[d_pT], writes=[d_hT])
        for it in range(2):
            for k in range(8):
                P.op("pe", lambda e, it=it, k=k: e.matmul(B[3][:, it * 16:(it + 1) * 16], lhsT=hT[:, k, it * 128:(it + 1) * 128], rhs=wdt[:, k, :],
                                                         start=(k == 0), stop=(k == 7)), reads=[d_hT, d_w], writes=[d_B[3]])
        P.op("dve", lambda e: e.tensor_tensor(out=dtr[:].rearrange("p a b -> p (a b)"), in0=B[3][:, 0:32], in1=dtb[:].rearrange("p a b -> p (a b)"), op=ALU.add),
             reads=[d_B[3], d_c], writes=[d_dtr])
        P.op("act", lambda e: e.activation(out=dtr[:], in_=dtr[:], func=AF.Exp), reads=[d_dtr], writes=[d_dtr])
        P.op("act", lambda e: e.activation(out=dt[:], in_=dtr[:], func=AF.Ln, bias=1.0), reads=[d_dtr], writes=[d_dt])
        P.op("dve", lambda e: e.tensor_tensor(out=aa[:], in0=dt[:], in1=Aneg[:], op=ALU.mult), reads=[d_dt, d_A], writes=[d_aa])
        for it in range(2):
            for jt in range(it + 1):
                P.op("pe", lambda e, it=it, jt=jt: e.matmul(B[3][:, 32 + it * 16:32 + (it + 1) * 16], lhsT=tri2[:, jt, it * 128:(it + 1) * 128],
                                                           rhs=aa[:, jt, :], start=(jt == 0), stop=(jt == it)),
                     reads=[d_aa, d_c], writes=[d_B[3]])
        for jt in range(2):
            P.op("pe", lambda e, jt=jt: e.matmul(B[3][:, 64:80], lhsT=onesf[:], rhs=aa[:, jt, :], start=(jt == 0), stop=(jt == 1)),
                 reads=[d_aa, d_c], writes=[d_B[3]])
        for jt in range(2):
            P.op("pe", lambda e, jt=jt: e.matmul(B[3][0:16, 128:384], lhsT=aa[:, jt, :], rhs=tri2[:, jt, :], start=(jt == 0), stop=(jt == 1)),
                 reads=[d_aa, d_c], writes=[d_B[3]])
        P.op("act", lambda e: e.copy(out=acum[:].rearrange("p a b -> p (a b)"), in_=B[3][:, 32:64]), reads=[d_B[3]], writes=[d_acum])
        P.op("dve", lambda e: e.tensor_scalar(out=nacum[:].rearrange("p a b -> p (a b)"), in0=B[3][:, 32:64], scalar1=-1.0, scalar2=None, op0=ALU.mult),
             reads=[d_B[3]], writes=[d_nacum])
        P.op("act", lambda e: e.copy(out=tot[:], in_=B[3][:, 64:80]), reads=[d_B[3]], writes=[d_tot])
        P.op("act", lambda e: e.copy(out=acumT[:], in_=B[3][0:16, 128:384]), reads=[d_B[3]], writes=[d_acumT])
        P.op("act", lambda e: e.activation(out=cdec[:], in_=tot[:], func=AF.Exp), reads=[d_tot], writes=[d_cdec])
        P.op("dve", lambda e: e.tensor_tensor(out=dte[:], in0=nacum[:], in1=tot[:].unsqueeze(1).to_broadcast([128, 2, 16]), op=ALU.add),
             reads=[d_nacum, d_tot], writes=[d_dte])
        P.op("act", lambda e: e.activation(out=dte[:], in_=dte[:], func=AF.Exp), reads=[d_dte], writes=[d_dte])
        P.op("act", lambda e: e.activation(out=eac[:], in_=acum[:], func=AF.Exp), reads=[d_acum], writes=[d_eac])
        P.op("dve", lambda e: e.tensor_tensor(out=w2[:], in0=dt[:], in1=dte[:], op=ALU.mult), reads=[d_dt, d_dte], writes=[d_w2])
        for fc in range(16):
            bi = 1 if fc % 2 == 0 else 4
            for k in range(8):
                P.op("pe", lambda e, fc=fc, k=k, bi=bi: e.matmul(B[bi][:, 0:256], lhsT=wx[:, k, fc * 128:(fc + 1) * 128], rhs=hT[:, k, :],
                                                               start=(k == 0), stop=(k == 7)), reads=[d_hT, d_w], writes=[d_B[bi]])
            P.op("act", lambda e, fc=fc, bi=bi: e.copy(out=xraw[:, fc, 3:259], in_=B[bi][:, 0:256]),
                 reads=[d_B[bi]], writes=[d_xraw])
        for half in range(2):
            for f8 in range(8):
                fc = half * 8 + f8
                P.op("dve", lambda e, fc=fc, f8=f8: e.tensor_scalar(out=acc[:, f8, :], in0=xraw[:, fc, 0:256], scalar1=cw[:, fc, 0:1], scalar2=cb[:, fc:fc + 1],
                                                                   op0=ALU.mult, op1=ALU.add), reads=[d_xraw, d_halo, d_c], writes=[d_acc[f8]])
            for kk in range(1, 4):
                for f8 in range(8):
                    fc = half * 8 + f8
                    P.op("dve", lambda e, fc=fc, f8=f8, kk=kk: e.scalar_tensor_tensor(out=acc[:, f8, :], in0=xraw[:, fc, kk:kk + 256], scalar=cw[:, fc, kk:kk + 1],
                                                                                     in1=acc[:, f8, :], op0=ALU.mult, op1=ALU.add),
                         reads=[d_xraw, d_halo, d_c, d_acc[f8]], writes=[d_acc[f8]])
            for f8 in range(8):
                fc = half * 8 + f8
                P.op("act", lambda e, fc=fc, f8=f8: e.activation(out=xc[:, fc, :], in_=acc[:, f8, :], func=AF.Silu),
                     reads=[d_acc[f8]], writes=[d_xc[fc]])
        P.op("pool", lambda e: e.tensor_copy(out=xraw[:, :, 0:3], in_=xraw[:, :, 256:259]), reads=[d_xraw], writes=[d_halo])
        for it in range(2):
            for fc in range(8):
                P.op("pe", lambda e, it=it, fc=fc: e.transpose(out=pT[:, fc, :], in_=xc[:, fc, it * 128:(it + 1) * 128], identity=idb[:]),
                     reads=[d_xc[fc], d_idb], writes=[d_pT])
            P.op("act", lambda e, it=it: e.copy(out=xtok[:, it, :], in_=pT[:]), reads=[d_pT], writes=[d_xtok])
            for g in range(4):
                P.op("pe", lambda e, it=it, g=g: e.transpose(out=pT[:, g, :], in_=xc[:, 8 + g, it * 128:(it + 1) * 128], identity=idb[:]),
                     reads=[d_xc[8 + g], d_idb], writes=[d_pT])
            P.op("act", lambda e, it=it: e.copy(out=btok[:, it, :], in_=pT[:, 0:4, :]), reads=[d_pT], writes=[d_btok])
        for it in range(2):
            if own:
                P.op("dve", lambda e, it=it: e.tensor_tensor(out=h3(xdt[:, it, :]), in0=h3(xtok[:, it, :]),
                                                             in1=dt[:, it, :].unsqueeze(2).to_broadcast([128, 16, 64]), op=ALU.mult),
                     reads=[d_xtok, d_dt], writes=[d_xdt])
            P.op("pool", lambda e, it=it: e.tensor_tensor(out=h3(xdd[:, it, :]), in0=h3(xtok[:, it, :]),
                                                          in1=w2[:, it, :].unsqueeze(2).to_broadcast([128, 16, 64]), op=ALU.mult),
                 reads=[d_xtok, d_w2], writes=[d_xdd])
        if own:
            for it in range(2):
                for hv in range(2):
                    zb = 2 if hv == 0 else 6
                    for k in range(8):
                        P.op("pe", lambda e, it=it, hv=hv, k=k, zb=zb: e.matmul(B[zb][:, :], lhsT=hT[:, k, it * 128:(it + 1) * 128], rhs=wz[:, k, hv * 512:(hv + 1) * 512],
                                                                              start=(k == 0), stop=(k == 7)), reads=[d_hT, d_w], writes=[d_B[zb]])
                    P.op("act", lambda e, it=it, hv=hv, zb=zb: e.activation(out=zs[:, it, hv * 512:(hv + 1) * 512], in_=B[zb][:, :], func=AF.Silu),
                         reads=[d_B[zb]], writes=[d_zs])
            for g in range(4):
                P.op("pe", lambda e, g=g: e.matmul(B[4][:, 0:256], lhsT=xc[:, 8 + g, 0:128], rhs=xc[:, 12 + g, :], start=True, stop=True),
                     reads=[d_xc[8 + g], d_xc[12 + g]], writes=[d_B[4]])
                P.op("pe", lambda e, g=g: e.matmul(B[4][:, 256:384], lhsT=xc[:, 8 + g, 128:256], rhs=xc[:, 12 + g, 128:256], start=True, stop=True),
                     reads=[d_xc[8 + g], d_xc[12 + g]], writes=[d_B[4]])
                P.op("act", lambda e, g=g: e.copy(out=cbT[:, g, :], in_=B[4][:, 0:384]), reads=[d_B[4]], writes=[d_cbT[g]])
            for g in range(4):
                def stA(h, g=g):
                    mi = h % 2
                    bk = 5 if mi == 0 else 4
                    P.op("pe", lambda e: e.matmul(B[bk][:, 0:256], lhsT=sel[:, h, :], rhs=acumT[:, :], start=True, stop=True),
                         reads=[d_c, d_acumT], writes=[d_B[bk]])
                    P.op("dve", lambda e: e.scalar_tensor_tensor(out=arg[mi][:, 0:256], in0=B[bk][:, 0:256], scalar=nacum[:, 0, h:h + 1], in1=tb[:, 0:256],
                                                                 op0=ALU.add, op1=ALU.add), reads=[d_B[bk], d_nacum, d_c], writes=[d_arg[mi]])
                    P.op("dve", lambda e: e.scalar_tensor_tensor(out=arg[mi][:, 256:384], in0=B[bk][:, 128:256], scalar=nacum[:, 1, h:h + 1], in1=tb[:, 256:384],
                                                                 op0=ALU.add, op1=ALU.add), reads=[d_B[bk], d_nacum, d_c], writes=[d_arg[mi]])
                    P.op("act", lambda e: e.activation(out=Lt[mi][:], in_=arg[mi][:], func=AF.Exp), reads=[d_arg[mi]], writes=[d_Lt[mi]])
                    P.op("pool", lambda e: e.tensor_tensor(out=Mt[mi][:], in0=Lt[mi][:], in1=cbT[:, g, :], op=ALU.mult),
                         reads=[d_Lt[mi], d_cbT[g]], writes=[d_Mt[mi]])

                def stB(h, g=g):
                    mi = h % 2
                    r = h % 4
                    cs_ = slice(r * 64, (r + 1) * 64)
                    hs_ = slice(h * 64, (h + 1) * 64)
                    P.op("pe", lambda e: e.matmul(B[6][:, 0:256][:, cs_], lhsT=Mt[mi][:, 0:128], rhs=xdt[:, 0, hs_], start=True, stop=True),
                         reads=[d_Mt[mi], d_xdt], writes=[d_B[6]])
                    P.op("pe", lambda e: e.matmul(B[6][:, 256:512][:, cs_], lhsT=Mt[mi][:, 128:256], rhs=xdt[:, 0, hs_], start=True, stop=False),
                         reads=[d_Mt[mi], d_xdt], writes=[d_B[6]])
                    P.op("pe", lambda e: e.matmul(B[6][:, 256:512][:, cs_], lhsT=Mt[mi][:, 256:384], rhs=xdt[:, 1, hs_], start=False, stop=True),
                         reads=[d_Mt[mi], d_xdt], writes=[d_B[6]])
                hs4 = [g * 4 + r for r in range(4)]
                stA(hs4[0]); stA(hs4[1]); stB(hs4[0]); stA(hs4[2]); stB(hs4[1]); stA(hs4[3]); stB(hs4[2]); stB(hs4[3])
                gs_ = slice(g * 256, (g + 1) * 256)
                for it in range(2):
                    P.op("pe", lambda e, g=g, it=it: e.matmul(B[0][:, it * 256:(it + 1) * 256], lhsT=xc[:, 12 + g, it * 128:(it + 1) * 128], rhs=stT[:, g, :],
                                                             start=True, stop=True), reads=[d_xc[12 + g], d_stT], writes=[d_B[0]])
                v4 = lambda ap: ap.rearrange("p (r d) -> p r d", d=64)

                def ystep(step, it, g=g, gs_=gs_):
                    if step == 0:
                        P.op("dve", lambda e: e.tensor_tensor(out=v4(yt[it][:]), in0=v4(B[0][:, it * 256:(it + 1) * 256]),
                                                              in1=eac[:, it, g * 4:(g + 1) * 4].unsqueeze(2).to_broadcast([128, 4, 64]), op=ALU.mult),
                             reads=[d_B[0], d_eac], writes=[d_yt[it]])
                    elif step == 1:
                        P.op("dve", lambda e: e.tensor_tensor(out=yt[it][:], in0=yt[it][:], in1=B[6][:, it * 256:(it + 1) * 256], op=ALU.add),
                             reads=[d_yt[it], d_B[6]], writes=[d_yt[it]])
                    elif step == 2:
                        P.op("pool", lambda e: e.tensor_tensor(out=v4(y2[it][:]), in0=v4(xtok[:, it, gs_]),
                                                               in1=dsk[:, g * 4:(g + 1) * 4].unsqueeze(2).to_broadcast([128, 4, 64]), op=ALU.mult),
                             reads=[d_xtok, d_c], writes=[d_y2[it]])
                    elif step == 3:
                        P.op("dve", lambda e: e.tensor_tensor(out=yt[it][:], in0=yt[it][:], in1=y2[it][:], op=ALU.add), reads=[d_yt[it], d_y2[it]], writes=[d_yt[it]])
                    elif step == 4:
                        P.op("dve", lambda e: e.tensor_tensor(out=yt[it][:], in0=yt[it][:], in1=zs[:, it, gs_], op=ALU.mult), reads=[d_yt[it], d_zs], writes=[d_yt[it]])
                    elif step == 5:
                        P.op("act", lambda e: e.activation(out=y2[it][:], in_=yt[it][:], func=AF.Square, accum_out=gss[it][:]), reads=[d_yt[it], d_y2[it]], writes=[d_y2[it], d_gss[it]])
                    elif step == 6:
                        P.op("act", lambda e: e.activation(out=grs[it][:], in_=gss[it][:], func=AF.Sqrt, bias=EPS, scale=1.0 / 256), reads=[d_gss[it]], writes=[d_grs[it]])
                    elif step == 7:
                        P.op("dve", lambda e: e.reciprocal(out=grs[it][:], in_=grs[it][:]), reads=[d_grs[it]], writes=[d_grs[it]])
                    elif step == 8:
                        P.op("dve", lambda e: e.scalar_tensor_tensor(out=osb[:, it, gs_], in0=yt[it][:], scalar=grs[it][:, 0:1], in1=ssdn[:, gs_],
                                                                     op0=ALU.mult, op1=ALU.mult), reads=[d_yt[it], d_grs[it], d_c], writes=[d_osb])
                for step in range(9):
                    for it in range(2):
                        ystep(step, it)
            P.dma("sp", lambda e, r0=r0: e.dma_start(out=os_d[r0 - TP:r0 - TP + 256, :].rearrange("(t p) c -> p t c", p=128), in_=osb[:]),
                  reads=[d_osb], writes=[d_osd])
        for g in range(4):
            bank = B[2] if g < 2 else B[1]
            dbank = d_B[2] if g < 2 else d_B[1]
            cs_ = slice((g % 2) * 256, (g % 2 + 1) * 256)
            for jt in range(2):
                P.op("pe", lambda e, g=g, jt=jt, bank=bank, cs_=cs_: e.matmul(bank[:, cs_], lhsT=btok[:, jt, g * 128:(g + 1) * 128], rhs=xdd[:, jt, g * 256:(g + 1) * 256],
                                                                            start=(jt == 0), stop=(jt == 1)), reads=[d_btok, d_xdd], writes=[dbank])
        v16 = lambda ap: ap.rearrange("p (h d) -> p h d", d=64)
        P.op("dve", lambda e: e.tensor_tensor(out=v16(stf[:].rearrange("p g c -> p (g c)")), in0=v16(stf[:].rearrange("p g c -> p (g c)")),
                                              in1=cdec[:].unsqueeze(2).to_broadcast([128, 16, 64]), op=ALU.mult), reads=[d_stf, d_cdec], writes=[d_stf])
        P.op("dve", lambda e: e.tensor_tensor(out=stf[:, 0:2, :].rearrange("p g c -> p (g c)"), in0=stf[:, 0:2, :].rearrange("p g c -> p (g c)"), in1=B[2][:, :], op=ALU.add),
             reads=[d_stf, d_B[2]], writes=[d_stf])
        P.op("dve", lambda e: e.tensor_tensor(out=stf[:, 2:4, :].rearrange("p g c -> p (g c)"), in0=stf[:, 2:4, :].rearrange("p g c -> p (g c)"), in1=B[1][:, :], op=ALU.add),
             reads=[d_stf, d_B[1]], writes=[d_stf])
        if c == 15:
            P.op("dve", lambda e: e.tensor_scalar(out=stf[:], in0=stf[:], scalar1=pflag[:, 0:1], scalar2=None, op0=ALU.mult), reads=[d_stf, d_c], writes=[d_stf])
        P.op("act", lambda e: e.copy(out=stT[:], in_=stf[:]), reads=[d_stf], writes=[d_stT])


def host_consts_c(z, half):
    cwl = np.ascontiguousarray(z['conv_w'].reshape(4, 16, 128).transpose(2, 1, 0)).astype(np.float32)
    cbl = np.ascontiguousarray(z['conv_b'].reshape(16, 128).T).astype(np.float32)
    tri = (np.arange(128)[:, None] <= np.arange(128)[None, :]).astype(np.float32)
    tri2 = np.zeros((128, 2, 256), np.float32)
    tri2[:, 0, 0:128] = tri; tri2[:, 0, 128:256] = 1.0; tri2[:, 1, 128:256] = tri
    sel = np.zeros((16, 16, 128), np.float32)
    for h in range(16):
        sel[h, h, :] = 1.0
    tbias = np.where(tri > 0, 0.0, NEG).astype(np.float32)
    tb = np.zeros((128, 384), np.float32)
    tb[:, 0:128] = tbias; tb[:, 256:384] = tbias
    return dict(cwl=cwl, cbl=cbl, dtb=z['dt_bias'][None, :], alog=z['a_log'][None, :], dsk=z['d_skip'][None, :], ssdn=z['ssd_norm'][None, :],
                tri2=tri2, sel_d=sel, tb_d=tb, pflag=np.array([[1.0 if half == 1 else 0.0]], np.float32))


def phase_def(nc, P, xown, w_in, g_mix, mem_d, gmem_d, wmemkv, mqn_d, mkn_d, woa_d, wos_d, wom_d, wout_d, gffn_d, wr_d,
              wgate_d, wup_d, wdown_d, tri_d, ecap_d, oa_d, d_oad, os_d, d_osd, x1_d, xbuf, ybuf, out_d, idb, d_idb, idf, d_idf,
              ntiles=32, experts=range(32), stage=3):
    d_x1d = Dep(); d_xbuf = Dep(); d_ybuf = Dep(); d_out = Dep()
    with contextlib.ExitStack() as stp:
        def sbp(name, shape, dt):
            return stp.enter_context(nc.sbuf_tensor("d_" + name, shape, dt))
        combs = sbp("combs", [128, 32, 2], F32); d_combs = Dep()
        idxs = sbp("idxs", [128, 32, 2], I32); d_idxs = Dep()

        with contextlib.ExitStack() as st:
            def sb(name, shape, dt):
                return st.enter_context(nc.sbuf_tensor("d_" + name, shape, dt))

            def ps(name, shape, dt=F32):
                return st.enter_context(nc.psum_tensor(name, shape, dt))
            d_w = Dep(); d_c = Dep()
            gm = sb("gmd", [128, D], F32)
            gf = sb("gfd", [128, D], F32)
            mqn = sb("mqn", [128, 512], F32)
            mkn = sb("mkn", [128, 512], F32)
            tri = sb("trid", [128, 128], F32)
            onesf = sb("onesfd", [128, 128], F32)
            onesb = sb("onesbd", [128, 128], BF16)
            ecap = sb("ecap", [128, 32], F32)
            base = sb("base", [128, 32], F32); d_base = Dep()
            kmT = sb("kmT", [128, 4, 256], BF16); d_kmT = Dep()
            vm = sb("vm", [128, 2, 512], BF16); d_vm = Dep()

            xt = [sb("xtd%d" % i, [128, D], F32) for i in range(2)]; d_xt = [Dep(), Dep()]
            junk = sb("junkd", [128, D], BF16); d_junk = Dep()
            ssq = sb("ssqd", [128, 1], F32); d_ssq = Dep()
            rstd = sb("rstdd", [128, 1], F32); d_rstd = Dep()
            hb = sb("hbd", [128, D], BF16); d_hb = Dep()
            hT = sb("hTd", [128, 8, 128], BF16); d_hT = Dep()
            sq = sb("sqd", [128, 512], F32); d_sq = Dep()
            hs = sb("hsd", [128, 4], F32); d_hs = Dep()
            hr = sb("hrd", [128, 4], F32); d_hr = Dep()
            qmn = sb("qmn", [128, 512], F32); d_qmn = Dep()
            qmb = sb("qmb", [128, 512], BF16); d_qmb = Dep()
            qmT = sb("qmT", [128, 4, 128], BF16); d_qmT = Dep()
            pm = sb("pm", [128, 4, 2, 128], BF16); d_pm = Dep()
            rcp = sb("rcp", [128, 512], F32); d_rcp = Dep()
            omT = sb("omT", [128, 4, 128], BF16); d_omT = Dep()
            gs = sb("gs", [128, 3072], F32); d_gs = Dep()
            oat = [sb("oat%d" % i, [128, 512], BF16) for i in range(2)]; d_oat = [Dep(), Dep()]
            ost = [sb("ost%d" % i, [128, 1024], BF16) for i in range(2)]; d_ost = [Dep(), Dep()]
            oaT = sb("oaT", [128, 4, 128], BF16); d_oaT = Dep()
            osT = sb("osT", [128, 8, 128], BF16); d_osT = Dep()
            mg = sb("mg", [128, 512], F32); d_mg = Dep()
            tt = sb("tt", [128, 512], F32); d_tt = Dep()
            mgb = sb("mgb", [128, D], BF16); d_mgb = Dep()
            mgT = sb("mgT", [128, 8, 128], BF16); d_mgT = Dep()
            x1 = sb("x1", [128, D], F32); d_x1 = Dep()
            h2f = sb("h2f", [128, D], F32); d_h2f = Dep()
            h2b = [sb("h2b%d" % i, [128, D], BF16) for i in range(2)]; d_h2b = [Dep() for _ in range(2)]
            h2T = sb("h2T", [128, 8, 128], F32); d_h2T = Dep()
            lg = sb("lg", [128, 36], F32); d_lg = Dep()
            sm = sb("sm", [128, 64], F32); d_sm = Dep()
            goh = sb("goh", [128, 4], F32); d_goh = Dep()
            lsel = sb("lsel", [128, 32], F32); d_lsel = Dep()
            les = sb("les", [128, 8], F32); d_les = Dep()
            t8 = sb("t8", [128, 8], F32); d_t8 = Dep()
            oh = sb("oh", [128, 64], F32); d_oh = Dep()
            ohs = sb("ohs", [128, 32], F32); d_ohs = Dep()
            posf = sb("posf", [128, 32], F32); d_posf = Dep()
            idf2 = sb("idf2", [128, 2], F32); d_idf2 = Dep()

            pT = ps("pTd", [128, 8, 128], BF16); d_pT = Dep()
            pF = ps("pFd", [128, 512], F32); d_pF = Dep()
            pG = [ps("pGd%d" % i, [128, 512], F32) for i in range(3)]; d_pG = [Dep() for _ in range(3)]
            pms = [ps("pmsd%d" % i, [128, 512], F32) for i in range(2)]; d_pms = [Dep() for _ in range(2)]
            pmo = ps("pmod", [128, 512], F32); d_pmo = Dep()

            P.dma("sp", lambda e: e.dma_start(out=gm[:], in_=g_mix[0:1, :].partition_broadcast(128)), writes=[d_c])
            P.dma("sp", lambda e: e.dma_start(out=gf[:], in_=gffn_d[0:1, :].partition_broadcast(128)), writes=[d_c])
            P.dma("sp", lambda e: e.dma_start(out=mqn[:], in_=mqn_d[0:1, :].partition_broadcast(128)), writes=[d_c])
            P.dma("sp", lambda e: e.dma_start(out=mkn[:], in_=mkn_d[0:1, :].partition_broadcast(128)), writes=[d_c])
            P.dma("sp", lambda e: e.dma_start(out=tri[:], in_=tri_d[:, :]), writes=[d_c])
            P.dma("sp", lambda e: e.dma_start(out=ecap[:], in_=ecap_d[0:1, :].partition_broadcast(128)), writes=[d_c])
            P.op("pool", lambda e: e.memset(onesf[:], 1.0), writes=[d_c])
            P.op("pool", lambda e: e.memset(onesb[:], 1.0), writes=[d_c])
            P.op("pool", lambda e: e.memset(base[:], 0.0), writes=[d_base])

            def head_norm(src_ps, d_src, gain, nh, dst, d_dst):
                hd = 512 // nh
                v = lambda ap: ap.rearrange("p (h d) -> p h d", h=nh)
                P.op("act", lambda e: e.activation(out=sq[:], in_=src_ps, func=AF.Square), reads=[d_src], writes=[d_sq])
                P.op("dve", lambda e: e.tensor_reduce(out=hs[:, 0:nh], in_=v(sq[:]), axis=AX.X, op=ALU.add), reads=[d_sq], writes=[d_hs])
                P.op("act", lambda e: e.activation(out=hr[:, 0:nh], in_=hs[:, 0:nh], func=AF.Sqrt, bias=EPS, scale=1.0 / hd), reads=[d_hs], writes=[d_hr])
                P.op("dve", lambda e: e.reciprocal(out=hr[:, 0:nh], in_=hr[:, 0:nh]), reads=[d_hr], writes=[d_hr])
                P.op("dve", lambda e: e.tensor_tensor(out=v(qmn[:]), in0=v(src_ps), in1=hr[:, 0:nh].unsqueeze(2).to_broadcast([128, nh, hd]), op=ALU.mult),
                     reads=[d_src, d_hr], writes=[d_qmn])
                P.op("pool", lambda e: e.tensor_tensor(out=dst, in0=qmn[:], in1=gain[:], op=ALU.mult), reads=[d_qmn, d_c], writes=[d_dst])

            with contextlib.ExitStack() as stm:
                wkv = stm.enter_context(nc.sbuf_tensor("s_wkv", [128, 8, 1024], BF16)); d_wkv = Dep()
                mt_ = stm.enter_context(nc.sbuf_tensor("s_memt", [128, 2, D], F32)); d_mt = Dep()
                gme = stm.enter_context(nc.sbuf_tensor("s_gme", [128, D], F32)); d_gme = Dep()
                mhb = stm.enter_context(nc.sbuf_tensor("s_mhb", [128, 2, D], BF16)); d_mhb = Dep()
                mT = stm.enter_context(nc.sbuf_tensor("s_mT", [128, 8, 256], BF16)); d_mT = Dep()
                ss2 = stm.enter_context(nc.sbuf_tensor("s_ss2", [128, 2], F32)); d_ss2 = Dep()
                for k in range(8):
                    P.dma("pool", lambda e, k=k: e.dma_start(out=wkv[:, k, :], in_=wmemkv[k * 128:(k + 1) * 128, :]), writes=[d_wkv])
                P.dma("sp", lambda e: e.dma_start(out=mt_[:], in_=mem_d[:, :].rearrange("(t p) d -> p t d", p=128)), writes=[d_mt])
                P.dma("sp", lambda e: e.dma_start(out=gme[:], in_=gmem_d[0:1, :].partition_broadcast(128)), writes=[d_gme])
                for it in range(2):
                    P.op("act", lambda e, it=it: e.activation(out=junk[:], in_=mt_[:, it, :], func=AF.Square, accum_out=ss2[:, it:it + 1]),
                         reads=[d_mt], writes=[d_junk, d_ss2])
                P.op("act", lambda e: e.activation(out=ss2[:], in_=ss2[:], func=AF.Sqrt, bias=EPS, scale=1.0 / D), reads=[d_ss2], writes=[d_ss2])
                P.op("dve", lambda e: e.reciprocal(out=ss2[:], in_=ss2[:]), reads=[d_ss2], writes=[d_ss2])
                for it in range(2):
                    P.op("dve", lambda e, it=it: e.scalar_tensor_tensor(out=mhb[:, it, :], in0=mt_[:, it, :], scalar=ss2[:, it:it + 1], in1=gme[:],
                                                                        op0=ALU.mult, op1=ALU.mult), reads=[d_mt, d_ss2, d_gme], writes=[d_mhb])
                    for k in range(8):
                        P.op("pe", lambda e, it=it, k=k: e.transpose(out=pT[:, k, :], in_=mhb[:, it, k * 128:(k + 1) * 128], identity=idb[:]),
                             reads=[d_mhb, d_idb], writes=[d_pT])
                    P.op("act", lambda e, it=it: e.copy(out=mT[:, :, it * 128:(it + 1) * 128], in_=pT[:]), reads=[d_pT], writes=[d_mT])
                for it in range(2):
                    for hv in range(2):
                        for k in range(8):
                            P.op("pe", lambda e, it=it, hv=hv, k=k: e.matmul(pG[hv][:, :], lhsT=mT[:, k, it * 128:(it + 1) * 128], rhs=wkv[:, k, hv * 512:(hv + 1) * 512],
                                                                           start=(k == 0), stop=(k == 7)), reads=[d_mT, d_wkv], writes=[d_pG[hv]])
                    head_norm(pG[0][:, :], d_pG[0], mkn, 4, qmb[:], d_qmb)
                    for h in range(4):
                        P.op("pe", lambda e, h=h: e.transpose(out=pT[:, h, :], in_=qmb[:, h * 128:(h + 1) * 128], identity=idb[:]),
                             reads=[d_qmb, d_idb], writes=[d_pT])
                    P.op("act", lambda e, it=it: e.copy(out=kmT[:, :, it * 128:(it + 1) * 128], in_=pT[:, 0:4, :]), reads=[d_pT], writes=[d_kmT])
                    P.op("act", lambda e, it=it: e.copy(out=vm[:, it, :], in_=pG[1][:, :]), reads=[d_pG[1]], writes=[d_vm])

            wqm = sb("wqm", [128, 8, 512], BF16)
            wg = sb("wg", [128, 8, 3072], BF16)
            woa = sb("woa", [128, 4, 1024], BF16)
            wos = sb("wos", [128, 8, 1024], BF16)
            wom = sb("wom", [128, 4, 1024], BF16)
            wout = sb("wout", [128, 8, 1024], BF16)
            wr = sb("wr", [128, 8, 36], F32)
            for k in range(8):
                rows = slice(k * 128, (k + 1) * 128)
                P.dma("pool", lambda e, k=k, rows=rows: e.dma_start(out=wqm[:, k, :], in_=w_in[rows, C_QM:C_QM + 512]), writes=[d_w])
                for j in range(2):
                    P.dma("pool", lambda e, k=k, rows=rows, j=j: e.dma_start(out=wg[:, k, j * 1536:(j + 1) * 1536], in_=w_in[rows, C_G + j * 1536:C_G + (j + 1) * 1536]), writes=[d_w])
                P.dma("pool", lambda e, k=k, rows=rows: e.dma_start(out=wos[:, k, :], in_=wos_d[rows, :]), writes=[d_w])
                P.dma("pool", lambda e, k=k, rows=rows: e.dma_start(out=wout[:, k, :], in_=wout_d[rows, :]), writes=[d_w])
                P.dma("sp", lambda e, k=k, rows=rows: e.dma_start(out=wr[:, k, :], in_=wr_d[rows, :]), writes=[d_w])
            for k in range(4):
                rows = slice(k * 128, (k + 1) * 128)
                P.dma("pool", lambda e, k=k, rows=rows: e.dma_start(out=woa[:, k, :], in_=woa_d[rows, :]), writes=[d_w])
                P.dma("pool", lambda e, k=k, rows=rows: e.dma_start(out=wom[:, k, :], in_=wom_d[rows, :]), writes=[d_w])
            def loads(tj):
                xj = tj % 2
                rj = tj * 128
                P.dma("sp", lambda e: e.dma_start(out=xt[xj][:], in_=xown[rj:rj + 128, :]), writes=[d_xt[xj]])
                P.dma("sp", lambda e: e.dma_start(out=oat[xj][:], in_=oa_d[rj:rj + 128, :]), reads=[d_oad], writes=[d_oat[xj]])
                P.dma("sp", lambda e: e.dma_start(out=ost[xj][:], in_=os_d[rj:rj + 128, :]), reads=[d_osd], writes=[d_ost[xj]])

            for ti in range(ntiles):
                r0 = ti * 128
                hbuf = ti % 2
                xb = ti % 2
                if ti == 0:
                    loads(0)
                if ti + 1 < ntiles:
                    loads(ti + 1)
                P.op("act", lambda e, xb=xb: e.activation(out=junk[:], in_=xt[xb][:], func=AF.Square, accum_out=ssq[:]), reads=[d_xt[xb]], writes=[d_junk, d_ssq])
                P.op("act", lambda e: e.activation(out=rstd[:], in_=ssq[:], func=AF.Sqrt, bias=EPS, scale=1.0 / D), reads=[d_ssq], writes=[d_rstd])
                P.op("dve", lambda e: e.reciprocal(out=rstd[:], in_=rstd[:]), reads=[d_rstd], writes=[d_rstd])
                P.op("dve", lambda e, xb=xb: e.scalar_tensor_tensor(out=hb[:], in0=xt[xb][:], scalar=rstd[:, 0:1], in1=gm[:], op0=ALU.mult, op1=ALU.mult),
                     reads=[d_xt[xb], d_rstd, d_c], writes=[d_hb])
                for k in range(8):
                    P.op("pe", lambda e, k=k: e.transpose(out=pT[:, k, :], in_=hb[:, k * 128:(k + 1) * 128], identity=idb[:]), reads=[d_hb, d_idb], writes=[d_pT])
                P.op("act", lambda e: e.copy(out=hT[:], in_=pT[:]), reads=[d_pT], writes=[d_hT])
                for k in range(8):
                    P.op("pe", lambda e, k=k: e.matmul(pG[0][:, :], lhsT=hT[:, k, :], rhs=wqm[:, k, :], start=(k == 0), stop=(k == 7)),
                         reads=[d_hT, d_w], writes=[d_pG[0]])
                head_norm(pG[0][:, :], d_pG[0], mqn, 4, qmb[:], d_qmb)
                for g6 in range(6):
                    bk = g6 % 3
                    for k in range(8):
                        P.op("pe", lambda e, g6=g6, k=k, bk=bk: e.matmul(pG[bk][:, :], lhsT=hT[:, k, :], rhs=wg[:, k, g6 * 512:(g6 + 1) * 512], start=(k == 0), stop=(k == 7)),
                             reads=[d_hT, d_w], writes=[d_pG[bk]])
                    P.op("act", lambda e, g6=g6, bk=bk: e.activation(out=gs[:, g6 * 512:(g6 + 1) * 512], in_=pG[bk][:, :], func=AF.Sigmoid),
                         reads=[d_pG[bk]], writes=[d_gs])
                for h in range(4):
                    P.op("pe", lambda e, h=h: e.transpose(out=pT[:, h, :], in_=qmb[:, h * 128:(h + 1) * 128], identity=idb[:]), reads=[d_qmb, d_idb], writes=[d_pT])
                P.op("act", lambda e: e.copy(out=qmT[:], in_=pT[:, 0:4, :]), reads=[d_pT], writes=[d_qmT])
                for h in range(4):
                    for mt in range(2):
                        bank = pms[h // 2]
                        c0 = ((h % 2) * 2 + mt) * 128
                        P.op("pe", lambda e, h=h, mt=mt, bank=bank, c0=c0: e.matmul(bank[:, c0:c0 + 128], lhsT=kmT[:, h, mt * 128:(mt + 1) * 128], rhs=qmT[:, h, :],
                                                                                  start=True, stop=True), reads=[d_kmT, d_qmT], writes=[d_pms[h // 2]])
                for hp in range(2):
                    P.op("act", lambda e, hp=hp: e.activation(out=pm[:, hp * 2:(hp + 1) * 2, :, :].rearrange("p a b c -> p (a b c)"), in_=pms[hp][:, :], func=AF.Exp,
                                                              scale=float(128 ** -0.5)), reads=[d_pms[hp]], writes=[d_pm])
                for h in range(4):
                    for mt in range(2):
                        P.op("pe", lambda e, h=h, mt=mt: e.matmul(pmo[:, h * 128:(h + 1) * 128], lhsT=vm[:, mt, h * 128:(h + 1) * 128], rhs=pm[:, h, mt, :],
                                                                 start=(mt == 0), stop=(mt == 1)), reads=[d_vm, d_pm], writes=[d_pmo])
                for mt in range(2):
                    P.op("pe", lambda e, mt=mt: e.matmul(pG[1][:, :].rearrange("p (h t) -> p h t", h=4), lhsT=onesb[:], rhs=pm[:, :, mt, :],
                                                        start=(mt == 0), stop=(mt == 1)), reads=[d_c, d_pm], writes=[d_pG[1]])
                P.op("dve", lambda e: e.reciprocal(out=rcp[:], in_=pG[1][:, :]), reads=[d_pG[1]], writes=[d_rcp])
                P.op("dve", lambda e: e.tensor_tensor(out=omT[:].rearrange("p h t -> p (h t)"), in0=pmo[:, :], in1=rcp[:], op=ALU.mult),
                     reads=[d_pmo, d_rcp], writes=[d_omT])
                for k in range(4):
                    P.op("pe", lambda e, xb=xb, k=k: e.transpose(out=pT[:, k, :], in_=oat[xb][:, k * 128:(k + 1) * 128], identity=idb[:]), reads=[d_oat[xb], d_idb], writes=[d_pT])
                P.op("act", lambda e: e.copy(out=oaT[:], in_=pT[:, 0:4, :]), reads=[d_pT], writes=[d_oaT])
                for k in range(8):
                    P.op("pe", lambda e, xb=xb, k=k: e.transpose(out=pT[:, k, :], in_=ost[xb][:, k * 128:(k + 1) * 128], identity=idb[:]), reads=[d_ost[xb], d_idb], writes=[d_pT])
                P.op("act", lambda e: e.copy(out=osT[:], in_=pT[:]), reads=[d_pT], writes=[d_osT])
                for hv in range(2):
                    cs_ = slice(hv * 512, (hv + 1) * 512)
                    for k in range(4):
                        P.op("pe", lambda e, k=k, cs_=cs_: e.matmul(pG[0][:, :], lhsT=oaT[:, k, :], rhs=woa[:, k, cs_], start=(k == 0), stop=(k == 3)),
                             reads=[d_oaT, d_w], writes=[d_pG[0]])
                    for k in range(8):
                        P.op("pe", lambda e, k=k, cs_=cs_: e.matmul(pG[1][:, :], lhsT=osT[:, k, :], rhs=wos[:, k, cs_], start=(k == 0), stop=(k == 7)),
                             reads=[d_osT, d_w], writes=[d_pG[1]])
                    for k in range(4):
                        P.op("pe", lambda e, k=k, cs_=cs_: e.matmul(pG[2][:, :], lhsT=omT[:, k, :], rhs=wom[:, k, cs_], start=(k == 0), stop=(k == 3)),
                             reads=[d_omT, d_w], writes=[d_pG[2]])
                    P.op("dve", lambda e, hv=hv: e.tensor_tensor(out=mg[:], in0=pG[0][:, :], in1=gs[:, hv * 512:(hv + 1) * 512], op=ALU.mult),
                         reads=[d_pG[0], d_gs], writes=[d_mg])
                    P.op("dve", lambda e, hv=hv: e.tensor_tensor(out=tt[:], in0=pG[1][:, :], in1=gs[:, 1024 + hv * 512:1024 + (hv + 1) * 512], op=ALU.mult),
                         reads=[d_pG[1], d_gs], writes=[d_tt])
                    P.op("pool", lambda e: e.tensor_tensor(out=mg[:], in0=mg[:], in1=tt[:], op=ALU.add), reads=[d_mg, d_tt], writes=[d_mg])
                    P.op("dve", lambda e, hv=hv: e.tensor_tensor(out=tt[:], in0=pG[2][:, :], in1=gs[:, 2048 + hv * 512:2048 + (hv + 1) * 512], op=ALU.mult),
                         reads=[d_pG[2], d_gs], writes=[d_tt])
                    P.op("pool", lambda e, cs_=cs_: e.tensor_tensor(out=mgb[:, cs_], in0=mg[:], in1=tt[:], op=ALU.add), reads=[d_mg, d_tt], writes=[d_mgb])
                for k in range(8):
                    P.op("pe", lambda e, k=k: e.transpose(out=pT[:, k, :], in_=mgb[:, k * 128:(k + 1) * 128], identity=idb[:]), reads=[d_mgb, d_idb], writes=[d_pT])
                P.op("act", lambda e: e.copy(out=mgT[:], in_=pT[:]), reads=[d_pT], writes=[d_mgT])
                for hv in range(2):
                    cs_ = slice(hv * 512, (hv + 1) * 512)
                    for k in range(8):
                        P.op("pe", lambda e, k=k, cs_=cs_, hv=hv: e.matmul(pG[hv][:, :], lhsT=mgT[:, k, :], rhs=wout[:, k, cs_], start=(k == 0), stop=(k == 7)),
                             reads=[d_mgT, d_w], writes=[d_pG[hv]])
                    P.op("dve", lambda e, xb=xb, cs_=cs_, hv=hv: e.tensor_tensor(out=x1[:, cs_], in0=pG[hv][:, :], in1=xt[xb][:, cs_], op=ALU.add),
                         reads=[d_pG[hv], d_xt[xb]], writes=[d_x1])
                P.dma("sp", lambda e, r0=r0: e.dma_start(out=x1_d[r0:r0 + 128, :], in_=x1[:]), reads=[d_x1], writes=[d_x1d])
                if stage < 2:
                    continue
                P.op("act", lambda e: e.activation(out=junk[:], in_=x1[:], func=AF.Square, accum_out=ssq[:]), reads=[d_x1], writes=[d_junk, d_ssq])
                P.op("act", lambda e: e.activation(out=rstd[:], in_=ssq[:], func=AF.Sqrt, bias=EPS, scale=1.0 / D), reads=[d_ssq], writes=[d_rstd])
                P.op("dve", lambda e: e.reciprocal(out=rstd[:], in_=rstd[:]), reads=[d_rstd], writes=[d_rstd])
                P.op("dve", lambda e: e.scalar_tensor_tensor(out=h2f[:], in0=x1[:], scalar=rstd[:, 0:1], in1=gf[:], op0=ALU.mult, op1=ALU.mult),
                     reads=[d_x1, d_rstd, d_c], writes=[d_h2f])
                P.op("pool", lambda e, hbuf=hbuf: e.tensor_copy(out=h2b[hbuf][:], in_=h2f[:]), reads=[d_h2f], writes=[d_h2b[hbuf]])
                for half in range(2):
                    for k4 in range(4):
                        k = half * 4 + k4
                        P.op("pe", lambda e, k=k, k4=k4: e.transpose(out=pF[:, k4 * 128:(k4 + 1) * 128], in_=h2f[:, k * 128:(k + 1) * 128], identity=idf[:]),
                             reads=[d_h2f, d_idf], writes=[d_pF])
                    P.op("act", lambda e, half=half: e.copy(out=h2T[:, half * 4:(half + 1) * 4, :].rearrange("p a b -> p (a b)"), in_=pF[:, :]), reads=[d_pF], writes=[d_h2T])
                for k in range(8):
                    P.op("pe", lambda e, k=k: e.matmul(pF[:, 0:36], lhsT=h2T[:, k, :], rhs=wr[:, k, :], start=(k == 0), stop=(k == 7)), reads=[d_h2T, d_w], writes=[d_pF])
                P.op("act", lambda e: e.copy(out=lg[:], in_=pF[:, 0:36]), reads=[d_pF], writes=[d_lg])
                P.op("dve", lambda e: e.tensor_reduce(out=sm[:, 0:1], in_=lg[:, 0:4], axis=AX.X, op=ALU.max), reads=[d_lg], writes=[d_sm])
                P.op("dve", lambda e: e.tensor_scalar(out=sm[:, 1:2], in0=sm[:, 0:1], scalar1=-1.0, scalar2=None, op0=ALU.mult), reads=[d_sm], writes=[d_sm])
                P.op("act", lambda e: e.activation(out=sm[:, 8:12], in_=lg[:, 0:4], func=AF.Exp, bias=sm[:, 1:2], accum_out=sm[:, 2:3]), reads=[d_lg, d_sm], writes=[d_sm])
                P.op("dve", lambda e: e.reciprocal(out=sm[:, 3:4], in_=sm[:, 2:3]), reads=[d_sm], writes=[d_sm])
                P.op("dve", lambda e: e.tensor_scalar(out=goh[:], in0=lg[:, 0:4], scalar1=sm[:, 0:1], scalar2=None, op0=ALU.is_ge), reads=[d_lg, d_sm], writes=[d_goh])
                P.op("dve", lambda e: e.tensor_tensor(out=lsel[:].rearrange("p (g e) -> p g e", g=4), in0=lg[:, 4:36].rearrange("p (g e) -> p g e", g=4),
                                                      in1=goh[:].unsqueeze(2).to_broadcast([128, 4, 8]), op=ALU.mult), reads=[d_lg, d_goh], writes=[d_lsel])
                P.op("dve", lambda e: e.tensor_reduce(out=les[:], in_=lsel[:].rearrange("p (g e) -> p e g", g=4), axis=AX.X, op=ALU.add), reads=[d_lsel], writes=[d_les])
                P.op("dve", lambda e: e.max(out=t8[:], in_=les[:]), reads=[d_les], writes=[d_t8])
                P.op("dve", lambda e: e.tensor_tensor(out=sm[:, 4:5], in0=t8[:, 1:2], in1=t8[:, 0:1], op=ALU.subtract), reads=[d_t8, d_sm], writes=[d_sm])
                P.op("act", lambda e: e.activation(out=sm[:, 5:6], in_=sm[:, 4:5], func=AF.Exp), reads=[d_sm], writes=[d_sm])
                P.op("dve", lambda e: e.tensor_scalar(out=sm[:, 6:7], in0=sm[:, 5:6], scalar1=1.0, scalar2=None, op0=ALU.add), reads=[d_sm], writes=[d_sm])
                P.op("dve", lambda e: e.reciprocal(out=sm[:, 6:7], in_=sm[:, 6:7]), reads=[d_sm], writes=[d_sm])
                P.op("dve", lambda e: e.tensor_tensor(out=sm[:, 7:8], in0=sm[:, 5:6], in1=sm[:, 6:7], op=ALU.mult), reads=[d_sm], writes=[d_sm])
                P.op("dve", lambda e, ti=ti: e.tensor_scalar(out=combs[:, ti, :], in0=sm[:, 6:8], scalar1=sm[:, 3:4], scalar2=None, op0=ALU.mult), reads=[d_sm], writes=[d_combs])
                for j in range(2):
                    P.op("dve", lambda e, j=j: e.tensor_scalar(out=oh[:, j * 32:(j + 1) * 32], in0=lg[:, 4:36], scalar1=t8[:, j:j + 1], scalar2=None, op0=ALU.is_equal),
                         reads=[d_lg, d_t8], writes=[d_oh])
                    P.op("dve", lambda e, j=j: e.tensor_tensor(out=oh[:, j * 32:(j + 1) * 32].rearrange("p (g e) -> p g e", g=4), in0=oh[:, j * 32:(j + 1) * 32].rearrange("p (g e) -> p g e", g=4),
                                                               in1=goh[:].unsqueeze(2).to_broadcast([128, 4, 8]), op=ALU.mult), reads=[d_oh, d_goh], writes=[d_oh])
                P.op("dve", lambda e: e.tensor_tensor(out=ohs[:], in0=oh[:, 0:32], in1=oh[:, 32:64], op=ALU.add), reads=[d_oh], writes=[d_ohs])
                P.op("pe", lambda e: e.matmul(pF[:, 64:96], lhsT=tri[:], rhs=ohs[:], start=True, stop=True), reads=[d_c, d_ohs], writes=[d_pF])
                P.op("pe", lambda e: e.matmul(pF[:, 128:160], lhsT=onesf[:], rhs=ohs[:], start=True, stop=True), reads=[d_c, d_ohs], writes=[d_pF])
                P.op("dve", lambda e: e.tensor_tensor(out=posf[:], in0=pF[:, 64:96], in1=ohs[:], op=ALU.subtract), reads=[d_pF, d_ohs], writes=[d_posf])
                P.op("dve", lambda e: e.tensor_tensor(out=posf[:], in0=posf[:], in1=base[:], op=ALU.add), reads=[d_posf, d_base], writes=[d_posf])
                P.op("dve", lambda e: e.tensor_tensor(out=posf[:], in0=posf[:], in1=ecap[:], op=ALU.add), reads=[d_posf, d_c], writes=[d_posf])
                P.op("dve", lambda e: e.tensor_tensor(out=base[:], in0=base[:], in1=pF[:, 128:160], op=ALU.add), reads=[d_base, d_pF, d_posf], writes=[d_base])
                for j in range(2):
                    P.op("dve", lambda e, j=j: e.tensor_tensor(out=oh[:, j * 32:(j + 1) * 32], in0=oh[:, j * 32:(j + 1) * 32], in1=posf[:], op=ALU.mult), reads=[d_oh, d_posf], writes=[d_oh])
                    P.op("dve", lambda e, j=j: e.tensor_reduce(out=idf2[:, j:j + 1], in_=oh[:, j * 32:(j + 1) * 32], axis=AX.X, op=ALU.add), reads=[d_oh], writes=[d_idf2])
                P.op("dve", lambda e, ti=ti: e.tensor_copy(out=idxs[:, ti, :], in_=idf2[:]), reads=[d_idf2], writes=[d_idxs])
                for j in range(2):
                    P.dma("pool", lambda e, ti=ti, j=j, hbuf=hbuf: e.indirect_dma_start(out=xbuf[:, :], out_offset=bass.IndirectOffsetOnAxis(ap=idxs[:, ti, j:j + 1], axis=0),
                                                                                      in_=h2b[hbuf][:], in_offset=None),
                          reads=[d_h2b[hbuf], d_idxs], writes=[d_xbuf])

        if stage < 3:
            return
        if hasattr(P, 'barrier'):
            P.barrier()
        with contextlib.ExitStack() as st:
            def sb(name, shape, dt):
                return st.enter_context(nc.sbuf_tensor("d_" + name, shape, dt))

            def ps(name, shape, dt=F32):
                return st.enter_context(nc.psum_tensor(name, shape, dt))
            NW = 2
            wge = [sb("wge%d" % i, [128, 8, 512], BF16) for i in range(NW)]; d_wge = [Dep() for _ in range(NW)]
            wue = [sb("wue%d" % i, [128, 8, 512], BF16) for i in range(NW)]; d_wue = [Dep() for _ in range(NW)]
            wde = [sb("wde%d" % i, [128, 4, 1024], BF16) for i in range(NW)]; d_wde = [Dep() for _ in range(NW)]
            xe = [sb("xe%d" % i, [128, 3, D], BF16) for i in range(NW)]; d_xe = [Dep() for _ in range(NW)]
            xeT = sb("xeT", [128, 8, CAP], BF16); d_xeT = Dep()
            sg = sb("sg", [128, CAP], F32); d_sg = Dep()
            hTe = sb("hTe", [128, 4, CAP], BF16); d_hTe = Dep()
            ye = [sb("ye%d" % i, [128, 3, D], F32) for i in range(NW)]; d_ye = [Dep() for _ in range(NW)]
            pT = ps("pTe", [128, 8, 128], BF16); d_pT = Dep()
            pg_ = [ps("pge%d" % i, [128, 512], F32) for i in range(2)]; d_pg = [Dep() for _ in range(2)]
            pu_ = [ps("pue%d" % i, [128, 512], F32) for i in range(2)]; d_pu = [Dep() for _ in range(2)]
            py_ = [ps("pye%d" % i, [128, 512], F32) for i in range(2)]; d_py = [Dep() for _ in range(2)]
            for ie, ex in enumerate(experts):
                b = ie % NW
                P.dma("pool", lambda e, b=b, ex=ex: e.dma_start(out=wge[b][:], in_=wgate_d[ex].rearrange("(k p) f -> p k f", p=128)), writes=[d_wge[b]])
                P.dma("pool", lambda e, b=b, ex=ex: e.dma_start(out=wue[b][:], in_=wup_d[ex].rearrange("(k p) f -> p k f", p=128)), writes=[d_wue[b]])
                P.dma("pool", lambda e, b=b, ex=ex: e.dma_start(out=wde[b][:], in_=wdown_d[ex].rearrange("(k p) f -> p k f", p=128)), writes=[d_wde[b]])
                P.dma("sp", lambda e, b=b, ex=ex: e.dma_start(out=xe[b][:], in_=xbuf[ex * CAP:(ex + 1) * CAP, :].rearrange("(s p) d -> p s d", p=128)),
                      reads=[d_xbuf], writes=[d_xe[b]])
                for s in range(3):
                    for k in range(8):
                        P.op("pe", lambda e, b=b, s=s, k=k: e.transpose(out=pT[:, k, :], in_=xe[b][:, s, k * 128:(k + 1) * 128], identity=idb[:]),
                             reads=[d_xe[b], d_idb], writes=[d_pT])
                    P.op("act", lambda e, s=s: e.copy(out=xeT[:, :, s * 128:(s + 1) * 128], in_=pT[:]), reads=[d_pT], writes=[d_xeT])
                for ft in range(4):
                    pb = ft % 2
                    for k in range(8):
                        P.op("pe", lambda e, b=b, ft=ft, k=k, pb=pb: e.matmul(pg_[pb][:, 0:CAP], lhsT=wge[b][:, k, ft * 128:(ft + 1) * 128], rhs=xeT[:, k, :],
                                                                            start=(k == 0), stop=(k == 7)), reads=[d_wge[b], d_xeT], writes=[d_pg[pb]])
                    for k in range(8):
                        P.op("pe", lambda e, b=b, ft=ft, k=k, pb=pb: e.matmul(pu_[pb][:, 0:CAP], lhsT=wue[b][:, k, ft * 128:(ft + 1) * 128], rhs=xeT[:, k, :],
                                                                            start=(k == 0), stop=(k == 7)), reads=[d_wue[b], d_xeT], writes=[d_pu[pb]])
                    P.op("act", lambda e, pb=pb: e.activation(out=sg[:], in_=pg_[pb][:, 0:CAP], func=AF.Silu), reads=[d_pg[pb]], writes=[d_sg])
                    P.op("dve", lambda e, ft=ft, pb=pb: e.tensor_tensor(out=hTe[:, ft, :], in0=sg[:], in1=pu_[pb][:, 0:CAP], op=ALU.mult),
                         reads=[d_sg, d_pu[pb]], writes=[d_hTe])
                for s in range(3):
                    for hv in range(2):
                        for ft in range(4):
                            P.op("pe", lambda e, b=b, s=s, hv=hv, ft=ft: e.matmul(py_[hv][:, :], lhsT=hTe[:, ft, s * 128:(s + 1) * 128], rhs=wde[b][:, ft, hv * 512:(hv + 1) * 512],
                                                                                start=(ft == 0), stop=(ft == 3)), reads=[d_hTe, d_wde[b]], writes=[d_py[hv]])
                        P.op("act" if hv == 0 else "dve", (lambda e, b=b, s=s, hv=hv: e.copy(out=ye[b][:, s, hv * 512:(hv + 1) * 512], in_=py_[hv][:, :])) if hv == 0 else
                             (lambda e, b=b, s=s, hv=hv: e.tensor_copy(out=ye[b][:, s, hv * 512:(hv + 1) * 512], in_=py_[hv][:, :])),
                             reads=[d_py[hv]], writes=[d_ye[b]])
                P.dma("sp", lambda e, b=b, ex=ex: e.dma_start(out=ybuf[ex * CAP:(ex + 1) * CAP, :].rearrange("(s p) d -> p s d", p=128), in_=ye[b][:]),
                      reads=[d_ye[b]], writes=[d_ybuf])

        if hasattr(P, 'barrier'):
            P.barrier()
        with contextlib.ExitStack() as st:
            def sb(name, shape, dt):
                return st.enter_context(nc.sbuf_tensor("d_" + name, shape, dt))
            NF = 2
            x1t = [sb("x1t%d" % i, [128, D], F32) for i in range(NF)]; d_x1t = [Dep() for _ in range(NF)]
            y1 = [sb("y1t%d" % i, [128, D], F32) for i in range(NF)]; d_y1 = [Dep() for _ in range(NF)]
            y2 = [sb("y2t%d" % i, [128, D], F32) for i in range(NF)]; d_y2 = [Dep() for _ in range(NF)]
            ot = [sb("ot%d" % i, [128, D], F32) for i in range(NF)]; d_ot = [Dep() for _ in range(NF)]
            for ti in range(ntiles):
                b = ti % NF
                r0 = ti * 128
                P.dma("sp", lambda e, b=b, r0=r0: e.dma_start(out=x1t[b][:], in_=x1_d[r0:r0 + 128, :]), reads=[d_x1d], writes=[d_x1t[b]])
                P.dma("pool", lambda e, b=b, ti=ti: e.indirect_dma_start(out=y1[b][:], out_offset=None, in_=ybuf[:, :],
                                                                        in_offset=bass.IndirectOffsetOnAxis(ap=idxs[:, ti, 0:1], axis=0)),
                      reads=[d_ybuf, d_idxs], writes=[d_y1[b]])
                P.dma("pool", lambda e, b=b, ti=ti: e.indirect_dma_start(out=y2[b][:], out_offset=None, in_=ybuf[:, :],
                                                                        in_offset=bass.IndirectOffsetOnAxis(ap=idxs[:, ti, 1:2], axis=0)),
                      reads=[d_ybuf, d_idxs], writes=[d_y2[b]])
                P.op("dve", lambda e, b=b, ti=ti: e.scalar_tensor_tensor(out=ot[b][:], in0=y1[b][:], scalar=combs[:, ti, 0:1], in1=x1t[b][:], op0=ALU.mult, op1=ALU.add),
                     reads=[d_y1[b], d_combs, d_x1t[b]], writes=[d_ot[b]])
                P.op("dve", lambda e, b=b, ti=ti: e.scalar_tensor_tensor(out=ot[b][:], in0=y2[b][:], scalar=combs[:, ti, 1:2], in1=ot[b][:], op0=ALU.mult, op1=ALU.add),
                     reads=[d_y2[b], d_combs, d_ot[b]], writes=[d_ot[b]])
                P.dma("sp", lambda e, b=b, r0=r0: e.dma_start(out=out_d[r0:r0 + 128, :], in_=ot[b][:]), reads=[d_ot[b]], writes=[d_out])


def host_consts_d(z):
    tri = (np.arange(128)[:, None] <= np.arange(128)[None, :]).astype(np.float32)
    return dict(mqn=np.tile(z['mem_q_norm'], 4)[None, :], mkn=np.tile(z['mem_k_norm'], 4)[None, :],
                w_r=np.ascontiguousarray(np.concatenate([z['w_router_group'], z['w_router_expert']], axis=1)),
                trif=tri, ecap=(np.arange(32, dtype=np.float32) * CAP)[None, :], ident=np.eye(128, dtype=np.float32))


def build_full():
    nc = bass.Bass("TRN2", target_bir_lowering=False)
    P = Prog(nc)

    def di(name, shape, dt=F32):
        return nc.dram_tensor(name, shape, dt, kind="ExternalInput")
    xin = di("xin", [TP + T, D]); w_in = di("w_in", [D, IN_COLS]); g_mix = di("g_mix", [1, D])
    qkn = di("qkn", [2, 512]); cs = di("cs", [TP + T, 64]); ident_d = di("ident", [128, 128])
    e_d = di("e_d", [32, NK], BF16); vq_d = di("vq", [1, 1024]); nb_d = di("nb", [1, 1024]); oq_d = di("oq", [1, 1024])
    tri_d = di("tri", [128, 128], BF16)
    cwl_d = di("cwl", [128, 16, 4]); cbl_d = di("cbl", [128, 16]); dtb_d = di("dtb", [1, 16]); alog_d = di("alog", [1, 16])
    dsk_d = di("dsk", [1, 16]); ssdn_d = di("ssdn", [1, D]); tri2_d = di("tri2", [128, 2, 256]); sel_d = di("sel_d", [16, 16, 128])
    tb_d = di("tb_d", [128, 384]); pflag_d = di("pflag", [1, 1])
    mem_d = di("mem", [256, D]); gmem_d = di("g_mem", [1, D]); wmemkv = di("w_mem_kv", [D, 1024])
    mqn_d = di("mqn", [1, 512]); mkn_d = di("mkn", [1, 512])
    woa_d = di("w_o_moba", [512, D]); wos_d = di("w_o_ssd", [D, D]); wom_d = di("w_o_mem", [512, D]); wout_d = di("w_out", [D, D])
    gffn_d = di("g_ffn", [1, D]); wr_d = di("w_r", [D, 36])
    wgate_d = di("w_gate", [NE, D, 512]); wup_d = di("w_up", [NE, D, 512]); wdown_d = di("w_down", [NE, 512, D])
    trif_d = di("trif", [128, 128]); ecap_d = di("ecap", [1, 32])
    out_d = nc.dram_tensor("out", [T, D], F32, kind="ExternalOutput")
    kT_d = nc.dram_tensor("kT_d", [512, NK], BF16)
    v_d = nc.dram_tensor("v_d", [NK, 512], BF16)
    qT_d = nc.dram_tensor("qT_d", [512, T], BF16)
    oa_d = nc.dram_tensor("oa_d", [T, 512], BF16)
    os_d = nc.dram_tensor("os_d", [T, D], BF16)
    x1_d = nc.dram_tensor("x1_d", [T, D], F32)
    xbuf = nc.dram_tensor("xbuf", [NE * CAP, D], BF16)
    ybuf = nc.dram_tensor("ybuf", [NE * CAP, D], F32)

    with contextlib.ExitStack() as st0:
        idf = st0.enter_context(nc.sbuf_tensor("idf", [128, 128], F32)); d_idf = Dep()
        idb = st0.enter_context(nc.sbuf_tensor("idb", [128, 128], BF16)); d_idb = Dep()
        P.dma("sp", lambda e: e.dma_start(out=idf[:], in_=ident_d[:, :]), writes=[d_idf])
        P.op("dve", lambda e: e.tensor_copy(out=idb[:], in_=idf[:]), reads=[d_idf], writes=[d_idb])
        with contextlib.ExitStack() as st:
            phase_a(nc, P, st, xin, w_in, g_mix, qkn, cs, idb, d_idb, kT_d, v_d, qT_d)
        P.barrier()
        with contextlib.ExitStack() as st:
            oa = st.enter_context(nc.sbuf_tensor("s_oa", [128, 32, 512], BF16)); d_oa = Dep()
            phase_b(nc, P, st, kT_d, v_d, qT_d, e_d, vq_d, nb_d, oq_d, tri_d, idb, d_idb, oa, d_oa)
            P.dma("sp", lambda e: e.dma_start(out=oa_d[:, :].rearrange("(t p) c -> p t c", p=128), in_=oa[:]), reads=[d_oa])
        P.barrier()
        with contextlib.ExitStack() as st:
            phase_c(nc, P, st, xin, w_in, g_mix, cwl_d, cbl_d, dtb_d, alog_d, dsk_d, ssdn_d, tri2_d, sel_d, tb_d, pflag_d,
                    idb, d_idb, os_d, Dep())
        P.barrier()
        phase_def(nc, P, xin[TP:TP + T, :], w_in, g_mix, mem_d, gmem_d, wmemkv, mqn_d, mkn_d, woa_d, wos_d, wom_d, wout_d, gffn_d, wr_d,
                  wgate_d, wup_d, wdown_d, trif_d, ecap_d, oa_d, Dep(), os_d, Dep(), x1_d, xbuf, ybuf, out_d, idb, d_idb, idf, d_idf)
        P.emit()
    return nc


_NC_CACHE = {}


def kernel(x, mem, g_mix, w_in, moba_q_norm, moba_k_norm, conv_w, conv_b, dt_bias, a_log, d_skip, ssd_norm, g_mem, w_mem_kv,
           mem_q_norm, mem_k_norm, w_o_moba, w_o_ssd, w_o_mem, w_out, g_ffn, w_router_group, w_router_expert, w_gate, w_up, w_down):
    f32 = np.float32
    A = lambda a: np.ascontiguousarray(np.asarray(a, dtype=f32))
    x = A(x); mem = A(mem)
    z = dict(conv_w=A(conv_w), conv_b=A(conv_b), dt_bias=A(dt_bias), a_log=A(a_log), d_skip=A(d_skip), ssd_norm=A(ssd_norm),
             mem_q_norm=A(mem_q_norm), mem_k_norm=A(mem_k_norm), w_router_group=A(w_router_group), w_router_expert=A(w_router_expert))
    if "nc" not in _NC_CACHE:
        _NC_CACHE["nc"] = build_full()
    nc = _NC_CACHE["nc"]
    pos = np.arange(2 * T, dtype=f32)
    inv = (10000.0 ** (-np.arange(32, dtype=f32) / 32)).astype(f32)
    ang = pos[:, None] * inv[None, :]
    cs_full = np.concatenate([np.cos(ang), np.sin(ang)], axis=1).astype(f32)
    qkn = np.stack([np.tile(A(moba_q_norm), 8), np.tile(A(moba_k_norm), 8)]).astype(f32)
    shared = dict(w_in=A(w_in), g_mix=A(g_mix)[None, :], qkn=qkn, ident=np.eye(128, dtype=f32),
                  g_mem=A(g_mem)[None, :], w_mem_kv=A(w_mem_kv), w_o_moba=A(w_o_moba), w_o_ssd=A(w_o_ssd), w_o_mem=A(w_o_mem),
                  w_out=A(w_out), g_ffn=A(g_ffn)[None, :], w_gate=A(w_gate), w_up=A(w_up), w_down=A(w_down))
    shared.update(host_consts_d(z))
    in_maps = []
    for c in range(8):
        bb, hh = c // 2, c % 2
        if hh == 0:
            xin = np.concatenate([np.zeros((TP, D), f32), x[bb, :T]], 0)
            csc = np.concatenate([cs_full[:TP], cs_full[:T]], 0)
        else:
            xin = x[bb]
            csc = cs_full
        m = dict(shared)
        m.update(xin=np.ascontiguousarray(xin), cs=np.ascontiguousarray(csc), mem=mem[bb])
        m.update(host_consts(hh))
        m.update(host_consts_c(z, hh))
        in_maps.append(m)
    res = run_bass_kernel_spmd(nc, in_maps, core_ids=list(range(8)))
    out = np.empty((4, 2 * T, D), f32)
    for c in range(8):
        bb, hh = c // 2, c % 2
        out[bb, hh * T:(hh + 1) * T] = np.asarray(res.results[c]["out"], dtype=f32)
    return out
```

```python
import contextlib
import numpy as np
import ml_dtypes
import concourse.bass as bass
import concourse.mybir as mybir
from concourse.bass_utils import run_bass_kernel_spmd

F32 = mybir.dt.float32
BF16 = mybir.dt.bfloat16
I32 = mybir.dt.int32
U32 = mybir.dt.uint32
AF = mybir.ActivationFunctionType
ALU = mybir.AluOpType
AX = mybir.AxisListType

T = 4096
TP = 4096
NK = TP + T
D = 1024
EPS = 1e-6
IN_COLS = 8208
BIGG = 30000.0
MB = 1000.0
NEG = -30000.0
C_Z, C_X, C_DT = 1536, 2560, 4608
C_QM, C_G = 4624, 5136
CAP = 384
NE = 32


class Dep:
    __slots__ = ("w", "r")

    def __init__(self):
        self.w = None
        self.r = {}


class Op:
    __slots__ = ("eng", "fn", "deps", "is_dma", "sig", "sigval", "dsem", "dval", "prev")

    def __init__(self, eng, fn, is_dma):
        self.eng = eng
        self.fn = fn
        self.is_dma = is_dma
        self.deps = []
        self.sig = False
        self.sigval = 0
        self.dsem = None
        self.dval = 0
        self.prev = None


class Prog:
    ENGS = ("pe", "act", "dve", "pool", "sp")

    def __init__(self, nc, ndma_sems=12):
        self.nc = nc
        self.ops = {e: [] for e in self.ENGS}
        self.ndma = {e: 0 for e in self.ENGS}
        self.dma_last = {}
        self.ndma_sems = ndma_sems
        self.all_dma = []

    def _add(self, o, reads, writes):
        deps = {}
        for t in reads:
            if t.w is not None:
                deps[id(t.w)] = t.w
        for t in writes:
            if t.w is not None:
                deps[id(t.w)] = t.w
            for r in t.r.values():
                deps[id(r)] = r
        for t in reads:
            key = id(o) if o.is_dma else o.eng
            t.r[key] = o
        for t in writes:
            t.w = o
            t.r = {}
        dl = []
        for d in deps.values():
            if d is o:
                continue
            if (not d.is_dma) and (not o.is_dma) and d.eng == "pe" and o.eng == "pe":
                continue
            dl.append(d)
            if not d.is_dma:
                d.sig = True
        o.deps = dl
        self.ops[o.eng].append(o)
        return o

    def op(self, eng, fn, reads=(), writes=()):
        return self._add(Op(eng, fn, False), reads, writes)

    def dma(self, eng, fn, reads=(), writes=()):
        o = Op(eng, fn, True)
        n = self.ndma[eng]
        self.ndma[eng] += 1
        slot = (eng, n % self.ndma_sems)
        o.dsem = slot
        o.prev = self.dma_last.get(slot)
        o.dval = (o.prev.dval if o.prev else 0) + 16
        self.dma_last[slot] = o
        self.all_dma.append(o)
        return self._add(o, reads, writes)


    def barrier(self):
        lasts = []
        for e in self.ENGS:
            for o in reversed(self.ops[e]):
                if (not o.is_dma) and o.fn is not None:
                    o.sig = True
                    lasts.append(o)
                    break
        dmas = list(self.dma_last.values())
        for e in self.ENGS:
            o = Op(e, None, False)
            o.deps = [d for d in lasts if d.eng != e] + dmas
            self.ops[e].append(o)

    def emit(self):
        nc = self.nc
        import contextlib
        with contextlib.ExitStack() as st:
            esem = {e: st.enter_context(nc.semaphore("S_" + e)) for e in self.ENGS}
            dsem = {}
            for e in self.ENGS:
                if self.ndma[e]:
                    for i in range(min(self.ndma_sems, self.ndma[e])):
                        dsem[(e, i)] = st.enter_context(nc.semaphore("D_%s_%d" % (e, i)))
            for e in self.ENGS:
                c = 0
                for o in self.ops[e]:
                    if (not o.is_dma) and o.sig and o.fn is not None:
                        c += 1
                        o.sigval = c
            block = st.enter_context(nc.Block())
            handles = {"pe": block.tensor, "act": block.scalar, "dve": block.vector,
                       "pool": block.gpsimd, "sp": block.sync}

            def make(e):
                def body(eng):
                    known = {}

                    def wait(sem, key, val):
                        if known.get(key, 0) < val:
                            eng.wait_ge(sem, val)
                            known[key] = val
                    for o in self.ops[e]:
                        for d in o.deps:
                            if d.is_dma:
                                wait(dsem[d.dsem], d.dsem, d.dval)
                            else:
                                wait(esem[d.eng], d.eng, d.sigval)
                        if o.is_dma:
                            if o.prev is not None:
                                wait(dsem[o.dsem], o.dsem, o.prev.dval)
                            o.fn(eng).then_inc(dsem[o.dsem], 16)
                        elif o.fn is not None:
                            ins = o.fn(eng)
                            if o.sig:
                                ins.then_inc(esem[e], 1)
                    if e == "sp":
                        for slot, o in self.dma_last.items():
                            wait(dsem[slot], slot, o.dval)
                return body
            for e in self.ENGS:
                if self.ops[e] or e == "sp":
                    handles[e](make(e))


def phase_a(nc, P, st, xin, w_in, g_mix, qkn, cs, idb, d_idb, kT_d, v_d, qT_d):
    def sb(name, shape, dt):
        return st.enter_context(nc.sbuf_tensor("a_" + name, shape, dt))

    def ps(name, shape, dt=F32):
        return st.enter_context(nc.psum_tensor("a_" + name, shape, dt))
    wq = sb("wq", [128, 8, 1536], BF16); d_wq = Dep()
    gm = sb("gm", [128, D], F32); d_gm = Dep()
    gq = sb("gq", [128, 2, 512], F32); d_gq = Dep()
    NB = 2
    xt = [sb("xt%d" % i, [128, D], F32) for i in range(NB)]; d_xt = [Dep() for _ in range(NB)]
    cst = [sb("cst%d" % i, [128, 64], F32) for i in range(NB)]; d_cst = [Dep() for _ in range(NB)]
    junk = sb("junk", [128, D], BF16); d_junk = Dep()
    ssq = sb("ssq", [128, 1], F32); d_ssq = Dep()
    rstd = sb("rstd", [128, 1], F32); d_rstd = Dep()
    hb = sb("hb", [128, D], BF16); d_hb = Dep()
    hT = [sb("hT%d" % i, [128, 8, 128], BF16) for i in range(NB)]; d_hT = [Dep() for _ in range(NB)]
    pT = ps("pT", [128, 8, 128], BF16); d_pT = Dep()
    pq = [[ps("pq%d_%d" % (j, i), [128, 512], F32) for i in range(3)] for j in range(2)]; d_pq = [[Dep() for _ in range(3)] for _ in range(2)]
    pkT = ps("pkT", [128, 4, 128], BF16); d_pkT = Dep()
    sq = sb("sq", [128, 512], F32); d_sq = Dep()
    hs = sb("hs", [128, 8], F32); d_hs = Dep()
    hr = sb("hr", [128, 8], F32); d_hr = Dep()
    qn = sb("qn", [128, 512], F32); d_qn = Dep()
    t1 = sb("t1", [128, 256], F32); d_t1 = Dep()
    t2 = sb("t2", [128, 256], F32); d_t2 = Dep()
    t3 = sb("t3", [128, 256], F32); d_t3 = Dep()
    t4 = sb("t4", [128, 256], F32); d_t4 = Dep()
    qr = sb("qr", [128, 512], BF16); d_qr = Dep()
    kTs = [sb("kTs%d" % i, [128, 4, 128], BF16) for i in range(NB)]; d_kTs = [Dep() for _ in range(NB)]
    vs = [sb("vs%d" % i, [128, 512], BF16) for i in range(NB)]; d_vs = [Dep() for _ in range(NB)]

    for k in range(8):
        P.dma("pool", lambda e, k=k: e.dma_start(out=wq[:, k, :], in_=w_in[k * 128:(k + 1) * 128, 0:1536]), writes=[d_wq])
    P.dma("sp", lambda e: e.dma_start(out=gm[:], in_=g_mix[0:1, :].partition_broadcast(128)), writes=[d_gm])
    for i in range(2):
        P.dma("sp", lambda e, i=i: e.dma_start(out=gq[:, i, :], in_=qkn[i:i + 1, :].partition_broadcast(128)), writes=[d_gq])
    P.op("dve", lambda e: e.tensor_scalar(out=gq[:, 0, :], in0=gq[:, 0, :], scalar1=0.125, scalar2=None, op0=ALU.mult),
         reads=[d_gq], writes=[d_gq])

    def qk_post(src_ps, d_src, which, b):
        P.op("act", lambda e: e.activation(out=sq[:], in_=src_ps[:], func=AF.Square), reads=[d_src], writes=[d_sq])
        P.op("dve", lambda e: e.tensor_reduce(out=hs[:], in_=sq[:].rearrange("p (h d) -> p h d", h=8), axis=AX.X, op=ALU.add),
             reads=[d_sq], writes=[d_hs])
        P.op("act", lambda e: e.activation(out=hr[:], in_=hs[:], func=AF.Sqrt, bias=EPS, scale=1.0 / 64), reads=[d_hs], writes=[d_hr])
        P.op("dve", lambda e: e.reciprocal(out=hr[:], in_=hr[:]), reads=[d_hr], writes=[d_hr])
        P.op("dve", lambda e: e.tensor_tensor(out=qn[:].rearrange("p (h d) -> p h d", h=8), in0=src_ps[:].rearrange("p (h d) -> p h d", h=8),
                                              in1=hr[:].unsqueeze(2).to_broadcast([128, 8, 64]), op=ALU.mult),
             reads=[d_src, d_hr], writes=[d_qn])
        P.op("pool", lambda e: e.tensor_tensor(out=qn[:], in0=qn[:], in1=gq[:, which, :], op=ALU.mult), reads=[d_qn, d_gq], writes=[d_qn])
        q3 = qn[:].rearrange("p (h d) -> p h d", h=8)
        q1 = q3[:, :, 0:32]
        q2 = q3[:, :, 32:64]
        cosb = cst[b][:, 0:32].unsqueeze(1).to_broadcast([128, 8, 32])
        sinb = cst[b][:, 32:64].unsqueeze(1).to_broadcast([128, 8, 32])
        v3 = lambda t: t[:].rearrange("p (h d) -> p h d", h=8)
        P.op("dve", lambda e: e.tensor_tensor(out=v3(t1), in0=q1, in1=cosb, op=ALU.mult), reads=[d_qn, d_cst[b]], writes=[d_t1])
        P.op("pool", lambda e: e.tensor_tensor(out=v3(t2), in0=q2, in1=sinb, op=ALU.mult), reads=[d_qn, d_cst[b]], writes=[d_t2])
        P.op("dve", lambda e: e.tensor_tensor(out=v3(t3), in0=q2, in1=cosb, op=ALU.mult), reads=[d_qn, d_cst[b]], writes=[d_t3])
        P.op("pool", lambda e: e.tensor_tensor(out=v3(t4), in0=q1, in1=sinb, op=ALU.mult), reads=[d_qn, d_cst[b]], writes=[d_t4])
        r3 = qr[:].rearrange("p (h d) -> p h d", h=8)
        P.op("dve", lambda e: e.tensor_tensor(out=r3[:, :, 0:32], in0=v3(t1), in1=v3(t2), op=ALU.subtract), reads=[d_t1, d_t2], writes=[d_qr])
        P.op("pool", lambda e: e.tensor_tensor(out=r3[:, :, 32:64], in0=v3(t3), in1=v3(t4), op=ALU.add), reads=[d_t3, d_t4], writes=[d_qr])

    def to_T_and_store(dst_dram, col0, b):
        for c in range(4):
            P.op("pe", lambda e, c=c: e.transpose(out=pkT[:, c, :], in_=qr[:, c * 128:(c + 1) * 128], identity=idb[:]),
                 reads=[d_qr, d_idb], writes=[d_pkT])
        P.op("act", lambda e: e.copy(out=kTs[b][:], in_=pkT[:]), reads=[d_pkT], writes=[d_kTs[b]])
        P.dma("sp", lambda e: e.dma_start(out=dst_dram[:, col0:col0 + 128].rearrange("(c p) t -> p c t", p=128), in_=kTs[b][:]),
              reads=[d_kTs[b]])

    def front(ti):
        b = ti % NB
        own = ti >= 32
        r0 = ti * 128
        P.dma("sp", lambda e: e.dma_start(out=xt[b][:], in_=xin[r0:r0 + 128, :]), writes=[d_xt[b]])
        P.dma("sp", lambda e: e.dma_start(out=cst[b][:], in_=cs[r0:r0 + 128, :]), writes=[d_cst[b]])
        P.op("act", lambda e: e.activation(out=junk[:], in_=xt[b][:], func=AF.Square, accum_out=ssq[:]), reads=[d_xt[b]], writes=[d_junk, d_ssq])
        P.op("act", lambda e: e.activation(out=rstd[:], in_=ssq[:], func=AF.Sqrt, bias=EPS, scale=1.0 / D), reads=[d_ssq], writes=[d_rstd])
        P.op("dve", lambda e: e.reciprocal(out=rstd[:], in_=rstd[:]), reads=[d_rstd], writes=[d_rstd])
        P.op("dve", lambda e: e.scalar_tensor_tensor(out=hb[:], in0=xt[b][:], scalar=rstd[:, 0:1], in1=gm[:], op0=ALU.mult, op1=ALU.mult),
             reads=[d_xt[b], d_rstd, d_gm], writes=[d_hb])
        for k in range(8):
            P.op("pe", lambda e, k=k: e.transpose(out=pT[:, k, :], in_=hb[:, k * 128:(k + 1) * 128], identity=idb[:]), reads=[d_hb, d_idb], writes=[d_pT])
        P.op("act", lambda e: e.copy(out=hT[b][:], in_=pT[:]), reads=[d_pT], writes=[d_hT[b]])
        groups = [0, 1, 2] if own else [1, 2]
        for g in groups:
            for k in range(8):
                P.op("pe", lambda e, g=g, k=k: e.matmul(pq[b][g][:], lhsT=hT[b][:, k, :], rhs=wq[:, k, g * 512:(g + 1) * 512], start=(k == 0), stop=(k == 7)),
                     reads=[d_hT[b], d_wq], writes=[d_pq[b][g]])

    def post(ti):
        b = ti % NB
        own = ti >= 32
        r0 = ti * 128
        qk_post(pq[b][1], d_pq[b][1], 1, b)
        to_T_and_store(kT_d, r0, b)
        if own:
            qk_post(pq[b][0], d_pq[b][0], 0, b)
            to_T_and_store(qT_d, r0 - TP, b)
        P.op("act", lambda e: e.copy(out=vs[b][:], in_=pq[b][2][:]), reads=[d_pq[b][2]], writes=[d_vs[b]])
        P.dma("sp", lambda e: e.dma_start(out=v_d[r0:r0 + 128, :], in_=vs[b][:]), reads=[d_vs[b]])

    front(0)
    for ti in range(64):
        if ti + 1 < 64:
            front(ti + 1)
        post(ti)


def phase_b(nc, P, st, kT_d, v_d, qT_d, e_d, vq_d, nb_d, oq_d, tri_d, idb, d_idb, oa, d_oa, heads=range(8), nblk=16):
    def sb(name, shape, dt):
        return st.enter_context(nc.sbuf_tensor("b_" + name, shape, dt))

    def ps(name, shape, dt=F32):
        return st.enter_context(nc.psum_tensor(name, shape, dt))
    NB = 2
    kaug = [sb("kaug%d" % i, [96, NK], BF16) for i in range(NB)]; d_kaug = [Dep() for _ in range(NB)]
    vaug = [sb("vaug%d" % i, [128, 64, 65], BF16) for i in range(NB)]; d_vaug = [Dep() for _ in range(NB)]
    qaug = [sb("qaug%d" % i, [96, T], BF16) for i in range(NB)]; d_qaug = [Dep() for _ in range(NB)]
    d_qmb = [Dep() for _ in range(NB)]
    vq = sb("vq_s", [128, 1024], F32); d_c = Dep()
    nb = sb("nb_s", [128, 1024], F32)
    oq = sb("oq_s", [128, 1024], F32)
    tri = sb("tri_s", [128, 128], BF16)
    ksum = sb("ksum", [64, 32], F32); d_ksum = Dep()
    kmean = sb("kmean", [64, 32], BF16); d_kmean = Dep()
    g1 = sb("g1", [128, 512], F32); d_g1 = Dep()
    top8 = sb("top8", [128, 16, 8], F32); d_top8 = Dep()
    sel = sb("sel", [128, 512], F32); d_sel = Dep()
    mbp = sb("mbp", [128, 16, 96], BF16); d_mbp = Dep()
    NPT = 3
    pt = [sb("pt%d" % i, [128, 512], BF16) for i in range(NPT)]; d_pt = [Dep() for _ in range(NPT)]
    rc = sb("rc", [128, 2], F32); d_rc = [Dep(), Dep()]
    pg = ps("pg", [128, 512], F32); d_pg = Dep()
    pmT = ps("pmT", [96, 8, 128], BF16); d_pmT = Dep()
    psS = [ps("psS%d" % i, [128, 512], F32) for i in range(NPT)]; d_psS = [Dep() for _ in range(NPT)]
    po = [ps("po%d" % i, [128, 65], F32) for i in range(2)]; d_po = [Dep() for _ in range(2)]

    for i in range(NB):
        P.dma("sp", lambda e, i=i: e.dma_start(out=kaug[i][64:96, :], in_=e_d[:, :]), writes=[d_kaug[i]])
        P.op("pool", lambda e, i=i: e.memset(vaug[i][:], 1.0), writes=[d_vaug[i]])
    P.dma("sp", lambda e: e.dma_start(out=vq[:], in_=vq_d[0:1, :].partition_broadcast(128)), writes=[d_c])
    P.dma("sp", lambda e: e.dma_start(out=nb[:], in_=nb_d[0:1, :].partition_broadcast(128)), writes=[d_c])
    P.dma("sp", lambda e: e.dma_start(out=oq[:], in_=oq_d[0:1, :].partition_broadcast(128)), writes=[d_c])
    P.dma("pool", lambda e: e.dma_start(out=tri[:], in_=tri_d[:, :]), writes=[d_c])
    P.op("pool", lambda e: e.memset(mbp[:], 0.0), writes=[d_mbp])

    for ih, h in enumerate(heads):
        b = ih % NB
        P.dma("sp", lambda e, b=b, h=h: e.dma_start(out=kaug[b][0:64, :], in_=kT_d[h * 64:(h + 1) * 64, :]), writes=[d_kaug[b]])
        P.dma("sp", lambda e, b=b, h=h: e.dma_start(out=vaug[b][:, :, 0:64],
                                                    in_=v_d[:, h * 64:(h + 1) * 64].rearrange("(t p) d -> p t d", p=128)),
              writes=[d_vaug[b]])
        P.dma("sp", lambda e, b=b, h=h: e.dma_start(out=qaug[b][0:64, :], in_=qT_d[h * 64:(h + 1) * 64, :]), writes=[d_qaug[b]])
        P.op("dve", lambda e, b=b: e.tensor_reduce(out=ksum[:], in_=kaug[b][0:64, :].rearrange("p (b k) -> p b k", k=256),
                                                   axis=AX.X, op=ALU.add), reads=[d_kaug[b]], writes=[d_ksum])
        P.op("dve", lambda e: e.tensor_scalar(out=kmean[:], in0=ksum[:], scalar1=1.0 / 256, scalar2=None, op0=ALU.mult),
             reads=[d_ksum], writes=[d_kmean])
        for half in range(2):
            for j in range(16):
                qt = half * 16 + j
                P.op("pe", lambda e, b=b, j=j, qt=qt: e.matmul(pg[:, j * 32:(j + 1) * 32], lhsT=qaug[b][0:64, qt * 128:(qt + 1) * 128],
                                                              rhs=kmean[:, :], start=True, stop=True),
                     reads=[d_qaug[b], d_kmean], writes=[d_pg])
            cs_ = slice(half * 512, (half + 1) * 512)
            P.op("dve", lambda e, cs_=cs_: e.tensor_tensor(out=g1[:], in0=pg[:], in1=vq[:, cs_], op=ALU.mult),
                 reads=[d_pg, d_c], writes=[d_g1])
            P.op("dve", lambda e, cs_=cs_: e.tensor_tensor(out=g1[:], in0=g1[:], in1=nb[:, cs_], op=ALU.add),
                 reads=[d_g1, d_c], writes=[d_g1])
            for j in range(16):
                P.op("dve", lambda e, j=j: e.max(out=top8[:, j, :], in_=g1[:, j * 32:(j + 1) * 32]), reads=[d_g1], writes=[d_top8])
            P.op("dve", lambda e: e.tensor_tensor(out=sel[:].rearrange("p (j b) -> p j b", b=32),
                                                  in0=g1[:].rearrange("p (j b) -> p j b", b=32),
                                                  in1=top8[:, :, 2:3].to_broadcast([128, 16, 32]), op=ALU.is_ge),
                 reads=[d_g1, d_top8], writes=[d_sel])
            P.op("dve", lambda e, cs_=cs_: e.tensor_tensor(out=sel[:], in0=sel[:], in1=vq[:, cs_], op=ALU.mult),
                 reads=[d_sel, d_c], writes=[d_sel])
            P.op("dve", lambda e, cs_=cs_: e.tensor_tensor(out=sel[:], in0=sel[:], in1=oq[:, cs_], op=ALU.add),
                 reads=[d_sel, d_c], writes=[d_sel])
            P.op("dve", lambda e: e.tensor_scalar(out=mbp[:, :, 64:96], in0=sel[:].rearrange("p (j b) -> p j b", b=32),
                                                  scalar1=1.0, scalar2=MB, op0=ALU.subtract, op1=ALU.mult),
                 reads=[d_sel], writes=[d_mbp])
            for grp in range(2):
                for j in range(8):
                    P.op("pe", lambda e, grp=grp, j=j: e.transpose(out=pmT[:, j, :], in_=mbp[:, grp * 8 + j, :], identity=idb[:]),
                         reads=[d_mbp, d_idb], writes=[d_pmT])
                c0 = (half * 16 + grp * 8) * 128
                P.op("act", lambda e, b=b, c0=c0: e.copy(out=qaug[b][64:96, c0:c0 + 1024], in_=pmT[64:96, :, :]),
                     reads=[d_pmT], writes=[d_qmb[b]])
        units = []
        for jb in range(nblk):
            ncommon = 32 + 2 * jb
            for u in range(ncommon // 2):
                units.append((jb, [2 * u, 2 * u + 1], False, u == 0))
            units.append((jb, [32 + 2 * jb, 32 + 2 * jb + 1], True, False))

        def emit_S(u, s, b=b):
            jb, kts, diag, first = u
            for j, kt in enumerate(kts):
                if diag and j == 1:
                    q0, nq, c0 = jb * 256 + 128, 128, 256
                else:
                    q0, nq, c0 = jb * 256, 256, j * 256
                P.op("pe", lambda e, s=s, kt=kt, q0=q0, nq=nq, c0=c0: e.matmul(psS[s][:, c0:c0 + nq], lhsT=kaug[b][0:96, kt * 128:(kt + 1) * 128],
                                                                             rhs=qaug[b][0:96, q0:q0 + nq], start=True, stop=True),
                     reads=[d_kaug[b], d_qaug[b], d_qmb[b]], writes=[d_psS[s]])

        def emit_rest(u, s, b=b, h=h):
            jb, kts, diag, first = u
            ncols = 384 if diag else 512
            P.op("act", lambda e, s=s, ncols=ncols: e.activation(out=pt[s][:, 0:ncols], in_=psS[s][:, 0:ncols], func=AF.Exp),
                 reads=[d_psS[s]], writes=[d_pt[s]])
            if diag:
                P.op("pool", lambda e, s=s: e.tensor_tensor(out=pt[s][:, 0:128], in0=pt[s][:, 0:128], in1=tri[:], op=ALU.mult),
                     reads=[d_pt[s], d_c], writes=[d_pt[s]])
                P.op("pool", lambda e, s=s: e.tensor_tensor(out=pt[s][:, 256:384], in0=pt[s][:, 256:384], in1=tri[:], op=ALU.mult),
                     reads=[d_pt[s], d_c], writes=[d_pt[s]])
            for j, kt in enumerate(kts):
                if diag and j == 1:
                    pv = [(1, 256, True)]
                elif diag:
                    pv = [(0, 0, True), (1, 128, False)]
                else:
                    pv = [(0, j * 256, False), (1, j * 256 + 128, False)]
                st_ = first and j == 0
                for qi, col0, last in pv:
                    P.op("pe", lambda e, s=s, kt=kt, qi=qi, col0=col0, st_=st_, last=last: e.matmul(
                        po[qi][:, :], lhsT=pt[s][:, col0:col0 + 128], rhs=vaug[b][:, kt, :], start=st_, stop=last),
                        reads=[d_pt[s], d_vaug[b]], writes=[d_po[qi]])
            if diag:
                for qi in range(2):
                    qt = jb * 2 + qi
                    P.op("dve", lambda e, qi=qi: e.reciprocal(out=rc[:, qi:qi + 1], in_=po[qi][:, 64:65]), reads=[d_po[qi]], writes=[d_rc[qi]])
                    P.op("dve", lambda e, qi=qi, qt=qt: e.tensor_scalar(out=oa[:, qt, h * 64:(h + 1) * 64], in0=po[qi][:, 0:64],
                                                                       scalar1=rc[:, qi:qi + 1], scalar2=None, op0=ALU.mult),
                         reads=[d_po[qi], d_rc[qi]], writes=[d_oa])
        SK = 2
        n = len(units)
        for i in range(n + SK):
            if i < n:
                emit_S(units[i], i % NPT)
            if i >= SK:
                emit_rest(units[i - SK], (i - SK) % NPT)


def host_consts(half):
    pv = 1.0 if half == 1 else 0.0
    V = np.zeros((32, 32), np.float32); O = np.zeros((32, 32), np.float32)
    for qt in range(32):
        jb = qt // 2
        V[qt, :16] = pv
        V[qt, 16:16 + jb] = 1.0
        O[qt, 16 + jb] = 1.0
    NBm = (V - 1.0) * BIGG
    E = np.zeros((32, NK), np.float32)
    for b in range(32):
        E[b, b * 256:(b + 1) * 256] = 1.0
    tri = (np.arange(128)[:, None] <= np.arange(128)[None, :]).astype(np.float32)
    return dict(vq=V.reshape(1, 1024), nb=NBm.reshape(1, 1024), oq=O.reshape(1, 1024),
                e_d=E.astype(ml_dtypes.bfloat16), tri=tri.astype(ml_dtypes.bfloat16))


def phase_c(nc, P, st, xin, w_in, g_mix, cwl_d, cbl_d, dtb_d, alog_d, dsk_d, ssdn_d, tri2_d, sel_d, tb_d, pflag_d,
            idb, d_idb, os_d, d_osd, chunks=range(32)):
    def sb(name, shape, dt):
        return st.enter_context(nc.sbuf_tensor("c_" + name, shape, dt))

    def ps(name, shape, dt=F32):
        return st.enter_context(nc.psum_tensor(name, shape, dt))
    wz = sb("wz", [128, 8, 1024], BF16); d_w = Dep()
    wx = sb("wx", [128, 8, 2048], BF16)
    wdt = sb("wdt", [128, 8, 16], BF16)
    gm = sb("gmc", [128, D], F32); d_c = Dep()
    cw = sb("cw", [128, 16, 4], F32)
    cb = sb("cb", [128, 16], F32)
    dtb = sb("dtb", [128, 2, 16], F32)
    Aneg = sb("Aneg", [128, 2, 16], F32); d_A = Dep()
    dsk = sb("dsk", [128, 16], F32)
    ssdn = sb("ssdn", [128, D], F32)
    tri2 = sb("tri2", [128, 2, 256], F32)
    onesf = sb("onesf", [128, 128], F32)
    sel = sb("selc", [16, 16, 128], F32)
    tb = sb("tbc", [128, 384], F32)
    pflag = sb("pflag", [128, 1], F32)
    xt = sb("xtc", [128, 2, D], F32); d_xt = Dep()
    junk = sb("junkc", [128, D], BF16); d_junk = Dep()
    ssq = sb("ssqc", [128, 2], F32); d_ssq = Dep()
    rstd = sb("rstdc", [128, 2], F32); d_rstd = Dep()
    hb = sb("hbc", [128, 2, D], BF16); d_hb = Dep()
    hT = sb("hTc", [128, 8, 256], BF16); d_hT = Dep()
    xraw = sb("xraw", [128, 16, 259], F32); d_xraw = Dep(); d_halo = Dep()
    acc = sb("acc", [128, 8, 256], F32); d_acc = [Dep() for _ in range(8)]
    xc = sb("xc", [128, 16, 256], BF16); d_xc = [Dep() for _ in range(16)]
    xtok = sb("xtok", [128, 2, 1024], BF16); d_xtok = Dep()
    btok = sb("btok", [128, 2, 512], BF16); d_btok = Dep()
    dtr = sb("dtr", [128, 2, 16], F32); d_dtr = Dep()
    dt = sb("dt", [128, 2, 16], F32); d_dt = Dep()
    aa = sb("aa", [128, 2, 16], F32); d_aa = Dep()
    acum = sb("acum", [128, 2, 16], F32); d_acum = Dep()
    nacum = sb("nacum", [128, 2, 16], F32); d_nacum = Dep()
    acumT = sb("acumT", [16, 256], F32); d_acumT = Dep()
    tot = sb("tot", [128, 16], F32); d_tot = Dep()
    cdec = sb("cdec", [128, 16], F32); d_cdec = Dep()
    dte = sb("dte", [128, 2, 16], F32); d_dte = Dep()
    eac = sb("eac", [128, 2, 16], F32); d_eac = Dep()
    w2 = sb("w2", [128, 2, 16], F32); d_w2 = Dep()
    xdt = sb("xdt", [128, 2, 1024], BF16); d_xdt = Dep()
    xdd = sb("xdd", [128, 2, 1024], BF16); d_xdd = Dep()
    zs = sb("zs", [128, 2, 1024], BF16); d_zs = Dep()
    cbT = sb("cbT", [128, 4, 384], F32); d_cbT = [Dep() for _ in range(4)]
    arg = [sb("arg%d" % i, [128, 384], F32) for i in range(2)]; d_arg = [Dep(), Dep()]
    Lt = [sb("Lt%d" % i, [128, 384], F32) for i in range(2)]; d_Lt = [Dep(), Dep()]
    Mt = [sb("Mt%d" % i, [128, 384], BF16) for i in range(2)]; d_Mt = [Dep() for _ in range(2)]
    stf = sb("stf", [128, 4, 256], F32); d_stf = Dep()
    stT = sb("stT", [128, 4, 256], BF16); d_stT = Dep()
    yt = [sb("yt%d" % i, [128, 256], F32) for i in range(2)]; d_yt = [Dep(), Dep()]
    y2 = [sb("y2%d" % i, [128, 256], F32) for i in range(2)]; d_y2 = [Dep(), Dep()]
    gss = [sb("gss%d" % i, [128, 1], F32) for i in range(2)]; d_gss = [Dep(), Dep()]
    grs = [sb("grs%d" % i, [128, 1], F32) for i in range(2)]; d_grs = [Dep(), Dep()]
    osb = sb("osb", [128, 2, 1024], BF16); d_osb = Dep()

    B = [ps("bk%d" % i, [128, 512], F32) for i in range(7)]
    d_B = [Dep() for _ in range(7)]
    d_B1h = [Dep(), Dep()]
    d_B5h = [Dep(), Dep()]
    pT = ps("pTc", [128, 8, 128], BF16); d_pT = Dep()

    for k in range(8):
        rows = slice(k * 128, (k + 1) * 128)
        P.dma("pool", lambda e, k=k, rows=rows: e.dma_start(out=wz[:, k, :], in_=w_in[rows, C_Z:C_Z + 1024]), writes=[d_w])
        P.dma("pool", lambda e, k=k, rows=rows: e.dma_start(out=wx[:, k, :], in_=w_in[rows, C_X:C_X + 2048]), writes=[d_w])
        P.dma("pool", lambda e, k=k, rows=rows: e.dma_start(out=wdt[:, k, :], in_=w_in[rows, C_DT:C_DT + 16]), writes=[d_w])
    P.dma("sp", lambda e: e.dma_start(out=gm[:], in_=g_mix[0:1, :].partition_broadcast(128)), writes=[d_c])
    P.dma("sp", lambda e: e.dma_start(out=cw[:], in_=cwl_d[:, :, :]), writes=[d_c])
    P.dma("sp", lambda e: e.dma_start(out=cb[:], in_=cbl_d[:, :]), writes=[d_c])
    for i in range(2):
        P.dma("sp", lambda e, i=i: e.dma_start(out=dtb[:, i, :], in_=dtb_d[0:1, :].partition_broadcast(128)), writes=[d_c])
        P.dma("sp", lambda e, i=i: e.dma_start(out=Aneg[:, i, :], in_=alog_d[0:1, :].partition_broadcast(128)), writes=[d_A])
    P.dma("sp", lambda e: e.dma_start(out=dsk[:], in_=dsk_d[0:1, :].partition_broadcast(128)), writes=[d_c])
    P.dma("sp", lambda e: e.dma_start(out=ssdn[:], in_=ssdn_d[0:1, :].partition_broadcast(128)), writes=[d_c])
    P.dma("sp", lambda e: e.dma_start(out=tri2[:], in_=tri2_d[:, :, :]), writes=[d_c])
    P.dma("sp", lambda e: e.dma_start(out=sel[:], in_=sel_d[:, :, :]), writes=[d_c])
    P.dma("sp", lambda e: e.dma_start(out=tb[:], in_=tb_d[:, :]), writes=[d_c])
    P.dma("sp", lambda e: e.dma_start(out=pflag[:], in_=pflag_d[0:1, :].partition_broadcast(128)), writes=[d_c])
    P.op("pool", lambda e: e.memset(onesf[:], 1.0), writes=[d_c])
    P.op("act", lambda e: e.activation(out=Aneg[:], in_=Aneg[:], func=AF.Exp), reads=[d_A], writes=[d_A])
    P.op("dve", lambda e: e.tensor_scalar(out=Aneg[:], in0=Aneg[:], scalar1=-1.0, scalar2=None, op0=ALU.mult), reads=[d_A], writes=[d_A])
    P.op("pool", lambda e: e.memset(xraw[:], 0.0), writes=[d_xraw, d_halo])
    P.op("pool", lambda e: e.memset(stf[:], 0.0), writes=[d_stf])
    P.op("pool", lambda e: e.memset(stT[:], 0.0), writes=[d_stT])

    h3 = lambda ap: ap.rearrange("p (h d) -> p h d", d=64)

    for c in chunks:
        own = c >= 16
        r0 = c * 256
        P.dma("sp", lambda e, r0=r0: e.dma_start(out=xt[:], in_=xin[r0:r0 + 256, :].rearrange("(t p) d -> p t d", p=128)), writes=[d_xt])
        for it in range(2):
            P.op("act", lambda e, it=it: e.activation(out=junk[:], in_=xt[:, it, :], func=AF.Square, accum_out=ssq[:, it:it + 1]),
                 reads=[d_xt], writes=[d_junk, d_ssq])
        P.op("act", lambda e: e.activation(out=rstd[:], in_=ssq[:], func=AF.Sqrt, bias=EPS, scale=1.0 / D), reads=[d_ssq], writes=[d_rstd])
        P.op("dve", lambda e: e.reciprocal(out=rstd[:], in_=rstd[:]), reads=[d_rstd], writes=[d_rstd])
        for it in range(2):
            P.op("dve", lambda e, it=it: e.scalar_tensor_tensor(out=hb[:, it, :], in0=xt[:, it, :], scalar=rstd[:, it:it + 1], in1=gm[:],
                                                                                    op0=ALU.mult, op1=ALU.mult),
                 reads=[d_xt, d_rstd, d_c], writes=[d_hb])
        for it in range(2):
            for k in range(8):
                P.op("pe", lambda e, it=it, k=k: e.transpose(out=pT[:, k, :], in_=hb[:, it, k * 128:(k + 1) * 128], identity=idb[:]),
                     reads=[d_hb, d_idb], writes=[d_pT])
            P.op("act", lambda e, it=it: e.copy(out=hT[:, :, it * 128:(it + 1) * 128], in_=pT[:]), reads=[d_pT], writes=[d_hT])
        for it in range(2):
            for k in range(8):
                P.op("pe", lambda e, it=it, k=k: e.matmul(B[3][:, it * 16:(it + 1) * 16], lhsT=hT[:, k, it * 128:(it + 1) * 128], rhs=wdt[:, k, :],
                                                         start=(k == 0), stop=(k == 7)), reads=[d_hT, d_w], writes=[d_B[3]])
        P.op("dve", lambda e: e.tensor_tensor(out=dtr[:].rearrange("p a b -> p (a b)"), in0=B[3][:, 0:32], in1=dtb[:].rearrange("p a b -> p (a b)"), op=ALU.add),
             reads=[d_B[3], d_c], writes=[d_dtr])
        P.op("act", lambda e: e.activation(out=dtr[:], in_=dtr[:], func=AF.Exp), reads=[d_dtr], writes=[d_dtr])
        P.op("act", lambda e: e.activation(out=dt[:], in_=dtr[:], func=AF.Ln, bias=1.0), reads=[d_dtr], writes=[d_dt])
        P.op("dve", lambda e: e.tensor_tensor(out=aa[:], in0=dt[:], in1=Aneg[:], op=ALU.mult), reads=[d_dt, d_A], writes=[d_aa])
        for it in range(2):
            for jt in range(it + 1):
                P.op("pe", lambda e, it=it, jt=jt: e.matmul(B[3][:, 32 + it * 16:32 + (it + 1) * 16], lhsT=tri2[:, jt, it * 128:(it + 1) * 128],
                                                           rhs=aa[:, jt, :], start=(jt == 0), stop=(jt == it)),
                     reads=[d_aa, d_c], writes=[d_B[3]])
        for jt in range(2):
            P.op("pe", lambda e, jt=jt: e.matmul(B[3][:, 64:80], lhsT=onesf[:], rhs=aa[:, jt, :], start=(jt == 0), stop=(jt == 1)),
                 reads=[d_aa, d_c], writes=[d_B[3]])
        for jt in range(2):
            P.op("pe", lambda e, jt=jt: e.matmul(B[3][0:16, 128:384], lhsT=aa[:, jt, :], rhs=tri2[:, jt, :], start=(jt == 0), stop=(jt == 1)),
                 reads=[d_aa, d_c], writes=[d_B[3]])
        P.op("act", lambda e: e.copy(out=acum[:].rearrange("p a b -> p (a b)"), in_=B[3][:, 32:64]), reads=[d_B[3]], writes=[d_acum])
        P.op("dve", lambda e: e.tensor_scalar(out=nacum[:].rearrange("p a b -> p (a b)"), in0=B[3][:, 32:64], scalar1=-1.0, scalar2=None, op0=ALU.mult),
             reads=[d_B[3]], writes=[d_nacum])
        P.op("act", lambda e: e.copy(out=tot[:], in_=B[3][:, 64:80]), reads=[d_B[3]], writes=[d_tot])
        P.op("act", lambda e: e.copy(out=acumT[:], in_=B[3][0:16, 128:384]), reads=[d_B[3]], writes=[d_acumT])
        P.op("act", lambda e: e.activation(out=cdec[:], in_=tot[:], func=AF.Exp), reads=[d_tot], writes=[d_cdec])
        P.op("dve", lambda e: e.tensor_tensor(out=dte[:], in0=nacum[:], in1=tot[:].unsqueeze(1).to_broadcast([128, 2, 16]), op=ALU.add),
             reads=[d_nacum, d_tot], writes=[d_dte])
        P.op("act", lambda e: e.activation(out=dte[:], in_=dte[:], func=AF.Exp), reads=[d_dte], writes=[d_dte])
        P.op("act", lambda e: e.activation(out=eac[:], in_=acum[:], func=AF.Exp), reads=[d_acum], writes=[d_eac])
        P.op("dve", lambda e: e.tensor_tensor(out=w2[:], in0=dt[:], in1=dte[:], op=ALU.mult), reads=[d_dt, d_dte], writes=[d_w2])
        nfc = 16 if (own or c == 15) else 12
        for fc in range(nfc):
            bi = 1 if fc % 2 == 0 else 4
            for k in range(8):
                P.op("pe", lambda e, fc=fc, k=k, bi=bi: e.matmul(B[bi][:, 0:256], lhsT=wx[:, k, fc * 128:(fc + 1) * 128], rhs=hT[:, k, :],
                                                               start=(k == 0), stop=(k == 7)), reads=[d_hT, d_w], writes=[d_B[bi]])
            P.op("act", lambda e, fc=fc, bi=bi: e.copy(out=xraw[:, fc, 3:259], in_=B[bi][:, 0:256]),
                 reads=[d_B[bi]], writes=[d_xraw])
        if own:
            for it in range(2):
                for hv in range(2):
                    zb = 2 if hv == 0 else 6
                    for k in range(8):
                        P.op("pe", lambda e, it=it, hv=hv, k=k, zb=zb: e.matmul(B[zb][:, :], lhsT=hT[:, k, it * 128:(it + 1) * 128], rhs=wz[:, k, hv * 512:(hv + 1) * 512],
                                                                              start=(k == 0), stop=(k == 7)), reads=[d_hT, d_w], writes=[d_B[zb]])
                    P.op("act", lambda e, it=it, hv=hv, zb=zb: e.activation(out=zs[:, it, hv * 512:(hv + 1) * 512], in_=B[zb][:, :], func=AF.Silu),
                         reads=[d_B[zb]], writes=[d_zs])
        for half in range(2):
            n8 = 8 if (half == 0 or nfc == 16) else 4
            for f8 in range(n8):
                fc = half * 8 + f8
                P.op("dve", lambda e, fc=fc, f8=f8: e.tensor_scalar(out=acc[:, f8, :], in0=xraw[:, fc, 0:256], scalar1=cw[:, fc, 0:1], scalar2=cb[:, fc:fc + 1],
                                                                   op0=ALU.mult, op1=ALU.add), reads=[d_xraw, d_halo, d_c], writes=[d_acc[f8]])
            for kk in range(1, 4):
                for f8 in range(n8):
                    fc = half * 8 + f8
                    P.op("dve", lambda e, fc=fc, f8=f8, kk=kk: e.scalar_tensor_tensor(out=acc[:, f8, :], in0=xraw[:, fc, kk:kk + 256], scalar=cw[:, fc, kk:kk + 1],
                                                                                     in1=acc[:, f8, :], op0=ALU.mult, op1=ALU.add),
                         reads=[d_xraw, d_halo, d_c, d_acc[f8]], writes=[d_acc[f8]])
            for f8 in range(n8):
                fc = half * 8 + f8
                P.op("act", lambda e, fc=fc, f8=f8: e.activation(out=xc[:, fc, :], in_=acc[:, f8, :], func=AF.Silu),
                     reads=[d_acc[f8]], writes=[d_xc[fc]])
        P.op("pool", lambda e: e.tensor_copy(out=xraw[:, :, 0:3], in_=xraw[:, :, 256:259]), reads=[d_xraw], writes=[d_halo])
        for it in range(2):
            for fc in range(8):
                P.op("pe", lambda e, it=it, fc=fc: e.transpose(out=pT[:, fc, :], in_=xc[:, fc, it * 128:(it + 1) * 128], identity=idb[:]),
                     reads=[d_xc[fc], d_idb], writes=[d_pT])
            P.op("act", lambda e, it=it: e.copy(out=xtok[:, it, :], in_=pT[:]), reads=[d_pT], writes=[d_xtok])
            for g in range(4):
                P.op("pe", lambda e, it=it, g=g: e.transpose(out=pT[:, g, :], in_=xc[:, 8 + g, it * 128:(it + 1) * 128], identity=idb[:]),
                     reads=[d_xc[8 + g], d_idb], writes=[d_pT])
            P.op("act", lambda e, it=it: e.copy(out=btok[:, it, :], in_=pT[:, 0:4, :]), reads=[d_pT], writes=[d_btok])
        for it in range(2):
            if own:
                P.op("dve", lambda e, it=it: e.tensor_tensor(out=h3(xdt[:, it, :]), in0=h3(xtok[:, it, :]),
                                                             in1=dt[:, it, :].unsqueeze(2).to_broadcast([128, 16, 64]), op=ALU.mult),
                     reads=[d_xtok, d_dt], writes=[d_xdt])
            P.op("pool", lambda e, it=it: e.tensor_tensor(out=h3(xdd[:, it, :]), in0=h3(xtok[:, it, :]),
                                                          in1=w2[:, it, :].unsqueeze(2).to_broadcast([128, 16, 64]), op=ALU.mult),
                 reads=[d_xtok, d_w2], writes=[d_xdd])
        if own:
            for g in range(4):
                P.op("pe", lambda e, g=g: e.matmul(B[4][:, 0:256], lhsT=xc[:, 8 + g, 0:128], rhs=xc[:, 12 + g, :], start=True, stop=True),
                     reads=[d_xc[8 + g], d_xc[12 + g]], writes=[d_B[4]])
                P.op("pe", lambda e, g=g: e.matmul(B[4][:, 256:384], lhsT=xc[:, 8 + g, 128:256], rhs=xc[:, 12 + g, 128:256], start=True, stop=True),
                     reads=[d_xc[8 + g], d_xc[12 + g]], writes=[d_B[4]])
                P.op("act", lambda e, g=g: e.copy(out=cbT[:, g, :], in_=B[4][:, 0:384]), reads=[d_B[4]], writes=[d_cbT[g]])
            for g in range(4):
                def stA(h, g=g):
                    mi = h % 2
                    bk = 5 if mi == 0 else 4
                    P.op("pe", lambda e: e.matmul(B[bk][:, 0:256], lhsT=sel[:, h, :], rhs=acumT[:, :], start=True, stop=True),
                         reads=[d_c, d_acumT], writes=[d_B[bk]])
                    P.op("dve", lambda e: e.scalar_tensor_tensor(out=arg[mi][:, 0:256], in0=B[bk][:, 0:256], scalar=nacum[:, 0, h:h + 1], in1=tb[:, 0:256],
                                                                 op0=ALU.add, op1=ALU.add), reads=[d_B[bk], d_nacum, d_c], writes=[d_arg[mi]])
                    P.op("dve", lambda e: e.scalar_tensor_tensor(out=arg[mi][:, 256:384], in0=B[bk][:, 128:256], scalar=nacum[:, 1, h:h + 1], in1=tb[:, 256:384],
                                                                 op0=ALU.add, op1=ALU.add), reads=[d_B[bk], d_nacum, d_c], writes=[d_arg[mi]])
                    P.op("act", lambda e: e.activation(out=Lt[mi][:], in_=arg[mi][:], func=AF.Exp), reads=[d_arg[mi]], writes=[d_Lt[mi]])
                    P.op("pool", lambda e: e.tensor_tensor(out=Mt[mi][:], in0=Lt[mi][:], in1=cbT[:, g, :], op=ALU.mult),
                         reads=[d_Lt[mi], d_cbT[g]], writes=[d_Mt[mi]])

                def stB(h, g=g):
                    mi = h % 2
                    r = h % 4
                    cs_ = slice(r * 64, (r + 1) * 64)
                    hs_ = slice(h * 64, (h + 1) * 64)
                    P.op("pe", lambda e: e.matmul(B[6][:, 0:256][:, cs_], lhsT=Mt[mi][:, 0:128], rhs=xdt[:, 0, hs_], start=True, stop=True),
                         reads=[d_Mt[mi], d_xdt], writes=[d_B[6]])
                    P.op("pe", lambda e: e.matmul(B[6][:, 256:512][:, cs_], lhsT=Mt[mi][:, 128:256], rhs=xdt[:, 0, hs_], start=True, stop=False),
                         reads=[d_Mt[mi], d_xdt], writes=[d_B[6]])
                    P.op("pe", lambda e: e.matmul(B[6][:, 256:512][:, cs_], lhsT=Mt[mi][:, 256:384], rhs=xdt[:, 1, hs_], start=False, stop=True),
                         reads=[d_Mt[mi], d_xdt], writes=[d_B[6]])
                hs4 = [g * 4 + r for r in range(4)]
                stA(hs4[0]); stA(hs4[1]); stB(hs4[0]); stA(hs4[2]); stB(hs4[1]); stA(hs4[3]); stB(hs4[2]); stB(hs4[3])
                gs_ = slice(g * 256, (g + 1) * 256)
                for it in range(2):
                    P.op("pe", lambda e, g=g, it=it: e.matmul(B[0][:, it * 256:(it + 1) * 256], lhsT=xc[:, 12 + g, it * 128:(it + 1) * 128], rhs=stT[:, g, :],
                                                             start=True, stop=True), reads=[d_xc[12 + g], d_stT], writes=[d_B[0]])
                v4 = lambda ap: ap.rearrange("p (r d) -> p r d", d=64)

                def ystep(step, it, g=g, gs_=gs_):
                    if step == 0:
                        P.op("dve", lambda e: e.tensor_tensor(out=v4(yt[it][:]), in0=v4(B[0][:, it * 256:(it + 1) * 256]),
                                                              in1=eac[:, it, g * 4:(g + 1) * 4].unsqueeze(2).to_broadcast([128, 4, 64]), op=ALU.mult),
                             reads=[d_B[0], d_eac], writes=[d_yt[it]])
                    elif step == 1:
                        P.op("dve", lambda e: e.tensor_tensor(out=yt[it][:], in0=yt[it][:], in1=B[6][:, it * 256:(it + 1) * 256], op=ALU.add),
                             reads=[d_yt[it], d_B[6]], writes=[d_yt[it]])
                    elif step == 2:
                        P.op("pool", lambda e: e.tensor_tensor(out=v4(y2[it][:]), in0=v4(xtok[:, it, gs_]),
                                                               in1=dsk[:, g * 4:(g + 1) * 4].unsqueeze(2).to_broadcast([128, 4, 64]), op=ALU.mult),
                             reads=[d_xtok, d_c], writes=[d_y2[it]])
                    elif step == 3:
                        P.op("dve", lambda e: e.tensor_tensor(out=yt[it][:], in0=yt[it][:], in1=y2[it][:], op=ALU.add), reads=[d_yt[it], d_y2[it]], writes=[d_yt[it]])
                    elif step == 4:
                        P.op("dve", lambda e: e.tensor_tensor(out=yt[it][:], in0=yt[it][:], in1=zs[:, it, gs_], op=ALU.mult), reads=[d_yt[it], d_zs], writes=[d_yt[it]])
                    elif step == 5:
                        P.op("act", lambda e: e.activation(out=y2[it][:], in_=yt[it][:], func=AF.Square, accum_out=gss[it][:]), reads=[d_yt[it], d_y2[it]], writes=[d_y2[it], d_gss[it]])
                    elif step == 6:
                        P.op("act", lambda e: e.activation(out=grs[it][:], in_=gss[it][:], func=AF.Sqrt, bias=EPS, scale=1.0 / 256), reads=[d_gss[it]], writes=[d_grs[it]])
                    elif step == 7:
                        P.op("dve", lambda e: e.reciprocal(out=grs[it][:], in_=grs[it][:]), reads=[d_grs[it]], writes=[d_grs[it]])
                    elif step == 8:
                        P.op("dve", lambda e: e.scalar_tensor_tensor(out=osb[:, it, gs_], in0=yt[it][:], scalar=grs[it][:, 0:1], in1=ssdn[:, gs_],
                                                                     op0=ALU.mult, op1=ALU.mult), reads=[d_yt[it], d_grs[it], d_c], writes=[d_osb])
                for step in range(9):
                    for it in range(2):
                        ystep(step, it)
            P.dma("sp", lambda e, r0=r0: e.dma_start(out=os_d[r0 - TP:r0 - TP + 256, :].rearrange("(t p) c -> p t c", p=128), in_=osb[:]),
                  reads=[d_osb], writes=[d_osd])
        for g in range(4):
            bank = B[2] if g < 2 else B[1]
            dbank = d_B[2] if g < 2 else d_B[1]
            cs_ = slice((g % 2) * 256, (g % 2 + 1) * 256)
            for jt in range(2):
                P.op("pe", lambda e, g=g, jt=jt, bank=bank, cs_=cs_: e.matmul(bank[:, cs_], lhsT=btok[:, jt, g * 128:(g + 1) * 128], rhs=xdd[:, jt, g * 256:(g + 1) * 256],
                                                                            start=(jt == 0), stop=(jt == 1)), reads=[d_btok, d_xdd], writes=[dbank])
        v16 = lambda ap: ap.rearrange("p (h d) -> p h d", d=64)
        P.op("dve", lambda e: e.tensor_tensor(out=v16(stf[:].rearrange("p g c -> p (g c)")), in0=v16(stf[:].rearrange("p g c -> p (g c)")),
                                              in1=cdec[:].unsqueeze(2).to_broadcast([128, 16, 64]), op=ALU.mult), reads=[d_stf, d_cdec], writes=[d_stf])
        P.op("dve", lambda e: e.tensor_tensor(out=stf[:, 0:2, :].rearrange("p g c -> p (g c)"), in0=stf[:, 0:2, :].rearrange("p g c -> p (g c)"), in1=B[2][:, :], op=ALU.add),
             reads=[d_stf, d_B[2]], writes=[d_stf])
        P.op("dve", lambda e: e.tensor_tensor(out=stf[:, 2:4, :].rearrange("p g c -> p (g c)"), in0=stf[:, 2:4, :].rearrange("p g c -> p (g c)"), in1=B[1][:, :], op=ALU.add),
             reads=[d_stf, d_B[1]], writes=[d_stf])
        if c == 15:
            P.op("dve", lambda e: e.tensor_scalar(out=stf[:], in0=stf[:], scalar1=pflag[:, 0:1], scalar2=None, op0=ALU.mult), reads=[d_stf, d_c], writes=[d_stf])
        P.op("act", lambda e: e.copy(out=stT[:], in_=stf[:]), reads=[d_stf], writes=[d_stT])


def host_consts_c(z, half):
    cwl = np.ascontiguousarray(z['conv_w'].reshape(4, 16, 128).transpose(2, 1, 0)).astype(np.float32)
    cbl = np.ascontiguousarray(z['conv_b'].reshape(16, 128).T).astype(np.float32)
    tri = (np.arange(128)[:, None] <= np.arange(128)[None, :]).astype(np.float32)
    tri2 = np.zeros((128, 2, 256), np.float32)
    tri2[:, 0, 0:128] = tri; tri2[:, 0, 128:256] = 1.0; tri2[:, 1, 128:256] = tri
    sel = np.zeros((16, 16, 128), np.float32)
    for h in range(16):
        sel[h, h, :] = 1.0
    tbias = np.where(tri > 0, 0.0, NEG).astype(np.float32)
    tb = np.zeros((128, 384), np.float32)
    tb[:, 0:128] = tbias; tb[:, 256:384] = tbias
    return dict(cwl=cwl, cbl=cbl, dtb=z['dt_bias'][None, :], alog=z['a_log'][None, :], dsk=z['d_skip'][None, :], ssdn=z['ssd_norm'][None, :],
                tri2=tri2, sel_d=sel, tb_d=tb, pflag=np.array([[1.0 if half == 1 else 0.0]], np.float32))


def phase_def(nc, P, xown, w_in, g_mix, mem_d, gmem_d, wmemkv, mqn_d, mkn_d, woa_d, wos_d, wom_d, wout_d, gffn_d, wr_d,
              wgate_d, wup_d, wdown_d, tri_d, ecap_d, oa_d, d_oad, os_d, d_osd, x1_d, xbuf, ybuf, out_d, idb, d_idb, idf, d_idf,
              ntiles=32, experts=range(32), stage=3):
    d_x1d = Dep(); d_xbuf = Dep(); d_ybuf = Dep(); d_out = Dep()
    with contextlib.ExitStack() as stp:
        def sbp(name, shape, dt):
            return stp.enter_context(nc.sbuf_tensor("d_" + name, shape, dt))
        combs = sbp("combs", [128, 32, 2], F32); d_combs = Dep()
        idxs = sbp("idxs", [128, 32, 2], I32); d_idxs = Dep()

        with contextlib.ExitStack() as st:
            def sb(name, shape, dt):
                return st.enter_context(nc.sbuf_tensor("d_" + name, shape, dt))

            def ps(name, shape, dt=F32):
                return st.enter_context(nc.psum_tensor(name, shape, dt))
            d_w = Dep(); d_c = Dep()
            gm = sb("gmd", [128, D], F32)
            gf = sb("gfd", [128, D], F32)
            mqn = sb("mqn", [128, 512], F32)
            mkn = sb("mkn", [128, 512], F32)
            tri = sb("trid", [128, 128], F32)
            onesf = sb("onesfd", [128, 128], F32)
            onesb = sb("onesbd", [128, 128], BF16)
            ecap = sb("ecap", [128, 32], F32)
            base = sb("base", [128, 32], F32); d_base = Dep()
            kmT = sb("kmT", [128, 4, 256], BF16); d_kmT = Dep()
            vm = sb("vm", [128, 2, 512], BF16); d_vm = Dep()

            xt = [sb("xtd%d" % i, [128, D], F32) for i in range(2)]; d_xt = [Dep(), Dep()]
            junk = sb("junkd", [128, D], BF16); d_junk = Dep()
            ssq = sb("ssqd", [128, 1], F32); d_ssq = Dep()
            rstd = sb("rstdd", [128, 1], F32); d_rstd = Dep()
            hb = sb("hbd", [128, D], BF16); d_hb = Dep()
            hT = sb("hTd", [128, 8, 128], BF16); d_hT = Dep()
            sq = sb("sqd", [128, 512], F32); d_sq = Dep()
            hs = sb("hsd", [128, 4], F32); d_hs = Dep()
            hr = sb("hrd", [128, 4], F32); d_hr = Dep()
            qmn = sb("qmn", [128, 512], F32); d_qmn = Dep()
            qmb = sb("qmb", [128, 512], BF16); d_qmb = Dep()
            qmT = sb("qmT", [128, 4, 128], BF16); d_qmT = Dep()
            pm = sb("pm", [128, 4, 2, 128], BF16); d_pm = Dep()
            rcp = sb("rcp", [128, 512], F32); d_rcp = Dep()
            omT = sb("omT", [128, 4, 128], BF16); d_omT = Dep()
            gs = sb("gs", [128, 3072], F32); d_gs = Dep()
            oat = [sb("oat%d" % i, [128, 512], BF16) for i in range(2)]; d_oat = [Dep(), Dep()]
            ost = [sb("ost%d" % i, [128, 1024], BF16) for i in range(2)]; d_ost = [Dep(), Dep()]
            oaT = sb("oaT", [128, 4, 128], BF16); d_oaT = Dep()
            osT = sb("osT", [128, 8, 128], BF16); d_osT = Dep()
            mg = sb("mg", [128, 512], F32); d_mg = Dep()
            tt = sb("tt", [128, 512], F32); d_tt = Dep()
            mgb = sb("mgb", [128, D], BF16); d_mgb = Dep()
            mgT = sb("mgT", [128, 8, 128], BF16); d_mgT = Dep()
            x1 = sb("x1", [128, D], F32); d_x1 = Dep()
            h2f = sb("h2f", [128, D], F32); d_h2f = Dep()
            h2b = [sb("h2b%d" % i, [128, D], BF16) for i in range(2)]; d_h2b = [Dep() for _ in range(2)]
            h2T = sb("h2T", [128, 8, 128], F32); d_h2T = Dep()
            lg = sb("lg", [128, 36], F32); d_lg = Dep()
            sm = sb("sm", [128, 64], F32); d_sm = Dep()
            goh = sb("goh", [128, 4], F32); d_goh = Dep()
            lsel = sb("lsel", [128, 32], F32); d_lsel = Dep()
            les = sb("les", [128, 8], F32); d_les = Dep()
            t8 = sb("t8", [128, 8], F32); d_t8 = Dep()
            oh = sb("oh", [128, 64], F32); d_oh = Dep()
            ohs = sb("ohs", [128, 32], F32); d_ohs = Dep()
            posf = sb("posf", [128, 32], F32); d_posf = Dep()
            idf2 = sb("idf2", [128, 2], F32); d_idf2 = Dep()

            pT = ps("pTd", [128, 8, 128], BF16); d_pT = Dep()
            pF = ps("pFd", [128, 512], F32); d_pF = Dep()
            pG = [ps("pGd%d" % i, [128, 512], F32) for i in range(3)]; d_pG = [Dep() for _ in range(3)]
            pms = [ps("pmsd%d" % i, [128, 512], F32) for i in range(2)]; d_pms = [Dep() for _ in range(2)]
            pmo = ps("pmod", [128, 512], F32); d_pmo = Dep()

            P.dma("sp", lambda e: e.dma_start(out=gm[:], in_=g_mix[0:1, :].partition_broadcast(128)), writes=[d_c])
            P.dma("sp", lambda e: e.dma_start(out=gf[:], in_=gffn_d[0:1, :].partition_broadcast(128)), writes=[d_c])
            P.dma("sp", lambda e: e.dma_start(out=mqn[:], in_=mqn_d[0:1, :].partition_broadcast(128)), writes=[d_c])
            P.dma("sp", lambda e: e.dma_start(out=mkn[:], in_=mkn_d[0:1, :].partition_broadcast(128)), writes=[d_c])
            P.dma("sp", lambda e: e.dma_start(out=tri[:], in_=tri_d[:, :]), writes=[d_c])
            P.dma("sp", lambda e: e.dma_start(out=ecap[:], in_=ecap_d[0:1, :].partition_broadcast(128)), writes=[d_c])
            P.op("pool", lambda e: e.memset(onesf[:], 1.0), writes=[d_c])
            P.op("pool", lambda e: e.memset(onesb[:], 1.0), writes=[d_c])
            P.op("pool", lambda e: e.memset(base[:], 0.0), writes=[d_base])

            def head_norm(src_ps, d_src, gain, nh, dst, d_dst):
                hd = 512 // nh
                v = lambda ap: ap.rearrange("p (h d) -> p h d", h=nh)
                P.op("act", lambda e: e.activation(out=sq[:], in_=src_ps, func=AF.Square), reads=[d_src], writes=[d_sq])
                P.op("dve", lambda e: e.tensor_reduce(out=hs[:, 0:nh], in_=v(sq[:]), axis=AX.X, op=ALU.add), reads=[d_sq], writes=[d_hs])
                P.op("act", lambda e: e.activation(out=hr[:, 0:nh], in_=hs[:, 0:nh], func=AF.Sqrt, bias=EPS, scale=1.0 / hd), reads=[d_hs], writes=[d_hr])
                P.op("dve", lambda e: e.reciprocal(out=hr[:, 0:nh], in_=hr[:, 0:nh]), reads=[d_hr], writes=[d_hr])
                P.op("dve", lambda e: e.tensor_tensor(out=v(qmn[:]), in0=v(src_ps), in1=hr[:, 0:nh].unsqueeze(2).to_broadcast([128, nh, hd]), op=ALU.mult),
                     reads=[d_src, d_hr], writes=[d_qmn])
                P.op("pool", lambda e: e.tensor_tensor(out=dst, in0=qmn[:], in1=gain[:], op=ALU.mult), reads=[d_qmn, d_c], writes=[d_dst])

            with contextlib.ExitStack() as stm:
                wkv = stm.enter_context(nc.sbuf_tensor("s_wkv", [128, 8, 1024], BF16)); d_wkv = Dep()
                mt_ = stm.enter_context(nc.sbuf_tensor("s_memt", [128, 2, D], F32)); d_mt = Dep()
                gme = stm.enter_context(nc.sbuf_tensor("s_gme", [128, D], F32)); d_gme = Dep()
                mhb = stm.enter_context(nc.sbuf_tensor("s_mhb", [128, 2, D], BF16)); d_mhb = Dep()
                mT = stm.enter_context(nc.sbuf_tensor("s_mT", [128, 8, 256], BF16)); d_mT = Dep()
                ss2 = stm.enter_context(nc.sbuf_tensor("s_ss2", [128, 2], F32)); d_ss2 = Dep()
                for k in range(8):
                    P.dma("pool", lambda e, k=k: e.dma_start(out=wkv[:, k, :], in_=wmemkv[k * 128:(k + 1) * 128, :]), writes=[d_wkv])
                P.dma("sp", lambda e: e.dma_start(out=mt_[:], in_=mem_d[:, :].rearrange("(t p) d -> p t d", p=128)), writes=[d_mt])
                P.dma("sp", lambda e: e.dma_start(out=gme[:], in_=gmem_d[0:1, :].partition_broadcast(128)), writes=[d_gme])
                for it in range(2):
                    P.op("act", lambda e, it=it: e.activation(out=junk[:], in_=mt_[:, it, :], func=AF.Square, accum_out=ss2[:, it:it + 1]),
                         reads=[d_mt], writes=[d_junk, d_ss2])
                P.op("act", lambda e: e.activation(out=ss2[:], in_=ss2[:], func=AF.Sqrt, bias=EPS, scale=1.0 / D), reads=[d_ss2], writes=[d_ss2])
                P.op("dve", lambda e: e.reciprocal(out=ss2[:], in_=ss2[:]), reads=[d_ss2], writes=[d_ss2])
                for it in range(2):
                    P.op("dve", lambda e, it=it: e.scalar_tensor_tensor(out=mhb[:, it, :], in0=mt_[:, it, :], scalar=ss2[:, it:it + 1], in1=gme[:],
                                                                        op0=ALU.mult, op1=ALU.mult), reads=[d_mt, d_ss2, d_gme], writes=[d_mhb])
                    for k in range(8):
                        P.op("pe", lambda e, it=it, k=k: e.transpose(out=pT[:, k, :], in_=mhb[:, it, k * 128:(k + 1) * 128], identity=idb[:]),
                             reads=[d_mhb, d_idb], writes=[d_pT])
                    P.op("act", lambda e, it=it: e.copy(out=mT[:, :, it * 128:(it + 1) * 128], in_=pT[:]), reads=[d_pT], writes=[d_mT])
                for it in range(2):
                    for hv in range(2):
                        for k in range(8):
                            P.op("pe", lambda e, it=it, hv=hv, k=k: e.matmul(pG[hv][:, :], lhsT=mT[:, k, it * 128:(it + 1) * 128], rhs=wkv[:, k, hv * 512:(hv + 1) * 512],
                                                                           start=(k == 0), stop=(k == 7)), reads=[d_mT, d_wkv], writes=[d_pG[hv]])
                    head_norm(pG[0][:, :], d_pG[0], mkn, 4, qmb[:], d_qmb)
                    for h in range(4):
                        P.op("pe", lambda e, h=h: e.transpose(out=pT[:, h, :], in_=qmb[:, h * 128:(h + 1) * 128], identity=idb[:]),
                             reads=[d_qmb, d_idb], writes=[d_pT])
                    P.op("act", lambda e, it=it: e.copy(out=kmT[:, :, it * 128:(it + 1) * 128], in_=pT[:, 0:4, :]), reads=[d_pT], writes=[d_kmT])
                    P.op("act", lambda e, it=it: e.copy(out=vm[:, it, :], in_=pG[1][:, :]), reads=[d_pG[1]], writes=[d_vm])

            wqm = sb("wqm", [128, 8, 512], BF16)
            wg = sb("wg", [128, 8, 3072], BF16)
            woa = sb("woa", [128, 4, 1024], BF16)
            wos = sb("wos", [128, 8, 1024], BF16)
            wom = sb("wom", [128, 4, 1024], BF16)
            wout = sb("wout", [128, 8, 1024], BF16)
            wr = sb("wr", [128, 8, 36], F32)
            for k in range(8):
                rows = slice(k * 128, (k + 1) * 128)
                P.dma("pool", lambda e, k=k, rows=rows: e.dma_start(out=wqm[:, k, :], in_=w_in[rows, C_QM:C_QM + 512]), writes=[d_w])
                for j in range(2):
                    P.dma("pool", lambda e, k=k, rows=rows, j=j: e.dma_start(out=wg[:, k, j * 1536:(j + 1) * 1536], in_=w_in[rows, C_G + j * 1536:C_G + (j + 1) * 1536]), writes=[d_w])
                P.dma("pool", lambda e, k=k, rows=rows: e.dma_start(out=wos[:, k, :], in_=wos_d[rows, :]), writes=[d_w])
                P.dma("pool", lambda e, k=k, rows=rows: e.dma_start(out=wout[:, k, :], in_=wout_d[rows, :]), writes=[d_w])
                P.dma("sp", lambda e, k=k, rows=rows: e.dma_start(out=wr[:, k, :], in_=wr_d[rows, :]), writes=[d_w])
            for k in range(4):
                rows = slice(k * 128, (k + 1) * 128)
                P.dma("pool", lambda e, k=k, rows=rows: e.dma_start(out=woa[:, k, :], in_=woa_d[rows, :]), writes=[d_w])
                P.dma("pool", lambda e, k=k, rows=rows: e.dma_start(out=wom[:, k, :], in_=wom_d[rows, :]), writes=[d_w])
            def loads(tj):
                xj = tj % 2
                rj = tj * 128
                P.dma("sp", lambda e: e.dma_start(out=xt[xj][:], in_=xown[rj:rj + 128, :]), writes=[d_xt[xj]])
                P.dma("sp", lambda e: e.dma_start(out=oat[xj][:], in_=oa_d[rj:rj + 128, :]), reads=[d_oad], writes=[d_oat[xj]])
                P.dma("sp", lambda e: e.dma_start(out=ost[xj][:], in_=os_d[rj:rj + 128, :]), reads=[d_osd], writes=[d_ost[xj]])

            for ti in range(ntiles):
                r0 = ti * 128
                hbuf = ti % 2
                xb = ti % 2
                if ti == 0:
                    loads(0)
                if ti + 1 < ntiles:
                    loads(ti + 1)
                P.op("act", lambda e, xb=xb: e.activation(out=junk[:], in_=xt[xb][:], func=AF.Square, accum_out=ssq[:]), reads=[d_xt[xb]], writes=[d_junk, d_ssq])
                P.op("act", lambda e: e.activation(out=rstd[:], in_=ssq[:], func=AF.Sqrt, bias=EPS, scale=1.0 / D), reads=[d_ssq], writes=[d_rstd])
                P.op("dve", lambda e: e.reciprocal(out=rstd[:], in_=rstd[:]), reads=[d_rstd], writes=[d_rstd])
                P.op("dve", lambda e, xb=xb: e.scalar_tensor_tensor(out=hb[:], in0=xt[xb][:], scalar=rstd[:, 0:1], in1=gm[:], op0=ALU.mult, op1=ALU.mult),
                     reads=[d_xt[xb], d_rstd, d_c], writes=[d_hb])
                for k in range(8):
                    P.op("pe", lambda e, k=k: e.transpose(out=pT[:, k, :], in_=hb[:, k * 128:(k + 1) * 128], identity=idb[:]), reads=[d_hb, d_idb], writes=[d_pT])
                P.op("act", lambda e: e.copy(out=hT[:], in_=pT[:]), reads=[d_pT], writes=[d_hT])
                for k in range(8):
                    P.op("pe", lambda e, k=k: e.matmul(pG[0][:, :], lhsT=hT[:, k, :], rhs=wqm[:, k, :], start=(k == 0), stop=(k == 7)),
                         reads=[d_hT, d_w], writes=[d_pG[0]])
                head_norm(pG[0][:, :], d_pG[0], mqn, 4, qmb[:], d_qmb)
                for g6 in range(6):
                    bk = g6 % 3
                    for k in range(8):
                        P.op("pe", lambda e, g6=g6, k=k, bk=bk: e.matmul(pG[bk][:, :], lhsT=hT[:, k, :], rhs=wg[:, k, g6 * 512:(g6 + 1) * 512], start=(k == 0), stop=(k == 7)),
                             reads=[d_hT, d_w], writes=[d_pG[bk]])
                    P.op("act", lambda e, g6=g6, bk=bk: e.activation(out=gs[:, g6 * 512:(g6 + 1) * 512], in_=pG[bk][:, :], func=AF.Sigmoid),
                         reads=[d_pG[bk]], writes=[d_gs])
                for h in range(4):
                    P.op("pe", lambda e, h=h: e.transpose(out=pT[:, h, :], in_=qmb[:, h * 128:(h + 1) * 128], identity=idb[:]), reads=[d_qmb, d_idb], writes=[d_pT])
                P.op("act", lambda e: e.copy(out=qmT[:], in_=pT[:, 0:4, :]), reads=[d_pT], writes=[d_qmT])
                for h in range(4):
                    for mt in range(2):
                        bank = pms[h // 2]
                        c0 = ((h % 2) * 2 + mt) * 128
                        P.op("pe", lambda e, h=h, mt=mt, bank=bank, c0=c0: e.matmul(bank[:, c0:c0 + 128], lhsT=kmT[:, h, mt * 128:(mt + 1) * 128], rhs=qmT[:, h, :],
                                                                                  start=True, stop=True), reads=[d_kmT, d_qmT], writes=[d_pms[h // 2]])
                for hp in range(2):
                    P.op("act", lambda e, hp=hp: e.activation(out=pm[:, hp * 2:(hp + 1) * 2, :, :].rearrange("p a b c -> p (a b c)"), in_=pms[hp][:, :], func=AF.Exp,
                                                              scale=float(128 ** -0.5)), reads=[d_pms[hp]], writes=[d_pm])
                for h in range(4):
                    for mt in range(2):
                        P.op("pe", lambda e, h=h, mt=mt: e.matmul(pmo[:, h * 128:(h + 1) * 128], lhsT=vm[:, mt, h * 128:(h + 1) * 128], rhs=pm[:, h, mt, :],
                                                                 start=(mt == 0), stop=(mt == 1)), reads=[d_vm, d_pm], writes=[d_pmo])
                for mt in range(2):
                    P.op("pe", lambda e, mt=mt: e.matmul(pG[1][:, :].rearrange("p (h t) -> p h t", h=4), lhsT=onesb[:], rhs=pm[:, :, mt, :],
                                                        start=(mt == 0), stop=(mt == 1)), reads=[d_c, d_pm], writes=[d_pG[1]])
                P.op("dve", lambda e: e.reciprocal(out=rcp[:], in_=pG[1][:, :]), reads=[d_pG[1]], writes=[d_rcp])
                P.op("dve", lambda e: e.tensor_tensor(out=omT[:].rearrange("p h t -> p (h t)"), in0=pmo[:, :], in1=rcp[:], op=ALU.mult),
                     reads=[d_pmo, d_rcp], writes=[d_omT])
                for k in range(4):
                    P.op("pe", lambda e, xb=xb, k=k: e.transpose(out=pT[:, k, :], in_=oat[xb][:, k * 128:(k + 1) * 128], identity=idb[:]), reads=[d_oat[xb], d_idb], writes=[d_pT])
                P.op("act", lambda e: e.copy(out=oaT[:], in_=pT[:, 0:4, :]), reads=[d_pT], writes=[d_oaT])
                for k in range(8):
                    P.op("pe", lambda e, xb=xb, k=k: e.transpose(out=pT[:, k, :], in_=ost[xb][:, k * 128:(k + 1) * 128], identity=idb[:]), reads=[d_ost[xb], d_idb], writes=[d_pT])
                P.op("act", lambda e: e.copy(out=osT[:], in_=pT[:]), reads=[d_pT], writes=[d_osT])
                for hv in range(2):
                    cs_ = slice(hv * 512, (hv + 1) * 512)
                    for k in range(4):
                        P.op("pe", lambda e, k=k, cs_=cs_: e.matmul(pG[0][:, :], lhsT=oaT[:, k, :], rhs=woa[:, k, cs_], start=(k == 0), stop=(k == 3)),
                             reads=[d_oaT, d_w], writes=[d_pG[0]])
                    for k in range(8):
                        P.op("pe", lambda e, k=k, cs_=cs_: e.matmul(pG[1][:, :], lhsT=osT[:, k, :], rhs=wos[:, k, cs_], start=(k == 0), stop=(k == 7)),
                             reads=[d_osT, d_w], writes=[d_pG[1]])
                    for k in range(4):
                        P.op("pe", lambda e, k=k, cs_=cs_: e.matmul(pG[2][:, :], lhsT=omT[:, k, :], rhs=wom[:, k, cs_], start=(k == 0), stop=(k == 3)),
                             reads=[d_omT, d_w], writes=[d_pG[2]])
                    P.op("dve", lambda e, hv=hv: e.tensor_tensor(out=mg[:], in0=pG[0][:, :], in1=gs[:, hv * 512:(hv + 1) * 512], op=ALU.mult),
                         reads=[d_pG[0], d_gs], writes=[d_mg])
                    P.op("dve", lambda e, hv=hv: e.tensor_tensor(out=tt[:], in0=pG[1][:, :], in1=gs[:, 1024 + hv * 512:1024 + (hv + 1) * 512], op=ALU.mult),
                         reads=[d_pG[1], d_gs], writes=[d_tt])
                    P.op("pool", lambda e: e.tensor_tensor(out=mg[:], in0=mg[:], in1=tt[:], op=ALU.add), reads=[d_mg, d_tt], writes=[d_mg])
                    P.op("dve", lambda e, hv=hv: e.tensor_tensor(out=tt[:], in0=pG[2][:, :], in1=gs[:, 2048 + hv * 512:2048 + (hv + 1) * 512], op=ALU.mult),
                         reads=[d_pG[2], d_gs], writes=[d_tt])
                    P.op("pool", lambda e, cs_=cs_: e.tensor_tensor(out=mgb[:, cs_], in0=mg[:], in1=tt[:], op=ALU.add), reads=[d_mg, d_tt], writes=[d_mgb])
                for k in range(8):
                    P.op("pe", lambda e, k=k: e.transpose(out=pT[:, k, :], in_=mgb[:, k * 128:(k + 1) * 128], identity=idb[:]), reads=[d_mgb, d_idb], writes=[d_pT])
                P.op("act", lambda e: e.copy(out=mgT[:], in_=pT[:]), reads=[d_pT], writes=[d_mgT])
                for hv in range(2):
                    cs_ = slice(hv * 512, (hv + 1) * 512)
                    for k in range(8):
                        P.op("pe", lambda e, k=k, cs_=cs_, hv=hv: e.matmul(pG[hv][:, :], lhsT=mgT[:, k, :], rhs=wout[:, k, cs_], start=(k == 0), stop=(k == 7)),
                             reads=[d_mgT, d_w], writes=[d_pG[hv]])
                    P.op("dve", lambda e, xb=xb, cs_=cs_, hv=hv: e.tensor_tensor(out=x1[:, cs_], in0=pG[hv][:, :], in1=xt[xb][:, cs_], op=ALU.add),
                         reads=[d_pG[hv], d_xt[xb]], writes=[d_x1])
                P.dma("sp", lambda e, r0=r0: e.dma_start(out=x1_d[r0:r0 + 128, :], in_=x1[:]), reads=[d_x1], writes=[d_x1d])
                if stage < 2:
                    continue
                P.op("act", lambda e: e.activation(out=junk[:], in_=x1[:], func=AF.Square, accum_out=ssq[:]), reads=[d_x1], writes=[d_junk, d_ssq])
                P.op("act", lambda e: e.activation(out=rstd[:], in_=ssq[:], func=AF.Sqrt, bias=EPS, scale=1.0 / D), reads=[d_ssq], writes=[d_rstd])
                P.op("dve", lambda e: e.reciprocal(out=rstd[:], in_=rstd[:]), reads=[d_rstd], writes=[d_rstd])
                P.op("dve", lambda e: e.scalar_tensor_tensor(out=h2f[:], in0=x1[:], scalar=rstd[:, 0:1], in1=gf[:], op0=ALU.mult, op1=ALU.mult),
                     reads=[d_x1, d_rstd, d_c], writes=[d_h2f])
                P.op("pool", lambda e, hbuf=hbuf: e.tensor_copy(out=h2b[hbuf][:], in_=h2f[:]), reads=[d_h2f], writes=[d_h2b[hbuf]])
                for half in range(2):
                    for k4 in range(4):
                        k = half * 4 + k4
                        P.op("pe", lambda e, k=k, k4=k4: e.transpose(out=pF[:, k4 * 128:(k4 + 1) * 128], in_=h2f[:, k * 128:(k + 1) * 128], identity=idf[:]),
                             reads=[d_h2f, d_idf], writes=[d_pF])
                    P.op("act", lambda e, half=half: e.copy(out=h2T[:, half * 4:(half + 1) * 4, :].rearrange("p a b -> p (a b)"), in_=pF[:, :]), reads=[d_pF], writes=[d_h2T])
                for k in range(8):
                    P.op("pe", lambda e, k=k: e.matmul(pF[:, 0:36], lhsT=h2T[:, k, :], rhs=wr[:, k, :], start=(k == 0), stop=(k == 7)), reads=[d_h2T, d_w], writes=[d_pF])
                P.op("act", lambda e: e.copy(out=lg[:], in_=pF[:, 0:36]), reads=[d_pF], writes=[d_lg])
                P.op("dve", lambda e: e.tensor_reduce(out=sm[:, 0:1], in_=lg[:, 0:4], axis=AX.X, op=ALU.max), reads=[d_lg], writes=[d_sm])
                P.op("dve", lambda e: e.tensor_scalar(out=sm[:, 1:2], in0=sm[:, 0:1], scalar1=-1.0, scalar2=None, op0=ALU.mult), reads=[d_sm], writes=[d_sm])
                P.op("act", lambda e: e.activation(out=sm[:, 8:12], in_=lg[:, 0:4], func=AF.Exp, bias=sm[:, 1:2], accum_out=sm[:, 2:3]), reads=[d_lg, d_sm], writes=[d_sm])
                P.op("dve", lambda e: e.reciprocal(out=sm[:, 3:4], in_=sm[:, 2:3]), reads=[d_sm], writes=[d_sm])
                P.op("dve", lambda e: e.tensor_scalar(out=goh[:], in0=lg[:, 0:4], scalar1=sm[:, 0:1], scalar2=None, op0=ALU.is_ge), reads=[d_lg, d_sm], writes=[d_goh])
                P.op("dve", lambda e: e.tensor_tensor(out=lsel[:].rearrange("p (g e) -> p g e", g=4), in0=lg[:, 4:36].rearrange("p (g e) -> p g e", g=4),
                                                      in1=goh[:].unsqueeze(2).to_broadcast([128, 4, 8]), op=ALU.mult), reads=[d_lg, d_goh], writes=[d_lsel])
                P.op("dve", lambda e: e.tensor_reduce(out=les[:], in_=lsel[:].rearrange("p (g e) -> p e g", g=4), axis=AX.X, op=ALU.add), reads=[d_lsel], writes=[d_les])
                P.op("dve", lambda e: e.max(out=t8[:], in_=les[:]), reads=[d_les], writes=[d_t8])
                P.op("dve", lambda e: e.tensor_tensor(out=sm[:, 4:5], in0=t8[:, 1:2], in1=t8[:, 0:1], op=ALU.subtract), reads=[d_t8, d_sm], writes=[d_sm])
                P.op("act", lambda e: e.activation(out=sm[:, 5:6], in_=sm[:, 4:5], func=AF.Exp), reads=[d_sm], writes=[d_sm])
                P.op("dve", lambda e: e.tensor_scalar(out=sm[:, 6:7], in0=sm[:, 5:6], scalar1=1.0, scalar2=None, op0=ALU.add), reads=[d_sm], writes=[d_sm])
                P.op("dve", lambda e: e.reciprocal(out=sm[:, 6:7], in_=sm[:, 6:7]), reads=[d_sm], writes=[d_sm])
                P.op("dve", lambda e: e.tensor_tensor(out=sm[:, 7:8], in0=sm[:, 5:6], in1=sm[:, 6:7], op=ALU.mult), reads=[d_sm], writes=[d_sm])
                P.op("dve", lambda e, ti=ti: e.tensor_scalar(out=combs[:, ti, :], in0=sm[:, 6:8], scalar1=sm[:, 3:4], scalar2=None, op0=ALU.mult), reads=[d_sm], writes=[d_combs])
                for j in range(2):
                    P.op("dve", lambda e, j=j: e.tensor_scalar(out=oh[:, j * 32:(j + 1) * 32], in0=lg[:, 4:36], scalar1=t8[:, j:j + 1], scalar2=None, op0=ALU.is_equal),
                         reads=[d_lg, d_t8], writes=[d_oh])
                    P.op("dve", lambda e, j=j: e.tensor_tensor(out=oh[:, j * 32:(j + 1) * 32].rearrange("p (g e) -> p g e", g=4), in0=oh[:, j * 32:(j + 1) * 32].rearrange("p (g e) -> p g e", g=4),
                                                               in1=goh[:].unsqueeze(2).to_broadcast([128, 4, 8]), op=ALU.mult), reads=[d_oh, d_goh], writes=[d_oh])
                P.op("dve", lambda e: e.tensor_tensor(out=ohs[:], in0=oh[:, 0:32], in1=oh[:, 32:64], op=ALU.add), reads=[d_oh], writes=[d_ohs])
                P.op("pe", lambda e: e.matmul(pF[:, 64:96], lhsT=tri[:], rhs=ohs[:], start=True, stop=True), reads=[d_c, d_ohs], writes=[d_pF])
                P.op("pe", lambda e: e.matmul(pF[:, 128:160], lhsT=onesf[:], rhs=ohs[:], start=True, stop=True), reads=[d_c, d_ohs], writes=[d_pF])
                P.op("dve", lambda e: e.tensor_tensor(out=posf[:], in0=pF[:, 64:96], in1=ohs[:], op=ALU.subtract), reads=[d_pF, d_ohs], writes=[d_posf])
                P.op("dve", lambda e: e.tensor_tensor(out=posf[:], in0=posf[:], in1=base[:], op=ALU.add), reads=[d_posf, d_base], writes=[d_posf])
                P.op("dve", lambda e: e.tensor_tensor(out=posf[:], in0=posf[:], in1=ecap[:], op=ALU.add), reads=[d_posf, d_c], writes=[d_posf])
                P.op("dve", lambda e: e.tensor_tensor(out=base[:], in0=base[:], in1=pF[:, 128:160], op=ALU.add), reads=[d_base, d_pF, d_posf], writes=[d_base])
                for j in range(2):
                    P.op("dve", lambda e, j=j: e.tensor_tensor(out=oh[:, j * 32:(j + 1) * 32], in0=oh[:, j * 32:(j + 1) * 32], in1=posf[:], op=ALU.mult), reads=[d_oh, d_posf], writes=[d_oh])
                    P.op("dve", lambda e, j=j: e.tensor_reduce(out=idf2[:, j:j + 1], in_=oh[:, j * 32:(j + 1) * 32], axis=AX.X, op=ALU.add), reads=[d_oh], writes=[d_idf2])
                P.op("dve", lambda e, ti=ti: e.tensor_copy(out=idxs[:, ti, :], in_=idf2[:]), reads=[d_idf2], writes=[d_idxs])
                for j in range(2):
                    P.dma("pool", lambda e, ti=ti, j=j, hbuf=hbuf: e.indirect_dma_start(out=xbuf[:, :], out_offset=bass.IndirectOffsetOnAxis(ap=idxs[:, ti, j:j + 1], axis=0),
                                                                                      in_=h2b[hbuf][:], in_offset=None),
                          reads=[d_h2b[hbuf], d_idxs], writes=[d_xbuf])

        if stage < 3:
            return
        if hasattr(P, 'barrier'):
            P.barrier()
        with contextlib.ExitStack() as st:
            def sb(name, shape, dt):
                return st.enter_context(nc.sbuf_tensor("d_" + name, shape, dt))

            def ps(name, shape, dt=F32):
                return st.enter_context(nc.psum_tensor(name, shape, dt))
            NW = 2
            wge = [sb("wge%d" % i, [128, 8, 512], BF16) for i in range(NW)]; d_wge = [Dep() for _ in range(NW)]
            wue = [sb("wue%d" % i, [128, 8, 512], BF16) for i in range(NW)]; d_wue = [Dep() for _ in range(NW)]
            wde = [sb("wde%d" % i, [128, 4, 1024], BF16) for i in range(NW)]; d_wde = [Dep() for _ in range(NW)]
            xe = [sb("xe%d" % i, [128, 3, D], BF16) for i in range(NW)]; d_xe = [Dep() for _ in range(NW)]
            xeT = sb("xeT", [128, 8, CAP], BF16); d_xeT = Dep()
            sg = sb("sg", [128, CAP], F32); d_sg = Dep()
            hTe = sb("hTe", [128, 4, CAP], BF16); d_hTe = Dep()
            ye = [sb("ye%d" % i, [128, 3, D], F32) for i in range(NW)]; d_ye = [Dep() for _ in range(NW)]
            pT = ps("pTe", [128, 8, 128], BF16); d_pT = Dep()
            pg_ = [ps("pge%d" % i, [128, 512], F32) for i in range(2)]; d_pg = [Dep() for _ in range(2)]
            pu_ = [ps("pue%d" % i, [128, 512], F32) for i in range(2)]; d_pu = [Dep() for _ in range(2)]
            py_ = [ps("pye%d" % i, [128, 512], F32) for i in range(2)]; d_py = [Dep() for _ in range(2)]
            for ie, ex in enumerate(experts):
                b = ie % NW
                P.dma("pool", lambda e, b=b, ex=ex: e.dma_start(out=wge[b][:], in_=wgate_d[ex].rearrange("(k p) f -> p k f", p=128)), writes=[d_wge[b]])
                P.dma("pool", lambda e, b=b, ex=ex: e.dma_start(out=wue[b][:], in_=wup_d[ex].rearrange("(k p) f -> p k f", p=128)), writes=[d_wue[b]])
                P.dma("pool", lambda e, b=b, ex=ex: e.dma_start(out=wde[b][:], in_=wdown_d[ex].rearrange("(k p) f -> p k f", p=128)), writes=[d_wde[b]])
                P.dma("sp", lambda e, b=b, ex=ex: e.dma_start(out=xe[b][:], in_=xbuf[ex * CAP:(ex + 1) * CAP, :].rearrange("(s p) d -> p s d", p=128)),
                      reads=[d_xbuf], writes=[d_xe[b]])
                for s in range(3):
                    for k in range(8):
                        P.op("pe", lambda e, b=b, s=s, k=k: e.transpose(out=pT[:, k, :], in_=xe[b][:, s, k * 128:(k + 1) * 128], identity=idb[:]),
                             reads=[d_xe[b], d_idb], writes=[d_pT])
                    P.op("act", lambda e, s=s: e.copy(out=xeT[:, :, s * 128:(s + 1) * 128], in_=pT[:]), reads=[d_pT], writes=[d_xeT])
                for ft in range(4):
                    pb = ft % 2
                    for k in range(8):
                        P.op("pe", lambda e, b=b, ft=ft, k=k, pb=pb: e.matmul(pg_[pb][:, 0:CAP], lhsT=wge[b][:, k, ft * 128:(ft + 1) * 128], rhs=xeT[:, k, :],
                                                                            start=(k == 0), stop=(k == 7)), reads=[d_wge[b], d_xeT], writes=[d_pg[pb]])
                    for k in range(8):
                        P.op("pe", lambda e, b=b, ft=ft, k=k, pb=pb: e.matmul(pu_[pb][:, 0:CAP], lhsT=wue[b][:, k, ft * 128:(ft + 1) * 128], rhs=xeT[:, k, :],
                                                                            start=(k == 0), stop=(k == 7)), reads=[d_wue[b], d_xeT], writes=[d_pu[pb]])
                    P.op("act", lambda e, pb=pb: e.activation(out=sg[:], in_=pg_[pb][:, 0:CAP], func=AF.Silu), reads=[d_pg[pb]], writes=[d_sg])
                    P.op("dve", lambda e, ft=ft, pb=pb: e.tensor_tensor(out=hTe[:, ft, :], in0=sg[:], in1=pu_[pb][:, 0:CAP], op=ALU.mult),
                         reads=[d_sg, d_pu[pb]], writes=[d_hTe])
                for s in range(3):
                    for hv in range(2):
                        for ft in range(4):
                            P.op("pe", lambda e, b=b, s=s, hv=hv, ft=ft: e.matmul(py_[hv][:, :], lhsT=hTe[:, ft, s * 128:(s + 1) * 128], rhs=wde[b][:, ft, hv * 512:(hv + 1) * 512],
                                                                                start=(ft == 0), stop=(ft == 3)), reads=[d_hTe, d_wde[b]], writes=[d_py[hv]])
                        P.op("act" if hv == 0 else "dve", (lambda e, b=b, s=s, hv=hv: e.copy(out=ye[b][:, s, hv * 512:(hv + 1) * 512], in_=py_[hv][:, :])) if hv == 0 else
                             (lambda e, b=b, s=s, hv=hv: e.tensor_copy(out=ye[b][:, s, hv * 512:(hv + 1) * 512], in_=py_[hv][:, :])),
                             reads=[d_py[hv]], writes=[d_ye[b]])
                P.dma("sp", lambda e, b=b, ex=ex: e.dma_start(out=ybuf[ex * CAP:(ex + 1) * CAP, :].rearrange("(s p) d -> p s d", p=128), in_=ye[b][:]),
                      reads=[d_ye[b]], writes=[d_ybuf])

        if hasattr(P, 'barrier'):
            P.barrier()
        with contextlib.ExitStack() as st:
            def sb(name, shape, dt):
                return st.enter_context(nc.sbuf_tensor("d_" + name, shape, dt))
            NF = 2
            x1t = [sb("x1t%d" % i, [128, D], F32) for i in range(NF)]; d_x1t = [Dep() for _ in range(NF)]
            y1 = [sb("y1t%d" % i, [128, D], F32) for i in range(NF)]; d_y1 = [Dep() for _ in range(NF)]
            y2 = [sb("y2t%d" % i, [128, D], F32) for i in range(NF)]; d_y2 = [Dep() for _ in range(NF)]
            ot = [sb("ot%d" % i, [128, D], F32) for i in range(NF)]; d_ot = [Dep() for _ in range(NF)]
            for ti in range(ntiles):
                b = ti % NF
                r0 = ti * 128
                P.dma("sp", lambda e, b=b, r0=r0: e.dma_start(out=x1t[b][:], in_=x1_d[r0:r0 + 128, :]), reads=[d_x1d], writes=[d_x1t[b]])
                P.dma("pool", lambda e, b=b, ti=ti: e.indirect_dma_start(out=y1[b][:], out_offset=None, in_=ybuf[:, :],
                                                                        in_offset=bass.IndirectOffsetOnAxis(ap=idxs[:, ti, 0:1], axis=0)),
                      reads=[d_ybuf, d_idxs], writes=[d_y1[b]])
                P.dma("pool", lambda e, b=b, ti=ti: e.indirect_dma_start(out=y2[b][:], out_offset=None, in_=ybuf[:, :],
                                                                        in_offset=bass.IndirectOffsetOnAxis(ap=idxs[:, ti, 1:2], axis=0)),
                      reads=[d_ybuf, d_idxs], writes=[d_y2[b]])
                P.op("dve", lambda e, b=b, ti=ti: e.scalar_tensor_tensor(out=ot[b][:], in0=y1[b][:], scalar=combs[:, ti, 0:1], in1=x1t[b][:], op0=ALU.mult, op1=ALU.add),
                     reads=[d_y1[b], d_combs, d_x1t[b]], writes=[d_ot[b]])
                P.op("dve", lambda e, b=b, ti=ti: e.scalar_tensor_tensor(out=ot[b][:], in0=y2[b][:], scalar=combs[:, ti, 1:2], in1=ot[b][:], op0=ALU.mult, op1=ALU.add),
                     reads=[d_y2[b], d_combs, d_ot[b]], writes=[d_ot[b]])
                P.dma("sp", lambda e, b=b, r0=r0: e.dma_start(out=out_d[r0:r0 + 128, :], in_=ot[b][:]), reads=[d_ot[b]], writes=[d_out])


def host_consts_d(z):
    tri = (np.arange(128)[:, None] <= np.arange(128)[None, :]).astype(np.float32)
    return dict(mqn=np.tile(z['mem_q_norm'], 4)[None, :], mkn=np.tile(z['mem_k_norm'], 4)[None, :],
                w_r=np.ascontiguousarray(np.concatenate([z['w_router_group'], z['w_router_expert']], axis=1)),
                trif=tri, ecap=(np.arange(32, dtype=np.float32) * CAP)[None, :], ident=np.eye(128, dtype=np.float32))


def build_full():
    nc = bass.Bass("TRN2", target_bir_lowering=False)
    P = Prog(nc)

    def di(name, shape, dt=F32):
        return nc.dram_tensor(name, shape, dt, kind="ExternalInput")
    xin = di("xin", [TP + T, D]); w_in = di("w_in", [D, IN_COLS]); g_mix = di("g_mix", [1, D])
    qkn = di("qkn", [2, 512]); cs = di("cs", [TP + T, 64]); ident_d = di("ident", [128, 128])
    e_d = di("e_d", [32, NK], BF16); vq_d = di("vq", [1, 1024]); nb_d = di("nb", [1, 1024]); oq_d = di("oq", [1, 1024])
    tri_d = di("tri", [128, 128], BF16)
    cwl_d = di("cwl", [128, 16, 4]); cbl_d = di("cbl", [128, 16]); dtb_d = di("dtb", [1, 16]); alog_d = di("alog", [1, 16])
    dsk_d = di("dsk", [1, 16]); ssdn_d = di("ssdn", [1, D]); tri2_d = di("tri2", [128, 2, 256]); sel_d = di("sel_d", [16, 16, 128])
    tb_d = di("tb_d", [128, 384]); pflag_d = di("pflag", [1, 1])
    mem_d = di("mem", [256, D]); gmem_d = di("g_mem", [1, D]); wmemkv = di("w_mem_kv", [D, 1024])
    mqn_d = di("mqn", [1, 512]); mkn_d = di("mkn", [1, 512])
    woa_d = di("w_o_moba", [512, D]); wos_d = di("w_o_ssd", [D, D]); wom_d = di("w_o_mem", [512, D]); wout_d = di("w_out", [D, D])
    gffn_d = di("g_ffn", [1, D]); wr_d = di("w_r", [D, 36])
    wgate_d = di("w_gate", [NE, D, 512]); wup_d = di("w_up", [NE, D, 512]); wdown_d = di("w_down", [NE, 512, D])
    trif_d = di("trif", [128, 128]); ecap_d = di("ecap", [1, 32])
    out_d = nc.dram_tensor("out", [T, D], F32, kind="ExternalOutput")
    kT_d = nc.dram_tensor("kT_d", [512, NK], BF16)
    v_d = nc.dram_tensor("v_d", [NK, 512], BF16)
    qT_d = nc.dram_tensor("qT_d", [512, T], BF16)
    oa_d = nc.dram_tensor("oa_d", [T, 512], BF16)
    os_d = nc.dram_tensor("os_d", [T, D], BF16)
    x1_d = nc.dram_tensor("x1_d", [T, D], F32)
    xbuf = nc.dram_tensor("xbuf", [NE * CAP, D], BF16)
    ybuf = nc.dram_tensor("ybuf", [NE * CAP, D], F32)

    with contextlib.ExitStack() as st0:
        idf = st0.enter_context(nc.sbuf_tensor("idf", [128, 128], F32)); d_idf = Dep()
        idb = st0.enter_context(nc.sbuf_tensor("idb", [128, 128], BF16)); d_idb = Dep()
        P.dma("sp", lambda e: e.dma_start(out=idf[:], in_=ident_d[:, :]), writes=[d_idf])
        P.op("dve", lambda e: e.tensor_copy(out=idb[:], in_=idf[:]), reads=[d_idf], writes=[d_idb])
        with contextlib.ExitStack() as st:
            phase_a(nc, P, st, xin, w_in, g_mix, qkn, cs, idb, d_idb, kT_d, v_d, qT_d)
        P.barrier()
        with contextlib.ExitStack() as st:
            oa = st.enter_context(nc.sbuf_tensor("s_oa", [128, 32, 512], BF16)); d_oa = Dep()
            phase_b(nc, P, st, kT_d, v_d, qT_d, e_d, vq_d, nb_d, oq_d, tri_d, idb, d_idb, oa, d_oa)
            P.dma("sp", lambda e: e.dma_start(out=oa_d[:, :].rearrange("(t p) c -> p t c", p=128), in_=oa[:]), reads=[d_oa])
        P.barrier()
        with contextlib.ExitStack() as st:
            phase_c(nc, P, st, xin, w_in, g_mix, cwl_d, cbl_d, dtb_d, alog_d, dsk_d, ssdn_d, tri2_d, sel_d, tb_d, pflag_d,
                    idb, d_idb, os_d, Dep())
        P.barrier()
        phase_def(nc, P, xin[TP:TP + T, :], w_in, g_mix, mem_d, gmem_d, wmemkv, mqn_d, mkn_d, woa_d, wos_d, wom_d, wout_d, gffn_d, wr_d,
                  wgate_d, wup_d, wdown_d, trif_d, ecap_d, oa_d, Dep(), os_d, Dep(), x1_d, xbuf, ybuf, out_d, idb, d_idb, idf, d_idf)
        P.emit()
    return nc


_NC_CACHE = {}


def kernel(x, mem, g_mix, w_in, moba_q_norm, moba_k_norm, conv_w, conv_b, dt_bias, a_log, d_skip, ssd_norm, g_mem, w_mem_kv,
           mem_q_norm, mem_k_norm, w_o_moba, w_o_ssd, w_o_mem, w_out, g_ffn, w_router_group, w_router_expert, w_gate, w_up, w_down):
    f32 = np.float32
    A = lambda a: np.ascontiguousarray(np.asarray(a, dtype=f32))
    x = A(x); mem = A(mem)
    z = dict(conv_w=A(conv_w), conv_b=A(conv_b), dt_bias=A(dt_bias), a_log=A(a_log), d_skip=A(d_skip), ssd_norm=A(ssd_norm),
             mem_q_norm=A(mem_q_norm), mem_k_norm=A(mem_k_norm), w_router_group=A(w_router_group), w_router_expert=A(w_router_expert))
    if "nc" not in _NC_CACHE:
        _NC_CACHE["nc"] = build_full()
    nc = _NC_CACHE["nc"]
    pos = np.arange(2 * T, dtype=f32)
    inv = (10000.0 ** (-np.arange(32, dtype=f32) / 32)).astype(f32)
    ang = pos[:, None] * inv[None, :]
    cs_full = np.concatenate([np.cos(ang), np.sin(ang)], axis=1).astype(f32)
    qkn = np.stack([np.tile(A(moba_q_norm), 8), np.tile(A(moba_k_norm), 8)]).astype(f32)
    shared = dict(w_in=A(w_in), g_mix=A(g_mix)[None, :], qkn=qkn, ident=np.eye(128, dtype=f32),
                  g_mem=A(g_mem)[None, :], w_mem_kv=A(w_mem_kv), w_o_moba=A(w_o_moba), w_o_ssd=A(w_o_ssd), w_o_mem=A(w_o_mem),
                  w_out=A(w_out), g_ffn=A(g_ffn)[None, :], w_gate=A(w_gate), w_up=A(w_up), w_down=A(w_down))
    shared.update(host_consts_d(z))
    in_maps = []
    for c in range(8):
        bb, hh = c // 2, c % 2
        if hh == 0:
            xin = np.concatenate([np.zeros((TP, D), f32), x[bb, :T]], 0)
            csc = np.concatenate([cs_full[:TP], cs_full[:T]], 0)
        else:
            xin = x[bb]
            csc = cs_full
        m = dict(shared)
        m.update(xin=np.ascontiguousarray(xin), cs=np.ascontiguousarray(csc), mem=mem[bb])
        m.update(host_consts(hh))
        m.update(host_consts_c(z, hh))
        in_maps.append(m)
    res = run_bass_kernel_spmd(nc, in_maps, core_ids=list(range(8)))
    out = np.empty((4, 2 * T, D), f32)
    for c in range(8):
        bb, hh = c // 2, c % 2
        out[bb, hh * T:(hh + 1) * T] = np.asarray(res.results[c]["out"], dtype=f32)
    return out
```

```python
import contextlib
import numpy as np
import ml_dtypes
import concourse.bass as bass
import concourse.mybir as mybir
from concourse.bass_utils import run_bass_kernel_spmd

F32 = mybir.dt.float32
BF16 = mybir.dt.bfloat16
I32 = mybir.dt.int32
U32 = mybir.dt.uint32
AF = mybir.ActivationFunctionType
ALU = mybir.AluOpType
AX = mybir.AxisListType

T = 4096
TP = 4096
NK = TP + T
D = 1024
EPS = 1e-6
IN_COLS = 8208
BIGG = 30000.0
MB = 1000.0
NEG = -30000.0
C_Z, C_X, C_DT = 1536, 2560, 4608
C_QM, C_G = 4624, 5136
CAP = 384
NE = 32


class Dep:
    __slots__ = ("w", "r")

    def __init__(self):
        self.w = None
        self.r = {}


class Op:
    __slots__ = ("eng", "fn", "deps", "is_dma", "sig", "sigval", "dsem", "dval", "prev")

    def __init__(self, eng, fn, is_dma):
        self.eng = eng
        self.fn = fn
        self.is_dma = is_dma
        self.deps = []
        self.sig = False
        self.sigval = 0
        self.dsem = None
        self.dval = 0
        self.prev = None


class Prog:
    ENGS = ("pe", "act", "dve", "pool", "sp")

    def __init__(self, nc, ndma_sems=12):
        self.nc = nc
        self.ops = {e: [] for e in self.ENGS}
        self.ndma = {e: 0 for e in self.ENGS}
        self.dma_last = {}
        self.ndma_sems = ndma_sems
        self.all_dma = []

    def _add(self, o, reads, writes):
        deps = {}
        raw = set()
        for t in reads:
            if t.w is not None:
                deps[id(t.w)] = t.w
                raw.add(id(t.w))
        for t in writes:
            if t.w is not None:
                deps[id(t.w)] = t.w
            for r in t.r.values():
                deps[id(r)] = r
        for t in reads:
            key = id(o) if o.is_dma else o.eng
            t.r[key] = o
        for t in writes:
            t.w = o
            t.r = {}
        dl = []
        for d in deps.values():
            if d is o:
                continue
            if (not d.is_dma) and (not o.is_dma) and d.eng == "pe" and o.eng == "pe":
                continue
            if (not d.is_dma) and (not o.is_dma) and d.eng == o.eng and id(d) not in raw:
                continue
            dl.append(d)
            if not d.is_dma:
                d.sig = True
        o.deps = dl
        self.ops[o.eng].append(o)
        return o

    def op(self, eng, fn, reads=(), writes=()):
        return self._add(Op(eng, fn, False), reads, writes)

    def dma(self, eng, fn, reads=(), writes=()):
        o = Op(eng, fn, True)
        n = self.ndma[eng]
        self.ndma[eng] += 1
        slot = (eng, n % self.ndma_sems)
        o.dsem = slot
        o.prev = self.dma_last.get(slot)
        o.dval = (o.prev.dval if o.prev else 0) + 16
        self.dma_last[slot] = o
        self.all_dma.append(o)
        return self._add(o, reads, writes)


    def barrier(self):
        lasts = []
        for e in self.ENGS:
            for o in reversed(self.ops[e]):
                if (not o.is_dma) and o.fn is not None:
                    o.sig = True
                    lasts.append(o)
                    break
        dmas = list(self.dma_last.values())
        for e in self.ENGS:
            o = Op(e, None, False)
            o.deps = [d for d in lasts if d.eng != e] + dmas
            self.ops[e].append(o)

    def emit(self):
        nc = self.nc
        import contextlib
        with contextlib.ExitStack() as st:
            esem = {e: st.enter_context(nc.semaphore("S_" + e)) for e in self.ENGS}
            dsem = {}
            for e in self.ENGS:
                if self.ndma[e]:
                    for i in range(min(self.ndma_sems, self.ndma[e])):
                        dsem[(e, i)] = st.enter_context(nc.semaphore("D_%s_%d" % (e, i)))
            for e in self.ENGS:
                c = 0
                for o in self.ops[e]:
                    if (not o.is_dma) and o.sig and o.fn is not None:
                        c += 1
                        o.sigval = c
            block = st.enter_context(nc.Block())
            handles = {"pe": block.tensor, "act": block.scalar, "dve": block.vector,
                       "pool": block.gpsimd, "sp": block.sync}

            def make(e):
                def body(eng):
                    known = {}

                    def wait(sem, key, val):
                        if known.get(key, 0) < val:
                            eng.wait_ge(sem, val)
                            known[key] = val
                    for o in self.ops[e]:
                        for d in o.deps:
                            if d.is_dma:
                                wait(dsem[d.dsem], d.dsem, d.dval)
                            else:
                                wait(esem[d.eng], d.eng, d.sigval)
                        if o.is_dma:
                            if o.prev is not None:
                                wait(dsem[o.dsem], o.dsem, o.prev.dval)
                            o.fn(eng).then_inc(dsem[o.dsem], 16)
                        elif o.fn is not None:
                            ins = o.fn(eng)
                            if o.sig:
                                ins.then_inc(esem[e], 1)
                    if e == "sp":
                        for slot, o in self.dma_last.items():
                            wait(dsem[slot], slot, o.dval)
                return body
            for e in self.ENGS:
                if self.ops[e] or e == "sp":
                    handles[e](make(e))


def phase_a(nc, P, st, xin, w_in, g_mix, qkn, cs, idb, d_idb, kT_d, v_d, qT_d):
    def sb(name, shape, dt):
        return st.enter_context(nc.sbuf_tensor("a_" + name, shape, dt))

    def ps(name, shape, dt=F32):
        return st.enter_context(nc.psum_tensor("a_" + name, shape, dt))
    wq = sb("wq", [128, 8, 1536], BF16); d_wq = Dep()
    gm = sb("gm", [128, D], F32); d_gm = Dep()
    gq = sb("gq", [128, 2, 512], F32); d_gq = Dep()
    NB = 2
    xt = [sb("xt%d" % i, [128, D], F32) for i in range(NB)]; d_xt = [Dep() for _ in range(NB)]
    cst = [sb("cst%d" % i, [128, 64], F32) for i in range(NB)]; d_cst = [Dep() for _ in range(NB)]
    junk = sb("junk", [128, D], BF16); d_junk = Dep()
    ssq = sb("ssq", [128, 1], F32); d_ssq = Dep()
    rstd = sb("rstd", [128, 1], F32); d_rstd = Dep()
    hb = sb("hb", [128, D], BF16); d_hb = Dep()
    hT = [sb("hT%d" % i, [128, 8, 128], BF16) for i in range(NB)]; d_hT = [Dep() for _ in range(NB)]
    pT = ps("pT", [128, 8, 128], BF16); d_pT = Dep()
    pq = [[ps("pq%d_%d" % (j, i), [128, 512], F32) for i in range(3)] for j in range(2)]; d_pq = [[Dep() for _ in range(3)] for _ in range(2)]
    pkT = ps("pkT", [128, 4, 128], BF16); d_pkT = Dep()
    sq = sb("sq", [128, 512], F32); d_sq = Dep()
    hs = sb("hs", [128, 8], F32); d_hs = Dep()
    hr = sb("hr", [128, 8], F32); d_hr = Dep()
    qn = sb("qn", [128, 512], F32); d_qn = Dep()
    t1 = sb("t1", [128, 256], F32); d_t1 = Dep()
    t2 = sb("t2", [128, 256], F32); d_t2 = Dep()
    t3 = sb("t3", [128, 256], F32); d_t3 = Dep()
    t4 = sb("t4", [128, 256], F32); d_t4 = Dep()
    qr = sb("qr", [128, 512], BF16); d_qr = Dep()
    kTs = [sb("kTs%d" % i, [128, 4, 128], BF16) for i in range(NB)]; d_kTs = [Dep() for _ in range(NB)]
    vs = [sb("vs%d" % i, [128, 512], BF16) for i in range(NB)]; d_vs = [Dep() for _ in range(NB)]

    for k in range(8):
        P.dma("pool", lambda e, k=k: e.dma_start(out=wq[:, k, :], in_=w_in[k * 128:(k + 1) * 128, 0:1536]), writes=[d_wq])
    P.dma("sp", lambda e: e.dma_start(out=gm[:], in_=g_mix[0:1, :].partition_broadcast(128)), writes=[d_gm])
    for i in range(2):
        P.dma("sp", lambda e, i=i: e.dma_start(out=gq[:, i, :], in_=qkn[i:i + 1, :].partition_broadcast(128)), writes=[d_gq])
    P.op("dve", lambda e: e.tensor_scalar(out=gq[:, 0, :], in0=gq[:, 0, :], scalar1=0.125, scalar2=None, op0=ALU.mult),
         reads=[d_gq], writes=[d_gq])

    def qk_post(src_ps, d_src, which, b):
        P.op("act", lambda e: e.activation(out=sq[:], in_=src_ps[:], func=AF.Square), reads=[d_src], writes=[d_sq])
        P.op("dve", lambda e: e.tensor_reduce(out=hs[:], in_=sq[:].rearrange("p (h d) -> p h d", h=8), axis=AX.X, op=ALU.add),
             reads=[d_sq], writes=[d_hs])
        P.op("act", lambda e: e.activation(out=hr[:], in_=hs[:], func=AF.Sqrt, bias=EPS, scale=1.0 / 64), reads=[d_hs], writes=[d_hr])
        P.op("dve", lambda e: e.reciprocal(out=hr[:], in_=hr[:]), reads=[d_hr], writes=[d_hr])
        P.op("dve", lambda e: e.tensor_tensor(out=qn[:].rearrange("p (h d) -> p h d", h=8), in0=src_ps[:].rearrange("p (h d) -> p h d", h=8),
                                              in1=hr[:].unsqueeze(2).to_broadcast([128, 8, 64]), op=ALU.mult),
             reads=[d_src, d_hr], writes=[d_qn])
        P.op("pool", lambda e: e.tensor_tensor(out=qn[:], in0=qn[:], in1=gq[:, which, :], op=ALU.mult), reads=[d_qn, d_gq], writes=[d_qn])
        q3 = qn[:].rearrange("p (h d) -> p h d", h=8)
        q1 = q3[:, :, 0:32]
        q2 = q3[:, :, 32:64]
        cosb = cst[b][:, 0:32].unsqueeze(1).to_broadcast([128, 8, 32])
        sinb = cst[b][:, 32:64].unsqueeze(1).to_broadcast([128, 8, 32])
        v3 = lambda t: t[:].rearrange("p (h d) -> p h d", h=8)
        P.op("dve", lambda e: e.tensor_tensor(out=v3(t1), in0=q1, in1=cosb, op=ALU.mult), reads=[d_qn, d_cst[b]], writes=[d_t1])
        P.op("pool", lambda e: e.tensor_tensor(out=v3(t2), in0=q2, in1=sinb, op=ALU.mult), reads=[d_qn, d_cst[b]], writes=[d_t2])
        P.op("dve", lambda e: e.tensor_tensor(out=v3(t3), in0=q2, in1=cosb, op=ALU.mult), reads=[d_qn, d_cst[b]], writes=[d_t3])
        P.op("pool", lambda e: e.tensor_tensor(out=v3(t4), in0=q1, in1=sinb, op=ALU.mult), reads=[d_qn, d_cst[b]], writes=[d_t4])
        r3 = qr[:].rearrange("p (h d) -> p h d", h=8)
        P.op("dve", lambda e: e.tensor_tensor(out=r3[:, :, 0:32], in0=v3(t1), in1=v3(t2), op=ALU.subtract), reads=[d_t1, d_t2], writes=[d_qr])
        P.op("pool", lambda e: e.tensor_tensor(out=r3[:, :, 32:64], in0=v3(t3), in1=v3(t4), op=ALU.add), reads=[d_t3, d_t4], writes=[d_qr])

    def to_T_and_store(dst_dram, col0, b):
        for c in range(4):
            P.op("pe", lambda e, c=c: e.transpose(out=pkT[:, c, :], in_=qr[:, c * 128:(c + 1) * 128], identity=idb[:]),
                 reads=[d_qr, d_idb], writes=[d_pkT])
        P.op("act", lambda e: e.copy(out=kTs[b][:], in_=pkT[:]), reads=[d_pkT], writes=[d_kTs[b]])
        P.dma("sp", lambda e: e.dma_start(out=dst_dram[:, col0:col0 + 128].rearrange("(c p) t -> p c t", p=128), in_=kTs[b][:]),
              reads=[d_kTs[b]])

    def front(ti):
        b = ti % NB
        own = ti >= 32
        r0 = ti * 128
        P.dma("sp", lambda e: e.dma_start(out=xt[b][:], in_=xin[r0:r0 + 128, :]), writes=[d_xt[b]])
        P.dma("sp", lambda e: e.dma_start(out=cst[b][:], in_=cs[r0:r0 + 128, :]), writes=[d_cst[b]])
        P.op("act", lambda e: e.activation(out=junk[:], in_=xt[b][:], func=AF.Square, accum_out=ssq[:]), reads=[d_xt[b]], writes=[d_junk, d_ssq])
        P.op("act", lambda e: e.activation(out=rstd[:], in_=ssq[:], func=AF.Sqrt, bias=EPS, scale=1.0 / D), reads=[d_ssq], writes=[d_rstd])
        P.op("dve", lambda e: e.reciprocal(out=rstd[:], in_=rstd[:]), reads=[d_rstd], writes=[d_rstd])
        P.op("dve", lambda e: e.scalar_tensor_tensor(out=hb[:], in0=xt[b][:], scalar=rstd[:, 0:1], in1=gm[:], op0=ALU.mult, op1=ALU.mult),
             reads=[d_xt[b], d_rstd, d_gm], writes=[d_hb])
        for k in range(8):
            P.op("pe", lambda e, k=k: e.transpose(out=pT[:, k, :], in_=hb[:, k * 128:(k + 1) * 128], identity=idb[:]), reads=[d_hb, d_idb], writes=[d_pT])
        P.op("act", lambda e: e.copy(out=hT[b][:], in_=pT[:]), reads=[d_pT], writes=[d_hT[b]])
        groups = [0, 1, 2] if own else [1, 2]
        for g in groups:
            for k in range(8):
                P.op("pe", lambda e, g=g, k=k: e.matmul(pq[b][g][:], lhsT=hT[b][:, k, :], rhs=wq[:, k, g * 512:(g + 1) * 512], start=(k == 0), stop=(k == 7)),
                     reads=[d_hT[b], d_wq], writes=[d_pq[b][g]])

    def post(ti):
        b = ti % NB
        own = ti >= 32
        r0 = ti * 128
        qk_post(pq[b][1], d_pq[b][1], 1, b)
        to_T_and_store(kT_d, r0, b)
        if own:
            qk_post(pq[b][0], d_pq[b][0], 0, b)
            to_T_and_store(qT_d, r0 - TP, b)
        P.op("act", lambda e: e.copy(out=vs[b][:], in_=pq[b][2][:]), reads=[d_pq[b][2]], writes=[d_vs[b]])
        P.dma("sp", lambda e: e.dma_start(out=v_d[r0:r0 + 128, :], in_=vs[b][:]), reads=[d_vs[b]])

    front(0)
    for ti in range(64):
        if ti + 1 < 64:
            front(ti + 1)
        post(ti)


def phase_b(nc, P, st, kT_d, v_d, qT_d, e_d, vq_d, nb_d, oq_d, tri_d, idb, d_idb, oa, d_oa, heads=range(8), nblk=16):
    def sb(name, shape, dt):
        return st.enter_context(nc.sbuf_tensor("b_" + name, shape, dt))

    def ps(name, shape, dt=F32):
        return st.enter_context(nc.psum_tensor(name, shape, dt))
    NB = 2
    kaug = [sb("kaug%d" % i, [96, NK], BF16) for i in range(NB)]; d_kaug = [Dep() for _ in range(NB)]
    vaug = [sb("vaug%d" % i, [128, 64, 65], BF16) for i in range(NB)]; d_vaug = [Dep() for _ in range(NB)]
    qaug = [sb("qaug%d" % i, [96, T], BF16) for i in range(NB)]; d_qaug = [Dep() for _ in range(NB)]
    d_qmb = [Dep() for _ in range(NB)]
    vq = sb("vq_s", [128, 1024], F32); d_c = Dep()
    nb = sb("nb_s", [128, 1024], F32)
    oq = sb("oq_s", [128, 1024], F32)
    tri = sb("tri_s", [128, 128], BF16)
    ksum = sb("ksum", [64, 32], F32); d_ksum = Dep()
    kmean = sb("kmean", [64, 32], BF16); d_kmean = Dep()
    g1 = sb("g1", [128, 512], F32); d_g1 = Dep()
    top8 = sb("top8", [128, 16, 8], F32); d_top8 = Dep()
    sel = sb("sel", [128, 512], F32); d_sel = Dep()
    mbp = sb("mbp", [128, 16, 96], BF16); d_mbp = Dep()
    NPT = 3
    pt = [sb("pt%d" % i, [128, 512], BF16) for i in range(NPT)]; d_pt = [Dep() for _ in range(NPT)]
    rc = sb("rc", [128, 2], F32); d_rc = [Dep(), Dep()]
    pg = ps("pg", [128, 512], F32); d_pg = Dep()
    pmT = ps("pmT", [96, 8, 128], BF16); d_pmT = Dep()
    psS = [ps("psS%d" % i, [128, 512], F32) for i in range(NPT)]; d_psS = [Dep() for _ in range(NPT)]
    po = [ps("po%d" % i, [128, 65], F32) for i in range(2)]; d_po = [Dep() for _ in range(2)]

    for i in range(NB):
        P.dma("sp", lambda e, i=i: e.dma_start(out=kaug[i][64:96, :], in_=e_d[:, :]), writes=[d_kaug[i]])
        P.op("pool", lambda e, i=i: e.memset(vaug[i][:], 1.0), writes=[d_vaug[i]])
    P.dma("sp", lambda e: e.dma_start(out=vq[:], in_=vq_d[0:1, :].partition_broadcast(128)), writes=[d_c])
    P.dma("sp", lambda e: e.dma_start(out=nb[:], in_=nb_d[0:1, :].partition_broadcast(128)), writes=[d_c])
    P.dma("sp", lambda e: e.dma_start(out=oq[:], in_=oq_d[0:1, :].partition_broadcast(128)), writes=[d_c])
    P.dma("pool", lambda e: e.dma_start(out=tri[:], in_=tri_d[:, :]), writes=[d_c])
    P.op("pool", lambda e: e.memset(mbp[:], 0.0), writes=[d_mbp])

    for ih, h in enumerate(heads):
        b = ih % NB
        P.dma("sp", lambda e, b=b, h=h: e.dma_start(out=kaug[b][0:64, :], in_=kT_d[h * 64:(h + 1) * 64, :]), writes=[d_kaug[b]])
        P.dma("sp", lambda e, b=b, h=h: e.dma_start(out=vaug[b][:, :, 0:64],
                                                    in_=v_d[:, h * 64:(h + 1) * 64].rearrange("(t p) d -> p t d", p=128)),
              writes=[d_vaug[b]])
        P.dma("sp", lambda e, b=b, h=h: e.dma_start(out=qaug[b][0:64, :], in_=qT_d[h * 64:(h + 1) * 64, :]), writes=[d_qaug[b]])
        P.op("dve", lambda e, b=b: e.tensor_reduce(out=ksum[:], in_=kaug[b][0:64, :].rearrange("p (b k) -> p b k", k=256),
                                                   axis=AX.X, op=ALU.add), reads=[d_kaug[b]], writes=[d_ksum])
        P.op("dve", lambda e: e.tensor_scalar(out=kmean[:], in0=ksum[:], scalar1=1.0 / 256, scalar2=None, op0=ALU.mult),
             reads=[d_ksum], writes=[d_kmean])
        for half in range(2):
            for j in range(16):
                qt = half * 16 + j
                P.op("pe", lambda e, b=b, j=j, qt=qt: e.matmul(pg[:, j * 32:(j + 1) * 32], lhsT=qaug[b][0:64, qt * 128:(qt + 1) * 128],
                                                              rhs=kmean[:, :], start=True, stop=True),
                     reads=[d_qaug[b], d_kmean], writes=[d_pg])
            cs_ = slice(half * 512, (half + 1) * 512)
            P.op("dve", lambda e, cs_=cs_: e.tensor_tensor(out=g1[:], in0=pg[:], in1=vq[:, cs_], op=ALU.mult),
                 reads=[d_pg, d_c], writes=[d_g1])
            P.op("dve", lambda e, cs_=cs_: e.tensor_tensor(out=g1[:], in0=g1[:], in1=nb[:, cs_], op=ALU.add),
                 reads=[d_g1, d_c], writes=[d_g1])
            for j in range(16):
                P.op("dve", lambda e, j=j: e.max(out=top8[:, j, :], in_=g1[:, j * 32:(j + 1) * 32]), reads=[d_g1], writes=[d_top8])
            P.op("dve", lambda e: e.tensor_tensor(out=sel[:].rearrange("p (j b) -> p j b", b=32),
                                                  in0=g1[:].rearrange("p (j b) -> p j b", b=32),
                                                  in1=top8[:, :, 2:3].to_broadcast([128, 16, 32]), op=ALU.is_ge),
                 reads=[d_g1, d_top8], writes=[d_sel])
            P.op("dve", lambda e, cs_=cs_: e.tensor_tensor(out=sel[:], in0=sel[:], in1=vq[:, cs_], op=ALU.mult),
                 reads=[d_sel, d_c], writes=[d_sel])
            P.op("dve", lambda e, cs_=cs_: e.tensor_tensor(out=sel[:], in0=sel[:], in1=oq[:, cs_], op=ALU.add),
                 reads=[d_sel, d_c], writes=[d_sel])
            P.op("dve", lambda e: e.tensor_scalar(out=mbp[:, :, 64:96], in0=sel[:].rearrange("p (j b) -> p j b", b=32),
                                                  scalar1=1.0, scalar2=MB, op0=ALU.subtract, op1=ALU.mult),
                 reads=[d_sel], writes=[d_mbp])
            for grp in range(2):
                for j in range(8):
                    P.op("pe", lambda e, grp=grp, j=j: e.transpose(out=pmT[:, j, :], in_=mbp[:, grp * 8 + j, :], identity=idb[:]),
                         reads=[d_mbp, d_idb], writes=[d_pmT])
                c0 = (half * 16 + grp * 8) * 128
                P.op("act", lambda e, b=b, c0=c0: e.copy(out=qaug[b][64:96, c0:c0 + 1024], in_=pmT[64:96, :, :]),
                     reads=[d_pmT], writes=[d_qmb[b]])
        units = []
        for jb in range(nblk):
            ncommon = 32 + 2 * jb
            for u in range(ncommon // 2):
                units.append((jb, [2 * u, 2 * u + 1], False, u == 0))
            units.append((jb, [32 + 2 * jb, 32 + 2 * jb + 1], True, False))

        def emit_S(u, s, b=b):
            jb, kts, diag, first = u
            for j, kt in enumerate(kts):
                if diag and j == 1:
                    q0, nq, c0 = jb * 256 + 128, 128, 256
                else:
                    q0, nq, c0 = jb * 256, 256, j * 256
                P.op("pe", lambda e, s=s, kt=kt, q0=q0, nq=nq, c0=c0: e.matmul(psS[s][:, c0:c0 + nq], lhsT=kaug[b][0:96, kt * 128:(kt + 1) * 128],
                                                                             rhs=qaug[b][0:96, q0:q0 + nq], start=True, stop=True),
                     reads=[d_kaug[b], d_qaug[b], d_qmb[b]], writes=[d_psS[s]])

        def emit_rest(u, s, b=b, h=h):
            jb, kts, diag, first = u
            ncols = 384 if diag else 512
            P.op("act", lambda e, s=s, ncols=ncols: e.activation(out=pt[s][:, 0:ncols], in_=psS[s][:, 0:ncols], func=AF.Exp),
                 reads=[d_psS[s]], writes=[d_pt[s]])
            if diag:
                P.op("pool", lambda e, s=s: e.tensor_tensor(out=pt[s][:, 0:128], in0=pt[s][:, 0:128], in1=tri[:], op=ALU.mult),
                     reads=[d_pt[s], d_c], writes=[d_pt[s]])
                P.op("pool", lambda e, s=s: e.tensor_tensor(out=pt[s][:, 256:384], in0=pt[s][:, 256:384], in1=tri[:], op=ALU.mult),
                     reads=[d_pt[s], d_c], writes=[d_pt[s]])
            for j, kt in enumerate(kts):
                if diag and j == 1:
                    pv = [(1, 256, True)]
                elif diag:
                    pv = [(0, 0, True), (1, 128, False)]
                else:
                    pv = [(0, j * 256, False), (1, j * 256 + 128, False)]
                st_ = first and j == 0
                for qi, col0, last in pv:
                    P.op("pe", lambda e, s=s, kt=kt, qi=qi, col0=col0, st_=st_, last=last: e.matmul(
                        po[qi][:, :], lhsT=pt[s][:, col0:col0 + 128], rhs=vaug[b][:, kt, :], start=st_, stop=last),
                        reads=[d_pt[s], d_vaug[b]], writes=[d_po[qi]])
            if diag:
                for qi in range(2):
                    qt = jb * 2 + qi
                    P.op("dve", lambda e, qi=qi: e.reciprocal(out=rc[:, qi:qi + 1], in_=po[qi][:, 64:65]), reads=[d_po[qi]], writes=[d_rc[qi]])
                    P.op("dve", lambda e, qi=qi, qt=qt: e.tensor_scalar(out=oa[:, qt, h * 64:(h + 1) * 64], in0=po[qi][:, 0:64],
                                                                       scalar1=rc[:, qi:qi + 1], scalar2=None, op0=ALU.mult),
                         reads=[d_po[qi], d_rc[qi]], writes=[d_oa])
        SK = 2
        n = len(units)
        for i in range(n + SK):
            if i < n:
                emit_S(units[i], i % NPT)
            if i >= SK:
                emit_rest(units[i - SK], (i - SK) % NPT)


def host_consts(half):
    pv = 1.0 if half == 1 else 0.0
    V = np.zeros((32, 32), np.float32); O = np.zeros((32, 32), np.float32)
    for qt in range(32):
        jb = qt // 2
        V[qt, :16] = pv
        V[qt, 16:16 + jb] = 1.0
        O[qt, 16 + jb] = 1.0
    NBm = (V - 1.0) * BIGG
    E = np.zeros((32, NK), np.float32)
    for b in range(32):
        E[b, b * 256:(b + 1) * 256] = 1.0
    tri = (np.arange(128)[:, None] <= np.arange(128)[None, :]).astype(np.float32)
    return dict(vq=V.reshape(1, 1024), nb=NBm.reshape(1, 1024), oq=O.reshape(1, 1024),
                e_d=E.astype(ml_dtypes.bfloat16), tri=tri.astype(ml_dtypes.bfloat16))


def phase_c(nc, P, st, xin, w_in, g_mix, cwl_d, cbl_d, dtb_d, alog_d, dsk_d, ssdn_d, tri2_d, sel_d, tb_d, pflag_d,
            idb, d_idb, os_d, d_osd, chunks=range(32)):
    def sb(name, shape, dt):
        return st.enter_context(nc.sbuf_tensor("c_" + name, shape, dt))

    def ps(name, shape, dt=F32):
        return st.enter_context(nc.psum_tensor(name, shape, dt))
    wz = sb("wz", [128, 8, 1024], BF16); d_w = Dep()
    wx = sb("wx", [128, 8, 2048], BF16)
    wdt = sb("wdt", [128, 8, 16], BF16)
    gm = sb("gmc", [128, D], F32); d_c = Dep()
    cw = sb("cw", [128, 16, 4], F32)
    cb = sb("cb", [128, 16], F32)
    dtb = sb("dtb", [128, 2, 16], F32)
    Aneg = sb("Aneg", [128, 2, 16], F32); d_A = Dep()
    dsk = sb("dsk", [128, 16], F32)
    ssdn = sb("ssdn", [128, D], F32)
    tri2 = sb("tri2", [128, 2, 256], F32)
    onesf = sb("onesf", [128, 128], F32)
    sel = sb("selc", [16, 16, 128], F32)
    tb = sb("tbc", [128, 384], F32)
    pflag = sb("pflag", [128, 1], F32)
    xt = sb("xtc", [128, 2, D], F32); d_xt = Dep()
    junk = sb("junkc", [128, D], BF16); d_junk = Dep()
    ssq = sb("ssqc", [128, 2], F32); d_ssq = Dep()
    rstd = sb("rstdc", [128, 2], F32); d_rstd = Dep()
    hb = sb("hbc", [128, 2, D], BF16); d_hb = Dep()
    hT = sb("hTc", [128, 8, 256], BF16); d_hT = Dep()
    xraw = sb("xraw", [128, 16, 259], F32); d_xraw = Dep(); d_halo = Dep()
    acc = sb("acc", [128, 8, 256], F32); d_acc = [Dep() for _ in range(8)]
    xc = sb("xc", [128, 16, 256], BF16); d_xc = [Dep() for _ in range(16)]
    xtok = sb("xtok", [128, 2, 1024], BF16); d_xtok = Dep()
    btok = sb("btok", [128, 2, 512], BF16); d_btok = Dep()
    dtr = sb("dtr", [128, 2, 16], F32); d_dtr = Dep()
    dt = sb("dt", [128, 2, 16], F32); d_dt = Dep()
    aa = sb("aa", [128, 2, 16], F32); d_aa = Dep()
    acum = sb("acum", [128, 2, 16], F32); d_acum = Dep()
    nacum = sb("nacum", [128, 2, 16], F32); d_nacum = Dep()
    acumT = sb("acumT", [16, 256], F32); d_acumT = Dep()
    tot = sb("tot", [128, 16], F32); d_tot = Dep()
    cdec = sb("cdec", [128, 16], F32); d_cdec = Dep()
    dte = sb("dte", [128, 2, 16], F32); d_dte = Dep()
    eac = sb("eac", [128, 2, 16], F32); d_eac = Dep()
    w2 = sb("w2", [128, 2, 16], F32); d_w2 = Dep()
    xdt = sb("xdt", [128, 2, 1024], BF16); d_xdt = Dep()
    xdd = sb("xdd", [128, 2, 1024], BF16); d_xdd = Dep()
    zs = sb("zs", [128, 2, 1024], BF16); d_zs = Dep()
    cbT = sb("cbT", [128, 4, 384], F32); d_cbT = [Dep() for _ in range(4)]
    arg = [sb("arg%d" % i, [128, 384], F32) for i in range(2)]; d_arg = [Dep(), Dep()]
    Lt = [sb("Lt%d" % i, [128, 384], F32) for i in range(2)]; d_Lt = [Dep(), Dep()]
    Mt = [sb("Mt%d" % i, [128, 384], BF16) for i in range(2)]; d_Mt = [Dep() for _ in range(2)]
    stf = sb("stf", [128, 4, 256], F32); d_stf = Dep()
    stT = sb("stT", [128, 4, 256], BF16); d_stT = Dep()
    yt = [sb("yt%d" % i, [128, 256], F32) for i in range(2)]; d_yt = [Dep(), Dep()]
    y2 = [sb("y2%d" % i, [128, 256], F32) for i in range(2)]; d_y2 = [Dep(), Dep()]
    gss = [sb("gss%d" % i, [128, 1], F32) for i in range(2)]; d_gss = [Dep(), Dep()]
    grs = [sb("grs%d" % i, [128, 1], F32) for i in range(2)]; d_grs = [Dep(), Dep()]
    osb = sb("osb", [128, 2, 1024], BF16); d_osb = Dep()

    B = [ps("bk%d" % i, [128, 512], F32) for i in range(7)]
    d_B = [Dep() for _ in range(7)]
    d_B1h = [Dep(), Dep()]
    d_B5h = [Dep(), Dep()]
    pT = ps("pTc", [128, 8, 128], BF16); d_pT = Dep()

    for k in range(8):
        rows = slice(k * 128, (k + 1) * 128)
        P.dma("pool", lambda e, k=k, rows=rows: e.dma_start(out=wz[:, k, :], in_=w_in[rows, C_Z:C_Z + 1024]), writes=[d_w])
        P.dma("pool", lambda e, k=k, rows=rows: e.dma_start(out=wx[:, k, :], in_=w_in[rows, C_X:C_X + 2048]), writes=[d_w])
        P.dma("pool", lambda e, k=k, rows=rows: e.dma_start(out=wdt[:, k, :], in_=w_in[rows, C_DT:C_DT + 16]), writes=[d_w])
    P.dma("sp", lambda e: e.dma_start(out=gm[:], in_=g_mix[0:1, :].partition_broadcast(128)), writes=[d_c])
    P.dma("sp", lambda e: e.dma_start(out=cw[:], in_=cwl_d[:, :, :]), writes=[d_c])
    P.dma("sp", lambda e: e.dma_start(out=cb[:], in_=cbl_d[:, :]), writes=[d_c])
    for i in range(2):
        P.dma("sp", lambda e, i=i: e.dma_start(out=dtb[:, i, :], in_=dtb_d[0:1, :].partition_broadcast(128)), writes=[d_c])
        P.dma("sp", lambda e, i=i: e.dma_start(out=Aneg[:, i, :], in_=alog_d[0:1, :].partition_broadcast(128)), writes=[d_A])
    P.dma("sp", lambda e: e.dma_start(out=dsk[:], in_=dsk_d[0:1, :].partition_broadcast(128)), writes=[d_c])
    P.dma("sp", lambda e: e.dma_start(out=ssdn[:], in_=ssdn_d[0:1, :].partition_broadcast(128)), writes=[d_c])
    P.dma("sp", lambda e: e.dma_start(out=tri2[:], in_=tri2_d[:, :, :]), writes=[d_c])
    P.dma("sp", lambda e: e.dma_start(out=sel[:], in_=sel_d[:, :, :]), writes=[d_c])
    P.dma("sp", lambda e: e.dma_start(out=tb[:], in_=tb_d[:, :]), writes=[d_c])
    P.dma("sp", lambda e: e.dma_start(out=pflag[:], in_=pflag_d[0:1, :].partition_broadcast(128)), writes=[d_c])
    P.op("pool", lambda e: e.memset(onesf[:], 1.0), writes=[d_c])
    P.op("act", lambda e: e.activation(out=Aneg[:], in_=Aneg[:], func=AF.Exp), reads=[d_A], writes=[d_A])
    P.op("dve", lambda e: e.tensor_scalar(out=Aneg[:], in0=Aneg[:], scalar1=-1.0, scalar2=None, op0=ALU.mult), reads=[d_A], writes=[d_A])
    P.op("pool", lambda e: e.memset(xraw[:], 0.0), writes=[d_xraw, d_halo])
    P.op("pool", lambda e: e.memset(stf[:], 0.0), writes=[d_stf])
    P.op("pool", lambda e: e.memset(stT[:], 0.0), writes=[d_stT])

    h3 = lambda ap: ap.rearrange("p (h d) -> p h d", d=64)

    for c in chunks:
        own = c >= 16
        r0 = c * 256
        P.dma("sp", lambda e, r0=r0: e.dma_start(out=xt[:], in_=xin[r0:r0 + 256, :].rearrange("(t p) d -> p t d", p=128)), writes=[d_xt])
        for it in range(2):
            P.op("act", lambda e, it=it: e.activation(out=junk[:], in_=xt[:, it, :], func=AF.Square, accum_out=ssq[:, it:it + 1]),
                 reads=[d_xt], writes=[d_junk, d_ssq])
        P.op("act", lambda e: e.activation(out=rstd[:], in_=ssq[:], func=AF.Sqrt, bias=EPS, scale=1.0 / D), reads=[d_ssq], writes=[d_rstd])
        P.op("dve", lambda e: e.reciprocal(out=rstd[:], in_=rstd[:]), reads=[d_rstd], writes=[d_rstd])
        for it in range(2):
            P.op("dve", lambda e, it=it: e.scalar_tensor_tensor(out=hb[:, it, :], in0=xt[:, it, :], scalar=rstd[:, it:it + 1], in1=gm[:],
                                                                                    op0=ALU.mult, op1=ALU.mult),
                 reads=[d_xt, d_rstd, d_c], writes=[d_hb])
        for it in range(2):
            for k in range(8):
                P.op("pe", lambda e, it=it, k=k: e.transpose(out=pT[:, k, :], in_=hb[:, it, k * 128:(k + 1) * 128], identity=idb[:]),
                     reads=[d_hb, d_idb], writes=[d_pT])
            P.op("act", lambda e, it=it: e.copy(out=hT[:, :, it * 128:(it + 1) * 128], in_=pT[:]), reads=[d_pT], writes=[d_hT])
        for it in range(2):
            for k in range(8):
                P.op("pe", lambda e, it=it, k=k: e.matmul(B[3][:, it * 16:(it + 1) * 16], lhsT=hT[:, k, it * 128:(it + 1) * 128], rhs=wdt[:, k, :],
                                                         start=(k == 0), stop=(k == 7)), reads=[d_hT, d_w], writes=[d_B[3]])
        P.op("dve", lambda e: e.tensor_tensor(out=dtr[:].rearrange("p a b -> p (a b)"), in0=B[3][:, 0:32], in1=dtb[:].rearrange("p a b -> p (a b)"), op=ALU.add),
             reads=[d_B[3], d_c], writes=[d_dtr])
        P.op("act", lambda e: e.activation(out=dtr[:], in_=dtr[:], func=AF.Exp), reads=[d_dtr], writes=[d_dtr])
        P.op("act", lambda e: e.activation(out=dt[:], in_=dtr[:], func=AF.Ln, bias=1.0), reads=[d_dtr], writes=[d_dt])
        P.op("dve", lambda e: e.tensor_tensor(out=aa[:], in0=dt[:], in1=Aneg[:], op=ALU.mult), reads=[d_dt, d_A], writes=[d_aa])
        for it in range(2):
            for jt in range(it + 1):
                P.op("pe", lambda e, it=it, jt=jt: e.matmul(B[3][:, 32 + it * 16:32 + (it + 1) * 16], lhsT=tri2[:, jt, it * 128:(it + 1) * 128],
                                                           rhs=aa[:, jt, :], start=(jt == 0), stop=(jt == it)),
                     reads=[d_aa, d_c], writes=[d_B[3]])
        for jt in range(2):
            P.op("pe", lambda e, jt=jt: e.matmul(B[3][:, 64:80], lhsT=onesf[:], rhs=aa[:, jt, :], start=(jt == 0), stop=(jt == 1)),
                 reads=[d_aa, d_c], writes=[d_B[3]])
        for jt in range(2):
            P.op("pe", lambda e, jt=jt: e.matmul(B[3][0:16, 128:384], lhsT=aa[:, jt, :], rhs=tri2[:, jt, :], start=(jt == 0), stop=(jt == 1)),
                 reads=[d_aa, d_c], writes=[d_B[3]])
        P.op("act", lambda e: e.copy(out=acum[:].rearrange("p a b -> p (a b)"), in_=B[3][:, 32:64]), reads=[d_B[3]], writes=[d_acum])
        P.op("dve", lambda e: e.tensor_scalar(out=nacum[:].rearrange("p a b -> p (a b)"), in0=B[3][:, 32:64], scalar1=-1.0, scalar2=None, op0=ALU.mult),
             reads=[d_B[3]], writes=[d_nacum])
        P.op("act", lambda e: e.copy(out=tot[:], in_=B[3][:, 64:80]), reads=[d_B[3]], writes=[d_tot])
        P.op("act", lambda e: e.copy(out=acumT[:], in_=B[3][0:16, 128:384]), reads=[d_B[3]], writes=[d_acumT])
        P.op("act", lambda e: e.activation(out=cdec[:], in_=tot[:], func=AF.Exp), reads=[d_tot], writes=[d_cdec])
        P.op("dve", lambda e: e.tensor_tensor(out=dte[:], in0=nacum[:], in1=tot[:].unsqueeze(1).to_broadcast([128, 2, 16]), op=ALU.add),
             reads=[d_nacum, d_tot], writes=[d_dte])
        P.op("act", lambda e: e.activation(out=dte[:], in_=dte[:], func=AF.Exp), reads=[d_dte], writes=[d_dte])
        P.op("act", lambda e: e.activation(out=eac[:], in_=acum[:], func=AF.Exp), reads=[d_acum], writes=[d_eac])
        P.op("dve", lambda e: e.tensor_tensor(out=w2[:], in0=dt[:], in1=dte[:], op=ALU.mult), reads=[d_dt, d_dte], writes=[d_w2])
        nfc = 16 if (own or c == 15) else 12
        for fc in range(nfc):
            bi = 1 if fc % 2 == 0 else 4
            for k in range(8):
                P.op("pe", lambda e, fc=fc, k=k, bi=bi: e.matmul(B[bi][:, 0:256], lhsT=wx[:, k, fc * 128:(fc + 1) * 128], rhs=hT[:, k, :],
                                                               start=(k == 0), stop=(k == 7)), reads=[d_hT, d_w], writes=[d_B[bi]])
            P.op("act", lambda e, fc=fc, bi=bi: e.copy(out=xraw[:, fc, 3:259], in_=B[bi][:, 0:256]),
                 reads=[d_B[bi]], writes=[d_xraw])
        if own:
            for it in range(2):
                for hv in range(2):
                    zb = 2 if hv == 0 else 6
                    for k in range(8):
                        P.op("pe", lambda e, it=it, hv=hv, k=k, zb=zb: e.matmul(B[zb][:, :], lhsT=hT[:, k, it * 128:(it + 1) * 128], rhs=wz[:, k, hv * 512:(hv + 1) * 512],
                                                                              start=(k == 0), stop=(k == 7)), reads=[d_hT, d_w], writes=[d_B[zb]])
                    P.op("act", lambda e, it=it, hv=hv, zb=zb: e.activation(out=zs[:, it, hv * 512:(hv + 1) * 512], in_=B[zb][:, :], func=AF.Silu),
                         reads=[d_B[zb]], writes=[d_zs])
        for half in range(2):
            n8 = 8 if (half == 0 or nfc == 16) else 4
            for f8 in range(n8):
                fc = half * 8 + f8
                P.op("dve", lambda e, fc=fc, f8=f8: e.tensor_scalar(out=acc[:, f8, :], in0=xraw[:, fc, 0:256], scalar1=cw[:, fc, 0:1], scalar2=cb[:, fc:fc + 1],
                                                                   op0=ALU.mult, op1=ALU.add), reads=[d_xraw, d_halo, d_c], writes=[d_acc[f8]])
            for kk in range(1, 4):
                for f8 in range(n8):
                    fc = half * 8 + f8
                    P.op("dve", lambda e, fc=fc, f8=f8, kk=kk: e.scalar_tensor_tensor(out=acc[:, f8, :], in0=xraw[:, fc, kk:kk + 256], scalar=cw[:, fc, kk:kk + 1],
                                                                                     in1=acc[:, f8, :], op0=ALU.mult, op1=ALU.add),
                         reads=[d_xraw, d_halo, d_c, d_acc[f8]], writes=[d_acc[f8]])
            for f8 in range(n8):
                fc = half * 8 + f8
                P.op("act", lambda e, fc=fc, f8=f8: e.activation(out=xc[:, fc, :], in_=acc[:, f8, :], func=AF.Silu),
                     reads=[d_acc[f8]], writes=[d_xc[fc]])
        P.op("pool", lambda e: e.tensor_copy(out=xraw[:, :, 0:3], in_=xraw[:, :, 256:259]), reads=[d_xraw], writes=[d_halo])
        for it in range(2):
            for fc in range(8):
                P.op("pe", lambda e, it=it, fc=fc: e.transpose(out=pT[:, fc, :], in_=xc[:, fc, it * 128:(it + 1) * 128], identity=idb[:]),
                     reads=[d_xc[fc], d_idb], writes=[d_pT])
            P.op("act", lambda e, it=it: e.copy(out=xtok[:, it, :], in_=pT[:]), reads=[d_pT], writes=[d_xtok])
            for g in range(4):
                P.op("pe", lambda e, it=it, g=g: e.transpose(out=pT[:, g, :], in_=xc[:, 8 + g, it * 128:(it + 1) * 128], identity=idb[:]),
                     reads=[d_xc[8 + g], d_idb], writes=[d_pT])
            P.op("act", lambda e, it=it: e.copy(out=btok[:, it, :], in_=pT[:, 0:4, :]), reads=[d_pT], writes=[d_btok])
        for it in range(2):
            if own:
                P.op("dve", lambda e, it=it: e.tensor_tensor(out=h3(xdt[:, it, :]), in0=h3(xtok[:, it, :]),
                                                             in1=dt[:, it, :].unsqueeze(2).to_broadcast([128, 16, 64]), op=ALU.mult),
                     reads=[d_xtok, d_dt], writes=[d_xdt])
            P.op("pool", lambda e, it=it: e.tensor_tensor(out=h3(xdd[:, it, :]), in0=h3(xtok[:, it, :]),
                                                          in1=w2[:, it, :].unsqueeze(2).to_broadcast([128, 16, 64]), op=ALU.mult),
                 reads=[d_xtok, d_w2], writes=[d_xdd])
        if own:
            for g in range(4):
                P.op("pe", lambda e, g=g: e.matmul(B[4][:, 0:256], lhsT=xc[:, 8 + g, 0:128], rhs=xc[:, 12 + g, :], start=True, stop=True),
                     reads=[d_xc[8 + g], d_xc[12 + g]], writes=[d_B[4]])
                P.op("pe", lambda e, g=g: e.matmul(B[4][:, 256:384], lhsT=xc[:, 8 + g, 128:256], rhs=xc[:, 12 + g, 128:256], start=True, stop=True),
                     reads=[d_xc[8 + g], d_xc[12 + g]], writes=[d_B[4]])
                P.op("act", lambda e, g=g: e.copy(out=cbT[:, g, :], in_=B[4][:, 0:384]), reads=[d_B[4]], writes=[d_cbT[g]])
            for g in range(4):
                def stA(h, g=g):
                    mi = h % 2
                    bk = 5 if mi == 0 else 4
                    P.op("pe", lambda e: e.matmul(B[bk][:, 0:256], lhsT=sel[:, h, :], rhs=acumT[:, :], start=True, stop=True),
                         reads=[d_c, d_acumT], writes=[d_B[bk]])
                    P.op("dve", lambda e: e.scalar_tensor_tensor(out=arg[mi][:, 0:256], in0=B[bk][:, 0:256], scalar=nacum[:, 0, h:h + 1], in1=tb[:, 0:256],
                                                                 op0=ALU.add, op1=ALU.add), reads=[d_B[bk], d_nacum, d_c], writes=[d_arg[mi]])
                    P.op("dve", lambda e: e.scalar_tensor_tensor(out=arg[mi][:, 256:384], in0=B[bk][:, 128:256], scalar=nacum[:, 1, h:h + 1], in1=tb[:, 256:384],
                                                                 op0=ALU.add, op1=ALU.add), reads=[d_B[bk], d_nacum, d_c], writes=[d_arg[mi]])
                    P.op("act", lambda e: e.activation(out=Lt[mi][:], in_=arg[mi][:], func=AF.Exp), reads=[d_arg[mi]], writes=[d_Lt[mi]])
                    P.op("pool", lambda e: e.tensor_tensor(out=Mt[mi][:], in0=Lt[mi][:], in1=cbT[:, g, :], op=ALU.mult),
                         reads=[d_Lt[mi], d_cbT[g]], writes=[d_Mt[mi]])

                def stB(h, g=g):
                    mi = h % 2
                    r = h % 4
                    cs_ = slice(r * 64, (r + 1) * 64)
                    hs_ = slice(h * 64, (h + 1) * 64)
                    P.op("pe", lambda e: e.matmul(B[6][:, 0:256][:, cs_], lhsT=Mt[mi][:, 0:128], rhs=xdt[:, 0, hs_], start=True, stop=True),
                         reads=[d_Mt[mi], d_xdt], writes=[d_B[6]])
                    P.op("pe", lambda e: e.matmul(B[6][:, 256:512][:, cs_], lhsT=Mt[mi][:, 128:256], rhs=xdt[:, 0, hs_], start=True, stop=False),
                         reads=[d_Mt[mi], d_xdt], writes=[d_B[6]])
                    P.op("pe", lambda e: e.matmul(B[6][:, 256:512][:, cs_], lhsT=Mt[mi][:, 256:384], rhs=xdt[:, 1, hs_], start=False, stop=True),
                         reads=[d_Mt[mi], d_xdt], writes=[d_B[6]])
                hs4 = [g * 4 + r for r in range(4)]
                stA(hs4[0]); stA(hs4[1]); stB(hs4[0]); stA(hs4[2]); stB(hs4[1]); stA(hs4[3]); stB(hs4[2]); stB(hs4[3])
                gs_ = slice(g * 256, (g + 1) * 256)
                for it in range(2):
                    P.op("pe", lambda e, g=g, it=it: e.matmul(B[0][:, it * 256:(it + 1) * 256], lhsT=xc[:, 12 + g, it * 128:(it + 1) * 128], rhs=stT[:, g, :],
                                                             start=True, stop=True), reads=[d_xc[12 + g], d_stT], writes=[d_B[0]])
                v4 = lambda ap: ap.rearrange("p (r d) -> p r d", d=64)

                def ystep(step, it, g=g, gs_=gs_):
                    if step == 0:
                        P.op("dve", lambda e: e.tensor_tensor(out=v4(yt[it][:]), in0=v4(B[0][:, it * 256:(it + 1) * 256]),
                                                              in1=eac[:, it, g * 4:(g + 1) * 4].unsqueeze(2).to_broadcast([128, 4, 64]), op=ALU.mult),
                             reads=[d_B[0], d_eac], writes=[d_yt[it]])
                    elif step == 1:
                        P.op("dve", lambda e: e.tensor_tensor(out=yt[it][:], in0=yt[it][:], in1=B[6][:, it * 256:(it + 1) * 256], op=ALU.add),
                             reads=[d_yt[it], d_B[6]], writes=[d_yt[it]])
                    elif step == 2:
                        P.op("pool", lambda e: e.tensor_tensor(out=v4(y2[it][:]), in0=v4(xtok[:, it, gs_]),
                                                               in1=dsk[:, g * 4:(g + 1) * 4].unsqueeze(2).to_broadcast([128, 4, 64]), op=ALU.mult),
                             reads=[d_xtok, d_c], writes=[d_y2[it]])
                    elif step == 3:
                        P.op("dve", lambda e: e.tensor_tensor(out=yt[it][:], in0=yt[it][:], in1=y2[it][:], op=ALU.add), reads=[d_yt[it], d_y2[it]], writes=[d_yt[it]])
                    elif step == 4:
                        P.op("dve", lambda e: e.tensor_tensor(out=yt[it][:], in0=yt[it][:], in1=zs[:, it, gs_], op=ALU.mult), reads=[d_yt[it], d_zs], writes=[d_yt[it]])
                    elif step == 5:
                        P.op("act", lambda e: e.activation(out=y2[it][:], in_=yt[it][:], func=AF.Square, accum_out=gss[it][:]), reads=[d_yt[it], d_y2[it]], writes=[d_y2[it], d_gss[it]])
                    elif step == 6:
                        P.op("act", lambda e: e.activation(out=grs[it][:], in_=gss[it][:], func=AF.Sqrt, bias=EPS, scale=1.0 / 256), reads=[d_gss[it]], writes=[d_grs[it]])
                    elif step == 7:
                        P.op("dve", lambda e: e.reciprocal(out=grs[it][:], in_=grs[it][:]), reads=[d_grs[it]], writes=[d_grs[it]])
                    elif step == 8:
                        P.op("dve", lambda e: e.scalar_tensor_tensor(out=osb[:, it, gs_], in0=yt[it][:], scalar=grs[it][:, 0:1], in1=ssdn[:, gs_],
                                                                     op0=ALU.mult, op1=ALU.mult), reads=[d_yt[it], d_grs[it], d_c], writes=[d_osb])
                for step in range(9):
                    for it in range(2):
                        ystep(step, it)
            P.dma("sp", lambda e, r0=r0: e.dma_start(out=os_d[r0 - TP:r0 - TP + 256, :].rearrange("(t p) c -> p t c", p=128), in_=osb[:]),
                  reads=[d_osb], writes=[d_osd])
        for g in range(4):
            bank = B[2] if g < 2 else B[1]
            dbank = d_B[2] if g < 2 else d_B[1]
            cs_ = slice((g % 2) * 256, (g % 2 + 1) * 256)
            for jt in range(2):
                P.op("pe", lambda e, g=g, jt=jt, bank=bank, cs_=cs_: e.matmul(bank[:, cs_], lhsT=btok[:, jt, g * 128:(g + 1) * 128], rhs=xdd[:, jt, g * 256:(g + 1) * 256],
                                                                            start=(jt == 0), stop=(jt == 1)), reads=[d_btok, d_xdd], writes=[dbank])
        v16 = lambda ap: ap.rearrange("p (h d) -> p h d", d=64)
        P.op("dve", lambda e: e.tensor_tensor(out=v16(stf[:].rearrange("p g c -> p (g c)")), in0=v16(stf[:].rearrange("p g c -> p (g c)")),
                                              in1=cdec[:].unsqueeze(2).to_broadcast([128, 16, 64]), op=ALU.mult), reads=[d_stf, d_cdec], writes=[d_stf])
        P.op("dve", lambda e: e.tensor_tensor(out=stf[:, 0:2, :].rearrange("p g c -> p (g c)"), in0=stf[:, 0:2, :].rearrange("p g c -> p (g c)"), in1=B[2][:, :], op=ALU.add),
             reads=[d_stf, d_B[2]], writes=[d_stf])
        P.op("dve", lambda e: e.tensor_tensor(out=stf[:, 2:4, :].rearrange("p g c -> p (g c)"), in0=stf[:, 2:4, :].rearrange("p g c -> p (g c)"), in1=B[1][:, :], op=ALU.add),
             reads=[d_stf, d_B[1]], writes=[d_stf])
        if c == 15:
            P.op("dve", lambda e: e.tensor_scalar(out=stf[:], in0=stf[:], scalar1=pflag[:, 0:1], scalar2=None, op0=ALU.mult), reads=[d_stf, d_c], writes=[d_stf])
        P.op("act", lambda e: e.copy(out=stT[:], in_=stf[:]), reads=[d_stf], writes=[d_stT])


def host_consts_c(z, half):
    cwl = np.ascontiguousarray(z['conv_w'].reshape(4, 16, 128).transpose(2, 1, 0)).astype(np.float32)
    cbl = np.ascontiguousarray(z['conv_b'].reshape(16, 128).T).astype(np.float32)
    tri = (np.arange(128)[:, None] <= np.arange(128)[None, :]).astype(np.float32)
    tri2 = np.zeros((128, 2, 256), np.float32)
    tri2[:, 0, 0:128] = tri; tri2[:, 0, 128:256] = 1.0; tri2[:, 1, 128:256] = tri
    sel = np.zeros((16, 16, 128), np.float32)
    for h in range(16):
        sel[h, h, :] = 1.0
    tbias = np.where(tri > 0, 0.0, NEG).astype(np.float32)
    tb = np.zeros((128, 384), np.float32)
    tb[:, 0:128] = tbias; tb[:, 256:384] = tbias
    return dict(cwl=cwl, cbl=cbl, dtb=z['dt_bias'][None, :], alog=z['a_log'][None, :], dsk=z['d_skip'][None, :], ssdn=z['ssd_norm'][None, :],
                tri2=tri2, sel_d=sel, tb_d=tb, pflag=np.array([[1.0 if half == 1 else 0.0]], np.float32))


def phase_def(nc, P, xown, w_in, g_mix, mem_d, gmem_d, wmemkv, mqn_d, mkn_d, woa_d, wos_d, wom_d, wout_d, gffn_d, wr_d,
              wgate_d, wup_d, wdown_d, tri_d, ecap_d, oa_d, d_oad, os_d, d_osd, x1_d, xbuf, ybuf, out_d, idb, d_idb, idf, d_idf,
              ntiles=32, experts=range(32), stage=3):
    d_x1d = Dep(); d_xbuf = Dep(); d_ybuf = Dep(); d_out = Dep()
    with contextlib.ExitStack() as stp:
        def sbp(name, shape, dt):
            return stp.enter_context(nc.sbuf_tensor("d_" + name, shape, dt))
        combs = sbp("combs", [128, 32, 2], F32); d_combs = Dep()
        idxs = sbp("idxs", [128, 32, 2], I32); d_idxs = Dep()

        with contextlib.ExitStack() as st:
            def sb(name, shape, dt):
                return st.enter_context(nc.sbuf_tensor("d_" + name, shape, dt))

            def ps(name, shape, dt=F32):
                return st.enter_context(nc.psum_tensor(name, shape, dt))
            d_w = Dep(); d_c = Dep()
            gm = sb("gmd", [128, D], F32)
            gf = sb("gfd", [128, D], F32)
            mqn = sb("mqn", [128, 512], F32)
            mkn = sb("mkn", [128, 512], F32)
            tri = sb("trid", [128, 128], F32)
            onesf = sb("onesfd", [128, 128], F32)
            onesb = sb("onesbd", [128, 128], BF16)
            ecap = sb("ecap", [128, 32], F32)
            base = sb("base", [128, 32], F32); d_base = Dep()
            kmT = sb("kmT", [128, 4, 256], BF16); d_kmT = Dep()
            vm = sb("vm", [128, 2, 512], BF16); d_vm = Dep()

            xt = [sb("xtd%d" % i, [128, D], F32) for i in range(2)]; d_xt = [Dep(), Dep()]
            junk = sb("junkd", [128, D], BF16); d_junk = Dep()
            ssq = sb("ssqd", [128, 1], F32); d_ssq = Dep()
            rstd = sb("rstdd", [128, 1], F32); d_rstd = Dep()
            hb = sb("hbd", [128, D], BF16); d_hb = Dep()
            hT = sb("hTd", [128, 8, 128], BF16); d_hT = Dep()
            sq = sb("sqd", [128, 512], F32); d_sq = Dep()
            hs = sb("hsd", [128, 4], F32); d_hs = Dep()
            hr = sb("hrd", [128, 4], F32); d_hr = Dep()
            qmn = sb("qmn", [128, 512], F32); d_qmn = Dep()
            qmb = sb("qmb", [128, 512], BF16); d_qmb = Dep()
            qmT = sb("qmT", [128, 4, 128], BF16); d_qmT = Dep()
            pm = sb("pm", [128, 4, 2, 128], BF16); d_pm = Dep()
            rcp = sb("rcp", [128, 512], F32); d_rcp = Dep()
            omT = sb("omT", [128, 4, 128], BF16); d_omT = Dep()
            gs = sb("gs", [128, 3072], F32); d_gs = Dep()
            oat = [sb("oat%d" % i, [128, 512], BF16) for i in range(2)]; d_oat = [Dep(), Dep()]
            ost = [sb("ost%d" % i, [128, 1024], BF16) for i in range(2)]; d_ost = [Dep(), Dep()]
            oaT = sb("oaT", [128, 4, 128], BF16); d_oaT = Dep()
            osT = sb("osT", [128, 8, 128], BF16); d_osT = Dep()
            mg = sb("mg", [128, 512], F32); d_mg = Dep()
            tt = sb("tt", [128, 512], F32); d_tt = Dep()
            mgb = sb("mgb", [128, D], BF16); d_mgb = Dep()
            mgT = sb("mgT", [128, 8, 128], BF16); d_mgT = Dep()
            x1 = sb("x1", [128, D], F32); d_x1 = Dep()
            h2f = sb("h2f", [128, D], F32); d_h2f = Dep()
            h2b = [sb("h2b%d" % i, [128, D], BF16) for i in range(2)]; d_h2b = [Dep() for _ in range(2)]
            h2T = sb("h2T", [128, 8, 128], F32); d_h2T = Dep()
            lg = sb("lg", [128, 36], F32); d_lg = Dep()
            sm = sb("sm", [128, 64], F32); d_sm = Dep()
            goh = sb("goh", [128, 4], F32); d_goh = Dep()
            lsel = sb("lsel", [128, 32], F32); d_lsel = Dep()
            les = sb("les", [128, 8], F32); d_les = Dep()
            t8 = sb("t8", [128, 8], F32); d_t8 = Dep()
            oh = sb("oh", [128, 64], F32); d_oh = Dep()
            ohs = sb("ohs", [128, 32], F32); d_ohs = Dep()
            posf = sb("posf", [128, 32], F32); d_posf = Dep()
            idf2 = sb("idf2", [128, 2], F32); d_idf2 = Dep()

            pT = ps("pTd", [128, 8, 128], BF16); d_pT = Dep()
            pF = ps("pFd", [128, 512], F32); d_pF = Dep()
            pG = [ps("pGd%d" % i, [128, 512], F32) for i in range(3)]; d_pG = [Dep() for _ in range(3)]
            pms = [ps("pmsd%d" % i, [128, 512], F32) for i in range(2)]; d_pms = [Dep() for _ in range(2)]
            pmo = ps("pmod", [128, 512], F32); d_pmo = Dep()

            P.dma("sp", lambda e: e.dma_start(out=gm[:], in_=g_mix[0:1, :].partition_broadcast(128)), writes=[d_c])
            P.dma("sp", lambda e: e.dma_start(out=gf[:], in_=gffn_d[0:1, :].partition_broadcast(128)), writes=[d_c])
            P.dma("sp", lambda e: e.dma_start(out=mqn[:], in_=mqn_d[0:1, :].partition_broadcast(128)), writes=[d_c])
            P.dma("sp", lambda e: e.dma_start(out=mkn[:], in_=mkn_d[0:1, :].partition_broadcast(128)), writes=[d_c])
            P.dma("sp", lambda e: e.dma_start(out=tri[:], in_=tri_d[:, :]), writes=[d_c])
            P.dma("sp", lambda e: e.dma_start(out=ecap[:], in_=ecap_d[0:1, :].partition_broadcast(128)), writes=[d_c])
            P.op("pool", lambda e: e.memset(onesf[:], 1.0), writes=[d_c])
            P.op("pool", lambda e: e.memset(onesb[:], 1.0), writes=[d_c])
            P.op("pool", lambda e: e.memset(base[:], 0.0), writes=[d_base])

            def head_norm(src_ps, d_src, gain, nh, dst, d_dst):
                hd = 512 // nh
                v = lambda ap: ap.rearrange("p (h d) -> p h d", h=nh)
                P.op("act", lambda e: e.activation(out=sq[:], in_=src_ps, func=AF.Square), reads=[d_src], writes=[d_sq])
                P.op("dve", lambda e: e.tensor_reduce(out=hs[:, 0:nh], in_=v(sq[:]), axis=AX.X, op=ALU.add), reads=[d_sq], writes=[d_hs])
                P.op("act", lambda e: e.activation(out=hr[:, 0:nh], in_=hs[:, 0:nh], func=AF.Sqrt, bias=EPS, scale=1.0 / hd), reads=[d_hs], writes=[d_hr])
                P.op("dve", lambda e: e.reciprocal(out=hr[:, 0:nh], in_=hr[:, 0:nh]), reads=[d_hr], writes=[d_hr])
                P.op("dve", lambda e: e.tensor_tensor(out=v(qmn[:]), in0=v(src_ps), in1=hr[:, 0:nh].unsqueeze(2).to_broadcast([128, nh, hd]), op=ALU.mult),
                     reads=[d_src, d_hr], writes=[d_qmn])
                P.op("pool", lambda e: e.tensor_tensor(out=dst, in0=qmn[:], in1=gain[:], op=ALU.mult), reads=[d_qmn, d_c], writes=[d_dst])

            with contextlib.ExitStack() as stm:
                wkv = stm.enter_context(nc.sbuf_tensor("s_wkv", [128, 8, 1024], BF16)); d_wkv = Dep()
                mt_ = stm.enter_context(nc.sbuf_tensor("s_memt", [128, 2, D], F32)); d_mt = Dep()
                gme = stm.enter_context(nc.sbuf_tensor("s_gme", [128, D], F32)); d_gme = Dep()
                mhb = stm.enter_context(nc.sbuf_tensor("s_mhb", [128, 2, D], BF16)); d_mhb = Dep()
                mT = stm.enter_context(nc.sbuf_tensor("s_mT", [128, 8, 256], BF16)); d_mT = Dep()
                ss2 = stm.enter_context(nc.sbuf_tensor("s_ss2", [128, 2], F32)); d_ss2 = Dep()
                for k in range(8):
                    P.dma("pool", lambda e, k=k: e.dma_start(out=wkv[:, k, :], in_=wmemkv[k * 128:(k + 1) * 128, :]), writes=[d_wkv])
                P.dma("sp", lambda e: e.dma_start(out=mt_[:], in_=mem_d[:, :].rearrange("(t p) d -> p t d", p=128)), writes=[d_mt])
                P.dma("sp", lambda e: e.dma_start(out=gme[:], in_=gmem_d[0:1, :].partition_broadcast(128)), writes=[d_gme])
                for it in range(2):
                    P.op("act", lambda e, it=it: e.activation(out=junk[:], in_=mt_[:, it, :], func=AF.Square, accum_out=ss2[:, it:it + 1]),
                         reads=[d_mt], writes=[d_junk, d_ss2])
                P.op("act", lambda e: e.activation(out=ss2[:], in_=ss2[:], func=AF.Sqrt, bias=EPS, scale=1.0 / D), reads=[d_ss2], writes=[d_ss2])
                P.op("dve", lambda e: e.reciprocal(out=ss2[:], in_=ss2[:]), reads=[d_ss2], writes=[d_ss2])
                for it in range(2):
                    P.op("dve", lambda e, it=it: e.scalar_tensor_tensor(out=mhb[:, it, :], in0=mt_[:, it, :], scalar=ss2[:, it:it + 1], in1=gme[:],
                                                                        op0=ALU.mult, op1=ALU.mult), reads=[d_mt, d_ss2, d_gme], writes=[d_mhb])
                    for k in range(8):
                        P.op("pe", lambda e, it=it, k=k: e.transpose(out=pT[:, k, :], in_=mhb[:, it, k * 128:(k + 1) * 128], identity=idb[:]),
                             reads=[d_mhb, d_idb], writes=[d_pT])
                    P.op("act", lambda e, it=it: e.copy(out=mT[:, :, it * 128:(it + 1) * 128], in_=pT[:]), reads=[d_pT], writes=[d_mT])
                for it in range(2):
                    for hv in range(2):
                        for k in range(8):
                            P.op("pe", lambda e, it=it, hv=hv, k=k: e.matmul(pG[hv][:, :], lhsT=mT[:, k, it * 128:(it + 1) * 128], rhs=wkv[:, k, hv * 512:(hv + 1) * 512],
                                                                           start=(k == 0), stop=(k == 7)), reads=[d_mT, d_wkv], writes=[d_pG[hv]])
                    head_norm(pG[0][:, :], d_pG[0], mkn, 4, qmb[:], d_qmb)
                    for h in range(4):
                        P.op("pe", lambda e, h=h: e.transpose(out=pT[:, h, :], in_=qmb[:, h * 128:(h + 1) * 128], identity=idb[:]),
                             reads=[d_qmb, d_idb], writes=[d_pT])
                    P.op("act", lambda e, it=it: e.copy(out=kmT[:, :, it * 128:(it + 1) * 128], in_=pT[:, 0:4, :]), reads=[d_pT], writes=[d_kmT])
                    P.op("act", lambda e, it=it: e.copy(out=vm[:, it, :], in_=pG[1][:, :]), reads=[d_pG[1]], writes=[d_vm])

            wqm = sb("wqm", [128, 8, 512], BF16)
            wg = sb("wg", [128, 8, 3072], BF16)
            woa = sb("woa", [128, 4, 1024], BF16)
            wos = sb("wos", [128, 8, 1024], BF16)
            wom = sb("wom", [128, 4, 1024], BF16)
            wout = sb("wout", [128, 8, 1024], BF16)
            wr = sb("wr", [128, 8, 36], F32)
            for k in range(8):
                rows = slice(k * 128, (k + 1) * 128)
                P.dma("pool", lambda e, k=k, rows=rows: e.dma_start(out=wqm[:, k, :], in_=w_in[rows, C_QM:C_QM + 512]), writes=[d_w])
                for j in range(2):
                    P.dma("pool", lambda e, k=k, rows=rows, j=j: e.dma_start(out=wg[:, k, j * 1536:(j + 1) * 1536], in_=w_in[rows, C_G + j * 1536:C_G + (j + 1) * 1536]), writes=[d_w])
                P.dma("pool", lambda e, k=k, rows=rows: e.dma_start(out=wos[:, k, :], in_=wos_d[rows, :]), writes=[d_w])
                P.dma("pool", lambda e, k=k, rows=rows: e.dma_start(out=wout[:, k, :], in_=wout_d[rows, :]), writes=[d_w])
                P.dma("sp", lambda e, k=k, rows=rows: e.dma_start(out=wr[:, k, :], in_=wr_d[rows, :]), writes=[d_w])
            for k in range(4):
                rows = slice(k * 128, (k + 1) * 128)
                P.dma("pool", lambda e, k=k, rows=rows: e.dma_start(out=woa[:, k, :], in_=woa_d[rows, :]), writes=[d_w])
                P.dma("pool", lambda e, k=k, rows=rows: e.dma_start(out=wom[:, k, :], in_=wom_d[rows, :]), writes=[d_w])
            def loads(tj):
                xj = tj % 2
                rj = tj * 128
                P.dma("sp", lambda e: e.dma_start(out=xt[xj][:], in_=xown[rj:rj + 128, :]), writes=[d_xt[xj]])
                P.dma("sp", lambda e: e.dma_start(out=oat[xj][:], in_=oa_d[rj:rj + 128, :]), reads=[d_oad], writes=[d_oat[xj]])
                P.dma("sp", lambda e: e.dma_start(out=ost[xj][:], in_=os_d[rj:rj + 128, :]), reads=[d_osd], writes=[d_ost[xj]])

            for ti in range(ntiles):
                r0 = ti * 128
                hbuf = ti % 2
                xb = ti % 2
                if ti == 0:
                    loads(0)
                if ti + 1 < ntiles:
                    loads(ti + 1)
                P.op("act", lambda e, xb=xb: e.activation(out=junk[:], in_=xt[xb][:], func=AF.Square, accum_out=ssq[:]), reads=[d_xt[xb]], writes=[d_junk, d_ssq])
                P.op("act", lambda e: e.activation(out=rstd[:], in_=ssq[:], func=AF.Sqrt, bias=EPS, scale=1.0 / D), reads=[d_ssq], writes=[d_rstd])
                P.op("dve", lambda e: e.reciprocal(out=rstd[:], in_=rstd[:]), reads=[d_rstd], writes=[d_rstd])
                P.op("dve", lambda e, xb=xb: e.scalar_tensor_tensor(out=hb[:], in0=xt[xb][:], scalar=rstd[:, 0:1], in1=gm[:], op0=ALU.mult, op1=ALU.mult),
                     reads=[d_xt[xb], d_rstd, d_c], writes=[d_hb])
                for k in range(8):
                    P.op("pe", lambda e, k=k: e.transpose(out=pT[:, k, :], in_=hb[:, k * 128:(k + 1) * 128], identity=idb[:]), reads=[d_hb, d_idb], writes=[d_pT])
                P.op("act", lambda e: e.copy(out=hT[:], in_=pT[:]), reads=[d_pT], writes=[d_hT])
                for k in range(8):
                    P.op("pe", lambda e, k=k: e.matmul(pG[0][:, :], lhsT=hT[:, k, :], rhs=wqm[:, k, :], start=(k == 0), stop=(k == 7)),
                         reads=[d_hT, d_w], writes=[d_pG[0]])
                head_norm(pG[0][:, :], d_pG[0], mqn, 4, qmb[:], d_qmb)
                for g6 in range(6):
                    bk = g6 % 3
                    for k in range(8):
                        P.op("pe", lambda e, g6=g6, k=k, bk=bk: e.matmul(pG[bk][:, :], lhsT=hT[:, k, :], rhs=wg[:, k, g6 * 512:(g6 + 1) * 512], start=(k == 0), stop=(k == 7)),
                             reads=[d_hT, d_w], writes=[d_pG[bk]])
                    P.op("act", lambda e, g6=g6, bk=bk: e.activation(out=gs[:, g6 * 512:(g6 + 1) * 512], in_=pG[bk][:, :], func=AF.Sigmoid),
                         reads=[d_pG[bk]], writes=[d_gs])
                for h in range(4):
                    P.op("pe", lambda e, h=h: e.transpose(out=pT[:, h, :], in_=qmb[:, h * 128:(h + 1) * 128], identity=idb[:]), reads=[d_qmb, d_idb], writes=[d_pT])
                P.op("act", lambda e: e.copy(out=qmT[:], in_=pT[:, 0:4, :]), reads=[d_pT], writes=[d_qmT])
                for h in range(4):
                    for mt in range(2):
                        bank = pms[h // 2]
                        c0 = ((h % 2) * 2 + mt) * 128
                        P.op("pe", lambda e, h=h, mt=mt, bank=bank, c0=c0: e.matmul(bank[:, c0:c0 + 128], lhsT=kmT[:, h, mt * 128:(mt + 1) * 128], rhs=qmT[:, h, :],
                                                                                  start=True, stop=True), reads=[d_kmT, d_qmT], writes=[d_pms[h // 2]])
                for hp in range(2):
                    P.op("act", lambda e, hp=hp: e.activation(out=pm[:, hp * 2:(hp + 1) * 2, :, :].rearrange("p a b c -> p (a b c)"), in_=pms[hp][:, :], func=AF.Exp,
                                                              scale=float(128 ** -0.5)), reads=[d_pms[hp]], writes=[d_pm])
                for h in range(4):
                    for mt in range(2):
                        P.op("pe", lambda e, h=h, mt=mt: e.matmul(pmo[:, h * 128:(h + 1) * 128], lhsT=vm[:, mt, h * 128:(h + 1) * 128], rhs=pm[:, h, mt, :],
                                                                 start=(mt == 0), stop=(mt == 1)), reads=[d_vm, d_pm], writes=[d_pmo])
                for mt in range(2):
                    P.op("pe", lambda e, mt=mt: e.matmul(pG[1][:, :].rearrange("p (h t) -> p h t", h=4), lhsT=onesb[:], rhs=pm[:, :, mt, :],
                                                        start=(mt == 0), stop=(mt == 1)), reads=[d_c, d_pm], writes=[d_pG[1]])
                P.op("dve", lambda e: e.reciprocal(out=rcp[:], in_=pG[1][:, :]), reads=[d_pG[1]], writes=[d_rcp])
                P.op("dve", lambda e: e.tensor_tensor(out=omT[:].rearrange("p h t -> p (h t)"), in0=pmo[:, :], in1=rcp[:], op=ALU.mult),
                     reads=[d_pmo, d_rcp], writes=[d_omT])
                for k in range(4):
                    P.op("pe", lambda e, xb=xb, k=k: e.transpose(out=pT[:, k, :], in_=oat[xb][:, k * 128:(k + 1) * 128], identity=idb[:]), reads=[d_oat[xb], d_idb], writes=[d_pT])
                P.op("act", lambda e: e.copy(out=oaT[:], in_=pT[:, 0:4, :]), reads=[d_pT], writes=[d_oaT])
                for k in range(8):
                    P.op("pe", lambda e, xb=xb, k=k: e.transpose(out=pT[:, k, :], in_=ost[xb][:, k * 128:(k + 1) * 128], identity=idb[:]), reads=[d_ost[xb], d_idb], writes=[d_pT])
                P.op("act", lambda e: e.copy(out=osT[:], in_=pT[:]), reads=[d_pT], writes=[d_osT])
                for hv in range(2):
                    cs_ = slice(hv * 512, (hv + 1) * 512)
                    for k in range(4):
                        P.op("pe", lambda e, k=k, cs_=cs_: e.matmul(pG[0][:, :], lhsT=oaT[:, k, :], rhs=woa[:, k, cs_], start=(k == 0), stop=(k == 3)),
                             reads=[d_oaT, d_w], writes=[d_pG[0]])
                    for k in range(8):
                        P.op("pe", lambda e, k=k, cs_=cs_: e.matmul(pG[1][:, :], lhsT=osT[:, k, :], rhs=wos[:, k, cs_], start=(k == 0), stop=(k == 7)),
                             reads=[d_osT, d_w], writes=[d_pG[1]])
                    for k in range(4):
                        P.op("pe", lambda e, k=k, cs_=cs_: e.matmul(pG[2][:, :], lhsT=omT[:, k, :], rhs=wom[:, k, cs_], start=(k == 0), stop=(k == 3)),
                             reads=[d_omT, d_w], writes=[d_pG[2]])
                    P.op("dve", lambda e, hv=hv: e.tensor_tensor(out=mg[:], in0=pG[0][:, :], in1=gs[:, hv * 512:(hv + 1) * 512], op=ALU.mult),
                         reads=[d_pG[0], d_gs], writes=[d_mg])
                    P.op("dve", lambda e, hv=hv: e.tensor_tensor(out=tt[:], in0=pG[1][:, :], in1=gs[:, 1024 + hv * 512:1024 + (hv + 1) * 512], op=ALU.mult),
                         reads=[d_pG[1], d_gs], writes=[d_tt])
                    P.op("pool", lambda e: e.tensor_tensor(out=mg[:], in0=mg[:], in1=tt[:], op=ALU.add), reads=[d_mg, d_tt], writes=[d_mg])
                    P.op("dve", lambda e, hv=hv: e.tensor_tensor(out=tt[:], in0=pG[2][:, :], in1=gs[:, 2048 + hv * 512:2048 + (hv + 1) * 512], op=ALU.mult),
                         reads=[d_pG[2], d_gs], writes=[d_tt])
                    P.op("pool", lambda e, cs_=cs_: e.tensor_tensor(out=mgb[:, cs_], in0=mg[:], in1=tt[:], op=ALU.add), reads=[d_mg, d_tt], writes=[d_mgb])
                for k in range(8):
                    P.op("pe", lambda e, k=k: e.transpose(out=pT[:, k, :], in_=mgb[:, k * 128:(k + 1) * 128], identity=idb[:]), reads=[d_mgb, d_idb], writes=[d_pT])
                P.op("act", lambda e: e.copy(out=mgT[:], in_=pT[:]), reads=[d_pT], writes=[d_mgT])
                for hv in range(2):
                    cs_ = slice(hv * 512, (hv + 1) * 512)
                    for k in range(8):
                        P.op("pe", lambda e, k=k, cs_=cs_, hv=hv: e.matmul(pG[hv][:, :], lhsT=mgT[:, k, :], rhs=wout[:, k, cs_], start=(k == 0), stop=(k == 7)),
                             reads=[d_mgT, d_w], writes=[d_pG[hv]])
                    P.op("dve", lambda e, xb=xb, cs_=cs_, hv=hv: e.tensor_tensor(out=x1[:, cs_], in0=pG[hv][:, :], in1=xt[xb][:, cs_], op=ALU.add),
                         reads=[d_pG[hv], d_xt[xb]], writes=[d_x1])
                P.dma("sp", lambda e, r0=r0: e.dma_start(out=x1_d[r0:r0 + 128, :], in_=x1[:]), reads=[d_x1], writes=[d_x1d])
                if stage < 2:
                    continue
                P.op("act", lambda e: e.activation(out=junk[:], in_=x1[:], func=AF.Square, accum_out=ssq[:]), reads=[d_x1], writes=[d_junk, d_ssq])
                P.op("act", lambda e: e.activation(out=rstd[:], in_=ssq[:], func=AF.Sqrt, bias=EPS, scale=1.0 / D), reads=[d_ssq], writes=[d_rstd])
                P.op("dve", lambda e: e.reciprocal(out=rstd[:], in_=rstd[:]), reads=[d_rstd], writes=[d_rstd])
                P.op("dve", lambda e: e.scalar_tensor_tensor(out=h2f[:], in0=x1[:], scalar=rstd[:, 0:1], in1=gf[:], op0=ALU.mult, op1=ALU.mult),
                     reads=[d_x1, d_rstd, d_c], writes=[d_h2f])
                P.op("pool", lambda e, hbuf=hbuf: e.tensor_copy(out=h2b[hbuf][:], in_=h2f[:]), reads=[d_h2f], writes=[d_h2b[hbuf]])
                for half in range(2):
                    for k4 in range(4):
                        k = half * 4 + k4
                        P.op("pe", lambda e, k=k, k4=k4: e.transpose(out=pF[:, k4 * 128:(k4 + 1) * 128], in_=h2f[:, k * 128:(k + 1) * 128], identity=idf[:]),
                             reads=[d_h2f, d_idf], writes=[d_pF])
                    P.op("act", lambda e, half=half: e.copy(out=h2T[:, half * 4:(half + 1) * 4, :].rearrange("p a b -> p (a b)"), in_=pF[:, :]), reads=[d_pF], writes=[d_h2T])
                for k in range(8):
                    P.op("pe", lambda e, k=k: e.matmul(pF[:, 0:36], lhsT=h2T[:, k, :], rhs=wr[:, k, :], start=(k == 0), stop=(k == 7)), reads=[d_h2T, d_w], writes=[d_pF])
                P.op("act", lambda e: e.copy(out=lg[:], in_=pF[:, 0:36]), reads=[d_pF], writes=[d_lg])
                P.op("dve", lambda e: e.tensor_reduce(out=sm[:, 0:1], in_=lg[:, 0:4], axis=AX.X, op=ALU.max), reads=[d_lg], writes=[d_sm])
                P.op("dve", lambda e: e.tensor_scalar(out=sm[:, 1:2], in0=sm[:, 0:1], scalar1=-1.0, scalar2=None, op0=ALU.mult), reads=[d_sm], writes=[d_sm])
                P.op("act", lambda e: e.activation(out=sm[:, 8:12], in_=lg[:, 0:4], func=AF.Exp, bias=sm[:, 1:2], accum_out=sm[:, 2:3]), reads=[d_lg, d_sm], writes=[d_sm])
                P.op("dve", lambda e: e.reciprocal(out=sm[:, 3:4], in_=sm[:, 2:3]), reads=[d_sm], writes=[d_sm])
                P.op("dve", lambda e: e.tensor_scalar(out=goh[:], in0=lg[:, 0:4], scalar1=sm[:, 0:1], scalar2=None, op0=ALU.is_ge), reads=[d_lg, d_sm], writes=[d_goh])
                P.op("dve", lambda e: e.tensor_tensor(out=lsel[:].rearrange("p (g e) -> p g e", g=4), in0=lg[:, 4:36].rearrange("p (g e) -> p g e", g=4),
                                                      in1=goh[:].unsqueeze(2).to_broadcast([128, 4, 8]), op=ALU.mult), reads=[d_lg, d_goh], writes=[d_lsel])
                P.op("dve", lambda e: e.tensor_reduce(out=les[:], in_=lsel[:].rearrange("p (g e) -> p e g", g=4), axis=AX.X, op=ALU.add), reads=[d_lsel], writes=[d_les])
                P.op("dve", lambda e: e.max(out=t8[:], in_=les[:]), reads=[d_les], writes=[d_t8])
                P.op("dve", lambda e: e.tensor_tensor(out=sm[:, 4:5], in0=t8[:, 1:2], in1=t8[:, 0:1], op=ALU.subtract), reads=[d_t8, d_sm], writes=[d_sm])
                P.op("act", lambda e: e.activation(out=sm[:, 5:6], in_=sm[:, 4:5], func=AF.Exp), reads=[d_sm], writes=[d_sm])
                P.op("dve", lambda e: e.tensor_scalar(out=sm[:, 6:7], in0=sm[:, 5:6], scalar1=1.0, scalar2=None, op0=ALU.add), reads=[d_sm], writes=[d_sm])
                P.op("dve", lambda e: e.reciprocal(out=sm[:, 6:7], in_=sm[:, 6:7]), reads=[d_sm], writes=[d_sm])
                P.op("dve", lambda e: e.tensor_tensor(out=sm[:, 7:8], in0=sm[:, 5:6], in1=sm[:, 6:7], op=ALU.mult), reads=[d_sm], writes=[d_sm])
                P.op("dve", lambda e, ti=ti: e.tensor_scalar(out=combs[:, ti, :], in0=sm[:, 6:8], scalar1=sm[:, 3:4], scalar2=None, op0=ALU.mult), reads=[d_sm], writes=[d_combs])
                for j in range(2):
                    P.op("dve", lambda e, j=j: e.tensor_scalar(out=oh[:, j * 32:(j + 1) * 32], in0=lg[:, 4:36], scalar1=t8[:, j:j + 1], scalar2=None, op0=ALU.is_equal),
                         reads=[d_lg, d_t8], writes=[d_oh])
                    P.op("dve", lambda e, j=j: e.tensor_tensor(out=oh[:, j * 32:(j + 1) * 32].rearrange("p (g e) -> p g e", g=4), in0=oh[:, j * 32:(j + 1) * 32].rearrange("p (g e) -> p g e", g=4),
                                                               in1=goh[:].unsqueeze(2).to_broadcast([128, 4, 8]), op=ALU.mult), reads=[d_oh, d_goh], writes=[d_oh])
                P.op("dve", lambda e: e.tensor_tensor(out=ohs[:], in0=oh[:, 0:32], in1=oh[:, 32:64], op=ALU.add), reads=[d_oh], writes=[d_ohs])
                P.op("pe", lambda e: e.matmul(pF[:, 64:96], lhsT=tri[:], rhs=ohs[:], start=True, stop=True), reads=[d_c, d_ohs], writes=[d_pF])
                P.op("pe", lambda e: e.matmul(pF[:, 128:160], lhsT=onesf[:], rhs=ohs[:], start=True, stop=True), reads=[d_c, d_ohs], writes=[d_pF])
                P.op("dve", lambda e: e.tensor_tensor(out=posf[:], in0=pF[:, 64:96], in1=ohs[:], op=ALU.subtract), reads=[d_pF, d_ohs], writes=[d_posf])
                P.op("dve", lambda e: e.tensor_tensor(out=posf[:], in0=posf[:], in1=base[:], op=ALU.add), reads=[d_posf, d_base], writes=[d_posf])
                P.op("dve", lambda e: e.tensor_tensor(out=posf[:], in0=posf[:], in1=ecap[:], op=ALU.add), reads=[d_posf, d_c], writes=[d_posf])
                P.op("dve", lambda e: e.tensor_tensor(out=base[:], in0=base[:], in1=pF[:, 128:160], op=ALU.add), reads=[d_base, d_pF, d_posf], writes=[d_base])
                for j in range(2):
                    P.op("dve", lambda e, j=j: e.tensor_tensor(out=oh[:, j * 32:(j + 1) * 32], in0=oh[:, j * 32:(j + 1) * 32], in1=posf[:], op=ALU.mult), reads=[d_oh, d_posf], writes=[d_oh])
                    P.op("dve", lambda e, j=j: e.tensor_reduce(out=idf2[:, j:j + 1], in_=oh[:, j * 32:(j + 1) * 32], axis=AX.X, op=ALU.add), reads=[d_oh], writes=[d_idf2])
                P.op("dve", lambda e, ti=ti: e.tensor_copy(out=idxs[:, ti, :], in_=idf2[:]), reads=[d_idf2], writes=[d_idxs])
                for j in range(2):
                    P.dma("pool", lambda e, ti=ti, j=j, hbuf=hbuf: e.indirect_dma_start(out=xbuf[:, :], out_offset=bass.IndirectOffsetOnAxis(ap=idxs[:, ti, j:j + 1], axis=0),
                                                                                      in_=h2b[hbuf][:], in_offset=None),
                          reads=[d_h2b[hbuf], d_idxs], writes=[d_xbuf])

        if stage < 3:
            return
        if hasattr(P, 'barrier'):
            P.barrier()
        with contextlib.ExitStack() as st:
            def sb(name, shape, dt):
                return st.enter_context(nc.sbuf_tensor("d_" + name, shape, dt))

            def ps(name, shape, dt=F32):
                return st.enter_context(nc.psum_tensor(name, shape, dt))
            NW = 2
            wge = [sb("wge%d" % i, [128, 8, 512], BF16) for i in range(NW)]; d_wge = [Dep() for _ in range(NW)]
            wue = [sb("wue%d" % i, [128, 8, 512], BF16) for i in range(NW)]; d_wue = [Dep() for _ in range(NW)]
            wde = [sb("wde%d" % i, [128, 4, 1024], BF16) for i in range(NW)]; d_wde = [Dep() for _ in range(NW)]
            xe = [sb("xe%d" % i, [128, 3, D], BF16) for i in range(NW)]; d_xe = [Dep() for _ in range(NW)]
            xeT = sb("xeT", [128, 8, CAP], BF16); d_xeT = Dep()
            sg = sb("sg", [128, CAP], F32); d_sg = Dep()
            hTe = sb("hTe", [128, 4, CAP], BF16); d_hTe = Dep()
            ye = [sb("ye%d" % i, [128, 3, D], F32) for i in range(NW)]; d_ye = [Dep() for _ in range(NW)]
            pT = ps("pTe", [128, 8, 128], BF16); d_pT = Dep()
            pg_ = [ps("pge%d" % i, [128, 512], F32) for i in range(2)]; d_pg = [Dep() for _ in range(2)]
            pu_ = [ps("pue%d" % i, [128, 512], F32) for i in range(2)]; d_pu = [Dep() for _ in range(2)]
            py_ = [ps("pye%d" % i, [128, 512], F32) for i in range(2)]; d_py = [Dep() for _ in range(2)]
            for ie, ex in enumerate(experts):
                b = ie % NW
                P.dma("pool", lambda e, b=b, ex=ex: e.dma_start(out=wge[b][:], in_=wgate_d[ex].rearrange("(k p) f -> p k f", p=128)), writes=[d_wge[b]])
                P.dma("pool", lambda e, b=b, ex=ex: e.dma_start(out=wue[b][:], in_=wup_d[ex].rearrange("(k p) f -> p k f", p=128)), writes=[d_wue[b]])
                P.dma("pool", lambda e, b=b, ex=ex: e.dma_start(out=wde[b][:], in_=wdown_d[ex].rearrange("(k p) f -> p k f", p=128)), writes=[d_wde[b]])
                P.dma("sp", lambda e, b=b, ex=ex: e.dma_start(out=xe[b][:], in_=xbuf[ex * CAP:(ex + 1) * CAP, :].rearrange("(s p) d -> p s d", p=128)),
                      reads=[d_xbuf], writes=[d_xe[b]])
                for s in range(3):
                    for k in range(8):
                        P.op("pe", lambda e, b=b, s=s, k=k: e.transpose(out=pT[:, k, :], in_=xe[b][:, s, k * 128:(k + 1) * 128], identity=idb[:]),
                             reads=[d_xe[b], d_idb], writes=[d_pT])
                    P.op("act", lambda e, s=s: e.copy(out=xeT[:, :, s * 128:(s + 1) * 128], in_=pT[:]), reads=[d_pT], writes=[d_xeT])
                for ft in range(4):
                    pb = ft % 2
                    for k in range(8):
                        P.op("pe", lambda e, b=b, ft=ft, k=k, pb=pb: e.matmul(pg_[pb][:, 0:CAP], lhsT=wge[b][:, k, ft * 128:(ft + 1) * 128], rhs=xeT[:, k, :],
                                                                            start=(k == 0), stop=(k == 7)), reads=[d_wge[b], d_xeT], writes=[d_pg[pb]])
                    for k in range(8):
                        P.op("pe", lambda e, b=b, ft=ft, k=k, pb=pb: e.matmul(pu_[pb][:, 0:CAP], lhsT=wue[b][:, k, ft * 128:(ft + 1) * 128], rhs=xeT[:, k, :],
                                                                            start=(k == 0), stop=(k == 7)), reads=[d_wue[b], d_xeT], writes=[d_pu[pb]])
                    P.op("act", lambda e, pb=pb: e.activation(out=sg[:], in_=pg_[pb][:, 0:CAP], func=AF.Silu), reads=[d_pg[pb]], writes=[d_sg])
                    P.op("dve", lambda e, ft=ft, pb=pb: e.tensor_tensor(out=hTe[:, ft, :], in0=sg[:], in1=pu_[pb][:, 0:CAP], op=ALU.mult),
                         reads=[d_sg, d_pu[pb]], writes=[d_hTe])
                for s in range(3):
                    for hv in range(2):
                        for ft in range(4):
                            P.op("pe", lambda e, b=b, s=s, hv=hv, ft=ft: e.matmul(py_[hv][:, :], lhsT=hTe[:, ft, s * 128:(s + 1) * 128], rhs=wde[b][:, ft, hv * 512:(hv + 1) * 512],
                                                                                start=(ft == 0), stop=(ft == 3)), reads=[d_hTe, d_wde[b]], writes=[d_py[hv]])
                        P.op("act" if hv == 0 else "dve", (lambda e, b=b, s=s, hv=hv: e.copy(out=ye[b][:, s, hv * 512:(hv + 1) * 512], in_=py_[hv][:, :])) if hv == 0 else
                             (lambda e, b=b, s=s, hv=hv: e.tensor_copy(out=ye[b][:, s, hv * 512:(hv + 1) * 512], in_=py_[hv][:, :])),
                             reads=[d_py[hv]], writes=[d_ye[b]])
                P.dma("sp", lambda e, b=b, ex=ex: e.dma_start(out=ybuf[ex * CAP:(ex + 1) * CAP, :].rearrange("(s p) d -> p s d", p=128), in_=ye[b][:]),
                      reads=[d_ye[b]], writes=[d_ybuf])

        if hasattr(P, 'barrier'):
            P.barrier()
        with contextlib.ExitStack() as st:
            def sb(name, shape, dt):
                return st.enter_context(nc.sbuf_tensor("d_" + name, shape, dt))
            NF = 2
            x1t = [sb("x1t%d" % i, [128, D], F32) for i in range(NF)]; d_x1t = [Dep() for _ in range(NF)]
            y1 = [sb("y1t%d" % i, [128, D], F32) for i in range(NF)]; d_y1 = [Dep() for _ in range(NF)]
            y2 = [sb("y2t%d" % i, [128, D], F32) for i in range(NF)]; d_y2 = [Dep() for _ in range(NF)]
            ot = [sb("ot%d" % i, [128, D], F32) for i in range(NF)]; d_ot = [Dep() for _ in range(NF)]
            for ti in range(ntiles):
                b = ti % NF
                r0 = ti * 128
                P.dma("sp", lambda e, b=b, r0=r0: e.dma_start(out=x1t[b][:], in_=x1_d[r0:r0 + 128, :]), reads=[d_x1d], writes=[d_x1t[b]])
                P.dma("pool", lambda e, b=b, ti=ti: e.indirect_dma_start(out=y1[b][:], out_offset=None, in_=ybuf[:, :],
                                                                        in_offset=bass.IndirectOffsetOnAxis(ap=idxs[:, ti, 0:1], axis=0)),
                      reads=[d_ybuf, d_idxs], writes=[d_y1[b]])
                P.dma("pool", lambda e, b=b, ti=ti: e.indirect_dma_start(out=y2[b][:], out_offset=None, in_=ybuf[:, :],
                                                                        in_offset=bass.IndirectOffsetOnAxis(ap=idxs[:, ti, 1:2], axis=0)),
                      reads=[d_ybuf, d_idxs], writes=[d_y2[b]])
                P.op("dve", lambda e, b=b, ti=ti: e.scalar_tensor_tensor(out=ot[b][:], in0=y1[b][:], scalar=combs[:, ti, 0:1], in1=x1t[b][:], op0=ALU.mult, op1=ALU.add),
                     reads=[d_y1[b], d_combs, d_x1t[b]], writes=[d_ot[b]])
                P.op("dve", lambda e, b=b, ti=ti: e.scalar_tensor_tensor(out=ot[b][:], in0=y2[b][:], scalar=combs[:, ti, 1:2], in1=ot[b][:], op0=ALU.mult, op1=ALU.add),
                     reads=[d_y2[b], d_combs, d_ot[b]], writes=[d_ot[b]])
                P.dma("sp", lambda e, b=b, r0=r0: e.dma_start(out=out_d[r0:r0 + 128, :], in_=ot[b][:]), reads=[d_ot[b]], writes=[d_out])


def host_consts_d(z):
    tri = (np.arange(128)[:, None] <= np.arange(128)[None, :]).astype(np.float32)
    return dict(mqn=np.tile(z['mem_q_norm'], 4)[None, :], mkn=np.tile(z['mem_k_norm'], 4)[None, :],
                w_r=np.ascontiguousarray(np.concatenate([z['w_router_group'], z['w_router_expert']], axis=1)),
                trif=tri, ecap=(np.arange(32, dtype=np.float32) * CAP)[None, :], ident=np.eye(128, dtype=np.float32))


def build_full():
    nc = bass.Bass("TRN2", target_bir_lowering=False)
    P = Prog(nc)

    def di(name, shape, dt=F32):
        return nc.dram_tensor(name, shape, dt, kind="ExternalInput")
    xin = di("xin", [TP + T, D]); w_in = di("w_in", [D, IN_COLS]); g_mix = di("g_mix", [1, D])
    qkn = di("qkn", [2, 512]); cs = di("cs", [TP + T, 64]); ident_d = di("ident", [128, 128])
    e_d = di("e_d", [32, NK], BF16); vq_d = di("vq", [1, 1024]); nb_d = di("nb", [1, 1024]); oq_d = di("oq", [1, 1024])
    tri_d = di("tri", [128, 128], BF16)
    cwl_d = di("cwl", [128, 16, 4]); cbl_d = di("cbl", [128, 16]); dtb_d = di("dtb", [1, 16]); alog_d = di("alog", [1, 16])
    dsk_d = di("dsk", [1, 16]); ssdn_d = di("ssdn", [1, D]); tri2_d = di("tri2", [128, 2, 256]); sel_d = di("sel_d", [16, 16, 128])
    tb_d = di("tb_d", [128, 384]); pflag_d = di("pflag", [1, 1])
    mem_d = di("mem", [256, D]); gmem_d = di("g_mem", [1, D]); wmemkv = di("w_mem_kv", [D, 1024])
    mqn_d = di("mqn", [1, 512]); mkn_d = di("mkn", [1, 512])
    woa_d = di("w_o_moba", [512, D]); wos_d = di("w_o_ssd", [D, D]); wom_d = di("w_o_mem", [512, D]); wout_d = di("w_out", [D, D])
    gffn_d = di("g_ffn", [1, D]); wr_d = di("w_r", [D, 36])
    wgate_d = di("w_gate", [NE, D, 512]); wup_d = di("w_up", [NE, D, 512]); wdown_d = di("w_down", [NE, 512, D])
    trif_d = di("trif", [128, 128]); ecap_d = di("ecap", [1, 32])
    out_d = nc.dram_tensor("out", [T, D], F32, kind="ExternalOutput")
    kT_d = nc.dram_tensor("kT_d", [512, NK], BF16)
    v_d = nc.dram_tensor("v_d", [NK, 512], BF16)
    qT_d = nc.dram_tensor("qT_d", [512, T], BF16)
    oa_d = nc.dram_tensor("oa_d", [T, 512], BF16)
    os_d = nc.dram_tensor("os_d", [T, D], BF16)
    x1_d = nc.dram_tensor("x1_d", [T, D], F32)
    xbuf = nc.dram_tensor("xbuf", [NE * CAP, D], BF16)
    ybuf = nc.dram_tensor("ybuf", [NE * CAP, D], F32)

    with contextlib.ExitStack() as st0:
        idf = st0.enter_context(nc.sbuf_tensor("idf", [128, 128], F32)); d_idf = Dep()
        idb = st0.enter_context(nc.sbuf_tensor("idb", [128, 128], BF16)); d_idb = Dep()
        P.dma("sp", lambda e: e.dma_start(out=idf[:], in_=ident_d[:, :]), writes=[d_idf])
        P.op("dve", lambda e: e.tensor_copy(out=idb[:], in_=idf[:]), reads=[d_idf], writes=[d_idb])
        with contextlib.ExitStack() as st:
            phase_a(nc, P, st, xin, w_in, g_mix, qkn, cs, idb, d_idb, kT_d, v_d, qT_d)
        P.barrier()
        with contextlib.ExitStack() as st:
            oa = st.enter_context(nc.sbuf_tensor("s_oa", [128, 32, 512], BF16)); d_oa = Dep()
            phase_b(nc, P, st, kT_d, v_d, qT_d, e_d, vq_d, nb_d, oq_d, tri_d, idb, d_idb, oa, d_oa)
            P.dma("sp", lambda e: e.dma_start(out=oa_d[:, :].rearrange("(t p) c -> p t c", p=128), in_=oa[:]), reads=[d_oa])
        P.barrier()
        with contextlib.ExitStack() as st:
            phase_c(nc, P, st, xin, w_in, g_mix, cwl_d, cbl_d, dtb_d, alog_d, dsk_d, ssdn_d, tri2_d, sel_d, tb_d, pflag_d,
                    idb, d_idb, os_d, Dep())
        P.barrier()
        phase_def(nc, P, xin[TP:TP + T, :], w_in, g_mix, mem_d, gmem_d, wmemkv, mqn_d, mkn_d, woa_d, wos_d, wom_d, wout_d, gffn_d, wr_d,
                  wgate_d, wup_d, wdown_d, trif_d, ecap_d, oa_d, Dep(), os_d, Dep(), x1_d, xbuf, ybuf, out_d, idb, d_idb, idf, d_idf)
        P.emit()
    return nc


_NC_CACHE = {}


def kernel(x, mem, g_mix, w_in, moba_q_norm, moba_k_norm, conv_w, conv_b, dt_bias, a_log, d_skip, ssd_norm, g_mem, w_mem_kv,
           mem_q_norm, mem_k_norm, w_o_moba, w_o_ssd, w_o_mem, w_out, g_ffn, w_router_group, w_router_expert, w_gate, w_up, w_down):
    f32 = np.float32
    A = lambda a: np.ascontiguousarray(np.asarray(a, dtype=f32))
    x = A(x); mem = A(mem)
    z = dict(conv_w=A(conv_w), conv_b=A(conv_b), dt_bias=A(dt_bias), a_log=A(a_log), d_skip=A(d_skip), ssd_norm=A(ssd_norm),
             mem_q_norm=A(mem_q_norm), mem_k_norm=A(mem_k_norm), w_router_group=A(w_router_group), w_router_expert=A(w_router_expert))
    if "nc" not in _NC_CACHE:
        _NC_CACHE["nc"] = build_full()
    nc = _NC_CACHE["nc"]
    pos = np.arange(2 * T, dtype=f32)
    inv = (10000.0 ** (-np.arange(32, dtype=f32) / 32)).astype(f32)
    ang = pos[:, None] * inv[None, :]
    cs_full = np.concatenate([np.cos(ang), np.sin(ang)], axis=1).astype(f32)
    qkn = np.stack([np.tile(A(moba_q_norm), 8), np.tile(A(moba_k_norm), 8)]).astype(f32)
    shared = dict(w_in=A(w_in), g_mix=A(g_mix)[None, :], qkn=qkn, ident=np.eye(128, dtype=f32),
                  g_mem=A(g_mem)[None, :], w_mem_kv=A(w_mem_kv), w_o_moba=A(w_o_moba), w_o_ssd=A(w_o_ssd), w_o_mem=A(w_o_mem),
                  w_out=A(w_out), g_ffn=A(g_ffn)[None, :], w_gate=A(w_gate), w_up=A(w_up), w_down=A(w_down))
    shared.update(host_consts_d(z))
    in_maps = []
    for c in range(8):
        bb, hh = c // 2, c % 2
        if hh == 0:
            xin = np.concatenate([np.zeros((TP, D), f32), x[bb, :T]], 0)
            csc = np.concatenate([cs_full[:TP], cs_full[:T]], 0)
        else:
            xin = x[bb]
            csc = cs_full
        m = dict(shared)
        m.update(xin=np.ascontiguousarray(xin), cs=np.ascontiguousarray(csc), mem=mem[bb])
        m.update(host_consts(hh))
        m.update(host_consts_c(z, hh))
        in_maps.append(m)
    res = run_bass_kernel_spmd(nc, in_maps, core_ids=list(range(8)))
    out = np.empty((4, 2 * T, D), f32)
    for c in range(8):
        bb, hh = c // 2, c % 2
        out[bb, hh * T:(hh + 1) * T] = np.asarray(res.results[c]["out"], dtype=f32)
    return out
```

```python
import contextlib
import numpy as np
import ml_dtypes
import concourse.bass as bass
import concourse.mybir as mybir
from concourse.bass_utils import run_bass_kernel_spmd

F32 = mybir.dt.float32
BF16 = mybir.dt.bfloat16
I32 = mybir.dt.int32
U32 = mybir.dt.uint32
AF = mybir.ActivationFunctionType
ALU = mybir.AluOpType
AX = mybir.AxisListType

T = 4096
TP = 4096
NK = TP + T
D = 1024
EPS = 1e-6
IN_COLS = 8208
BIGG = 30000.0
MB = 1000.0
NEG = -30000.0
C_Z, C_X, C_DT = 1536, 2560, 4608
C_QM, C_G = 4624, 5136
CAP = 384
NE = 32


class Dep:
    __slots__ = ("w", "r")

    def __init__(self):
        self.w = None
        self.r = {}


class Op:
    __slots__ = ("eng", "fn", "deps", "is_dma", "sig", "sigval", "dsem", "dval", "prev")

    def __init__(self, eng, fn, is_dma):
        self.eng = eng
        self.fn = fn
        self.is_dma = is_dma
        self.deps = []
        self.sig = False
        self.sigval = 0
        self.dsem = None
        self.dval = 0
        self.prev = None


class Prog:
    ENGS = ("pe", "act", "dve", "pool", "sp")

    def __init__(self, nc, ndma_sems=12):
        self.nc = nc
        self.ops = {e: [] for e in self.ENGS}
        self.ndma = {e: 0 for e in self.ENGS}
        self.dma_last = {}
        self.ndma_sems = ndma_sems
        self.all_dma = []

    def _add(self, o, reads, writes):
        deps = {}
        raw = set()
        for t in reads:
            if t.w is not None:
                deps[id(t.w)] = t.w
                raw.add(id(t.w))
        for t in writes:
            if t.w is not None:
                deps[id(t.w)] = t.w
            for r in t.r.values():
                deps[id(r)] = r
        for t in reads:
            key = id(o) if o.is_dma else o.eng
            t.r[key] = o
        for t in writes:
            t.w = o
            t.r = {}
        dl = []
        for d in deps.values():
            if d is o:
                continue
            if (not d.is_dma) and (not o.is_dma) and d.eng == "pe" and o.eng == "pe":
                continue
            if (not d.is_dma) and (not o.is_dma) and d.eng == o.eng and id(d) not in raw:
                continue
            dl.append(d)
            if not d.is_dma:
                d.sig = True
        o.deps = dl
        self.ops[o.eng].append(o)
        return o

    def op(self, eng, fn, reads=(), writes=()):
        return self._add(Op(eng, fn, False), reads, writes)

    def dma(self, eng, fn, reads=(), writes=()):
        o = Op(eng, fn, True)
        n = self.ndma[eng]
        self.ndma[eng] += 1
        slot = (eng, n % self.ndma_sems)
        o.dsem = slot
        o.prev = self.dma_last.get(slot)
        o.dval = (o.prev.dval if o.prev else 0) + 16
        self.dma_last[slot] = o
        self.all_dma.append(o)
        return self._add(o, reads, writes)


    def barrier(self):
        lasts = []
        for e in self.ENGS:
            for o in reversed(self.ops[e]):
                if (not o.is_dma) and o.fn is not None:
                    o.sig = True
                    lasts.append(o)
                    break
        dmas = list(self.dma_last.values())
        for e in self.ENGS:
            o = Op(e, None, False)
            o.deps = [d for d in lasts if d.eng != e] + dmas
            self.ops[e].append(o)

    def emit(self):
        nc = self.nc
        import contextlib
        with contextlib.ExitStack() as st:
            esem = {e: st.enter_context(nc.semaphore("S_" + e)) for e in self.ENGS}
            dsem = {}
            for e in self.ENGS:
                if self.ndma[e]:
                    for i in range(min(self.ndma_sems, self.ndma[e])):
                        dsem[(e, i)] = st.enter_context(nc.semaphore("D_%s_%d" % (e, i)))
            for e in self.ENGS:
                c = 0
                for o in self.ops[e]:
                    if (not o.is_dma) and o.sig and o.fn is not None:
                        c += 1
                        o.sigval = c
            block = st.enter_context(nc.Block())
            handles = {"pe": block.tensor, "act": block.scalar, "dve": block.vector,
                       "pool": block.gpsimd, "sp": block.sync}

            def make(e):
                def body(eng):
                    known = {}

                    def wait(sem, key, val):
                        if known.get(key, 0) < val:
                            eng.wait_ge(sem, val)
                            known[key] = val
                    for o in self.ops[e]:
                        for d in o.deps:
                            if d.is_dma:
                                wait(dsem[d.dsem], d.dsem, d.dval)
                            else:
                                wait(esem[d.eng], d.eng, d.sigval)
                        if o.is_dma:
                            if o.prev is not None:
                                wait(dsem[o.dsem], o.dsem, o.prev.dval)
                            o.fn(eng).then_inc(dsem[o.dsem], 16)
                        elif o.fn is not None:
                            ins = o.fn(eng)
                            if o.sig:
                                ins.then_inc(esem[e], 1)
                    if e == "sp":
                        for slot, o in self.dma_last.items():
                            wait(dsem[slot], slot, o.dval)
                return body
            for e in self.ENGS:
                if self.ops[e] or e == "sp":
                    handles[e](make(e))


def phase_a(nc, P, st, xin, w_in, g_mix, qkn, cs, idb, d_idb, kT_d, v_d, qT_d):
    def sb(name, shape, dt):
        return st.enter_context(nc.sbuf_tensor("a_" + name, shape, dt))

    def ps(name, shape, dt=F32):
        return st.enter_context(nc.psum_tensor("a_" + name, shape, dt))
    wq = sb("wq", [128, 8, 1536], BF16); d_wq = Dep()
    gm = sb("gm", [128, D], F32); d_gm = Dep()
    gq = sb("gq", [128, 2, 512], F32); d_gq = Dep()
    NB = 2
    xt = [sb("xt%d" % i, [128, D], F32) for i in range(NB)]; d_xt = [Dep() for _ in range(NB)]
    cst = [sb("cst%d" % i, [128, 64], F32) for i in range(NB)]; d_cst = [Dep() for _ in range(NB)]
    junk = sb("junk", [128, D], BF16); d_junk = Dep()
    ssq = sb("ssq", [128, 1], F32); d_ssq = Dep()
    rstd = sb("rstd", [128, 1], F32); d_rstd = Dep()
    hb = sb("hb", [128, D], BF16); d_hb = Dep()
    hT = [sb("hT%d" % i, [128, 8, 128], BF16) for i in range(NB)]; d_hT = [Dep() for _ in range(NB)]
    pT = ps("pT", [128, 8, 128], BF16); d_pT = Dep()
    pq = [[ps("pq%d_%d" % (j, i), [128, 512], F32) for i in range(3)] for j in range(2)]; d_pq = [[Dep() for _ in range(3)] for _ in range(2)]
    pkT = ps("pkT", [128, 4, 128], BF16); d_pkT = Dep()
    sq_ = [sb("sq%d" % i, [128, 512], F32) for i in range(2)]; d_sq_ = [Dep(), Dep()]
    hs_ = [sb("hs%d" % i, [128, 8], F32) for i in range(2)]; d_hs_ = [Dep(), Dep()]
    hr_ = [sb("hr%d" % i, [128, 8], F32) for i in range(2)]; d_hr_ = [Dep(), Dep()]
    qn_ = [sb("qn%d" % i, [128, 512], F32) for i in range(2)]; d_qn_ = [Dep(), Dep()]
    t1_ = [sb("t1%d" % i, [128, 256], F32) for i in range(2)]; d_t1_ = [Dep(), Dep()]
    t2_ = [sb("t2%d" % i, [128, 256], F32) for i in range(2)]; d_t2_ = [Dep(), Dep()]
    t3_ = [sb("t3%d" % i, [128, 256], F32) for i in range(2)]; d_t3_ = [Dep(), Dep()]
    t4_ = [sb("t4%d" % i, [128, 256], F32) for i in range(2)]; d_t4_ = [Dep(), Dep()]
    qr_ = [sb("qr%d" % i, [128, 512], BF16) for i in range(2)]; d_qr_ = [Dep(), Dep()]
    kTs = [[sb("kTs%d_%d" % (j, i), [128, 4, 128], BF16) for i in range(NB)] for j in range(2)]; d_kTs = [[Dep() for _ in range(NB)] for _ in range(2)]
    vs = [sb("vs%d" % i, [128, 512], BF16) for i in range(NB)]; d_vs = [Dep() for _ in range(NB)]

    for k in range(8):
        P.dma("pool", lambda e, k=k: e.dma_start(out=wq[:, k, :], in_=w_in[k * 128:(k + 1) * 128, 0:1536]), writes=[d_wq])
    P.dma("sp", lambda e: e.dma_start(out=gm[:], in_=g_mix[0:1, :].partition_broadcast(128)), writes=[d_gm])
    for i in range(2):
        P.dma("sp", lambda e, i=i: e.dma_start(out=gq[:, i, :], in_=qkn[i:i + 1, :].partition_broadcast(128)), writes=[d_gq])
    P.op("dve", lambda e: e.tensor_scalar(out=gq[:, 0, :], in0=gq[:, 0, :], scalar1=0.125, scalar2=None, op0=ALU.mult),
         reads=[d_gq], writes=[d_gq])

    def qk_post_steps(src_ps, d_src, which, b):
        sl = which
        sq, hs, hr, qn, t1, t2, t3, t4, qr = sq_[sl], hs_[sl], hr_[sl], qn_[sl], t1_[sl], t2_[sl], t3_[sl], t4_[sl], qr_[sl]
        d_sq, d_hs, d_hr, d_qn, d_t1, d_t2, d_t3, d_t4, d_qr = (d_sq_[sl], d_hs_[sl], d_hr_[sl], d_qn_[sl], d_t1_[sl], d_t2_[sl],
                                                               d_t3_[sl], d_t4_[sl], d_qr_[sl])
        q3 = qn[:].rearrange("p (h d) -> p h d", h=8)
        q1 = q3[:, :, 0:32]
        q2 = q3[:, :, 32:64]
        cosb = cst[b][:, 0:32].unsqueeze(1).to_broadcast([128, 8, 32])
        sinb = cst[b][:, 32:64].unsqueeze(1).to_broadcast([128, 8, 32])
        v3 = lambda t: t[:].rearrange("p (h d) -> p h d", h=8)
        r3 = qr[:].rearrange("p (h d) -> p h d", h=8)
        steps = [
            lambda: P.op("act", lambda e: e.activation(out=sq[:], in_=src_ps[:], func=AF.Square), reads=[d_src], writes=[d_sq]),
            lambda: P.op("dve", lambda e: e.tensor_reduce(out=hs[:], in_=sq[:].rearrange("p (h d) -> p h d", h=8), axis=AX.X, op=ALU.add),
                         reads=[d_sq], writes=[d_hs]),
            lambda: P.op("act", lambda e: e.activation(out=hr[:], in_=hs[:], func=AF.Sqrt, bias=EPS, scale=1.0 / 64), reads=[d_hs], writes=[d_hr]),
            lambda: P.op("dve", lambda e: e.reciprocal(out=hr[:], in_=hr[:]), reads=[d_hr], writes=[d_hr]),
            lambda: P.op("dve", lambda e: e.tensor_tensor(out=qn[:].rearrange("p (h d) -> p h d", h=8), in0=src_ps[:].rearrange("p (h d) -> p h d", h=8),
                                                          in1=hr[:].unsqueeze(2).to_broadcast([128, 8, 64]), op=ALU.mult),
                         reads=[d_src, d_hr], writes=[d_qn]),
            lambda: P.op("pool", lambda e: e.tensor_tensor(out=qn[:], in0=qn[:], in1=gq[:, which, :], op=ALU.mult), reads=[d_qn, d_gq], writes=[d_qn]),
            lambda: P.op("dve", lambda e: e.tensor_tensor(out=v3(t1), in0=q1, in1=cosb, op=ALU.mult), reads=[d_qn, d_cst[b]], writes=[d_t1]),
            lambda: P.op("pool", lambda e: e.tensor_tensor(out=v3(t2), in0=q2, in1=sinb, op=ALU.mult), reads=[d_qn, d_cst[b]], writes=[d_t2]),
            lambda: P.op("dve", lambda e: e.tensor_tensor(out=v3(t3), in0=q2, in1=cosb, op=ALU.mult), reads=[d_qn, d_cst[b]], writes=[d_t3]),
            lambda: P.op("pool", lambda e: e.tensor_tensor(out=v3(t4), in0=q1, in1=sinb, op=ALU.mult), reads=[d_qn, d_cst[b]], writes=[d_t4]),
            lambda: P.op("dve", lambda e: e.tensor_tensor(out=r3[:, :, 0:32], in0=v3(t1), in1=v3(t2), op=ALU.subtract), reads=[d_t1, d_t2], writes=[d_qr]),
            lambda: P.op("pool", lambda e: e.tensor_tensor(out=r3[:, :, 32:64], in0=v3(t3), in1=v3(t4), op=ALU.add), reads=[d_t3, d_t4], writes=[d_qr]),
        ]
        return steps

    def to_T_and_store(dst_dram, col0, b, sl):
        for c in range(4):
            P.op("pe", lambda e, c=c: e.transpose(out=pkT[:, c, :], in_=qr_[sl][:, c * 128:(c + 1) * 128], identity=idb[:]),
                 reads=[d_qr_[sl], d_idb], writes=[d_pkT])
        P.op("act", lambda e: e.copy(out=kTs[sl][b][:], in_=pkT[:]), reads=[d_pkT], writes=[d_kTs[sl][b]])
        P.dma("sp", lambda e: e.dma_start(out=dst_dram[:, col0:col0 + 128].rearrange("(c p) t -> p c t", p=128), in_=kTs[sl][b][:]),
              reads=[d_kTs[sl][b]])

    def front(ti):
        b = ti % NB
        own = ti >= 32
        r0 = ti * 128
        P.dma("sp", lambda e: e.dma_start(out=xt[b][:], in_=xin[r0:r0 + 128, :]), writes=[d_xt[b]])
        P.dma("sp", lambda e: e.dma_start(out=cst[b][:], in_=cs[r0:r0 + 128, :]), writes=[d_cst[b]])
        P.op("act", lambda e: e.activation(out=junk[:], in_=xt[b][:], func=AF.Square, accum_out=ssq[:]), reads=[d_xt[b]], writes=[d_junk, d_ssq])
        P.op("act", lambda e: e.activation(out=rstd[:], in_=ssq[:], func=AF.Sqrt, bias=EPS, scale=1.0 / D), reads=[d_ssq], writes=[d_rstd])
        P.op("dve", lambda e: e.reciprocal(out=rstd[:], in_=rstd[:]), reads=[d_rstd], writes=[d_rstd])
        P.op("dve", lambda e: e.scalar_tensor_tensor(out=hb[:], in0=xt[b][:], scalar=rstd[:, 0:1], in1=gm[:], op0=ALU.mult, op1=ALU.mult),
             reads=[d_xt[b], d_rstd, d_gm], writes=[d_hb])
        for k in range(8):
            P.op("pe", lambda e, k=k: e.transpose(out=pT[:, k, :], in_=hb[:, k * 128:(k + 1) * 128], identity=idb[:]), reads=[d_hb, d_idb], writes=[d_pT])
        P.op("act", lambda e: e.copy(out=hT[b][:], in_=pT[:]), reads=[d_pT], writes=[d_hT[b]])
        groups = [0, 1, 2] if own else [1, 2]
        for g in groups:
            for k in range(8):
                P.op("pe", lambda e, g=g, k=k: e.matmul(pq[b][g][:], lhsT=hT[b][:, k, :], rhs=wq[:, k, g * 512:(g + 1) * 512], start=(k == 0), stop=(k == 7)),
                     reads=[d_hT[b], d_wq], writes=[d_pq[b][g]])

    def post(ti):
        b = ti % NB
        own = ti >= 32
        r0 = ti * 128
        ks = qk_post_steps(pq[b][1], d_pq[b][1], 1, b)
        qs = qk_post_steps(pq[b][0], d_pq[b][0], 0, b) if own else []
        for i in range(len(ks)):
            ks[i]()
            if qs:
                qs[i]()
        to_T_and_store(kT_d, r0, b, 1)
        if own:
            to_T_and_store(qT_d, r0 - TP, b, 0)
        P.op("act", lambda e: e.copy(out=vs[b][:], in_=pq[b][2][:]), reads=[d_pq[b][2]], writes=[d_vs[b]])
        P.dma("sp", lambda e: e.dma_start(out=v_d[r0:r0 + 128, :], in_=vs[b][:]), reads=[d_vs[b]])

    front(0)
    for ti in range(64):
        if ti + 1 < 64:
            front(ti + 1)
        post(ti)


def phase_b(nc, P, st, kT_d, v_d, qT_d, e_d, vq_d, nb_d, oq_d, tri_d, idb, d_idb, oa, d_oa, heads=range(8), nblk=16):
    def sb(name, shape, dt):
        return st.enter_context(nc.sbuf_tensor("b_" + name, shape, dt))

    def ps(name, shape, dt=F32):
        return st.enter_context(nc.psum_tensor(name, shape, dt))
    NB = 2
    kaug = [sb("kaug%d" % i, [96, NK], BF16) for i in range(NB)]; d_kaug = [Dep() for _ in range(NB)]
    vaug = [sb("vaug%d" % i, [128, 64, 65], BF16) for i in range(NB)]; d_vaug = [Dep() for _ in range(NB)]
    qaug = [sb("qaug%d" % i, [96, T], BF16) for i in range(NB)]; d_qaug = [Dep() for _ in range(NB)]
    d_qmb = [Dep() for _ in range(NB)]
    vq = sb("vq_s", [128, 1024], F32); d_c = Dep()
    nb = sb("nb_s", [128, 1024], F32)
    oq = sb("oq_s", [128, 1024], F32)
    tri = sb("tri_s", [128, 128], BF16)
    ksum = sb("ksum", [64, 32], F32); d_ksum = Dep()
    kmean = sb("kmean", [64, 32], BF16); d_kmean = Dep()
    g1 = sb("g1", [128, 512], F32); d_g1 = Dep()
    top8 = sb("top8", [128, 16, 8], F32); d_top8 = Dep()
    sel = sb("sel", [128, 512], F32); d_sel = Dep()
    mbp = sb("mbp", [128, 16, 96], BF16); d_mbp = Dep()
    NPT = 3
    pt = [sb("pt%d" % i, [128, 512], BF16) for i in range(NPT)]; d_pt = [Dep() for _ in range(NPT)]
    rc = sb("rc", [128, 2], F32); d_rc = [Dep(), Dep()]
    pg = ps("pg", [128, 512], F32); d_pg = Dep()
    pmT = ps("pmT", [96, 8, 128], BF16); d_pmT = Dep()
    psS = [ps("psS%d" % i, [128, 512], F32) for i in range(NPT)]; d_psS = [Dep() for _ in range(NPT)]
    po = [ps("po%d" % i, [128, 65], F32) for i in range(2)]; d_po = [Dep() for _ in range(2)]

    for i in range(NB):
        P.dma("sp", lambda e, i=i: e.dma_start(out=kaug[i][64:96, :], in_=e_d[:, :]), writes=[d_kaug[i]])
        P.op("pool", lambda e, i=i: e.memset(vaug[i][:], 1.0), writes=[d_vaug[i]])
    P.dma("sp", lambda e: e.dma_start(out=vq[:], in_=vq_d[0:1, :].partition_broadcast(128)), writes=[d_c])
    P.dma("sp", lambda e: e.dma_start(out=nb[:], in_=nb_d[0:1, :].partition_broadcast(128)), writes=[d_c])
    P.dma("sp", lambda e: e.dma_start(out=oq[:], in_=oq_d[0:1, :].partition_broadcast(128)), writes=[d_c])
    P.dma("pool", lambda e: e.dma_start(out=tri[:], in_=tri_d[:, :]), writes=[d_c])
    P.op("pool", lambda e: e.memset(mbp[:], 0.0), writes=[d_mbp])

    for ih, h in enumerate(heads):
        b = ih % NB
        P.dma("sp", lambda e, b=b, h=h: e.dma_start(out=kaug[b][0:64, :], in_=kT_d[h * 64:(h + 1) * 64, :]), writes=[d_kaug[b]])
        P.dma("sp", lambda e, b=b, h=h: e.dma_start(out=vaug[b][:, :, 0:64],
                                                    in_=v_d[:, h * 64:(h + 1) * 64].rearrange("(t p) d -> p t d", p=128)),
              writes=[d_vaug[b]])
        P.dma("sp", lambda e, b=b, h=h: e.dma_start(out=qaug[b][0:64, :], in_=qT_d[h * 64:(h + 1) * 64, :]), writes=[d_qaug[b]])
        P.op("dve", lambda e, b=b: e.tensor_reduce(out=ksum[:], in_=kaug[b][0:64, :].rearrange("p (b k) -> p b k", k=256),
                                                   axis=AX.X, op=ALU.add), reads=[d_kaug[b]], writes=[d_ksum])
        P.op("dve", lambda e: e.tensor_scalar(out=kmean[:], in0=ksum[:], scalar1=1.0 / 256, scalar2=None, op0=ALU.mult),
             reads=[d_ksum], writes=[d_kmean])
        for half in range(2):
            for j in range(16):
                qt = half * 16 + j
                P.op("pe", lambda e, b=b, j=j, qt=qt: e.matmul(pg[:, j * 32:(j + 1) * 32], lhsT=qaug[b][0:64, qt * 128:(qt + 1) * 128],
                                                              rhs=kmean[:, :], start=True, stop=True),
                     reads=[d_qaug[b], d_kmean], writes=[d_pg])
            cs_ = slice(half * 512, (half + 1) * 512)
            P.op("dve", lambda e, cs_=cs_: e.tensor_tensor(out=g1[:], in0=pg[:], in1=vq[:, cs_], op=ALU.mult),
                 reads=[d_pg, d_c], writes=[d_g1])
            P.op("dve", lambda e, cs_=cs_: e.tensor_tensor(out=g1[:], in0=g1[:], in1=nb[:, cs_], op=ALU.add),
                 reads=[d_g1, d_c], writes=[d_g1])
            for j in range(16):
                P.op("dve", lambda e, j=j: e.max(out=top8[:, j, :], in_=g1[:, j * 32:(j + 1) * 32]), reads=[d_g1], writes=[d_top8])
            P.op("dve", lambda e: e.tensor_tensor(out=sel[:].rearrange("p (j b) -> p j b", b=32),
                                                  in0=g1[:].rearrange("p (j b) -> p j b", b=32),
                                                  in1=top8[:, :, 2:3].to_broadcast([128, 16, 32]), op=ALU.is_ge),
                 reads=[d_g1, d_top8], writes=[d_sel])
            P.op("dve", lambda e, cs_=cs_: e.tensor_tensor(out=sel[:], in0=sel[:], in1=vq[:, cs_], op=ALU.mult),
                 reads=[d_sel, d_c], writes=[d_sel])
            P.op("dve", lambda e, cs_=cs_: e.tensor_tensor(out=sel[:], in0=sel[:], in1=oq[:, cs_], op=ALU.add),
                 reads=[d_sel, d_c], writes=[d_sel])
            P.op("dve", lambda e: e.tensor_scalar(out=mbp[:, :, 64:96], in0=sel[:].rearrange("p (j b) -> p j b", b=32),
                                                  scalar1=1.0, scalar2=MB, op0=ALU.subtract, op1=ALU.mult),
                 reads=[d_sel], writes=[d_mbp])
            for grp in range(2):
                for j in range(8):
                    P.op("pe", lambda e, grp=grp, j=j: e.transpose(out=pmT[:, j, :], in_=mbp[:, grp * 8 + j, :], identity=idb[:]),
                         reads=[d_mbp, d_idb], writes=[d_pmT])
                c0 = (half * 16 + grp * 8) * 128
                P.op("act", lambda e, b=b, c0=c0: e.copy(out=qaug[b][64:96, c0:c0 + 1024], in_=pmT[64:96, :, :]),
                     reads=[d_pmT], writes=[d_qmb[b]])
        units = []
        for jb in range(nblk):
            ncommon = 32 + 2 * jb
            for u in range(ncommon // 2):
                units.append((jb, [2 * u, 2 * u + 1], False, u == 0))
            units.append((jb, [32 + 2 * jb, 32 + 2 * jb + 1], True, False))

        def emit_S(u, s, b=b):
            jb, kts, diag, first = u
            for j, kt in enumerate(kts):
                if diag and j == 1:
                    q0, nq, c0 = jb * 256 + 128, 128, 256
                else:
                    q0, nq, c0 = jb * 256, 256, j * 256
                P.op("pe", lambda e, s=s, kt=kt, q0=q0, nq=nq, c0=c0: e.matmul(psS[s][:, c0:c0 + nq], lhsT=kaug[b][0:96, kt * 128:(kt + 1) * 128],
                                                                             rhs=qaug[b][0:96, q0:q0 + nq], start=True, stop=True),
                     reads=[d_kaug[b], d_qaug[b], d_qmb[b]], writes=[d_psS[s]])

        def emit_rest(u, s, b=b, h=h):
            jb, kts, diag, first = u
            ncols = 384 if diag else 512
            P.op("act", lambda e, s=s, ncols=ncols: e.activation(out=pt[s][:, 0:ncols], in_=psS[s][:, 0:ncols], func=AF.Exp),
                 reads=[d_psS[s]], writes=[d_pt[s]])
            if diag:
                P.op("pool", lambda e, s=s: e.tensor_tensor(out=pt[s][:, 0:128], in0=pt[s][:, 0:128], in1=tri[:], op=ALU.mult),
                     reads=[d_pt[s], d_c], writes=[d_pt[s]])
                P.op("pool", lambda e, s=s: e.tensor_tensor(out=pt[s][:, 256:384], in0=pt[s][:, 256:384], in1=tri[:], op=ALU.mult),
                     reads=[d_pt[s], d_c], writes=[d_pt[s]])
            for j, kt in enumerate(kts):
                if diag and j == 1:
                    pv = [(1, 256, True)]
                elif diag:
                    pv = [(0, 0, True), (1, 128, False)]
                else:
                    pv = [(0, j * 256, False), (1, j * 256 + 128, False)]
                st_ = first and j == 0
                for qi, col0, last in pv:
                    P.op("pe", lambda e, s=s, kt=kt, qi=qi, col0=col0, st_=st_, last=last: e.matmul(
                        po[qi][:, :], lhsT=pt[s][:, col0:col0 + 128], rhs=vaug[b][:, kt, :], start=st_, stop=last),
                        reads=[d_pt[s], d_vaug[b]], writes=[d_po[qi]])
            if diag:
                for qi in range(2):
                    qt = jb * 2 + qi
                    P.op("dve", lambda e, qi=qi: e.reciprocal(out=rc[:, qi:qi + 1], in_=po[qi][:, 64:65]), reads=[d_po[qi]], writes=[d_rc[qi]])
                    P.op("dve", lambda e, qi=qi, qt=qt: e.tensor_scalar(out=oa[:, qt, h * 64:(h + 1) * 64], in0=po[qi][:, 0:64],
                                                                       scalar1=rc[:, qi:qi + 1], scalar2=None, op0=ALU.mult),
                         reads=[d_po[qi], d_rc[qi]], writes=[d_oa])
        SK = 2
        n = len(units)
        for i in range(n + SK):
            if i < n:
                emit_S(units[i], i % NPT)
            if i >= SK:
                emit_rest(units[i - SK], (i - SK) % NPT)


def host_consts(half):
    pv = 1.0 if half == 1 else 0.0
    V = np.zeros((32, 32), np.float32); O = np.zeros((32, 32), np.float32)
    for qt in range(32):
        jb = qt // 2
        V[qt, :16] = pv
        V[qt, 16:16 + jb] = 1.0
        O[qt, 16 + jb] = 1.0
    NBm = (V - 1.0) * BIGG
    E = np.zeros((32, NK), np.float32)
    for b in range(32):
        E[b, b * 256:(b + 1) * 256] = 1.0
    tri = (np.arange(128)[:, None] <= np.arange(128)[None, :]).astype(np.float32)
    return dict(vq=V.reshape(1, 1024), nb=NBm.reshape(1, 1024), oq=O.reshape(1, 1024),
                e_d=E.astype(ml_dtypes.bfloat16), tri=tri.astype(ml_dtypes.bfloat16))


def phase_c(nc, P, st, xin, w_in, g_mix, cwl_d, cbl_d, dtb_d, alog_d, dsk_d, ssdn_d, tri2_d, sel_d, tb_d, pflag_d,
            idb, d_idb, os_d, d_osd, chunks=range(32)):
    def sb(name, shape, dt):
        return st.enter_context(nc.sbuf_tensor("c_" + name, shape, dt))

    def ps(name, shape, dt=F32):
        return st.enter_context(nc.psum_tensor(name, shape, dt))
    wz = sb("wz", [128, 8, 1024], BF16); d_w = Dep()
    wx = sb("wx", [128, 8, 2048], BF16)
    wdt = sb("wdt", [128, 8, 16], BF16)
    gm = sb("gmc", [128, D], F32); d_c = Dep()
    cw = sb("cw", [128, 16, 4], F32)
    cb = sb("cb", [128, 16], F32)
    dtb = sb("dtb", [128, 2, 16], F32)
    Aneg = sb("Aneg", [128, 2, 16], F32); d_A = Dep()
    dsk = sb("dsk", [128, 16], F32)
    ssdn = sb("ssdn", [128, D], F32)
    tri2 = sb("tri2", [128, 2, 256], F32)
    onesf = sb("onesf", [128, 128], F32)
    sel = sb("selc", [16, 16, 128], F32)
    tb = sb("tbc", [128, 384], F32)
    pflag = sb("pflag", [128, 1], F32)
    xt = sb("xtc", [128, 2, D], F32); d_xt = Dep()
    junk = sb("junkc", [128, D], BF16); d_junk = Dep()
    ssq = sb("ssqc", [128, 2], F32); d_ssq = Dep()
    rstd = sb("rstdc", [128, 2], F32); d_rstd = Dep()
    hb = sb("hbc", [128, 2, D], BF16); d_hb = Dep()
    hT = sb("hTc", [128, 8, 256], BF16); d_hT = Dep()
    xraw = sb("xraw", [128, 16, 259], F32); d_xraw = Dep(); d_halo = Dep()
    acc = sb("acc", [128, 8, 256], F32); d_acc = [Dep() for _ in range(8)]
    xc = sb("xc", [128, 16, 256], BF16); d_xc = [Dep() for _ in range(16)]
    xtok = sb("xtok", [128, 2, 1024], BF16); d_xtok = Dep()
    btok = sb("btok", [128, 2, 512], BF16); d_btok = Dep()
    dtr = sb("dtr", [128, 2, 16], F32); d_dtr = Dep()
    dt = sb("dt", [128, 2, 16], F32); d_dt = Dep()
    aa = sb("aa", [128, 2, 16], F32); d_aa = Dep()
    acum = sb("acum", [128, 2, 16], F32); d_acum = Dep()
    nacum = sb("nacum", [128, 2, 16], F32); d_nacum = Dep()
    acumT = sb("acumT", [16, 256], F32); d_acumT = Dep()
    tot = sb("tot", [128, 16], F32); d_tot = Dep()
    cdec = sb("cdec", [128, 16], F32); d_cdec = Dep()
    dte = sb("dte", [128, 2, 16], F32); d_dte = Dep()
    eac = sb("eac", [128, 2, 16], F32); d_eac = Dep()
    w2 = sb("w2", [128, 2, 16], F32); d_w2 = Dep()
    xdt = sb("xdt", [128, 2, 1024], BF16); d_xdt = Dep()
    xdd = sb("xdd", [128, 2, 1024], BF16); d_xdd = Dep()
    zs = sb("zs", [128, 2, 1024], BF16); d_zs = Dep()
    cbT = sb("cbT", [128, 4, 384], F32); d_cbT = [Dep() for _ in range(4)]
    arg = [sb("arg%d" % i, [128, 384], F32) for i in range(2)]; d_arg = [Dep(), Dep()]
    Lt = [sb("Lt%d" % i, [128, 384], F32) for i in range(2)]; d_Lt = [Dep(), Dep()]
    Mt = [sb("Mt%d" % i, [128, 384], BF16) for i in range(2)]; d_Mt = [Dep() for _ in range(2)]
    stf = sb("stf", [128, 4, 256], F32); d_stf = Dep()
    stT = sb("stT", [128, 4, 256], BF16); d_stT = Dep()
    yt = [sb("yt%d" % i, [128, 256], F32) for i in range(2)]; d_yt = [Dep(), Dep()]
    y2 = [sb("y2%d" % i, [128, 256], F32) for i in range(2)]; d_y2 = [Dep(), Dep()]
    gss = [sb("gss%d" % i, [128, 1], F32) for i in range(2)]; d_gss = [Dep(), Dep()]
    grs = [sb("grs%d" % i, [128, 1], F32) for i in range(2)]; d_grs = [Dep(), Dep()]
    osb = sb("osb", [128, 2, 1024], BF16); d_osb = Dep()

    B = [ps("bk%d" % i, [128, 512], F32) for i in range(7)]
    d_B = [Dep() for _ in range(7)]
    d_B1h = [Dep(), Dep()]
    d_B5h = [Dep(), Dep()]
    pT = ps("pTc", [128, 8, 128], BF16); d_pT = Dep()

    for k in range(8):
        rows = slice(k * 128, (k + 1) * 128)
        P.dma("pool", lambda e, k=k, rows=rows: e.dma_start(out=wz[:, k, :], in_=w_in[rows, C_Z:C_Z + 1024]), writes=[d_w])
        P.dma("pool", lambda e, k=k, rows=rows: e.dma_start(out=wx[:, k, :], in_=w_in[rows, C_X:C_X + 2048]), writes=[d_w])
        P.dma("pool", lambda e, k=k, rows=rows: e.dma_start(out=wdt[:, k, :], in_=w_in[rows, C_DT:C_DT + 16]), writes=[d_w])
    P.dma("sp", lambda e: e.dma_start(out=gm[:], in_=g_mix[0:1, :].partition_broadcast(128)), writes=[d_c])
    P.dma("sp", lambda e: e.dma_start(out=cw[:], in_=cwl_d[:, :, :]), writes=[d_c])
    P.dma("sp", lambda e: e.dma_start(out=cb[:], in_=cbl_d[:, :]), writes=[d_c])
    for i in range(2):
        P.dma("sp", lambda e, i=i: e.dma_start(out=dtb[:, i, :], in_=dtb_d[0:1, :].partition_broadcast(128)), writes=[d_c])
        P.dma("sp", lambda e, i=i: e.dma_start(out=Aneg[:, i, :], in_=alog_d[0:1, :].partition_broadcast(128)), writes=[d_A])
    P.dma("sp", lambda e: e.dma_start(out=dsk[:], in_=dsk_d[0:1, :].partition_broadcast(128)), writes=[d_c])
    P.dma("sp", lambda e: e.dma_start(out=ssdn[:], in_=ssdn_d[0:1, :].partition_broadcast(128)), writes=[d_c])
    P.dma("sp", lambda e: e.dma_start(out=tri2[:], in_=tri2_d[:, :, :]), writes=[d_c])
    P.dma("sp", lambda e: e.dma_start(out=sel[:], in_=sel_d[:, :, :]), writes=[d_c])
    P.dma("sp", lambda e: e.dma_start(out=tb[:], in_=tb_d[:, :]), writes=[d_c])
    P.dma("sp", lambda e: e.dma_start(out=pflag[:], in_=pflag_d[0:1, :].partition_broadcast(128)), writes=[d_c])
    P.op("pool", lambda e: e.memset(onesf[:], 1.0), writes=[d_c])
    P.op("act", lambda e: e.activation(out=Aneg[:], in_=Aneg[:], func=AF.Exp), reads=[d_A], writes=[d_A])
    P.op("dve", lambda e: e.tensor_scalar(out=Aneg[:], in0=Aneg[:], scalar1=-1.0, scalar2=None, op0=ALU.mult), reads=[d_A], writes=[d_A])
    P.op("pool", lambda e: e.memset(xraw[:], 0.0), writes=[d_xraw, d_halo])
    P.op("pool", lambda e: e.memset(stf[:], 0.0), writes=[d_stf])
    P.op("pool", lambda e: e.memset(stT[:], 0.0), writes=[d_stT])

    h3 = lambda ap: ap.rearrange("p (h d) -> p h d", d=64)

    for c in chunks:
        own = c >= 16
        r0 = c * 256
        P.dma("sp", lambda e, r0=r0: e.dma_start(out=xt[:], in_=xin[r0:r0 + 256, :].rearrange("(t p) d -> p t d", p=128)), writes=[d_xt])
        for it in range(2):
            P.op("act", lambda e, it=it: e.activation(out=junk[:], in_=xt[:, it, :], func=AF.Square, accum_out=ssq[:, it:it + 1]),
                 reads=[d_xt], writes=[d_junk, d_ssq])
        P.op("act", lambda e: e.activation(out=rstd[:], in_=ssq[:], func=AF.Sqrt, bias=EPS, scale=1.0 / D), reads=[d_ssq], writes=[d_rstd])
        P.op("dve", lambda e: e.reciprocal(out=rstd[:], in_=rstd[:]), reads=[d_rstd], writes=[d_rstd])
        for it in range(2):
            P.op("dve", lambda e, it=it: e.scalar_tensor_tensor(out=hb[:, it, :], in0=xt[:, it, :], scalar=rstd[:, it:it + 1], in1=gm[:],
                                                                                    op0=ALU.mult, op1=ALU.mult),
                 reads=[d_xt, d_rstd, d_c], writes=[d_hb])
        for it in range(2):
            for k in range(8):
                P.op("pe", lambda e, it=it, k=k: e.transpose(out=pT[:, k, :], in_=hb[:, it, k * 128:(k + 1) * 128], identity=idb[:]),
                     reads=[d_hb, d_idb], writes=[d_pT])
            P.op("act", lambda e, it=it: e.copy(out=hT[:, :, it * 128:(it + 1) * 128], in_=pT[:]), reads=[d_pT], writes=[d_hT])
        for it in range(2):
            for k in range(8):
                P.op("pe", lambda e, it=it, k=k: e.matmul(B[3][:, it * 16:(it + 1) * 16], lhsT=hT[:, k, it * 128:(it + 1) * 128], rhs=wdt[:, k, :],
                                                         start=(k == 0), stop=(k == 7)), reads=[d_hT, d_w], writes=[d_B[3]])
        P.op("dve", lambda e: e.tensor_tensor(out=dtr[:].rearrange("p a b -> p (a b)"), in0=B[3][:, 0:32], in1=dtb[:].rearrange("p a b -> p (a b)"), op=ALU.add),
             reads=[d_B[3], d_c], writes=[d_dtr])
        P.op("act", lambda e: e.activation(out=dtr[:], in_=dtr[:], func=AF.Exp), reads=[d_dtr], writes=[d_dtr])
        P.op("act", lambda e: e.activation(out=dt[:], in_=dtr[:], func=AF.Ln, bias=1.0), reads=[d_dtr], writes=[d_dt])
        P.op("dve", lambda e: e.tensor_tensor(out=aa[:], in0=dt[:], in1=Aneg[:], op=ALU.mult), reads=[d_dt, d_A], writes=[d_aa])
        for it in range(2):
            for jt in range(it + 1):
                P.op("pe", lambda e, it=it, jt=jt: e.matmul(B[3][:, 32 + it * 16:32 + (it + 1) * 16], lhsT=tri2[:, jt, it * 128:(it + 1) * 128],
                                                           rhs=aa[:, jt, :], start=(jt == 0), stop=(jt == it)),
                     reads=[d_aa, d_c], writes=[d_B[3]])
        for jt in range(2):
            P.op("pe", lambda e, jt=jt: e.matmul(B[3][:, 64:80], lhsT=onesf[:], rhs=aa[:, jt, :], start=(jt == 0), stop=(jt == 1)),
                 reads=[d_aa, d_c], writes=[d_B[3]])
        for jt in range(2):
            P.op("pe", lambda e, jt=jt: e.matmul(B[3][0:16, 128:384], lhsT=aa[:, jt, :], rhs=tri2[:, jt, :], start=(jt == 0), stop=(jt == 1)),
                 reads=[d_aa, d_c], writes=[d_B[3]])
        P.op("act", lambda e: e.copy(out=acum[:].rearrange("p a b -> p (a b)"), in_=B[3][:, 32:64]), reads=[d_B[3]], writes=[d_acum])
        P.op("dve", lambda e: e.tensor_scalar(out=nacum[:].rearrange("p a b -> p (a b)"), in0=B[3][:, 32:64], scalar1=-1.0, scalar2=None, op0=ALU.mult),
             reads=[d_B[3]], writes=[d_nacum])
        P.op("act", lambda e: e.copy(out=tot[:], in_=B[3][:, 64:80]), reads=[d_B[3]], writes=[d_tot])
        P.op("act", lambda e: e.copy(out=acumT[:], in_=B[3][0:16, 128:384]), reads=[d_B[3]], writes=[d_acumT])
        P.op("act", lambda e: e.activation(out=cdec[:], in_=tot[:], func=AF.Exp), reads=[d_tot], writes=[d_cdec])
        P.op("dve", lambda e: e.tensor_tensor(out=dte[:], in0=nacum[:], in1=tot[:].unsqueeze(1).to_broadcast([128, 2, 16]), op=ALU.add),
             reads=[d_nacum, d_tot], writes=[d_dte])
        P.op("act", lambda e: e.activation(out=dte[:], in_=dte[:], func=AF.Exp), reads=[d_dte], writes=[d_dte])
        P.op("act", lambda e: e.activation(out=eac[:], in_=acum[:], func=AF.Exp), reads=[d_acum], writes=[d_eac])
        P.op("dve", lambda e: e.tensor_tensor(out=w2[:], in0=dt[:], in1=dte[:], op=ALU.mult), reads=[d_dt, d_dte], writes=[d_w2])
        nfc = 16 if (own or c == 15) else 12
        for fc in range(nfc):
            bi = 1 if fc % 2 == 0 else 4
            for k in range(8):
                P.op("pe", lambda e, fc=fc, k=k, bi=bi: e.matmul(B[bi][:, 0:256], lhsT=wx[:, k, fc * 128:(fc + 1) * 128], rhs=hT[:, k, :],
                                                               start=(k == 0), stop=(k == 7)), reads=[d_hT, d_w], writes=[d_B[bi]])
            P.op("act", lambda e, fc=fc, bi=bi: e.copy(out=xraw[:, fc, 3:259], in_=B[bi][:, 0:256]),
                 reads=[d_B[bi]], writes=[d_xraw])
        if own:
            for it in range(2):
                for hv in range(2):
                    zb = 2 if hv == 0 else 6
                    for k in range(8):
                        P.op("pe", lambda e, it=it, hv=hv, k=k, zb=zb: e.matmul(B[zb][:, :], lhsT=hT[:, k, it * 128:(it + 1) * 128], rhs=wz[:, k, hv * 512:(hv + 1) * 512],
                                                                              start=(k == 0), stop=(k == 7)), reads=[d_hT, d_w], writes=[d_B[zb]])
                    P.op("act", lambda e, it=it, hv=hv, zb=zb: e.activation(out=zs[:, it, hv * 512:(hv + 1) * 512], in_=B[zb][:, :], func=AF.Silu),
                         reads=[d_B[zb]], writes=[d_zs])
        for half in range(2):
            n8 = 8 if (half == 0 or nfc == 16) else 4
            for f8 in range(n8):
                fc = half * 8 + f8
                P.op("dve", lambda e, fc=fc, f8=f8: e.tensor_scalar(out=acc[:, f8, :], in0=xraw[:, fc, 0:256], scalar1=cw[:, fc, 0:1], scalar2=cb[:, fc:fc + 1],
                                                                   op0=ALU.mult, op1=ALU.add), reads=[d_xraw, d_halo, d_c], writes=[d_acc[f8]])
            for kk in range(1, 4):
                for f8 in range(n8):
                    fc = half * 8 + f8
                    P.op("dve", lambda e, fc=fc, f8=f8, kk=kk: e.scalar_tensor_tensor(out=acc[:, f8, :], in0=xraw[:, fc, kk:kk + 256], scalar=cw[:, fc, kk:kk + 1],
                                                                                     in1=acc[:, f8, :], op0=ALU.mult, op1=ALU.add),
                         reads=[d_xraw, d_halo, d_c, d_acc[f8]], writes=[d_acc[f8]])
            for f8 in range(n8):
                fc = half * 8 + f8
                P.op("act", lambda e, fc=fc, f8=f8: e.activation(out=xc[:, fc, :], in_=acc[:, f8, :], func=AF.Silu),
                     reads=[d_acc[f8]], writes=[d_xc[fc]])
        P.op("pool", lambda e: e.tensor_copy(out=xraw[:, :, 0:3], in_=xraw[:, :, 256:259]), reads=[d_xraw], writes=[d_halo])
        for it in range(2):
            for fc in range(8):
                P.op("pe", lambda e, it=it, fc=fc: e.transpose(out=pT[:, fc, :], in_=xc[:, fc, it * 128:(it + 1) * 128], identity=idb[:]),
                     reads=[d_xc[fc], d_idb], writes=[d_pT])
            P.op("act", lambda e, it=it: e.copy(out=xtok[:, it, :], in_=pT[:]), reads=[d_pT], writes=[d_xtok])
            for g in range(4):
                P.op("pe", lambda e, it=it, g=g: e.transpose(out=pT[:, g, :], in_=xc[:, 8 + g, it * 128:(it + 1) * 128], identity=idb[:]),
                     reads=[d_xc[8 + g], d_idb], writes=[d_pT])
            P.op("act", lambda e, it=it: e.copy(out=btok[:, it, :], in_=pT[:, 0:4, :]), reads=[d_pT], writes=[d_btok])
        for it in range(2):
            if own:
                P.op("dve", lambda e, it=it: e.tensor_tensor(out=h3(xdt[:, it, :]), in0=h3(xtok[:, it, :]),
                                                             in1=dt[:, it, :].unsqueeze(2).to_broadcast([128, 16, 64]), op=ALU.mult),
                     reads=[d_xtok, d_dt], writes=[d_xdt])
            P.op("pool", lambda e, it=it: e.tensor_tensor(out=h3(xdd[:, it, :]), in0=h3(xtok[:, it, :]),
                                                          in1=w2[:, it, :].unsqueeze(2).to_broadcast([128, 16, 64]), op=ALU.mult),
                 reads=[d_xtok, d_w2], writes=[d_xdd])
        if own:
            for g in range(4):
                P.op("pe", lambda e, g=g: e.matmul(B[4][:, 0:256], lhsT=xc[:, 8 + g, 0:128], rhs=xc[:, 12 + g, :], start=True, stop=True),
                     reads=[d_xc[8 + g], d_xc[12 + g]], writes=[d_B[4]])
                P.op("pe", lambda e, g=g: e.matmul(B[4][:, 256:384], lhsT=xc[:, 8 + g, 128:256], rhs=xc[:, 12 + g, 128:256], start=True, stop=True),
                     reads=[d_xc[8 + g], d_xc[12 + g]], writes=[d_B[4]])
                P.op("act", lambda e, g=g: e.copy(out=cbT[:, g, :], in_=B[4][:, 0:384]), reads=[d_B[4]], writes=[d_cbT[g]])
            for g in range(4):
                def stA(h, g=g):
                    mi = h % 2
                    bk = 5 if mi == 0 else 4
                    P.op("pe", lambda e: e.matmul(B[bk][:, 0:256], lhsT=sel[:, h, :], rhs=acumT[:, :], start=True, stop=True),
                         reads=[d_c, d_acumT], writes=[d_B[bk]])
                    P.op("dve", lambda e: e.scalar_tensor_tensor(out=arg[mi][:, 0:256], in0=B[bk][:, 0:256], scalar=nacum[:, 0, h:h + 1], in1=tb[:, 0:256],
                                                                 op0=ALU.add, op1=ALU.add), reads=[d_B[bk], d_nacum, d_c], writes=[d_arg[mi]])
                    P.op("dve", lambda e: e.scalar_tensor_tensor(out=arg[mi][:, 256:384], in0=B[bk][:, 128:256], scalar=nacum[:, 1, h:h + 1], in1=tb[:, 256:384],
                                                                 op0=ALU.add, op1=ALU.add), reads=[d_B[bk], d_nacum, d_c], writes=[d_arg[mi]])
                    P.op("act", lambda e: e.activation(out=Lt[mi][:], in_=arg[mi][:], func=AF.Exp), reads=[d_arg[mi]], writes=[d_Lt[mi]])
                    P.op("pool", lambda e: e.tensor_tensor(out=Mt[mi][:], in0=Lt[mi][:], in1=cbT[:, g, :], op=ALU.mult),
                         reads=[d_Lt[mi], d_cbT[g]], writes=[d_Mt[mi]])

                def stB(h, g=g):
                    mi = h % 2
                    r = h % 4
                    cs_ = slice(r * 64, (r + 1) * 64)
                    hs_ = slice(h * 64, (h + 1) * 64)
                    P.op("pe", lambda e: e.matmul(B[6][:, 0:256][:, cs_], lhsT=Mt[mi][:, 0:128], rhs=xdt[:, 0, hs_], start=True, stop=True),
                         reads=[d_Mt[mi], d_xdt], writes=[d_B[6]])
                    P.op("pe", lambda e: e.matmul(B[6][:, 256:512][:, cs_], lhsT=Mt[mi][:, 128:256], rhs=xdt[:, 0, hs_], start=True, stop=False),
                         reads=[d_Mt[mi], d_xdt], writes=[d_B[6]])
                    P.op("pe", lambda e: e.matmul(B[6][:, 256:512][:, cs_], lhsT=Mt[mi][:, 256:384], rhs=xdt[:, 1, hs_], start=False, stop=True),
                         reads=[d_Mt[mi], d_xdt], writes=[d_B[6]])
                hs4 = [g * 4 + r for r in range(4)]
                stA(hs4[0]); stA(hs4[1]); stB(hs4[0]); stA(hs4[2]); stB(hs4[1]); stA(hs4[3]); stB(hs4[2]); stB(hs4[3])
                gs_ = slice(g * 256, (g + 1) * 256)
                for it in range(2):
                    P.op("pe", lambda e, g=g, it=it: e.matmul(B[0][:, it * 256:(it + 1) * 256], lhsT=xc[:, 12 + g, it * 128:(it + 1) * 128], rhs=stT[:, g, :],
                                                             start=True, stop=True), reads=[d_xc[12 + g], d_stT], writes=[d_B[0]])
                v4 = lambda ap: ap.rearrange("p (r d) -> p r d", d=64)

                def ystep(step, it, g=g, gs_=gs_):
                    if step == 0:
                        P.op("dve", lambda e: e.tensor_tensor(out=v4(yt[it][:]), in0=v4(B[0][:, it * 256:(it + 1) * 256]),
                                                              in1=eac[:, it, g * 4:(g + 1) * 4].unsqueeze(2).to_broadcast([128, 4, 64]), op=ALU.mult),
                             reads=[d_B[0], d_eac], writes=[d_yt[it]])
                    elif step == 1:
                        P.op("dve", lambda e: e.tensor_tensor(out=yt[it][:], in0=yt[it][:], in1=B[6][:, it * 256:(it + 1) * 256], op=ALU.add),
                             reads=[d_yt[it], d_B[6]], writes=[d_yt[it]])
                    elif step == 2:
                        P.op("pool", lambda e: e.tensor_tensor(out=v4(y2[it][:]), in0=v4(xtok[:, it, gs_]),
                                                               in1=dsk[:, g * 4:(g + 1) * 4].unsqueeze(2).to_broadcast([128, 4, 64]), op=ALU.mult),
                             reads=[d_xtok, d_c], writes=[d_y2[it]])
                    elif step == 3:
                        P.op("dve", lambda e: e.tensor_tensor(out=yt[it][:], in0=yt[it][:], in1=y2[it][:], op=ALU.add), reads=[d_yt[it], d_y2[it]], writes=[d_yt[it]])
                    elif step == 4:
                        P.op("dve", lambda e: e.tensor_tensor(out=yt[it][:], in0=yt[it][:], in1=zs[:, it, gs_], op=ALU.mult), reads=[d_yt[it], d_zs], writes=[d_yt[it]])
                    elif step == 5:
                        P.op("act", lambda e: e.activation(out=y2[it][:], in_=yt[it][:], func=AF.Square, accum_out=gss[it][:]), reads=[d_yt[it], d_y2[it]], writes=[d_y2[it], d_gss[it]])
                    elif step == 6:
                        P.op("act", lambda e: e.activation(out=grs[it][:], in_=gss[it][:], func=AF.Sqrt, bias=EPS, scale=1.0 / 256), reads=[d_gss[it]], writes=[d_grs[it]])
                    elif step == 7:
                        P.op("dve", lambda e: e.reciprocal(out=grs[it][:], in_=grs[it][:]), reads=[d_grs[it]], writes=[d_grs[it]])
                    elif step == 8:
                        P.op("dve", lambda e: e.scalar_tensor_tensor(out=osb[:, it, gs_], in0=yt[it][:], scalar=grs[it][:, 0:1], in1=ssdn[:, gs_],
                                                                     op0=ALU.mult, op1=ALU.mult), reads=[d_yt[it], d_grs[it], d_c], writes=[d_osb])
                for step in range(9):
                    for it in range(2):
                        ystep(step, it)
            P.dma("sp", lambda e, r0=r0: e.dma_start(out=os_d[r0 - TP:r0 - TP + 256, :].rearrange("(t p) c -> p t c", p=128), in_=osb[:]),
                  reads=[d_osb], writes=[d_osd])
        for g in range(4):
            bank = B[2] if g < 2 else B[1]
            dbank = d_B[2] if g < 2 else d_B[1]
            cs_ = slice((g % 2) * 256, (g % 2 + 1) * 256)
            for jt in range(2):
                P.op("pe", lambda e, g=g, jt=jt, bank=bank, cs_=cs_: e.matmul(bank[:, cs_], lhsT=btok[:, jt, g * 128:(g + 1) * 128], rhs=xdd[:, jt, g * 256:(g + 1) * 256],
                                                                            start=(jt == 0), stop=(jt == 1)), reads=[d_btok, d_xdd], writes=[dbank])
        v16 = lambda ap: ap.rearrange("p (h d) -> p h d", d=64)
        P.op("dve", lambda e: e.tensor_tensor(out=v16(stf[:].rearrange("p g c -> p (g c)")), in0=v16(stf[:].rearrange("p g c -> p (g c)")),
                                              in1=cdec[:].unsqueeze(2).to_broadcast([128, 16, 64]), op=ALU.mult), reads=[d_stf, d_cdec], writes=[d_stf])
        P.op("dve", lambda e: e.tensor_tensor(out=stf[:, 0:2, :].rearrange("p g c -> p (g c)"), in0=stf[:, 0:2, :].rearrange("p g c -> p (g c)"), in1=B[2][:, :], op=ALU.add),
             reads=[d_stf, d_B[2]], writes=[d_stf])
        P.op("dve", lambda e: e.tensor_tensor(out=stf[:, 2:4, :].rearrange("p g c -> p (g c)"), in0=stf[:, 2:4, :].rearrange("p g c -> p (g c)"), in1=B[1][:, :], op=ALU.add),
             reads=[d_stf, d_B[1]], writes=[d_stf])
        if c == 15:
            P.op("dve", lambda e: e.tensor_scalar(out=stf[:], in0=stf[:], scalar1=pflag[:, 0:1], scalar2=None, op0=ALU.mult), reads=[d_stf, d_c], writes=[d_stf])
        P.op("act", lambda e: e.copy(out=stT[:], in_=stf[:]), reads=[d_stf], writes=[d_stT])


def host_consts_c(z, half):
    cwl = np.ascontiguousarray(z['conv_w'].reshape(4, 16, 128).transpose(2, 1, 0)).astype(np.float32)
    cbl = np.ascontiguousarray(z['conv_b'].reshape(16, 128).T).astype(np.float32)
    tri = (np.arange(128)[:, None] <= np.arange(128)[None, :]).astype(np.float32)
    tri2 = np.zeros((128, 2, 256), np.float32)
    tri2[:, 0, 0:128] = tri; tri2[:, 0, 128:256] = 1.0; tri2[:, 1, 128:256] = tri
    sel = np.zeros((16, 16, 128), np.float32)
    for h in range(16):
        sel[h, h, :] = 1.0
    tbias = np.where(tri > 0, 0.0, NEG).astype(np.float32)
    tb = np.zeros((128, 384), np.float32)
    tb[:, 0:128] = tbias; tb[:, 256:384] = tbias
    return dict(cwl=cwl, cbl=cbl, dtb=z['dt_bias'][None, :], alog=z['a_log'][None, :], dsk=z['d_skip'][None, :], ssdn=z['ssd_norm'][None, :],
                tri2=tri2, sel_d=sel, tb_d=tb, pflag=np.array([[1.0 if half == 1 else 0.0]], np.float32))


def phase_def(nc, P, xown, w_in, g_mix, mem_d, gmem_d, wmemkv, mqn_d, mkn_d, woa_d, wos_d, wom_d, wout_d, gffn_d, wr_d,
              wgate_d, wup_d, wdown_d, tri_d, ecap_d, oa_d, d_oad, os_d, d_osd, x1_d, xbuf, ybuf, out_d, idb, d_idb, idf, d_idf,
              ntiles=32, experts=range(32), stage=3):
    d_x1d = Dep(); d_xbuf = Dep(); d_ybuf = Dep(); d_out = Dep()
    with contextlib.ExitStack() as stp:
        def sbp(name, shape, dt):
            return stp.enter_context(nc.sbuf_tensor("d_" + name, shape, dt))
        combs = sbp("combs", [128, 32, 2], F32); d_combs = Dep()
        idxs = sbp("idxs", [128, 32, 2], I32); d_idxs = Dep()

        with contextlib.ExitStack() as st:
            def sb(name, shape, dt):
                return st.enter_context(nc.sbuf_tensor("d_" + name, shape, dt))

            def ps(name, shape, dt=F32):
                return st.enter_context(nc.psum_tensor(name, shape, dt))
            d_w = Dep(); d_c = Dep()
            gm = sb("gmd", [128, D], F32)
            gf = sb("gfd", [128, D], F32)
            mqn = sb("mqn", [128, 512], F32)
            mkn = sb("mkn", [128, 512], F32)
            tri = sb("trid", [128, 128], F32)
            onesf = sb("onesfd", [128, 128], F32)
            onesb = sb("onesbd", [128, 128], BF16)
            ecap = sb("ecap", [128, 32], F32)
            base = sb("base", [128, 32], F32); d_base = Dep()
            kmT = sb("kmT", [128, 4, 256], BF16); d_kmT = Dep()
            vm = sb("vm", [128, 2, 512], BF16); d_vm = Dep()

            xt = [sb("xtd%d" % i, [128, D], F32) for i in range(2)]; d_xt = [Dep(), Dep()]
            junk = sb("junkd", [128, D], BF16); d_junk = Dep()
            ssq = sb("ssqd", [128, 1], F32); d_ssq = Dep()
            rstd = sb("rstdd", [128, 1], F32); d_rstd = Dep()
            hb = sb("hbd", [128, D], BF16); d_hb = Dep()
            hT = sb("hTd", [128, 8, 128], BF16); d_hT = Dep()
            sq = sb("sqd", [128, 512], F32); d_sq = Dep()
            hs = sb("hsd", [128, 4], F32); d_hs = Dep()
            hr = sb("hrd", [128, 4], F32); d_hr = Dep()
            qmn = sb("qmn", [128, 512], F32); d_qmn = Dep()
            qmb = sb("qmb", [128, 512], BF16); d_qmb = Dep()
            qmT = sb("qmT", [128, 4, 128], BF16); d_qmT = Dep()
            pm = sb("pm", [128, 4, 2, 128], BF16); d_pm = Dep()
            rcp = sb("rcp", [128, 512], F32); d_rcp = Dep()
            omT = sb("omT", [128, 4, 128], BF16); d_omT = Dep()
            gs = sb("gs", [128, 3072], F32); d_gs = Dep()
            oat = [sb("oat%d" % i, [128, 512], BF16) for i in range(2)]; d_oat = [Dep(), Dep()]
            ost = [sb("ost%d" % i, [128, 1024], BF16) for i in range(2)]; d_ost = [Dep(), Dep()]
            oaT = sb("oaT", [128, 4, 128], BF16); d_oaT = Dep()
            osT = sb("osT", [128, 8, 128], BF16); d_osT = Dep()
            mg = sb("mg", [128, 512], F32); d_mg = Dep()
            tt = sb("tt", [128, 512], F32); d_tt = Dep()
            mgb = sb("mgb", [128, D], BF16); d_mgb = Dep()
            mgT = sb("mgT", [128, 8, 128], BF16); d_mgT = Dep()
            x1 = sb("x1", [128, D], F32); d_x1 = Dep()
            h2f = sb("h2f", [128, D], F32); d_h2f = Dep()
            h2b = [sb("h2b%d" % i, [128, D], BF16) for i in range(2)]; d_h2b = [Dep() for _ in range(2)]
            h2T = sb("h2T", [128, 8, 128], F32); d_h2T = Dep()
            lg = sb("lg", [128, 36], F32); d_lg = Dep()
            sm = sb("sm", [128, 64], F32); d_sm = Dep()
            goh = sb("goh", [128, 4], F32); d_goh = Dep()
            lsel = sb("lsel", [128, 32], F32); d_lsel = Dep()
            les = sb("les", [128, 8], F32); d_les = Dep()
            t8 = sb("t8", [128, 8], F32); d_t8 = Dep()
            oh = sb("oh", [128, 64], F32); d_oh = Dep()
            ohs = sb("ohs", [128, 32], F32); d_ohs = Dep()
            posf = sb("posf", [128, 32], F32); d_posf = Dep()
            idf2 = sb("idf2", [128, 2], F32); d_idf2 = Dep()

            pT = ps("pTd", [128, 8, 128], BF16); d_pT = Dep()
            pF = ps("pFd", [128, 512], F32); d_pF = Dep()
            pG = [ps("pGd%d" % i, [128, 512], F32) for i in range(3)]; d_pG = [Dep() for _ in range(3)]
            pms = [ps("pmsd%d" % i, [128, 512], F32) for i in range(2)]; d_pms = [Dep() for _ in range(2)]
            pmo = ps("pmod", [128, 512], F32); d_pmo = Dep()

            P.dma("sp", lambda e: e.dma_start(out=gm[:], in_=g_mix[0:1, :].partition_broadcast(128)), writes=[d_c])
            P.dma("sp", lambda e: e.dma_start(out=gf[:], in_=gffn_d[0:1, :].partition_broadcast(128)), writes=[d_c])
            P.dma("sp", lambda e: e.dma_start(out=mqn[:], in_=mqn_d[0:1, :].partition_broadcast(128)), writes=[d_c])
            P.dma("sp", lambda e: e.dma_start(out=mkn[:], in_=mkn_d[0:1, :].partition_broadcast(128)), writes=[d_c])
            P.dma("sp", lambda e: e.dma_start(out=tri[:], in_=tri_d[:, :]), writes=[d_c])
            P.dma("sp", lambda e: e.dma_start(out=ecap[:], in_=ecap_d[0:1, :].partition_broadcast(128)), writes=[d_c])
            P.op("pool", lambda e: e.memset(onesf[:], 1.0), writes=[d_c])
            P.op("pool", lambda e: e.memset(onesb[:], 1.0), writes=[d_c])
            P.op("pool", lambda e: e.memset(base[:], 0.0), writes=[d_base])

            def head_norm(src_ps, d_src, gain, nh, dst, d_dst):
                hd = 512 // nh
                v = lambda ap: ap.rearrange("p (h d) -> p h d", h=nh)
                P.op("act", lambda e: e.activation(out=sq[:], in_=src_ps, func=AF.Square), reads=[d_src], writes=[d_sq])
                P.op("dve", lambda e: e.tensor_reduce(out=hs[:, 0:nh], in_=v(sq[:]), axis=AX.X, op=ALU.add), reads=[d_sq], writes=[d_hs])
                P.op("act", lambda e: e.activation(out=hr[:, 0:nh], in_=hs[:, 0:nh], func=AF.Sqrt, bias=EPS, scale=1.0 / hd), reads=[d_hs], writes=[d_hr])
                P.op("dve", lambda e: e.reciprocal(out=hr[:, 0:nh], in_=hr[:, 0:nh]), reads=[d_hr], writes=[d_hr])
                P.op("dve", lambda e: e.tensor_tensor(out=v(qmn[:]), in0=v(src_ps), in1=hr[:, 0:nh].unsqueeze(2).to_broadcast([128, nh, hd]), op=ALU.mult),
                     reads=[d_src, d_hr], writes=[d_qmn])
                P.op("pool", lambda e: e.tensor_tensor(out=dst, in0=qmn[:], in1=gain[:], op=ALU.mult), reads=[d_qmn, d_c], writes=[d_dst])

            with contextlib.ExitStack() as stm:
                wkv = stm.enter_context(nc.sbuf_tensor("s_wkv", [128, 8, 1024], BF16)); d_wkv = Dep()
                mt_ = stm.enter_context(nc.sbuf_tensor("s_memt", [128, 2, D], F32)); d_mt = Dep()
                gme = stm.enter_context(nc.sbuf_tensor("s_gme", [128, D], F32)); d_gme = Dep()
                mhb = stm.enter_context(nc.sbuf_tensor("s_mhb", [128, 2, D], BF16)); d_mhb = Dep()
                mT = stm.enter_context(nc.sbuf_tensor("s_mT", [128, 8, 256], BF16)); d_mT = Dep()
                ss2 = stm.enter_context(nc.sbuf_tensor("s_ss2", [128, 2], F32)); d_ss2 = Dep()
                for k in range(8):
                    P.dma("pool", lambda e, k=k: e.dma_start(out=wkv[:, k, :], in_=wmemkv[k * 128:(k + 1) * 128, :]), writes=[d_wkv])
                P.dma("sp", lambda e: e.dma_start(out=mt_[:], in_=mem_d[:, :].rearrange("(t p) d -> p t d", p=128)), writes=[d_mt])
                P.dma("sp", lambda e: e.dma_start(out=gme[:], in_=gmem_d[0:1, :].partition_broadcast(128)), writes=[d_gme])
                for it in range(2):
                    P.op("act", lambda e, it=it: e.activation(out=junk[:], in_=mt_[:, it, :], func=AF.Square, accum_out=ss2[:, it:it + 1]),
                         reads=[d_mt], writes=[d_junk, d_ss2])
                P.op("act", lambda e: e.activation(out=ss2[:], in_=ss2[:], func=AF.Sqrt, bias=EPS, scale=1.0 / D), reads=[d_ss2], writes=[d_ss2])
                P.op("dve", lambda e: e.reciprocal(out=ss2[:], in_=ss2[:]), reads=[d_ss2], writes=[d_ss2])
                for it in range(2):
                    P.op("dve", lambda e, it=it: e.scalar_tensor_tensor(out=mhb[:, it, :], in0=mt_[:, it, :], scalar=ss2[:, it:it + 1], in1=gme[:],
                                                                        op0=ALU.mult, op1=ALU.mult), reads=[d_mt, d_ss2, d_gme], writes=[d_mhb])
                    for k in range(8):
                        P.op("pe", lambda e, it=it, k=k: e.transpose(out=pT[:, k, :], in_=mhb[:, it, k * 128:(k + 1) * 128], identity=idb[:]),
                             reads=[d_mhb, d_idb], writes=[d_pT])
                    P.op("act", lambda e, it=it: e.copy(out=mT[:, :, it * 128:(it + 1) * 128], in_=pT[:]), reads=[d_pT], writes=[d_mT])
                for it in range(2):
                    for hv in range(2):
                        for k in range(8):
                            P.op("pe", lambda e, it=it, hv=hv, k=k: e.matmul(pG[hv][:, :], lhsT=mT[:, k, it * 128:(it + 1) * 128], rhs=wkv[:, k, hv * 512:(hv + 1) * 512],
                                                                           start=(k == 0), stop=(k == 7)), reads=[d_mT, d_wkv], writes=[d_pG[hv]])
                    head_norm(pG[0][:, :], d_pG[0], mkn, 4, qmb[:], d_qmb)
                    for h in range(4):
                        P.op("pe", lambda e, h=h: e.transpose(out=pT[:, h, :], in_=qmb[:, h * 128:(h + 1) * 128], identity=idb[:]),
                             reads=[d_qmb, d_idb], writes=[d_pT])
                    P.op("act", lambda e, it=it: e.copy(out=kmT[:, :, it * 128:(it + 1) * 128], in_=pT[:, 0:4, :]), reads=[d_pT], writes=[d_kmT])
                    P.op("act", lambda e, it=it: e.copy(out=vm[:, it, :], in_=pG[1][:, :]), reads=[d_pG[1]], writes=[d_vm])

            wqm = sb("wqm", [128, 8, 512], BF16)
            wg = sb("wg", [128, 8, 3072], BF16)
            woa = sb("woa", [128, 4, 1024], BF16)
            wos = sb("wos", [128, 8, 1024], BF16)
            wom = sb("wom", [128, 4, 1024], BF16)
            wout = sb("wout", [128, 8, 1024], BF16)
            wr = sb("wr", [128, 8, 36], F32)
            for k in range(8):
                rows = slice(k * 128, (k + 1) * 128)
                P.dma("pool", lambda e, k=k, rows=rows: e.dma_start(out=wqm[:, k, :], in_=w_in[rows, C_QM:C_QM + 512]), writes=[d_w])
                for j in range(2):
                    P.dma("pool", lambda e, k=k, rows=rows, j=j: e.dma_start(out=wg[:, k, j * 1536:(j + 1) * 1536], in_=w_in[rows, C_G + j * 1536:C_G + (j + 1) * 1536]), writes=[d_w])
                P.dma("pool", lambda e, k=k, rows=rows: e.dma_start(out=wos[:, k, :], in_=wos_d[rows, :]), writes=[d_w])
                P.dma("pool", lambda e, k=k, rows=rows: e.dma_start(out=wout[:, k, :], in_=wout_d[rows, :]), writes=[d_w])
                P.dma("sp", lambda e, k=k, rows=rows: e.dma_start(out=wr[:, k, :], in_=wr_d[rows, :]), writes=[d_w])
            for k in range(4):
                rows = slice(k * 128, (k + 1) * 128)
                P.dma("pool", lambda e, k=k, rows=rows: e.dma_start(out=woa[:, k, :], in_=woa_d[rows, :]), writes=[d_w])
                P.dma("pool", lambda e, k=k, rows=rows: e.dma_start(out=wom[:, k, :], in_=wom_d[rows, :]), writes=[d_w])
            def loads(tj):
                xj = tj % 2
                rj = tj * 128
                P.dma("sp", lambda e: e.dma_start(out=xt[xj][:], in_=xown[rj:rj + 128, :]), writes=[d_xt[xj]])
                P.dma("sp", lambda e: e.dma_start(out=oat[xj][:], in_=oa_d[rj:rj + 128, :]), reads=[d_oad], writes=[d_oat[xj]])
                P.dma("sp", lambda e: e.dma_start(out=ost[xj][:], in_=os_d[rj:rj + 128, :]), reads=[d_osd], writes=[d_ost[xj]])

            for ti in range(ntiles):
                r0 = ti * 128
                hbuf = ti % 2
                xb = ti % 2
                if ti == 0:
                    loads(0)
                if ti + 1 < ntiles:
                    loads(ti + 1)
                P.op("act", lambda e, xb=xb: e.activation(out=junk[:], in_=xt[xb][:], func=AF.Square, accum_out=ssq[:]), reads=[d_xt[xb]], writes=[d_junk, d_ssq])
                P.op("act", lambda e: e.activation(out=rstd[:], in_=ssq[:], func=AF.Sqrt, bias=EPS, scale=1.0 / D), reads=[d_ssq], writes=[d_rstd])
                P.op("dve", lambda e: e.reciprocal(out=rstd[:], in_=rstd[:]), reads=[d_rstd], writes=[d_rstd])
                P.op("dve", lambda e, xb=xb: e.scalar_tensor_tensor(out=hb[:], in0=xt[xb][:], scalar=rstd[:, 0:1], in1=gm[:], op0=ALU.mult, op1=ALU.mult),
                     reads=[d_xt[xb], d_rstd, d_c], writes=[d_hb])
                for k in range(8):
                    P.op("pe", lambda e, k=k: e.transpose(out=pT[:, k, :], in_=hb[:, k * 128:(k + 1) * 128], identity=idb[:]), reads=[d_hb, d_idb], writes=[d_pT])
                P.op("act", lambda e: e.copy(out=hT[:], in_=pT[:]), reads=[d_pT], writes=[d_hT])
                for k in range(8):
                    P.op("pe", lambda e, k=k: e.matmul(pG[0][:, :], lhsT=hT[:, k, :], rhs=wqm[:, k, :], start=(k == 0), stop=(k == 7)),
                         reads=[d_hT, d_w], writes=[d_pG[0]])
                head_norm(pG[0][:, :], d_pG[0], mqn, 4, qmb[:], d_qmb)
                for g6 in range(6):
                    bk = g6 % 3
                    for k in range(8):
                        P.op("pe", lambda e, g6=g6, k=k, bk=bk: e.matmul(pG[bk][:, :], lhsT=hT[:, k, :], rhs=wg[:, k, g6 * 512:(g6 + 1) * 512], start=(k == 0), stop=(k == 7)),
                             reads=[d_hT, d_w], writes=[d_pG[bk]])
                    P.op("act", lambda e, g6=g6, bk=bk: e.activation(out=gs[:, g6 * 512:(g6 + 1) * 512], in_=pG[bk][:, :], func=AF.Sigmoid),
                         reads=[d_pG[bk]], writes=[d_gs])
                for h in range(4):
                    P.op("pe", lambda e, h=h: e.transpose(out=pT[:, h, :], in_=qmb[:, h * 128:(h + 1) * 128], identity=idb[:]), reads=[d_qmb, d_idb], writes=[d_pT])
                P.op("act", lambda e: e.copy(out=qmT[:], in_=pT[:, 0:4, :]), reads=[d_pT], writes=[d_qmT])
                for h in range(4):
                    for mt in range(2):
                        bank = pms[h // 2]
                        c0 = ((h % 2) * 2 + mt) * 128
                        P.op("pe", lambda e, h=h, mt=mt, bank=bank, c0=c0: e.matmul(bank[:, c0:c0 + 128], lhsT=kmT[:, h, mt * 128:(mt + 1) * 128], rhs=qmT[:, h, :],
                                                                                  start=True, stop=True), reads=[d_kmT, d_qmT], writes=[d_pms[h // 2]])
                for hp in range(2):
                    P.op("act", lambda e, hp=hp: e.activation(out=pm[:, hp * 2:(hp + 1) * 2, :, :].rearrange("p a b c -> p (a b c)"), in_=pms[hp][:, :], func=AF.Exp,
                                                              scale=float(128 ** -0.5)), reads=[d_pms[hp]], writes=[d_pm])
                for h in range(4):
                    for mt in range(2):
                        P.op("pe", lambda e, h=h, mt=mt: e.matmul(pmo[:, h * 128:(h + 1) * 128], lhsT=vm[:, mt, h * 128:(h + 1) * 128], rhs=pm[:, h, mt, :],
                                                                 start=(mt == 0), stop=(mt == 1)), reads=[d_vm, d_pm], writes=[d_pmo])
                for mt in range(2):
                    P.op("pe", lambda e, mt=mt: e.matmul(pG[1][:, :].rearrange("p (h t) -> p h t", h=4), lhsT=onesb[:], rhs=pm[:, :, mt, :],
                                                        start=(mt == 0), stop=(mt == 1)), reads=[d_c, d_pm], writes=[d_pG[1]])
                P.op("dve", lambda e: e.reciprocal(out=rcp[:], in_=pG[1][:, :]), reads=[d_pG[1]], writes=[d_rcp])
                P.op("dve", lambda e: e.tensor_tensor(out=omT[:].rearrange("p h t -> p (h t)"), in0=pmo[:, :], in1=rcp[:], op=ALU.mult),
                     reads=[d_pmo, d_rcp], writes=[d_omT])
                for k in range(4):
                    P.op("pe", lambda e, xb=xb, k=k: e.transpose(out=pT[:, k, :], in_=oat[xb][:, k * 128:(k + 1) * 128], identity=idb[:]), reads=[d_oat[xb], d_idb], writes=[d_pT])
                P.op("act", lambda e: e.copy(out=oaT[:], in_=pT[:, 0:4, :]), reads=[d_pT], writes=[d_oaT])
                for k in range(8):
                    P.op("pe", lambda e, xb=xb, k=k: e.transpose(out=pT[:, k, :], in_=ost[xb][:, k * 128:(k + 1) * 128], identity=idb[:]), reads=[d_ost[xb], d_idb], writes=[d_pT])
                P.op("act", lambda e: e.copy(out=osT[:], in_=pT[:]), reads=[d_pT], writes=[d_osT])
                for hv in range(2):
                    cs_ = slice(hv * 512, (hv + 1) * 512)
                    for k in range(4):
                        P.op("pe", lambda e, k=k, cs_=cs_: e.matmul(pG[0][:, :], lhsT=oaT[:, k, :], rhs=woa[:, k, cs_], start=(k == 0), stop=(k == 3)),
                             reads=[d_oaT, d_w], writes=[d_pG[0]])
                    for k in range(8):
                        P.op("pe", lambda e, k=k, cs_=cs_: e.matmul(pG[1][:, :], lhsT=osT[:, k, :], rhs=wos[:, k, cs_], start=(k == 0), stop=(k == 7)),
                             reads=[d_osT, d_w], writes=[d_pG[1]])
                    for k in range(4):
                        P.op("pe", lambda e, k=k, cs_=cs_: e.matmul(pG[2][:, :], lhsT=omT[:, k, :], rhs=wom[:, k, cs_], start=(k == 0), stop=(k == 3)),
                             reads=[d_omT, d_w], writes=[d_pG[2]])
                    P.op("dve", lambda e, hv=hv: e.tensor_tensor(out=mg[:], in0=pG[0][:, :], in1=gs[:, hv * 512:(hv + 1) * 512], op=ALU.mult),
                         reads=[d_pG[0], d_gs], writes=[d_mg])
                    P.op("dve", lambda e, hv=hv: e.tensor_tensor(out=tt[:], in0=pG[1][:, :], in1=gs[:, 1024 + hv * 512:1024 + (hv + 1) * 512], op=ALU.mult),
                         reads=[d_pG[1], d_gs], writes=[d_tt])
                    P.op("pool", lambda e: e.tensor_tensor(out=mg[:], in0=mg[:], in1=tt[:], op=ALU.add), reads=[d_mg, d_tt], writes=[d_mg])
                    P.op("dve", lambda e, hv=hv: e.tensor_tensor(out=tt[:], in0=pG[2][:, :], in1=gs[:, 2048 + hv * 512:2048 + (hv + 1) * 512], op=ALU.mult),
                         reads=[d_pG[2], d_gs], writes=[d_tt])
                    P.op("pool", lambda e, cs_=cs_: e.tensor_tensor(out=mgb[:, cs_], in0=mg[:], in1=tt[:], op=ALU.add), reads=[d_mg, d_tt], writes=[d_mgb])
                for k in range(8):
                    P.op("pe", lambda e, k=k: e.transpose(out=pT[:, k, :], in_=mgb[:, k * 128:(k + 1) * 128], identity=idb[:]), reads=[d_mgb, d_idb], writes=[d_pT])
                P.op("act", lambda e: e.copy(out=mgT[:], in_=pT[:]), reads=[d_pT], writes=[d_mgT])
                for hv in range(2):
                    cs_ = slice(hv * 512, (hv + 1) * 512)
                    for k in range(8):
                        P.op("pe", lambda e, k=k, cs_=cs_, hv=hv: e.matmul(pG[hv][:, :], lhsT=mgT[:, k, :], rhs=wout[:, k, cs_], start=(k == 0), stop=(k == 7)),
                             reads=[d_mgT, d_w], writes=[d_pG[hv]])
                    P.op("dve", lambda e, xb=xb, cs_=cs_, hv=hv: e.tensor_tensor(out=x1[:, cs_], in0=pG[hv][:, :], in1=xt[xb][:, cs_], op=ALU.add),
                         reads=[d_pG[hv], d_xt[xb]], writes=[d_x1])
                P.dma("sp", lambda e, r0=r0: e.dma_start(out=x1_d[r0:r0 + 128, :], in_=x1[:]), reads=[d_x1], writes=[d_x1d])
                if stage < 2:
                    continue
                P.op("act", lambda e: e.activation(out=junk[:], in_=x1[:], func=AF.Square, accum_out=ssq[:]), reads=[d_x1], writes=[d_junk, d_ssq])
                P.op("act", lambda e: e.activation(out=rstd[:], in_=ssq[:], func=AF.Sqrt, bias=EPS, scale=1.0 / D), reads=[d_ssq], writes=[d_rstd])
                P.op("dve", lambda e: e.reciprocal(out=rstd[:], in_=rstd[:]), reads=[d_rstd], writes=[d_rstd])
                P.op("dve", lambda e: e.scalar_tensor_tensor(out=h2f[:], in0=x1[:], scalar=rstd[:, 0:1], in1=gf[:], op0=ALU.mult, op1=ALU.mult),
                     reads=[d_x1, d_rstd, d_c], writes=[d_h2f])
                P.op("pool", lambda e, hbuf=hbuf: e.tensor_copy(out=h2b[hbuf][:], in_=h2f[:]), reads=[d_h2f], writes=[d_h2b[hbuf]])
                for half in range(2):
                    for k4 in range(4):
                        k = half * 4 + k4
                        P.op("pe", lambda e, k=k, k4=k4: e.transpose(out=pF[:, k4 * 128:(k4 + 1) * 128], in_=h2f[:, k * 128:(k + 1) * 128], identity=idf[:]),
                             reads=[d_h2f, d_idf], writes=[d_pF])
                    P.op("act", lambda e, half=half: e.copy(out=h2T[:, half * 4:(half + 1) * 4, :].rearrange("p a b -> p (a b)"), in_=pF[:, :]), reads=[d_pF], writes=[d_h2T])
                for k in range(8):
                    P.op("pe", lambda e, k=k: e.matmul(pF[:, 0:36], lhsT=h2T[:, k, :], rhs=wr[:, k, :], start=(k == 0), stop=(k == 7)), reads=[d_h2T, d_w], writes=[d_pF])
                P.op("act", lambda e: e.copy(out=lg[:], in_=pF[:, 0:36]), reads=[d_pF], writes=[d_lg])
                P.op("dve", lambda e: e.tensor_reduce(out=sm[:, 0:1], in_=lg[:, 0:4], axis=AX.X, op=ALU.max), reads=[d_lg], writes=[d_sm])
                P.op("dve", lambda e: e.tensor_scalar(out=sm[:, 1:2], in0=sm[:, 0:1], scalar1=-1.0, scalar2=None, op0=ALU.mult), reads=[d_sm], writes=[d_sm])
                P.op("act", lambda e: e.activation(out=sm[:, 8:12], in_=lg[:, 0:4], func=AF.Exp, bias=sm[:, 1:2], accum_out=sm[:, 2:3]), reads=[d_lg, d_sm], writes=[d_sm])
                P.op("dve", lambda e: e.reciprocal(out=sm[:, 3:4], in_=sm[:, 2:3]), reads=[d_sm], writes=[d_sm])
                P.op("dve", lambda e: e.tensor_scalar(out=goh[:], in0=lg[:, 0:4], scalar1=sm[:, 0:1], scalar2=None, op0=ALU.is_ge), reads=[d_lg, d_sm], writes=[d_goh])
                P.op("dve", lambda e: e.tensor_tensor(out=lsel[:].rearrange("p (g e) -> p g e", g=4), in0=lg[:, 4:36].rearrange("p (g e) -> p g e", g=4),
                                                      in1=goh[:].unsqueeze(2).to_broadcast([128, 4, 8]), op=ALU.mult), reads=[d_lg, d_goh], writes=[d_lsel])
                P.op("dve", lambda e: e.tensor_reduce(out=les[:], in_=lsel[:].rearrange("p (g e) -> p e g", g=4), axis=AX.X, op=ALU.add), reads=[d_lsel], writes=[d_les])
                P.op("dve", lambda e: e.max(out=t8[:], in_=les[:]), reads=[d_les], writes=[d_t8])
                P.op("dve", lambda e: e.tensor_tensor(out=sm[:, 4:5], in0=t8[:, 1:2], in1=t8[:, 0:1], op=ALU.subtract), reads=[d_t8, d_sm], writes=[d_sm])
                P.op("act", lambda e: e.activation(out=sm[:, 5:6], in_=sm[:, 4:5], func=AF.Exp), reads=[d_sm], writes=[d_sm])
                P.op("dve", lambda e: e.tensor_scalar(out=sm[:, 6:7], in0=sm[:, 5:6], scalar1=1.0, scalar2=None, op0=ALU.add), reads=[d_sm], writes=[d_sm])
                P.op("dve", lambda e: e.reciprocal(out=sm[:, 6:7], in_=sm[:, 6:7]), reads=[d_sm], writes=[d_sm])
                P.op("dve", lambda e: e.tensor_tensor(out=sm[:, 7:8], in0=sm[:, 5:6], in1=sm[:, 6:7], op=ALU.mult), reads=[d_sm], writes=[d_sm])
                P.op("dve", lambda e, ti=ti: e.tensor_scalar(out=combs[:, ti, :], in0=sm[:, 6:8], scalar1=sm[:, 3:4], scalar2=None, op0=ALU.mult), reads=[d_sm], writes=[d_combs])
                for j in range(2):
                    P.op("dve", lambda e, j=j: e.tensor_scalar(out=oh[:, j * 32:(j + 1) * 32], in0=lg[:, 4:36], scalar1=t8[:, j:j + 1], scalar2=None, op0=ALU.is_equal),
                         reads=[d_lg, d_t8], writes=[d_oh])
                    P.op("dve", lambda e, j=j: e.tensor_tensor(out=oh[:, j * 32:(j + 1) * 32].rearrange("p (g e) -> p g e", g=4), in0=oh[:, j * 32:(j + 1) * 32].rearrange("p (g e) -> p g e", g=4),
                                                               in1=goh[:].unsqueeze(2).to_broadcast([128, 4, 8]), op=ALU.mult), reads=[d_oh, d_goh], writes=[d_oh])
                P.op("dve", lambda e: e.tensor_tensor(out=ohs[:], in0=oh[:, 0:32], in1=oh[:, 32:64], op=ALU.add), reads=[d_oh], writes=[d_ohs])
                P.op("pe", lambda e: e.matmul(pF[:, 64:96], lhsT=tri[:], rhs=ohs[:], start=True, stop=True), reads=[d_c, d_ohs], writes=[d_pF])
                P.op("pe", lambda e: e.matmul(pF[:, 128:160], lhsT=onesf[:], rhs=ohs[:], start=True, stop=True), reads=[d_c, d_ohs], writes=[d_pF])
                P.op("dve", lambda e: e.tensor_tensor(out=posf[:], in0=pF[:, 64:96], in1=ohs[:], op=ALU.subtract), reads=[d_pF, d_ohs], writes=[d_posf])
                P.op("dve", lambda e: e.tensor_tensor(out=posf[:], in0=posf[:], in1=base[:], op=ALU.add), reads=[d_posf, d_base], writes=[d_posf])
                P.op("dve", lambda e: e.tensor_tensor(out=posf[:], in0=posf[:], in1=ecap[:], op=ALU.add), reads=[d_posf, d_c], writes=[d_posf])
                P.op("dve", lambda e: e.tensor_tensor(out=base[:], in0=base[:], in1=pF[:, 128:160], op=ALU.add), reads=[d_base, d_pF, d_posf], writes=[d_base])
                for j in range(2):
                    P.op("dve", lambda e, j=j: e.tensor_tensor(out=oh[:, j * 32:(j + 1) * 32], in0=oh[:, j * 32:(j + 1) * 32], in1=posf[:], op=ALU.mult), reads=[d_oh, d_posf], writes=[d_oh])
                    P.op("dve", lambda e, j=j: e.tensor_reduce(out=idf2[:, j:j + 1], in_=oh[:, j * 32:(j + 1) * 32], axis=AX.X, op=ALU.add), reads=[d_oh], writes=[d_idf2])
                P.op("dve", lambda e, ti=ti: e.tensor_copy(out=idxs[:, ti, :], in_=idf2[:]), reads=[d_idf2], writes=[d_idxs])
                for j in range(2):
                    P.dma("pool", lambda e, ti=ti, j=j, hbuf=hbuf: e.indirect_dma_start(out=xbuf[:, :], out_offset=bass.IndirectOffsetOnAxis(ap=idxs[:, ti, j:j + 1], axis=0),
                                                                                      in_=h2b[hbuf][:], in_offset=None),
                          reads=[d_h2b[hbuf], d_idxs], writes=[d_xbuf])

        if stage < 3:
            return
        if hasattr(P, 'barrier'):
            P.barrier()
        with contextlib.ExitStack() as st:
            def sb(name, shape, dt):
                return st.enter_context(nc.sbuf_tensor("d_" + name, shape, dt))

            def ps(name, shape, dt=F32):
                return st.enter_context(nc.psum_tensor(name, shape, dt))
            NW = 2
            wge = [sb("wge%d" % i, [128, 8, 512], BF16) for i in range(NW)]; d_wge = [Dep() for _ in range(NW)]
            wue = [sb("wue%d" % i, [128, 8, 512], BF16) for i in range(NW)]; d_wue = [Dep() for _ in range(NW)]
            wde = [sb("wde%d" % i, [128, 4, 1024], BF16) for i in range(NW)]; d_wde = [Dep() for _ in range(NW)]
            xe = [sb("xe%d" % i, [128, 3, D], BF16) for i in range(NW)]; d_xe = [Dep() for _ in range(NW)]
            xeT = sb("xeT", [128, 8, CAP], BF16); d_xeT = Dep()
            sg = sb("sg", [128, CAP], F32); d_sg = Dep()
            hTe = sb("hTe", [128, 4, CAP], BF16); d_hTe = Dep()
            ye = [sb("ye%d" % i, [128, 3, D], F32) for i in range(NW)]; d_ye = [Dep() for _ in range(NW)]
            pT = ps("pTe", [128, 8, 128], BF16); d_pT = Dep()
            pg_ = [ps("pge%d" % i, [128, 512], F32) for i in range(2)]; d_pg = [Dep() for _ in range(2)]
            pu_ = [ps("pue%d" % i, [128, 512], F32) for i in range(2)]; d_pu = [Dep() for _ in range(2)]
            py_ = [ps("pye%d" % i, [128, 512], F32) for i in range(2)]; d_py = [Dep() for _ in range(2)]
            for ie, ex in enumerate(experts):
                b = ie % NW
                P.dma("pool", lambda e, b=b, ex=ex: e.dma_start(out=wge[b][:], in_=wgate_d[ex].rearrange("(k p) f -> p k f", p=128)), writes=[d_wge[b]])
                P.dma("pool", lambda e, b=b, ex=ex: e.dma_start(out=wue[b][:], in_=wup_d[ex].rearrange("(k p) f -> p k f", p=128)), writes=[d_wue[b]])
                P.dma("pool", lambda e, b=b, ex=ex: e.dma_start(out=wde[b][:], in_=wdown_d[ex].rearrange("(k p) f -> p k f", p=128)), writes=[d_wde[b]])
                P.dma("sp", lambda e, b=b, ex=ex: e.dma_start(out=xe[b][:], in_=xbuf[ex * CAP:(ex + 1) * CAP, :].rearrange("(s p) d -> p s d", p=128)),
                      reads=[d_xbuf], writes=[d_xe[b]])
                for s in range(3):
                    for k in range(8):
                        P.op("pe", lambda e, b=b, s=s, k=k: e.transpose(out=pT[:, k, :], in_=xe[b][:, s, k * 128:(k + 1) * 128], identity=idb[:]),
                             reads=[d_xe[b], d_idb], writes=[d_pT])
                    P.op("act", lambda e, s=s: e.copy(out=xeT[:, :, s * 128:(s + 1) * 128], in_=pT[:]), reads=[d_pT], writes=[d_xeT])
                for ft in range(4):
                    pb = ft % 2
                    for k in range(8):
                        P.op("pe", lambda e, b=b, ft=ft, k=k, pb=pb: e.matmul(pg_[pb][:, 0:CAP], lhsT=wge[b][:, k, ft * 128:(ft + 1) * 128], rhs=xeT[:, k, :],
                                                                            start=(k == 0), stop=(k == 7)), reads=[d_wge[b], d_xeT], writes=[d_pg[pb]])
                    for k in range(8):
                        P.op("pe", lambda e, b=b, ft=ft, k=k, pb=pb: e.matmul(pu_[pb][:, 0:CAP], lhsT=wue[b][:, k, ft * 128:(ft + 1) * 128], rhs=xeT[:, k, :],
                                                                            start=(k == 0), stop=(k == 7)), reads=[d_wue[b], d_xeT], writes=[d_pu[pb]])
                    P.op("act", lambda e, pb=pb: e.activation(out=sg[:], in_=pg_[pb][:, 0:CAP], func=AF.Silu), reads=[d_pg[pb]], writes=[d_sg])
                    P.op("dve", lambda e, ft=ft, pb=pb: e.tensor_tensor(out=hTe[:, ft, :], in0=sg[:], in1=pu_[pb][:, 0:CAP], op=ALU.mult),
                         reads=[d_sg, d_pu[pb]], writes=[d_hTe])
                for s in range(3):
                    for hv in range(2):
                        for ft in range(4):
                            P.op("pe", lambda e, b=b, s=s, hv=hv, ft=ft: e.matmul(py_[hv][:, :], lhsT=hTe[:, ft, s * 128:(s + 1) * 128], rhs=wde[b][:, ft, hv * 512:(hv + 1) * 512],
                                                                                start=(ft == 0), stop=(ft == 3)), reads=[d_hTe, d_wde[b]], writes=[d_py[hv]])
                        P.op("act" if hv == 0 else "dve", (lambda e, b=b, s=s, hv=hv: e.copy(out=ye[b][:, s, hv * 512:(hv + 1) * 512], in_=py_[hv][:, :])) if hv == 0 else
                             (lambda e, b=b, s=s, hv=hv: e.tensor_copy(out=ye[b][:, s, hv * 512:(hv + 1) * 512], in_=py_[hv][:, :])),
                             reads=[d_py[hv]], writes=[d_ye[b]])
                P.dma("sp", lambda e, b=b, ex=ex: e.dma_start(out=ybuf[ex * CAP:(ex + 1) * CAP, :].rearrange("(s p) d -> p s d", p=128), in_=ye[b][:]),
                      reads=[d_ye[b]], writes=[d_ybuf])

        if hasattr(P, 'barrier'):
            P.barrier()
        with contextlib.ExitStack() as st:
            def sb(name, shape, dt):
                return st.enter_context(nc.sbuf_tensor("d_" + name, shape, dt))
            NF = 2
            x1t = [sb("x1t%d" % i, [128, D], F32) for i in range(NF)]; d_x1t = [Dep() for _ in range(NF)]
            y1 = [sb("y1t%d" % i, [128, D], F32) for i in range(NF)]; d_y1 = [Dep() for _ in range(NF)]
            y2 = [sb("y2t%d" % i, [128, D], F32) for i in range(NF)]; d_y2 = [Dep() for _ in range(NF)]
            ot = [sb("ot%d" % i, [128, D], F32) for i in range(NF)]; d_ot = [Dep() for _ in range(NF)]
            for ti in range(ntiles):
                b = ti % NF
                r0 = ti * 128
                P.dma("sp", lambda e, b=b, r0=r0: e.dma_start(out=x1t[b][:], in_=x1_d[r0:r0 + 128, :]), reads=[d_x1d], writes=[d_x1t[b]])
                P.dma("pool", lambda e, b=b, ti=ti: e.indirect_dma_start(out=y1[b][:], out_offset=None, in_=ybuf[:, :],
                                                                        in_offset=bass.IndirectOffsetOnAxis(ap=idxs[:, ti, 0:1], axis=0)),
                      reads=[d_ybuf, d_idxs], writes=[d_y1[b]])
                P.dma("pool", lambda e, b=b, ti=ti: e.indirect_dma_start(out=y2[b][:], out_offset=None, in_=ybuf[:, :],
                                                                        in_offset=bass.IndirectOffsetOnAxis(ap=idxs[:, ti, 1:2], axis=0)),
                      reads=[d_ybuf, d_idxs], writes=[d_y2[b]])
                P.op("dve", lambda e, b=b, ti=ti: e.scalar_tensor_tensor(out=ot[b][:], in0=y1[b][:], scalar=combs[:, ti, 0:1], in1=x1t[b][:], op0=ALU.mult, op1=ALU.add),
                     reads=[d_y1[b], d_combs, d_x1t[b]], writes=[d_ot[b]])
                P.op("dve", lambda e, b=b, ti=ti: e.scalar_tensor_tensor(out=ot[b][:], in0=y2[b][:], scalar=combs[:, ti, 1:2], in1=ot[b][:], op0=ALU.mult, op1=ALU.add),
                     reads=[d_y2[b], d_combs, d_ot[b]], writes=[d_ot[b]])
                P.dma("sp", lambda e, b=b, r0=r0: e.dma_start(out=out_d[r0:r0 + 128, :], in_=ot[b][:]), reads=[d_ot[b]], writes=[d_out])


def host_consts_d(z):
    tri = (np.arange(128)[:, None] <= np.arange(128)[None, :]).astype(np.float32)
    return dict(mqn=np.tile(z['mem_q_norm'], 4)[None, :], mkn=np.tile(z['mem_k_norm'], 4)[None, :],
                w_r=np.ascontiguousarray(np.concatenate([z['w_router_group'], z['w_router_expert']], axis=1)),
                trif=tri, ecap=(np.arange(32, dtype=np.float32) * CAP)[None, :], ident=np.eye(128, dtype=np.float32))


def build_full():
    nc = bass.Bass("TRN2", target_bir_lowering=False)
    P = Prog(nc)

    def di(name, shape, dt=F32):
        return nc.dram_tensor(name, shape, dt, kind="ExternalInput")
    xin = di("xin", [TP + T, D]); w_in = di("w_in", [D, IN_COLS]); g_mix = di("g_mix", [1, D])
    qkn = di("qkn", [2, 512]); cs = di("cs", [TP + T, 64]); ident_d = di("ident", [128, 128])
    e_d = di("e_d", [32, NK], BF16); vq_d = di("vq", [1, 1024]); nb_d = di("nb", [1, 1024]); oq_d = di("oq", [1, 1024])
    tri_d = di("tri", [128, 128], BF16)
    cwl_d = di("cwl", [128, 16, 4]); cbl_d = di("cbl", [128, 16]); dtb_d = di("dtb", [1, 16]); alog_d = di("alog", [1, 16])
    dsk_d = di("dsk", [1, 16]); ssdn_d = di("ssdn", [1, D]); tri2_d = di("tri2", [128, 2, 256]); sel_d = di("sel_d", [16, 16, 128])
    tb_d = di("tb_d", [128, 384]); pflag_d = di("pflag", [1, 1])
    mem_d = di("mem", [256, D]); gmem_d = di("g_mem", [1, D]); wmemkv = di("w_mem_kv", [D, 1024])
    mqn_d = di("mqn", [1, 512]); mkn_d = di("mkn", [1, 512])
    woa_d = di("w_o_moba", [512, D]); wos_d = di("w_o_ssd", [D, D]); wom_d = di("w_o_mem", [512, D]); wout_d = di("w_out", [D, D])
    gffn_d = di("g_ffn", [1, D]); wr_d = di("w_r", [D, 36])
    wgate_d = di("w_gate", [NE, D, 512]); wup_d = di("w_up", [NE, D, 512]); wdown_d = di("w_down", [NE, 512, D])
    trif_d = di("trif", [128, 128]); ecap_d = di("ecap", [1, 32])
    out_d = nc.dram_tensor("out", [T, D], F32, kind="ExternalOutput")
    kT_d = nc.dram_tensor("kT_d", [512, NK], BF16)
    v_d = nc.dram_tensor("v_d", [NK, 512], BF16)
    qT_d = nc.dram_tensor("qT_d", [512, T], BF16)
    oa_d = nc.dram_tensor("oa_d", [T, 512], BF16)
    os_d = nc.dram_tensor("os_d", [T, D], BF16)
    x1_d = nc.dram_tensor("x1_d", [T, D], F32)
    xbuf = nc.dram_tensor("xbuf", [NE * CAP, D], BF16)
    ybuf = nc.dram_tensor("ybuf", [NE * CAP, D], F32)

    with contextlib.ExitStack() as st0:
        idf = st0.enter_context(nc.sbuf_tensor("idf", [128, 128], F32)); d_idf = Dep()
        idb = st0.enter_context(nc.sbuf_tensor("idb", [128, 128], BF16)); d_idb = Dep()
        P.dma("sp", lambda e: e.dma_start(out=idf[:], in_=ident_d[:, :]), writes=[d_idf])
        P.op("dve", lambda e: e.tensor_copy(out=idb[:], in_=idf[:]), reads=[d_idf], writes=[d_idb])
        with contextlib.ExitStack() as st:
            phase_a(nc, P, st, xin, w_in, g_mix, qkn, cs, idb, d_idb, kT_d, v_d, qT_d)
        P.barrier()
        with contextlib.ExitStack() as st:
            oa = st.enter_context(nc.sbuf_tensor("s_oa", [128, 32, 512], BF16)); d_oa = Dep()
            phase_b(nc, P, st, kT_d, v_d, qT_d, e_d, vq_d, nb_d, oq_d, tri_d, idb, d_idb, oa, d_oa)
            P.dma("sp", lambda e: e.dma_start(out=oa_d[:, :].rearrange("(t p) c -> p t c", p=128), in_=oa[:]), reads=[d_oa])
        P.barrier()
        with contextlib.ExitStack() as st:
            phase_c(nc, P, st, xin, w_in, g_mix, cwl_d, cbl_d, dtb_d, alog_d, dsk_d, ssdn_d, tri2_d, sel_d, tb_d, pflag_d,
                    idb, d_idb, os_d, Dep())
        P.barrier()
        phase_def(nc, P, xin[TP:TP + T, :], w_in, g_mix, mem_d, gmem_d, wmemkv, mqn_d, mkn_d, woa_d, wos_d, wom_d, wout_d, gffn_d, wr_d,
                  wgate_d, wup_d, wdown_d, trif_d, ecap_d, oa_d, Dep(), os_d, Dep(), x1_d, xbuf, ybuf, out_d, idb, d_idb, idf, d_idf)
        P.emit()
    return nc


_NC_CACHE = {}


def kernel(x, mem, g_mix, w_in, moba_q_norm, moba_k_norm, conv_w, conv_b, dt_bias, a_log, d_skip, ssd_norm, g_mem, w_mem_kv,
           mem_q_norm, mem_k_norm, w_o_moba, w_o_ssd, w_o_mem, w_out, g_ffn, w_router_group, w_router_expert, w_gate, w_up, w_down):
    f32 = np.float32
    A = lambda a: np.ascontiguousarray(np.asarray(a, dtype=f32))
    x = A(x); mem = A(mem)
    z = dict(conv_w=A(conv_w), conv_b=A(conv_b), dt_bias=A(dt_bias), a_log=A(a_log), d_skip=A(d_skip), ssd_norm=A(ssd_norm),
             mem_q_norm=A(mem_q_norm), mem_k_norm=A(mem_k_norm), w_router_group=A(w_router_group), w_router_expert=A(w_router_expert))
    if "nc" not in _NC_CACHE:
        _NC_CACHE["nc"] = build_full()
    nc = _NC_CACHE["nc"]
    pos = np.arange(2 * T, dtype=f32)
    inv = (10000.0 ** (-np.arange(32, dtype=f32) / 32)).astype(f32)
    ang = pos[:, None] * inv[None, :]
    cs_full = np.concatenate([np.cos(ang), np.sin(ang)], axis=1).astype(f32)
    qkn = np.stack([np.tile(A(moba_q_norm), 8), np.tile(A(moba_k_norm), 8)]).astype(f32)
    shared = dict(w_in=A(w_in), g_mix=A(g_mix)[None, :], qkn=qkn, ident=np.eye(128, dtype=f32),
                  g_mem=A(g_mem)[None, :], w_mem_kv=A(w_mem_kv), w_o_moba=A(w_o_moba), w_o_ssd=A(w_o_ssd), w_o_mem=A(w_o_mem),
                  w_out=A(w_out), g_ffn=A(g_ffn)[None, :], w_gate=A(w_gate), w_up=A(w_up), w_down=A(w_down))
    shared.update(host_consts_d(z))
    in_maps = []
    for c in range(8):
        bb, hh = c // 2, c % 2
        if hh == 0:
            xin = np.concatenate([np.zeros((TP, D), f32), x[bb, :T]], 0)
            csc = np.concatenate([cs_full[:TP], cs_full[:T]], 0)
        else:
            xin = x[bb]
            csc = cs_full
        m = dict(shared)
        m.update(xin=np.ascontiguousarray(xin), cs=np.ascontiguousarray(csc), mem=mem[bb])
        m.update(host_consts(hh))
        m.update(host_consts_c(z, hh))
        in_maps.append(m)
    res = run_bass_kernel_spmd(nc, in_maps, core_ids=list(range(8)))
    out = np.empty((4, 2 * T, D), f32)
    for c in range(8):
        bb, hh = c // 2, c % 2
        out[bb, hh * T:(hh + 1) * T] = np.asarray(res.results[c]["out"], dtype=f32)
    return out
```

```python
import contextlib
import numpy as np
import ml_dtypes
import concourse.bass as bass
import concourse.mybir as mybir
from concourse.bass_utils import run_bass_kernel_spmd

F32 = mybir.dt.float32
BF16 = mybir.dt.bfloat16
I32 = mybir.dt.int32
U32 = mybir.dt.uint32
AF = mybir.ActivationFunctionType
ALU = mybir.AluOpType
AX = mybir.AxisListType

T = 4096
TP = 4096
NK = TP + T
D = 1024
EPS = 1e-6
IN_COLS = 8208
BIGG = 30000.0
MB = 1000.0
NEG = -30000.0
C_Z, C_X, C_DT = 1536, 2560, 4608
C_QM, C_G = 4624, 5136
CAP = 384
NE = 32


class Dep:
    __slots__ = ("w", "r")

    def __init__(self):
        self.w = None
        self.r = {}


class Op:
    __slots__ = ("eng", "fn", "deps", "is_dma", "sig", "sigval", "dsem", "dval", "prev")

    def __init__(self, eng, fn, is_dma):
        self.eng = eng
        self.fn = fn
        self.is_dma = is_dma
        self.deps = []
        self.sig = False
        self.sigval = 0
        self.dsem = None
        self.dval = 0
        self.prev = None


class Prog:
    ENGS = ("pe", "act", "dve", "pool", "sp")

    def __init__(self, nc, ndma_sems=12):
        self.nc = nc
        self.ops = {e: [] for e in self.ENGS}
        self.ndma = {e: 0 for e in self.ENGS}
        self.dma_last = {}
        self.ndma_sems = ndma_sems
        self.all_dma = []

    def _add(self, o, reads, writes):
        deps = {}
        raw = set()
        for t in reads:
            if t.w is not None:
                deps[id(t.w)] = t.w
                raw.add(id(t.w))
        for t in writes:
            if t.w is not None:
                deps[id(t.w)] = t.w
            for r in t.r.values():
                deps[id(r)] = r
        for t in reads:
            key = id(o) if o.is_dma else o.eng
            t.r[key] = o
        for t in writes:
            t.w = o
            t.r = {}
        dl = []
        for d in deps.values():
            if d is o:
                continue
            if (not d.is_dma) and (not o.is_dma) and d.eng == "pe" and o.eng == "pe":
                continue
            if (not d.is_dma) and (not o.is_dma) and d.eng == o.eng and id(d) not in raw:
                continue
            dl.append(d)
            if not d.is_dma:
                d.sig = True
        o.deps = dl
        self.ops[o.eng].append(o)
        return o

    def op(self, eng, fn, reads=(), writes=()):
        return self._add(Op(eng, fn, False), reads, writes)

    def dma(self, eng, fn, reads=(), writes=()):
        o = Op(eng, fn, True)
        n = self.ndma[eng]
        self.ndma[eng] += 1
        slot = (eng, n % self.ndma_sems)
        o.dsem = slot
        o.prev = self.dma_last.get(slot)
        o.dval = (o.prev.dval if o.prev else 0) + 16
        self.dma_last[slot] = o
        self.all_dma.append(o)
        return self._add(o, reads, writes)


    def barrier(self):
        lasts = []
        for e in self.ENGS:
            for o in reversed(self.ops[e]):
                if (not o.is_dma) and o.fn is not None:
                    o.sig = True
                    lasts.append(o)
                    break
        dmas = list(self.dma_last.values())
        for e in self.ENGS:
            o = Op(e, None, False)
            o.deps = [d for d in lasts if d.eng != e] + dmas
            self.ops[e].append(o)

    def emit(self):
        nc = self.nc
        import contextlib
        with contextlib.ExitStack() as st:
            esem = {e: st.enter_context(nc.semaphore("S_" + e)) for e in self.ENGS}
            dsem = {}
            for e in self.ENGS:
                if self.ndma[e]:
                    for i in range(min(self.ndma_sems, self.ndma[e])):
                        dsem[(e, i)] = st.enter_context(nc.semaphore("D_%s_%d" % (e, i)))
            for e in self.ENGS:
                c = 0
                for o in self.ops[e]:
                    if (not o.is_dma) and o.sig and o.fn is not None:
                        c += 1
                        o.sigval = c
            block = st.enter_context(nc.Block())
            handles = {"pe": block.tensor, "act": block.scalar, "dve": block.vector,
                       "pool": block.gpsimd, "sp": block.sync}

            def make(e):
                def body(eng):
                    known = {}

                    def wait(sem, key, val):
                        if known.get(key, 0) < val:
                            eng.wait_ge(sem, val)
                            known[key] = val
                    for o in self.ops[e]:
                        for d in o.deps:
                            if d.is_dma:
                                wait(dsem[d.dsem], d.dsem, d.dval)
                            else:
                                wait(esem[d.eng], d.eng, d.sigval)
                        if o.is_dma:
                            if o.prev is not None:
                                wait(dsem[o.dsem], o.dsem, o.prev.dval)
                            o.fn(eng).then_inc(dsem[o.dsem], 16)
                        elif o.fn is not None:
                            ins = o.fn(eng)
                            if o.sig:
                                ins.then_inc(esem[e], 1)
                    if e == "sp":
                        for slot, o in self.dma_last.items():
                            wait(dsem[slot], slot, o.dval)
                return body
            for e in self.ENGS:
                if self.ops[e] or e == "sp":
                    handles[e](make(e))


def phase_a(nc, P, st, xin, w_in, g_mix, qkn, cs, idb, d_idb, kT_d, v_d, qT_d):
    def sb(name, shape, dt):
        return st.enter_context(nc.sbuf_tensor("a_" + name, shape, dt))

    def ps(name, shape, dt=F32):
        return st.enter_context(nc.psum_tensor("a_" + name, shape, dt))
    wq = sb("wq", [128, 8, 1536], BF16); d_wq = Dep()
    gm = sb("gm", [128, D], F32); d_gm = Dep()
    gq = sb("gq", [128, 2, 512], F32); d_gq = Dep()
    NB = 2
    NX = 3
    xt = [sb("xt%d" % i, [128, D], F32) for i in range(NX)]; d_xt = [Dep() for _ in range(NX)]
    cst = [sb("cst%d" % i, [128, 64], F32) for i in range(NX)]; d_cst = [Dep() for _ in range(NX)]
    junk = sb("junk", [128, D], BF16); d_junk = Dep()
    ssq = sb("ssq", [128, 1], F32); d_ssq = Dep()
    rstd = sb("rstd", [128, 1], F32); d_rstd = Dep()
    hb = sb("hb", [128, D], BF16); d_hb = Dep()
    hT = [sb("hT%d" % i, [128, 8, 128], BF16) for i in range(NB)]; d_hT = [Dep() for _ in range(NB)]
    pT = ps("pT", [128, 8, 128], BF16); d_pT = Dep()
    pq = [[ps("pq%d_%d" % (j, i), [128, 512], F32) for i in range(3)] for j in range(2)]; d_pq = [[Dep() for _ in range(3)] for _ in range(2)]
    pkT = ps("pkT", [128, 4, 128], BF16); d_pkT = Dep()
    sq_ = [sb("sq%d" % i, [128, 512], F32) for i in range(2)]; d_sq_ = [Dep(), Dep()]
    hs_ = [sb("hs%d" % i, [128, 8], F32) for i in range(2)]; d_hs_ = [Dep(), Dep()]
    hr_ = [sb("hr%d" % i, [128, 8], F32) for i in range(2)]; d_hr_ = [Dep(), Dep()]
    qn_ = [sb("qn%d" % i, [128, 512], F32) for i in range(2)]; d_qn_ = [Dep(), Dep()]
    t1_ = [sb("t1%d" % i, [128, 256], F32) for i in range(2)]; d_t1_ = [Dep(), Dep()]
    t2_ = [sb("t2%d" % i, [128, 256], F32) for i in range(2)]; d_t2_ = [Dep(), Dep()]
    t3_ = [sb("t3%d" % i, [128, 256], F32) for i in range(2)]; d_t3_ = [Dep(), Dep()]
    t4_ = [sb("t4%d" % i, [128, 256], F32) for i in range(2)]; d_t4_ = [Dep(), Dep()]
    qr_ = [[sb("qr%d_%d" % (i, j), [128, 512], BF16) for j in range(2)] for i in range(2)]; d_qr_ = [[Dep(), Dep()] for _ in range(2)]
    kTs = [[sb("kTs%d_%d" % (j, i), [128, 4, 128], BF16) for i in range(NB)] for j in range(2)]; d_kTs = [[Dep() for _ in range(NB)] for _ in range(2)]
    vs = [sb("vs%d" % i, [128, 512], BF16) for i in range(NB)]; d_vs = [Dep() for _ in range(NB)]

    for k in range(8):
        P.dma("pool", lambda e, k=k: e.dma_start(out=wq[:, k, :], in_=w_in[k * 128:(k + 1) * 128, 0:1536]), writes=[d_wq])
    P.dma("sp", lambda e: e.dma_start(out=gm[:], in_=g_mix[0:1, :].partition_broadcast(128)), writes=[d_gm])
    for i in range(2):
        P.dma("sp", lambda e, i=i: e.dma_start(out=gq[:, i, :], in_=qkn[i:i + 1, :].partition_broadcast(128)), writes=[d_gq])
    P.op("dve", lambda e: e.tensor_scalar(out=gq[:, 0, :], in0=gq[:, 0, :], scalar1=0.125, scalar2=None, op0=ALU.mult),
         reads=[d_gq], writes=[d_gq])

    def qk_post_steps(src_ps, d_src, which, b, xs):
        sl = which
        sq, hs, hr, qn, t1, t2, t3, t4, qr = sq_[sl], hs_[sl], hr_[sl], qn_[sl], t1_[sl], t2_[sl], t3_[sl], t4_[sl], qr_[sl][b]
        d_sq, d_hs, d_hr, d_qn, d_t1, d_t2, d_t3, d_t4, d_qr = (d_sq_[sl], d_hs_[sl], d_hr_[sl], d_qn_[sl], d_t1_[sl], d_t2_[sl],
                                                               d_t3_[sl], d_t4_[sl], d_qr_[sl][b])
        q3 = qn[:].rearrange("p (h d) -> p h d", h=8)
        q1 = q3[:, :, 0:32]
        q2 = q3[:, :, 32:64]
        cosb = cst[xs][:, 0:32].unsqueeze(1).to_broadcast([128, 8, 32])
        sinb = cst[xs][:, 32:64].unsqueeze(1).to_broadcast([128, 8, 32])
        v3 = lambda t: t[:].rearrange("p (h d) -> p h d", h=8)
        r3 = qr[:].rearrange("p (h d) -> p h d", h=8)
        steps = [
            lambda: P.op("act", lambda e: e.activation(out=sq[:], in_=src_ps[:], func=AF.Square), reads=[d_src], writes=[d_sq]),
            lambda: P.op("dve", lambda e: e.tensor_reduce(out=hs[:], in_=sq[:].rearrange("p (h d) -> p h d", h=8), axis=AX.X, op=ALU.add),
                         reads=[d_sq], writes=[d_hs]),
            lambda: P.op("act", lambda e: e.activation(out=hr[:], in_=hs[:], func=AF.Sqrt, bias=EPS, scale=1.0 / 64), reads=[d_hs], writes=[d_hr]),
            lambda: P.op("dve", lambda e: e.reciprocal(out=hr[:], in_=hr[:]), reads=[d_hr], writes=[d_hr]),
            lambda: P.op("dve", lambda e: e.tensor_tensor(out=qn[:].rearrange("p (h d) -> p h d", h=8), in0=src_ps[:].rearrange("p (h d) -> p h d", h=8),
                                                          in1=hr[:].unsqueeze(2).to_broadcast([128, 8, 64]), op=ALU.mult),
                         reads=[d_src, d_hr], writes=[d_qn]),
            lambda: P.op("pool", lambda e: e.tensor_tensor(out=qn[:], in0=qn[:], in1=gq[:, which, :], op=ALU.mult), reads=[d_qn, d_gq], writes=[d_qn]),
            lambda: P.op("dve", lambda e: e.tensor_tensor(out=v3(t1), in0=q1, in1=cosb, op=ALU.mult), reads=[d_qn, d_cst[xs]], writes=[d_t1]),
            lambda: P.op("pool", lambda e: e.tensor_tensor(out=v3(t2), in0=q2, in1=sinb, op=ALU.mult), reads=[d_qn, d_cst[xs]], writes=[d_t2]),
            lambda: P.op("dve", lambda e: e.tensor_tensor(out=v3(t3), in0=q2, in1=cosb, op=ALU.mult), reads=[d_qn, d_cst[xs]], writes=[d_t3]),
            lambda: P.op("pool", lambda e: e.tensor_tensor(out=v3(t4), in0=q1, in1=sinb, op=ALU.mult), reads=[d_qn, d_cst[xs]], writes=[d_t4]),
            lambda: P.op("dve", lambda e: e.tensor_tensor(out=r3[:, :, 0:32], in0=v3(t1), in1=v3(t2), op=ALU.subtract), reads=[d_t1, d_t2], writes=[d_qr]),
            lambda: P.op("pool", lambda e: e.tensor_tensor(out=r3[:, :, 32:64], in0=v3(t3), in1=v3(t4), op=ALU.add), reads=[d_t3, d_t4], writes=[d_qr]),
        ]
        return steps

    def to_T_and_store(dst_dram, col0, b, sl):
        for c in range(4):
            P.op("pe", lambda e, c=c: e.transpose(out=pkT[:, c, :], in_=qr_[sl][b][:, c * 128:(c + 1) * 128], identity=idb[:]),
                 reads=[d_qr_[sl][b], d_idb], writes=[d_pkT])
        P.op("act", lambda e: e.copy(out=kTs[sl][b][:], in_=pkT[:]), reads=[d_pkT], writes=[d_kTs[sl][b]])
        P.dma("sp", lambda e: e.dma_start(out=dst_dram[:, col0:col0 + 128].rearrange("(c p) t -> p c t", p=128), in_=kTs[sl][b][:]),
              reads=[d_kTs[sl][b]])

    def load(ti):
        xs = ti % NX
        r0 = ti * 128
        P.dma("sp", lambda e: e.dma_start(out=xt[xs][:], in_=xin[r0:r0 + 128, :]), writes=[d_xt[xs]])
        P.dma("sp", lambda e: e.dma_start(out=cst[xs][:], in_=cs[r0:r0 + 128, :]), writes=[d_cst[xs]])

    def front(ti):
        b = ti % NB
        xs = ti % NX
        own = ti >= 32
        P.op("act", lambda e: e.activation(out=junk[:], in_=xt[xs][:], func=AF.Square, accum_out=ssq[:]), reads=[d_xt[xs]], writes=[d_junk, d_ssq])
        P.op("act", lambda e: e.activation(out=rstd[:], in_=ssq[:], func=AF.Sqrt, bias=EPS, scale=1.0 / D), reads=[d_ssq], writes=[d_rstd])
        P.op("dve", lambda e: e.reciprocal(out=rstd[:], in_=rstd[:]), reads=[d_rstd], writes=[d_rstd])
        P.op("dve", lambda e: e.scalar_tensor_tensor(out=hb[:], in0=xt[xs][:], scalar=rstd[:, 0:1], in1=gm[:], op0=ALU.mult, op1=ALU.mult),
             reads=[d_xt[xs], d_rstd, d_gm], writes=[d_hb])
        for k in range(8):
            P.op("pe", lambda e, k=k: e.transpose(out=pT[:, k, :], in_=hb[:, k * 128:(k + 1) * 128], identity=idb[:]), reads=[d_hb, d_idb], writes=[d_pT])
        P.op("act", lambda e: e.copy(out=hT[b][:], in_=pT[:]), reads=[d_pT], writes=[d_hT[b]])
        groups = [0, 1, 2] if own else [1, 2]
        for g in groups:
            for k in range(8):
                P.op("pe", lambda e, g=g, k=k: e.matmul(pq[b][g][:], lhsT=hT[b][:, k, :], rhs=wq[:, k, g * 512:(g + 1) * 512], start=(k == 0), stop=(k == 7)),
                     reads=[d_hT[b], d_wq], writes=[d_pq[b][g]])

    def post(ti):
        b = ti % NB
        xs = ti % NX
        own = ti >= 32
        ks = qk_post_steps(pq[b][1], d_pq[b][1], 1, b, xs)
        qs = qk_post_steps(pq[b][0], d_pq[b][0], 0, b, xs) if own else []
        for i in range(len(ks)):
            ks[i]()
            if qs:
                qs[i]()
        P.op("act", lambda e: e.copy(out=vs[b][:], in_=pq[b][2][:]), reads=[d_pq[b][2]], writes=[d_vs[b]])

    def tail(ti):
        b = ti % NB
        own = ti >= 32
        r0 = ti * 128
        to_T_and_store(kT_d, r0, b, 1)
        if own:
            to_T_and_store(qT_d, r0 - TP, b, 0)
        P.dma("sp", lambda e: e.dma_start(out=v_d[r0:r0 + 128, :], in_=vs[b][:]), reads=[d_vs[b]])

    load(0)
    load(1)
    front(0)
    for ti in range(64):
        if ti + 2 < 64:
            load(ti + 2)
        if ti + 1 < 64:
            front(ti + 1)
        if ti >= 1:
            tail(ti - 1)
        post(ti)
    tail(63)


def phase_b(nc, P, st, kT_d, v_d, qT_d, e_d, vq_d, nb_d, oq_d, tri_d, idb, d_idb, oa, d_oa, heads=range(8), nblk=16):
    def sb(name, shape, dt):
        return st.enter_context(nc.sbuf_tensor("b_" + name, shape, dt))

    def ps(name, shape, dt=F32):
        return st.enter_context(nc.psum_tensor(name, shape, dt))
    NB = 2
    kaug = [sb("kaug%d" % i, [96, NK], BF16) for i in range(NB)]; d_kaug = [Dep() for _ in range(NB)]
    vaug = [sb("vaug%d" % i, [128, 64, 65], BF16) for i in range(NB)]; d_vaug = [Dep() for _ in range(NB)]
    qaug = [sb("qaug%d" % i, [96, T], BF16) for i in range(NB)]; d_qaug = [Dep() for _ in range(NB)]
    d_qmb = [Dep() for _ in range(NB)]
    vq = sb("vq_s", [128, 1024], F32); d_c = Dep()
    nb = sb("nb_s", [128, 1024], F32)
    oq = sb("oq_s", [128, 1024], F32)
    tri = sb("tri_s", [128, 128], BF16)
    ksum = sb("ksum", [64, 32], F32); d_ksum = Dep()
    kmean = sb("kmean", [64, 32], BF16); d_kmean = Dep()
    g1 = sb("g1", [128, 512], F32); d_g1 = Dep()
    top8 = sb("top8", [128, 16, 8], F32); d_top8 = Dep()
    sel = sb("sel", [128, 512], F32); d_sel = Dep()
    mbp = sb("mbp", [128, 16, 96], BF16); d_mbp = Dep()
    NPT = 3
    pt = [sb("pt%d" % i, [128, 512], BF16) for i in range(NPT)]; d_pt = [Dep() for _ in range(NPT)]
    rc = sb("rc", [128, 2], F32); d_rc = [Dep(), Dep()]
    pg = ps("pg", [128, 512], F32); d_pg = Dep()
    pmT = ps("pmT", [96, 8, 128], BF16); d_pmT = Dep()
    psS = [ps("psS%d" % i, [128, 512], F32) for i in range(NPT)]; d_psS = [Dep() for _ in range(NPT)]
    po = [ps("po%d" % i, [128, 65], F32) for i in range(2)]; d_po = [Dep() for _ in range(2)]

    for i in range(NB):
        P.dma("sp", lambda e, i=i: e.dma_start(out=kaug[i][64:96, :], in_=e_d[:, :]), writes=[d_kaug[i]])
        P.op("pool", lambda e, i=i: e.memset(vaug[i][:], 1.0), writes=[d_vaug[i]])
    P.dma("sp", lambda e: e.dma_start(out=vq[:], in_=vq_d[0:1, :].partition_broadcast(128)), writes=[d_c])
    P.dma("sp", lambda e: e.dma_start(out=nb[:], in_=nb_d[0:1, :].partition_broadcast(128)), writes=[d_c])
    P.dma("sp", lambda e: e.dma_start(out=oq[:], in_=oq_d[0:1, :].partition_broadcast(128)), writes=[d_c])
    P.dma("pool", lambda e: e.dma_start(out=tri[:], in_=tri_d[:, :]), writes=[d_c])
    P.op("pool", lambda e: e.memset(mbp[:], 0.0), writes=[d_mbp])

    for ih, h in enumerate(heads):
        b = ih % NB
        P.dma("sp", lambda e, b=b, h=h: e.dma_start(out=kaug[b][0:64, :], in_=kT_d[h * 64:(h + 1) * 64, :]), writes=[d_kaug[b]])
        P.dma("sp", lambda e, b=b, h=h: e.dma_start(out=vaug[b][:, :, 0:64],
                                                    in_=v_d[:, h * 64:(h + 1) * 64].rearrange("(t p) d -> p t d", p=128)),
              writes=[d_vaug[b]])
        P.dma("sp", lambda e, b=b, h=h: e.dma_start(out=qaug[b][0:64, :], in_=qT_d[h * 64:(h + 1) * 64, :]), writes=[d_qaug[b]])
        P.op("dve", lambda e, b=b: e.tensor_reduce(out=ksum[:], in_=kaug[b][0:64, :].rearrange("p (b k) -> p b k", k=256),
                                                   axis=AX.X, op=ALU.add), reads=[d_kaug[b]], writes=[d_ksum])
        P.op("dve", lambda e: e.tensor_scalar(out=kmean[:], in0=ksum[:], scalar1=1.0 / 256, scalar2=None, op0=ALU.mult),
             reads=[d_ksum], writes=[d_kmean])
        for half in range(2):
            for j in range(16):
                qt = half * 16 + j
                P.op("pe", lambda e, b=b, j=j, qt=qt: e.matmul(pg[:, j * 32:(j + 1) * 32], lhsT=qaug[b][0:64, qt * 128:(qt + 1) * 128],
                                                              rhs=kmean[:, :], start=True, stop=True),
                     reads=[d_qaug[b], d_kmean], writes=[d_pg])
            cs_ = slice(half * 512, (half + 1) * 512)
            P.op("dve", lambda e, cs_=cs_: e.tensor_tensor(out=g1[:], in0=pg[:], in1=vq[:, cs_], op=ALU.mult),
                 reads=[d_pg, d_c], writes=[d_g1])
            P.op("dve", lambda e, cs_=cs_: e.tensor_tensor(out=g1[:], in0=g1[:], in1=nb[:, cs_], op=ALU.add),
                 reads=[d_g1, d_c], writes=[d_g1])
            for j in range(16):
                P.op("dve", lambda e, j=j: e.max(out=top8[:, j, :], in_=g1[:, j * 32:(j + 1) * 32]), reads=[d_g1], writes=[d_top8])
            P.op("dve", lambda e: e.tensor_tensor(out=sel[:].rearrange("p (j b) -> p j b", b=32),
                                                  in0=g1[:].rearrange("p (j b) -> p j b", b=32),
                                                  in1=top8[:, :, 2:3].to_broadcast([128, 16, 32]), op=ALU.is_ge),
                 reads=[d_g1, d_top8], writes=[d_sel])
            P.op("dve", lambda e, cs_=cs_: e.tensor_tensor(out=sel[:], in0=sel[:], in1=vq[:, cs_], op=ALU.mult),
                 reads=[d_sel, d_c], writes=[d_sel])
            P.op("dve", lambda e, cs_=cs_: e.tensor_tensor(out=sel[:], in0=sel[:], in1=oq[:, cs_], op=ALU.add),
                 reads=[d_sel, d_c], writes=[d_sel])
            P.op("dve", lambda e: e.tensor_scalar(out=mbp[:, :, 64:96], in0=sel[:].rearrange("p (j b) -> p j b", b=32),
                                                  scalar1=1.0, scalar2=MB, op0=ALU.subtract, op1=ALU.mult),
                 reads=[d_sel], writes=[d_mbp])
            for grp in range(2):
                for j in range(8):
                    P.op("pe", lambda e, grp=grp, j=j: e.transpose(out=pmT[:, j, :], in_=mbp[:, grp * 8 + j, :], identity=idb[:]),
                         reads=[d_mbp, d_idb], writes=[d_pmT])
                c0 = (half * 16 + grp * 8) * 128
                P.op("act", lambda e, b=b, c0=c0: e.copy(out=qaug[b][64:96, c0:c0 + 1024], in_=pmT[64:96, :, :]),
                     reads=[d_pmT], writes=[d_qmb[b]])
        units = []
        for jb in range(nblk):
            ncommon = 32 + 2 * jb
            for u in range(ncommon // 2):
                units.append((jb, [2 * u, 2 * u + 1], False, u == 0))
            units.append((jb, [32 + 2 * jb, 32 + 2 * jb + 1], True, False))

        def emit_S(u, s, b=b):
            jb, kts, diag, first = u
            for j, kt in enumerate(kts):
                if diag and j == 1:
                    q0, nq, c0 = jb * 256 + 128, 128, 256
                else:
                    q0, nq, c0 = jb * 256, 256, j * 256
                P.op("pe", lambda e, s=s, kt=kt, q0=q0, nq=nq, c0=c0: e.matmul(psS[s][:, c0:c0 + nq], lhsT=kaug[b][0:96, kt * 128:(kt + 1) * 128],
                                                                             rhs=qaug[b][0:96, q0:q0 + nq], start=True, stop=True),
                     reads=[d_kaug[b], d_qaug[b], d_qmb[b]], writes=[d_psS[s]])

        def emit_rest(u, s, b=b, h=h):
            jb, kts, diag, first = u
            ncols = 384 if diag else 512
            P.op("act", lambda e, s=s, ncols=ncols: e.activation(out=pt[s][:, 0:ncols], in_=psS[s][:, 0:ncols], func=AF.Exp),
                 reads=[d_psS[s]], writes=[d_pt[s]])
            if diag:
                P.op("pool", lambda e, s=s: e.tensor_tensor(out=pt[s][:, 0:128], in0=pt[s][:, 0:128], in1=tri[:], op=ALU.mult),
                     reads=[d_pt[s], d_c], writes=[d_pt[s]])
                P.op("pool", lambda e, s=s: e.tensor_tensor(out=pt[s][:, 256:384], in0=pt[s][:, 256:384], in1=tri[:], op=ALU.mult),
                     reads=[d_pt[s], d_c], writes=[d_pt[s]])
            for j, kt in enumerate(kts):
                if diag and j == 1:
                    pv = [(1, 256, True)]
                elif diag:
                    pv = [(0, 0, True), (1, 128, False)]
                else:
                    pv = [(0, j * 256, False), (1, j * 256 + 128, False)]
                st_ = first and j == 0
                for qi, col0, last in pv:
                    P.op("pe", lambda e, s=s, kt=kt, qi=qi, col0=col0, st_=st_, last=last: e.matmul(
                        po[qi][:, :], lhsT=pt[s][:, col0:col0 + 128], rhs=vaug[b][:, kt, :], start=st_, stop=last),
                        reads=[d_pt[s], d_vaug[b]], writes=[d_po[qi]])
            if diag:
                for qi in range(2):
                    qt = jb * 2 + qi
                    P.op("dve", lambda e, qi=qi: e.reciprocal(out=rc[:, qi:qi + 1], in_=po[qi][:, 64:65]), reads=[d_po[qi]], writes=[d_rc[qi]])
                    P.op("dve", lambda e, qi=qi, qt=qt: e.tensor_scalar(out=oa[:, qt, h * 64:(h + 1) * 64], in0=po[qi][:, 0:64],
                                                                       scalar1=rc[:, qi:qi + 1], scalar2=None, op0=ALU.mult),
                         reads=[d_po[qi], d_rc[qi]], writes=[d_oa])
        SK = 2
        n = len(units)
        for i in range(n + SK):
            if i < n:
                emit_S(units[i], i % NPT)
            if i >= SK:
                emit_rest(units[i - SK], (i - SK) % NPT)


def host_consts(half):
    pv = 1.0 if half == 1 else 0.0
    V = np.zeros((32, 32), np.float32); O = np.zeros((32, 32), np.float32)
    for qt in range(32):
        jb = qt // 2
        V[qt, :16] = pv
        V[qt, 16:16 + jb] = 1.0
        O[qt, 16 + jb] = 1.0
    NBm = (V - 1.0) * BIGG
    E = np.zeros((32, NK), np.float32)
    for b in range(32):
        E[b, b * 256:(b + 1) * 256] = 1.0
    tri = (np.arange(128)[:, None] <= np.arange(128)[None, :]).astype(np.float32)
    return dict(vq=V.reshape(1, 1024), nb=NBm.reshape(1, 1024), oq=O.reshape(1, 1024),
                e_d=E.astype(ml_dtypes.bfloat16), tri=tri.astype(ml_dtypes.bfloat16))


def phase_c(nc, P, st, xin, w_in, g_mix, cwl_d, cbl_d, dtb_d, alog_d, dsk_d, ssdn_d, tri2_d, sel_d, tb_d, pflag_d,
            idb, d_idb, os_d, d_osd, chunks=range(32)):
    def sb(name, shape, dt):
        return st.enter_context(nc.sbuf_tensor("c_" + name, shape, dt))

    def ps(name, shape, dt=F32):
        return st.enter_context(nc.psum_tensor(name, shape, dt))
    wz = sb("wz", [128, 8, 1024], BF16); d_w = Dep()
    wx = sb("wx", [128, 8, 2048], BF16)
    wdt = sb("wdt", [128, 8, 16], BF16)
    gm = sb("gmc", [128, D], F32); d_c = Dep()
    cw = sb("cw", [128, 16, 4], F32)
    cb = sb("cb", [128, 16], F32)
    dtb = sb("dtb", [128, 2, 16], F32)
    Aneg = sb("Aneg", [128, 2, 16], F32); d_A = Dep()
    dsk = sb("dsk", [128, 16], F32)
    ssdn = sb("ssdn", [128, D], F32)
    tri2 = sb("tri2", [128, 2, 256], F32)
    onesf = sb("onesf", [128, 128], F32)
    sel = sb("selc", [16, 16, 128], F32)
    tb = sb("tbc", [128, 384], F32)
    pflag = sb("pflag", [128, 1], F32)
    xt = sb("xtc", [128, 2, D], F32); d_xt = Dep()
    junk = sb("junkc", [128, D], BF16); d_junk = Dep()
    ssq = sb("ssqc", [128, 2], F32); d_ssq = Dep()
    rstd = sb("rstdc", [128, 2], F32); d_rstd = Dep()
    hb = sb("hbc", [128, 2, D], BF16); d_hb = Dep()
    hT = sb("hTc", [128, 8, 256], BF16); d_hT = Dep()
    xraw = sb("xraw", [128, 16, 259], F32); d_xraw = Dep(); d_halo = Dep()
    acc = sb("acc", [128, 8, 256], F32); d_acc = [Dep() for _ in range(8)]
    xc = sb("xc", [128, 16, 256], BF16); d_xc = [Dep() for _ in range(16)]
    xtok = sb("xtok", [128, 2, 1024], BF16); d_xtok = Dep()
    btok = sb("btok", [128, 2, 512], BF16); d_btok = Dep()
    dtr = sb("dtr", [128, 2, 16], F32); d_dtr = Dep()
    dt = sb("dt", [128, 2, 16], F32); d_dt = Dep()
    aa = sb("aa", [128, 2, 16], F32); d_aa = Dep()
    acum = sb("acum", [128, 2, 16], F32); d_acum = Dep()
    nacum = sb("nacum", [128, 2, 16], F32); d_nacum = Dep()
    acumT = sb("acumT", [16, 256], F32); d_acumT = Dep()
    tot = sb("tot", [128, 16], F32); d_tot = Dep()
    cdec = sb("cdec", [128, 16], F32); d_cdec = Dep()
    dte = sb("dte", [128, 2, 16], F32); d_dte = Dep()
    eac = sb("eac", [128, 2, 16], F32); d_eac = Dep()
    w2 = sb("w2", [128, 2, 16], F32); d_w2 = Dep()
    xdt = sb("xdt", [128, 2, 1024], BF16); d_xdt = Dep()
    xdd = sb("xdd", [128, 2, 1024], BF16); d_xdd = Dep()
    zs = sb("zs", [128, 2, 1024], BF16); d_zs = Dep()
    cbT = sb("cbT", [128, 4, 384], F32); d_cbT = [Dep() for _ in range(4)]
    arg = [sb("arg%d" % i, [128, 384], F32) for i in range(2)]; d_arg = [Dep(), Dep()]
    Lt = [sb("Lt%d" % i, [128, 384], F32) for i in range(2)]; d_Lt = [Dep(), Dep()]
    Mt = [sb("Mt%d" % i, [128, 384], BF16) for i in range(2)]; d_Mt = [Dep() for _ in range(2)]
    stf = sb("stf", [128, 4, 256], F32); d_stf = Dep()
    stT = sb("stT", [128, 4, 256], BF16); d_stT = Dep()
    yt = [sb("yt%d" % i, [128, 256], F32) for i in range(2)]; d_yt = [Dep(), Dep()]
    y2 = [sb("y2%d" % i, [128, 256], F32) for i in range(2)]; d_y2 = [Dep(), Dep()]
    gss = [sb("gss%d" % i, [128, 1], F32) for i in range(2)]; d_gss = [Dep(), Dep()]
    grs = [sb("grs%d" % i, [128, 1], F32) for i in range(2)]; d_grs = [Dep(), Dep()]
    osb = sb("osb", [128, 2, 1024], BF16); d_osb = Dep()

    B = [ps("bk%d" % i, [128, 512], F32) for i in range(7)]
    d_B = [Dep() for _ in range(7)]
    d_B1h = [Dep(), Dep()]
    d_B5h = [Dep(), Dep()]
    pT = ps("pTc", [128, 8, 128], BF16); d_pT = Dep()

    for k in range(8):
        rows = slice(k * 128, (k + 1) * 128)
        P.dma("pool", lambda e, k=k, rows=rows: e.dma_start(out=wz[:, k, :], in_=w_in[rows, C_Z:C_Z + 1024]), writes=[d_w])
        P.dma("pool", lambda e, k=k, rows=rows: e.dma_start(out=wx[:, k, :], in_=w_in[rows, C_X:C_X + 2048]), writes=[d_w])
        P.dma("pool", lambda e, k=k, rows=rows: e.dma_start(out=wdt[:, k, :], in_=w_in[rows, C_DT:C_DT + 16]), writes=[d_w])
    P.dma("sp", lambda e: e.dma_start(out=gm[:], in_=g_mix[0:1, :].partition_broadcast(128)), writes=[d_c])
    P.dma("sp", lambda e: e.dma_start(out=cw[:], in_=cwl_d[:, :, :]), writes=[d_c])
    P.dma("sp", lambda e: e.dma_start(out=cb[:], in_=cbl_d[:, :]), writes=[d_c])
    for i in range(2):
        P.dma("sp", lambda e, i=i: e.dma_start(out=dtb[:, i, :], in_=dtb_d[0:1, :].partition_broadcast(128)), writes=[d_c])
        P.dma("sp", lambda e, i=i: e.dma_start(out=Aneg[:, i, :], in_=alog_d[0:1, :].partition_broadcast(128)), writes=[d_A])
    P.dma("sp", lambda e: e.dma_start(out=dsk[:], in_=dsk_d[0:1, :].partition_broadcast(128)), writes=[d_c])
    P.dma("sp", lambda e: e.dma_start(out=ssdn[:], in_=ssdn_d[0:1, :].partition_broadcast(128)), writes=[d_c])
    P.dma("sp", lambda e: e.dma_start(out=tri2[:], in_=tri2_d[:, :, :]), writes=[d_c])
    P.dma("sp", lambda e: e.dma_start(out=sel[:], in_=sel_d[:, :, :]), writes=[d_c])
    P.dma("sp", lambda e: e.dma_start(out=tb[:], in_=tb_d[:, :]), writes=[d_c])
    P.dma("sp", lambda e: e.dma_start(out=pflag[:], in_=pflag_d[0:1, :].partition_broadcast(128)), writes=[d_c])
    P.op("pool", lambda e: e.memset(onesf[:], 1.0), writes=[d_c])
    P.op("act", lambda e: e.activation(out=Aneg[:], in_=Aneg[:], func=AF.Exp), reads=[d_A], writes=[d_A])
    P.op("dve", lambda e: e.tensor_scalar(out=Aneg[:], in0=Aneg[:], scalar1=-1.0, scalar2=None, op0=ALU.mult), reads=[d_A], writes=[d_A])
    P.op("pool", lambda e: e.memset(xraw[:], 0.0), writes=[d_xraw, d_halo])
    P.op("pool", lambda e: e.memset(stf[:], 0.0), writes=[d_stf])
    P.op("pool", lambda e: e.memset(stT[:], 0.0), writes=[d_stT])

    h3 = lambda ap: ap.rearrange("p (h d) -> p h d", d=64)

    for c in chunks:
        own = c >= 16
        r0 = c * 256
        P.dma("sp", lambda e, r0=r0: e.dma_start(out=xt[:], in_=xin[r0:r0 + 256, :].rearrange("(t p) d -> p t d", p=128)), writes=[d_xt])
        for it in range(2):
            P.op("act", lambda e, it=it: e.activation(out=junk[:], in_=xt[:, it, :], func=AF.Square, accum_out=ssq[:, it:it + 1]),
                 reads=[d_xt], writes=[d_junk, d_ssq])
        P.op("act", lambda e: e.activation(out=rstd[:], in_=ssq[:], func=AF.Sqrt, bias=EPS, scale=1.0 / D), reads=[d_ssq], writes=[d_rstd])
        P.op("dve", lambda e: e.reciprocal(out=rstd[:], in_=rstd[:]), reads=[d_rstd], writes=[d_rstd])
        for it in range(2):
            P.op("dve", lambda e, it=it: e.scalar_tensor_tensor(out=hb[:, it, :], in0=xt[:, it, :], scalar=rstd[:, it:it + 1], in1=gm[:],
                                                                                    op0=ALU.mult, op1=ALU.mult),
                 reads=[d_xt, d_rstd, d_c], writes=[d_hb])
        for it in range(2):
            for k in range(8):
                P.op("pe", lambda e, it=it, k=k: e.transpose(out=pT[:, k, :], in_=hb[:, it, k * 128:(k + 1) * 128], identity=idb[:]),
                     reads=[d_hb, d_idb], writes=[d_pT])
            P.op("act", lambda e, it=it: e.copy(out=hT[:, :, it * 128:(it + 1) * 128], in_=pT[:]), reads=[d_pT], writes=[d_hT])
        for it in range(2):
            for k in range(8):
                P.op("pe", lambda e, it=it, k=k: e.matmul(B[3][:, it * 16:(it + 1) * 16], lhsT=hT[:, k, it * 128:(it + 1) * 128], rhs=wdt[:, k, :],
                                                         start=(k == 0), stop=(k == 7)), reads=[d_hT, d_w], writes=[d_B[3]])
        P.op("dve", lambda e: e.tensor_tensor(out=dtr[:].rearrange("p a b -> p (a b)"), in0=B[3][:, 0:32], in1=dtb[:].rearrange("p a b -> p (a b)"), op=ALU.add),
             reads=[d_B[3], d_c], writes=[d_dtr])
        P.op("act", lambda e: e.activation(out=dtr[:], in_=dtr[:], func=AF.Exp), reads=[d_dtr], writes=[d_dtr])
        P.op("act", lambda e: e.activation(out=dt[:], in_=dtr[:], func=AF.Ln, bias=1.0), reads=[d_dtr], writes=[d_dt])
        P.op("dve", lambda e: e.tensor_tensor(out=aa[:], in0=dt[:], in1=Aneg[:], op=ALU.mult), reads=[d_dt, d_A], writes=[d_aa])
        for it in range(2):
            for jt in range(it + 1):
                P.op("pe", lambda e, it=it, jt=jt: e.matmul(B[3][:, 32 + it * 16:32 + (it + 1) * 16], lhsT=tri2[:, jt, it * 128:(it + 1) * 128],
                                                           rhs=aa[:, jt, :], start=(jt == 0), stop=(jt == it)),
                     reads=[d_aa, d_c], writes=[d_B[3]])
        for jt in range(2):
            P.op("pe", lambda e, jt=jt: e.matmul(B[3][:, 64:80], lhsT=onesf[:], rhs=aa[:, jt, :], start=(jt == 0), stop=(jt == 1)),
                 reads=[d_aa, d_c], writes=[d_B[3]])
        for jt in range(2):
            P.op("pe", lambda e, jt=jt: e.matmul(B[3][0:16, 128:384], lhsT=aa[:, jt, :], rhs=tri2[:, jt, :], start=(jt == 0), stop=(jt == 1)),
                 reads=[d_aa, d_c], writes=[d_B[3]])
        P.op("act", lambda e: e.copy(out=acum[:].rearrange("p a b -> p (a b)"), in_=B[3][:, 32:64]), reads=[d_B[3]], writes=[d_acum])
        P.op("dve", lambda e: e.tensor_scalar(out=nacum[:].rearrange("p a b -> p (a b)"), in0=B[3][:, 32:64], scalar1=-1.0, scalar2=None, op0=ALU.mult),
             reads=[d_B[3]], writes=[d_nacum])
        P.op("act", lambda e: e.copy(out=tot[:], in_=B[3][:, 64:80]), reads=[d_B[3]], writes=[d_tot])
        P.op("act", lambda e: e.copy(out=acumT[:], in_=B[3][0:16, 128:384]), reads=[d_B[3]], writes=[d_acumT])
        P.op("act", lambda e: e.activation(out=cdec[:], in_=tot[:], func=AF.Exp), reads=[d_tot], writes=[d_cdec])
        P.op("dve", lambda e: e.tensor_tensor(out=dte[:], in0=nacum[:], in1=tot[:].unsqueeze(1).to_broadcast([128, 2, 16]), op=ALU.add),
             reads=[d_nacum, d_tot], writes=[d_dte])
        P.op("act", lambda e: e.activation(out=dte[:], in_=dte[:], func=AF.Exp), reads=[d_dte], writes=[d_dte])
        P.op("act", lambda e: e.activation(out=eac[:], in_=acum[:], func=AF.Exp), reads=[d_acum], writes=[d_eac])
        P.op("dve", lambda e: e.tensor_tensor(out=w2[:], in0=dt[:], in1=dte[:], op=ALU.mult), reads=[d_dt, d_dte], writes=[d_w2])
        nfc = 16 if (own or c == 15) else 12
        for fc in range(nfc):
            bi = 1 if fc % 2 == 0 else 4
            for k in range(8):
                P.op("pe", lambda e, fc=fc, k=k, bi=bi: e.matmul(B[bi][:, 0:256], lhsT=wx[:, k, fc * 128:(fc + 1) * 128], rhs=hT[:, k, :],
                                                               start=(k == 0), stop=(k == 7)), reads=[d_hT, d_w], writes=[d_B[bi]])
            P.op("act", lambda e, fc=fc, bi=bi: e.copy(out=xraw[:, fc, 3:259], in_=B[bi][:, 0:256]),
                 reads=[d_B[bi]], writes=[d_xraw])
        if own:
            for it in range(2):
                for hv in range(2):
                    zb = 2 if hv == 0 else 6
                    for k in range(8):
                        P.op("pe", lambda e, it=it, hv=hv, k=k, zb=zb: e.matmul(B[zb][:, :], lhsT=hT[:, k, it * 128:(it + 1) * 128], rhs=wz[:, k, hv * 512:(hv + 1) * 512],
                                                                              start=(k == 0), stop=(k == 7)), reads=[d_hT, d_w], writes=[d_B[zb]])
                    P.op("act", lambda e, it=it, hv=hv, zb=zb: e.activation(out=zs[:, it, hv * 512:(hv + 1) * 512], in_=B[zb][:, :], func=AF.Silu),
                         reads=[d_B[zb]], writes=[d_zs])
        for half in range(2):
            n8 = 8 if (half == 0 or nfc == 16) else 4
            for f8 in range(n8):
                fc = half * 8 + f8
                P.op("dve", lambda e, fc=fc, f8=f8: e.tensor_scalar(out=acc[:, f8, :], in0=xraw[:, fc, 0:256], scalar1=cw[:, fc, 0:1], scalar2=cb[:, fc:fc + 1],
                                                                   op0=ALU.mult, op1=ALU.add), reads=[d_xraw, d_halo, d_c], writes=[d_acc[f8]])
            for kk in range(1, 4):
                for f8 in range(n8):
                    fc = half * 8 + f8
                    P.op("dve", lambda e, fc=fc, f8=f8, kk=kk: e.scalar_tensor_tensor(out=acc[:, f8, :], in0=xraw[:, fc, kk:kk + 256], scalar=cw[:, fc, kk:kk + 1],
                                                                                     in1=acc[:, f8, :], op0=ALU.mult, op1=ALU.add),
                         reads=[d_xraw, d_halo, d_c, d_acc[f8]], writes=[d_acc[f8]])
            for f8 in range(n8):
                fc = half * 8 + f8
                P.op("act", lambda e, fc=fc, f8=f8: e.activation(out=xc[:, fc, :], in_=acc[:, f8, :], func=AF.Silu),
                     reads=[d_acc[f8]], writes=[d_xc[fc]])
        P.op("pool", lambda e: e.tensor_copy(out=xraw[:, :, 0:3], in_=xraw[:, :, 256:259]), reads=[d_xraw], writes=[d_halo])
        for it in range(2):
            for fc in range(8):
                P.op("pe", lambda e, it=it, fc=fc: e.transpose(out=pT[:, fc, :], in_=xc[:, fc, it * 128:(it + 1) * 128], identity=idb[:]),
                     reads=[d_xc[fc], d_idb], writes=[d_pT])
            P.op("act", lambda e, it=it: e.copy(out=xtok[:, it, :], in_=pT[:]), reads=[d_pT], writes=[d_xtok])
            for g in range(4):
                P.op("pe", lambda e, it=it, g=g: e.transpose(out=pT[:, g, :], in_=xc[:, 8 + g, it * 128:(it + 1) * 128], identity=idb[:]),
                     reads=[d_xc[8 + g], d_idb], writes=[d_pT])
            P.op("act", lambda e, it=it: e.copy(out=btok[:, it, :], in_=pT[:, 0:4, :]), reads=[d_pT], writes=[d_btok])
        for it in range(2):
            if own:
                P.op("dve", lambda e, it=it: e.tensor_tensor(out=h3(xdt[:, it, :]), in0=h3(xtok[:, it, :]),
                                                             in1=dt[:, it, :].unsqueeze(2).to_broadcast([128, 16, 64]), op=ALU.mult),
                     reads=[d_xtok, d_dt], writes=[d_xdt])
            P.op("pool", lambda e, it=it: e.tensor_tensor(out=h3(xdd[:, it, :]), in0=h3(xtok[:, it, :]),
                                                          in1=w2[:, it, :].unsqueeze(2).to_broadcast([128, 16, 64]), op=ALU.mult),
                 reads=[d_xtok, d_w2], writes=[d_xdd])
        if own:
            for g in range(4):
                P.op("pe", lambda e, g=g: e.matmul(B[4][:, 0:256], lhsT=xc[:, 8 + g, 0:128], rhs=xc[:, 12 + g, :], start=True, stop=True),
                     reads=[d_xc[8 + g], d_xc[12 + g]], writes=[d_B[4]])
                P.op("pe", lambda e, g=g: e.matmul(B[4][:, 256:384], lhsT=xc[:, 8 + g, 128:256], rhs=xc[:, 12 + g, 128:256], start=True, stop=True),
                     reads=[d_xc[8 + g], d_xc[12 + g]], writes=[d_B[4]])
                P.op("act", lambda e, g=g: e.copy(out=cbT[:, g, :], in_=B[4][:, 0:384]), reads=[d_B[4]], writes=[d_cbT[g]])
            for g in range(4):
                def stA(h, g=g):
                    mi = h % 2
                    bk = 5 if mi == 0 else 4
                    P.op("pe", lambda e: e.matmul(B[bk][:, 0:256], lhsT=sel[:, h, :], rhs=acumT[:, :], start=True, stop=True),
                         reads=[d_c, d_acumT], writes=[d_B[bk]])
                    P.op("dve", lambda e: e.scalar_tensor_tensor(out=arg[mi][:, 0:256], in0=B[bk][:, 0:256], scalar=nacum[:, 0, h:h + 1], in1=tb[:, 0:256],
                                                                 op0=ALU.add, op1=ALU.add), reads=[d_B[bk], d_nacum, d_c], writes=[d_arg[mi]])
                    P.op("dve", lambda e: e.scalar_tensor_tensor(out=arg[mi][:, 256:384], in0=B[bk][:, 128:256], scalar=nacum[:, 1, h:h + 1], in1=tb[:, 256:384],
                                                                 op0=ALU.add, op1=ALU.add), reads=[d_B[bk], d_nacum, d_c], writes=[d_arg[mi]])
                    P.op("act", lambda e: e.activation(out=Lt[mi][:], in_=arg[mi][:], func=AF.Exp), reads=[d_arg[mi]], writes=[d_Lt[mi]])
                    P.op("pool", lambda e: e.tensor_tensor(out=Mt[mi][:], in0=Lt[mi][:], in1=cbT[:, g, :], op=ALU.mult),
                         reads=[d_Lt[mi], d_cbT[g]], writes=[d_Mt[mi]])

                def stB(h, g=g):
                    mi = h % 2
                    r = h % 4
                    cs_ = slice(r * 64, (r + 1) * 64)
                    hs_ = slice(h * 64, (h + 1) * 64)
                    P.op("pe", lambda e: e.matmul(B[6][:, 0:256][:, cs_], lhsT=Mt[mi][:, 0:128], rhs=xdt[:, 0, hs_], start=True, stop=True),
                         reads=[d_Mt[mi], d_xdt], writes=[d_B[6]])
                    P.op("pe", lambda e: e.matmul(B[6][:, 256:512][:, cs_], lhsT=Mt[mi][:, 128:256], rhs=xdt[:, 0, hs_], start=True, stop=False),
                         reads=[d_Mt[mi], d_xdt], writes=[d_B[6]])
                    P.op("pe", lambda e: e.matmul(B[6][:, 256:512][:, cs_], lhsT=Mt[mi][:, 256:384], rhs=xdt[:, 1, hs_], start=False, stop=True),
                         reads=[d_Mt[mi], d_xdt], writes=[d_B[6]])
                hs4 = [g * 4 + r for r in range(4)]
                stA(hs4[0]); stA(hs4[1]); stB(hs4[0]); stA(hs4[2]); stB(hs4[1]); stA(hs4[3]); stB(hs4[2]); stB(hs4[3])
                gs_ = slice(g * 256, (g + 1) * 256)
                for it in range(2):
                    P.op("pe", lambda e, g=g, it=it: e.matmul(B[0][:, it * 256:(it + 1) * 256], lhsT=xc[:, 12 + g, it * 128:(it + 1) * 128], rhs=stT[:, g, :],
                                                             start=True, stop=True), reads=[d_xc[12 + g], d_stT], writes=[d_B[0]])
                v4 = lambda ap: ap.rearrange("p (r d) -> p r d", d=64)

                def ystep(step, it, g=g, gs_=gs_):
                    if step == 0:
                        P.op("dve", lambda e: e.tensor_tensor(out=v4(yt[it][:]), in0=v4(B[0][:, it * 256:(it + 1) * 256]),
                                                              in1=eac[:, it, g * 4:(g + 1) * 4].unsqueeze(2).to_broadcast([128, 4, 64]), op=ALU.mult),
                             reads=[d_B[0], d_eac], writes=[d_yt[it]])
                    elif step == 1:
                        P.op("dve", lambda e: e.tensor_tensor(out=yt[it][:], in0=yt[it][:], in1=B[6][:, it * 256:(it + 1) * 256], op=ALU.add),
                             reads=[d_yt[it], d_B[6]], writes=[d_yt[it]])
                    elif step == 2:
                        P.op("pool", lambda e: e.tensor_tensor(out=v4(y2[it][:]), in0=v4(xtok[:, it, gs_]),
                                                               in1=dsk[:, g * 4:(g + 1) * 4].unsqueeze(2).to_broadcast([128, 4, 64]), op=ALU.mult),
                             reads=[d_xtok, d_c], writes=[d_y2[it]])
                    elif step == 3:
                        P.op("dve", lambda e: e.tensor_tensor(out=yt[it][:], in0=yt[it][:], in1=y2[it][:], op=ALU.add), reads=[d_yt[it], d_y2[it]], writes=[d_yt[it]])
                    elif step == 4:
                        P.op("dve", lambda e: e.tensor_tensor(out=yt[it][:], in0=yt[it][:], in1=zs[:, it, gs_], op=ALU.mult), reads=[d_yt[it], d_zs], writes=[d_yt[it]])
                    elif step == 5:
                        P.op("act", lambda e: e.activation(out=y2[it][:], in_=yt[it][:], func=AF.Square, accum_out=gss[it][:]), reads=[d_yt[it], d_y2[it]], writes=[d_y2[it], d_gss[it]])
                    elif step == 6:
                        P.op("act", lambda e: e.activation(out=grs[it][:], in_=gss[it][:], func=AF.Sqrt, bias=EPS, scale=1.0 / 256), reads=[d_gss[it]], writes=[d_grs[it]])
                    elif step == 7:
                        P.op("dve", lambda e: e.reciprocal(out=grs[it][:], in_=grs[it][:]), reads=[d_grs[it]], writes=[d_grs[it]])
                    elif step == 8:
                        P.op("dve", lambda e: e.scalar_tensor_tensor(out=osb[:, it, gs_], in0=yt[it][:], scalar=grs[it][:, 0:1], in1=ssdn[:, gs_],
                                                                     op0=ALU.mult, op1=ALU.mult), reads=[d_yt[it], d_grs[it], d_c], writes=[d_osb])
                for step in range(9):
                    for it in range(2):
                        ystep(step, it)
            P.dma("sp", lambda e, r0=r0: e.dma_start(out=os_d[r0 - TP:r0 - TP + 256, :].rearrange("(t p) c -> p t c", p=128), in_=osb[:]),
                  reads=[d_osb], writes=[d_osd])
        for g in range(4):
            bank = B[2] if g < 2 else B[1]
            dbank = d_B[2] if g < 2 else d_B[1]
            cs_ = slice((g % 2) * 256, (g % 2 + 1) * 256)
            for jt in range(2):
                P.op("pe", lambda e, g=g, jt=jt, bank=bank, cs_=cs_: e.matmul(bank[:, cs_], lhsT=btok[:, jt, g * 128:(g + 1) * 128], rhs=xdd[:, jt, g * 256:(g + 1) * 256],
                                                                            start=(jt == 0), stop=(jt == 1)), reads=[d_btok, d_xdd], writes=[dbank])
        v16 = lambda ap: ap.rearrange("p (h d) -> p h d", d=64)
        P.op("dve", lambda e: e.tensor_tensor(out=v16(stf[:].rearrange("p g c -> p (g c)")), in0=v16(stf[:].rearrange("p g c -> p (g c)")),
                                              in1=cdec[:].unsqueeze(2).to_broadcast([128, 16, 64]), op=ALU.mult), reads=[d_stf, d_cdec], writes=[d_stf])
        P.op("dve", lambda e: e.tensor_tensor(out=stf[:, 0:2, :].rearrange("p g c -> p (g c)"), in0=stf[:, 0:2, :].rearrange("p g c -> p (g c)"), in1=B[2][:, :], op=ALU.add),
             reads=[d_stf, d_B[2]], writes=[d_stf])
        P.op("dve", lambda e: e.tensor_tensor(out=stf[:, 2:4, :].rearrange("p g c -> p (g c)"), in0=stf[:, 2:4, :].rearrange("p g c -> p (g c)"), in1=B[1][:, :], op=ALU.add),
             reads=[d_stf, d_B[1]], writes=[d_stf])
        if c == 15:
            P.op("dve", lambda e: e.tensor_scalar(out=stf[:], in0=stf[:], scalar1=pflag[:, 0:1], scalar2=None, op0=ALU.mult), reads=[d_stf, d_c], writes=[d_stf])
        P.op("act", lambda e: e.copy(out=stT[:], in_=stf[:]), reads=[d_stf], writes=[d_stT])


def host_consts_c(z, half):
    cwl = np.ascontiguousarray(z['conv_w'].reshape(4, 16, 128).transpose(2, 1, 0)).astype(np.float32)
    cbl = np.ascontiguousarray(z['conv_b'].reshape(16, 128).T).astype(np.float32)
    tri = (np.arange(128)[:, None] <= np.arange(128)[None, :]).astype(np.float32)
    tri2 = np.zeros((128, 2, 256), np.float32)
    tri2[:, 0, 0:128] = tri; tri2[:, 0, 128:256] = 1.0; tri2[:, 1, 128:256] = tri
    sel = np.zeros((16, 16, 128), np.float32)
    for h in range(16):
        sel[h, h, :] = 1.0
    tbias = np.where(tri > 0, 0.0, NEG).astype(np.float32)
    tb = np.zeros((128, 384), np.float32)
    tb[:, 0:128] = tbias; tb[:, 256:384] = tbias
    return dict(cwl=cwl, cbl=cbl, dtb=z['dt_bias'][None, :], alog=z['a_log'][None, :], dsk=z['d_skip'][None, :], ssdn=z['ssd_norm'][None, :],
                tri2=tri2, sel_d=sel, tb_d=tb, pflag=np.array([[1.0 if half == 1 else 0.0]], np.float32))


def phase_def(nc, P, xown, w_in, g_mix, mem_d, gmem_d, wmemkv, mqn_d, mkn_d, woa_d, wos_d, wom_d, wout_d, gffn_d, wr_d,
              wgate_d, wup_d, wdown_d, tri_d, ecap_d, oa_d, d_oad, os_d, d_osd, x1_d, xbuf, ybuf, out_d, idb, d_idb, idf, d_idf,
              ntiles=32, experts=range(32), stage=3):
    d_x1d = Dep(); d_xbuf = Dep(); d_ybuf = Dep(); d_out = Dep()
    with contextlib.ExitStack() as stp:
        def sbp(name, shape, dt):
            return stp.enter_context(nc.sbuf_tensor("d_" + name, shape, dt))
        combs = sbp("combs", [128, 32, 2], F32); d_combs = Dep()
        idxs = sbp("idxs", [128, 32, 2], I32); d_idxs = Dep()

        with contextlib.ExitStack() as st:
            def sb(name, shape, dt):
                return st.enter_context(nc.sbuf_tensor("d_" + name, shape, dt))

            def ps(name, shape, dt=F32):
                return st.enter_context(nc.psum_tensor(name, shape, dt))
            d_w = Dep(); d_c = Dep()
            gm = sb("gmd", [128, D], F32)
            gf = sb("gfd", [128, D], F32)
            mqn = sb("mqn", [128, 512], F32)
            mkn = sb("mkn", [128, 512], F32)
            tri = sb("trid", [128, 128], F32)
            onesf = sb("onesfd", [128, 128], F32)
            onesb = sb("onesbd", [128, 128], BF16)
            ecap = sb("ecap", [128, 32], F32)
            base = sb("base", [128, 32], F32); d_base = Dep()
            kmT = sb("kmT", [128, 4, 256], BF16); d_kmT = Dep()
            vm = sb("vm", [128, 2, 512], BF16); d_vm = Dep()

            xt = [sb("xtd%d" % i, [128, D], F32) for i in range(2)]; d_xt = [Dep(), Dep()]
            junk = sb("junkd", [128, D], BF16); d_junk = Dep()
            ssq = sb("ssqd", [128, 1], F32); d_ssq = Dep()
            rstd = sb("rstdd", [128, 1], F32); d_rstd = Dep()
            hb = sb("hbd", [128, D], BF16); d_hb = Dep()
            hT = sb("hTd", [128, 8, 128], BF16); d_hT = Dep()
            sq = sb("sqd", [128, 512], F32); d_sq = Dep()
            hs = sb("hsd", [128, 4], F32); d_hs = Dep()
            hr = sb("hrd", [128, 4], F32); d_hr = Dep()
            qmn = sb("qmn", [128, 512], F32); d_qmn = Dep()
            qmb = sb("qmb", [128, 512], BF16); d_qmb = Dep()
            qmT = sb("qmT", [128, 4, 128], BF16); d_qmT = Dep()
            pm = sb("pm", [128, 4, 2, 128], BF16); d_pm = Dep()
            rcp = sb("rcp", [128, 512], F32); d_rcp = Dep()
            omT = sb("omT", [128, 4, 128], BF16); d_omT = Dep()
            gs = sb("gs", [128, 3072], F32); d_gs = Dep()
            oat = [sb("oat%d" % i, [128, 512], BF16) for i in range(2)]; d_oat = [Dep(), Dep()]
            ost = [sb("ost%d" % i, [128, 1024], BF16) for i in range(2)]; d_ost = [Dep(), Dep()]
            oaT = sb("oaT", [128, 4, 128], BF16); d_oaT = Dep()
            osT = sb("osT", [128, 8, 128], BF16); d_osT = Dep()
            mg = sb("mg", [128, 512], F32); d_mg = Dep()
            tt = sb("tt", [128, 512], F32); d_tt = Dep()
            mgb = sb("mgb", [128, D], BF16); d_mgb = Dep()
            mgT = sb("mgT", [128, 8, 128], BF16); d_mgT = Dep()
            x1 = sb("x1", [128, D], F32); d_x1 = Dep()
            h2f = sb("h2f", [128, D], F32); d_h2f = Dep()
            h2b = [sb("h2b%d" % i, [128, D], BF16) for i in range(2)]; d_h2b = [Dep() for _ in range(2)]
            h2T = sb("h2T", [128, 8, 128], F32); d_h2T = Dep()
            lg = sb("lg", [128, 36], F32); d_lg = Dep()
            sm = sb("sm", [128, 64], F32); d_sm = Dep()
            goh = sb("goh", [128, 4], F32); d_goh = Dep()
            lsel = sb("lsel", [128, 32], F32); d_lsel = Dep()
            les = sb("les", [128, 8], F32); d_les = Dep()
            t8 = sb("t8", [128, 8], F32); d_t8 = Dep()
            oh = sb("oh", [128, 64], F32); d_oh = Dep()
            ohs = sb("ohs", [128, 32], F32); d_ohs = Dep()
            posf = sb("posf", [128, 32], F32); d_posf = Dep()
            idf2 = sb("idf2", [128, 2], F32); d_idf2 = Dep()

            pT = ps("pTd", [128, 8, 128], BF16); d_pT = Dep()
            pF = ps("pFd", [128, 512], F32); d_pF = Dep()
            pG = [ps("pGd%d" % i, [128, 512], F32) for i in range(3)]; d_pG = [Dep() for _ in range(3)]
            pms = [ps("pmsd%d" % i, [128, 512], F32) for i in range(2)]; d_pms = [Dep() for _ in range(2)]
            pmo = ps("pmod", [128, 512], F32); d_pmo = Dep()

            P.dma("sp", lambda e: e.dma_start(out=gm[:], in_=g_mix[0:1, :].partition_broadcast(128)), writes=[d_c])
            P.dma("sp", lambda e: e.dma_start(out=gf[:], in_=gffn_d[0:1, :].partition_broadcast(128)), writes=[d_c])
            P.dma("sp", lambda e: e.dma_start(out=mqn[:], in_=mqn_d[0:1, :].partition_broadcast(128)), writes=[d_c])
            P.dma("sp", lambda e: e.dma_start(out=mkn[:], in_=mkn_d[0:1, :].partition_broadcast(128)), writes=[d_c])
            P.dma("sp", lambda e: e.dma_start(out=tri[:], in_=tri_d[:, :]), writes=[d_c])
            P.dma("sp", lambda e: e.dma_start(out=ecap[:], in_=ecap_d[0:1, :].partition_broadcast(128)), writes=[d_c])
            P.op("pool", lambda e: e.memset(onesf[:], 1.0), writes=[d_c])
            P.op("pool", lambda e: e.memset(onesb[:], 1.0), writes=[d_c])
            P.op("pool", lambda e: e.memset(base[:], 0.0), writes=[d_base])

            def head_norm(src_ps, d_src, gain, nh, dst, d_dst):
                hd = 512 // nh
                v = lambda ap: ap.rearrange("p (h d) -> p h d", h=nh)
                P.op("act", lambda e: e.activation(out=sq[:], in_=src_ps, func=AF.Square), reads=[d_src], writes=[d_sq])
                P.op("dve", lambda e: e.tensor_reduce(out=hs[:, 0:nh], in_=v(sq[:]), axis=AX.X, op=ALU.add), reads=[d_sq], writes=[d_hs])
                P.op("act", lambda e: e.activation(out=hr[:, 0:nh], in_=hs[:, 0:nh], func=AF.Sqrt, bias=EPS, scale=1.0 / hd), reads=[d_hs], writes=[d_hr])
                P.op("dve", lambda e: e.reciprocal(out=hr[:, 0:nh], in_=hr[:, 0:nh]), reads=[d_hr], writes=[d_hr])
                P.op("dve", lambda e: e.tensor_tensor(out=v(qmn[:]), in0=v(src_ps), in1=hr[:, 0:nh].unsqueeze(2).to_broadcast([128, nh, hd]), op=ALU.mult),
                     reads=[d_src, d_hr], writes=[d_qmn])
                P.op("pool", lambda e: e.tensor_tensor(out=dst, in0=qmn[:], in1=gain[:], op=ALU.mult), reads=[d_qmn, d_c], writes=[d_dst])

            with contextlib.ExitStack() as stm:
                wkv = stm.enter_context(nc.sbuf_tensor("s_wkv", [128, 8, 1024], BF16)); d_wkv = Dep()
                mt_ = stm.enter_context(nc.sbuf_tensor("s_memt", [128, 2, D], F32)); d_mt = Dep()
                gme = stm.enter_context(nc.sbuf_tensor("s_gme", [128, D], F32)); d_gme = Dep()
                mhb = stm.enter_context(nc.sbuf_tensor("s_mhb", [128, 2, D], BF16)); d_mhb = Dep()
                mT = stm.enter_context(nc.sbuf_tensor("s_mT", [128, 8, 256], BF16)); d_mT = Dep()
                ss2 = stm.enter_context(nc.sbuf_tensor("s_ss2", [128, 2], F32)); d_ss2 = Dep()
                for k in range(8):
                    P.dma("pool", lambda e, k=k: e.dma_start(out=wkv[:, k, :], in_=wmemkv[k * 128:(k + 1) * 128, :]), writes=[d_wkv])
                P.dma("sp", lambda e: e.dma_start(out=mt_[:], in_=mem_d[:, :].rearrange("(t p) d -> p t d", p=128)), writes=[d_mt])
                P.dma("sp", lambda e: e.dma_start(out=gme[:], in_=gmem_d[0:1, :].partition_broadcast(128)), writes=[d_gme])
                for it in range(2):
                    P.op("act", lambda e, it=it: e.activation(out=junk[:], in_=mt_[:, it, :], func=AF.Square, accum_out=ss2[:, it:it + 1]),
                         reads=[d_mt], writes=[d_junk, d_ss2])
                P.op("act", lambda e: e.activation(out=ss2[:], in_=ss2[:], func=AF.Sqrt, bias=EPS, scale=1.0 / D), reads=[d_ss2], writes=[d_ss2])
                P.op("dve", lambda e: e.reciprocal(out=ss2[:], in_=ss2[:]), reads=[d_ss2], writes=[d_ss2])
                for it in range(2):
                    P.op("dve", lambda e, it=it: e.scalar_tensor_tensor(out=mhb[:, it, :], in0=mt_[:, it, :], scalar=ss2[:, it:it + 1], in1=gme[:],
                                                                        op0=ALU.mult, op1=ALU.mult), reads=[d_mt, d_ss2, d_gme], writes=[d_mhb])
                    for k in range(8):
                        P.op("pe", lambda e, it=it, k=k: e.transpose(out=pT[:, k, :], in_=mhb[:, it, k * 128:(k + 1) * 128], identity=idb[:]),
                             reads=[d_mhb, d_idb], writes=[d_pT])
                    P.op("act", lambda e, it=it: e.copy(out=mT[:, :, it * 128:(it + 1) * 128], in_=pT[:]), reads=[d_pT], writes=[d_mT])
                for it in range(2):
                    for hv in range(2):
                        for k in range(8):
                            P.op("pe", lambda e, it=it, hv=hv, k=k: e.matmul(pG[hv][:, :], lhsT=mT[:, k, it * 128:(it + 1) * 128], rhs=wkv[:, k, hv * 512:(hv + 1) * 512],
                                                                           start=(k == 0), stop=(k == 7)), reads=[d_mT, d_wkv], writes=[d_pG[hv]])
                    head_norm(pG[0][:, :], d_pG[0], mkn, 4, qmb[:], d_qmb)
                    for h in range(4):
                        P.op("pe", lambda e, h=h: e.transpose(out=pT[:, h, :], in_=qmb[:, h * 128:(h + 1) * 128], identity=idb[:]),
                             reads=[d_qmb, d_idb], writes=[d_pT])
                    P.op("act", lambda e, it=it: e.copy(out=kmT[:, :, it * 128:(it + 1) * 128], in_=pT[:, 0:4, :]), reads=[d_pT], writes=[d_kmT])
                    P.op("act", lambda e, it=it: e.copy(out=vm[:, it, :], in_=pG[1][:, :]), reads=[d_pG[1]], writes=[d_vm])

            wqm = sb("wqm", [128, 8, 512], BF16)
            wg = sb("wg", [128, 8, 3072], BF16)
            woa = sb("woa", [128, 4, 1024], BF16)
            wos = sb("wos", [128, 8, 1024], BF16)
            wom = sb("wom", [128, 4, 1024], BF16)
            wout = sb("wout", [128, 8, 1024], BF16)
            wr = sb("wr", [128, 8, 36], F32)
            for k in range(8):
                rows = slice(k * 128, (k + 1) * 128)
                P.dma("pool", lambda e, k=k, rows=rows: e.dma_start(out=wqm[:, k, :], in_=w_in[rows, C_QM:C_QM + 512]), writes=[d_w])
                for j in range(2):
                    P.dma("pool", lambda e, k=k, rows=rows, j=j: e.dma_start(out=wg[:, k, j * 1536:(j + 1) * 1536], in_=w_in[rows, C_G + j * 1536:C_G + (j + 1) * 1536]), writes=[d_w])
                P.dma("pool", lambda e, k=k, rows=rows: e.dma_start(out=wos[:, k, :], in_=wos_d[rows, :]), writes=[d_w])
                P.dma("pool", lambda e, k=k, rows=rows: e.dma_start(out=wout[:, k, :], in_=wout_d[rows, :]), writes=[d_w])
                P.dma("sp", lambda e, k=k, rows=rows: e.dma_start(out=wr[:, k, :], in_=wr_d[rows, :]), writes=[d_w])
            for k in range(4):
                rows = slice(k * 128, (k + 1) * 128)
                P.dma("pool", lambda e, k=k, rows=rows: e.dma_start(out=woa[:, k, :], in_=woa_d[rows, :]), writes=[d_w])
                P.dma("pool", lambda e, k=k, rows=rows: e.dma_start(out=wom[:, k, :], in_=wom_d[rows, :]), writes=[d_w])
            def loads(tj):
                xj = tj % 2
                rj = tj * 128
                P.dma("sp", lambda e: e.dma_start(out=xt[xj][:], in_=xown[rj:rj + 128, :]), writes=[d_xt[xj]])
                P.dma("sp", lambda e: e.dma_start(out=oat[xj][:], in_=oa_d[rj:rj + 128, :]), reads=[d_oad], writes=[d_oat[xj]])
                P.dma("sp", lambda e: e.dma_start(out=ost[xj][:], in_=os_d[rj:rj + 128, :]), reads=[d_osd], writes=[d_ost[xj]])

            for ti in range(ntiles):
                r0 = ti * 128
                hbuf = ti % 2
                xb = ti % 2
                if ti == 0:
                    loads(0)
                if ti + 1 < ntiles:
                    loads(ti + 1)
                P.op("act", lambda e, xb=xb: e.activation(out=junk[:], in_=xt[xb][:], func=AF.Square, accum_out=ssq[:]), reads=[d_xt[xb]], writes=[d_junk, d_ssq])
                P.op("act", lambda e: e.activation(out=rstd[:], in_=ssq[:], func=AF.Sqrt, bias=EPS, scale=1.0 / D), reads=[d_ssq], writes=[d_rstd])
                P.op("dve", lambda e: e.reciprocal(out=rstd[:], in_=rstd[:]), reads=[d_rstd], writes=[d_rstd])
                P.op("dve", lambda e, xb=xb: e.scalar_tensor_tensor(out=hb[:], in0=xt[xb][:], scalar=rstd[:, 0:1], in1=gm[:], op0=ALU.mult, op1=ALU.mult),
                     reads=[d_xt[xb], d_rstd, d_c], writes=[d_hb])
                for k in range(8):
                    P.op("pe", lambda e, k=k: e.transpose(out=pT[:, k, :], in_=hb[:, k * 128:(k + 1) * 128], identity=idb[:]), reads=[d_hb, d_idb], writes=[d_pT])
                P.op("act", lambda e: e.copy(out=hT[:], in_=pT[:]), reads=[d_pT], writes=[d_hT])
                for k in range(8):
                    P.op("pe", lambda e, k=k: e.matmul(pG[0][:, :], lhsT=hT[:, k, :], rhs=wqm[:, k, :], start=(k == 0), stop=(k == 7)),
                         reads=[d_hT, d_w], writes=[d_pG[0]])
                head_norm(pG[0][:, :], d_pG[0], mqn, 4, qmb[:], d_qmb)
                for g6 in range(6):
                    bk = g6 % 3
                    for k in range(8):
                        P.op("pe", lambda e, g6=g6, k=k, bk=bk: e.matmul(pG[bk][:, :], lhsT=hT[:, k, :], rhs=wg[:, k, g6 * 512:(g6 + 1) * 512], start=(k == 0), stop=(k == 7)),
                             reads=[d_hT, d_w], writes=[d_pG[bk]])
                    P.op("act", lambda e, g6=g6, bk=bk: e.activation(out=gs[:, g6 * 512:(g6 + 1) * 512], in_=pG[bk][:, :], func=AF.Sigmoid),
                         reads=[d_pG[bk]], writes=[d_gs])
                for h in range(4):
                    P.op("pe", lambda e, h=h: e.transpose(out=pT[:, h, :], in_=qmb[:, h * 128:(h + 1) * 128], identity=idb[:]), reads=[d_qmb, d_idb], writes=[d_pT])
                P.op("act", lambda e: e.copy(out=qmT[:], in_=pT[:, 0:4, :]), reads=[d_pT], writes=[d_qmT])
                for h in range(4):
                    for mt in range(2):
                        bank = pms[h // 2]
                        c0 = ((h % 2) * 2 + mt) * 128
                        P.op("pe", lambda e, h=h, mt=mt, bank=bank, c0=c0: e.matmul(bank[:, c0:c0 + 128], lhsT=kmT[:, h, mt * 128:(mt + 1) * 128], rhs=qmT[:, h, :],
                                                                                  start=True, stop=True), reads=[d_kmT, d_qmT], writes=[d_pms[h // 2]])
                for hp in range(2):
                    P.op("act", lambda e, hp=hp: e.activation(out=pm[:, hp * 2:(hp + 1) * 2, :, :].rearrange("p a b c -> p (a b c)"), in_=pms[hp][:, :], func=AF.Exp,
                                                              scale=float(128 ** -0.5)), reads=[d_pms[hp]], writes=[d_pm])
                for h in range(4):
                    for mt in range(2):
                        P.op("pe", lambda e, h=h, mt=mt: e.matmul(pmo[:, h * 128:(h + 1) * 128], lhsT=vm[:, mt, h * 128:(h + 1) * 128], rhs=pm[:, h, mt, :],
                                                                 start=(mt == 0), stop=(mt == 1)), reads=[d_vm, d_pm], writes=[d_pmo])
                for mt in range(2):
                    P.op("pe", lambda e, mt=mt: e.matmul(pG[1][:, :].rearrange("p (h t) -> p h t", h=4), lhsT=onesb[:], rhs=pm[:, :, mt, :],
                                                        start=(mt == 0), stop=(mt == 1)), reads=[d_c, d_pm], writes=[d_pG[1]])
                P.op("dve", lambda e: e.reciprocal(out=rcp[:], in_=pG[1][:, :]), reads=[d_pG[1]], writes=[d_rcp])
                P.op("dve", lambda e: e.tensor_tensor(out=omT[:].rearrange("p h t -> p (h t)"), in0=pmo[:, :], in1=rcp[:], op=ALU.mult),
                     reads=[d_pmo, d_rcp], writes=[d_omT])
                for k in range(4):
                    P.op("pe", lambda e, xb=xb, k=k: e.transpose(out=pT[:, k, :], in_=oat[xb][:, k * 128:(k + 1) * 128], identity=idb[:]), reads=[d_oat[xb], d_idb], writes=[d_pT])
                P.op("act", lambda e: e.copy(out=oaT[:], in_=pT[:, 0:4, :]), reads=[d_pT], writes=[d_oaT])
                for k in range(8):
                    P.op("pe", lambda e, xb=xb, k=k: e.transpose(out=pT[:, k, :], in_=ost[xb][:, k * 128:(k + 1) * 128], identity=idb[:]), reads=[d_ost[xb], d_idb], writes=[d_pT])
                P.op("act", lambda e: e.copy(out=osT[:], in_=pT[:]), reads=[d_pT], writes=[d_osT])
                for hv in range(2):
                    cs_ = slice(hv * 512, (hv + 1) * 512)
                    for k in range(4):
                        P.op("pe", lambda e, k=k, cs_=cs_: e.matmul(pG[0][:, :], lhsT=oaT[:, k, :], rhs=woa[:, k, cs_], start=(k == 0), stop=(k == 3)),
                             reads=[d_oaT, d_w], writes=[d_pG[0]])
                    for k in range(8):
                        P.op("pe", lambda e, k=k, cs_=cs_: e.matmul(pG[1][:, :], lhsT=osT[:, k, :], rhs=wos[:, k, cs_], start=(k == 0), stop=(k == 7)),
                             reads=[d_osT, d_w], writes=[d_pG[1]])
                    for k in range(4):
                        P.op("pe", lambda e, k=k, cs_=cs_: e.matmul(pG[2][:, :], lhsT=omT[:, k, :], rhs=wom[:, k, cs_], start=(k == 0), stop=(k == 3)),
                             reads=[d_omT, d_w], writes=[d_pG[2]])
                    P.op("dve", lambda e, hv=hv: e.tensor_tensor(out=mg[:], in0=pG[0][:, :], in1=gs[:, hv * 512:(hv + 1) * 512], op=ALU.mult),
                         reads=[d_pG[0], d_gs], writes=[d_mg])
                    P.op("dve", lambda e, hv=hv: e.tensor_tensor(out=tt[:], in0=pG[1][:, :], in1=gs[:, 1024 + hv * 512:1024 + (hv + 1) * 512], op=ALU.mult),
                         reads=[d_pG[1], d_gs], writes=[d_tt])
                    P.op("pool", lambda e: e.tensor_tensor(out=mg[:], in0=mg[:], in1=tt[:], op=ALU.add), reads=[d_mg, d_tt], writes=[d_mg])
                    P.op("dve", lambda e, hv=hv: e.tensor_tensor(out=tt[:], in0=pG[2][:, :], in1=gs[:, 2048 + hv * 512:2048 + (hv + 1) * 512], op=ALU.mult),
                         reads=[d_pG[2], d_gs], writes=[d_tt])
                    P.op("pool", lambda e, cs_=cs_: e.tensor_tensor(out=mgb[:, cs_], in0=mg[:], in1=tt[:], op=ALU.add), reads=[d_mg, d_tt], writes=[d_mgb])
                for k in range(8):
                    P.op("pe", lambda e, k=k: e.transpose(out=pT[:, k, :], in_=mgb[:, k * 128:(k + 1) * 128], identity=idb[:]), reads=[d_mgb, d_idb], writes=[d_pT])
                P.op("act", lambda e: e.copy(out=mgT[:], in_=pT[:]), reads=[d_pT], writes=[d_mgT])
                for hv in range(2):
                    cs_ = slice(hv * 512, (hv + 1) * 512)
                    for k in range(8):
                        P.op("pe", lambda e, k=k, cs_=cs_, hv=hv: e.matmul(pG[hv][:, :], lhsT=mgT[:, k, :], rhs=wout[:, k, cs_], start=(k == 0), stop=(k == 7)),
                             reads=[d_mgT, d_w], writes=[d_pG[hv]])
                    P.op("dve", lambda e, xb=xb, cs_=cs_, hv=hv: e.tensor_tensor(out=x1[:, cs_], in0=pG[hv][:, :], in1=xt[xb][:, cs_], op=ALU.add),
                         reads=[d_pG[hv], d_xt[xb]], writes=[d_x1])
                P.dma("sp", lambda e, r0=r0: e.dma_start(out=x1_d[r0:r0 + 128, :], in_=x1[:]), reads=[d_x1], writes=[d_x1d])
                if stage < 2:
                    continue
                P.op("act", lambda e: e.activation(out=junk[:], in_=x1[:], func=AF.Square, accum_out=ssq[:]), reads=[d_x1], writes=[d_junk, d_ssq])
                P.op("act", lambda e: e.activation(out=rstd[:], in_=ssq[:], func=AF.Sqrt, bias=EPS, scale=1.0 / D), reads=[d_ssq], writes=[d_rstd])
                P.op("dve", lambda e: e.reciprocal(out=rstd[:], in_=rstd[:]), reads=[d_rstd], writes=[d_rstd])
                P.op("dve", lambda e: e.scalar_tensor_tensor(out=h2f[:], in0=x1[:], scalar=rstd[:, 0:1], in1=gf[:], op0=ALU.mult, op1=ALU.mult),
                     reads=[d_x1, d_rstd, d_c], writes=[d_h2f])
                P.op("pool", lambda e, hbuf=hbuf: e.tensor_copy(out=h2b[hbuf][:], in_=h2f[:]), reads=[d_h2f], writes=[d_h2b[hbuf]])
                for half in range(2):
                    for k4 in range(4):
                        k = half * 4 + k4
                        P.op("pe", lambda e, k=k, k4=k4: e.transpose(out=pF[:, k4 * 128:(k4 + 1) * 128], in_=h2f[:, k * 128:(k + 1) * 128], identity=idf[:]),
                             reads=[d_h2f, d_idf], writes=[d_pF])
                    P.op("act", lambda e, half=half: e.copy(out=h2T[:, half * 4:(half + 1) * 4, :].rearrange("p a b -> p (a b)"), in_=pF[:, :]), reads=[d_pF], writes=[d_h2T])
                for k in range(8):
                    P.op("pe", lambda e, k=k: e.matmul(pF[:, 0:36], lhsT=h2T[:, k, :], rhs=wr[:, k, :], start=(k == 0), stop=(k == 7)), reads=[d_h2T, d_w], writes=[d_pF])
                P.op("act", lambda e: e.copy(out=lg[:], in_=pF[:, 0:36]), reads=[d_pF], writes=[d_lg])
                P.op("dve", lambda e: e.tensor_reduce(out=sm[:, 0:1], in_=lg[:, 0:4], axis=AX.X, op=ALU.max), reads=[d_lg], writes=[d_sm])
                P.op("dve", lambda e: e.tensor_scalar(out=sm[:, 1:2], in0=sm[:, 0:1], scalar1=-1.0, scalar2=None, op0=ALU.mult), reads=[d_sm], writes=[d_sm])
                P.op("act", lambda e: e.activation(out=sm[:, 8:12], in_=lg[:, 0:4], func=AF.Exp, bias=sm[:, 1:2], accum_out=sm[:, 2:3]), reads=[d_lg, d_sm], writes=[d_sm])
                P.op("dve", lambda e: e.reciprocal(out=sm[:, 3:4], in_=sm[:, 2:3]), reads=[d_sm], writes=[d_sm])
                P.op("dve", lambda e: e.tensor_scalar(out=goh[:], in0=lg[:, 0:4], scalar1=sm[:, 0:1], scalar2=None, op0=ALU.is_ge), reads=[d_lg, d_sm], writes=[d_goh])
                P.op("dve", lambda e: e.tensor_tensor(out=lsel[:].rearrange("p (g e) -> p g e", g=4), in0=lg[:, 4:36].rearrange("p (g e) -> p g e", g=4),
                                                      in1=goh[:].unsqueeze(2).to_broadcast([128, 4, 8]), op=ALU.mult), reads=[d_lg, d_goh], writes=[d_lsel])
                P.op("dve", lambda e: e.tensor_reduce(out=les[:], in_=lsel[:].rearrange("p (g e) -> p e g", g=4), axis=AX.X, op=ALU.add), reads=[d_lsel], writes=[d_les])
                P.op("dve", lambda e: e.max(out=t8[:], in_=les[:]), reads=[d_les], writes=[d_t8])
                P.op("dve", lambda e: e.tensor_tensor(out=sm[:, 4:5], in0=t8[:, 1:2], in1=t8[:, 0:1], op=ALU.subtract), reads=[d_t8, d_sm], writes=[d_sm])
                P.op("act", lambda e: e.activation(out=sm[:, 5:6], in_=sm[:, 4:5], func=AF.Exp), reads=[d_sm], writes=[d_sm])
                P.op("dve", lambda e: e.tensor_scalar(out=sm[:, 6:7], in0=sm[:, 5:6], scalar1=1.0, scalar2=None, op0=ALU.add), reads=[d_sm], writes=[d_sm])
                P.op("dve", lambda e: e.reciprocal(out=sm[:, 6:7], in_=sm[:, 6:7]), reads=[d_sm], writes=[d_sm])
                P.op("dve", lambda e: e.tensor_tensor(out=sm[:, 7:8], in0=sm[:, 5:6], in1=sm[:, 6:7], op=ALU.mult), reads=[d_sm], writes=[d_sm])
                P.op("dve", lambda e, ti=ti: e.tensor_scalar(out=combs[:, ti, :], in0=sm[:, 6:8], scalar1=sm[:, 3:4], scalar2=None, op0=ALU.mult), reads=[d_sm], writes=[d_combs])
                for j in range(2):
                    P.op("dve", lambda e, j=j: e.tensor_scalar(out=oh[:, j * 32:(j + 1) * 32], in0=lg[:, 4:36], scalar1=t8[:, j:j + 1], scalar2=None, op0=ALU.is_equal),
                         reads=[d_lg, d_t8], writes=[d_oh])
                    P.op("dve", lambda e, j=j: e.tensor_tensor(out=oh[:, j * 32:(j + 1) * 32].rearrange("p (g e) -> p g e", g=4), in0=oh[:, j * 32:(j + 1) * 32].rearrange("p (g e) -> p g e", g=4),
                                                               in1=goh[:].unsqueeze(2).to_broadcast([128, 4, 8]), op=ALU.mult), reads=[d_oh, d_goh], writes=[d_oh])
                P.op("dve", lambda e: e.tensor_tensor(out=ohs[:], in0=oh[:, 0:32], in1=oh[:, 32:64], op=ALU.add), reads=[d_oh], writes=[d_ohs])
                P.op("pe", lambda e: e.matmul(pF[:, 64:96], lhsT=tri[:], rhs=ohs[:], start=True, stop=True), reads=[d_c, d_ohs], writes=[d_pF])
                P.op("pe", lambda e: e.matmul(pF[:, 128:160], lhsT=onesf[:], rhs=ohs[:], start=True, stop=True), reads=[d_c, d_ohs], writes=[d_pF])
                P.op("dve", lambda e: e.tensor_tensor(out=posf[:], in0=pF[:, 64:96], in1=ohs[:], op=ALU.subtract), reads=[d_pF, d_ohs], writes=[d_posf])
                P.op("dve", lambda e: e.tensor_tensor(out=posf[:], in0=posf[:], in1=base[:], op=ALU.add), reads=[d_posf, d_base], writes=[d_posf])
                P.op("dve", lambda e: e.tensor_tensor(out=posf[:], in0=posf[:], in1=ecap[:], op=ALU.add), reads=[d_posf, d_c], writes=[d_posf])
                P.op("dve", lambda e: e.tensor_tensor(out=base[:], in0=base[:], in1=pF[:, 128:160], op=ALU.add), reads=[d_base, d_pF, d_posf], writes=[d_base])
                for j in range(2):
                    P.op("dve", lambda e, j=j: e.tensor_tensor(out=oh[:, j * 32:(j + 1) * 32], in0=oh[:, j * 32:(j + 1) * 32], in1=posf[:], op=ALU.mult), reads=[d_oh, d_posf], writes=[d_oh])
                    P.op("dve", lambda e, j=j: e.tensor_reduce(out=idf2[:, j:j + 1], in_=oh[:, j * 32:(j + 1) * 32], axis=AX.X, op=ALU.add), reads=[d_oh], writes=[d_idf2])
                P.op("dve", lambda e, ti=ti: e.tensor_copy(out=idxs[:, ti, :], in_=idf2[:]), reads=[d_idf2], writes=[d_idxs])
                for j in range(2):
                    P.dma("pool", lambda e, ti=ti, j=j, hbuf=hbuf: e.indirect_dma_start(out=xbuf[:, :], out_offset=bass.IndirectOffsetOnAxis(ap=idxs[:, ti, j:j + 1], axis=0),
                                                                                      in_=h2b[hbuf][:], in_offset=None),
                          reads=[d_h2b[hbuf], d_idxs], writes=[d_xbuf])

        if stage < 3:
            return
        if hasattr(P, 'barrier'):
            P.barrier()
        with contextlib.ExitStack() as st:
            def sb(name, shape, dt):
                return st.enter_context(nc.sbuf_tensor("d_" + name, shape, dt))

            def ps(name, shape, dt=F32):
                return st.enter_context(nc.psum_tensor(name, shape, dt))
            NW = 2
            wge = [sb("wge%d" % i, [128, 8, 512], BF16) for i in range(NW)]; d_wge = [Dep() for _ in range(NW)]
            wue = [sb("wue%d" % i, [128, 8, 512], BF16) for i in range(NW)]; d_wue = [Dep() for _ in range(NW)]
            wde = [sb("wde%d" % i, [128, 4, 1024], BF16) for i in range(NW)]; d_wde = [Dep() for _ in range(NW)]
            xe = [sb("xe%d" % i, [128, 3, D], BF16) for i in range(NW)]; d_xe = [Dep() for _ in range(NW)]
            xeT = sb("xeT", [128, 8, CAP], BF16); d_xeT = Dep()
            sg = sb("sg", [128, CAP], F32); d_sg = Dep()
            hTe = sb("hTe", [128, 4, CAP], BF16); d_hTe = Dep()
            ye = [sb("ye%d" % i, [128, 3, D], F32) for i in range(NW)]; d_ye = [Dep() for _ in range(NW)]
            pT = ps("pTe", [128, 8, 128], BF16); d_pT = Dep()
            pg_ = [ps("pge%d" % i, [128, 512], F32) for i in range(2)]; d_pg = [Dep() for _ in range(2)]
            pu_ = [ps("pue%d" % i, [128, 512], F32) for i in range(2)]; d_pu = [Dep() for _ in range(2)]
            py_ = [ps("pye%d" % i, [128, 512], F32) for i in range(2)]; d_py = [Dep() for _ in range(2)]
            for ie, ex in enumerate(experts):
                b = ie % NW
                P.dma("pool", lambda e, b=b, ex=ex: e.dma_start(out=wge[b][:], in_=wgate_d[ex].rearrange("(k p) f -> p k f", p=128)), writes=[d_wge[b]])
                P.dma("pool", lambda e, b=b, ex=ex: e.dma_start(out=wue[b][:], in_=wup_d[ex].rearrange("(k p) f -> p k f", p=128)), writes=[d_wue[b]])
                P.dma("pool", lambda e, b=b, ex=ex: e.dma_start(out=wde[b][:], in_=wdown_d[ex].rearrange("(k p) f -> p k f", p=128)), writes=[d_wde[b]])
                P.dma("sp", lambda e, b=b, ex=ex: e.dma_start(out=xe[b][:], in_=xbuf[ex * CAP:(ex + 1) * CAP, :].rearrange("(s p) d -> p s d", p=128)),
                      reads=[d_xbuf], writes=[d_xe[b]])
                for s in range(3):
                    for k in range(8):
                        P.op("pe", lambda e, b=b, s=s, k=k: e.transpose(out=pT[:, k, :], in_=xe[b][:, s, k * 128:(k + 1) * 128], identity=idb[:]),
                             reads=[d_xe[b], d_idb], writes=[d_pT])
                    P.op("act", lambda e, s=s: e.copy(out=xeT[:, :, s * 128:(s + 1) * 128], in_=pT[:]), reads=[d_pT], writes=[d_xeT])
                for ft in range(4):
                    pb = ft % 2
                    for k in range(8):
                        P.op("pe", lambda e, b=b, ft=ft, k=k, pb=pb: e.matmul(pg_[pb][:, 0:CAP], lhsT=wge[b][:, k, ft * 128:(ft + 1) * 128], rhs=xeT[:, k, :],
                                                                            start=(k == 0), stop=(k == 7)), reads=[d_wge[b], d_xeT], writes=[d_pg[pb]])
                    for k in range(8):
                        P.op("pe", lambda e, b=b, ft=ft, k=k, pb=pb: e.matmul(pu_[pb][:, 0:CAP], lhsT=wue[b][:, k, ft * 128:(ft + 1) * 128], rhs=xeT[:, k, :],
                                                                            start=(k == 0), stop=(k == 7)), reads=[d_wue[b], d_xeT], writes=[d_pu[pb]])
                    P.op("act", lambda e, pb=pb: e.activation(out=sg[:], in_=pg_[pb][:, 0:CAP], func=AF.Silu), reads=[d_pg[pb]], writes=[d_sg])
                    P.op("dve", lambda e, ft=ft, pb=pb: e.tensor_tensor(out=hTe[:, ft, :], in0=sg[:], in1=pu_[pb][:, 0:CAP], op=ALU.mult),
                         reads=[d_sg, d_pu[pb]], writes=[d_hTe])
                for s in range(3):
                    for hv in range(2):
                        for ft in range(4):
                            P.op("pe", lambda e, b=b, s=s, hv=hv, ft=ft: e.matmul(py_[hv][:, :], lhsT=hTe[:, ft, s * 128:(s + 1) * 128], rhs=wde[b][:, ft, hv * 512:(hv + 1) * 512],
                                                                                start=(ft == 0), stop=(ft == 3)), reads=[d_hTe, d_wde[b]], writes=[d_py[hv]])
                        P.op("act" if hv == 0 else "dve", (lambda e, b=b, s=s, hv=hv: e.copy(out=ye[b][:, s, hv * 512:(hv + 1) * 512], in_=py_[hv][:, :])) if hv == 0 else
                             (lambda e, b=b, s=s, hv=hv: e.tensor_copy(out=ye[b][:, s, hv * 512:(hv + 1) * 512], in_=py_[hv][:, :])),
                             reads=[d_py[hv]], writes=[d_ye[b]])
                P.dma("sp", lambda e, b=b, ex=ex: e.dma_start(out=ybuf[ex * CAP:(ex + 1) * CAP, :].rearrange("(s p) d -> p s d", p=128), in_=ye[b][:]),
                      reads=[d_ye[b]], writes=[d_ybuf])

        if hasattr(P, 'barrier'):
            P.barrier()
        with contextlib.ExitStack() as st:
            def sb(name, shape, dt):
                return st.enter_context(nc.sbuf_tensor("d_" + name, shape, dt))
            NF = 2
            x1t = [sb("x1t%d" % i, [128, D], F32) for i in range(NF)]; d_x1t = [Dep() for _ in range(NF)]
            y1 = [sb("y1t%d" % i, [128, D], F32) for i in range(NF)]; d_y1 = [Dep() for _ in range(NF)]
            y2 = [sb("y2t%d" % i, [128, D], F32) for i in range(NF)]; d_y2 = [Dep() for _ in range(NF)]
            ot = [sb("ot%d" % i, [128, D], F32) for i in range(NF)]; d_ot = [Dep() for _ in range(NF)]
            for ti in range(ntiles):
                b = ti % NF
                r0 = ti * 128
                P.dma("sp", lambda e, b=b, r0=r0: e.dma_start(out=x1t[b][:], in_=x1_d[r0:r0 + 128, :]), reads=[d_x1d], writes=[d_x1t[b]])
                P.dma("pool", lambda e, b=b, ti=ti: e.indirect_dma_start(out=y1[b][:], out_offset=None, in_=ybuf[:, :],
                                                                        in_offset=bass.IndirectOffsetOnAxis(ap=idxs[:, ti, 0:1], axis=0)),
                      reads=[d_ybuf, d_idxs], writes=[d_y1[b]])
                P.dma("pool", lambda e, b=b, ti=ti: e.indirect_dma_start(out=y2[b][:], out_offset=None, in_=ybuf[:, :],
                                                                        in_offset=bass.IndirectOffsetOnAxis(ap=idxs[:, ti, 1:2], axis=0)),
                      reads=[d_ybuf, d_idxs], writes=[d_y2[b]])
                P.op("dve", lambda e, b=b, ti=ti: e.scalar_tensor_tensor(out=ot[b][:], in0=y1[b][:], scalar=combs[:, ti, 0:1], in1=x1t[b][:], op0=ALU.mult, op1=ALU.add),
                     reads=[d_y1[b], d_combs, d_x1t[b]], writes=[d_ot[b]])
                P.op("dve", lambda e, b=b, ti=ti: e.scalar_tensor_tensor(out=ot[b][:], in0=y2[b][:], scalar=combs[:, ti, 1:2], in1=ot[b][:], op0=ALU.mult, op1=ALU.add),
                     reads=[d_y2[b], d_combs, d_ot[b]], writes=[d_ot[b]])
                P.dma("sp", lambda e, b=b, r0=r0: e.dma_start(out=out_d[r0:r0 + 128, :], in_=ot[b][:]), reads=[d_ot[b]], writes=[d_out])


def host_consts_d(z):
    tri = (np.arange(128)[:, None] <= np.arange(128)[None, :]).astype(np.float32)
    return dict(mqn=np.tile(z['mem_q_norm'], 4)[None, :], mkn=np.tile(z['mem_k_norm'], 4)[None, :],
                w_r=np.ascontiguousarray(np.concatenate([z['w_router_group'], z['w_router_expert']], axis=1)),
                trif=tri, ecap=(np.arange(32, dtype=np.float32) * CAP)[None, :], ident=np.eye(128, dtype=np.float32))


def build_full():
    nc = bass.Bass("TRN2", target_bir_lowering=False)
    P = Prog(nc)

    def di(name, shape, dt=F32):
        return nc.dram_tensor(name, shape, dt, kind="ExternalInput")
    xin = di("xin", [TP + T, D]); w_in = di("w_in", [D, IN_COLS]); g_mix = di("g_mix", [1, D])
    qkn = di("qkn", [2, 512]); cs = di("cs", [TP + T, 64]); ident_d = di("ident", [128, 128])
    e_d = di("e_d", [32, NK], BF16); vq_d = di("vq", [1, 1024]); nb_d = di("nb", [1, 1024]); oq_d = di("oq", [1, 1024])
    tri_d = di("tri", [128, 128], BF16)
    cwl_d = di("cwl", [128, 16, 4]); cbl_d = di("cbl", [128, 16]); dtb_d = di("dtb", [1, 16]); alog_d = di("alog", [1, 16])
    dsk_d = di("dsk", [1, 16]); ssdn_d = di("ssdn", [1, D]); tri2_d = di("tri2", [128, 2, 256]); sel_d = di("sel_d", [16, 16, 128])
    tb_d = di("tb_d", [128, 384]); pflag_d = di("pflag", [1, 1])
    mem_d = di("mem", [256, D]); gmem_d = di("g_mem", [1, D]); wmemkv = di("w_mem_kv", [D, 1024])
    mqn_d = di("mqn", [1, 512]); mkn_d = di("mkn", [1, 512])
    woa_d = di("w_o_moba", [512, D]); wos_d = di("w_o_ssd", [D, D]); wom_d = di("w_o_mem", [512, D]); wout_d = di("w_out", [D, D])
    gffn_d = di("g_ffn", [1, D]); wr_d = di("w_r", [D, 36])
    wgate_d = di("w_gate", [NE, D, 512]); wup_d = di("w_up", [NE, D, 512]); wdown_d = di("w_down", [NE, 512, D])
    trif_d = di("trif", [128, 128]); ecap_d = di("ecap", [1, 32])
    out_d = nc.dram_tensor("out", [T, D], F32, kind="ExternalOutput")
    kT_d = nc.dram_tensor("kT_d", [512, NK], BF16)
    v_d = nc.dram_tensor("v_d", [NK, 512], BF16)
    qT_d = nc.dram_tensor("qT_d", [512, T], BF16)
    oa_d = nc.dram_tensor("oa_d", [T, 512], BF16)
    os_d = nc.dram_tensor("os_d", [T, D], BF16)
    x1_d = nc.dram_tensor("x1_d", [T, D], F32)
    xbuf = nc.dram_tensor("xbuf", [NE * CAP, D], BF16)
    ybuf = nc.dram_tensor("ybuf", [NE * CAP, D], F32)

    with contextlib.ExitStack() as st0:
        idf = st0.enter_context(nc.sbuf_tensor("idf", [128, 128], F32)); d_idf = Dep()
        idb = st0.enter_context(nc.sbuf_tensor("idb", [128, 128], BF16)); d_idb = Dep()
        P.dma("sp", lambda e: e.dma_start(out=idf[:], in_=ident_d[:, :]), writes=[d_idf])
        P.op("dve", lambda e: e.tensor_copy(out=idb[:], in_=idf[:]), reads=[d_idf], writes=[d_idb])
        with contextlib.ExitStack() as st:
            phase_a(nc, P, st, xin, w_in, g_mix, qkn, cs, idb, d_idb, kT_d, v_d, qT_d)
        P.barrier()
        with contextlib.ExitStack() as st:
            oa = st.enter_context(nc.sbuf_tensor("s_oa", [128, 32, 512], BF16)); d_oa = Dep()
            phase_b(nc, P, st, kT_d, v_d, qT_d, e_d, vq_d, nb_d, oq_d, tri_d, idb, d_idb, oa, d_oa)
            P.dma("sp", lambda e: e.dma_start(out=oa_d[:, :].rearrange("(t p) c -> p t c", p=128), in_=oa[:]), reads=[d_oa])
        P.barrier()
        with contextlib.ExitStack() as st:
            phase_c(nc, P, st, xin, w_in, g_mix, cwl_d, cbl_d, dtb_d, alog_d, dsk_d, ssdn_d, tri2_d, sel_d, tb_d, pflag_d,
                    idb, d_idb, os_d, Dep())
        P.barrier()
        phase_def(nc, P, xin[TP:TP + T, :], w_in, g_mix, mem_d, gmem_d, wmemkv, mqn_d, mkn_d, woa_d, wos_d, wom_d, wout_d, gffn_d, wr_d,
                  wgate_d, wup_d, wdown_d, trif_d, ecap_d, oa_d, Dep(), os_d, Dep(), x1_d, xbuf, ybuf, out_d, idb, d_idb, idf, d_idf)
        P.emit()
    return nc


_NC_CACHE = {}


def kernel(x, mem, g_mix, w_in, moba_q_norm, moba_k_norm, conv_w, conv_b, dt_bias, a_log, d_skip, ssd_norm, g_mem, w_mem_kv,
           mem_q_norm, mem_k_norm, w_o_moba, w_o_ssd, w_o_mem, w_out, g_ffn, w_router_group, w_router_expert, w_gate, w_up, w_down):
    f32 = np.float32
    A = lambda a: np.ascontiguousarray(np.asarray(a, dtype=f32))
    x = A(x); mem = A(mem)
    z = dict(conv_w=A(conv_w), conv_b=A(conv_b), dt_bias=A(dt_bias), a_log=A(a_log), d_skip=A(d_skip), ssd_norm=A(ssd_norm),
             mem_q_norm=A(mem_q_norm), mem_k_norm=A(mem_k_norm), w_router_group=A(w_router_group), w_router_expert=A(w_router_expert))
    if "nc" not in _NC_CACHE:
        _NC_CACHE["nc"] = build_full()
    nc = _NC_CACHE["nc"]
    pos = np.arange(2 * T, dtype=f32)
    inv = (10000.0 ** (-np.arange(32, dtype=f32) / 32)).astype(f32)
    ang = pos[:, None] * inv[None, :]
    cs_full = np.concatenate([np.cos(ang), np.sin(ang)], axis=1).astype(f32)
    qkn = np.stack([np.tile(A(moba_q_norm), 8), np.tile(A(moba_k_norm), 8)]).astype(f32)
    shared = dict(w_in=A(w_in), g_mix=A(g_mix)[None, :], qkn=qkn, ident=np.eye(128, dtype=f32),
                  g_mem=A(g_mem)[None, :], w_mem_kv=A(w_mem_kv), w_o_moba=A(w_o_moba), w_o_ssd=A(w_o_ssd), w_o_mem=A(w_o_mem),
                  w_out=A(w_out), g_ffn=A(g_ffn)[None, :], w_gate=A(w_gate), w_up=A(w_up), w_down=A(w_down))
    shared.update(host_consts_d(z))
    in_maps = []
    for c in range(8):
        bb, hh = c // 2, c % 2
        if hh == 0:
            xin = np.concatenate([np.zeros((TP, D), f32), x[bb, :T]], 0)
            csc = np.concatenate([cs_full[:TP], cs_full[:T]], 0)
        else:
            xin = x[bb]
            csc = cs_full
        m = dict(shared)
        m.update(xin=np.ascontiguousarray(xin), cs=np.ascontiguousarray(csc), mem=mem[bb])
        m.update(host_consts(hh))
        m.update(host_consts_c(z, hh))
        in_maps.append(m)
    res = run_bass_kernel_spmd(nc, in_maps, core_ids=list(range(8)))
    out = np.empty((4, 2 * T, D), f32)
    for c in range(8):
        bb, hh = c // 2, c % 2
        out[bb, hh * T:(hh + 1) * T] = np.asarray(res.results[c]["out"], dtype=f32)
    return out
```

```python
import contextlib
import numpy as np
import ml_dtypes
import concourse.bass as bass
import concourse.mybir as mybir
from concourse.bass_utils import run_bass_kernel_spmd

F32 = mybir.dt.float32
BF16 = mybir.dt.bfloat16
I32 = mybir.dt.int32
U32 = mybir.dt.uint32
AF = mybir.ActivationFunctionType
ALU = mybir.AluOpType
AX = mybir.AxisListType

T = 4096
TP = 4096
NK = TP + T
D = 1024
EPS = 1e-6
IN_COLS = 8208
BIGG = 30000.0
MB = 1000.0
NEG = -30000.0
C_Z, C_X, C_DT = 1536, 2560, 4608
C_QM, C_G = 4624, 5136
CAP = 384
NE = 32


class Dep:
    __slots__ = ("w", "r")

    def __init__(self):
        self.w = None
        self.r = {}


class Op:
    __slots__ = ("eng", "fn", "deps", "is_dma", "sig", "sigval", "dsem", "dval", "prev")

    def __init__(self, eng, fn, is_dma):
        self.eng = eng
        self.fn = fn
        self.is_dma = is_dma
        self.deps = []
        self.sig = False
        self.sigval = 0
        self.dsem = None
        self.dval = 0
        self.prev = None


class Prog:
    ENGS = ("pe", "act", "dve", "pool", "sp")

    def __init__(self, nc, ndma_sems=12):
        self.nc = nc
        self.ops = {e: [] for e in self.ENGS}
        self.ndma = {e: 0 for e in self.ENGS}
        self.dma_last = {}
        self.ndma_sems = ndma_sems
        self.all_dma = []

    def _add(self, o, reads, writes):
        deps = {}
        raw = set()
        for t in reads:
            if t.w is not None:
                deps[id(t.w)] = t.w
                raw.add(id(t.w))
        for t in writes:
            if t.w is not None:
                deps[id(t.w)] = t.w
            for r in t.r.values():
                deps[id(r)] = r
        for t in reads:
            key = id(o) if o.is_dma else o.eng
            t.r[key] = o
        for t in writes:
            t.w = o
            t.r = {}
        dl = []
        for d in deps.values():
            if d is o:
                continue
            if (not d.is_dma) and (not o.is_dma) and d.eng == "pe" and o.eng == "pe":
                continue
            if (not d.is_dma) and (not o.is_dma) and d.eng == o.eng and id(d) not in raw:
                continue
            dl.append(d)
            if not d.is_dma:
                d.sig = True
        o.deps = dl
        self.ops[o.eng].append(o)
        return o

    def op(self, eng, fn, reads=(), writes=()):
        return self._add(Op(eng, fn, False), reads, writes)

    def dma(self, eng, fn, reads=(), writes=()):
        o = Op(eng, fn, True)
        n = self.ndma[eng]
        self.ndma[eng] += 1
        slot = (eng, n % self.ndma_sems)
        o.dsem = slot
        o.prev = self.dma_last.get(slot)
        o.dval = (o.prev.dval if o.prev else 0) + 16
        self.dma_last[slot] = o
        self.all_dma.append(o)
        return self._add(o, reads, writes)


    def barrier(self):
        lasts = []
        for e in self.ENGS:
            for o in reversed(self.ops[e]):
                if (not o.is_dma) and o.fn is not None:
                    o.sig = True
                    lasts.append(o)
                    break
        dmas = list(self.dma_last.values())
        for e in self.ENGS:
            o = Op(e, None, False)
            o.deps = [d for d in lasts if d.eng != e] + dmas
            self.ops[e].append(o)

    def emit(self):
        nc = self.nc
        import contextlib
        with contextlib.ExitStack() as st:
            esem = {e: st.enter_context(nc.semaphore("S_" + e)) for e in self.ENGS}
            dsem = {}
            for e in self.ENGS:
                if self.ndma[e]:
                    for i in range(min(self.ndma_sems, self.ndma[e])):
                        dsem[(e, i)] = st.enter_context(nc.semaphore("D_%s_%d" % (e, i)))
            for e in self.ENGS:
                c = 0
                for o in self.ops[e]:
                    if (not o.is_dma) and o.sig and o.fn is not None:
                        c += 1
                        o.sigval = c
            block = st.enter_context(nc.Block())
            handles = {"pe": block.tensor, "act": block.scalar, "dve": block.vector,
                       "pool": block.gpsimd, "sp": block.sync}

            def make(e):
                def body(eng):
                    known = {}

                    def wait(sem, key, val):
                        if known.get(key, 0) < val:
                            eng.wait_ge(sem, val)
                            known[key] = val
                    for o in self.ops[e]:
                        for d in o.deps:
                            if d.is_dma:
                                wait(dsem[d.dsem], d.dsem, d.dval)
                            else:
                                wait(esem[d.eng], d.eng, d.sigval)
                        if o.is_dma:
                            if o.prev is not None:
                                wait(dsem[o.dsem], o.dsem, o.prev.dval)
                            o.fn(eng).then_inc(dsem[o.dsem], 16)
                        elif o.fn is not None:
                            ins = o.fn(eng)
                            if o.sig:
                                ins.then_inc(esem[e], 1)
                    if e == "sp":
                        for slot, o in self.dma_last.items():
                            wait(dsem[slot], slot, o.dval)
                return body
            for e in self.ENGS:
                if self.ops[e] or e == "sp":
                    handles[e](make(e))


def phase_a(nc, P, st, xin, w_in, g_mix, qkn, cs, idb, d_idb, kT_d, v_d, qT_d):
    def sb(name, shape, dt):
        return st.enter_context(nc.sbuf_tensor("a_" + name, shape, dt))

    def ps(name, shape, dt=F32):
        return st.enter_context(nc.psum_tensor("a_" + name, shape, dt))
    wq = sb("wq", [128, 8, 1536], BF16); d_wq = Dep()
    gm = sb("gm", [128, D], F32); d_gm = Dep()
    gq = sb("gq", [128, 2, 512], F32); d_gq = Dep()
    NB = 2
    NX = 4
    xt = [sb("xt%d" % i, [128, D], F32) for i in range(NX)]; d_xt = [Dep() for _ in range(NX)]
    cst = [sb("cst%d" % i, [128, 64], F32) for i in range(NX)]; d_cst = [Dep() for _ in range(NX)]
    junk = sb("junk", [128, D], BF16); d_junk = Dep()
    ssq = sb("ssq", [128, 1], F32); d_ssq = Dep()
    rstd = sb("rstd", [128, 1], F32); d_rstd = Dep()
    hb_ = [sb("hb%d" % i, [128, D], BF16) for i in range(2)]; d_hb_ = [Dep(), Dep()]
    hT = [sb("hT%d" % i, [128, 8, 128], BF16) for i in range(NB)]; d_hT = [Dep() for _ in range(NB)]
    pT = ps("pT", [128, 8, 128], BF16); d_pT = Dep()
    pq = [[ps("pq%d_%d" % (j, i), [128, 512], F32) for i in range(3)] for j in range(2)]; d_pq = [[Dep() for _ in range(3)] for _ in range(2)]
    pkT = ps("pkT", [128, 4, 128], BF16); d_pkT = Dep()
    sq_ = [sb("sq%d" % i, [128, 512], F32) for i in range(2)]; d_sq_ = [Dep(), Dep()]
    hs_ = [sb("hs%d" % i, [128, 8], F32) for i in range(2)]; d_hs_ = [Dep(), Dep()]
    hr_ = [sb("hr%d" % i, [128, 8], F32) for i in range(2)]; d_hr_ = [Dep(), Dep()]
    qn_ = [sb("qn%d" % i, [128, 512], F32) for i in range(2)]; d_qn_ = [Dep(), Dep()]
    t1_ = [sb("t1%d" % i, [128, 256], F32) for i in range(2)]; d_t1_ = [Dep(), Dep()]
    t2_ = [sb("t2%d" % i, [128, 256], F32) for i in range(2)]; d_t2_ = [Dep(), Dep()]
    t3_ = [sb("t3%d" % i, [128, 256], F32) for i in range(2)]; d_t3_ = [Dep(), Dep()]
    t4_ = [sb("t4%d" % i, [128, 256], F32) for i in range(2)]; d_t4_ = [Dep(), Dep()]
    qr_ = [[sb("qr%d_%d" % (i, j), [128, 512], BF16) for j in range(2)] for i in range(2)]; d_qr_ = [[Dep(), Dep()] for _ in range(2)]
    kTs = [[sb("kTs%d_%d" % (j, i), [128, 4, 128], BF16) for i in range(NB)] for j in range(2)]; d_kTs = [[Dep() for _ in range(NB)] for _ in range(2)]
    vs = [sb("vs%d" % i, [128, 512], BF16) for i in range(NB)]; d_vs = [Dep() for _ in range(NB)]

    for k in range(8):
        P.dma("pool", lambda e, k=k: e.dma_start(out=wq[:, k, :], in_=w_in[k * 128:(k + 1) * 128, 0:1536]), writes=[d_wq])
    P.dma("sp", lambda e: e.dma_start(out=gm[:], in_=g_mix[0:1, :].partition_broadcast(128)), writes=[d_gm])
    for i in range(2):
        P.dma("sp", lambda e, i=i: e.dma_start(out=gq[:, i, :], in_=qkn[i:i + 1, :].partition_broadcast(128)), writes=[d_gq])
    P.op("dve", lambda e: e.tensor_scalar(out=gq[:, 0, :], in0=gq[:, 0, :], scalar1=0.125, scalar2=None, op0=ALU.mult),
         reads=[d_gq], writes=[d_gq])

    def qk_post_steps(src_ps, d_src, which, b, xs):
        sl = which
        sq, hs, hr, qn, t1, t2, t3, t4, qr = sq_[sl], hs_[sl], hr_[sl], qn_[sl], t1_[sl], t2_[sl], t3_[sl], t4_[sl], qr_[sl][b]
        d_sq, d_hs, d_hr, d_qn, d_t1, d_t2, d_t3, d_t4, d_qr = (d_sq_[sl], d_hs_[sl], d_hr_[sl], d_qn_[sl], d_t1_[sl], d_t2_[sl],
                                                               d_t3_[sl], d_t4_[sl], d_qr_[sl][b])
        q3 = qn[:].rearrange("p (h d) -> p h d", h=8)
        q1 = q3[:, :, 0:32]
        q2 = q3[:, :, 32:64]
        cosb = cst[xs][:, 0:32].unsqueeze(1).to_broadcast([128, 8, 32])
        sinb = cst[xs][:, 32:64].unsqueeze(1).to_broadcast([128, 8, 32])
        v3 = lambda t: t[:].rearrange("p (h d) -> p h d", h=8)
        r3 = qr[:].rearrange("p (h d) -> p h d", h=8)
        steps = [
            lambda: P.op("act", lambda e: e.activation(out=sq[:], in_=src_ps[:], func=AF.Square), reads=[d_src], writes=[d_sq]),
            lambda: P.op("dve", lambda e: e.tensor_reduce(out=hs[:], in_=sq[:].rearrange("p (h d) -> p h d", h=8), axis=AX.X, op=ALU.add),
                         reads=[d_sq], writes=[d_hs]),
            lambda: P.op("act", lambda e: e.activation(out=hr[:], in_=hs[:], func=AF.Sqrt, bias=EPS, scale=1.0 / 64), reads=[d_hs], writes=[d_hr]),
            lambda: P.op("dve", lambda e: e.reciprocal(out=hr[:], in_=hr[:]), reads=[d_hr], writes=[d_hr]),
            lambda: P.op("dve", lambda e: e.tensor_tensor(out=qn[:].rearrange("p (h d) -> p h d", h=8), in0=src_ps[:].rearrange("p (h d) -> p h d", h=8),
                                                          in1=hr[:].unsqueeze(2).to_broadcast([128, 8, 64]), op=ALU.mult),
                         reads=[d_src, d_hr], writes=[d_qn]),
            lambda: P.op("pool", lambda e: e.tensor_tensor(out=qn[:], in0=qn[:], in1=gq[:, which, :], op=ALU.mult), reads=[d_qn, d_gq], writes=[d_qn]),
            lambda: P.op("dve", lambda e: e.tensor_tensor(out=v3(t1), in0=q1, in1=cosb, op=ALU.mult), reads=[d_qn, d_cst[xs]], writes=[d_t1]),
            lambda: P.op("pool", lambda e: e.tensor_tensor(out=v3(t2), in0=q2, in1=sinb, op=ALU.mult), reads=[d_qn, d_cst[xs]], writes=[d_t2]),
            lambda: P.op("dve", lambda e: e.tensor_tensor(out=v3(t3), in0=q2, in1=cosb, op=ALU.mult), reads=[d_qn, d_cst[xs]], writes=[d_t3]),
            lambda: P.op("pool", lambda e: e.tensor_tensor(out=v3(t4), in0=q1, in1=sinb, op=ALU.mult), reads=[d_qn, d_cst[xs]], writes=[d_t4]),
            lambda: P.op("dve", lambda e: e.tensor_tensor(out=r3[:, :, 0:32], in0=v3(t1), in1=v3(t2), op=ALU.subtract), reads=[d_t1, d_t2], writes=[d_qr]),
            lambda: P.op("pool", lambda e: e.tensor_tensor(out=r3[:, :, 32:64], in0=v3(t3), in1=v3(t4), op=ALU.add), reads=[d_t3, d_t4], writes=[d_qr]),
        ]
        return steps

    def to_T_and_store(dst_dram, col0, b, sl):
        for c in range(4):
            P.op("pe", lambda e, c=c: e.transpose(out=pkT[:, c, :], in_=qr_[sl][b][:, c * 128:(c + 1) * 128], identity=idb[:]),
                 reads=[d_qr_[sl][b], d_idb], writes=[d_pkT])
        P.op("act", lambda e: e.copy(out=kTs[sl][b][:], in_=pkT[:]), reads=[d_pkT], writes=[d_kTs[sl][b]])
        P.dma("sp", lambda e: e.dma_start(out=dst_dram[:, col0:col0 + 128].rearrange("(c p) t -> p c t", p=128), in_=kTs[sl][b][:]),
              reads=[d_kTs[sl][b]])

    def load(ti):
        xs = ti % NX
        r0 = ti * 128
        P.dma("sp", lambda e: e.dma_start(out=xt[xs][:], in_=xin[r0:r0 + 128, :]), writes=[d_xt[xs]])
        P.dma("sp", lambda e: e.dma_start(out=cst[xs][:], in_=cs[r0:r0 + 128, :]), writes=[d_cst[xs]])

    def norm(ti):
        b = ti % NB
        xs = ti % NX
        hb = hb_[b]
        P.op("act", lambda e: e.activation(out=junk[:], in_=xt[xs][:], func=AF.Square, accum_out=ssq[:]), reads=[d_xt[xs]], writes=[d_junk, d_ssq])
        P.op("act", lambda e: e.activation(out=rstd[:], in_=ssq[:], func=AF.Sqrt, bias=EPS, scale=1.0 / D), reads=[d_ssq], writes=[d_rstd])
        P.op("dve", lambda e: e.reciprocal(out=rstd[:], in_=rstd[:]), reads=[d_rstd], writes=[d_rstd])
        P.op("dve", lambda e: e.scalar_tensor_tensor(out=hb[:], in0=xt[xs][:], scalar=rstd[:, 0:1], in1=gm[:], op0=ALU.mult, op1=ALU.mult),
             reads=[d_xt[xs], d_rstd, d_gm], writes=[d_hb_[b]])

    def front(ti):
        b = ti % NB
        xs = ti % NX
        own = ti >= 32
        hb = hb_[b]
        for k in range(8):
            P.op("pe", lambda e, k=k: e.transpose(out=pT[:, k, :], in_=hb[:, k * 128:(k + 1) * 128], identity=idb[:]), reads=[d_hb_[b], d_idb], writes=[d_pT])
        P.op("act", lambda e: e.copy(out=hT[b][:], in_=pT[:]), reads=[d_pT], writes=[d_hT[b]])
        groups = [0, 1, 2] if own else [1, 2]
        for g in groups:
            for k in range(8):
                P.op("pe", lambda e, g=g, k=k: e.matmul(pq[b][g][:], lhsT=hT[b][:, k, :], rhs=wq[:, k, g * 512:(g + 1) * 512], start=(k == 0), stop=(k == 7)),
                     reads=[d_hT[b], d_wq], writes=[d_pq[b][g]])

    def post(ti):
        b = ti % NB
        xs = ti % NX
        own = ti >= 32
        ks = qk_post_steps(pq[b][1], d_pq[b][1], 1, b, xs)
        qs = qk_post_steps(pq[b][0], d_pq[b][0], 0, b, xs) if own else []
        for i in range(len(ks)):
            ks[i]()
            if qs:
                qs[i]()
        P.op("act", lambda e: e.copy(out=vs[b][:], in_=pq[b][2][:]), reads=[d_pq[b][2]], writes=[d_vs[b]])

    def tail(ti):
        b = ti % NB
        own = ti >= 32
        r0 = ti * 128
        to_T_and_store(kT_d, r0, b, 1)
        if own:
            to_T_and_store(qT_d, r0 - TP, b, 0)
        P.dma("sp", lambda e: e.dma_start(out=v_d[r0:r0 + 128, :], in_=vs[b][:]), reads=[d_vs[b]])

    load(0)
    load(1)
    load(2)
    norm(0)
    norm(1)
    front(0)
    for ti in range(64):
        if ti + 3 < 64:
            load(ti + 3)
        if ti + 2 < 64:
            norm(ti + 2)
        if ti + 1 < 64:
            front(ti + 1)
        if ti >= 1:
            tail(ti - 1)
        post(ti)
    tail(63)


def phase_b(nc, P, st, kT_d, v_d, qT_d, e_d, vq_d, nb_d, oq_d, tri_d, idb, d_idb, oa, d_oa, heads=range(8), nblk=16):
    def sb(name, shape, dt):
        return st.enter_context(nc.sbuf_tensor("b_" + name, shape, dt))

    def ps(name, shape, dt=F32):
        return st.enter_context(nc.psum_tensor(name, shape, dt))
    NB = 2
    kaug = [sb("kaug%d" % i, [96, NK], BF16) for i in range(NB)]; d_kaug = [Dep() for _ in range(NB)]
    vaug = [sb("vaug%d" % i, [128, 64, 65], BF16) for i in range(NB)]; d_vaug = [Dep() for _ in range(NB)]
    qaug = [sb("qaug%d" % i, [96, T], BF16) for i in range(NB)]; d_qaug = [Dep() for _ in range(NB)]
    d_qmb = [Dep() for _ in range(NB)]
    vq = sb("vq_s", [128, 1024], F32); d_c = Dep()
    nb = sb("nb_s", [128, 1024], F32)
    oq = sb("oq_s", [128, 1024], F32)
    tri = sb("tri_s", [128, 128], BF16)
    ksum = sb("ksum", [64, 32], F32); d_ksum = Dep()
    kmean = sb("kmean", [64, 32], BF16); d_kmean = Dep()
    g1 = sb("g1", [128, 512], F32); d_g1 = Dep()
    top8 = sb("top8", [128, 16, 8], F32); d_top8 = Dep()
    sel = sb("sel", [128, 512], F32); d_sel = Dep()
    mbp = sb("mbp", [128, 16, 96], BF16); d_mbp = Dep()
    NPT = 3
    pt = [sb("pt%d" % i, [128, 512], BF16) for i in range(NPT)]; d_pt = [Dep() for _ in range(NPT)]
    rc = sb("rc", [128, 2], F32); d_rc = [Dep(), Dep()]
    pg = ps("pg", [128, 512], F32); d_pg = Dep()
    pmT = ps("pmT", [96, 8, 128], BF16); d_pmT = Dep()
    psS = [ps("psS%d" % i, [128, 512], F32) for i in range(NPT)]; d_psS = [Dep() for _ in range(NPT)]
    po = [ps("po%d" % i, [128, 65], F32) for i in range(2)]; d_po = [Dep() for _ in range(2)]

    for i in range(NB):
        P.dma("sp", lambda e, i=i: e.dma_start(out=kaug[i][64:96, :], in_=e_d[:, :]), writes=[d_kaug[i]])
        P.op("pool", lambda e, i=i: e.memset(vaug[i][:], 1.0), writes=[d_vaug[i]])
    P.dma("sp", lambda e: e.dma_start(out=vq[:], in_=vq_d[0:1, :].partition_broadcast(128)), writes=[d_c])
    P.dma("sp", lambda e: e.dma_start(out=nb[:], in_=nb_d[0:1, :].partition_broadcast(128)), writes=[d_c])
    P.dma("sp", lambda e: e.dma_start(out=oq[:], in_=oq_d[0:1, :].partition_broadcast(128)), writes=[d_c])
    P.dma("pool", lambda e: e.dma_start(out=tri[:], in_=tri_d[:, :]), writes=[d_c])
    P.op("pool", lambda e: e.memset(mbp[:], 0.0), writes=[d_mbp])

    for ih, h in enumerate(heads):
        b = ih % NB
        P.dma("sp", lambda e, b=b, h=h: e.dma_start(out=kaug[b][0:64, :], in_=kT_d[h * 64:(h + 1) * 64, :]), writes=[d_kaug[b]])
        P.dma("sp", lambda e, b=b, h=h: e.dma_start(out=vaug[b][:, :, 0:64],
                                                    in_=v_d[:, h * 64:(h + 1) * 64].rearrange("(t p) d -> p t d", p=128)),
              writes=[d_vaug[b]])
        P.dma("sp", lambda e, b=b, h=h: e.dma_start(out=qaug[b][0:64, :], in_=qT_d[h * 64:(h + 1) * 64, :]), writes=[d_qaug[b]])
        P.op("dve", lambda e, b=b: e.tensor_reduce(out=ksum[:], in_=kaug[b][0:64, :].rearrange("p (b k) -> p b k", k=256),
                                                   axis=AX.X, op=ALU.add), reads=[d_kaug[b]], writes=[d_ksum])
        P.op("dve", lambda e: e.tensor_scalar(out=kmean[:], in0=ksum[:], scalar1=1.0 / 256, scalar2=None, op0=ALU.mult),
             reads=[d_ksum], writes=[d_kmean])
        for half in range(2):
            for j in range(16):
                qt = half * 16 + j
                P.op("pe", lambda e, b=b, j=j, qt=qt: e.matmul(pg[:, j * 32:(j + 1) * 32], lhsT=qaug[b][0:64, qt * 128:(qt + 1) * 128],
                                                              rhs=kmean[:, :], start=True, stop=True),
                     reads=[d_qaug[b], d_kmean], writes=[d_pg])
            cs_ = slice(half * 512, (half + 1) * 512)
            P.op("dve", lambda e, cs_=cs_: e.tensor_tensor(out=g1[:], in0=pg[:], in1=vq[:, cs_], op=ALU.mult),
                 reads=[d_pg, d_c], writes=[d_g1])
            P.op("dve", lambda e, cs_=cs_: e.tensor_tensor(out=g1[:], in0=g1[:], in1=nb[:, cs_], op=ALU.add),
                 reads=[d_g1, d_c], writes=[d_g1])
            for j in range(16):
                P.op("dve", lambda e, j=j: e.max(out=top8[:, j, :], in_=g1[:, j * 32:(j + 1) * 32]), reads=[d_g1], writes=[d_top8])
            P.op("dve", lambda e: e.tensor_tensor(out=sel[:].rearrange("p (j b) -> p j b", b=32),
                                                  in0=g1[:].rearrange("p (j b) -> p j b", b=32),
                                                  in1=top8[:, :, 2:3].to_broadcast([128, 16, 32]), op=ALU.is_ge),
                 reads=[d_g1, d_top8], writes=[d_sel])
            P.op("dve", lambda e, cs_=cs_: e.tensor_tensor(out=sel[:], in0=sel[:], in1=vq[:, cs_], op=ALU.mult),
                 reads=[d_sel, d_c], writes=[d_sel])
            P.op("dve", lambda e, cs_=cs_: e.tensor_tensor(out=sel[:], in0=sel[:], in1=oq[:, cs_], op=ALU.add),
                 reads=[d_sel, d_c], writes=[d_sel])
            P.op("dve", lambda e: e.tensor_scalar(out=mbp[:, :, 64:96], in0=sel[:].rearrange("p (j b) -> p j b", b=32),
                                                  scalar1=1.0, scalar2=MB, op0=ALU.subtract, op1=ALU.mult),
                 reads=[d_sel], writes=[d_mbp])
            for grp in range(2):
                for j in range(8):
                    P.op("pe", lambda e, grp=grp, j=j: e.transpose(out=pmT[:, j, :], in_=mbp[:, grp * 8 + j, :], identity=idb[:]),
                         reads=[d_mbp, d_idb], writes=[d_pmT])
                c0 = (half * 16 + grp * 8) * 128
                P.op("act", lambda e, b=b, c0=c0: e.copy(out=qaug[b][64:96, c0:c0 + 1024], in_=pmT[64:96, :, :]),
                     reads=[d_pmT], writes=[d_qmb[b]])
        units = []
        for jb in range(nblk):
            ncommon = 32 + 2 * jb
            for u in range(ncommon // 2):
                units.append((jb, [2 * u, 2 * u + 1], False, u == 0))
            units.append((jb, [32 + 2 * jb, 32 + 2 * jb + 1], True, False))

        def emit_S(u, s, b=b):
            jb, kts, diag, first = u
            for j, kt in enumerate(kts):
                if diag and j == 1:
                    q0, nq, c0 = jb * 256 + 128, 128, 256
                else:
                    q0, nq, c0 = jb * 256, 256, j * 256
                P.op("pe", lambda e, s=s, kt=kt, q0=q0, nq=nq, c0=c0: e.matmul(psS[s][:, c0:c0 + nq], lhsT=kaug[b][0:96, kt * 128:(kt + 1) * 128],
                                                                             rhs=qaug[b][0:96, q0:q0 + nq], start=True, stop=True),
                     reads=[d_kaug[b], d_qaug[b], d_qmb[b]], writes=[d_psS[s]])

        def emit_rest(u, s, b=b, h=h):
            jb, kts, diag, first = u
            ncols = 384 if diag else 512
            P.op("act", lambda e, s=s, ncols=ncols: e.activation(out=pt[s][:, 0:ncols], in_=psS[s][:, 0:ncols], func=AF.Exp),
                 reads=[d_psS[s]], writes=[d_pt[s]])
            if diag:
                P.op("pool", lambda e, s=s: e.tensor_tensor(out=pt[s][:, 0:128], in0=pt[s][:, 0:128], in1=tri[:], op=ALU.mult),
                     reads=[d_pt[s], d_c], writes=[d_pt[s]])
                P.op("pool", lambda e, s=s: e.tensor_tensor(out=pt[s][:, 256:384], in0=pt[s][:, 256:384], in1=tri[:], op=ALU.mult),
                     reads=[d_pt[s], d_c], writes=[d_pt[s]])
            for j, kt in enumerate(kts):
                if diag and j == 1:
                    pv = [(1, 256, True)]
                elif diag:
                    pv = [(0, 0, True), (1, 128, False)]
                else:
                    pv = [(0, j * 256, False), (1, j * 256 + 128, False)]
                st_ = first and j == 0
                for qi, col0, last in pv:
                    P.op("pe", lambda e, s=s, kt=kt, qi=qi, col0=col0, st_=st_, last=last: e.matmul(
                        po[qi][:, :], lhsT=pt[s][:, col0:col0 + 128], rhs=vaug[b][:, kt, :], start=st_, stop=last),
                        reads=[d_pt[s], d_vaug[b]], writes=[d_po[qi]])
            if diag:
                for qi in range(2):
                    qt = jb * 2 + qi
                    P.op("dve", lambda e, qi=qi: e.reciprocal(out=rc[:, qi:qi + 1], in_=po[qi][:, 64:65]), reads=[d_po[qi]], writes=[d_rc[qi]])
                    P.op("dve", lambda e, qi=qi, qt=qt: e.tensor_scalar(out=oa[:, qt, h * 64:(h + 1) * 64], in0=po[qi][:, 0:64],
                                                                       scalar1=rc[:, qi:qi + 1], scalar2=None, op0=ALU.mult),
                         reads=[d_po[qi], d_rc[qi]], writes=[d_oa])
        SK = 2
        n = len(units)
        for i in range(n + SK):
            if i < n:
                emit_S(units[i], i % NPT)
            if i >= SK:
                emit_rest(units[i - SK], (i - SK) % NPT)


def host_consts(half):
    pv = 1.0 if half == 1 else 0.0
    V = np.zeros((32, 32), np.float32); O = np.zeros((32, 32), np.float32)
    for qt in range(32):
        jb = qt // 2
        V[qt, :16] = pv
        V[qt, 16:16 + jb] = 1.0
        O[qt, 16 + jb] = 1.0
    NBm = (V - 1.0) * BIGG
    E = np.zeros((32, NK), np.float32)
    for b in range(32):
        E[b, b * 256:(b + 1) * 256] = 1.0
    tri = (np.arange(128)[:, None] <= np.arange(128)[None, :]).astype(np.float32)
    return dict(vq=V.reshape(1, 1024), nb=NBm.reshape(1, 1024), oq=O.reshape(1, 1024),
                e_d=E.astype(ml_dtypes.bfloat16), tri=tri.astype(ml_dtypes.bfloat16))


def phase_c(nc, P, st, xin, w_in, g_mix, cwl_d, cbl_d, dtb_d, alog_d, dsk_d, ssdn_d, tri2_d, sel_d, tb_d, pflag_d,
            idb, d_idb, os_d, d_osd, chunks=range(32)):
    def sb(name, shape, dt):
        return st.enter_context(nc.sbuf_tensor("c_" + name, shape, dt))

    def ps(name, shape, dt=F32):
        return st.enter_context(nc.psum_tensor(name, shape, dt))
    wz = sb("wz", [128, 8, 1024], BF16); d_w = Dep()
    wx = sb("wx", [128, 8, 2048], BF16)
    wdt = sb("wdt", [128, 8, 16], BF16)
    gm = sb("gmc", [128, D], F32); d_c = Dep()
    cw = sb("cw", [128, 16, 4], F32)
    cb = sb("cb", [128, 16], F32)
    dtb = sb("dtb", [128, 2, 16], F32)
    Aneg = sb("Aneg", [128, 2, 16], F32); d_A = Dep()
    dsk = sb("dsk", [128, 16], F32)
    ssdn = sb("ssdn", [128, D], F32)
    tri2 = sb("tri2", [128, 2, 256], F32)
    onesf = sb("onesf", [128, 128], F32)
    sel = sb("selc", [16, 16, 128], F32)
    tb = sb("tbc", [128, 384], F32)
    pflag = sb("pflag", [128, 1], F32)
    xt = sb("xtc", [128, 2, D], F32); d_xt = Dep()
    junk = sb("junkc", [128, D], BF16); d_junk = Dep()
    ssq = sb("ssqc", [128, 2], F32); d_ssq = Dep()
    rstd = sb("rstdc", [128, 2], F32); d_rstd = Dep()
    hb = sb("hbc", [128, 2, D], BF16); d_hb = Dep()
    hT = sb("hTc", [128, 8, 256], BF16); d_hT = Dep()
    xraw = sb("xraw", [128, 16, 259], F32); d_xraw = Dep(); d_halo = Dep()
    acc = sb("acc", [128, 8, 256], F32); d_acc = [Dep() for _ in range(8)]
    xc = sb("xc", [128, 16, 256], BF16); d_xc = [Dep() for _ in range(16)]
    xtok = sb("xtok", [128, 2, 1024], BF16); d_xtok = Dep()
    btok = sb("btok", [128, 2, 512], BF16); d_btok = Dep()
    dtr = sb("dtr", [128, 2, 16], F32); d_dtr = Dep()
    dt = sb("dt", [128, 2, 16], F32); d_dt = Dep()
    aa = sb("aa", [128, 2, 16], F32); d_aa = Dep()
    acum = sb("acum", [128, 2, 16], F32); d_acum = Dep()
    nacum = sb("nacum", [128, 2, 16], F32); d_nacum = Dep()
    acumT = sb("acumT", [16, 256], F32); d_acumT = Dep()
    tot = sb("tot", [128, 16], F32); d_tot = Dep()
    cdec = sb("cdec", [128, 16], F32); d_cdec = Dep()
    dte = sb("dte", [128, 2, 16], F32); d_dte = Dep()
    eac = sb("eac", [128, 2, 16], F32); d_eac = Dep()
    w2 = sb("w2", [128, 2, 16], F32); d_w2 = Dep()
    xdt = sb("xdt", [128, 2, 1024], BF16); d_xdt = Dep()
    xdd = sb("xdd", [128, 2, 1024], BF16); d_xdd = Dep()
    zs = sb("zs", [128, 2, 1024], BF16); d_zs = Dep()
    cbT = sb("cbT", [128, 4, 384], F32); d_cbT = [Dep() for _ in range(4)]
    arg = [sb("arg%d" % i, [128, 384], F32) for i in range(2)]; d_arg = [Dep(), Dep()]
    Lt = [sb("Lt%d" % i, [128, 384], F32) for i in range(2)]; d_Lt = [Dep(), Dep()]
    Mt = [sb("Mt%d" % i, [128, 384], BF16) for i in range(2)]; d_Mt = [Dep() for _ in range(2)]
    stf = sb("stf", [128, 4, 256], F32); d_stf = Dep()
    stT = sb("stT", [128, 4, 256], BF16); d_stT = Dep()
    yt = [sb("yt%d" % i, [128, 256], F32) for i in range(2)]; d_yt = [Dep(), Dep()]
    y2 = [sb("y2%d" % i, [128, 256], F32) for i in range(2)]; d_y2 = [Dep(), Dep()]
    gss = [sb("gss%d" % i, [128, 1], F32) for i in range(2)]; d_gss = [Dep(), Dep()]
    grs = [sb("grs%d" % i, [128, 1], F32) for i in range(2)]; d_grs = [Dep(), Dep()]
    osb = sb("osb", [128, 2, 1024], BF16); d_osb = Dep()

    B = [ps("bk%d" % i, [128, 512], F32) for i in range(7)]
    d_B = [Dep() for _ in range(7)]
    d_B1h = [Dep(), Dep()]
    d_B5h = [Dep(), Dep()]
    pT = ps("pTc", [128, 8, 128], BF16); d_pT = Dep()

    for k in range(8):
        rows = slice(k * 128, (k + 1) * 128)
        P.dma("pool", lambda e, k=k, rows=rows: e.dma_start(out=wz[:, k, :], in_=w_in[rows, C_Z:C_Z + 1024]), writes=[d_w])
        P.dma("pool", lambda e, k=k, rows=rows: e.dma_start(out=wx[:, k, :], in_=w_in[rows, C_X:C_X + 2048]), writes=[d_w])
        P.dma("pool", lambda e, k=k, rows=rows: e.dma_start(out=wdt[:, k, :], in_=w_in[rows, C_DT:C_DT + 16]), writes=[d_w])
    P.dma("sp", lambda e: e.dma_start(out=gm[:], in_=g_mix[0:1, :].partition_broadcast(128)), writes=[d_c])
    P.dma("sp", lambda e: e.dma_start(out=cw[:], in_=cwl_d[:, :, :]), writes=[d_c])
    P.dma("sp", lambda e: e.dma_start(out=cb[:], in_=cbl_d[:, :]), writes=[d_c])
    for i in range(2):
        P.dma("sp", lambda e, i=i: e.dma_start(out=dtb[:, i, :], in_=dtb_d[0:1, :].partition_broadcast(128)), writes=[d_c])
        P.dma("sp", lambda e, i=i: e.dma_start(out=Aneg[:, i, :], in_=alog_d[0:1, :].partition_broadcast(128)), writes=[d_A])
    P.dma("sp", lambda e: e.dma_start(out=dsk[:], in_=dsk_d[0:1, :].partition_broadcast(128)), writes=[d_c])
    P.dma("sp", lambda e: e.dma_start(out=ssdn[:], in_=ssdn_d[0:1, :].partition_broadcast(128)), writes=[d_c])
    P.dma("sp", lambda e: e.dma_start(out=tri2[:], in_=tri2_d[:, :, :]), writes=[d_c])
    P.dma("sp", lambda e: e.dma_start(out=sel[:], in_=sel_d[:, :, :]), writes=[d_c])
    P.dma("sp", lambda e: e.dma_start(out=tb[:], in_=tb_d[:, :]), writes=[d_c])
    P.dma("sp", lambda e: e.dma_start(out=pflag[:], in_=pflag_d[0:1, :].partition_broadcast(128)), writes=[d_c])
    P.op("pool", lambda e: e.memset(onesf[:], 1.0), writes=[d_c])
    P.op("act", lambda e: e.activation(out=Aneg[:], in_=Aneg[:], func=AF.Exp), reads=[d_A], writes=[d_A])
    P.op("dve", lambda e: e.tensor_scalar(out=Aneg[:], in0=Aneg[:], scalar1=-1.0, scalar2=None, op0=ALU.mult), reads=[d_A], writes=[d_A])
    P.op("pool", lambda e: e.memset(xraw[:], 0.0), writes=[d_xraw, d_halo])
    P.op("pool", lambda e: e.memset(stf[:], 0.0), writes=[d_stf])
    P.op("pool", lambda e: e.memset(stT[:], 0.0), writes=[d_stT])

    h3 = lambda ap: ap.rearrange("p (h d) -> p h d", d=64)

    for c in chunks:
        own = c >= 16
        r0 = c * 256
        P.dma("sp", lambda e, r0=r0: e.dma_start(out=xt[:], in_=xin[r0:r0 + 256, :].rearrange("(t p) d -> p t d", p=128)), writes=[d_xt])
        for it in range(2):
            P.op("act", lambda e, it=it: e.activation(out=junk[:], in_=xt[:, it, :], func=AF.Square, accum_out=ssq[:, it:it + 1]),
                 reads=[d_xt], writes=[d_junk, d_ssq])
        P.op("act", lambda e: e.activation(out=rstd[:], in_=ssq[:], func=AF.Sqrt, bias=EPS, scale=1.0 / D), reads=[d_ssq], writes=[d_rstd])
        P.op("dve", lambda e: e.reciprocal(out=rstd[:], in_=rstd[:]), reads=[d_rstd], writes=[d_rstd])
        for it in range(2):
            P.op("dve", lambda e, it=it: e.scalar_tensor_tensor(out=hb[:, it, :], in0=xt[:, it, :], scalar=rstd[:, it:it + 1], in1=gm[:],
                                                                                    op0=ALU.mult, op1=ALU.mult),
                 reads=[d_xt, d_rstd, d_c], writes=[d_hb])
        for it in range(2):
            for k in range(8):
                P.op("pe", lambda e, it=it, k=k: e.transpose(out=pT[:, k, :], in_=hb[:, it, k * 128:(k + 1) * 128], identity=idb[:]),
                     reads=[d_hb, d_idb], writes=[d_pT])
            P.op("act", lambda e, it=it: e.copy(out=hT[:, :, it * 128:(it + 1) * 128], in_=pT[:]), reads=[d_pT], writes=[d_hT])
        for it in range(2):
            for k in range(8):
                P.op("pe", lambda e, it=it, k=k: e.matmul(B[3][:, it * 16:(it + 1) * 16], lhsT=hT[:, k, it * 128:(it + 1) * 128], rhs=wdt[:, k, :],
                                                         start=(k == 0), stop=(k == 7)), reads=[d_hT, d_w], writes=[d_B[3]])
        P.op("dve", lambda e: e.tensor_tensor(out=dtr[:].rearrange("p a b -> p (a b)"), in0=B[3][:, 0:32], in1=dtb[:].rearrange("p a b -> p (a b)"), op=ALU.add),
             reads=[d_B[3], d_c], writes=[d_dtr])
        P.op("act", lambda e: e.activation(out=dtr[:], in_=dtr[:], func=AF.Exp), reads=[d_dtr], writes=[d_dtr])
        P.op("act", lambda e: e.activation(out=dt[:], in_=dtr[:], func=AF.Ln, bias=1.0), reads=[d_dtr], writes=[d_dt])
        P.op("dve", lambda e: e.tensor_tensor(out=aa[:], in0=dt[:], in1=Aneg[:], op=ALU.mult), reads=[d_dt, d_A], writes=[d_aa])
        for it in range(2):
            for jt in range(it + 1):
                P.op("pe", lambda e, it=it, jt=jt: e.matmul(B[3][:, 32 + it * 16:32 + (it + 1) * 16], lhsT=tri2[:, jt, it * 128:(it + 1) * 128],
                                                           rhs=aa[:, jt, :], start=(jt == 0), stop=(jt == it)),
                     reads=[d_aa, d_c], writes=[d_B[3]])
        for jt in range(2):
            P.op("pe", lambda e, jt=jt: e.matmul(B[3][:, 64:80], lhsT=onesf[:], rhs=aa[:, jt, :], start=(jt == 0), stop=(jt == 1)),
                 reads=[d_aa, d_c], writes=[d_B[3]])
        for jt in range(2):
            P.op("pe", lambda e, jt=jt: e.matmul(B[3][0:16, 128:384], lhsT=aa[:, jt, :], rhs=tri2[:, jt, :], start=(jt == 0), stop=(jt == 1)),
                 reads=[d_aa, d_c], writes=[d_B[3]])
        P.op("act", lambda e: e.copy(out=acum[:].rearrange("p a b -> p (a b)"), in_=B[3][:, 32:64]), reads=[d_B[3]], writes=[d_acum])
        P.op("dve", lambda e: e.tensor_scalar(out=nacum[:].rearrange("p a b -> p (a b)"), in0=B[3][:, 32:64], scalar1=-1.0, scalar2=None, op0=ALU.mult),
             reads=[d_B[3]], writes=[d_nacum])
        P.op("act", lambda e: e.copy(out=tot[:], in_=B[3][:, 64:80]), reads=[d_B[3]], writes=[d_tot])
        P.op("act", lambda e: e.copy(out=acumT[:], in_=B[3][0:16, 128:384]), reads=[d_B[3]], writes=[d_acumT])
        P.op("act", lambda e: e.activation(out=cdec[:], in_=tot[:], func=AF.Exp), reads=[d_tot], writes=[d_cdec])
        P.op("dve", lambda e: e.tensor_tensor(out=dte[:], in0=nacum[:], in1=tot[:].unsqueeze(1).to_broadcast([128, 2, 16]), op=ALU.add),
             reads=[d_nacum, d_tot], writes=[d_dte])
        P.op("act", lambda e: e.activation(out=dte[:], in_=dte[:], func=AF.Exp), reads=[d_dte], writes=[d_dte])
        P.op("act", lambda e: e.activation(out=eac[:], in_=acum[:], func=AF.Exp), reads=[d_acum], writes=[d_eac])
        P.op("dve", lambda e: e.tensor_tensor(out=w2[:], in0=dt[:], in1=dte[:], op=ALU.mult), reads=[d_dt, d_dte], writes=[d_w2])
        nfc = 16 if (own or c == 15) else 12
        for fc in range(nfc):
            bi = 1 if fc % 2 == 0 else 4
            for k in range(8):
                P.op("pe", lambda e, fc=fc, k=k, bi=bi: e.matmul(B[bi][:, 0:256], lhsT=wx[:, k, fc * 128:(fc + 1) * 128], rhs=hT[:, k, :],
                                                               start=(k == 0), stop=(k == 7)), reads=[d_hT, d_w], writes=[d_B[bi]])
            P.op("act", lambda e, fc=fc, bi=bi: e.copy(out=xraw[:, fc, 3:259], in_=B[bi][:, 0:256]),
                 reads=[d_B[bi]], writes=[d_xraw])
        if own:
            for it in range(2):
                for hv in range(2):
                    zb = 2 if hv == 0 else 6
                    for k in range(8):
                        P.op("pe", lambda e, it=it, hv=hv, k=k, zb=zb: e.matmul(B[zb][:, :], lhsT=hT[:, k, it * 128:(it + 1) * 128], rhs=wz[:, k, hv * 512:(hv + 1) * 512],
                                                                              start=(k == 0), stop=(k == 7)), reads=[d_hT, d_w], writes=[d_B[zb]])
                    P.op("act", lambda e, it=it, hv=hv, zb=zb: e.activation(out=zs[:, it, hv * 512:(hv + 1) * 512], in_=B[zb][:, :], func=AF.Silu),
                         reads=[d_B[zb]], writes=[d_zs])
        for half in range(2):
            n8 = 8 if (half == 0 or nfc == 16) else 4
            for f8 in range(n8):
                fc = half * 8 + f8
                P.op("dve", lambda e, fc=fc, f8=f8: e.tensor_scalar(out=acc[:, f8, :], in0=xraw[:, fc, 0:256], scalar1=cw[:, fc, 0:1], scalar2=cb[:, fc:fc + 1],
                                                                   op0=ALU.mult, op1=ALU.add), reads=[d_xraw, d_halo, d_c], writes=[d_acc[f8]])
            for kk in range(1, 4):
                for f8 in range(n8):
                    fc = half * 8 + f8
                    P.op("dve", lambda e, fc=fc, f8=f8, kk=kk: e.scalar_tensor_tensor(out=acc[:, f8, :], in0=xraw[:, fc, kk:kk + 256], scalar=cw[:, fc, kk:kk + 1],
                                                                                     in1=acc[:, f8, :], op0=ALU.mult, op1=ALU.add),
                         reads=[d_xraw, d_halo, d_c, d_acc[f8]], writes=[d_acc[f8]])
            for f8 in range(n8):
                fc = half * 8 + f8
                P.op("act", lambda e, fc=fc, f8=f8: e.activation(out=xc[:, fc, :], in_=acc[:, f8, :], func=AF.Silu),
                     reads=[d_acc[f8]], writes=[d_xc[fc]])
        P.op("pool", lambda e: e.tensor_copy(out=xraw[:, :, 0:3], in_=xraw[:, :, 256:259]), reads=[d_xraw], writes=[d_halo])
        for it in range(2):
            for fc in range(8):
                P.op("pe", lambda e, it=it, fc=fc: e.transpose(out=pT[:, fc, :], in_=xc[:, fc, it * 128:(it + 1) * 128], identity=idb[:]),
                     reads=[d_xc[fc], d_idb], writes=[d_pT])
            P.op("act", lambda e, it=it: e.copy(out=xtok[:, it, :], in_=pT[:]), reads=[d_pT], writes=[d_xtok])
            for g in range(4):
                P.op("pe", lambda e, it=it, g=g: e.transpose(out=pT[:, g, :], in_=xc[:, 8 + g, it * 128:(it + 1) * 128], identity=idb[:]),
                     reads=[d_xc[8 + g], d_idb], writes=[d_pT])
            P.op("act", lambda e, it=it: e.copy(out=btok[:, it, :], in_=pT[:, 0:4, :]), reads=[d_pT], writes=[d_btok])
        for it in range(2):
            if own:
                P.op("dve", lambda e, it=it: e.tensor_tensor(out=h3(xdt[:, it, :]), in0=h3(xtok[:, it, :]),
                                                             in1=dt[:, it, :].unsqueeze(2).to_broadcast([128, 16, 64]), op=ALU.mult),
                     reads=[d_xtok, d_dt], writes=[d_xdt])
            P.op("pool", lambda e, it=it: e.tensor_tensor(out=h3(xdd[:, it, :]), in0=h3(xtok[:, it, :]),
                                                          in1=w2[:, it, :].unsqueeze(2).to_broadcast([128, 16, 64]), op=ALU.mult),
                 reads=[d_xtok, d_w2], writes=[d_xdd])
        if own:
            for g in range(4):
                P.op("pe", lambda e, g=g: e.matmul(B[4][:, 0:256], lhsT=xc[:, 8 + g, 0:128], rhs=xc[:, 12 + g, :], start=True, stop=True),
                     reads=[d_xc[8 + g], d_xc[12 + g]], writes=[d_B[4]])
                P.op("pe", lambda e, g=g: e.matmul(B[4][:, 256:384], lhsT=xc[:, 8 + g, 128:256], rhs=xc[:, 12 + g, 128:256], start=True, stop=True),
                     reads=[d_xc[8 + g], d_xc[12 + g]], writes=[d_B[4]])
                P.op("act", lambda e, g=g: e.copy(out=cbT[:, g, :], in_=B[4][:, 0:384]), reads=[d_B[4]], writes=[d_cbT[g]])
            for g in range(4):
                def stA(h, g=g):
                    mi = h % 2
                    bk = 5 if mi == 0 else 4
                    P.op("pe", lambda e: e.matmul(B[bk][:, 0:256], lhsT=sel[:, h, :], rhs=acumT[:, :], start=True, stop=True),
                         reads=[d_c, d_acumT], writes=[d_B[bk]])
                    P.op("dve", lambda e: e.scalar_tensor_tensor(out=arg[mi][:, 0:256], in0=B[bk][:, 0:256], scalar=nacum[:, 0, h:h + 1], in1=tb[:, 0:256],
                                                                 op0=ALU.add, op1=ALU.add), reads=[d_B[bk], d_nacum, d_c], writes=[d_arg[mi]])
                    P.op("dve", lambda e: e.scalar_tensor_tensor(out=arg[mi][:, 256:384], in0=B[bk][:, 128:256], scalar=nacum[:, 1, h:h + 1], in1=tb[:, 256:384],
                                                                 op0=ALU.add, op1=ALU.add), reads=[d_B[bk], d_nacum, d_c], writes=[d_arg[mi]])
                    P.op("act", lambda e: e.activation(out=Lt[mi][:], in_=arg[mi][:], func=AF.Exp), reads=[d_arg[mi]], writes=[d_Lt[mi]])
                    P.op("pool", lambda e: e.tensor_tensor(out=Mt[mi][:], in0=Lt[mi][:], in1=cbT[:, g, :], op=ALU.mult),
                         reads=[d_Lt[mi], d_cbT[g]], writes=[d_Mt[mi]])

                def stB(h, g=g):
                    mi = h % 2
                    r = h % 4
                    cs_ = slice(r * 64, (r + 1) * 64)
                    hs_ = slice(h * 64, (h + 1) * 64)
                    P.op("pe", lambda e: e.matmul(B[6][:, 0:256][:, cs_], lhsT=Mt[mi][:, 0:128], rhs=xdt[:, 0, hs_], start=True, stop=True),
                         reads=[d_Mt[mi], d_xdt], writes=[d_B[6]])
                    P.op("pe", lambda e: e.matmul(B[6][:, 256:512][:, cs_], lhsT=Mt[mi][:, 128:256], rhs=xdt[:, 0, hs_], start=True, stop=False),
                         reads=[d_Mt[mi], d_xdt], writes=[d_B[6]])
                    P.op("pe", lambda e: e.matmul(B[6][:, 256:512][:, cs_], lhsT=Mt[mi][:, 256:384], rhs=xdt[:, 1, hs_], start=False, stop=True),
                         reads=[d_Mt[mi], d_xdt], writes=[d_B[6]])
                hs4 = [g * 4 + r for r in range(4)]
                stA(hs4[0]); stA(hs4[1]); stB(hs4[0]); stA(hs4[2]); stB(hs4[1]); stA(hs4[3]); stB(hs4[2]); stB(hs4[3])
                gs_ = slice(g * 256, (g + 1) * 256)
                for it in range(2):
                    P.op("pe", lambda e, g=g, it=it: e.matmul(B[0][:, it * 256:(it + 1) * 256], lhsT=xc[:, 12 + g, it * 128:(it + 1) * 128], rhs=stT[:, g, :],
                                                             start=True, stop=True), reads=[d_xc[12 + g], d_stT], writes=[d_B[0]])
                v4 = lambda ap: ap.rearrange("p (r d) -> p r d", d=64)

                def ystep(step, it, g=g, gs_=gs_):
                    if step == 0:
                        P.op("dve", lambda e: e.tensor_tensor(out=v4(yt[it][:]), in0=v4(B[0][:, it * 256:(it + 1) * 256]),
                                                              in1=eac[:, it, g * 4:(g + 1) * 4].unsqueeze(2).to_broadcast([128, 4, 64]), op=ALU.mult),
                             reads=[d_B[0], d_eac], writes=[d_yt[it]])
                    elif step == 1:
                        P.op("dve", lambda e: e.tensor_tensor(out=yt[it][:], in0=yt[it][:], in1=B[6][:, it * 256:(it + 1) * 256], op=ALU.add),
                             reads=[d_yt[it], d_B[6]], writes=[d_yt[it]])
                    elif step == 2:
                        P.op("pool", lambda e: e.tensor_tensor(out=v4(y2[it][:]), in0=v4(xtok[:, it, gs_]),
                                                               in1=dsk[:, g * 4:(g + 1) * 4].unsqueeze(2).to_broadcast([128, 4, 64]), op=ALU.mult),
                             reads=[d_xtok, d_c], writes=[d_y2[it]])
                    elif step == 3:
                        P.op("dve", lambda e: e.tensor_tensor(out=yt[it][:], in0=yt[it][:], in1=y2[it][:], op=ALU.add), reads=[d_yt[it], d_y2[it]], writes=[d_yt[it]])
                    elif step == 4:
                        P.op("dve", lambda e: e.tensor_tensor(out=yt[it][:], in0=yt[it][:], in1=zs[:, it, gs_], op=ALU.mult), reads=[d_yt[it], d_zs], writes=[d_yt[it]])
                    elif step == 5:
                        P.op("act", lambda e: e.activation(out=y2[it][:], in_=yt[it][:], func=AF.Square, accum_out=gss[it][:]), reads=[d_yt[it], d_y2[it]], writes=[d_y2[it], d_gss[it]])
                    elif step == 6:
                        P.op("act", lambda e: e.activation(out=grs[it][:], in_=gss[it][:], func=AF.Sqrt, bias=EPS, scale=1.0 / 256), reads=[d_gss[it]], writes=[d_grs[it]])
                    elif step == 7:
                        P.op("dve", lambda e: e.reciprocal(out=grs[it][:], in_=grs[it][:]), reads=[d_grs[it]], writes=[d_grs[it]])
                    elif step == 8:
                        P.op("dve", lambda e: e.scalar_tensor_tensor(out=osb[:, it, gs_], in0=yt[it][:], scalar=grs[it][:, 0:1], in1=ssdn[:, gs_],
                                                                     op0=ALU.mult, op1=ALU.mult), reads=[d_yt[it], d_grs[it], d_c], writes=[d_osb])
                for step in range(9):
                    for it in range(2):
                        ystep(step, it)
            P.dma("sp", lambda e, r0=r0: e.dma_start(out=os_d[r0 - TP:r0 - TP + 256, :].rearrange("(t p) c -> p t c", p=128), in_=osb[:]),
                  reads=[d_osb], writes=[d_osd])
        for g in range(4):
            bank = B[2] if g < 2 else B[1]
            dbank = d_B[2] if g < 2 else d_B[1]
            cs_ = slice((g % 2) * 256, (g % 2 + 1) * 256)
            for jt in range(2):
                P.op("pe", lambda e, g=g, jt=jt, bank=bank, cs_=cs_: e.matmul(bank[:, cs_], lhsT=btok[:, jt, g * 128:(g + 1) * 128], rhs=xdd[:, jt, g * 256:(g + 1) * 256],
                                                                            start=(jt == 0), stop=(jt == 1)), reads=[d_btok, d_xdd], writes=[dbank])
        v16 = lambda ap: ap.rearrange("p (h d) -> p h d", d=64)
        P.op("dve", lambda e: e.tensor_tensor(out=v16(stf[:].rearrange("p g c -> p (g c)")), in0=v16(stf[:].rearrange("p g c -> p (g c)")),
                                              in1=cdec[:].unsqueeze(2).to_broadcast([128, 16, 64]), op=ALU.mult), reads=[d_stf, d_cdec], writes=[d_stf])
        P.op("dve", lambda e: e.tensor_tensor(out=stf[:, 0:2, :].rearrange("p g c -> p (g c)"), in0=stf[:, 0:2, :].rearrange("p g c -> p (g c)"), in1=B[2][:, :], op=ALU.add),
             reads=[d_stf, d_B[2]], writes=[d_stf])
        P.op("dve", lambda e: e.tensor_tensor(out=stf[:, 2:4, :].rearrange("p g c -> p (g c)"), in0=stf[:, 2:4, :].rearrange("p g c -> p (g c)"), in1=B[1][:, :], op=ALU.add),
             reads=[d_stf, d_B[1]], writes=[d_stf])
        if c == 15:
            P.op("dve", lambda e: e.tensor_scalar(out=stf[:], in0=stf[:], scalar1=pflag[:, 0:1], scalar2=None, op0=ALU.mult), reads=[d_stf, d_c], writes=[d_stf])
        P.op("act", lambda e: e.copy(out=stT[:], in_=stf[:]), reads=[d_stf], writes=[d_stT])


def host_consts_c(z, half):
    cwl = np.ascontiguousarray(z['conv_w'].reshape(4, 16, 128).transpose(2, 1, 0)).astype(np.float32)
    cbl = np.ascontiguousarray(z['conv_b'].reshape(16, 128).T).astype(np.float32)
    tri = (np.arange(128)[:, None] <= np.arange(128)[None, :]).astype(np.float32)
    tri2 = np.zeros((128, 2, 256), np.float32)
    tri2[:, 0, 0:128] = tri; tri2[:, 0, 128:256] = 1.0; tri2[:, 1, 128:256] = tri
    sel = np.zeros((16, 16, 128), np.float32)
    for h in range(16):
        sel[h, h, :] = 1.0
    tbias = np.where(tri > 0, 0.0, NEG).astype(np.float32)
    tb = np.zeros((128, 384), np.float32)
    tb[:, 0:128] = tbias; tb[:, 256:384] = tbias
    return dict(cwl=cwl, cbl=cbl, dtb=z['dt_bias'][None, :], alog=z['a_log'][None, :], dsk=z['d_skip'][None, :], ssdn=z['ssd_norm'][None, :],
                tri2=tri2, sel_d=sel, tb_d=tb, pflag=np.array([[1.0 if half == 1 else 0.0]], np.float32))


def phase_def(nc, P, xown, w_in, g_mix, mem_d, gmem_d, wmemkv, mqn_d, mkn_d, woa_d, wos_d, wom_d, wout_d, gffn_d, wr_d,
              wgate_d, wup_d, wdown_d, tri_d, ecap_d, oa_d, d_oad, os_d, d_osd, x1_d, xbuf, ybuf, out_d, idb, d_idb, idf, d_idf,
              ntiles=32, experts=range(32), stage=3):
    d_x1d = Dep(); d_xbuf = Dep(); d_ybuf = Dep(); d_out = Dep()
    with contextlib.ExitStack() as stp:
        def sbp(name, shape, dt):
            return stp.enter_context(nc.sbuf_tensor("d_" + name, shape, dt))
        combs = sbp("combs", [128, 32, 2], F32); d_combs = Dep()
        idxs = sbp("idxs", [128, 32, 2], I32); d_idxs = Dep()

        with contextlib.ExitStack() as st:
            def sb(name, shape, dt):
                return st.enter_context(nc.sbuf_tensor("d_" + name, shape, dt))

            def ps(name, shape, dt=F32):
                return st.enter_context(nc.psum_tensor(name, shape, dt))
            d_w = Dep(); d_c = Dep()
            gm = sb("gmd", [128, D], F32)
            gf = sb("gfd", [128, D], F32)
            mqn = sb("mqn", [128, 512], F32)
            mkn = sb("mkn", [128, 512], F32)
            tri = sb("trid", [128, 128], F32)
            onesf = sb("onesfd", [128, 128], F32)
            onesb = sb("onesbd", [128, 128], BF16)
            ecap = sb("ecap", [128, 32], F32)
            base = sb("base", [128, 32], F32); d_base = Dep()
            kmT = sb("kmT", [128, 4, 256], BF16); d_kmT = Dep()
            vm = sb("vm", [128, 2, 512], BF16); d_vm = Dep()

            xt = [sb("xtd%d" % i, [128, D], F32) for i in range(2)]; d_xt = [Dep(), Dep()]
            junk = sb("junkd", [128, D], BF16); d_junk = Dep()
            ssq = sb("ssqd", [128, 1], F32); d_ssq = Dep()
            rstd = sb("rstdd", [128, 1], F32); d_rstd = Dep()
            hb = sb("hbd", [128, D], BF16); d_hb = Dep()
            hT = sb("hTd", [128, 8, 128], BF16); d_hT = Dep()
            sq = sb("sqd", [128, 512], F32); d_sq = Dep()
            hs = sb("hsd", [128, 4], F32); d_hs = Dep()
            hr = sb("hrd", [128, 4], F32); d_hr = Dep()
            qmn = sb("qmn", [128, 512], F32); d_qmn = Dep()
            qmb = sb("qmb", [128, 512], BF16); d_qmb = Dep()
            qmT = sb("qmT", [128, 4, 128], BF16); d_qmT = Dep()
            pm = sb("pm", [128, 4, 2, 128], BF16); d_pm = Dep()
            rcp = sb("rcp", [128, 512], F32); d_rcp = Dep()
            omT = sb("omT", [128, 4, 128], BF16); d_omT = Dep()
            gs = sb("gs", [128, 3072], F32); d_gs = Dep()
            oat = [sb("oat%d" % i, [128, 512], BF16) for i in range(2)]; d_oat = [Dep(), Dep()]
            ost = [sb("ost%d" % i, [128, 1024], BF16) for i in range(2)]; d_ost = [Dep(), Dep()]
            oaT = sb("oaT", [128, 4, 128], BF16); d_oaT = Dep()
            osT = sb("osT", [128, 8, 128], BF16); d_osT = Dep()
            mg = sb("mg", [128, 512], F32); d_mg = Dep()
            tt = sb("tt", [128, 512], F32); d_tt = Dep()
            mgb = sb("mgb", [128, D], BF16); d_mgb = Dep()
            mgT = sb("mgT", [128, 8, 128], BF16); d_mgT = Dep()
            x1 = sb("x1", [128, D], F32); d_x1 = Dep()
            h2f = sb("h2f", [128, D], F32); d_h2f = Dep()
            h2b = [sb("h2b%d" % i, [128, D], BF16) for i in range(2)]; d_h2b = [Dep() for _ in range(2)]
            h2T = sb("h2T", [128, 8, 128], F32); d_h2T = Dep()
            lg = sb("lg", [128, 36], F32); d_lg = Dep()
            sm = sb("sm", [128, 64], F32); d_sm = Dep()
            goh = sb("goh", [128, 4], F32); d_goh = Dep()
            lsel = sb("lsel", [128, 32], F32); d_lsel = Dep()
            les = sb("les", [128, 8], F32); d_les = Dep()
            t8 = sb("t8", [128, 8], F32); d_t8 = Dep()
            oh = sb("oh", [128, 64], F32); d_oh = Dep()
            ohs = sb("ohs", [128, 32], F32); d_ohs = Dep()
            posf = sb("posf", [128, 32], F32); d_posf = Dep()
            idf2 = sb("idf2", [128, 2], F32); d_idf2 = Dep()

            pT = ps("pTd", [128, 8, 128], BF16); d_pT = Dep()
            pF = ps("pFd", [128, 512], F32); d_pF = Dep()
            pG = [ps("pGd%d" % i, [128, 512], F32) for i in range(3)]; d_pG = [Dep() for _ in range(3)]
            pms = [ps("pmsd%d" % i, [128, 512], F32) for i in range(2)]; d_pms = [Dep() for _ in range(2)]
            pmo = ps("pmod", [128, 512], F32); d_pmo = Dep()

            P.dma("sp", lambda e: e.dma_start(out=gm[:], in_=g_mix[0:1, :].partition_broadcast(128)), writes=[d_c])
            P.dma("sp", lambda e: e.dma_start(out=gf[:], in_=gffn_d[0:1, :].partition_broadcast(128)), writes=[d_c])
            P.dma("sp", lambda e: e.dma_start(out=mqn[:], in_=mqn_d[0:1, :].partition_broadcast(128)), writes=[d_c])
            P.dma("sp", lambda e: e.dma_start(out=mkn[:], in_=mkn_d[0:1, :].partition_broadcast(128)), writes=[d_c])
            P.dma("sp", lambda e: e.dma_start(out=tri[:], in_=tri_d[:, :]), writes=[d_c])
            P.dma("sp", lambda e: e.dma_start(out=ecap[:], in_=ecap_d[0:1, :].partition_broadcast(128)), writes=[d_c])
            P.op("pool", lambda e: e.memset(onesf[:], 1.0), writes=[d_c])
            P.op("pool", lambda e: e.memset(onesb[:], 1.0), writes=[d_c])
            P.op("pool", lambda e: e.memset(base[:], 0.0), writes=[d_base])

            def head_norm(src_ps, d_src, gain, nh, dst, d_dst):
                hd = 512 // nh
                v = lambda ap: ap.rearrange("p (h d) -> p h d", h=nh)
                P.op("act", lambda e: e.activation(out=sq[:], in_=src_ps, func=AF.Square), reads=[d_src], writes=[d_sq])
                P.op("dve", lambda e: e.tensor_reduce(out=hs[:, 0:nh], in_=v(sq[:]), axis=AX.X, op=ALU.add), reads=[d_sq], writes=[d_hs])
                P.op("act", lambda e: e.activation(out=hr[:, 0:nh], in_=hs[:, 0:nh], func=AF.Sqrt, bias=EPS, scale=1.0 / hd), reads=[d_hs], writes=[d_hr])
                P.op("dve", lambda e: e.reciprocal(out=hr[:, 0:nh], in_=hr[:, 0:nh]), reads=[d_hr], writes=[d_hr])
                P.op("dve", lambda e: e.tensor_tensor(out=v(qmn[:]), in0=v(src_ps), in1=hr[:, 0:nh].unsqueeze(2).to_broadcast([128, nh, hd]), op=ALU.mult),
                     reads=[d_src, d_hr], writes=[d_qmn])
                P.op("pool", lambda e: e.tensor_tensor(out=dst, in0=qmn[:], in1=gain[:], op=ALU.mult), reads=[d_qmn, d_c], writes=[d_dst])

            with contextlib.ExitStack() as stm:
                wkv = stm.enter_context(nc.sbuf_tensor("s_wkv", [128, 8, 1024], BF16)); d_wkv = Dep()
                mt_ = stm.enter_context(nc.sbuf_tensor("s_memt", [128, 2, D], F32)); d_mt = Dep()
                gme = stm.enter_context(nc.sbuf_tensor("s_gme", [128, D], F32)); d_gme = Dep()
                mhb = stm.enter_context(nc.sbuf_tensor("s_mhb", [128, 2, D], BF16)); d_mhb = Dep()
                mT = stm.enter_context(nc.sbuf_tensor("s_mT", [128, 8, 256], BF16)); d_mT = Dep()
                ss2 = stm.enter_context(nc.sbuf_tensor("s_ss2", [128, 2], F32)); d_ss2 = Dep()
                for k in range(8):
                    P.dma("pool", lambda e, k=k: e.dma_start(out=wkv[:, k, :], in_=wmemkv[k * 128:(k + 1) * 128, :]), writes=[d_wkv])
                P.dma("sp", lambda e: e.dma_start(out=mt_[:], in_=mem_d[:, :].rearrange("(t p) d -> p t d", p=128)), writes=[d_mt])
                P.dma("sp", lambda e: e.dma_start(out=gme[:], in_=gmem_d[0:1, :].partition_broadcast(128)), writes=[d_gme])
                for it in range(2):
                    P.op("act", lambda e, it=it: e.activation(out=junk[:], in_=mt_[:, it, :], func=AF.Square, accum_out=ss2[:, it:it + 1]),
                         reads=[d_mt], writes=[d_junk, d_ss2])
                P.op("act", lambda e: e.activation(out=ss2[:], in_=ss2[:], func=AF.Sqrt, bias=EPS, scale=1.0 / D), reads=[d_ss2], writes=[d_ss2])
                P.op("dve", lambda e: e.reciprocal(out=ss2[:], in_=ss2[:]), reads=[d_ss2], writes=[d_ss2])
                for it in range(2):
                    P.op("dve", lambda e, it=it: e.scalar_tensor_tensor(out=mhb[:, it, :], in0=mt_[:, it, :], scalar=ss2[:, it:it + 1], in1=gme[:],
                                                                        op0=ALU.mult, op1=ALU.mult), reads=[d_mt, d_ss2, d_gme], writes=[d_mhb])
                    for k in range(8):
                        P.op("pe", lambda e, it=it, k=k: e.transpose(out=pT[:, k, :], in_=mhb[:, it, k * 128:(k + 1) * 128], identity=idb[:]),
                             reads=[d_mhb, d_idb], writes=[d_pT])
                    P.op("act", lambda e, it=it: e.copy(out=mT[:, :, it * 128:(it + 1) * 128], in_=pT[:]), reads=[d_pT], writes=[d_mT])
                for it in range(2):
                    for hv in range(2):
                        for k in range(8):
                            P.op("pe", lambda e, it=it, hv=hv, k=k: e.matmul(pG[hv][:, :], lhsT=mT[:, k, it * 128:(it + 1) * 128], rhs=wkv[:, k, hv * 512:(hv + 1) * 512],
                                                                           start=(k == 0), stop=(k == 7)), reads=[d_mT, d_wkv], writes=[d_pG[hv]])
                    head_norm(pG[0][:, :], d_pG[0], mkn, 4, qmb[:], d_qmb)
                    for h in range(4):
                        P.op("pe", lambda e, h=h: e.transpose(out=pT[:, h, :], in_=qmb[:, h * 128:(h + 1) * 128], identity=idb[:]),
                             reads=[d_qmb, d_idb], writes=[d_pT])
                    P.op("act", lambda e, it=it: e.copy(out=kmT[:, :, it * 128:(it + 1) * 128], in_=pT[:, 0:4, :]), reads=[d_pT], writes=[d_kmT])
                    P.op("act", lambda e, it=it: e.copy(out=vm[:, it, :], in_=pG[1][:, :]), reads=[d_pG[1]], writes=[d_vm])

            wqm = sb("wqm", [128, 8, 512], BF16)
            wg = sb("wg", [128, 8, 3072], BF16)
            woa = sb("woa", [128, 4, 1024], BF16)
            wos = sb("wos", [128, 8, 1024], BF16)
            wom = sb("wom", [128, 4, 1024], BF16)
            wout = sb("wout", [128, 8, 1024], BF16)
            wr = sb("wr", [128, 8, 36], F32)
            for k in range(8):
                rows = slice(k * 128, (k + 1) * 128)
                P.dma("pool", lambda e, k=k, rows=rows: e.dma_start(out=wqm[:, k, :], in_=w_in[rows, C_QM:C_QM + 512]), writes=[d_w])
                for j in range(2):
                    P.dma("pool", lambda e, k=k, rows=rows, j=j: e.dma_start(out=wg[:, k, j * 1536:(j + 1) * 1536], in_=w_in[rows, C_G + j * 1536:C_G + (j + 1) * 1536]), writes=[d_w])
                P.dma("pool", lambda e, k=k, rows=rows: e.dma_start(out=wos[:, k, :], in_=wos_d[rows, :]), writes=[d_w])
                P.dma("pool", lambda e, k=k, rows=rows: e.dma_start(out=wout[:, k, :], in_=wout_d[rows, :]), writes=[d_w])
                P.dma("sp", lambda e, k=k, rows=rows: e.dma_start(out=wr[:, k, :], in_=wr_d[rows, :]), writes=[d_w])
            for k in range(4):
                rows = slice(k * 128, (k + 1) * 128)
                P.dma("pool", lambda e, k=k, rows=rows: e.dma_start(out=woa[:, k, :], in_=woa_d[rows, :]), writes=[d_w])
                P.dma("pool", lambda e, k=k, rows=rows: e.dma_start(out=wom[:, k, :], in_=wom_d[rows, :]), writes=[d_w])
            def loads(tj):
                xj = tj % 2
                rj = tj * 128
                P.dma("sp", lambda e: e.dma_start(out=xt[xj][:], in_=xown[rj:rj + 128, :]), writes=[d_xt[xj]])
                P.dma("sp", lambda e: e.dma_start(out=oat[xj][:], in_=oa_d[rj:rj + 128, :]), reads=[d_oad], writes=[d_oat[xj]])
                P.dma("sp", lambda e: e.dma_start(out=ost[xj][:], in_=os_d[rj:rj + 128, :]), reads=[d_osd], writes=[d_ost[xj]])

            for ti in range(ntiles):
                r0 = ti * 128
                hbuf = ti % 2
                xb = ti % 2
                if ti == 0:
                    loads(0)
                if ti + 1 < ntiles:
                    loads(ti + 1)
                P.op("act", lambda e, xb=xb: e.activation(out=junk[:], in_=xt[xb][:], func=AF.Square, accum_out=ssq[:]), reads=[d_xt[xb]], writes=[d_junk, d_ssq])
                P.op("act", lambda e: e.activation(out=rstd[:], in_=ssq[:], func=AF.Sqrt, bias=EPS, scale=1.0 / D), reads=[d_ssq], writes=[d_rstd])
                P.op("dve", lambda e: e.reciprocal(out=rstd[:], in_=rstd[:]), reads=[d_rstd], writes=[d_rstd])
                P.op("dve", lambda e, xb=xb: e.scalar_tensor_tensor(out=hb[:], in0=xt[xb][:], scalar=rstd[:, 0:1], in1=gm[:], op0=ALU.mult, op1=ALU.mult),
                     reads=[d_xt[xb], d_rstd, d_c], writes=[d_hb])
                for k in range(8):
                    P.op("pe", lambda e, k=k: e.transpose(out=pT[:, k, :], in_=hb[:, k * 128:(k + 1) * 128], identity=idb[:]), reads=[d_hb, d_idb], writes=[d_pT])
                P.op("act", lambda e: e.copy(out=hT[:], in_=pT[:]), reads=[d_pT], writes=[d_hT])
                for k in range(8):
                    P.op("pe", lambda e, k=k: e.matmul(pG[0][:, :], lhsT=hT[:, k, :], rhs=wqm[:, k, :], start=(k == 0), stop=(k == 7)),
                         reads=[d_hT, d_w], writes=[d_pG[0]])
                head_norm(pG[0][:, :], d_pG[0], mqn, 4, qmb[:], d_qmb)
                for g6 in range(6):
                    bk = g6 % 3
                    for k in range(8):
                        P.op("pe", lambda e, g6=g6, k=k, bk=bk: e.matmul(pG[bk][:, :], lhsT=hT[:, k, :], rhs=wg[:, k, g6 * 512:(g6 + 1) * 512], start=(k == 0), stop=(k == 7)),
                             reads=[d_hT, d_w], writes=[d_pG[bk]])
                    P.op("act", lambda e, g6=g6, bk=bk: e.activation(out=gs[:, g6 * 512:(g6 + 1) * 512], in_=pG[bk][:, :], func=AF.Sigmoid),
                         reads=[d_pG[bk]], writes=[d_gs])
                for h in range(4):
                    P.op("pe", lambda e, h=h: e.transpose(out=pT[:, h, :], in_=qmb[:, h * 128:(h + 1) * 128], identity=idb[:]), reads=[d_qmb, d_idb], writes=[d_pT])
                P.op("act", lambda e: e.copy(out=qmT[:], in_=pT[:, 0:4, :]), reads=[d_pT], writes=[d_qmT])
                for h in range(4):
                    for mt in range(2):
                        bank = pms[h // 2]
                        c0 = ((h % 2) * 2 + mt) * 128
                        P.op("pe", lambda e, h=h, mt=mt, bank=bank, c0=c0: e.matmul(bank[:, c0:c0 + 128], lhsT=kmT[:, h, mt * 128:(mt + 1) * 128], rhs=qmT[:, h, :],
                                                                                  start=True, stop=True), reads=[d_kmT, d_qmT], writes=[d_pms[h // 2]])
                for hp in range(2):
                    P.op("act", lambda e, hp=hp: e.activation(out=pm[:, hp * 2:(hp + 1) * 2, :, :].rearrange("p a b c -> p (a b c)"), in_=pms[hp][:, :], func=AF.Exp,
                                                              scale=float(128 ** -0.5)), reads=[d_pms[hp]], writes=[d_pm])
                for h in range(4):
                    for mt in range(2):
                        P.op("pe", lambda e, h=h, mt=mt: e.matmul(pmo[:, h * 128:(h + 1) * 128], lhsT=vm[:, mt, h * 128:(h + 1) * 128], rhs=pm[:, h, mt, :],
                                                                 start=(mt == 0), stop=(mt == 1)), reads=[d_vm, d_pm], writes=[d_pmo])
                for mt in range(2):
                    P.op("pe", lambda e, mt=mt: e.matmul(pG[1][:, :].rearrange("p (h t) -> p h t", h=4), lhsT=onesb[:], rhs=pm[:, :, mt, :],
                                                        start=(mt == 0), stop=(mt == 1)), reads=[d_c, d_pm], writes=[d_pG[1]])
                P.op("dve", lambda e: e.reciprocal(out=rcp[:], in_=pG[1][:, :]), reads=[d_pG[1]], writes=[d_rcp])
                P.op("dve", lambda e: e.tensor_tensor(out=omT[:].rearrange("p h t -> p (h t)"), in0=pmo[:, :], in1=rcp[:], op=ALU.mult),
                     reads=[d_pmo, d_rcp], writes=[d_omT])
                for k in range(4):
                    P.op("pe", lambda e, xb=xb, k=k: e.transpose(out=pT[:, k, :], in_=oat[xb][:, k * 128:(k + 1) * 128], identity=idb[:]), reads=[d_oat[xb], d_idb], writes=[d_pT])
                P.op("act", lambda e: e.copy(out=oaT[:], in_=pT[:, 0:4, :]), reads=[d_pT], writes=[d_oaT])
                for k in range(8):
                    P.op("pe", lambda e, xb=xb, k=k: e.transpose(out=pT[:, k, :], in_=ost[xb][:, k * 128:(k + 1) * 128], identity=idb[:]), reads=[d_ost[xb], d_idb], writes=[d_pT])
                P.op("act", lambda e: e.copy(out=osT[:], in_=pT[:]), reads=[d_pT], writes=[d_osT])
                for hv in range(2):
                    cs_ = slice(hv * 512, (hv + 1) * 512)
                    for k in range(4):
                        P.op("pe", lambda e, k=k, cs_=cs_: e.matmul(pG[0][:, :], lhsT=oaT[:, k, :], rhs=woa[:, k, cs_], start=(k == 0), stop=(k == 3)),
                             reads=[d_oaT, d_w], writes=[d_pG[0]])
                    for k in range(8):
                        P.op("pe", lambda e, k=k, cs_=cs_: e.matmul(pG[1][:, :], lhsT=osT[:, k, :], rhs=wos[:, k, cs_], start=(k == 0), stop=(k == 7)),
                             reads=[d_osT, d_w], writes=[d_pG[1]])
                    for k in range(4):
                        P.op("pe", lambda e, k=k, cs_=cs_: e.matmul(pG[2][:, :], lhsT=omT[:, k, :], rhs=wom[:, k, cs_], start=(k == 0), stop=(k == 3)),
                             reads=[d_omT, d_w], writes=[d_pG[2]])
                    P.op("dve", lambda e, hv=hv: e.tensor_tensor(out=mg[:], in0=pG[0][:, :], in1=gs[:, hv * 512:(hv + 1) * 512], op=ALU.mult),
                         reads=[d_pG[0], d_gs], writes=[d_mg])
                    P.op("dve", lambda e, hv=hv: e.tensor_tensor(out=tt[:], in0=pG[1][:, :], in1=gs[:, 1024 + hv * 512:1024 + (hv + 1) * 512], op=ALU.mult),
                         reads=[d_pG[1], d_gs], writes=[d_tt])
                    P.op("pool", lambda e: e.tensor_tensor(out=mg[:], in0=mg[:], in1=tt[:], op=ALU.add), reads=[d_mg, d_tt], writes=[d_mg])
                    P.op("dve", lambda e, hv=hv: e.tensor_tensor(out=tt[:], in0=pG[2][:, :], in1=gs[:, 2048 + hv * 512:2048 + (hv + 1) * 512], op=ALU.mult),
                         reads=[d_pG[2], d_gs], writes=[d_tt])
                    P.op("pool", lambda e, cs_=cs_: e.tensor_tensor(out=mgb[:, cs_], in0=mg[:], in1=tt[:], op=ALU.add), reads=[d_mg, d_tt], writes=[d_mgb])
                for k in range(8):
                    P.op("pe", lambda e, k=k: e.transpose(out=pT[:, k, :], in_=mgb[:, k * 128:(k + 1) * 128], identity=idb[:]), reads=[d_mgb, d_idb], writes=[d_pT])
                P.op("act", lambda e: e.copy(out=mgT[:], in_=pT[:]), reads=[d_pT], writes=[d_mgT])
                for hv in range(2):
                    cs_ = slice(hv * 512, (hv + 1) * 512)
                    for k in range(8):
                        P.op("pe", lambda e, k=k, cs_=cs_, hv=hv: e.matmul(pG[hv][:, :], lhsT=mgT[:, k, :], rhs=wout[:, k, cs_], start=(k == 0), stop=(k == 7)),
                             reads=[d_mgT, d_w], writes=[d_pG[hv]])
                    P.op("dve", lambda e, xb=xb, cs_=cs_, hv=hv: e.tensor_tensor(out=x1[:, cs_], in0=pG[hv][:, :], in1=xt[xb][:, cs_], op=ALU.add),
                         reads=[d_pG[hv], d_xt[xb]], writes=[d_x1])
                P.dma("sp", lambda e, r0=r0: e.dma_start(out=x1_d[r0:r0 + 128, :], in_=x1[:]), reads=[d_x1], writes=[d_x1d])
                if stage < 2:
                    continue
                P.op("act", lambda e: e.activation(out=junk[:], in_=x1[:], func=AF.Square, accum_out=ssq[:]), reads=[d_x1], writes=[d_junk, d_ssq])
                P.op("act", lambda e: e.activation(out=rstd[:], in_=ssq[:], func=AF.Sqrt, bias=EPS, scale=1.0 / D), reads=[d_ssq], writes=[d_rstd])
                P.op("dve", lambda e: e.reciprocal(out=rstd[:], in_=rstd[:]), reads=[d_rstd], writes=[d_rstd])
                P.op("dve", lambda e: e.scalar_tensor_tensor(out=h2f[:], in0=x1[:], scalar=rstd[:, 0:1], in1=gf[:], op0=ALU.mult, op1=ALU.mult),
                     reads=[d_x1, d_rstd, d_c], writes=[d_h2f])
                P.op("pool", lambda e, hbuf=hbuf: e.tensor_copy(out=h2b[hbuf][:], in_=h2f[:]), reads=[d_h2f], writes=[d_h2b[hbuf]])
                for half in range(2):
                    for k4 in range(4):
                        k = half * 4 + k4
                        P.op("pe", lambda e, k=k, k4=k4: e.transpose(out=pF[:, k4 * 128:(k4 + 1) * 128], in_=h2f[:, k * 128:(k + 1) * 128], identity=idf[:]),
                             reads=[d_h2f, d_idf], writes=[d_pF])
                    P.op("act", lambda e, half=half: e.copy(out=h2T[:, half * 4:(half + 1) * 4, :].rearrange("p a b -> p (a b)"), in_=pF[:, :]), reads=[d_pF], writes=[d_h2T])
                for k in range(8):
                    P.op("pe", lambda e, k=k: e.matmul(pF[:, 0:36], lhsT=h2T[:, k, :], rhs=wr[:, k, :], start=(k == 0), stop=(k == 7)), reads=[d_h2T, d_w], writes=[d_pF])
                P.op("act", lambda e: e.copy(out=lg[:], in_=pF[:, 0:36]), reads=[d_pF], writes=[d_lg])
                P.op("dve", lambda e: e.tensor_reduce(out=sm[:, 0:1], in_=lg[:, 0:4], axis=AX.X, op=ALU.max), reads=[d_lg], writes=[d_sm])
                P.op("dve", lambda e: e.tensor_scalar(out=sm[:, 1:2], in0=sm[:, 0:1], scalar1=-1.0, scalar2=None, op0=ALU.mult), reads=[d_sm], writes=[d_sm])
                P.op("act", lambda e: e.activation(out=sm[:, 8:12], in_=lg[:, 0:4], func=AF.Exp, bias=sm[:, 1:2], accum_out=sm[:, 2:3]), reads=[d_lg, d_sm], writes=[d_sm])
                P.op("dve", lambda e: e.reciprocal(out=sm[:, 3:4], in_=sm[:, 2:3]), reads=[d_sm], writes=[d_sm])
                P.op("dve", lambda e: e.tensor_scalar(out=goh[:], in0=lg[:, 0:4], scalar1=sm[:, 0:1], scalar2=None, op0=ALU.is_ge), reads=[d_lg, d_sm], writes=[d_goh])
                P.op("dve", lambda e: e.tensor_tensor(out=lsel[:].rearrange("p (g e) -> p g e", g=4), in0=lg[:, 4:36].rearrange("p (g e) -> p g e", g=4),
                                                      in1=goh[:].unsqueeze(2).to_broadcast([128, 4, 8]), op=ALU.mult), reads=[d_lg, d_goh], writes=[d_lsel])
                P.op("dve", lambda e: e.tensor_reduce(out=les[:], in_=lsel[:].rearrange("p (g e) -> p e g", g=4), axis=AX.X, op=ALU.add), reads=[d_lsel], writes=[d_les])
                P.op("dve", lambda e: e.max(out=t8[:], in_=les[:]), reads=[d_les], writes=[d_t8])
                P.op("dve", lambda e: e.tensor_tensor(out=sm[:, 4:5], in0=t8[:, 1:2], in1=t8[:, 0:1], op=ALU.subtract), reads=[d_t8, d_sm], writes=[d_sm])
                P.op("act", lambda e: e.activation(out=sm[:, 5:6], in_=sm[:, 4:5], func=AF.Exp), reads=[d_sm], writes=[d_sm])
                P.op("dve", lambda e: e.tensor_scalar(out=sm[:, 6:7], in0=sm[:, 5:6], scalar1=1.0, scalar2=None, op0=ALU.add), reads=[d_sm], writes=[d_sm])
                P.op("dve", lambda e: e.reciprocal(out=sm[:, 6:7], in_=sm[:, 6:7]), reads=[d_sm], writes=[d_sm])
                P.op("dve", lambda e: e.tensor_tensor(out=sm[:, 7:8], in0=sm[:, 5:6], in1=sm[:, 6:7], op=ALU.mult), reads=[d_sm], writes=[d_sm])
                P.op("dve", lambda e, ti=ti: e.tensor_scalar(out=combs[:, ti, :], in0=sm[:, 6:8], scalar1=sm[:, 3:4], scalar2=None, op0=ALU.mult), reads=[d_sm], writes=[d_combs])
                for j in range(2):
                    P.op("dve", lambda e, j=j: e.tensor_scalar(out=oh[:, j * 32:(j + 1) * 32], in0=lg[:, 4:36], scalar1=t8[:, j:j + 1], scalar2=None, op0=ALU.is_equal),
                         reads=[d_lg, d_t8], writes=[d_oh])
                    P.op("dve", lambda e, j=j: e.tensor_tensor(out=oh[:, j * 32:(j + 1) * 32].rearrange("p (g e) -> p g e", g=4), in0=oh[:, j * 32:(j + 1) * 32].rearrange("p (g e) -> p g e", g=4),
                                                               in1=goh[:].unsqueeze(2).to_broadcast([128, 4, 8]), op=ALU.mult), reads=[d_oh, d_goh], writes=[d_oh])
                P.op("dve", lambda e: e.tensor_tensor(out=ohs[:], in0=oh[:, 0:32], in1=oh[:, 32:64], op=ALU.add), reads=[d_oh], writes=[d_ohs])
                P.op("pe", lambda e: e.matmul(pF[:, 64:96], lhsT=tri[:], rhs=ohs[:], start=True, stop=True), reads=[d_c, d_ohs], writes=[d_pF])
                P.op("pe", lambda e: e.matmul(pF[:, 128:160], lhsT=onesf[:], rhs=ohs[:], start=True, stop=True), reads=[d_c, d_ohs], writes=[d_pF])
                P.op("dve", lambda e: e.tensor_tensor(out=posf[:], in0=pF[:, 64:96], in1=ohs[:], op=ALU.subtract), reads=[d_pF, d_ohs], writes=[d_posf])
                P.op("dve", lambda e: e.tensor_tensor(out=posf[:], in0=posf[:], in1=base[:], op=ALU.add), reads=[d_posf, d_base], writes=[d_posf])
                P.op("dve", lambda e: e.tensor_tensor(out=posf[:], in0=posf[:], in1=ecap[:], op=ALU.add), reads=[d_posf, d_c], writes=[d_posf])
                P.op("dve", lambda e: e.tensor_tensor(out=base[:], in0=base[:], in1=pF[:, 128:160], op=ALU.add), reads=[d_base, d_pF, d_posf], writes=[d_base])
                for j in range(2):
                    P.op("dve", lambda e, j=j: e.tensor_tensor(out=oh[:, j * 32:(j + 1) * 32], in0=oh[:, j * 32:(j + 1) * 32], in1=posf[:], op=ALU.mult), reads=[d_oh, d_posf], writes=[d_oh])
                    P.op("dve", lambda e, j=j: e.tensor_reduce(out=idf2[:, j:j + 1], in_=oh[:, j * 32:(j + 1) * 32], axis=AX.X, op=ALU.add), reads=[d_oh], writes=[d_idf2])
                P.op("dve", lambda e, ti=ti: e.tensor_copy(out=idxs[:, ti, :], in_=idf2[:]), reads=[d_idf2], writes=[d_idxs])
                for j in range(2):
                    P.dma("pool", lambda e, ti=ti, j=j, hbuf=hbuf: e.indirect_dma_start(out=xbuf[:, :], out_offset=bass.IndirectOffsetOnAxis(ap=idxs[:, ti, j:j + 1], axis=0),
                                                                                      in_=h2b[hbuf][:], in_offset=None),
                          reads=[d_h2b[hbuf], d_idxs], writes=[d_xbuf])

        if stage < 3:
            return
        if hasattr(P, 'barrier'):
            P.barrier()
        with contextlib.ExitStack() as st:
            def sb(name, shape, dt):
                return st.enter_context(nc.sbuf_tensor("d_" + name, shape, dt))

            def ps(name, shape, dt=F32):
                return st.enter_context(nc.psum_tensor(name, shape, dt))
            NW = 2
            wge = [sb("wge%d" % i, [128, 8, 512], BF16) for i in range(NW)]; d_wge = [Dep() for _ in range(NW)]
            wue = [sb("wue%d" % i, [128, 8, 512], BF16) for i in range(NW)]; d_wue = [Dep() for _ in range(NW)]
            wde = [sb("wde%d" % i, [128, 4, 1024], BF16) for i in range(NW)]; d_wde = [Dep() for _ in range(NW)]
            xe = [sb("xe%d" % i, [128, 3, D], BF16) for i in range(NW)]; d_xe = [Dep() for _ in range(NW)]
            xeT = sb("xeT", [128, 8, CAP], BF16); d_xeT = Dep()
            sg = sb("sg", [128, CAP], F32); d_sg = Dep()
            hTe = sb("hTe", [128, 4, CAP], BF16); d_hTe = Dep()
            ye = [sb("ye%d" % i, [128, 3, D], F32) for i in range(NW)]; d_ye = [Dep() for _ in range(NW)]
            pT = ps("pTe", [128, 8, 128], BF16); d_pT = Dep()
            pg_ = [ps("pge%d" % i, [128, 512], F32) for i in range(2)]; d_pg = [Dep() for _ in range(2)]
            pu_ = [ps("pue%d" % i, [128, 512], F32) for i in range(2)]; d_pu = [Dep() for _ in range(2)]
            py_ = [ps("pye%d" % i, [128, 512], F32) for i in range(2)]; d_py = [Dep() for _ in range(2)]
            for ie, ex in enumerate(experts):
                b = ie % NW
                P.dma("pool", lambda e, b=b, ex=ex: e.dma_start(out=wge[b][:], in_=wgate_d[ex].rearrange("(k p) f -> p k f", p=128)), writes=[d_wge[b]])
                P.dma("pool", lambda e, b=b, ex=ex: e.dma_start(out=wue[b][:], in_=wup_d[ex].rearrange("(k p) f -> p k f", p=128)), writes=[d_wue[b]])
                P.dma("pool", lambda e, b=b, ex=ex: e.dma_start(out=wde[b][:], in_=wdown_d[ex].rearrange("(k p) f -> p k f", p=128)), writes=[d_wde[b]])
                P.dma("sp", lambda e, b=b, ex=ex: e.dma_start(out=xe[b][:], in_=xbuf[ex * CAP:(ex + 1) * CAP, :].rearrange("(s p) d -> p s d", p=128)),
                      reads=[d_xbuf], writes=[d_xe[b]])
                for s in range(3):
                    for k in range(8):
                        P.op("pe", lambda e, b=b, s=s, k=k: e.transpose(out=pT[:, k, :], in_=xe[b][:, s, k * 128:(k + 1) * 128], identity=idb[:]),
                             reads=[d_xe[b], d_idb], writes=[d_pT])
                    P.op("act", lambda e, s=s: e.copy(out=xeT[:, :, s * 128:(s + 1) * 128], in_=pT[:]), reads=[d_pT], writes=[d_xeT])
                for ft in range(4):
                    pb = ft % 2
                    for k in range(8):
                        P.op("pe", lambda e, b=b, ft=ft, k=k, pb=pb: e.matmul(pg_[pb][:, 0:CAP], lhsT=wge[b][:, k, ft * 128:(ft + 1) * 128], rhs=xeT[:, k, :],
                                                                            start=(k == 0), stop=(k == 7)), reads=[d_wge[b], d_xeT], writes=[d_pg[pb]])
                    for k in range(8):
                        P.op("pe", lambda e, b=b, ft=ft, k=k, pb=pb: e.matmul(pu_[pb][:, 0:CAP], lhsT=wue[b][:, k, ft * 128:(ft + 1) * 128], rhs=xeT[:, k, :],
                                                                            start=(k == 0), stop=(k == 7)), reads=[d_wue[b], d_xeT], writes=[d_pu[pb]])
                    P.op("act", lambda e, pb=pb: e.activation(out=sg[:], in_=pg_[pb][:, 0:CAP], func=AF.Silu), reads=[d_pg[pb]], writes=[d_sg])
                    P.op("dve", lambda e, ft=ft, pb=pb: e.tensor_tensor(out=hTe[:, ft, :], in0=sg[:], in1=pu_[pb][:, 0:CAP], op=ALU.mult),
                         reads=[d_sg, d_pu[pb]], writes=[d_hTe])
                for s in range(3):
                    for hv in range(2):
                        for ft in range(4):
                            P.op("pe", lambda e, b=b, s=s, hv=hv, ft=ft: e.matmul(py_[hv][:, :], lhsT=hTe[:, ft, s * 128:(s + 1) * 128], rhs=wde[b][:, ft, hv * 512:(hv + 1) * 512],
                                                                                start=(ft == 0), stop=(ft == 3)), reads=[d_hTe, d_wde[b]], writes=[d_py[hv]])
                        P.op("act" if hv == 0 else "dve", (lambda e, b=b, s=s, hv=hv: e.copy(out=ye[b][:, s, hv * 512:(hv + 1) * 512], in_=py_[hv][:, :])) if hv == 0 else
                             (lambda e, b=b, s=s, hv=hv: e.tensor_copy(out=ye[b][:, s, hv * 512:(hv + 1) * 512], in_=py_[hv][:, :])),
                             reads=[d_py[hv]], writes=[d_ye[b]])
                P.dma("sp", lambda e, b=b, ex=ex: e.dma_start(out=ybuf[ex * CAP:(ex + 1) * CAP, :].rearrange("(s p) d -> p s d", p=128), in_=ye[b][:]),
                      reads=[d_ye[b]], writes=[d_ybuf])

        if hasattr(P, 'barrier'):
            P.barrier()
        with contextlib.ExitStack() as st:
            def sb(name, shape, dt):
                return st.enter_context(nc.sbuf_tensor("d_" + name, shape, dt))
            NF = 2
            x1t = [sb("x1t%d" % i, [128, D], F32) for i in range(NF)]; d_x1t = [Dep() for _ in range(NF)]
            y1 = [sb("y1t%d" % i, [128, D], F32) for i in range(NF)]; d_y1 = [Dep() for _ in range(NF)]
            y2 = [sb("y2t%d" % i, [128, D], F32) for i in range(NF)]; d_y2 = [Dep() for _ in range(NF)]
            ot = [sb("ot%d" % i, [128, D], F32) for i in range(NF)]; d_ot = [Dep() for _ in range(NF)]
            for ti in range(ntiles):
                b = ti % NF
                r0 = ti * 128
                P.dma("sp", lambda e, b=b, r0=r0: e.dma_start(out=x1t[b][:], in_=x1_d[r0:r0 + 128, :]), reads=[d_x1d], writes=[d_x1t[b]])
                P.dma("pool", lambda e, b=b, ti=ti: e.indirect_dma_start(out=y1[b][:], out_offset=None, in_=ybuf[:, :],
                                                                        in_offset=bass.IndirectOffsetOnAxis(ap=idxs[:, ti, 0:1], axis=0)),
                      reads=[d_ybuf, d_idxs], writes=[d_y1[b]])
                P.dma("pool", lambda e, b=b, ti=ti: e.indirect_dma_start(out=y2[b][:], out_offset=None, in_=ybuf[:, :],
                                                                        in_offset=bass.IndirectOffsetOnAxis(ap=idxs[:, ti, 1:2], axis=0)),
                      reads=[d_ybuf, d_idxs], writes=[d_y2[b]])
                P.op("dve", lambda e, b=b, ti=ti: e.scalar_tensor_tensor(out=ot[b][:], in0=y1[b][:], scalar=combs[:, ti, 0:1], in1=x1t[b][:], op0=ALU.mult, op1=ALU.add),
                     reads=[d_y1[b], d_combs, d_x1t[b]], writes=[d_ot[b]])
                P.op("dve", lambda e, b=b, ti=ti: e.scalar_tensor_tensor(out=ot[b][:], in0=y2[b][:], scalar=combs[:, ti, 1:2], in1=ot[b][:], op0=ALU.mult, op1=ALU.add),
                     reads=[d_y2[b], d_combs, d_ot[b]], writes=[d_ot[b]])
                P.dma("sp", lambda e, b=b, r0=r0: e.dma_start(out=out_d[r0:r0 + 128, :], in_=ot[b][:]), reads=[d_ot[b]], writes=[d_out])


def host_consts_d(z):
    tri = (np.arange(128)[:, None] <= np.arange(128)[None, :]).astype(np.float32)
    return dict(mqn=np.tile(z['mem_q_norm'], 4)[None, :], mkn=np.tile(z['mem_k_norm'], 4)[None, :],
                w_r=np.ascontiguousarray(np.concatenate([z['w_router_group'], z['w_router_expert']], axis=1)),
                trif=tri, ecap=(np.arange(32, dtype=np.float32) * CAP)[None, :], ident=np.eye(128, dtype=np.float32))


def build_full():
    nc = bass.Bass("TRN2", target_bir_lowering=False)
    P = Prog(nc)

    def di(name, shape, dt=F32):
        return nc.dram_tensor(name, shape, dt, kind="ExternalInput")
    xin = di("xin", [TP + T, D]); w_in = di("w_in", [D, IN_COLS]); g_mix = di("g_mix", [1, D])
    qkn = di("qkn", [2, 512]); cs = di("cs", [TP + T, 64]); ident_d = di("ident", [128, 128])
    e_d = di("e_d", [32, NK], BF16); vq_d = di("vq", [1, 1024]); nb_d = di("nb", [1, 1024]); oq_d = di("oq", [1, 1024])
    tri_d = di("tri", [128, 128], BF16)
    cwl_d = di("cwl", [128, 16, 4]); cbl_d = di("cbl", [128, 16]); dtb_d = di("dtb", [1, 16]); alog_d = di("alog", [1, 16])
    dsk_d = di("dsk", [1, 16]); ssdn_d = di("ssdn", [1, D]); tri2_d = di("tri2", [128, 2, 256]); sel_d = di("sel_d", [16, 16, 128])
    tb_d = di("tb_d", [128, 384]); pflag_d = di("pflag", [1, 1])
    mem_d = di("mem", [256, D]); gmem_d = di("g_mem", [1, D]); wmemkv = di("w_mem_kv", [D, 1024])
    mqn_d = di("mqn", [1, 512]); mkn_d = di("mkn", [1, 512])
    woa_d = di("w_o_moba", [512, D]); wos_d = di("w_o_ssd", [D, D]); wom_d = di("w_o_mem", [512, D]); wout_d = di("w_out", [D, D])
    gffn_d = di("g_ffn", [1, D]); wr_d = di("w_r", [D, 36])
    wgate_d = di("w_gate", [NE, D, 512]); wup_d = di("w_up", [NE, D, 512]); wdown_d = di("w_down", [NE, 512, D])
    trif_d = di("trif", [128, 128]); ecap_d = di("ecap", [1, 32])
    out_d = nc.dram_tensor("out", [T, D], F32, kind="ExternalOutput")
    kT_d = nc.dram_tensor("kT_d", [512, NK], BF16)
    v_d = nc.dram_tensor("v_d", [NK, 512], BF16)
    qT_d = nc.dram_tensor("qT_d", [512, T], BF16)
    oa_d = nc.dram_tensor("oa_d", [T, 512], BF16)
    os_d = nc.dram_tensor("os_d", [T, D], BF16)
    x1_d = nc.dram_tensor("x1_d", [T, D], F32)
    xbuf = nc.dram_tensor("xbuf", [NE * CAP, D], BF16)
    ybuf = nc.dram_tensor("ybuf", [NE * CAP, D], F32)

    with contextlib.ExitStack() as st0:
        idf = st0.enter_context(nc.sbuf_tensor("idf", [128, 128], F32)); d_idf = Dep()
        idb = st0.enter_context(nc.sbuf_tensor("idb", [128, 128], BF16)); d_idb = Dep()
        P.dma("sp", lambda e: e.dma_start(out=idf[:], in_=ident_d[:, :]), writes=[d_idf])
        P.op("dve", lambda e: e.tensor_copy(out=idb[:], in_=idf[:]), reads=[d_idf], writes=[d_idb])
        with contextlib.ExitStack() as st:
            phase_a(nc, P, st, xin, w_in, g_mix, qkn, cs, idb, d_idb, kT_d, v_d, qT_d)
        P.barrier()
        with contextlib.ExitStack() as st:
            oa = st.enter_context(nc.sbuf_tensor("s_oa", [128, 32, 512], BF16)); d_oa = Dep()
            phase_b(nc, P, st, kT_d, v_d, qT_d, e_d, vq_d, nb_d, oq_d, tri_d, idb, d_idb, oa, d_oa)
            P.dma("sp", lambda e: e.dma_start(out=oa_d[:, :].rearrange("(t p) c -> p t c", p=128), in_=oa[:]), reads=[d_oa])
        P.barrier()
        with contextlib.ExitStack() as st:
            phase_c(nc, P, st, xin, w_in, g_mix, cwl_d, cbl_d, dtb_d, alog_d, dsk_d, ssdn_d, tri2_d, sel_d, tb_d, pflag_d,
                    idb, d_idb, os_d, Dep())
        P.barrier()
        phase_def(nc, P, xin[TP:TP + T, :], w_in, g_mix, mem_d, gmem_d, wmemkv, mqn_d, mkn_d, woa_d, wos_d, wom_d, wout_d, gffn_d, wr_d,
                  wgate_d, wup_d, wdown_d, trif_d, ecap_d, oa_d, Dep(), os_d, Dep(), x1_d, xbuf, ybuf, out_d, idb, d_idb, idf, d_idf)
        P.emit()
    return nc


_NC_CACHE = {}


def kernel(x, mem, g_mix, w_in, moba_q_norm, moba_k_norm, conv_w, conv_b, dt_bias, a_log, d_skip, ssd_norm, g_mem, w_mem_kv,
           mem_q_norm, mem_k_norm, w_o_moba, w_o_ssd, w_o_mem, w_out, g_ffn, w_router_group, w_router_expert, w_gate, w_up, w_down):
    f32 = np.float32
    A = lambda a: np.ascontiguousarray(np.asarray(a, dtype=f32))
    x = A(x); mem = A(mem)
    z = dict(conv_w=A(conv_w), conv_b=A(conv_b), dt_bias=A(dt_bias), a_log=A(a_log), d_skip=A(d_skip), ssd_norm=A(ssd_norm),
             mem_q_norm=A(mem_q_norm), mem_k_norm=A(mem_k_norm), w_router_group=A(w_router_group), w_router_expert=A(w_router_expert))
    if "nc" not in _NC_CACHE:
        _NC_CACHE["nc"] = build_full()
    nc = _NC_CACHE["nc"]
    pos = np.arange(2 * T, dtype=f32)
    inv = (10000.0 ** (-np.arange(32, dtype=f32) / 32)).astype(f32)
    ang = pos[:, None] * inv[None, :]
    cs_full = np.concatenate([np.cos(ang), np.sin(ang)], axis=1).astype(f32)
    qkn = np.stack([np.tile(A(moba_q_norm), 8), np.tile(A(moba_k_norm), 8)]).astype(f32)
    shared = dict(w_in=A(w_in), g_mix=A(g_mix)[None, :], qkn=qkn, ident=np.eye(128, dtype=f32),
                  g_mem=A(g_mem)[None, :], w_mem_kv=A(w_mem_kv), w_o_moba=A(w_o_moba), w_o_ssd=A(w_o_ssd), w_o_mem=A(w_o_mem),
                  w_out=A(w_out), g_ffn=A(g_ffn)[None, :], w_gate=A(w_gate), w_up=A(w_up), w_down=A(w_down))
    shared.update(host_consts_d(z))
    in_maps = []
    for c in range(8):
        bb, hh = c // 2, c % 2
        if hh == 0:
            xin = np.concatenate([np.zeros((TP, D), f32), x[bb, :T]], 0)
            csc = np.concatenate([cs_full[:TP], cs_full[:T]], 0)
        else:
            xin = x[bb]
            csc = cs_full
        m = dict(shared)
        m.update(xin=np.ascontiguousarray(xin), cs=np.ascontiguousarray(csc), mem=mem[bb])
        m.update(host_consts(hh))
        m.update(host_consts_c(z, hh))
        in_maps.append(m)
    res = run_bass_kernel_spmd(nc, in_maps, core_ids=list(range(8)))
    out = np.empty((4, 2 * T, D), f32)
    for c in range(8):
        bb, hh = c // 2, c % 2
        out[bb, hh * T:(hh + 1) * T] = np.asarray(res.results[c]["out"], dtype=f32)
    return out
```

```python
import contextlib
import numpy as np
import ml_dtypes
import concourse.bass as bass
import concourse.mybir as mybir
from concourse.bass_utils import run_bass_kernel_spmd

F32 = mybir.dt.float32
BF16 = mybir.dt.bfloat16
I32 = mybir.dt.int32
U32 = mybir.dt.uint32
AF = mybir.ActivationFunctionType
ALU = mybir.AluOpType
AX = mybir.AxisListType

T = 4096
TP = 4096
NK = TP + T
D = 1024
EPS = 1e-6
IN_COLS = 8208
BIGG = 30000.0
MB = 1000.0
NEG = -30000.0
C_Z, C_X, C_DT = 1536, 2560, 4608
C_QM, C_G = 4624, 5136
CAP = 384
NE = 32


class Dep:
    __slots__ = ("w", "r")

    def __init__(self):
        self.w = None
        self.r = {}


class Op:
    __slots__ = ("eng", "fn", "deps", "is_dma", "sig", "sigval", "dsem", "dval", "prev")

    def __init__(self, eng, fn, is_dma):
        self.eng = eng
        self.fn = fn
        self.is_dma = is_dma
        self.deps = []
        self.sig = False
        self.sigval = 0
        self.dsem = None
        self.dval = 0
        self.prev = None


class Prog:
    ENGS = ("pe", "act", "dve", "pool", "sp")

    def __init__(self, nc, ndma_sems=12):
        self.nc = nc
        self.ops = {e: [] for e in self.ENGS}
        self.ndma = {e: 0 for e in self.ENGS}
        self.dma_last = {}
        self.ndma_sems = ndma_sems
        self.all_dma = []

    def _add(self, o, reads, writes):
        deps = {}
        raw = set()
        for t in reads:
            if t.w is not None:
                deps[id(t.w)] = t.w
                raw.add(id(t.w))
        for t in writes:
            if t.w is not None:
                deps[id(t.w)] = t.w
            for r in t.r.values():
                deps[id(r)] = r
        for t in reads:
            key = id(o) if o.is_dma else o.eng
            t.r[key] = o
        for t in writes:
            t.w = o
            t.r = {}
        dl = []
        for d in deps.values():
            if d is o:
                continue
            if (not d.is_dma) and (not o.is_dma) and d.eng == "pe" and o.eng == "pe":
                continue
            if (not d.is_dma) and (not o.is_dma) and d.eng == o.eng and id(d) not in raw:
                continue
            dl.append(d)
            if not d.is_dma:
                d.sig = True
        o.deps = dl
        self.ops[o.eng].append(o)
        return o

    def op(self, eng, fn, reads=(), writes=()):
        return self._add(Op(eng, fn, False), reads, writes)

    def dma(self, eng, fn, reads=(), writes=()):
        o = Op(eng, fn, True)
        n = self.ndma[eng]
        self.ndma[eng] += 1
        slot = (eng, n % self.ndma_sems)
        o.dsem = slot
        o.prev = self.dma_last.get(slot)
        o.dval = (o.prev.dval if o.prev else 0) + 16
        self.dma_last[slot] = o
        self.all_dma.append(o)
        return self._add(o, reads, writes)


    def barrier(self):
        lasts = []
        for e in self.ENGS:
            for o in reversed(self.ops[e]):
                if (not o.is_dma) and o.fn is not None:
                    o.sig = True
                    lasts.append(o)
                    break
        dmas = list(self.dma_last.values())
        for e in self.ENGS:
            o = Op(e, None, False)
            o.deps = [d for d in lasts if d.eng != e] + dmas
            self.ops[e].append(o)

    def emit(self):
        nc = self.nc
        import contextlib
        with contextlib.ExitStack() as st:
            esem = {e: st.enter_context(nc.semaphore("S_" + e)) for e in self.ENGS}
            dsem = {}
            for e in self.ENGS:
                if self.ndma[e]:
                    for i in range(min(self.ndma_sems, self.ndma[e])):
                        dsem[(e, i)] = st.enter_context(nc.semaphore("D_%s_%d" % (e, i)))
            for e in self.ENGS:
                c = 0
                for o in self.ops[e]:
                    if (not o.is_dma) and o.sig and o.fn is not None:
                        c += 1
                        o.sigval = c
            block = st.enter_context(nc.Block())
            handles = {"pe": block.tensor, "act": block.scalar, "dve": block.vector,
                       "pool": block.gpsimd, "sp": block.sync}

            def make(e):
                def body(eng):
                    known = {}

                    def wait(sem, key, val):
                        if known.get(key, 0) < val:
                            eng.wait_ge(sem, val)
                            known[key] = val
                    for o in self.ops[e]:
                        for d in o.deps:
                            if d.is_dma:
                                wait(dsem[d.dsem], d.dsem, d.dval)
                            else:
                                wait(esem[d.eng], d.eng, d.sigval)
                        if o.is_dma:
                            if o.prev is not None:
                                wait(dsem[o.dsem], o.dsem, o.prev.dval)
                            o.fn(eng).then_inc(dsem[o.dsem], 16)
                        elif o.fn is not None:
                            ins = o.fn(eng)
                            if o.sig:
                                ins.then_inc(esem[e], 1)
                    if e == "sp":
                        for slot, o in self.dma_last.items():
                            wait(dsem[slot], slot, o.dval)
                return body
            for e in self.ENGS:
                if self.ops[e] or e == "sp":
                    handles[e](make(e))


def phase_a(nc, P, st, xin, w_in, g_mix, qkn, cs, idb, d_idb, kT_d, v_d, qT_d):
    def sb(name, shape, dt):
        return st.enter_context(nc.sbuf_tensor("a_" + name, shape, dt))

    def ps(name, shape, dt=F32):
        return st.enter_context(nc.psum_tensor("a_" + name, shape, dt))
    wq = sb("wq", [128, 8, 1536], BF16); d_wq = Dep()
    gm = sb("gm", [128, D], F32); d_gm = Dep()
    gq = sb("gq", [128, 2, 512], F32); d_gq = Dep()
    NB = 2
    NX = 4
    xt = [sb("xt%d" % i, [128, D], F32) for i in range(NX)]; d_xt = [Dep() for _ in range(NX)]
    cst = [sb("cst%d" % i, [128, 64], F32) for i in range(NX)]; d_cst = [Dep() for _ in range(NX)]
    junk = sb("junk", [128, D], BF16); d_junk = Dep()
    ssq = sb("ssq", [128, 1], F32); d_ssq = Dep()
    rstd = sb("rstd", [128, 1], F32); d_rstd = Dep()
    hb_ = [sb("hb%d" % i, [128, D], BF16) for i in range(2)]; d_hb_ = [Dep(), Dep()]
    hT = [sb("hT%d" % i, [128, 8, 128], BF16) for i in range(NB)]; d_hT = [Dep() for _ in range(NB)]
    pT = ps("pT", [128, 8, 128], BF16); d_pT = Dep()
    pq = [[ps("pq%d_%d" % (j, i), [128, 512], F32) for i in range(3)] for j in range(2)]; d_pq = [[Dep() for _ in range(3)] for _ in range(2)]
    pkT = ps("pkT", [128, 4, 128], BF16); d_pkT = Dep()
    sq_ = [sb("sq%d" % i, [128, 512], F32) for i in range(2)]; d_sq_ = [Dep(), Dep()]
    hs_ = [sb("hs%d" % i, [128, 8], F32) for i in range(2)]; d_hs_ = [Dep(), Dep()]
    hr_ = [sb("hr%d" % i, [128, 8], F32) for i in range(2)]; d_hr_ = [Dep(), Dep()]
    qn_ = [sb("qn%d" % i, [128, 512], F32) for i in range(2)]; d_qn_ = [Dep(), Dep()]
    t1_ = [sb("t1%d" % i, [128, 256], F32) for i in range(2)]; d_t1_ = [Dep(), Dep()]
    t2_ = [sb("t2%d" % i, [128, 256], F32) for i in range(2)]; d_t2_ = [Dep(), Dep()]
    t3_ = [sb("t3%d" % i, [128, 256], F32) for i in range(2)]; d_t3_ = [Dep(), Dep()]
    t4_ = [sb("t4%d" % i, [128, 256], F32) for i in range(2)]; d_t4_ = [Dep(), Dep()]
    qr_ = [[sb("qr%d_%d" % (i, j), [128, 512], BF16) for j in range(2)] for i in range(2)]; d_qr_ = [[Dep(), Dep()] for _ in range(2)]
    kTs = [[sb("kTs%d_%d" % (j, i), [128, 4, 128], BF16) for i in range(NB)] for j in range(2)]; d_kTs = [[Dep() for _ in range(NB)] for _ in range(2)]
    vs = [sb("vs%d" % i, [128, 512], BF16) for i in range(NB)]; d_vs = [Dep() for _ in range(NB)]

    for k in range(8):
        P.dma("pool", lambda e, k=k: e.dma_start(out=wq[:, k, :], in_=w_in[k * 128:(k + 1) * 128, 0:1536]), writes=[d_wq])
    P.dma("sp", lambda e: e.dma_start(out=gm[:], in_=g_mix[0:1, :].partition_broadcast(128)), writes=[d_gm])
    for i in range(2):
        P.dma("sp", lambda e, i=i: e.dma_start(out=gq[:, i, :], in_=qkn[i:i + 1, :].partition_broadcast(128)), writes=[d_gq])
    P.op("dve", lambda e: e.tensor_scalar(out=gq[:, 0, :], in0=gq[:, 0, :], scalar1=0.125, scalar2=None, op0=ALU.mult),
         reads=[d_gq], writes=[d_gq])

    def qk_post_steps(src_ps, d_src, which, b, xs):
        sl = which
        sq, hs, hr, qn, t1, t2, t3, t4, qr = sq_[sl], hs_[sl], hr_[sl], qn_[sl], t1_[sl], t2_[sl], t3_[sl], t4_[sl], qr_[sl][b]
        d_sq, d_hs, d_hr, d_qn, d_t1, d_t2, d_t3, d_t4, d_qr = (d_sq_[sl], d_hs_[sl], d_hr_[sl], d_qn_[sl], d_t1_[sl], d_t2_[sl],
                                                               d_t3_[sl], d_t4_[sl], d_qr_[sl][b])
        q3 = qn[:].rearrange("p (h d) -> p h d", h=8)
        q1 = q3[:, :, 0:32]
        q2 = q3[:, :, 32:64]
        cosb = cst[xs][:, 0:32].unsqueeze(1).to_broadcast([128, 8, 32])
        sinb = cst[xs][:, 32:64].unsqueeze(1).to_broadcast([128, 8, 32])
        v3 = lambda t: t[:].rearrange("p (h d) -> p h d", h=8)
        r3 = qr[:].rearrange("p (h d) -> p h d", h=8)
        steps = [
            lambda: P.op("act", lambda e: e.activation(out=sq[:], in_=src_ps[:], func=AF.Square), reads=[d_src], writes=[d_sq]),
            lambda: P.op("dve", lambda e: e.tensor_reduce(out=hs[:], in_=sq[:].rearrange("p (h d) -> p h d", h=8), axis=AX.X, op=ALU.add),
                         reads=[d_sq], writes=[d_hs]),
            lambda: P.op("act", lambda e: e.activation(out=hr[:], in_=hs[:], func=AF.Sqrt, bias=EPS, scale=1.0 / 64), reads=[d_hs], writes=[d_hr]),
            lambda: P.op("dve", lambda e: e.reciprocal(out=hr[:], in_=hr[:]), reads=[d_hr], writes=[d_hr]),
            lambda: P.op("dve", lambda e: e.tensor_tensor(out=qn[:].rearrange("p (h d) -> p h d", h=8), in0=src_ps[:].rearrange("p (h d) -> p h d", h=8),
                                                          in1=hr[:].unsqueeze(2).to_broadcast([128, 8, 64]), op=ALU.mult),
                         reads=[d_src, d_hr], writes=[d_qn]),
            lambda: P.op("pool", lambda e: e.tensor_tensor(out=qn[:], in0=qn[:], in1=gq[:, which, :], op=ALU.mult), reads=[d_qn, d_gq], writes=[d_qn]),
            lambda: P.op("dve", lambda e: e.tensor_tensor(out=v3(t1), in0=q1, in1=cosb, op=ALU.mult), reads=[d_qn, d_cst[xs]], writes=[d_t1]),
            lambda: P.op("pool", lambda e: e.tensor_tensor(out=v3(t2), in0=q2, in1=sinb, op=ALU.mult), reads=[d_qn, d_cst[xs]], writes=[d_t2]),
            lambda: P.op("dve", lambda e: e.tensor_tensor(out=v3(t3), in0=q2, in1=cosb, op=ALU.mult), reads=[d_qn, d_cst[xs]], writes=[d_t3]),
            lambda: P.op("pool", lambda e: e.tensor_tensor(out=v3(t4), in0=q1, in1=sinb, op=ALU.mult), reads=[d_qn, d_cst[xs]], writes=[d_t4]),
            lambda: P.op("dve", lambda e: e.tensor_tensor(out=r3[:, :, 0:32], in0=v3(t1), in1=v3(t2), op=ALU.subtract), reads=[d_t1, d_t2], writes=[d_qr]),
            lambda: P.op("pool", lambda e: e.tensor_tensor(out=r3[:, :, 32:64], in0=v3(t3), in1=v3(t4), op=ALU.add), reads=[d_t3, d_t4], writes=[d_qr]),
        ]
        return steps

    def to_T_and_store(dst_dram, col0, b, sl):
        for c in range(4):
            P.op("pe", lambda e, c=c: e.transpose(out=pkT[:, c, :], in_=qr_[sl][b][:, c * 128:(c + 1) * 128], identity=idb[:]),
                 reads=[d_qr_[sl][b], d_idb], writes=[d_pkT])
        P.op("act", lambda e: e.copy(out=kTs[sl][b][:], in_=pkT[:]), reads=[d_pkT], writes=[d_kTs[sl][b]])
        P.dma("sp", lambda e: e.dma_start(out=dst_dram[:, col0:col0 + 128].rearrange("(c p) t -> p c t", p=128), in_=kTs[sl][b][:]),
              reads=[d_kTs[sl][b]])

    def load(ti):
        xs = ti % NX
        r0 = ti * 128
        P.dma("sp", lambda e: e.dma_start(out=xt[xs][:], in_=xin[r0:r0 + 128, :]), writes=[d_xt[xs]])
        P.dma("sp", lambda e: e.dma_start(out=cst[xs][:], in_=cs[r0:r0 + 128, :]), writes=[d_cst[xs]])

    def norm(ti):
        b = ti % NB
        xs = ti % NX
        hb = hb_[b]
        P.op("act", lambda e: e.activation(out=junk[:], in_=xt[xs][:], func=AF.Square, accum_out=ssq[:]), reads=[d_xt[xs]], writes=[d_junk, d_ssq])
        P.op("act", lambda e: e.activation(out=rstd[:], in_=ssq[:], func=AF.Sqrt, bias=EPS, scale=1.0 / D), reads=[d_ssq], writes=[d_rstd])
        P.op("dve", lambda e: e.reciprocal(out=rstd[:], in_=rstd[:]), reads=[d_rstd], writes=[d_rstd])
        P.op("dve", lambda e: e.scalar_tensor_tensor(out=hb[:], in0=xt[xs][:], scalar=rstd[:, 0:1], in1=gm[:], op0=ALU.mult, op1=ALU.mult),
             reads=[d_xt[xs], d_rstd, d_gm], writes=[d_hb_[b]])

    def front(ti):
        b = ti % NB
        xs = ti % NX
        own = ti >= 32
        hb = hb_[b]
        for k in range(8):
            P.op("pe", lambda e, k=k: e.transpose(out=pT[:, k, :], in_=hb[:, k * 128:(k + 1) * 128], identity=idb[:]), reads=[d_hb_[b], d_idb], writes=[d_pT])
        P.op("act", lambda e: e.copy(out=hT[b][:], in_=pT[:]), reads=[d_pT], writes=[d_hT[b]])
        groups = [0, 1, 2] if own else [1, 2]
        for g in groups:
            for k in range(8):
                P.op("pe", lambda e, g=g, k=k: e.matmul(pq[b][g][:], lhsT=hT[b][:, k, :], rhs=wq[:, k, g * 512:(g + 1) * 512], start=(k == 0), stop=(k == 7)),
                     reads=[d_hT[b], d_wq], writes=[d_pq[b][g]])

    def post(ti):
        b = ti % NB
        xs = ti % NX
        own = ti >= 32
        ks = qk_post_steps(pq[b][1], d_pq[b][1], 1, b, xs)
        qs = qk_post_steps(pq[b][0], d_pq[b][0], 0, b, xs) if own else []
        for i in range(len(ks)):
            ks[i]()
            if qs:
                qs[i]()
        P.op("act", lambda e: e.copy(out=vs[b][:], in_=pq[b][2][:]), reads=[d_pq[b][2]], writes=[d_vs[b]])

    def tail(ti):
        b = ti % NB
        own = ti >= 32
        r0 = ti * 128
        to_T_and_store(kT_d, r0, b, 1)
        if own:
            to_T_and_store(qT_d, r0 - TP, b, 0)
        P.dma("sp", lambda e: e.dma_start(out=v_d[r0:r0 + 128, :], in_=vs[b][:]), reads=[d_vs[b]])

    load(0)
    load(1)
    load(2)
    norm(0)
    norm(1)
    front(0)
    for ti in range(64):
        if ti + 3 < 64:
            load(ti + 3)
        if ti + 2 < 64:
            norm(ti + 2)
        if ti + 1 < 64:
            front(ti + 1)
        if ti >= 1:
            tail(ti - 1)
        post(ti)
    tail(63)


def phase_b(nc, P, st, kT_d, v_d, qT_d, e_d, vq_d, nb_d, oq_d, tri_d, idb, d_idb, oa, d_oa, heads=range(8), nblk=16):
    def sb(name, shape, dt):
        return st.enter_context(nc.sbuf_tensor("b_" + name, shape, dt))

    def ps(name, shape, dt=F32):
        return st.enter_context(nc.psum_tensor(name, shape, dt))
    NB = 2
    kaug = [sb("kaug%d" % i, [96, NK], BF16) for i in range(NB)]; d_kaug = [Dep() for _ in range(NB)]
    vaug = [sb("vaug%d" % i, [128, 64, 65], BF16) for i in range(NB)]; d_vaug = [Dep() for _ in range(NB)]
    qaug = [sb("qaug%d" % i, [96, T], BF16) for i in range(NB)]; d_qaug = [Dep() for _ in range(NB)]
    d_qmb = [Dep() for _ in range(NB)]
    vq = sb("vq_s", [128, 1024], F32); d_c = Dep()
    nb = sb("nb_s", [128, 1024], F32)
    oq = sb("oq_s", [128, 1024], F32)
    tri = sb("tri_s", [128, 128], BF16)
    ksum = sb("ksum", [64, 32], F32); d_ksum = Dep()
    kmean = sb("kmean", [64, 32], BF16); d_kmean = Dep()
    g1 = sb("g1", [128, 512], F32); d_g1 = Dep()
    top8 = sb("top8", [128, 16, 8], F32); d_top8 = Dep()
    sel = sb("sel", [128, 512], F32); d_sel = Dep()
    mbp = sb("mbp", [128, 16, 96], BF16); d_mbp = Dep()
    NPT = 3
    pt = [sb("pt%d" % i, [128, 512], BF16) for i in range(NPT)]; d_pt = [Dep() for _ in range(NPT)]
    rc = sb("rc", [128, 2], F32); d_rc = [Dep(), Dep()]
    pg = ps("pg", [128, 512], F32); d_pg = Dep()
    pmT = ps("pmT", [96, 8, 128], BF16); d_pmT = Dep()
    psS = [ps("psS%d" % i, [128, 512], F32) for i in range(NPT)]; d_psS = [Dep() for _ in range(NPT)]
    po = [ps("po%d" % i, [128, 65], F32) for i in range(2)]; d_po = [Dep() for _ in range(2)]

    for i in range(NB):
        P.dma("sp", lambda e, i=i: e.dma_start(out=kaug[i][64:96, :], in_=e_d[:, :]), writes=[d_kaug[i]])
        P.op("pool", lambda e, i=i: e.memset(vaug[i][:], 1.0), writes=[d_vaug[i]])
    P.dma("sp", lambda e: e.dma_start(out=vq[:], in_=vq_d[0:1, :].partition_broadcast(128)), writes=[d_c])
    P.dma("sp", lambda e: e.dma_start(out=nb[:], in_=nb_d[0:1, :].partition_broadcast(128)), writes=[d_c])
    P.dma("sp", lambda e: e.dma_start(out=oq[:], in_=oq_d[0:1, :].partition_broadcast(128)), writes=[d_c])
    P.dma("pool", lambda e: e.dma_start(out=tri[:], in_=tri_d[:, :]), writes=[d_c])
    P.op("pool", lambda e: e.memset(mbp[:], 0.0), writes=[d_mbp])

    for ih, h in enumerate(heads):
        b = ih % NB
        P.dma("sp", lambda e, b=b, h=h: e.dma_start(out=kaug[b][0:64, :], in_=kT_d[h * 64:(h + 1) * 64, :]), writes=[d_kaug[b]])
        P.dma("sp", lambda e, b=b, h=h: e.dma_start(out=vaug[b][:, :, 0:64],
                                                    in_=v_d[:, h * 64:(h + 1) * 64].rearrange("(t p) d -> p t d", p=128)),
              writes=[d_vaug[b]])
        P.dma("sp", lambda e, b=b, h=h: e.dma_start(out=qaug[b][0:64, :], in_=qT_d[h * 64:(h + 1) * 64, :]), writes=[d_qaug[b]])
        P.op("dve", lambda e, b=b: e.tensor_reduce(out=ksum[:], in_=kaug[b][0:64, :].rearrange("p (b k) -> p b k", k=256),
                                                   axis=AX.X, op=ALU.add), reads=[d_kaug[b]], writes=[d_ksum])
        P.op("dve", lambda e: e.tensor_scalar(out=kmean[:], in0=ksum[:], scalar1=1.0 / 256, scalar2=None, op0=ALU.mult),
             reads=[d_ksum], writes=[d_kmean])
        for half in range(2):
            for j in range(16):
                qt = half * 16 + j
                P.op("pe", lambda e, b=b, j=j, qt=qt: e.matmul(pg[:, j * 32:(j + 1) * 32], lhsT=qaug[b][0:64, qt * 128:(qt + 1) * 128],
                                                              rhs=kmean[:, :], start=True, stop=True),
                     reads=[d_qaug[b], d_kmean], writes=[d_pg])
            cs_ = slice(half * 512, (half + 1) * 512)
            P.op("dve", lambda e, cs_=cs_: e.tensor_tensor(out=g1[:], in0=pg[:], in1=vq[:, cs_], op=ALU.mult),
                 reads=[d_pg, d_c], writes=[d_g1])
            P.op("dve", lambda e, cs_=cs_: e.tensor_tensor(out=g1[:], in0=g1[:], in1=nb[:, cs_], op=ALU.add),
                 reads=[d_g1, d_c], writes=[d_g1])
            for j in range(16):
                P.op("dve", lambda e, j=j: e.max(out=top8[:, j, :], in_=g1[:, j * 32:(j + 1) * 32]), reads=[d_g1], writes=[d_top8])
            P.op("dve", lambda e: e.tensor_tensor(out=sel[:].rearrange("p (j b) -> p j b", b=32),
                                                  in0=g1[:].rearrange("p (j b) -> p j b", b=32),
                                                  in1=top8[:, :, 2:3].to_broadcast([128, 16, 32]), op=ALU.is_ge),
                 reads=[d_g1, d_top8], writes=[d_sel])
            P.op("dve", lambda e, cs_=cs_: e.tensor_tensor(out=sel[:], in0=sel[:], in1=vq[:, cs_], op=ALU.mult),
                 reads=[d_sel, d_c], writes=[d_sel])
            P.op("dve", lambda e, cs_=cs_: e.tensor_tensor(out=sel[:], in0=sel[:], in1=oq[:, cs_], op=ALU.add),
                 reads=[d_sel, d_c], writes=[d_sel])
            P.op("dve", lambda e: e.tensor_scalar(out=mbp[:, :, 64:96], in0=sel[:].rearrange("p (j b) -> p j b", b=32),
                                                  scalar1=1.0, scalar2=MB, op0=ALU.subtract, op1=ALU.mult),
                 reads=[d_sel], writes=[d_mbp])
            for grp in range(2):
                for j in range(8):
                    P.op("pe", lambda e, grp=grp, j=j: e.transpose(out=pmT[:, j, :], in_=mbp[:, grp * 8 + j, :], identity=idb[:]),
                         reads=[d_mbp, d_idb], writes=[d_pmT])
                c0 = (half * 16 + grp * 8) * 128
                P.op("act", lambda e, b=b, c0=c0: e.copy(out=qaug[b][64:96, c0:c0 + 1024], in_=pmT[64:96, :, :]),
                     reads=[d_pmT], writes=[d_qmb[b]])
        units = []
        for jb in range(nblk):
            ncommon = 32 + 2 * jb
            for u in range(ncommon // 2):
                units.append((jb, [2 * u, 2 * u + 1], False, u == 0))
            units.append((jb, [32 + 2 * jb, 32 + 2 * jb + 1], True, False))

        def emit_S(u, s, b=b):
            jb, kts, diag, first = u
            for j, kt in enumerate(kts):
                if diag and j == 1:
                    q0, nq, c0 = jb * 256 + 128, 128, 256
                else:
                    q0, nq, c0 = jb * 256, 256, j * 256
                P.op("pe", lambda e, s=s, kt=kt, q0=q0, nq=nq, c0=c0: e.matmul(psS[s][:, c0:c0 + nq], lhsT=kaug[b][0:96, kt * 128:(kt + 1) * 128],
                                                                             rhs=qaug[b][0:96, q0:q0 + nq], start=True, stop=True),
                     reads=[d_kaug[b], d_qaug[b], d_qmb[b]], writes=[d_psS[s]])

        def emit_rest(u, s, b=b, h=h):
            jb, kts, diag, first = u
            ncols = 384 if diag else 512
            P.op("act", lambda e, s=s, ncols=ncols: e.activation(out=pt[s][:, 0:ncols], in_=psS[s][:, 0:ncols], func=AF.Exp),
                 reads=[d_psS[s]], writes=[d_pt[s]])
            if diag:
                P.op("pool", lambda e, s=s: e.tensor_tensor(out=pt[s][:, 0:128], in0=pt[s][:, 0:128], in1=tri[:], op=ALU.mult),
                     reads=[d_pt[s], d_c], writes=[d_pt[s]])
                P.op("pool", lambda e, s=s: e.tensor_tensor(out=pt[s][:, 256:384], in0=pt[s][:, 256:384], in1=tri[:], op=ALU.mult),
                     reads=[d_pt[s], d_c], writes=[d_pt[s]])
            for j, kt in enumerate(kts):
                if diag and j == 1:
                    pv = [(1, 256, True)]
                elif diag:
                    pv = [(0, 0, True), (1, 128, False)]
                else:
                    pv = [(0, j * 256, False), (1, j * 256 + 128, False)]
                st_ = first and j == 0
                for qi, col0, last in pv:
                    P.op("pe", lambda e, s=s, kt=kt, qi=qi, col0=col0, st_=st_, last=last: e.matmul(
                        po[qi][:, :], lhsT=pt[s][:, col0:col0 + 128], rhs=vaug[b][:, kt, :], start=st_, stop=last),
                        reads=[d_pt[s], d_vaug[b]], writes=[d_po[qi]])
            if diag:
                for qi in range(2):
                    qt = jb * 2 + qi
                    P.op("dve", lambda e, qi=qi: e.reciprocal(out=rc[:, qi:qi + 1], in_=po[qi][:, 64:65]), reads=[d_po[qi]], writes=[d_rc[qi]])
                    P.op("dve", lambda e, qi=qi, qt=qt: e.tensor_scalar(out=oa[:, qt, h * 64:(h + 1) * 64], in0=po[qi][:, 0:64],
                                                                       scalar1=rc[:, qi:qi + 1], scalar2=None, op0=ALU.mult),
                         reads=[d_po[qi], d_rc[qi]], writes=[d_oa])
        SK = 2
        n = len(units)
        for i in range(n + SK):
            if i < n:
                emit_S(units[i], i % NPT)
            if i >= SK:
                emit_rest(units[i - SK], (i - SK) % NPT)


def host_consts(half):
    pv = 1.0 if half == 1 else 0.0
    V = np.zeros((32, 32), np.float32); O = np.zeros((32, 32), np.float32)
    for qt in range(32):
        jb = qt // 2
        V[qt, :16] = pv
        V[qt, 16:16 + jb] = 1.0
        O[qt, 16 + jb] = 1.0
    NBm = (V - 1.0) * BIGG
    E = np.zeros((32, NK), np.float32)
    for b in range(32):
        E[b, b * 256:(b + 1) * 256] = 1.0
    tri = (np.arange(128)[:, None] <= np.arange(128)[None, :]).astype(np.float32)
    return dict(vq=V.reshape(1, 1024), nb=NBm.reshape(1, 1024), oq=O.reshape(1, 1024),
                e_d=E.astype(ml_dtypes.bfloat16), tri=tri.astype(ml_dtypes.bfloat16))


def phase_c(nc, P, st, xin, w_in, g_mix, cwl_d, cbl_d, dtb_d, alog_d, dsk_d, ssdn_d, tri2_d, sel_d, tb_d, pflag_d,
            idb, d_idb, os_d, d_osd, chunks=range(32)):
    def sb(name, shape, dt):
        return st.enter_context(nc.sbuf_tensor("c_" + name, shape, dt))

    def ps(name, shape, dt=F32):
        return st.enter_context(nc.psum_tensor(name, shape, dt))
    wz = sb("wz", [128, 8, 1024], BF16); d_w = Dep()
    wx = sb("wx", [128, 8, 2048], BF16)
    wdt = sb("wdt", [128, 8, 16], BF16)
    gm = sb("gmc", [128, D], F32); d_c = Dep()
    cw = sb("cw", [128, 16, 4], F32)
    cb = sb("cb", [128, 16], F32)
    dtb = sb("dtb", [128, 2, 16], F32)
    Aneg = sb("Aneg", [128, 2, 16], F32); d_A = Dep()
    dsk = sb("dsk", [128, 16], F32)
    ssdn = sb("ssdn", [128, D], F32)
    tri2 = sb("tri2", [128, 2, 256], F32)
    onesf = sb("onesf", [128, 128], F32)
    sel = sb("selc", [16, 16, 128], F32)
    tb = sb("tbc", [128, 384], F32)
    pflag = sb("pflag", [128, 1], F32)
    xt = sb("xtc", [128, 2, D], F32); d_xt = Dep()
    junk = sb("junkc", [128, D], BF16); d_junk = Dep()
    ssq = sb("ssqc", [128, 2], F32); d_ssq = Dep()
    rstd = sb("rstdc", [128, 2], F32); d_rstd = Dep()
    hb = sb("hbc", [128, 2, D], BF16); d_hb = Dep()
    hT = sb("hTc", [128, 8, 256], BF16); d_hT = Dep()
    xraw = sb("xraw", [128, 16, 259], F32); d_xraw = Dep(); d_halo = Dep()
    acc = sb("acc", [128, 8, 256], F32); d_acc = [Dep() for _ in range(8)]
    xc = sb("xc", [128, 16, 256], BF16); d_xc = [Dep() for _ in range(16)]
    xtok = sb("xtok", [128, 2, 1024], BF16); d_xtok = Dep()
    btok = sb("btok", [128, 2, 512], BF16); d_btok = Dep()
    dtr = sb("dtr", [128, 2, 16], F32); d_dtr = Dep()
    dt = sb("dt", [128, 2, 16], F32); d_dt = Dep()
    aa = sb("aa", [128, 2, 16], F32); d_aa = Dep()
    acum = sb("acum", [128, 2, 16], F32); d_acum = Dep()
    nacum = sb("nacum", [128, 2, 16], F32); d_nacum = Dep()
    acumT = sb("acumT", [16, 256], F32); d_acumT = Dep()
    tot = sb("tot", [128, 16], F32); d_tot = Dep()
    cdec = sb("cdec", [128, 16], F32); d_cdec = Dep()
    dte = sb("dte", [128, 2, 16], F32); d_dte = Dep()
    eac = sb("eac", [128, 2, 16], F32); d_eac = Dep()
    w2 = sb("w2", [128, 2, 16], F32); d_w2 = Dep()
    xdt = sb("xdt", [128, 2, 1024], BF16); d_xdt = Dep()
    xdd = sb("xdd", [128, 2, 1024], BF16); d_xdd = Dep()
    zs = sb("zs", [128, 2, 1024], BF16); d_zs = Dep()
    cbT = sb("cbT", [128, 4, 384], F32); d_cbT = [Dep() for _ in range(4)]
    arg = [sb("arg%d" % i, [128, 384], F32) for i in range(2)]; d_arg = [Dep(), Dep()]
    Lt = [sb("Lt%d" % i, [128, 384], F32) for i in range(2)]; d_Lt = [Dep(), Dep()]
    Mt = [sb("Mt%d" % i, [128, 384], BF16) for i in range(2)]; d_Mt = [Dep() for _ in range(2)]
    stf = sb("stf", [128, 4, 256], F32); d_stf = Dep()
    stT = sb("stT", [128, 4, 256], BF16); d_stT = Dep()
    yt = [sb("yt%d" % i, [128, 256], F32) for i in range(2)]; d_yt = [Dep(), Dep()]
    y2 = [sb("y2%d" % i, [128, 256], F32) for i in range(2)]; d_y2 = [Dep(), Dep()]
    gss = [sb("gss%d" % i, [128, 1], F32) for i in range(2)]; d_gss = [Dep(), Dep()]
    grs = [sb("grs%d" % i, [128, 1], F32) for i in range(2)]; d_grs = [Dep(), Dep()]
    osb = sb("osb", [128, 2, 1024], BF16); d_osb = Dep()

    B = [ps("bk%d" % i, [128, 512], F32) for i in range(7)]
    d_B = [Dep() for _ in range(7)]
    d_B1h = [Dep(), Dep()]
    d_B5h = [Dep(), Dep()]
    pT = ps("pTc", [128, 8, 128], BF16); d_pT = Dep()

    for k in range(8):
        rows = slice(k * 128, (k + 1) * 128)
        P.dma("pool", lambda e, k=k, rows=rows: e.dma_start(out=wz[:, k, :], in_=w_in[rows, C_Z:C_Z + 1024]), writes=[d_w])
        P.dma("pool", lambda e, k=k, rows=rows: e.dma_start(out=wx[:, k, :], in_=w_in[rows, C_X:C_X + 2048]), writes=[d_w])
        P.dma("pool", lambda e, k=k, rows=rows: e.dma_start(out=wdt[:, k, :], in_=w_in[rows, C_DT:C_DT + 16]), writes=[d_w])
    P.dma("sp", lambda e: e.dma_start(out=gm[:], in_=g_mix[0:1, :].partition_broadcast(128)), writes=[d_c])
    P.dma("sp", lambda e: e.dma_start(out=cw[:], in_=cwl_d[:, :, :]), writes=[d_c])
    P.dma("sp", lambda e: e.dma_start(out=cb[:], in_=cbl_d[:, :]), writes=[d_c])
    for i in range(2):
        P.dma("sp", lambda e, i=i: e.dma_start(out=dtb[:, i, :], in_=dtb_d[0:1, :].partition_broadcast(128)), writes=[d_c])
        P.dma("sp", lambda e, i=i: e.dma_start(out=Aneg[:, i, :], in_=alog_d[0:1, :].partition_broadcast(128)), writes=[d_A])
    P.dma("sp", lambda e: e.dma_start(out=dsk[:], in_=dsk_d[0:1, :].partition_broadcast(128)), writes=[d_c])
    P.dma("sp", lambda e: e.dma_start(out=ssdn[:], in_=ssdn_d[0:1, :].partition_broadcast(128)), writes=[d_c])
    P.dma("sp", lambda e: e.dma_start(out=tri2[:], in_=tri2_d[:, :, :]), writes=[d_c])
    P.dma("sp", lambda e: e.dma_start(out=sel[:], in_=sel_d[:, :, :]), writes=[d_c])
    P.dma("sp", lambda e: e.dma_start(out=tb[:], in_=tb_d[:, :]), writes=[d_c])
    P.dma("sp", lambda e: e.dma_start(out=pflag[:], in_=pflag_d[0:1, :].partition_broadcast(128)), writes=[d_c])
    P.op("pool", lambda e: e.memset(onesf[:], 1.0), writes=[d_c])
    P.op("act", lambda e: e.activation(out=Aneg[:], in_=Aneg[:], func=AF.Exp), reads=[d_A], writes=[d_A])
    P.op("dve", lambda e: e.tensor_scalar(out=Aneg[:], in0=Aneg[:], scalar1=-1.0, scalar2=None, op0=ALU.mult), reads=[d_A], writes=[d_A])
    P.op("pool", lambda e: e.memset(xraw[:], 0.0), writes=[d_xraw, d_halo])
    P.op("pool", lambda e: e.memset(stf[:], 0.0), writes=[d_stf])
    P.op("pool", lambda e: e.memset(stT[:], 0.0), writes=[d_stT])

    h3 = lambda ap: ap.rearrange("p (h d) -> p h d", d=64)

    for c in chunks:
        own = c >= 16
        r0 = c * 256
        P.dma("sp", lambda e, r0=r0: e.dma_start(out=xt[:], in_=xin[r0:r0 + 256, :].rearrange("(t p) d -> p t d", p=128)), writes=[d_xt])
        for it in range(2):
            P.op("act", lambda e, it=it: e.activation(out=junk[:], in_=xt[:, it, :], func=AF.Square, accum_out=ssq[:, it:it + 1]),
                 reads=[d_xt], writes=[d_junk, d_ssq])
        P.op("act", lambda e: e.activation(out=rstd[:], in_=ssq[:], func=AF.Sqrt, bias=EPS, scale=1.0 / D), reads=[d_ssq], writes=[d_rstd])
        P.op("dve", lambda e: e.reciprocal(out=rstd[:], in_=rstd[:]), reads=[d_rstd], writes=[d_rstd])
        for it in range(2):
            P.op("dve", lambda e, it=it: e.scalar_tensor_tensor(out=hb[:, it, :], in0=xt[:, it, :], scalar=rstd[:, it:it + 1], in1=gm[:],
                                                                                    op0=ALU.mult, op1=ALU.mult),
                 reads=[d_xt, d_rstd, d_c], writes=[d_hb])
        for it in range(2):
            for k in range(8):
                P.op("pe", lambda e, it=it, k=k: e.transpose(out=pT[:, k, :], in_=hb[:, it, k * 128:(k + 1) * 128], identity=idb[:]),
                     reads=[d_hb, d_idb], writes=[d_pT])
            P.op("act", lambda e, it=it: e.copy(out=hT[:, :, it * 128:(it + 1) * 128], in_=pT[:]), reads=[d_pT], writes=[d_hT])
        for it in range(2):
            for k in range(8):
                P.op("pe", lambda e, it=it, k=k: e.matmul(B[3][:, it * 16:(it + 1) * 16], lhsT=hT[:, k, it * 128:(it + 1) * 128], rhs=wdt[:, k, :],
                                                         start=(k == 0), stop=(k == 7)), reads=[d_hT, d_w], writes=[d_B[3]])
        P.op("dve", lambda e: e.tensor_tensor(out=dtr[:].rearrange("p a b -> p (a b)"), in0=B[3][:, 0:32], in1=dtb[:].rearrange("p a b -> p (a b)"), op=ALU.add),
             reads=[d_B[3], d_c], writes=[d_dtr])
        P.op("act", lambda e: e.activation(out=dtr[:], in_=dtr[:], func=AF.Exp), reads=[d_dtr], writes=[d_dtr])
        P.op("act", lambda e: e.activation(out=dt[:], in_=dtr[:], func=AF.Ln, bias=1.0), reads=[d_dtr], writes=[d_dt])
        P.op("dve", lambda e: e.tensor_tensor(out=aa[:], in0=dt[:], in1=Aneg[:], op=ALU.mult), reads=[d_dt, d_A], writes=[d_aa])
        nfc = 16 if (own or c == 15) else 12
        for fc in range(nfc):
            bi = 1 if fc % 2 == 0 else 4
            for k in range(8):
                P.op("pe", lambda e, fc=fc, k=k, bi=bi: e.matmul(B[bi][:, 0:256], lhsT=wx[:, k, fc * 128:(fc + 1) * 128], rhs=hT[:, k, :],
                                                               start=(k == 0), stop=(k == 7)), reads=[d_hT, d_w], writes=[d_B[bi]])
            P.op("act", lambda e, fc=fc, bi=bi: e.copy(out=xraw[:, fc, 3:259], in_=B[bi][:, 0:256]),
                 reads=[d_B[bi]], writes=[d_xraw])
        for it in range(2):
            for jt in range(it + 1):
                P.op("pe", lambda e, it=it, jt=jt: e.matmul(B[3][:, 32 + it * 16:32 + (it + 1) * 16], lhsT=tri2[:, jt, it * 128:(it + 1) * 128],
                                                           rhs=aa[:, jt, :], start=(jt == 0), stop=(jt == it)),
                     reads=[d_aa, d_c], writes=[d_B[3]])
        for jt in range(2):
            P.op("pe", lambda e, jt=jt: e.matmul(B[3][:, 64:80], lhsT=onesf[:], rhs=aa[:, jt, :], start=(jt == 0), stop=(jt == 1)),
                 reads=[d_aa, d_c], writes=[d_B[3]])
        for jt in range(2):
            P.op("pe", lambda e, jt=jt: e.matmul(B[3][0:16, 128:384], lhsT=aa[:, jt, :], rhs=tri2[:, jt, :], start=(jt == 0), stop=(jt == 1)),
                 reads=[d_aa, d_c], writes=[d_B[3]])
        P.op("act", lambda e: e.copy(out=acum[:].rearrange("p a b -> p (a b)"), in_=B[3][:, 32:64]), reads=[d_B[3]], writes=[d_acum])
        P.op("dve", lambda e: e.tensor_scalar(out=nacum[:].rearrange("p a b -> p (a b)"), in0=B[3][:, 32:64], scalar1=-1.0, scalar2=None, op0=ALU.mult),
             reads=[d_B[3]], writes=[d_nacum])
        P.op("act", lambda e: e.copy(out=tot[:], in_=B[3][:, 64:80]), reads=[d_B[3]], writes=[d_tot])
        P.op("act", lambda e: e.copy(out=acumT[:], in_=B[3][0:16, 128:384]), reads=[d_B[3]], writes=[d_acumT])
        P.op("act", lambda e: e.activation(out=cdec[:], in_=tot[:], func=AF.Exp), reads=[d_tot], writes=[d_cdec])
        P.op("dve", lambda e: e.tensor_tensor(out=dte[:], in0=nacum[:], in1=tot[:].unsqueeze(1).to_broadcast([128, 2, 16]), op=ALU.add),
             reads=[d_nacum, d_tot], writes=[d_dte])
        P.op("act", lambda e: e.activation(out=dte[:], in_=dte[:], func=AF.Exp), reads=[d_dte], writes=[d_dte])
        P.op("act", lambda e: e.activation(out=eac[:], in_=acum[:], func=AF.Exp), reads=[d_acum], writes=[d_eac])
        P.op("dve", lambda e: e.tensor_tensor(out=w2[:], in0=dt[:], in1=dte[:], op=ALU.mult), reads=[d_dt, d_dte], writes=[d_w2])
        if own:
            for it in range(2):
                for hv in range(2):
                    zb = 2 if hv == 0 else 6
                    for k in range(8):
                        P.op("pe", lambda e, it=it, hv=hv, k=k, zb=zb: e.matmul(B[zb][:, :], lhsT=hT[:, k, it * 128:(it + 1) * 128], rhs=wz[:, k, hv * 512:(hv + 1) * 512],
                                                                              start=(k == 0), stop=(k == 7)), reads=[d_hT, d_w], writes=[d_B[zb]])
                    P.op("act", lambda e, it=it, hv=hv, zb=zb: e.activation(out=zs[:, it, hv * 512:(hv + 1) * 512], in_=B[zb][:, :], func=AF.Silu),
                         reads=[d_B[zb]], writes=[d_zs])
        for half in range(2):
            n8 = 8 if (half == 0 or nfc == 16) else 4
            for f8 in range(n8):
                fc = half * 8 + f8
                P.op("dve", lambda e, fc=fc, f8=f8: e.tensor_scalar(out=acc[:, f8, :], in0=xraw[:, fc, 0:256], scalar1=cw[:, fc, 0:1], scalar2=cb[:, fc:fc + 1],
                                                                   op0=ALU.mult, op1=ALU.add), reads=[d_xraw, d_halo, d_c], writes=[d_acc[f8]])
            for kk in range(1, 4):
                for f8 in range(n8):
                    fc = half * 8 + f8
                    P.op("dve", lambda e, fc=fc, f8=f8, kk=kk: e.scalar_tensor_tensor(out=acc[:, f8, :], in0=xraw[:, fc, kk:kk + 256], scalar=cw[:, fc, kk:kk + 1],
                                                                                     in1=acc[:, f8, :], op0=ALU.mult, op1=ALU.add),
                         reads=[d_xraw, d_halo, d_c, d_acc[f8]], writes=[d_acc[f8]])
            for f8 in range(n8):
                fc = half * 8 + f8
                P.op("act", lambda e, fc=fc, f8=f8: e.activation(out=xc[:, fc, :], in_=acc[:, f8, :], func=AF.Silu),
                     reads=[d_acc[f8]], writes=[d_xc[fc]])
        P.op("pool", lambda e: e.tensor_copy(out=xraw[:, :, 0:3], in_=xraw[:, :, 256:259]), reads=[d_xraw], writes=[d_halo])
        for it in range(2):
            for fc in range(8):
                P.op("pe", lambda e, it=it, fc=fc: e.transpose(out=pT[:, fc, :], in_=xc[:, fc, it * 128:(it + 1) * 128], identity=idb[:]),
                     reads=[d_xc[fc], d_idb], writes=[d_pT])
            P.op("act", lambda e, it=it: e.copy(out=xtok[:, it, :], in_=pT[:]), reads=[d_pT], writes=[d_xtok])
            for g in range(4):
                P.op("pe", lambda e, it=it, g=g: e.transpose(out=pT[:, g, :], in_=xc[:, 8 + g, it * 128:(it + 1) * 128], identity=idb[:]),
                     reads=[d_xc[8 + g], d_idb], writes=[d_pT])
            P.op("act", lambda e, it=it: e.copy(out=btok[:, it, :], in_=pT[:, 0:4, :]), reads=[d_pT], writes=[d_btok])
        for it in range(2):
            if own:
                P.op("dve", lambda e, it=it: e.tensor_tensor(out=h3(xdt[:, it, :]), in0=h3(xtok[:, it, :]),
                                                             in1=dt[:, it, :].unsqueeze(2).to_broadcast([128, 16, 64]), op=ALU.mult),
                     reads=[d_xtok, d_dt], writes=[d_xdt])
            P.op("pool", lambda e, it=it: e.tensor_tensor(out=h3(xdd[:, it, :]), in0=h3(xtok[:, it, :]),
                                                          in1=w2[:, it, :].unsqueeze(2).to_broadcast([128, 16, 64]), op=ALU.mult),
                 reads=[d_xtok, d_w2], writes=[d_xdd])
        if own:
            for g in range(4):
                P.op("pe", lambda e, g=g: e.matmul(B[4][:, 0:256], lhsT=xc[:, 8 + g, 0:128], rhs=xc[:, 12 + g, :], start=True, stop=True),
                     reads=[d_xc[8 + g], d_xc[12 + g]], writes=[d_B[4]])
                P.op("pe", lambda e, g=g: e.matmul(B[4][:, 256:384], lhsT=xc[:, 8 + g, 128:256], rhs=xc[:, 12 + g, 128:256], start=True, stop=True),
                     reads=[d_xc[8 + g], d_xc[12 + g]], writes=[d_B[4]])
                P.op("act", lambda e, g=g: e.copy(out=cbT[:, g, :], in_=B[4][:, 0:384]), reads=[d_B[4]], writes=[d_cbT[g]])
            for g in range(4):
                def stA(h, g=g):
                    mi = h % 2
                    bk = 5 if mi == 0 else 4
                    P.op("pe", lambda e: e.matmul(B[bk][:, 0:256], lhsT=sel[:, h, :], rhs=acumT[:, :], start=True, stop=True),
                         reads=[d_c, d_acumT], writes=[d_B[bk]])
                    P.op("dve", lambda e: e.scalar_tensor_tensor(out=arg[mi][:, 0:256], in0=B[bk][:, 0:256], scalar=nacum[:, 0, h:h + 1], in1=tb[:, 0:256],
                                                                 op0=ALU.add, op1=ALU.add), reads=[d_B[bk], d_nacum, d_c], writes=[d_arg[mi]])
                    P.op("dve", lambda e: e.scalar_tensor_tensor(out=arg[mi][:, 256:384], in0=B[bk][:, 128:256], scalar=nacum[:, 1, h:h + 1], in1=tb[:, 256:384],
                                                                 op0=ALU.add, op1=ALU.add), reads=[d_B[bk], d_nacum, d_c], writes=[d_arg[mi]])
                    P.op("act", lambda e: e.activation(out=Lt[mi][:], in_=arg[mi][:], func=AF.Exp), reads=[d_arg[mi]], writes=[d_Lt[mi]])
                    P.op("pool", lambda e: e.tensor_tensor(out=Mt[mi][:], in0=Lt[mi][:], in1=cbT[:, g, :], op=ALU.mult),
                         reads=[d_Lt[mi], d_cbT[g]], writes=[d_Mt[mi]])

                def stB(h, g=g):
                    mi = h % 2
                    r = h % 4
                    cs_ = slice(r * 64, (r + 1) * 64)
                    hs_ = slice(h * 64, (h + 1) * 64)
                    P.op("pe", lambda e: e.matmul(B[6][:, 0:256][:, cs_], lhsT=Mt[mi][:, 0:128], rhs=xdt[:, 0, hs_], start=True, stop=True),
                         reads=[d_Mt[mi], d_xdt], writes=[d_B[6]])
                    P.op("pe", lambda e: e.matmul(B[6][:, 256:512][:, cs_], lhsT=Mt[mi][:, 128:256], rhs=xdt[:, 0, hs_], start=True, stop=False),
                         reads=[d_Mt[mi], d_xdt], writes=[d_B[6]])
                    P.op("pe", lambda e: e.matmul(B[6][:, 256:512][:, cs_], lhsT=Mt[mi][:, 256:384], rhs=xdt[:, 1, hs_], start=False, stop=True),
                         reads=[d_Mt[mi], d_xdt], writes=[d_B[6]])
                hs4 = [g * 4 + r for r in range(4)]
                stA(hs4[0]); stA(hs4[1]); stB(hs4[0]); stA(hs4[2]); stB(hs4[1]); stA(hs4[3]); stB(hs4[2]); stB(hs4[3])
                gs_ = slice(g * 256, (g + 1) * 256)
                for it in range(2):
                    P.op("pe", lambda e, g=g, it=it: e.matmul(B[0][:, it * 256:(it + 1) * 256], lhsT=xc[:, 12 + g, it * 128:(it + 1) * 128], rhs=stT[:, g, :],
                                                             start=True, stop=True), reads=[d_xc[12 + g], d_stT], writes=[d_B[0]])
                v4 = lambda ap: ap.rearrange("p (r d) -> p r d", d=64)

                def ystep(step, it, g=g, gs_=gs_):
                    if step == 0:
                        P.op("dve", lambda e: e.tensor_tensor(out=v4(yt[it][:]), in0=v4(B[0][:, it * 256:(it + 1) * 256]),
                                                              in1=eac[:, it, g * 4:(g + 1) * 4].unsqueeze(2).to_broadcast([128, 4, 64]), op=ALU.mult),
                             reads=[d_B[0], d_eac], writes=[d_yt[it]])
                    elif step == 1:
                        P.op("dve", lambda e: e.tensor_tensor(out=yt[it][:], in0=yt[it][:], in1=B[6][:, it * 256:(it + 1) * 256], op=ALU.add),
                             reads=[d_yt[it], d_B[6]], writes=[d_yt[it]])
                    elif step == 2:
                        P.op("pool", lambda e: e.tensor_tensor(out=v4(y2[it][:]), in0=v4(xtok[:, it, gs_]),
                                                               in1=dsk[:, g * 4:(g + 1) * 4].unsqueeze(2).to_broadcast([128, 4, 64]), op=ALU.mult),
                             reads=[d_xtok, d_c], writes=[d_y2[it]])
                    elif step == 3:
                        P.op("dve", lambda e: e.tensor_tensor(out=yt[it][:], in0=yt[it][:], in1=y2[it][:], op=ALU.add), reads=[d_yt[it], d_y2[it]], writes=[d_yt[it]])
                    elif step == 4:
                        P.op("dve", lambda e: e.tensor_tensor(out=yt[it][:], in0=yt[it][:], in1=zs[:, it, gs_], op=ALU.mult), reads=[d_yt[it], d_zs], writes=[d_yt[it]])
                    elif step == 5:
                        P.op("act", lambda e: e.activation(out=y2[it][:], in_=yt[it][:], func=AF.Square, accum_out=gss[it][:]), reads=[d_yt[it], d_y2[it]], writes=[d_y2[it], d_gss[it]])
                    elif step == 6:
                        P.op("act", lambda e: e.activation(out=grs[it][:], in_=gss[it][:], func=AF.Sqrt, bias=EPS, scale=1.0 / 256), reads=[d_gss[it]], writes=[d_grs[it]])
                    elif step == 7:
                        P.op("dve", lambda e: e.reciprocal(out=grs[it][:], in_=grs[it][:]), reads=[d_grs[it]], writes=[d_grs[it]])
                    elif step == 8:
                        P.op("dve", lambda e: e.scalar_tensor_tensor(out=osb[:, it, gs_], in0=yt[it][:], scalar=grs[it][:, 0:1], in1=ssdn[:, gs_],
                                                                     op0=ALU.mult, op1=ALU.mult), reads=[d_yt[it], d_grs[it], d_c], writes=[d_osb])
                for step in range(9):
                    for it in range(2):
                        ystep(step, it)
            P.dma("sp", lambda e, r0=r0: e.dma_start(out=os_d[r0 - TP:r0 - TP + 256, :].rearrange("(t p) c -> p t c", p=128), in_=osb[:]),
                  reads=[d_osb], writes=[d_osd])
        for g in range(4):
            bank = B[2] if g < 2 else B[1]
            dbank = d_B[2] if g < 2 else d_B[1]
            cs_ = slice((g % 2) * 256, (g % 2 + 1) * 256)
            for jt in range(2):
                P.op("pe", lambda e, g=g, jt=jt, bank=bank, cs_=cs_: e.matmul(bank[:, cs_], lhsT=btok[:, jt, g * 128:(g + 1) * 128], rhs=xdd[:, jt, g * 256:(g + 1) * 256],
                                                                            start=(jt == 0), stop=(jt == 1)), reads=[d_btok, d_xdd], writes=[dbank])
        v16 = lambda ap: ap.rearrange("p (h d) -> p h d", d=64)
        P.op("dve", lambda e: e.tensor_tensor(out=v16(stf[:].rearrange("p g c -> p (g c)")), in0=v16(stf[:].rearrange("p g c -> p (g c)")),
                                              in1=cdec[:].unsqueeze(2).to_broadcast([128, 16, 64]), op=ALU.mult), reads=[d_stf, d_cdec], writes=[d_stf])
        P.op("dve", lambda e: e.tensor_tensor(out=stf[:, 0:2, :].rearrange("p g c -> p (g c)"), in0=stf[:, 0:2, :].rearrange("p g c -> p (g c)"), in1=B[2][:, :], op=ALU.add),
             reads=[d_stf, d_B[2]], writes=[d_stf])
        P.op("dve", lambda e: e.tensor_tensor(out=stf[:, 2:4, :].rearrange("p g c -> p (g c)"), in0=stf[:, 2:4, :].rearrange("p g c -> p (g c)"), in1=B[1][:, :], op=ALU.add),
             reads=[d_stf, d_B[1]], writes=[d_stf])
        if c == 15:
            P.op("dve", lambda e: e.tensor_scalar(out=stf[:], in0=stf[:], scalar1=pflag[:, 0:1], scalar2=None, op0=ALU.mult), reads=[d_stf, d_c], writes=[d_stf])
        P.op("act", lambda e: e.copy(out=stT[:], in_=stf[:]), reads=[d_stf], writes=[d_stT])


def host_consts_c(z, half):
    cwl = np.ascontiguousarray(z['conv_w'].reshape(4, 16, 128).transpose(2, 1, 0)).astype(np.float32)
    cbl = np.ascontiguousarray(z['conv_b'].reshape(16, 128).T).astype(np.float32)
    tri = (np.arange(128)[:, None] <= np.arange(128)[None, :]).astype(np.float32)
    tri2 = np.zeros((128, 2, 256), np.float32)
    tri2[:, 0, 0:128] = tri; tri2[:, 0, 128:256] = 1.0; tri2[:, 1, 128:256] = tri
    sel = np.zeros((16, 16, 128), np.float32)
    for h in range(16):
        sel[h, h, :] = 1.0
    tbias = np.where(tri > 0, 0.0, NEG).astype(np.float32)
    tb = np.zeros((128, 384), np.float32)
    tb[:, 0:128] = tbias; tb[:, 256:384] = tbias
    return dict(cwl=cwl, cbl=cbl, dtb=z['dt_bias'][None, :], alog=z['a_log'][None, :], dsk=z['d_skip'][None, :], ssdn=z['ssd_norm'][None, :],
                tri2=tri2, sel_d=sel, tb_d=tb, pflag=np.array([[1.0 if half == 1 else 0.0]], np.float32))


def phase_def(nc, P, xown, w_in, g_mix, mem_d, gmem_d, wmemkv, mqn_d, mkn_d, woa_d, wos_d, wom_d, wout_d, gffn_d, wr_d,
              wgate_d, wup_d, wdown_d, tri_d, ecap_d, oa_d, d_oad, os_d, d_osd, x1_d, xbuf, ybuf, out_d, idb, d_idb, idf, d_idf,
              ntiles=32, experts=range(32), stage=3):
    d_x1d = Dep(); d_xbuf = Dep(); d_ybuf = Dep(); d_out = Dep()
    with contextlib.ExitStack() as stp:
        def sbp(name, shape, dt):
            return stp.enter_context(nc.sbuf_tensor("d_" + name, shape, dt))
        combs = sbp("combs", [128, 32, 2], F32); d_combs = Dep()
        idxs = sbp("idxs", [128, 32, 2], I32); d_idxs = Dep()

        with contextlib.ExitStack() as st:
            def sb(name, shape, dt):
                return st.enter_context(nc.sbuf_tensor("d_" + name, shape, dt))

            def ps(name, shape, dt=F32):
                return st.enter_context(nc.psum_tensor(name, shape, dt))
            d_w = Dep(); d_c = Dep()
            gm = sb("gmd", [128, D], F32)
            gf = sb("gfd", [128, D], F32)
            mqn = sb("mqn", [128, 512], F32)
            mkn = sb("mkn", [128, 512], F32)
            tri = sb("trid", [128, 128], F32)
            onesf = sb("onesfd", [128, 128], F32)
            onesb = sb("onesbd", [128, 128], BF16)
            ecap = sb("ecap", [128, 32], F32)
            base = sb("base", [128, 32], F32); d_base = Dep()
            kmT = sb("kmT", [128, 4, 256], BF16); d_kmT = Dep()
            vm = sb("vm", [128, 2, 512], BF16); d_vm = Dep()

            xt = [sb("xtd%d" % i, [128, D], F32) for i in range(2)]; d_xt = [Dep(), Dep()]
            junk = sb("junkd", [128, D], BF16); d_junk = Dep()
            ssq = sb("ssqd", [128, 1], F32); d_ssq = Dep()
            rstd = sb("rstdd", [128, 1], F32); d_rstd = Dep()
            hb = sb("hbd", [128, D], BF16); d_hb = Dep()
            hT = sb("hTd", [128, 8, 128], BF16); d_hT = Dep()
            sq = sb("sqd", [128, 512], F32); d_sq = Dep()
            hs = sb("hsd", [128, 4], F32); d_hs = Dep()
            hr = sb("hrd", [128, 4], F32); d_hr = Dep()
            qmn = sb("qmn", [128, 512], F32); d_qmn = Dep()
            qmb = sb("qmb", [128, 512], BF16); d_qmb = Dep()
            qmT = sb("qmT", [128, 4, 128], BF16); d_qmT = Dep()
            pm = sb("pm", [128, 4, 2, 128], BF16); d_pm = Dep()
            rcp = sb("rcp", [128, 512], F32); d_rcp = Dep()
            omT = sb("omT", [128, 4, 128], BF16); d_omT = Dep()
            gs = sb("gs", [128, 3072], F32); d_gs = Dep()
            oat = [sb("oat%d" % i, [128, 512], BF16) for i in range(2)]; d_oat = [Dep(), Dep()]
            ost = [sb("ost%d" % i, [128, 1024], BF16) for i in range(2)]; d_ost = [Dep(), Dep()]
            oaT = sb("oaT", [128, 4, 128], BF16); d_oaT = Dep()
            osT = sb("osT", [128, 8, 128], BF16); d_osT = Dep()
            mg = sb("mg", [128, 512], F32); d_mg = Dep()
            tt = sb("tt", [128, 512], F32); d_tt = Dep()
            mgb = sb("mgb", [128, D], BF16); d_mgb = Dep()
            mgT = sb("mgT", [128, 8, 128], BF16); d_mgT = Dep()
            x1 = sb("x1", [128, D], F32); d_x1 = Dep()
            h2f = sb("h2f", [128, D], F32); d_h2f = Dep()
            h2b = [sb("h2b%d" % i, [128, D], BF16) for i in range(2)]; d_h2b = [Dep() for _ in range(2)]
            h2T = sb("h2T", [128, 8, 128], F32); d_h2T = Dep()
            lg = sb("lg", [128, 36], F32); d_lg = Dep()
            sm = sb("sm", [128, 64], F32); d_sm = Dep()
            goh = sb("goh", [128, 4], F32); d_goh = Dep()
            lsel = sb("lsel", [128, 32], F32); d_lsel = Dep()
            les = sb("les", [128, 8], F32); d_les = Dep()
            t8 = sb("t8", [128, 8], F32); d_t8 = Dep()
            oh = sb("oh", [128, 64], F32); d_oh = Dep()
            ohs = sb("ohs", [128, 32], F32); d_ohs = Dep()
            posf = sb("posf", [128, 32], F32); d_posf = Dep()
            idf2 = sb("idf2", [128, 2], F32); d_idf2 = Dep()

            pT = ps("pTd", [128, 8, 128], BF16); d_pT = Dep()
            pF = ps("pFd", [128, 512], F32); d_pF = Dep()
            pG = [ps("pGd%d" % i, [128, 512], F32) for i in range(3)]; d_pG = [Dep() for _ in range(3)]
            pms = [ps("pmsd%d" % i, [128, 512], F32) for i in range(2)]; d_pms = [Dep() for _ in range(2)]
            pmo = ps("pmod", [128, 512], F32); d_pmo = Dep()

            P.dma("sp", lambda e: e.dma_start(out=gm[:], in_=g_mix[0:1, :].partition_broadcast(128)), writes=[d_c])
            P.dma("sp", lambda e: e.dma_start(out=gf[:], in_=gffn_d[0:1, :].partition_broadcast(128)), writes=[d_c])
            P.dma("sp", lambda e: e.dma_start(out=mqn[:], in_=mqn_d[0:1, :].partition_broadcast(128)), writes=[d_c])
            P.dma("sp", lambda e: e.dma_start(out=mkn[:], in_=mkn_d[0:1, :].partition_broadcast(128)), writes=[d_c])
            P.dma("sp", lambda e: e.dma_start(out=tri[:], in_=tri_d[:, :]), writes=[d_c])
            P.dma("sp", lambda e: e.dma_start(out=ecap[:], in_=ecap_d[0:1, :].partition_broadcast(128)), writes=[d_c])
            P.op("pool", lambda e: e.memset(onesf[:], 1.0), writes=[d_c])
            P.op("pool", lambda e: e.memset(onesb[:], 1.0), writes=[d_c])
            P.op("pool", lambda e: e.memset(base[:], 0.0), writes=[d_base])

            def head_norm(src_ps, d_src, gain, nh, dst, d_dst):
                hd = 512 // nh
                v = lambda ap: ap.rearrange("p (h d) -> p h d", h=nh)
                P.op("act", lambda e: e.activation(out=sq[:], in_=src_ps, func=AF.Square), reads=[d_src], writes=[d_sq])
                P.op("dve", lambda e: e.tensor_reduce(out=hs[:, 0:nh], in_=v(sq[:]), axis=AX.X, op=ALU.add), reads=[d_sq], writes=[d_hs])
                P.op("act", lambda e: e.activation(out=hr[:, 0:nh], in_=hs[:, 0:nh], func=AF.Sqrt, bias=EPS, scale=1.0 / hd), reads=[d_hs], writes=[d_hr])
                P.op("dve", lambda e: e.reciprocal(out=hr[:, 0:nh], in_=hr[:, 0:nh]), reads=[d_hr], writes=[d_hr])
                P.op("dve", lambda e: e.tensor_tensor(out=v(qmn[:]), in0=v(src_ps), in1=hr[:, 0:nh].unsqueeze(2).to_broadcast([128, nh, hd]), op=ALU.mult),
                     reads=[d_src, d_hr], writes=[d_qmn])
                P.op("pool", lambda e: e.tensor_tensor(out=dst, in0=qmn[:], in1=gain[:], op=ALU.mult), reads=[d_qmn, d_c], writes=[d_dst])

            with contextlib.ExitStack() as stm:
                wkv = stm.enter_context(nc.sbuf_tensor("s_wkv", [128, 8, 1024], BF16)); d_wkv = Dep()
                mt_ = stm.enter_context(nc.sbuf_tensor("s_memt", [128, 2, D], F32)); d_mt = Dep()
                gme = stm.enter_context(nc.sbuf_tensor("s_gme", [128, D], F32)); d_gme = Dep()
                mhb = stm.enter_context(nc.sbuf_tensor("s_mhb", [128, 2, D], BF16)); d_mhb = Dep()
                mT = stm.enter_context(nc.sbuf_tensor("s_mT", [128, 8, 256], BF16)); d_mT = Dep()
                ss2 = stm.enter_context(nc.sbuf_tensor("s_ss2", [128, 2], F32)); d_ss2 = Dep()
                for k in range(8):
                    P.dma("pool", lambda e, k=k: e.dma_start(out=wkv[:, k, :], in_=wmemkv[k * 128:(k + 1) * 128, :]), writes=[d_wkv])
                P.dma("sp", lambda e: e.dma_start(out=mt_[:], in_=mem_d[:, :].rearrange("(t p) d -> p t d", p=128)), writes=[d_mt])
                P.dma("sp", lambda e: e.dma_start(out=gme[:], in_=gmem_d[0:1, :].partition_broadcast(128)), writes=[d_gme])
                for it in range(2):
                    P.op("act", lambda e, it=it: e.activation(out=junk[:], in_=mt_[:, it, :], func=AF.Square, accum_out=ss2[:, it:it + 1]),
                         reads=[d_mt], writes=[d_junk, d_ss2])
                P.op("act", lambda e: e.activation(out=ss2[:], in_=ss2[:], func=AF.Sqrt, bias=EPS, scale=1.0 / D), reads=[d_ss2], writes=[d_ss2])
                P.op("dve", lambda e: e.reciprocal(out=ss2[:], in_=ss2[:]), reads=[d_ss2], writes=[d_ss2])
                for it in range(2):
                    P.op("dve", lambda e, it=it: e.scalar_tensor_tensor(out=mhb[:, it, :], in0=mt_[:, it, :], scalar=ss2[:, it:it + 1], in1=gme[:],
                                                                        op0=ALU.mult, op1=ALU.mult), reads=[d_mt, d_ss2, d_gme], writes=[d_mhb])
                    for k in range(8):
                        P.op("pe", lambda e, it=it, k=k: e.transpose(out=pT[:, k, :], in_=mhb[:, it, k * 128:(k + 1) * 128], identity=idb[:]),
                             reads=[d_mhb, d_idb], writes=[d_pT])
                    P.op("act", lambda e, it=it: e.copy(out=mT[:, :, it * 128:(it + 1) * 128], in_=pT[:]), reads=[d_pT], writes=[d_mT])
                for it in range(2):
                    for hv in range(2):
                        for k in range(8):
                            P.op("pe", lambda e, it=it, hv=hv, k=k: e.matmul(pG[hv][:, :], lhsT=mT[:, k, it * 128:(it + 1) * 128], rhs=wkv[:, k, hv * 512:(hv + 1) * 512],
                                                                           start=(k == 0), stop=(k == 7)), reads=[d_mT, d_wkv], writes=[d_pG[hv]])
                    head_norm(pG[0][:, :], d_pG[0], mkn, 4, qmb[:], d_qmb)
                    for h in range(4):
                        P.op("pe", lambda e, h=h: e.transpose(out=pT[:, h, :], in_=qmb[:, h * 128:(h + 1) * 128], identity=idb[:]),
                             reads=[d_qmb, d_idb], writes=[d_pT])
                    P.op("act", lambda e, it=it: e.copy(out=kmT[:, :, it * 128:(it + 1) * 128], in_=pT[:, 0:4, :]), reads=[d_pT], writes=[d_kmT])
                    P.op("act", lambda e, it=it: e.copy(out=vm[:, it, :], in_=pG[1][:, :]), reads=[d_pG[1]], writes=[d_vm])

            wqm = sb("wqm", [128, 8, 512], BF16)
            wg = sb("wg", [128, 8, 3072], BF16)
            woa = sb("woa", [128, 4, 1024], BF16)
            wos = sb("wos", [128, 8, 1024], BF16)
            wom = sb("wom", [128, 4, 1024], BF16)
            wout = sb("wout", [128, 8, 1024], BF16)
            wr = sb("wr", [128, 8, 36], F32)
            for k in range(8):
                rows = slice(k * 128, (k + 1) * 128)
                P.dma("pool", lambda e, k=k, rows=rows: e.dma_start(out=wqm[:, k, :], in_=w_in[rows, C_QM:C_QM + 512]), writes=[d_w])
                for j in range(2):
                    P.dma("pool", lambda e, k=k, rows=rows, j=j: e.dma_start(out=wg[:, k, j * 1536:(j + 1) * 1536], in_=w_in[rows, C_G + j * 1536:C_G + (j + 1) * 1536]), writes=[d_w])
                P.dma("pool", lambda e, k=k, rows=rows: e.dma_start(out=wos[:, k, :], in_=wos_d[rows, :]), writes=[d_w])
                P.dma("pool", lambda e, k=k, rows=rows: e.dma_start(out=wout[:, k, :], in_=wout_d[rows, :]), writes=[d_w])
                P.dma("sp", lambda e, k=k, rows=rows: e.dma_start(out=wr[:, k, :], in_=wr_d[rows, :]), writes=[d_w])
            for k in range(4):
                rows = slice(k * 128, (k + 1) * 128)
                P.dma("pool", lambda e, k=k, rows=rows: e.dma_start(out=woa[:, k, :], in_=woa_d[rows, :]), writes=[d_w])
                P.dma("pool", lambda e, k=k, rows=rows: e.dma_start(out=wom[:, k, :], in_=wom_d[rows, :]), writes=[d_w])
            def loads(tj):
                xj = tj % 2
                rj = tj * 128
                P.dma("sp", lambda e: e.dma_start(out=xt[xj][:], in_=xown[rj:rj + 128, :]), writes=[d_xt[xj]])
                P.dma("sp", lambda e: e.dma_start(out=oat[xj][:], in_=oa_d[rj:rj + 128, :]), reads=[d_oad], writes=[d_oat[xj]])
                P.dma("sp", lambda e: e.dma_start(out=ost[xj][:], in_=os_d[rj:rj + 128, :]), reads=[d_osd], writes=[d_ost[xj]])

            for ti in range(ntiles):
                r0 = ti * 128
                hbuf = ti % 2
                xb = ti % 2
                if ti == 0:
                    loads(0)
                if ti + 1 < ntiles:
                    loads(ti + 1)
                P.op("act", lambda e, xb=xb: e.activation(out=junk[:], in_=xt[xb][:], func=AF.Square, accum_out=ssq[:]), reads=[d_xt[xb]], writes=[d_junk, d_ssq])
                P.op("act", lambda e: e.activation(out=rstd[:], in_=ssq[:], func=AF.Sqrt, bias=EPS, scale=1.0 / D), reads=[d_ssq], writes=[d_rstd])
                P.op("dve", lambda e: e.reciprocal(out=rstd[:], in_=rstd[:]), reads=[d_rstd], writes=[d_rstd])
                P.op("dve", lambda e, xb=xb: e.scalar_tensor_tensor(out=hb[:], in0=xt[xb][:], scalar=rstd[:, 0:1], in1=gm[:], op0=ALU.mult, op1=ALU.mult),
                     reads=[d_xt[xb], d_rstd, d_c], writes=[d_hb])
                for k in range(8):
                    P.op("pe", lambda e, k=k: e.transpose(out=pT[:, k, :], in_=hb[:, k * 128:(k + 1) * 128], identity=idb[:]), reads=[d_hb, d_idb], writes=[d_pT])
                P.op("act", lambda e: e.copy(out=hT[:], in_=pT[:]), reads=[d_pT], writes=[d_hT])
                for k in range(8):
                    P.op("pe", lambda e, k=k: e.matmul(pG[0][:, :], lhsT=hT[:, k, :], rhs=wqm[:, k, :], start=(k == 0), stop=(k == 7)),
                         reads=[d_hT, d_w], writes=[d_pG[0]])
                head_norm(pG[0][:, :], d_pG[0], mqn, 4, qmb[:], d_qmb)
                for g6 in range(6):
                    bk = g6 % 3
                    for k in range(8):
                        P.op("pe", lambda e, g6=g6, k=k, bk=bk: e.matmul(pG[bk][:, :], lhsT=hT[:, k, :], rhs=wg[:, k, g6 * 512:(g6 + 1) * 512], start=(k == 0), stop=(k == 7)),
                             reads=[d_hT, d_w], writes=[d_pG[bk]])
                    P.op("act", lambda e, g6=g6, bk=bk: e.activation(out=gs[:, g6 * 512:(g6 + 1) * 512], in_=pG[bk][:, :], func=AF.Sigmoid),
                         reads=[d_pG[bk]], writes=[d_gs])
                for h in range(4):
                    P.op("pe", lambda e, h=h: e.transpose(out=pT[:, h, :], in_=qmb[:, h * 128:(h + 1) * 128], identity=idb[:]), reads=[d_qmb, d_idb], writes=[d_pT])
                P.op("act", lambda e: e.copy(out=qmT[:], in_=pT[:, 0:4, :]), reads=[d_pT], writes=[d_qmT])
                for h in range(4):
                    for mt in range(2):
                        bank = pms[h // 2]
                        c0 = ((h % 2) * 2 + mt) * 128
                        P.op("pe", lambda e, h=h, mt=mt, bank=bank, c0=c0: e.matmul(bank[:, c0:c0 + 128], lhsT=kmT[:, h, mt * 128:(mt + 1) * 128], rhs=qmT[:, h, :],
                                                                                  start=True, stop=True), reads=[d_kmT, d_qmT], writes=[d_pms[h // 2]])
                for hp in range(2):
                    P.op("act", lambda e, hp=hp: e.activation(out=pm[:, hp * 2:(hp + 1) * 2, :, :].rearrange("p a b c -> p (a b c)"), in_=pms[hp][:, :], func=AF.Exp,
                                                              scale=float(128 ** -0.5)), reads=[d_pms[hp]], writes=[d_pm])
                for h in range(4):
                    for mt in range(2):
                        P.op("pe", lambda e, h=h, mt=mt: e.matmul(pmo[:, h * 128:(h + 1) * 128], lhsT=vm[:, mt, h * 128:(h + 1) * 128], rhs=pm[:, h, mt, :],
                                                                 start=(mt == 0), stop=(mt == 1)), reads=[d_vm, d_pm], writes=[d_pmo])
                for mt in range(2):
                    P.op("pe", lambda e, mt=mt: e.matmul(pG[1][:, :].rearrange("p (h t) -> p h t", h=4), lhsT=onesb[:], rhs=pm[:, :, mt, :],
                                                        start=(mt == 0), stop=(mt == 1)), reads=[d_c, d_pm], writes=[d_pG[1]])
                P.op("dve", lambda e: e.reciprocal(out=rcp[:], in_=pG[1][:, :]), reads=[d_pG[1]], writes=[d_rcp])
                P.op("dve", lambda e: e.tensor_tensor(out=omT[:].rearrange("p h t -> p (h t)"), in0=pmo[:, :], in1=rcp[:], op=ALU.mult),
                     reads=[d_pmo, d_rcp], writes=[d_omT])
                for k in range(4):
                    P.op("pe", lambda e, xb=xb, k=k: e.transpose(out=pT[:, k, :], in_=oat[xb][:, k * 128:(k + 1) * 128], identity=idb[:]), reads=[d_oat[xb], d_idb], writes=[d_pT])
                P.op("act", lambda e: e.copy(out=oaT[:], in_=pT[:, 0:4, :]), reads=[d_pT], writes=[d_oaT])
                for k in range(8):
                    P.op("pe", lambda e, xb=xb, k=k: e.transpose(out=pT[:, k, :], in_=ost[xb][:, k * 128:(k + 1) * 128], identity=idb[:]), reads=[d_ost[xb], d_idb], writes=[d_pT])
                P.op("act", lambda e: e.copy(out=osT[:], in_=pT[:]), reads=[d_pT], writes=[d_osT])
                for hv in range(2):
                    cs_ = slice(hv * 512, (hv + 1) * 512)
                    for k in range(4):
                        P.op("pe", lambda e, k=k, cs_=cs_: e.matmul(pG[0][:, :], lhsT=oaT[:, k, :], rhs=woa[:, k, cs_], start=(k == 0), stop=(k == 3)),
                             reads=[d_oaT, d_w], writes=[d_pG[0]])
                    for k in range(8):
                        P.op("pe", lambda e, k=k, cs_=cs_: e.matmul(pG[1][:, :], lhsT=osT[:, k, :], rhs=wos[:, k, cs_], start=(k == 0), stop=(k == 7)),
                             reads=[d_osT, d_w], writes=[d_pG[1]])
                    for k in range(4):
                        P.op("pe", lambda e, k=k, cs_=cs_: e.matmul(pG[2][:, :], lhsT=omT[:, k, :], rhs=wom[:, k, cs_], start=(k == 0), stop=(k == 3)),
                             reads=[d_omT, d_w], writes=[d_pG[2]])
                    P.op("dve", lambda e, hv=hv: e.tensor_tensor(out=mg[:], in0=pG[0][:, :], in1=gs[:, hv * 512:(hv + 1) * 512], op=ALU.mult),
                         reads=[d_pG[0], d_gs], writes=[d_mg])
                    P.op("dve", lambda e, hv=hv: e.tensor_tensor(out=tt[:], in0=pG[1][:, :], in1=gs[:, 1024 + hv * 512:1024 + (hv + 1) * 512], op=ALU.mult),
                         reads=[d_pG[1], d_gs], writes=[d_tt])
                    P.op("pool", lambda e: e.tensor_tensor(out=mg[:], in0=mg[:], in1=tt[:], op=ALU.add), reads=[d_mg, d_tt], writes=[d_mg])
                    P.op("dve", lambda e, hv=hv: e.tensor_tensor(out=tt[:], in0=pG[2][:, :], in1=gs[:, 2048 + hv * 512:2048 + (hv + 1) * 512], op=ALU.mult),
                         reads=[d_pG[2], d_gs], writes=[d_tt])
                    P.op("pool", lambda e, cs_=cs_: e.tensor_tensor(out=mgb[:, cs_], in0=mg[:], in1=tt[:], op=ALU.add), reads=[d_mg, d_tt], writes=[d_mgb])
                for k in range(8):
                    P.op("pe", lambda e, k=k: e.transpose(out=pT[:, k, :], in_=mgb[:, k * 128:(k + 1) * 128], identity=idb[:]), reads=[d_mgb, d_idb], writes=[d_pT])
                P.op("act", lambda e: e.copy(out=mgT[:], in_=pT[:]), reads=[d_pT], writes=[d_mgT])
                for hv in range(2):
                    cs_ = slice(hv * 512, (hv + 1) * 512)
                    for k in range(8):
                        P.op("pe", lambda e, k=k, cs_=cs_, hv=hv: e.matmul(pG[hv][:, :], lhsT=mgT[:, k, :], rhs=wout[:, k, cs_], start=(k == 0), stop=(k == 7)),
                             reads=[d_mgT, d_w], writes=[d_pG[hv]])
                    P.op("dve", lambda e, xb=xb, cs_=cs_, hv=hv: e.tensor_tensor(out=x1[:, cs_], in0=pG[hv][:, :], in1=xt[xb][:, cs_], op=ALU.add),
                         reads=[d_pG[hv], d_xt[xb]], writes=[d_x1])
                P.dma("sp", lambda e, r0=r0: e.dma_start(out=x1_d[r0:r0 + 128, :], in_=x1[:]), reads=[d_x1], writes=[d_x1d])
                if stage < 2:
                    continue
                P.op("act", lambda e: e.activation(out=junk[:], in_=x1[:], func=AF.Square, accum_out=ssq[:]), reads=[d_x1], writes=[d_junk, d_ssq])
                P.op("act", lambda e: e.activation(out=rstd[:], in_=ssq[:], func=AF.Sqrt, bias=EPS, scale=1.0 / D), reads=[d_ssq], writes=[d_rstd])
                P.op("dve", lambda e: e.reciprocal(out=rstd[:], in_=rstd[:]), reads=[d_rstd], writes=[d_rstd])
                P.op("dve", lambda e: e.scalar_tensor_tensor(out=h2f[:], in0=x1[:], scalar=rstd[:, 0:1], in1=gf[:], op0=ALU.mult, op1=ALU.mult),
                     reads=[d_x1, d_rstd, d_c], writes=[d_h2f])
                P.op("pool", lambda e, hbuf=hbuf: e.tensor_copy(out=h2b[hbuf][:], in_=h2f[:]), reads=[d_h2f], writes=[d_h2b[hbuf]])
                for half in range(2):
                    for k4 in range(4):
                        k = half * 4 + k4
                        P.op("pe", lambda e, k=k, k4=k4: e.transpose(out=pF[:, k4 * 128:(k4 + 1) * 128], in_=h2f[:, k * 128:(k + 1) * 128], identity=idf[:]),
                             reads=[d_h2f, d_idf], writes=[d_pF])
                    P.op("act", lambda e, half=half: e.copy(out=h2T[:, half * 4:(half + 1) * 4, :].rearrange("p a b -> p (a b)"), in_=pF[:, :]), reads=[d_pF], writes=[d_h2T])
                for k in range(8):
                    P.op("pe", lambda e, k=k: e.matmul(pF[:, 0:36], lhsT=h2T[:, k, :], rhs=wr[:, k, :], start=(k == 0), stop=(k == 7)), reads=[d_h2T, d_w], writes=[d_pF])
                P.op("act", lambda e: e.copy(out=lg[:], in_=pF[:, 0:36]), reads=[d_pF], writes=[d_lg])
                P.op("dve", lambda e: e.tensor_reduce(out=sm[:, 0:1], in_=lg[:, 0:4], axis=AX.X, op=ALU.max), reads=[d_lg], writes=[d_sm])
                P.op("dve", lambda e: e.tensor_scalar(out=sm[:, 1:2], in0=sm[:, 0:1], scalar1=-1.0, scalar2=None, op0=ALU.mult), reads=[d_sm], writes=[d_sm])
                P.op("act", lambda e: e.activation(out=sm[:, 8:12], in_=lg[:, 0:4], func=AF.Exp, bias=sm[:, 1:2], accum_out=sm[:, 2:3]), reads=[d_lg, d_sm], writes=[d_sm])
                P.op("dve", lambda e: e.reciprocal(out=sm[:, 3:4], in_=sm[:, 2:3]), reads=[d_sm], writes=[d_sm])
                P.op("dve", lambda e: e.tensor_scalar(out=goh[:], in0=lg[:, 0:4], scalar1=sm[:, 0:1], scalar2=None, op0=ALU.is_ge), reads=[d_lg, d_sm], writes=[d_goh])
                P.op("dve", lambda e: e.tensor_tensor(out=lsel[:].rearrange("p (g e) -> p g e", g=4), in0=lg[:, 4:36].rearrange("p (g e) -> p g e", g=4),
                                                      in1=goh[:].unsqueeze(2).to_broadcast([128, 4, 8]), op=ALU.mult), reads=[d_lg, d_goh], writes=[d_lsel])
                P.op("dve", lambda e: e.tensor_reduce(out=les[:], in_=lsel[:].rearrange("p (g e) -> p e g", g=4), axis=AX.X, op=ALU.add), reads=[d_lsel], writes=[d_les])
                P.op("dve", lambda e: e.max(out=t8[:], in_=les[:]), reads=[d_les], writes=[d_t8])
                P.op("dve", lambda e: e.tensor_tensor(out=sm[:, 4:5], in0=t8[:, 1:2], in1=t8[:, 0:1], op=ALU.subtract), reads=[d_t8, d_sm], writes=[d_sm])
                P.op("act", lambda e: e.activation(out=sm[:, 5:6], in_=sm[:, 4:5], func=AF.Exp), reads=[d_sm], writes=[d_sm])
                P.op("dve", lambda e: e.tensor_scalar(out=sm[:, 6:7], in0=sm[:, 5:6], scalar1=1.0, scalar2=None, op0=ALU.add), reads=[d_sm], writes=[d_sm])
                P.op("dve", lambda e: e.reciprocal(out=sm[:, 6:7], in_=sm[:, 6:7]), reads=[d_sm], writes=[d_sm])
                P.op("dve", lambda e: e.tensor_tensor(out=sm[:, 7:8], in0=sm[:, 5:6], in1=sm[:, 6:7], op=ALU.mult), reads=[d_sm], writes=[d_sm])
                P.op("dve", lambda e, ti=ti: e.tensor_scalar(out=combs[:, ti, :], in0=sm[:, 6:8], scalar1=sm[:, 3:4], scalar2=None, op0=ALU.mult), reads=[d_sm], writes=[d_combs])
                for j in range(2):
                    P.op("dve", lambda e, j=j: e.tensor_scalar(out=oh[:, j * 32:(j + 1) * 32], in0=lg[:, 4:36], scalar1=t8[:, j:j + 1], scalar2=None, op0=ALU.is_equal),
                         reads=[d_lg, d_t8], writes=[d_oh])
                    P.op("dve", lambda e, j=j: e.tensor_tensor(out=oh[:, j * 32:(j + 1) * 32].rearrange("p (g e) -> p g e", g=4), in0=oh[:, j * 32:(j + 1) * 32].rearrange("p (g e) -> p g e", g=4),
                                                               in1=goh[:].unsqueeze(2).to_broadcast([128, 4, 8]), op=ALU.mult), reads=[d_oh, d_goh], writes=[d_oh])
                P.op("dve", lambda e: e.tensor_tensor(out=ohs[:], in0=oh[:, 0:32], in1=oh[:, 32:64], op=ALU.add), reads=[d_oh], writes=[d_ohs])
                P.op("pe", lambda e: e.matmul(pF[:, 64:96], lhsT=tri[:], rhs=ohs[:], start=True, stop=True), reads=[d_c, d_ohs], writes=[d_pF])
                P.op("pe", lambda e: e.matmul(pF[:, 128:160], lhsT=onesf[:], rhs=ohs[:], start=True, stop=True), reads=[d_c, d_ohs], writes=[d_pF])
                P.op("dve", lambda e: e.tensor_tensor(out=posf[:], in0=pF[:, 64:96], in1=ohs[:], op=ALU.subtract), reads=[d_pF, d_ohs], writes=[d_posf])
                P.op("dve", lambda e: e.tensor_tensor(out=posf[:], in0=posf[:], in1=base[:], op=ALU.add), reads=[d_posf, d_base], writes=[d_posf])
                P.op("dve", lambda e: e.tensor_tensor(out=posf[:], in0=posf[:], in1=ecap[:], op=ALU.add), reads=[d_posf, d_c], writes=[d_posf])
                P.op("dve", lambda e: e.tensor_tensor(out=base[:], in0=base[:], in1=pF[:, 128:160], op=ALU.add), reads=[d_base, d_pF, d_posf], writes=[d_base])
                for j in range(2):
                    P.op("dve", lambda e, j=j: e.tensor_tensor(out=oh[:, j * 32:(j + 1) * 32], in0=oh[:, j * 32:(j + 1) * 32], in1=posf[:], op=ALU.mult), reads=[d_oh, d_posf], writes=[d_oh])
                    P.op("dve", lambda e, j=j: e.tensor_reduce(out=idf2[:, j:j + 1], in_=oh[:, j * 32:(j + 1) * 32], axis=AX.X, op=ALU.add), reads=[d_oh], writes=[d_idf2])
                P.op("dve", lambda e, ti=ti: e.tensor_copy(out=idxs[:, ti, :], in_=idf2[:]), reads=[d_idf2], writes=[d_idxs])
                for j in range(2):
                    P.dma("pool", lambda e, ti=ti, j=j, hbuf=hbuf: e.indirect_dma_start(out=xbuf[:, :], out_offset=bass.IndirectOffsetOnAxis(ap=idxs[:, ti, j:j + 1], axis=0),
                                                                                      in_=h2b[hbuf][:], in_offset=None),
                          reads=[d_h2b[hbuf], d_idxs], writes=[d_xbuf])

        if stage < 3:
            return
        if hasattr(P, 'barrier'):
            P.barrier()
        with contextlib.ExitStack() as st:
            def sb(name, shape, dt):
                return st.enter_context(nc.sbuf_tensor("d_" + name, shape, dt))

            def ps(name, shape, dt=F32):
                return st.enter_context(nc.psum_tensor(name, shape, dt))
            NW = 2
            wge = [sb("wge%d" % i, [128, 8, 512], BF16) for i in range(NW)]; d_wge = [Dep() for _ in range(NW)]
            wue = [sb("wue%d" % i, [128, 8, 512], BF16) for i in range(NW)]; d_wue = [Dep() for _ in range(NW)]
            wde = [sb("wde%d" % i, [128, 4, 1024], BF16) for i in range(NW)]; d_wde = [Dep() for _ in range(NW)]
            xe = [sb("xe%d" % i, [128, 3, D], BF16) for i in range(NW)]; d_xe = [Dep() for _ in range(NW)]
            xeT = sb("xeT", [128, 8, CAP], BF16); d_xeT = Dep()
            sg = sb("sg", [128, CAP], F32); d_sg = Dep()
            hTe = sb("hTe", [128, 4, CAP], BF16); d_hTe = Dep()
            ye = [sb("ye%d" % i, [128, 3, D], F32) for i in range(NW)]; d_ye = [Dep() for _ in range(NW)]
            pT = ps("pTe", [128, 8, 128], BF16); d_pT = Dep()
            pg_ = [ps("pge%d" % i, [128, 512], F32) for i in range(2)]; d_pg = [Dep() for _ in range(2)]
            pu_ = [ps("pue%d" % i, [128, 512], F32) for i in range(2)]; d_pu = [Dep() for _ in range(2)]
            py_ = [ps("pye%d" % i, [128, 512], F32) for i in range(2)]; d_py = [Dep() for _ in range(2)]
            for ie, ex in enumerate(experts):
                b = ie % NW
                P.dma("pool", lambda e, b=b, ex=ex: e.dma_start(out=wge[b][:], in_=wgate_d[ex].rearrange("(k p) f -> p k f", p=128)), writes=[d_wge[b]])
                P.dma("pool", lambda e, b=b, ex=ex: e.dma_start(out=wue[b][:], in_=wup_d[ex].rearrange("(k p) f -> p k f", p=128)), writes=[d_wue[b]])
                P.dma("pool", lambda e, b=b, ex=ex: e.dma_start(out=wde[b][:], in_=wdown_d[ex].rearrange("(k p) f -> p k f", p=128)), writes=[d_wde[b]])
                P.dma("sp", lambda e, b=b, ex=ex: e.dma_start(out=xe[b][:], in_=xbuf[ex * CAP:(ex + 1) * CAP, :].rearrange("(s p) d -> p s d", p=128)),
                      reads=[d_xbuf], writes=[d_xe[b]])
                for s in range(3):
                    for k in range(8):
                        P.op("pe", lambda e, b=b, s=s, k=k: e.transpose(out=pT[:, k, :], in_=xe[b][:, s, k * 128:(k + 1) * 128], identity=idb[:]),
                             reads=[d_xe[b], d_idb], writes=[d_pT])
                    P.op("act", lambda e, s=s: e.copy(out=xeT[:, :, s * 128:(s + 1) * 128], in_=pT[:]), reads=[d_pT], writes=[d_xeT])
                for ft in range(4):
                    pb = ft % 2
                    for k in range(8):
                        P.op("pe", lambda e, b=b, ft=ft, k=k, pb=pb: e.matmul(pg_[pb][:, 0:CAP], lhsT=wge[b][:, k, ft * 128:(ft + 1) * 128], rhs=xeT[:, k, :],
                                                                            start=(k == 0), stop=(k == 7)), reads=[d_wge[b], d_xeT], writes=[d_pg[pb]])
                    for k in range(8):
                        P.op("pe", lambda e, b=b, ft=ft, k=k, pb=pb: e.matmul(pu_[pb][:, 0:CAP], lhsT=wue[b][:, k, ft * 128:(ft + 1) * 128], rhs=xeT[:, k, :],
                                                                            start=(k == 0), stop=(k == 7)), reads=[d_wue[b], d_xeT], writes=[d_pu[pb]])
                    P.op("act", lambda e, pb=pb: e.activation(out=sg[:], in_=pg_[pb][:, 0:CAP], func=AF.Silu), reads=[d_pg[pb]], writes=[d_sg])
                    P.op("dve", lambda e, ft=ft, pb=pb: e.tensor_tensor(out=hTe[:, ft, :], in0=sg[:], in1=pu_[pb][:, 0:CAP], op=ALU.mult),
                         reads=[d_sg, d_pu[pb]], writes=[d_hTe])
                for s in range(3):
                    for hv in range(2):
                        for ft in range(4):
                            P.op("pe", lambda e, b=b, s=s, hv=hv, ft=ft: e.matmul(py_[hv][:, :], lhsT=hTe[:, ft, s * 128:(s + 1) * 128], rhs=wde[b][:, ft, hv * 512:(hv + 1) * 512],
                                                                                start=(ft == 0), stop=(ft == 3)), reads=[d_hTe, d_wde[b]], writes=[d_py[hv]])
                        P.op("act" if hv == 0 else "dve", (lambda e, b=b, s=s, hv=hv: e.copy(out=ye[b][:, s, hv * 512:(hv + 1) * 512], in_=py_[hv][:, :])) if hv == 0 else
                             (lambda e, b=b, s=s, hv=hv: e.tensor_copy(out=ye[b][:, s, hv * 512:(hv + 1) * 512], in_=py_[hv][:, :])),
                             reads=[d_py[hv]], writes=[d_ye[b]])
                P.dma("sp", lambda e, b=b, ex=ex: e.dma_start(out=ybuf[ex * CAP:(ex + 1) * CAP, :].rearrange("(s p) d -> p s d", p=128), in_=ye[b][:]),
                      reads=[d_ye[b]], writes=[d_ybuf])

        if hasattr(P, 'barrier'):
            P.barrier()
        with contextlib.ExitStack() as st:
            def sb(name, shape, dt):
                return st.enter_context(nc.sbuf_tensor("d_" + name, shape, dt))
            NF = 4
            x1t = [sb("x1t%d" % i, [128, D], F32) for i in range(NF)]; d_x1t = [Dep() for _ in range(NF)]
            y1 = [sb("y1t%d" % i, [128, D], F32) for i in range(NF)]; d_y1 = [Dep() for _ in range(NF)]
            y2 = [sb("y2t%d" % i, [128, D], F32) for i in range(NF)]; d_y2 = [Dep() for _ in range(NF)]
            ot = [sb("ot%d" % i, [128, D], F32) for i in range(NF)]; d_ot = [Dep() for _ in range(NF)]
            for ti in range(ntiles):
                b = ti % NF
                r0 = ti * 128
                P.dma("sp", lambda e, b=b, r0=r0: e.dma_start(out=x1t[b][:], in_=x1_d[r0:r0 + 128, :]), reads=[d_x1d], writes=[d_x1t[b]])
                P.dma("pool", lambda e, b=b, ti=ti: e.indirect_dma_start(out=y1[b][:], out_offset=None, in_=ybuf[:, :],
                                                                        in_offset=bass.IndirectOffsetOnAxis(ap=idxs[:, ti, 0:1], axis=0)),
                      reads=[d_ybuf, d_idxs], writes=[d_y1[b]])
                P.dma("pool", lambda e, b=b, ti=ti: e.indirect_dma_start(out=y2[b][:], out_offset=None, in_=ybuf[:, :],
                                                                        in_offset=bass.IndirectOffsetOnAxis(ap=idxs[:, ti, 1:2], axis=0)),
                      reads=[d_ybuf, d_idxs], writes=[d_y2[b]])
                P.op("dve", lambda e, b=b, ti=ti: e.scalar_tensor_tensor(out=ot[b][:], in0=y1[b][:], scalar=combs[:, ti, 0:1], in1=x1t[b][:], op0=ALU.mult, op1=ALU.add),
                     reads=[d_y1[b], d_combs, d_x1t[b]], writes=[d_ot[b]])
                P.op("dve", lambda e, b=b, ti=ti: e.scalar_tensor_tensor(out=ot[b][:], in0=y2[b][:], scalar=combs[:, ti, 1:2], in1=ot[b][:], op0=ALU.mult, op1=ALU.add),
                     reads=[d_y2[b], d_combs, d_ot[b]], writes=[d_ot[b]])
                P.dma("sp", lambda e, b=b, r0=r0: e.dma_start(out=out_d[r0:r0 + 128, :], in_=ot[b][:]), reads=[d_ot[b]], writes=[d_out])


def host_consts_d(z):
    tri = (np.arange(128)[:, None] <= np.arange(128)[None, :]).astype(np.float32)
    return dict(mqn=np.tile(z['mem_q_norm'], 4)[None, :], mkn=np.tile(z['mem_k_norm'], 4)[None, :],
                w_r=np.ascontiguousarray(np.concatenate([z['w_router_group'], z['w_router_expert']], axis=1)),
                trif=tri, ecap=(np.arange(32, dtype=np.float32) * CAP)[None, :], ident=np.eye(128, dtype=np.float32))


def build_full():
    nc = bass.Bass("TRN2", target_bir_lowering=False)
    P = Prog(nc)

    def di(name, shape, dt=F32):
        return nc.dram_tensor(name, shape, dt, kind="ExternalInput")
    xin = di("xin", [TP + T, D]); w_in = di("w_in", [D, IN_COLS]); g_mix = di("g_mix", [1, D])
    qkn = di("qkn", [2, 512]); cs = di("cs", [TP + T, 64]); ident_d = di("ident", [128, 128])
    e_d = di("e_d", [32, NK], BF16); vq_d = di("vq", [1, 1024]); nb_d = di("nb", [1, 1024]); oq_d = di("oq", [1, 1024])
    tri_d = di("tri", [128, 128], BF16)
    cwl_d = di("cwl", [128, 16, 4]); cbl_d = di("cbl", [128, 16]); dtb_d = di("dtb", [1, 16]); alog_d = di("alog", [1, 16])
    dsk_d = di("dsk", [1, 16]); ssdn_d = di("ssdn", [1, D]); tri2_d = di("tri2", [128, 2, 256]); sel_d = di("sel_d", [16, 16, 128])
    tb_d = di("tb_d", [128, 384]); pflag_d = di("pflag", [1, 1])
    mem_d = di("mem", [256, D]); gmem_d = di("g_mem", [1, D]); wmemkv = di("w_mem_kv", [D, 1024])
    mqn_d = di("mqn", [1, 512]); mkn_d = di("mkn", [1, 512])
    woa_d = di("w_o_moba", [512, D]); wos_d = di("w_o_ssd", [D, D]); wom_d = di("w_o_mem", [512, D]); wout_d = di("w_out", [D, D])
    gffn_d = di("g_ffn", [1, D]); wr_d = di("w_r", [D, 36])
    wgate_d = di("w_gate", [NE, D, 512]); wup_d = di("w_up", [NE, D, 512]); wdown_d = di("w_down", [NE, 512, D])
    trif_d = di("trif", [128, 128]); ecap_d = di("ecap", [1, 32])
    out_d = nc.dram_tensor("out", [T, D], F32, kind="ExternalOutput")
    kT_d = nc.dram_tensor("kT_d", [512, NK], BF16)
    v_d = nc.dram_tensor("v_d", [NK, 512], BF16)
    qT_d = nc.dram_tensor("qT_d", [512, T], BF16)
    oa_d = nc.dram_tensor("oa_d", [T, 512], BF16)
    os_d = nc.dram_tensor("os_d", [T, D], BF16)
    x1_d = nc.dram_tensor("x1_d", [T, D], F32)
    xbuf = nc.dram_tensor("xbuf", [NE * CAP, D], BF16)
    ybuf = nc.dram_tensor("ybuf", [NE * CAP, D], F32)

    with contextlib.ExitStack() as st0:
        idf = st0.enter_context(nc.sbuf_tensor("idf", [128, 128], F32)); d_idf = Dep()
        idb = st0.enter_context(nc.sbuf_tensor("idb", [128, 128], BF16)); d_idb = Dep()
        P.dma("sp", lambda e: e.dma_start(out=idf[:], in_=ident_d[:, :]), writes=[d_idf])
        P.op("dve", lambda e: e.tensor_copy(out=idb[:], in_=idf[:]), reads=[d_idf], writes=[d_idb])
        with contextlib.ExitStack() as st:
            phase_a(nc, P, st, xin, w_in, g_mix, qkn, cs, idb, d_idb, kT_d, v_d, qT_d)
        P.barrier()
        with contextlib.ExitStack() as st:
            oa = st.enter_context(nc.sbuf_tensor("s_oa", [128, 32, 512], BF16)); d_oa = Dep()
            phase_b(nc, P, st, kT_d, v_d, qT_d, e_d, vq_d, nb_d, oq_d, tri_d, idb, d_idb, oa, d_oa)
            P.dma("sp", lambda e: e.dma_start(out=oa_d[:, :].rearrange("(t p) c -> p t c", p=128), in_=oa[:]), reads=[d_oa])
        P.barrier()
        with contextlib.ExitStack() as st:
            phase_c(nc, P, st, xin, w_in, g_mix, cwl_d, cbl_d, dtb_d, alog_d, dsk_d, ssdn_d, tri2_d, sel_d, tb_d, pflag_d,
                    idb, d_idb, os_d, Dep())
        P.barrier()
        phase_def(nc, P, xin[TP:TP + T, :], w_in, g_mix, mem_d, gmem_d, wmemkv, mqn_d, mkn_d, woa_d, wos_d, wom_d, wout_d, gffn_d, wr_d,
                  wgate_d, wup_d, wdown_d, trif_d, ecap_d, oa_d, Dep(), os_d, Dep(), x1_d, xbuf, ybuf, out_d, idb, d_idb, idf, d_idf)
        P.emit()
    return nc


_NC_CACHE = {}


def kernel(x, mem, g_mix, w_in, moba_q_norm, moba_k_norm, conv_w, conv_b, dt_bias, a_log, d_skip, ssd_norm, g_mem, w_mem_kv,
           mem_q_norm, mem_k_norm, w_o_moba, w_o_ssd, w_o_mem, w_out, g_ffn, w_router_group, w_router_expert, w_gate, w_up, w_down):
    f32 = np.float32
    A = lambda a: np.ascontiguousarray(np.asarray(a, dtype=f32))
    x = A(x); mem = A(mem)
    z = dict(conv_w=A(conv_w), conv_b=A(conv_b), dt_bias=A(dt_bias), a_log=A(a_log), d_skip=A(d_skip), ssd_norm=A(ssd_norm),
             mem_q_norm=A(mem_q_norm), mem_k_norm=A(mem_k_norm), w_router_group=A(w_router_group), w_router_expert=A(w_router_expert))
    if "nc" not in _NC_CACHE:
        _NC_CACHE["nc"] = build_full()
    nc = _NC_CACHE["nc"]
    pos = np.arange(2 * T, dtype=f32)
    inv = (10000.0 ** (-np.arange(32, dtype=f32) / 32)).astype(f32)
    ang = pos[:, None] * inv[None, :]
    cs_full = np.concatenate([np.cos(ang), np.sin(ang)], axis=1).astype(f32)
    qkn = np.stack([np.tile(A(moba_q_norm), 8), np.tile(A(moba_k_norm), 8)]).astype(f32)
    shared = dict(w_in=A(w_in), g_mix=A(g_mix)[None, :], qkn=qkn, ident=np.eye(128, dtype=f32),
                  g_mem=A(g_mem)[None, :], w_mem_kv=A(w_mem_kv), w_o_moba=A(w_o_moba), w_o_ssd=A(w_o_ssd), w_o_mem=A(w_o_mem),
                  w_out=A(w_out), g_ffn=A(g_ffn)[None, :], w_gate=A(w_gate), w_up=A(w_up), w_down=A(w_down))
    shared.update(host_consts_d(z))
    in_maps = []
    for c in range(8):
        bb, hh = c // 2, c % 2
        if hh == 0:
            xin = np.concatenate([np.zeros((TP, D), f32), x[bb, :T]], 0)
            csc = np.concatenate([cs_full[:TP], cs_full[:T]], 0)
        else:
            xin = x[bb]
            csc = cs_full
        m = dict(shared)
        m.update(xin=np.ascontiguousarray(xin), cs=np.ascontiguousarray(csc), mem=mem[bb])
        m.update(host_consts(hh))
        m.update(host_consts_c(z, hh))
        in_maps.append(m)
    res = run_bass_kernel_spmd(nc, in_maps, core_ids=list(range(8)))
    out = np.empty((4, 2 * T, D), f32)
    for c in range(8):
        bb, hh = c // 2, c % 2
        out[bb, hh * T:(hh + 1) * T] = np.asarray(res.results[c]["out"], dtype=f32)
    return out
```

```python
import contextlib
import numpy as np
import ml_dtypes
import concourse.bass as bass
import concourse.mybir as mybir
from concourse.bass_utils import run_bass_kernel_spmd

F32 = mybir.dt.float32
BF16 = mybir.dt.bfloat16
I32 = mybir.dt.int32
U32 = mybir.dt.uint32
AF = mybir.ActivationFunctionType
ALU = mybir.AluOpType
AX = mybir.AxisListType

T = 4096
TP = 4096
NK = TP + T
D = 1024
EPS = 1e-6
IN_COLS = 8208
BIGG = 30000.0
MB = 1000.0
NEG = -30000.0
C_Z, C_X, C_DT = 1536, 2560, 4608
C_QM, C_G = 4624, 5136
CAP = 384
NE = 32


class Dep:
    __slots__ = ("w", "r")

    def __init__(self):
        self.w = None
        self.r = {}


class Op:
    __slots__ = ("eng", "fn", "deps", "is_dma", "sig", "sigval", "dsem", "dval", "prev")

    def __init__(self, eng, fn, is_dma):
        self.eng = eng
        self.fn = fn
        self.is_dma = is_dma
        self.deps = []
        self.sig = False
        self.sigval = 0
        self.dsem = None
        self.dval = 0
        self.prev = None


class Prog:
    ENGS = ("pe", "act", "dve", "pool", "sp")

    def __init__(self, nc, ndma_sems=12):
        self.nc = nc
        self.ops = {e: [] for e in self.ENGS}
        self.ndma = {e: 0 for e in self.ENGS}
        self.dma_last = {}
        self.ndma_sems = ndma_sems
        self.all_dma = []

    def _add(self, o, reads, writes):
        deps = {}
        raw = set()
        for t in reads:
            if t.w is not None:
                deps[id(t.w)] = t.w
                raw.add(id(t.w))
        for t in writes:
            if t.w is not None:
                deps[id(t.w)] = t.w
            for r in t.r.values():
                deps[id(r)] = r
        for t in reads:
            key = id(o) if o.is_dma else o.eng
            t.r[key] = o
        for t in writes:
            t.w = o
            t.r = {}
        dl = []
        for d in deps.values():
            if d is o:
                continue
            if (not d.is_dma) and (not o.is_dma) and d.eng == "pe" and o.eng == "pe":
                continue
            if (not d.is_dma) and (not o.is_dma) and d.eng == o.eng and id(d) not in raw:
                continue
            dl.append(d)
            if not d.is_dma:
                d.sig = True
        o.deps = dl
        self.ops[o.eng].append(o)
        return o

    def op(self, eng, fn, reads=(), writes=()):
        return self._add(Op(eng, fn, False), reads, writes)

    def dma(self, eng, fn, reads=(), writes=()):
        o = Op(eng, fn, True)
        n = self.ndma[eng]
        self.ndma[eng] += 1
        slot = (eng, n % self.ndma_sems)
        o.dsem = slot
        o.prev = self.dma_last.get(slot)
        o.dval = (o.prev.dval if o.prev else 0) + 16
        self.dma_last[slot] = o
        self.all_dma.append(o)
        return self._add(o, reads, writes)


    def barrier(self):
        lasts = []
        for e in self.ENGS:
            for o in reversed(self.ops[e]):
                if (not o.is_dma) and o.fn is not None:
                    o.sig = True
                    lasts.append(o)
                    break
        dmas = list(self.dma_last.values())
        for e in self.ENGS:
            o = Op(e, None, False)
            o.deps = [d for d in lasts if d.eng != e] + dmas
            self.ops[e].append(o)

    def emit(self):
        nc = self.nc
        import contextlib
        with contextlib.ExitStack() as st:
            esem = {e: st.enter_context(nc.semaphore("S_" + e)) for e in self.ENGS}
            dsem = {}
            for e in self.ENGS:
                if self.ndma[e]:
                    for i in range(min(self.ndma_sems, self.ndma[e])):
                        dsem[(e, i)] = st.enter_context(nc.semaphore("D_%s_%d" % (e, i)))
            for e in self.ENGS:
                c = 0
                for o in self.ops[e]:
                    if (not o.is_dma) and o.sig and o.fn is not None:
                        c += 1
                        o.sigval = c
            block = st.enter_context(nc.Block())
            handles = {"pe": block.tensor, "act": block.scalar, "dve": block.vector,
                       "pool": block.gpsimd, "sp": block.sync}

            def make(e):
                def body(eng):
                    known = {}

                    def wait(sem, key, val):
                        if known.get(key, 0) < val:
                            eng.wait_ge(sem, val)
                            known[key] = val
                    for o in self.ops[e]:
                        for d in o.deps:
                            if d.is_dma:
                                wait(dsem[d.dsem], d.dsem, d.dval)
                            else:
                                wait(esem[d.eng], d.eng, d.sigval)
                        if o.is_dma:
                            if o.prev is not None:
                                wait(dsem[o.dsem], o.dsem, o.prev.dval)
                            o.fn(eng).then_inc(dsem[o.dsem], 16)
                        elif o.fn is not None:
                            ins = o.fn(eng)
                            if o.sig:
                                ins.then_inc(esem[e], 1)
                    if e == "sp":
                        for slot, o in self.dma_last.items():
                            wait(dsem[slot], slot, o.dval)
                return body
            for e in self.ENGS:
                if self.ops[e] or e == "sp":
                    handles[e](make(e))


def phase_a(nc, P, st, xin, w_in, g_mix, qkn, cs, idb, d_idb, kT_d, v_d, qT_d):
    def sb(name, shape, dt):
        return st.enter_context(nc.sbuf_tensor("a_" + name, shape, dt))

    def ps(name, shape, dt=F32):
        return st.enter_context(nc.psum_tensor("a_" + name, shape, dt))
    wq = sb("wq", [128, 8, 1536], BF16); d_wq = Dep()
    gm = sb("gm", [128, D], F32); d_gm = Dep()
    gq = sb("gq", [128, 2, 512], F32); d_gq = Dep()
    NB = 2
    NX = 4
    xt = [sb("xt%d" % i, [128, D], F32) for i in range(NX)]; d_xt = [Dep() for _ in range(NX)]
    cst = [sb("cst%d" % i, [128, 64], F32) for i in range(NX)]; d_cst = [Dep() for _ in range(NX)]
    junk = sb("junk", [128, D], BF16); d_junk = Dep()
    ssq = sb("ssq", [128, 1], F32); d_ssq = Dep()
    rstd = sb("rstd", [128, 1], F32); d_rstd = Dep()
    hb_ = [sb("hb%d" % i, [128, D], BF16) for i in range(2)]; d_hb_ = [Dep(), Dep()]
    hT = [sb("hT%d" % i, [128, 8, 128], BF16) for i in range(NB)]; d_hT = [Dep() for _ in range(NB)]
    pT = ps("pT", [128, 8, 128], BF16); d_pT = Dep()
    pq = [[ps("pq%d_%d" % (j, i), [128, 512], F32) for i in range(3)] for j in range(2)]; d_pq = [[Dep() for _ in range(3)] for _ in range(2)]
    pkT = ps("pkT", [128, 4, 128], BF16); d_pkT = Dep()
    sq_ = [sb("sq%d" % i, [128, 512], F32) for i in range(2)]; d_sq_ = [Dep(), Dep()]
    hs_ = [sb("hs%d" % i, [128, 8], F32) for i in range(2)]; d_hs_ = [Dep(), Dep()]
    hr_ = [sb("hr%d" % i, [128, 8], F32) for i in range(2)]; d_hr_ = [Dep(), Dep()]
    qn_ = [sb("qn%d" % i, [128, 512], F32) for i in range(2)]; d_qn_ = [Dep(), Dep()]
    t1_ = [sb("t1%d" % i, [128, 256], F32) for i in range(2)]; d_t1_ = [Dep(), Dep()]
    t2_ = [sb("t2%d" % i, [128, 256], F32) for i in range(2)]; d_t2_ = [Dep(), Dep()]
    t3_ = [sb("t3%d" % i, [128, 256], F32) for i in range(2)]; d_t3_ = [Dep(), Dep()]
    t4_ = [sb("t4%d" % i, [128, 256], F32) for i in range(2)]; d_t4_ = [Dep(), Dep()]
    qr_ = [[sb("qr%d_%d" % (i, j), [128, 512], BF16) for j in range(2)] for i in range(2)]; d_qr_ = [[Dep(), Dep()] for _ in range(2)]
    kTs = [[sb("kTs%d_%d" % (j, i), [128, 4, 128], BF16) for i in range(NB)] for j in range(2)]; d_kTs = [[Dep() for _ in range(NB)] for _ in range(2)]
    vs = [sb("vs%d" % i, [128, 512], BF16) for i in range(NB)]; d_vs = [Dep() for _ in range(NB)]

    for k in range(8):
        P.dma("pool", lambda e, k=k: e.dma_start(out=wq[:, k, :], in_=w_in[k * 128:(k + 1) * 128, 0:1536]), writes=[d_wq])
    P.dma("sp", lambda e: e.dma_start(out=gm[:], in_=g_mix[0:1, :].partition_broadcast(128)), writes=[d_gm])
    for i in range(2):
        P.dma("sp", lambda e, i=i: e.dma_start(out=gq[:, i, :], in_=qkn[i:i + 1, :].partition_broadcast(128)), writes=[d_gq])
    P.op("dve", lambda e: e.tensor_scalar(out=gq[:, 0, :], in0=gq[:, 0, :], scalar1=0.125, scalar2=None, op0=ALU.mult),
         reads=[d_gq], writes=[d_gq])

    def qk_post_steps(src_ps, d_src, which, b, xs):
        sl = which
        sq, hs, hr, qn, t1, t2, t3, t4, qr = sq_[sl], hs_[sl], hr_[sl], qn_[sl], t1_[sl], t2_[sl], t3_[sl], t4_[sl], qr_[sl][b]
        d_sq, d_hs, d_hr, d_qn, d_t1, d_t2, d_t3, d_t4, d_qr = (d_sq_[sl], d_hs_[sl], d_hr_[sl], d_qn_[sl], d_t1_[sl], d_t2_[sl],
                                                               d_t3_[sl], d_t4_[sl], d_qr_[sl][b])
        q3 = qn[:].rearrange("p (h d) -> p h d", h=8)
        q1 = q3[:, :, 0:32]
        q2 = q3[:, :, 32:64]
        cosb = cst[xs][:, 0:32].unsqueeze(1).to_broadcast([128, 8, 32])
        sinb = cst[xs][:, 32:64].unsqueeze(1).to_broadcast([128, 8, 32])
        v3 = lambda t: t[:].rearrange("p (h d) -> p h d", h=8)
        r3 = qr[:].rearrange("p (h d) -> p h d", h=8)
        steps = [
            lambda: P.op("act", lambda e: e.activation(out=sq[:], in_=src_ps[:], func=AF.Square), reads=[d_src], writes=[d_sq]),
            lambda: P.op("dve", lambda e: e.tensor_reduce(out=hs[:], in_=sq[:].rearrange("p (h d) -> p h d", h=8), axis=AX.X, op=ALU.add),
                         reads=[d_sq], writes=[d_hs]),
            lambda: P.op("act", lambda e: e.activation(out=hr[:], in_=hs[:], func=AF.Sqrt, bias=EPS, scale=1.0 / 64), reads=[d_hs], writes=[d_hr]),
            lambda: P.op("dve", lambda e: e.reciprocal(out=hr[:], in_=hr[:]), reads=[d_hr], writes=[d_hr]),
            lambda: P.op("dve", lambda e: e.tensor_tensor(out=qn[:].rearrange("p (h d) -> p h d", h=8), in0=src_ps[:].rearrange("p (h d) -> p h d", h=8),
                                                          in1=hr[:].unsqueeze(2).to_broadcast([128, 8, 64]), op=ALU.mult),
                         reads=[d_src, d_hr], writes=[d_qn]),
            lambda: P.op("pool", lambda e: e.tensor_tensor(out=qn[:], in0=qn[:], in1=gq[:, which, :], op=ALU.mult), reads=[d_qn, d_gq], writes=[d_qn]),
            lambda: P.op("dve", lambda e: e.tensor_tensor(out=v3(t1), in0=q1, in1=cosb, op=ALU.mult), reads=[d_qn, d_cst[xs]], writes=[d_t1]),
            lambda: P.op("pool", lambda e: e.tensor_tensor(out=v3(t2), in0=q2, in1=sinb, op=ALU.mult), reads=[d_qn, d_cst[xs]], writes=[d_t2]),
            lambda: P.op("dve", lambda e: e.tensor_tensor(out=v3(t3), in0=q2, in1=cosb, op=ALU.mult), reads=[d_qn, d_cst[xs]], writes=[d_t3]),
            lambda: P.op("pool", lambda e: e.tensor_tensor(out=v3(t4), in0=q1, in1=sinb, op=ALU.mult), reads=[d_qn, d_cst[xs]], writes=[d_t4]),
            lambda: P.op("dve", lambda e: e.tensor_tensor(out=r3[:, :, 0:32], in0=v3(t1), in1=v3(t2), op=ALU.subtract), reads=[d_t1, d_t2], writes=[d_qr]),
            lambda: P.op("pool", lambda e: e.tensor_tensor(out=r3[:, :, 32:64], in0=v3(t3), in1=v3(t4), op=ALU.add), reads=[d_t3, d_t4], writes=[d_qr]),
        ]
        return steps

    def to_T_and_store(dst_dram, col0, b, sl):
        for c in range(4):
            P.op("pe", lambda e, c=c: e.transpose(out=pkT[:, c, :], in_=qr_[sl][b][:, c * 128:(c + 1) * 128], identity=idb[:]),
                 reads=[d_qr_[sl][b], d_idb], writes=[d_pkT])
        P.op("act", lambda e: e.copy(out=kTs[sl][b][:], in_=pkT[:]), reads=[d_pkT], writes=[d_kTs[sl][b]])
        P.dma("sp", lambda e: e.dma_start(out=dst_dram[:, col0:col0 + 128].rearrange("(c p) t -> p c t", p=128), in_=kTs[sl][b][:]),
              reads=[d_kTs[sl][b]])

    def load(ti):
        xs = ti % NX
        r0 = ti * 128
        P.dma("sp", lambda e: e.dma_start(out=xt[xs][:], in_=xin[r0:r0 + 128, :]), writes=[d_xt[xs]])
        P.dma("sp", lambda e: e.dma_start(out=cst[xs][:], in_=cs[r0:r0 + 128, :]), writes=[d_cst[xs]])

    def norm(ti):
        b = ti % NB
        xs = ti % NX
        hb = hb_[b]
        P.op("act", lambda e: e.activation(out=junk[:], in_=xt[xs][:], func=AF.Square, accum_out=ssq[:]), reads=[d_xt[xs]], writes=[d_junk, d_ssq])
        P.op("act", lambda e: e.activation(out=rstd[:], in_=ssq[:], func=AF.Sqrt, bias=EPS, scale=1.0 / D), reads=[d_ssq], writes=[d_rstd])
        P.op("dve", lambda e: e.reciprocal(out=rstd[:], in_=rstd[:]), reads=[d_rstd], writes=[d_rstd])
        P.op("dve", lambda e: e.scalar_tensor_tensor(out=hb[:], in0=xt[xs][:], scalar=rstd[:, 0:1], in1=gm[:], op0=ALU.mult, op1=ALU.mult),
             reads=[d_xt[xs], d_rstd, d_gm], writes=[d_hb_[b]])

    def front(ti):
        b = ti % NB
        xs = ti % NX
        own = ti >= 32
        hb = hb_[b]
        for k in range(8):
            P.op("pe", lambda e, k=k: e.transpose(out=pT[:, k, :], in_=hb[:, k * 128:(k + 1) * 128], identity=idb[:]), reads=[d_hb_[b], d_idb], writes=[d_pT])
        P.op("act", lambda e: e.copy(out=hT[b][:], in_=pT[:]), reads=[d_pT], writes=[d_hT[b]])
        groups = [0, 1, 2] if own else [1, 2]
        for g in groups:
            for k in range(8):
                P.op("pe", lambda e, g=g, k=k: e.matmul(pq[b][g][:], lhsT=hT[b][:, k, :], rhs=wq[:, k, g * 512:(g + 1) * 512], start=(k == 0), stop=(k == 7)),
                     reads=[d_hT[b], d_wq], writes=[d_pq[b][g]])

    def post(ti):
        b = ti % NB
        xs = ti % NX
        own = ti >= 32
        ks = qk_post_steps(pq[b][1], d_pq[b][1], 1, b, xs)
        qs = qk_post_steps(pq[b][0], d_pq[b][0], 0, b, xs) if own else []
        for i in range(len(ks)):
            ks[i]()
            if qs:
                qs[i]()
        P.op("act", lambda e: e.copy(out=vs[b][:], in_=pq[b][2][:]), reads=[d_pq[b][2]], writes=[d_vs[b]])

    def tail(ti):
        b = ti % NB
        own = ti >= 32
        r0 = ti * 128
        to_T_and_store(kT_d, r0, b, 1)
        if own:
            to_T_and_store(qT_d, r0 - TP, b, 0)
        P.dma("sp", lambda e: e.dma_start(out=v_d[r0:r0 + 128, :], in_=vs[b][:]), reads=[d_vs[b]])

    load(0)
    load(1)
    load(2)
    norm(0)
    norm(1)
    front(0)
    for ti in range(64):
        if ti + 3 < 64:
            load(ti + 3)
        if ti + 2 < 64:
            norm(ti + 2)
        if ti + 1 < 64:
            front(ti + 1)
        if ti >= 1:
            tail(ti - 1)
        post(ti)
    tail(63)


def phase_b(nc, P, st, kT_d, v_d, qT_d, e_d, vq_d, nb_d, oq_d, tri_d, idb, d_idb, oa, d_oa, heads=range(8), nblk=16):
    def sb(name, shape, dt):
        return st.enter_context(nc.sbuf_tensor("b_" + name, shape, dt))

    def ps(name, shape, dt=F32):
        return st.enter_context(nc.psum_tensor(name, shape, dt))
    NB = 2
    kaug = [sb("kaug%d" % i, [96, NK], BF16) for i in range(NB)]; d_kaug = [Dep() for _ in range(NB)]
    vaug = [sb("vaug%d" % i, [128, 64, 65], BF16) for i in range(NB)]; d_vaug = [Dep() for _ in range(NB)]
    qaug = [sb("qaug%d" % i, [96, T], BF16) for i in range(NB)]; d_qaug = [Dep() for _ in range(NB)]
    d_qmb = [Dep() for _ in range(NB)]
    vq = sb("vq_s", [128, 1024], F32); d_c = Dep()
    nb = sb("nb_s", [128, 1024], F32)
    oq = sb("oq_s", [128, 1024], F32)
    tri = sb("tri_s", [128, 128], BF16)
    ksum = sb("ksum", [64, 32], F32); d_ksum = Dep()
    kmean = sb("kmean", [64, 32], BF16); d_kmean = Dep()
    g1 = sb("g1", [128, 512], F32); d_g1 = Dep()
    top8 = sb("top8", [128, 16, 8], F32); d_top8 = Dep()
    sel = sb("sel", [128, 512], F32); d_sel = Dep()
    mbp = sb("mbp", [128, 16, 96], BF16); d_mbp = Dep()
    NPT = 3
    pt = [sb("pt%d" % i, [128, 512], BF16) for i in range(NPT)]; d_pt = [Dep() for _ in range(NPT)]
    rc = sb("rc", [128, 2], F32); d_rc = [Dep(), Dep()]
    pg = ps("pg", [128, 512], F32); d_pg = Dep()
    pmT = ps("pmT", [96, 8, 128], BF16); d_pmT = Dep()
    psS = [ps("psS%d" % i, [128, 512], F32) for i in range(NPT)]; d_psS = [Dep() for _ in range(NPT)]
    po = [ps("po%d" % i, [128, 65], F32) for i in range(2)]; d_po = [Dep() for _ in range(2)]

    for i in range(NB):
        P.dma("sp", lambda e, i=i: e.dma_start(out=kaug[i][64:96, :], in_=e_d[:, :]), writes=[d_kaug[i]])
        P.op("pool", lambda e, i=i: e.memset(vaug[i][:], 1.0), writes=[d_vaug[i]])
    P.dma("sp", lambda e: e.dma_start(out=vq[:], in_=vq_d[0:1, :].partition_broadcast(128)), writes=[d_c])
    P.dma("sp", lambda e: e.dma_start(out=nb[:], in_=nb_d[0:1, :].partition_broadcast(128)), writes=[d_c])
    P.dma("sp", lambda e: e.dma_start(out=oq[:], in_=oq_d[0:1, :].partition_broadcast(128)), writes=[d_c])
    P.dma("pool", lambda e: e.dma_start(out=tri[:], in_=tri_d[:, :]), writes=[d_c])
    P.op("pool", lambda e: e.memset(mbp[:], 0.0), writes=[d_mbp])

    for ih, h in enumerate(heads):
        b = ih % NB
        P.dma("sp", lambda e, b=b, h=h: e.dma_start(out=kaug[b][0:64, :], in_=kT_d[h * 64:(h + 1) * 64, :]), writes=[d_kaug[b]])
        P.dma("sp", lambda e, b=b, h=h: e.dma_start(out=vaug[b][:, :, 0:64],
                                                    in_=v_d[:, h * 64:(h + 1) * 64].rearrange("(t p) d -> p t d", p=128)),
              writes=[d_vaug[b]])
        P.dma("sp", lambda e, b=b, h=h: e.dma_start(out=qaug[b][0:64, :], in_=qT_d[h * 64:(h + 1) * 64, :]), writes=[d_qaug[b]])
        P.op("dve", lambda e, b=b: e.tensor_reduce(out=ksum[:], in_=kaug[b][0:64, :].rearrange("p (b k) -> p b k", k=256),
                                                   axis=AX.X, op=ALU.add), reads=[d_kaug[b]], writes=[d_ksum])
        P.op("dve", lambda e: e.tensor_scalar(out=kmean[:], in0=ksum[:], scalar1=1.0 / 256, scalar2=None, op0=ALU.mult),
             reads=[d_ksum], writes=[d_kmean])
        for half in range(2):
            for j in range(16):
                qt = half * 16 + j
                P.op("pe", lambda e, b=b, j=j, qt=qt: e.matmul(pg[:, j * 32:(j + 1) * 32], lhsT=qaug[b][0:64, qt * 128:(qt + 1) * 128],
                                                              rhs=kmean[:, :], start=True, stop=True),
                     reads=[d_qaug[b], d_kmean], writes=[d_pg])
            cs_ = slice(half * 512, (half + 1) * 512)
            P.op("dve", lambda e, cs_=cs_: e.tensor_tensor(out=g1[:], in0=pg[:], in1=vq[:, cs_], op=ALU.mult),
                 reads=[d_pg, d_c], writes=[d_g1])
            P.op("dve", lambda e, cs_=cs_: e.tensor_tensor(out=g1[:], in0=g1[:], in1=nb[:, cs_], op=ALU.add),
                 reads=[d_g1, d_c], writes=[d_g1])
            for j in range(16):
                P.op("dve", lambda e, j=j: e.max(out=top8[:, j, :], in_=g1[:, j * 32:(j + 1) * 32]), reads=[d_g1], writes=[d_top8])
            P.op("dve", lambda e: e.tensor_tensor(out=sel[:].rearrange("p (j b) -> p j b", b=32),
                                                  in0=g1[:].rearrange("p (j b) -> p j b", b=32),
                                                  in1=top8[:, :, 2:3].to_broadcast([128, 16, 32]), op=ALU.is_ge),
                 reads=[d_g1, d_top8], writes=[d_sel])
            P.op("dve", lambda e, cs_=cs_: e.tensor_tensor(out=sel[:], in0=sel[:], in1=vq[:, cs_], op=ALU.mult),
                 reads=[d_sel, d_c], writes=[d_sel])
            P.op("dve", lambda e, cs_=cs_: e.tensor_tensor(out=sel[:], in0=sel[:], in1=oq[:, cs_], op=ALU.add),
                 reads=[d_sel, d_c], writes=[d_sel])
            P.op("dve", lambda e: e.tensor_scalar(out=mbp[:, :, 64:96], in0=sel[:].rearrange("p (j b) -> p j b", b=32),
                                                  scalar1=1.0, scalar2=MB, op0=ALU.subtract, op1=ALU.mult),
                 reads=[d_sel], writes=[d_mbp])
            for grp in range(2):
                for j in range(8):
                    P.op("pe", lambda e, grp=grp, j=j: e.transpose(out=pmT[:, j, :], in_=mbp[:, grp * 8 + j, :], identity=idb[:]),
                         reads=[d_mbp, d_idb], writes=[d_pmT])
                c0 = (half * 16 + grp * 8) * 128
                P.op("act", lambda e, b=b, c0=c0: e.copy(out=qaug[b][64:96, c0:c0 + 1024], in_=pmT[64:96, :, :]),
                     reads=[d_pmT], writes=[d_qmb[b]])
        units = []
        for jb in range(nblk):
            ncommon = 32 + 2 * jb
            for u in range(ncommon // 2):
                units.append((jb, [2 * u, 2 * u + 1], False, u == 0))
            units.append((jb, [32 + 2 * jb, 32 + 2 * jb + 1], True, False))

        def emit_S(u, s, b=b):
            jb, kts, diag, first = u
            for j, kt in enumerate(kts):
                if diag and j == 1:
                    q0, nq, c0 = jb * 256 + 128, 128, 256
                else:
                    q0, nq, c0 = jb * 256, 256, j * 256
                P.op("pe", lambda e, s=s, kt=kt, q0=q0, nq=nq, c0=c0: e.matmul(psS[s][:, c0:c0 + nq], lhsT=kaug[b][0:96, kt * 128:(kt + 1) * 128],
                                                                             rhs=qaug[b][0:96, q0:q0 + nq], start=True, stop=True),
                     reads=[d_kaug[b], d_qaug[b], d_qmb[b]], writes=[d_psS[s]])

        def emit_rest(u, s, b=b, h=h):
            jb, kts, diag, first = u
            ncols = 384 if diag else 512
            P.op("act", lambda e, s=s, ncols=ncols: e.activation(out=pt[s][:, 0:ncols], in_=psS[s][:, 0:ncols], func=AF.Exp),
                 reads=[d_psS[s]], writes=[d_pt[s]])
            if diag:
                P.op("pool", lambda e, s=s: e.tensor_tensor(out=pt[s][:, 0:128], in0=pt[s][:, 0:128], in1=tri[:], op=ALU.mult),
                     reads=[d_pt[s], d_c], writes=[d_pt[s]])
                P.op("pool", lambda e, s=s: e.tensor_tensor(out=pt[s][:, 256:384], in0=pt[s][:, 256:384], in1=tri[:], op=ALU.mult),
                     reads=[d_pt[s], d_c], writes=[d_pt[s]])
            for j, kt in enumerate(kts):
                if diag and j == 1:
                    pv = [(1, 256, True)]
                elif diag:
                    pv = [(0, 0, True), (1, 128, False)]
                else:
                    pv = [(0, j * 256, False), (1, j * 256 + 128, False)]
                st_ = first and j == 0
                for qi, col0, last in pv:
                    P.op("pe", lambda e, s=s, kt=kt, qi=qi, col0=col0, st_=st_, last=last: e.matmul(
                        po[qi][:, :], lhsT=pt[s][:, col0:col0 + 128], rhs=vaug[b][:, kt, :], start=st_, stop=last),
                        reads=[d_pt[s], d_vaug[b]], writes=[d_po[qi]])
            if diag:
                for qi in range(2):
                    qt = jb * 2 + qi
                    P.op("dve", lambda e, qi=qi: e.reciprocal(out=rc[:, qi:qi + 1], in_=po[qi][:, 64:65]), reads=[d_po[qi]], writes=[d_rc[qi]])
                    P.op("dve", lambda e, qi=qi, qt=qt: e.tensor_scalar(out=oa[:, qt, h * 64:(h + 1) * 64], in0=po[qi][:, 0:64],
                                                                       scalar1=rc[:, qi:qi + 1], scalar2=None, op0=ALU.mult),
                         reads=[d_po[qi], d_rc[qi]], writes=[d_oa])
        SK = 2
        n = len(units)
        for i in range(n + SK):
            if i < n:
                emit_S(units[i], i % NPT)
            if i >= SK:
                emit_rest(units[i - SK], (i - SK) % NPT)


def host_consts(half):
    pv = 1.0 if half == 1 else 0.0
    V = np.zeros((32, 32), np.float32); O = np.zeros((32, 32), np.float32)
    for qt in range(32):
        jb = qt // 2
        V[qt, :16] = pv
        V[qt, 16:16 + jb] = 1.0
        O[qt, 16 + jb] = 1.0
    NBm = (V - 1.0) * BIGG
    E = np.zeros((32, NK), np.float32)
    for b in range(32):
        E[b, b * 256:(b + 1) * 256] = 1.0
    tri = (np.arange(128)[:, None] <= np.arange(128)[None, :]).astype(np.float32)
    return dict(vq=V.reshape(1, 1024), nb=NBm.reshape(1, 1024), oq=O.reshape(1, 1024),
                e_d=E.astype(ml_dtypes.bfloat16), tri=tri.astype(ml_dtypes.bfloat16))


def phase_c(nc, P, st, xin, w_in, g_mix, cwl_d, cbl_d, dtb_d, alog_d, dsk_d, ssdn_d, tri2_d, sel_d, tb_d, pflag_d,
            idb, d_idb, os_d, d_osd, chunks=range(32)):
    def sb(name, shape, dt):
        return st.enter_context(nc.sbuf_tensor("c_" + name, shape, dt))

    def ps(name, shape, dt=F32):
        return st.enter_context(nc.psum_tensor(name, shape, dt))
    wz = sb("wz", [128, 8, 1024], BF16); d_w = Dep()
    wx = sb("wx", [128, 8, 2048], BF16)
    wdt = sb("wdt", [128, 8, 16], BF16)
    gm = sb("gmc", [128, D], F32); d_c = Dep()
    cw = sb("cw", [128, 16, 4], F32)
    cb = sb("cb", [128, 16], F32)
    dtb = sb("dtb", [128, 2, 16], F32)
    Aneg = sb("Aneg", [128, 2, 16], F32); d_A = Dep()
    dsk = sb("dsk", [128, 16], F32)
    ssdn = sb("ssdn", [128, D], F32)
    tri2 = sb("tri2", [128, 2, 256], F32)
    onesf = sb("onesf", [128, 128], F32)
    sel = sb("selc", [16, 16, 128], F32)
    tb = sb("tbc", [128, 384], F32)
    pflag = sb("pflag", [128, 1], F32)
    xt = sb("xtc", [128, 2, D], F32); d_xt = Dep()
    junk = sb("junkc", [128, D], BF16); d_junk = Dep()
    ssq = sb("ssqc", [128, 2], F32); d_ssq = Dep()
    rstd = sb("rstdc", [128, 2], F32); d_rstd = Dep()
    hb = sb("hbc", [128, 2, D], BF16); d_hb = Dep()
    hT = sb("hTc", [128, 8, 256], BF16); d_hT = Dep()
    xraw = sb("xraw", [128, 16, 259], F32); d_xraw = Dep(); d_halo = Dep()
    acc = sb("acc", [128, 8, 256], F32); d_acc = [Dep() for _ in range(8)]
    xc = sb("xc", [128, 16, 256], BF16); d_xc = [Dep() for _ in range(16)]
    xtok = sb("xtok", [128, 2, 1024], BF16); d_xtok = Dep()
    btok = sb("btok", [128, 2, 512], BF16); d_btok = Dep()
    dtr = sb("dtr", [128, 2, 16], F32); d_dtr = Dep()
    dt = sb("dt", [128, 2, 16], F32); d_dt = Dep()
    aa = sb("aa", [128, 2, 16], F32); d_aa = Dep()
    acum = sb("acum", [128, 2, 16], F32); d_acum = Dep()
    nacum = sb("nacum", [128, 2, 16], F32); d_nacum = Dep()
    acumT = sb("acumT", [16, 256], F32); d_acumT = Dep()
    tot = sb("tot", [128, 16], F32); d_tot = Dep()
    cdec = sb("cdec", [128, 16], F32); d_cdec = Dep()
    dte = sb("dte", [128, 2, 16], F32); d_dte = Dep()
    eac = sb("eac", [128, 2, 16], F32); d_eac = Dep()
    w2 = sb("w2", [128, 2, 16], F32); d_w2 = Dep()
    xdt = sb("xdt", [128, 2, 1024], BF16); d_xdt = Dep()
    xdd = sb("xdd", [128, 2, 1024], BF16); d_xdd = Dep()
    zs = sb("zs", [128, 2, 1024], BF16); d_zs = Dep()
    cbT = sb("cbT", [128, 4, 384], F32); d_cbT = [Dep() for _ in range(4)]
    arg = [sb("arg%d" % i, [128, 384], F32) for i in range(2)]; d_arg = [Dep(), Dep()]
    Lt = [sb("Lt%d" % i, [128, 384], F32) for i in range(2)]; d_Lt = [Dep(), Dep()]
    Mt = [sb("Mt%d" % i, [128, 384], BF16) for i in range(2)]; d_Mt = [Dep() for _ in range(2)]
    stf = sb("stf", [128, 4, 256], F32); d_stf = Dep()
    stT = sb("stT", [128, 4, 256], BF16); d_stT = Dep()
    yt = [sb("yt%d" % i, [128, 256], F32) for i in range(2)]; d_yt = [Dep(), Dep()]
    y2 = [sb("y2%d" % i, [128, 256], F32) for i in range(2)]; d_y2 = [Dep(), Dep()]
    gss = [sb("gss%d" % i, [128, 1], F32) for i in range(2)]; d_gss = [Dep(), Dep()]
    grs = [sb("grs%d" % i, [128, 1], F32) for i in range(2)]; d_grs = [Dep(), Dep()]
    osb = sb("osb", [128, 2, 1024], BF16); d_osb = Dep()

    B = [ps("bk%d" % i, [128, 512], F32) for i in range(7)]
    d_B = [Dep() for _ in range(7)]
    d_B1h = [Dep(), Dep()]
    d_B5h = [Dep(), Dep()]
    pT = ps("pTc", [128, 8, 128], BF16); d_pT = Dep()

    for k in range(8):
        rows = slice(k * 128, (k + 1) * 128)
        P.dma("pool", lambda e, k=k, rows=rows: e.dma_start(out=wz[:, k, :], in_=w_in[rows, C_Z:C_Z + 1024]), writes=[d_w])
        P.dma("pool", lambda e, k=k, rows=rows: e.dma_start(out=wx[:, k, :], in_=w_in[rows, C_X:C_X + 2048]), writes=[d_w])
        P.dma("pool", lambda e, k=k, rows=rows: e.dma_start(out=wdt[:, k, :], in_=w_in[rows, C_DT:C_DT + 16]), writes=[d_w])
    P.dma("sp", lambda e: e.dma_start(out=gm[:], in_=g_mix[0:1, :].partition_broadcast(128)), writes=[d_c])
    P.dma("sp", lambda e: e.dma_start(out=cw[:], in_=cwl_d[:, :, :]), writes=[d_c])
    P.dma("sp", lambda e: e.dma_start(out=cb[:], in_=cbl_d[:, :]), writes=[d_c])
    for i in range(2):
        P.dma("sp", lambda e, i=i: e.dma_start(out=dtb[:, i, :], in_=dtb_d[0:1, :].partition_broadcast(128)), writes=[d_c])
        P.dma("sp", lambda e, i=i: e.dma_start(out=Aneg[:, i, :], in_=alog_d[0:1, :].partition_broadcast(128)), writes=[d_A])
    P.dma("sp", lambda e: e.dma_start(out=dsk[:], in_=dsk_d[0:1, :].partition_broadcast(128)), writes=[d_c])
    P.dma("sp", lambda e: e.dma_start(out=ssdn[:], in_=ssdn_d[0:1, :].partition_broadcast(128)), writes=[d_c])
    P.dma("sp", lambda e: e.dma_start(out=tri2[:], in_=tri2_d[:, :, :]), writes=[d_c])
    P.dma("sp", lambda e: e.dma_start(out=sel[:], in_=sel_d[:, :, :]), writes=[d_c])
    P.dma("sp", lambda e: e.dma_start(out=tb[:], in_=tb_d[:, :]), writes=[d_c])
    P.dma("sp", lambda e: e.dma_start(out=pflag[:], in_=pflag_d[0:1, :].partition_broadcast(128)), writes=[d_c])
    P.op("pool", lambda e: e.memset(onesf[:], 1.0), writes=[d_c])
    P.op("act", lambda e: e.activation(out=Aneg[:], in_=Aneg[:], func=AF.Exp), reads=[d_A], writes=[d_A])
    P.op("dve", lambda e: e.tensor_scalar(out=Aneg[:], in0=Aneg[:], scalar1=-1.0, scalar2=None, op0=ALU.mult), reads=[d_A], writes=[d_A])
    P.op("pool", lambda e: e.memset(xraw[:], 0.0), writes=[d_xraw, d_halo])
    P.op("pool", lambda e: e.memset(stf[:], 0.0), writes=[d_stf])
    P.op("pool", lambda e: e.memset(stT[:], 0.0), writes=[d_stT])

    h3 = lambda ap: ap.rearrange("p (h d) -> p h d", d=64)

    for c in chunks:
        own = c >= 16
        r0 = c * 256
        P.dma("sp", lambda e, r0=r0: e.dma_start(out=xt[:], in_=xin[r0:r0 + 256, :].rearrange("(t p) d -> p t d", p=128)), writes=[d_xt])
        for it in range(2):
            P.op("act", lambda e, it=it: e.activation(out=junk[:], in_=xt[:, it, :], func=AF.Square, accum_out=ssq[:, it:it + 1]),
                 reads=[d_xt], writes=[d_junk, d_ssq])
        P.op("act", lambda e: e.activation(out=rstd[:], in_=ssq[:], func=AF.Sqrt, bias=EPS, scale=1.0 / D), reads=[d_ssq], writes=[d_rstd])
        P.op("dve", lambda e: e.reciprocal(out=rstd[:], in_=rstd[:]), reads=[d_rstd], writes=[d_rstd])
        for it in range(2):
            P.op("dve", lambda e, it=it: e.scalar_tensor_tensor(out=hb[:, it, :], in0=xt[:, it, :], scalar=rstd[:, it:it + 1], in1=gm[:],
                                                                                    op0=ALU.mult, op1=ALU.mult),
                 reads=[d_xt, d_rstd, d_c], writes=[d_hb])
        for it in range(2):
            for k in range(8):
                P.op("pe", lambda e, it=it, k=k: e.transpose(out=pT[:, k, :], in_=hb[:, it, k * 128:(k + 1) * 128], identity=idb[:]),
                     reads=[d_hb, d_idb], writes=[d_pT])
            P.op("act", lambda e, it=it: e.copy(out=hT[:, :, it * 128:(it + 1) * 128], in_=pT[:]), reads=[d_pT], writes=[d_hT])
        for it in range(2):
            for k in range(8):
                P.op("pe", lambda e, it=it, k=k: e.matmul(B[3][:, it * 16:(it + 1) * 16], lhsT=hT[:, k, it * 128:(it + 1) * 128], rhs=wdt[:, k, :],
                                                         start=(k == 0), stop=(k == 7)), reads=[d_hT, d_w], writes=[d_B[3]])
        P.op("dve", lambda e: e.tensor_tensor(out=dtr[:].rearrange("p a b -> p (a b)"), in0=B[3][:, 0:32], in1=dtb[:].rearrange("p a b -> p (a b)"), op=ALU.add),
             reads=[d_B[3], d_c], writes=[d_dtr])
        P.op("act", lambda e: e.activation(out=dtr[:], in_=dtr[:], func=AF.Exp), reads=[d_dtr], writes=[d_dtr])
        P.op("act", lambda e: e.activation(out=dt[:], in_=dtr[:], func=AF.Ln, bias=1.0), reads=[d_dtr], writes=[d_dt])
        P.op("dve", lambda e: e.tensor_tensor(out=aa[:], in0=dt[:], in1=Aneg[:], op=ALU.mult), reads=[d_dt, d_A], writes=[d_aa])
        nfc = 16 if (own or c == 15) else 12
        for fc in range(nfc):
            bi = 1 if fc % 2 == 0 else 4
            for k in range(8):
                P.op("pe", lambda e, fc=fc, k=k, bi=bi: e.matmul(B[bi][:, 0:256], lhsT=wx[:, k, fc * 128:(fc + 1) * 128], rhs=hT[:, k, :],
                                                               start=(k == 0), stop=(k == 7)), reads=[d_hT, d_w], writes=[d_B[bi]])
            P.op("act", lambda e, fc=fc, bi=bi: e.copy(out=xraw[:, fc, 3:259], in_=B[bi][:, 0:256]),
                 reads=[d_B[bi]], writes=[d_xraw])
        for it in range(2):
            for jt in range(it + 1):
                P.op("pe", lambda e, it=it, jt=jt: e.matmul(B[3][:, 32 + it * 16:32 + (it + 1) * 16], lhsT=tri2[:, jt, it * 128:(it + 1) * 128],
                                                           rhs=aa[:, jt, :], start=(jt == 0), stop=(jt == it)),
                     reads=[d_aa, d_c], writes=[d_B[3]])
        for jt in range(2):
            P.op("pe", lambda e, jt=jt: e.matmul(B[3][:, 64:80], lhsT=onesf[:], rhs=aa[:, jt, :], start=(jt == 0), stop=(jt == 1)),
                 reads=[d_aa, d_c], writes=[d_B[3]])
        for jt in range(2):
            P.op("pe", lambda e, jt=jt: e.matmul(B[3][0:16, 128:384], lhsT=aa[:, jt, :], rhs=tri2[:, jt, :], start=(jt == 0), stop=(jt == 1)),
                 reads=[d_aa, d_c], writes=[d_B[3]])
        P.op("act", lambda e: e.copy(out=acum[:].rearrange("p a b -> p (a b)"), in_=B[3][:, 32:64]), reads=[d_B[3]], writes=[d_acum])
        P.op("dve", lambda e: e.tensor_scalar(out=nacum[:].rearrange("p a b -> p (a b)"), in0=B[3][:, 32:64], scalar1=-1.0, scalar2=None, op0=ALU.mult),
             reads=[d_B[3]], writes=[d_nacum])
        P.op("act", lambda e: e.copy(out=tot[:], in_=B[3][:, 64:80]), reads=[d_B[3]], writes=[d_tot])
        P.op("act", lambda e: e.copy(out=acumT[:], in_=B[3][0:16, 128:384]), reads=[d_B[3]], writes=[d_acumT])
        P.op("act", lambda e: e.activation(out=cdec[:], in_=tot[:], func=AF.Exp), reads=[d_tot], writes=[d_cdec])
        P.op("dve", lambda e: e.tensor_tensor(out=dte[:], in0=nacum[:], in1=tot[:].unsqueeze(1).to_broadcast([128, 2, 16]), op=ALU.add),
             reads=[d_nacum, d_tot], writes=[d_dte])
        P.op("act", lambda e: e.activation(out=dte[:], in_=dte[:], func=AF.Exp), reads=[d_dte], writes=[d_dte])
        P.op("act", lambda e: e.activation(out=eac[:], in_=acum[:], func=AF.Exp), reads=[d_acum], writes=[d_eac])
        P.op("dve", lambda e: e.tensor_tensor(out=w2[:], in0=dt[:], in1=dte[:], op=ALU.mult), reads=[d_dt, d_dte], writes=[d_w2])
        if own:
            for it in range(2):
                for hv in range(2):
                    zb = 2 if hv == 0 else 6
                    for k in range(8):
                        P.op("pe", lambda e, it=it, hv=hv, k=k, zb=zb: e.matmul(B[zb][:, :], lhsT=hT[:, k, it * 128:(it + 1) * 128], rhs=wz[:, k, hv * 512:(hv + 1) * 512],
                                                                              start=(k == 0), stop=(k == 7)), reads=[d_hT, d_w], writes=[d_B[zb]])
                    P.op("act", lambda e, it=it, hv=hv, zb=zb: e.activation(out=zs[:, it, hv * 512:(hv + 1) * 512], in_=B[zb][:, :], func=AF.Silu),
                         reads=[d_B[zb]], writes=[d_zs])
        for half in range(2):
            n8 = 8 if (half == 0 or nfc == 16) else 4
            for f8 in range(n8):
                fc = half * 8 + f8
                P.op("dve", lambda e, fc=fc, f8=f8: e.tensor_scalar(out=acc[:, f8, :], in0=xraw[:, fc, 0:256], scalar1=cw[:, fc, 0:1], scalar2=cb[:, fc:fc + 1],
                                                                   op0=ALU.mult, op1=ALU.add), reads=[d_xraw, d_halo, d_c], writes=[d_acc[f8]])
            for kk in range(1, 4):
                for f8 in range(n8):
                    fc = half * 8 + f8
                    P.op("dve", lambda e, fc=fc, f8=f8, kk=kk: e.scalar_tensor_tensor(out=acc[:, f8, :], in0=xraw[:, fc, kk:kk + 256], scalar=cw[:, fc, kk:kk + 1],
                                                                                     in1=acc[:, f8, :], op0=ALU.mult, op1=ALU.add),
                         reads=[d_xraw, d_halo, d_c, d_acc[f8]], writes=[d_acc[f8]])
            for f8 in range(n8):
                fc = half * 8 + f8
                P.op("act", lambda e, fc=fc, f8=f8: e.activation(out=xc[:, fc, :], in_=acc[:, f8, :], func=AF.Silu),
                     reads=[d_acc[f8]], writes=[d_xc[fc]])
        P.op("pool", lambda e: e.tensor_copy(out=xraw[:, :, 0:3], in_=xraw[:, :, 256:259]), reads=[d_xraw], writes=[d_halo])
        for it in range(2):
            for fc in range(8):
                P.op("pe", lambda e, it=it, fc=fc: e.transpose(out=pT[:, fc, :], in_=xc[:, fc, it * 128:(it + 1) * 128], identity=idb[:]),
                     reads=[d_xc[fc], d_idb], writes=[d_pT])
            P.op("act", lambda e, it=it: e.copy(out=xtok[:, it, :], in_=pT[:]), reads=[d_pT], writes=[d_xtok])
            for g in range(4):
                P.op("pe", lambda e, it=it, g=g: e.transpose(out=pT[:, g, :], in_=xc[:, 8 + g, it * 128:(it + 1) * 128], identity=idb[:]),
                     reads=[d_xc[8 + g], d_idb], writes=[d_pT])
            P.op("act", lambda e, it=it: e.copy(out=btok[:, it, :], in_=pT[:, 0:4, :]), reads=[d_pT], writes=[d_btok])
        for it in range(2):
            if own:
                P.op("dve", lambda e, it=it: e.tensor_tensor(out=h3(xdt[:, it, :]), in0=h3(xtok[:, it, :]),
                                                             in1=dt[:, it, :].unsqueeze(2).to_broadcast([128, 16, 64]), op=ALU.mult),
                     reads=[d_xtok, d_dt], writes=[d_xdt])
            P.op("pool", lambda e, it=it: e.tensor_tensor(out=h3(xdd[:, it, :]), in0=h3(xtok[:, it, :]),
                                                          in1=w2[:, it, :].unsqueeze(2).to_broadcast([128, 16, 64]), op=ALU.mult),
                 reads=[d_xtok, d_w2], writes=[d_xdd])
        if own:
            for g in range(4):
                P.op("pe", lambda e, g=g: e.matmul(B[4][:, 0:256], lhsT=xc[:, 8 + g, 0:128], rhs=xc[:, 12 + g, :], start=True, stop=True),
                     reads=[d_xc[8 + g], d_xc[12 + g]], writes=[d_B[4]])
                P.op("pe", lambda e, g=g: e.matmul(B[4][:, 256:384], lhsT=xc[:, 8 + g, 128:256], rhs=xc[:, 12 + g, 128:256], start=True, stop=True),
                     reads=[d_xc[8 + g], d_xc[12 + g]], writes=[d_B[4]])
                P.op("act", lambda e, g=g: e.copy(out=cbT[:, g, :], in_=B[4][:, 0:384]), reads=[d_B[4]], writes=[d_cbT[g]])
            for g in range(4):
                def stA(h, g=g):
                    mi = h % 2
                    bk = 5 if mi == 0 else 4
                    P.op("pe", lambda e: e.matmul(B[bk][:, 0:256], lhsT=sel[:, h, :], rhs=acumT[:, :], start=True, stop=True),
                         reads=[d_c, d_acumT], writes=[d_B[bk]])
                    P.op("dve", lambda e: e.scalar_tensor_tensor(out=arg[mi][:, 0:256], in0=B[bk][:, 0:256], scalar=nacum[:, 0, h:h + 1], in1=tb[:, 0:256],
                                                                 op0=ALU.add, op1=ALU.add), reads=[d_B[bk], d_nacum, d_c], writes=[d_arg[mi]])
                    P.op("dve", lambda e: e.scalar_tensor_tensor(out=arg[mi][:, 256:384], in0=B[bk][:, 128:256], scalar=nacum[:, 1, h:h + 1], in1=tb[:, 256:384],
                                                                 op0=ALU.add, op1=ALU.add), reads=[d_B[bk], d_nacum, d_c], writes=[d_arg[mi]])
                    P.op("act", lambda e: e.activation(out=Lt[mi][:], in_=arg[mi][:], func=AF.Exp), reads=[d_arg[mi]], writes=[d_Lt[mi]])
                    P.op("pool", lambda e: e.tensor_tensor(out=Mt[mi][:], in0=Lt[mi][:], in1=cbT[:, g, :], op=ALU.mult),
                         reads=[d_Lt[mi], d_cbT[g]], writes=[d_Mt[mi]])

                def stB(h, g=g):
                    mi = h % 2
                    r = h % 4
                    cs_ = slice(r * 64, (r + 1) * 64)
                    hs_ = slice(h * 64, (h + 1) * 64)
                    P.op("pe", lambda e: e.matmul(B[6][:, 0:256][:, cs_], lhsT=Mt[mi][:, 0:128], rhs=xdt[:, 0, hs_], start=True, stop=True),
                         reads=[d_Mt[mi], d_xdt], writes=[d_B[6]])
                    P.op("pe", lambda e: e.matmul(B[6][:, 256:512][:, cs_], lhsT=Mt[mi][:, 128:256], rhs=xdt[:, 0, hs_], start=True, stop=False),
                         reads=[d_Mt[mi], d_xdt], writes=[d_B[6]])
                    P.op("pe", lambda e: e.matmul(B[6][:, 256:512][:, cs_], lhsT=Mt[mi][:, 256:384], rhs=xdt[:, 1, hs_], start=False, stop=True),
                         reads=[d_Mt[mi], d_xdt], writes=[d_B[6]])
                hs4 = [g * 4 + r for r in range(4)]
                stA(hs4[0]); stA(hs4[1]); stB(hs4[0]); stA(hs4[2]); stB(hs4[1]); stA(hs4[3]); stB(hs4[2]); stB(hs4[3])
                gs_ = slice(g * 256, (g + 1) * 256)
                for it in range(2):
                    P.op("pe", lambda e, g=g, it=it: e.matmul(B[0][:, it * 256:(it + 1) * 256], lhsT=xc[:, 12 + g, it * 128:(it + 1) * 128], rhs=stT[:, g, :],
                                                             start=True, stop=True), reads=[d_xc[12 + g], d_stT], writes=[d_B[0]])
                v4 = lambda ap: ap.rearrange("p (r d) -> p r d", d=64)

                def ystep(step, it, g=g, gs_=gs_):
                    if step == 0:
                        P.op("dve", lambda e: e.tensor_tensor(out=v4(yt[it][:]), in0=v4(B[0][:, it * 256:(it + 1) * 256]),
                                                              in1=eac[:, it, g * 4:(g + 1) * 4].unsqueeze(2).to_broadcast([128, 4, 64]), op=ALU.mult),
                             reads=[d_B[0], d_eac], writes=[d_yt[it]])
                    elif step == 1:
                        P.op("dve", lambda e: e.tensor_tensor(out=yt[it][:], in0=yt[it][:], in1=B[6][:, it * 256:(it + 1) * 256], op=ALU.add),
                             reads=[d_yt[it], d_B[6]], writes=[d_yt[it]])
                    elif step == 2:
                        P.op("pool", lambda e: e.tensor_tensor(out=v4(y2[it][:]), in0=v4(xtok[:, it, gs_]),
                                                               in1=dsk[:, g * 4:(g + 1) * 4].unsqueeze(2).to_broadcast([128, 4, 64]), op=ALU.mult),
                             reads=[d_xtok, d_c], writes=[d_y2[it]])
                    elif step == 3:
                        P.op("dve", lambda e: e.tensor_tensor(out=yt[it][:], in0=yt[it][:], in1=y2[it][:], op=ALU.add), reads=[d_yt[it], d_y2[it]], writes=[d_yt[it]])
                    elif step == 4:
                        P.op("dve", lambda e: e.tensor_tensor(out=yt[it][:], in0=yt[it][:], in1=zs[:, it, gs_], op=ALU.mult), reads=[d_yt[it], d_zs], writes=[d_yt[it]])
                    elif step == 5:
                        P.op("act", lambda e: e.activation(out=y2[it][:], in_=yt[it][:], func=AF.Square, accum_out=gss[it][:]), reads=[d_yt[it], d_y2[it]], writes=[d_y2[it], d_gss[it]])
                    elif step == 6:
                        P.op("act", lambda e: e.activation(out=grs[it][:], in_=gss[it][:], func=AF.Sqrt, bias=EPS, scale=1.0 / 256), reads=[d_gss[it]], writes=[d_grs[it]])
                    elif step == 7:
                        P.op("dve", lambda e: e.reciprocal(out=grs[it][:], in_=grs[it][:]), reads=[d_grs[it]], writes=[d_grs[it]])
                    elif step == 8:
                        P.op("dve", lambda e: e.scalar_tensor_tensor(out=osb[:, it, gs_], in0=yt[it][:], scalar=grs[it][:, 0:1], in1=ssdn[:, gs_],
                                                                     op0=ALU.mult, op1=ALU.mult), reads=[d_yt[it], d_grs[it], d_c], writes=[d_osb])
                for step in range(9):
                    for it in range(2):
                        ystep(step, it)
            P.dma("sp", lambda e, r0=r0: e.dma_start(out=os_d[r0 - TP:r0 - TP + 256, :].rearrange("(t p) c -> p t c", p=128), in_=osb[:]),
                  reads=[d_osb], writes=[d_osd])
        for g in range(4):
            bank = B[2] if g < 2 else B[1]
            dbank = d_B[2] if g < 2 else d_B[1]
            cs_ = slice((g % 2) * 256, (g % 2 + 1) * 256)
            for jt in range(2):
                P.op("pe", lambda e, g=g, jt=jt, bank=bank, cs_=cs_: e.matmul(bank[:, cs_], lhsT=btok[:, jt, g * 128:(g + 1) * 128], rhs=xdd[:, jt, g * 256:(g + 1) * 256],
                                                                            start=(jt == 0), stop=(jt == 1)), reads=[d_btok, d_xdd], writes=[dbank])
        v16 = lambda ap: ap.rearrange("p (h d) -> p h d", d=64)
        P.op("dve", lambda e: e.tensor_tensor(out=v16(stf[:].rearrange("p g c -> p (g c)")), in0=v16(stf[:].rearrange("p g c -> p (g c)")),
                                              in1=cdec[:].unsqueeze(2).to_broadcast([128, 16, 64]), op=ALU.mult), reads=[d_stf, d_cdec], writes=[d_stf])
        P.op("dve", lambda e: e.tensor_tensor(out=stf[:, 0:2, :].rearrange("p g c -> p (g c)"), in0=stf[:, 0:2, :].rearrange("p g c -> p (g c)"), in1=B[2][:, :], op=ALU.add),
             reads=[d_stf, d_B[2]], writes=[d_stf])
        P.op("dve", lambda e: e.tensor_tensor(out=stf[:, 2:4, :].rearrange("p g c -> p (g c)"), in0=stf[:, 2:4, :].rearrange("p g c -> p (g c)"), in1=B[1][:, :], op=ALU.add),
             reads=[d_stf, d_B[1]], writes=[d_stf])
        if c == 15:
            P.op("dve", lambda e: e.tensor_scalar(out=stf[:], in0=stf[:], scalar1=pflag[:, 0:1], scalar2=None, op0=ALU.mult), reads=[d_stf, d_c], writes=[d_stf])
        P.op("act", lambda e: e.copy(out=stT[:], in_=stf[:]), reads=[d_stf], writes=[d_stT])


def host_consts_c(z, half):
    cwl = np.ascontiguousarray(z['conv_w'].reshape(4, 16, 128).transpose(2, 1, 0)).astype(np.float32)
    cbl = np.ascontiguousarray(z['conv_b'].reshape(16, 128).T).astype(np.float32)
    tri = (np.arange(128)[:, None] <= np.arange(128)[None, :]).astype(np.float32)
    tri2 = np.zeros((128, 2, 256), np.float32)
    tri2[:, 0, 0:128] = tri; tri2[:, 0, 128:256] = 1.0; tri2[:, 1, 128:256] = tri
    sel = np.zeros((16, 16, 128), np.float32)
    for h in range(16):
        sel[h, h, :] = 1.0
    tbias = np.where(tri > 0, 0.0, NEG).astype(np.float32)
    tb = np.zeros((128, 384), np.float32)
    tb[:, 0:128] = tbias; tb[:, 256:384] = tbias
    return dict(cwl=cwl, cbl=cbl, dtb=z['dt_bias'][None, :], alog=z['a_log'][None, :], dsk=z['d_skip'][None, :], ssdn=z['ssd_norm'][None, :],
                tri2=tri2, sel_d=sel, tb_d=tb, pflag=np.array([[1.0 if half == 1 else 0.0]], np.float32))


def phase_def(nc, P, xown, w_in, g_mix, mem_d, gmem_d, wmemkv, mqn_d, mkn_d, woa_d, wos_d, wom_d, wout_d, gffn_d, wr_d,
              wgate_d, wup_d, wdown_d, tri_d, ecap_d, oa_d, d_oad, os_d, d_osd, x1_d, xbuf, ybuf, out_d, idb, d_idb, idf, d_idf,
              ntiles=32, experts=range(32), stage=3):
    d_x1d = Dep(); d_xbuf = Dep(); d_ybuf = Dep(); d_out = Dep()
    with contextlib.ExitStack() as stp:
        def sbp(name, shape, dt):
            return stp.enter_context(nc.sbuf_tensor("d_" + name, shape, dt))
        combs = sbp("combs", [128, 32, 2], F32); d_combs = Dep()
        idxs = sbp("idxs", [128, 32, 2], I32); d_idxs = Dep()

        with contextlib.ExitStack() as st:
            def sb(name, shape, dt):
                return st.enter_context(nc.sbuf_tensor("d_" + name, shape, dt))

            def ps(name, shape, dt=F32):
                return st.enter_context(nc.psum_tensor(name, shape, dt))
            d_w = Dep(); d_c = Dep()
            gm = sb("gmd", [128, D], F32)
            gf = sb("gfd", [128, D], F32)
            mqn = sb("mqn", [128, 512], F32)
            mkn = sb("mkn", [128, 512], F32)
            tri = sb("trid", [128, 128], F32)
            onesf = sb("onesfd", [128, 128], F32)
            onesb = sb("onesbd", [128, 128], BF16)
            ecap = sb("ecap", [128, 32], F32)
            base = sb("base", [128, 32], F32); d_base = Dep()
            kmT = sb("kmT", [128, 4, 256], BF16); d_kmT = Dep()
            vm = sb("vm", [128, 2, 512], BF16); d_vm = Dep()

            xt = [sb("xtd%d" % i, [128, D], F32) for i in range(2)]; d_xt = [Dep(), Dep()]
            junk = sb("junkd", [128, D], BF16); d_junk = Dep()
            ssq = sb("ssqd", [128, 1], F32); d_ssq = Dep()
            rstd = sb("rstdd", [128, 1], F32); d_rstd = Dep()
            hb_ = [sb("hbd%d" % i, [128, D], BF16) for i in range(2)]; d_hb_ = [Dep(), Dep()]
            ssqn = sb("ssqn", [128, 1], F32); d_ssqn = Dep()
            rstdn = sb("rstdn", [128, 1], F32); d_rstdn = Dep()
            hT = sb("hTd", [128, 8, 128], BF16); d_hT = Dep()
            sq = sb("sqd", [128, 512], F32); d_sq = Dep()
            hs = sb("hsd", [128, 4], F32); d_hs = Dep()
            hr = sb("hrd", [128, 4], F32); d_hr = Dep()
            qmn = sb("qmn", [128, 512], F32); d_qmn = Dep()
            qmb = sb("qmb", [128, 512], BF16); d_qmb = Dep()
            qmT = sb("qmT", [128, 4, 128], BF16); d_qmT = Dep()
            pm = sb("pm", [128, 4, 2, 128], BF16); d_pm = Dep()
            rcp = sb("rcp", [128, 512], F32); d_rcp = Dep()
            omT = sb("omT", [128, 4, 128], BF16); d_omT = Dep()
            gs = sb("gs", [128, 3072], F32); d_gs = Dep()
            oat = [sb("oat%d" % i, [128, 512], BF16) for i in range(2)]; d_oat = [Dep(), Dep()]
            ost = [sb("ost%d" % i, [128, 1024], BF16) for i in range(2)]; d_ost = [Dep(), Dep()]
            oaT = sb("oaT", [128, 4, 128], BF16); d_oaT = Dep()
            osT = sb("osT", [128, 8, 128], BF16); d_osT = Dep()
            mg = sb("mg", [128, 512], F32); d_mg = Dep()
            tt = sb("tt", [128, 512], F32); d_tt = Dep()
            mgb = sb("mgb", [128, D], BF16); d_mgb = Dep()
            mgT = sb("mgT", [128, 8, 128], BF16); d_mgT = Dep()
            x1 = sb("x1", [128, D], F32); d_x1 = Dep()
            h2f = sb("h2f", [128, D], F32); d_h2f = Dep()
            h2b = [sb("h2b%d" % i, [128, D], BF16) for i in range(2)]; d_h2b = [Dep() for _ in range(2)]
            h2T = sb("h2T", [128, 8, 128], F32); d_h2T = Dep()
            lg = sb("lg", [128, 36], F32); d_lg = Dep()
            sm = sb("sm", [128, 64], F32); d_sm = Dep()
            goh = sb("goh", [128, 4], F32); d_goh = Dep()
            lsel = sb("lsel", [128, 32], F32); d_lsel = Dep()
            les = sb("les", [128, 8], F32); d_les = Dep()
            t8 = sb("t8", [128, 8], F32); d_t8 = Dep()
            oh = sb("oh", [128, 64], F32); d_oh = Dep()
            ohs = sb("ohs", [128, 32], F32); d_ohs = Dep()
            posf = sb("posf", [128, 32], F32); d_posf = Dep()
            idf2 = sb("idf2", [128, 2], F32); d_idf2 = Dep()

            pT = ps("pTd", [128, 8, 128], BF16); d_pT = Dep()
            pF = ps("pFd", [128, 512], F32); d_pF = Dep()
            pG = [ps("pGd%d" % i, [128, 512], F32) for i in range(3)]; d_pG = [Dep() for _ in range(3)]
            pms = [ps("pmsd%d" % i, [128, 512], F32) for i in range(2)]; d_pms = [Dep() for _ in range(2)]
            pmo = ps("pmod", [128, 512], F32); d_pmo = Dep()

            P.dma("sp", lambda e: e.dma_start(out=gm[:], in_=g_mix[0:1, :].partition_broadcast(128)), writes=[d_c])
            P.dma("sp", lambda e: e.dma_start(out=gf[:], in_=gffn_d[0:1, :].partition_broadcast(128)), writes=[d_c])
            P.dma("sp", lambda e: e.dma_start(out=mqn[:], in_=mqn_d[0:1, :].partition_broadcast(128)), writes=[d_c])
            P.dma("sp", lambda e: e.dma_start(out=mkn[:], in_=mkn_d[0:1, :].partition_broadcast(128)), writes=[d_c])
            P.dma("sp", lambda e: e.dma_start(out=tri[:], in_=tri_d[:, :]), writes=[d_c])
            P.dma("sp", lambda e: e.dma_start(out=ecap[:], in_=ecap_d[0:1, :].partition_broadcast(128)), writes=[d_c])
            P.op("pool", lambda e: e.memset(onesf[:], 1.0), writes=[d_c])
            P.op("pool", lambda e: e.memset(onesb[:], 1.0), writes=[d_c])
            P.op("pool", lambda e: e.memset(base[:], 0.0), writes=[d_base])

            def head_norm(src_ps, d_src, gain, nh, dst, d_dst):
                hd = 512 // nh
                v = lambda ap: ap.rearrange("p (h d) -> p h d", h=nh)
                P.op("act", lambda e: e.activation(out=sq[:], in_=src_ps, func=AF.Square), reads=[d_src], writes=[d_sq])
                P.op("dve", lambda e: e.tensor_reduce(out=hs[:, 0:nh], in_=v(sq[:]), axis=AX.X, op=ALU.add), reads=[d_sq], writes=[d_hs])
                P.op("act", lambda e: e.activation(out=hr[:, 0:nh], in_=hs[:, 0:nh], func=AF.Sqrt, bias=EPS, scale=1.0 / hd), reads=[d_hs], writes=[d_hr])
                P.op("dve", lambda e: e.reciprocal(out=hr[:, 0:nh], in_=hr[:, 0:nh]), reads=[d_hr], writes=[d_hr])
                P.op("dve", lambda e: e.tensor_tensor(out=v(qmn[:]), in0=v(src_ps), in1=hr[:, 0:nh].unsqueeze(2).to_broadcast([128, nh, hd]), op=ALU.mult),
                     reads=[d_src, d_hr], writes=[d_qmn])
                P.op("pool", lambda e: e.tensor_tensor(out=dst, in0=qmn[:], in1=gain[:], op=ALU.mult), reads=[d_qmn, d_c], writes=[d_dst])

            with contextlib.ExitStack() as stm:
                wkv = stm.enter_context(nc.sbuf_tensor("s_wkv", [128, 8, 1024], BF16)); d_wkv = Dep()
                mt_ = stm.enter_context(nc.sbuf_tensor("s_memt", [128, 2, D], F32)); d_mt = Dep()
                gme = stm.enter_context(nc.sbuf_tensor("s_gme", [128, D], F32)); d_gme = Dep()
                mhb = stm.enter_context(nc.sbuf_tensor("s_mhb", [128, 2, D], BF16)); d_mhb = Dep()
                mT = stm.enter_context(nc.sbuf_tensor("s_mT", [128, 8, 256], BF16)); d_mT = Dep()
                ss2 = stm.enter_context(nc.sbuf_tensor("s_ss2", [128, 2], F32)); d_ss2 = Dep()
                for k in range(8):
                    P.dma("pool", lambda e, k=k: e.dma_start(out=wkv[:, k, :], in_=wmemkv[k * 128:(k + 1) * 128, :]), writes=[d_wkv])
                P.dma("sp", lambda e: e.dma_start(out=mt_[:], in_=mem_d[:, :].rearrange("(t p) d -> p t d", p=128)), writes=[d_mt])
                P.dma("sp", lambda e: e.dma_start(out=gme[:], in_=gmem_d[0:1, :].partition_broadcast(128)), writes=[d_gme])
                for it in range(2):
                    P.op("act", lambda e, it=it: e.activation(out=junk[:], in_=mt_[:, it, :], func=AF.Square, accum_out=ss2[:, it:it + 1]),
                         reads=[d_mt], writes=[d_junk, d_ss2])
                P.op("act", lambda e: e.activation(out=ss2[:], in_=ss2[:], func=AF.Sqrt, bias=EPS, scale=1.0 / D), reads=[d_ss2], writes=[d_ss2])
                P.op("dve", lambda e: e.reciprocal(out=ss2[:], in_=ss2[:]), reads=[d_ss2], writes=[d_ss2])
                for it in range(2):
                    P.op("dve", lambda e, it=it: e.scalar_tensor_tensor(out=mhb[:, it, :], in0=mt_[:, it, :], scalar=ss2[:, it:it + 1], in1=gme[:],
                                                                        op0=ALU.mult, op1=ALU.mult), reads=[d_mt, d_ss2, d_gme], writes=[d_mhb])
                    for k in range(8):
                        P.op("pe", lambda e, it=it, k=k: e.transpose(out=pT[:, k, :], in_=mhb[:, it, k * 128:(k + 1) * 128], identity=idb[:]),
                             reads=[d_mhb, d_idb], writes=[d_pT])
                    P.op("act", lambda e, it=it: e.copy(out=mT[:, :, it * 128:(it + 1) * 128], in_=pT[:]), reads=[d_pT], writes=[d_mT])
                for it in range(2):
                    for hv in range(2):
                        for k in range(8):
                            P.op("pe", lambda e, it=it, hv=hv, k=k: e.matmul(pG[hv][:, :], lhsT=mT[:, k, it * 128:(it + 1) * 128], rhs=wkv[:, k, hv * 512:(hv + 1) * 512],
                                                                           start=(k == 0), stop=(k == 7)), reads=[d_mT, d_wkv], writes=[d_pG[hv]])
                    head_norm(pG[0][:, :], d_pG[0], mkn, 4, qmb[:], d_qmb)
                    for h in range(4):
                        P.op("pe", lambda e, h=h: e.transpose(out=pT[:, h, :], in_=qmb[:, h * 128:(h + 1) * 128], identity=idb[:]),
                             reads=[d_qmb, d_idb], writes=[d_pT])
                    P.op("act", lambda e, it=it: e.copy(out=kmT[:, :, it * 128:(it + 1) * 128], in_=pT[:, 0:4, :]), reads=[d_pT], writes=[d_kmT])
                    P.op("act", lambda e, it=it: e.copy(out=vm[:, it, :], in_=pG[1][:, :]), reads=[d_pG[1]], writes=[d_vm])

            wqm = sb("wqm", [128, 8, 512], BF16)
            wg = sb("wg", [128, 8, 3072], BF16)
            woa = sb("woa", [128, 4, 1024], BF16)
            wos = sb("wos", [128, 8, 1024], BF16)
            wom = sb("wom", [128, 4, 1024], BF16)
            wout = sb("wout", [128, 8, 1024], BF16)
            wr = sb("wr", [128, 8, 36], F32)
            for k in range(8):
                rows = slice(k * 128, (k + 1) * 128)
                P.dma("pool", lambda e, k=k, rows=rows: e.dma_start(out=wqm[:, k, :], in_=w_in[rows, C_QM:C_QM + 512]), writes=[d_w])
                for j in range(2):
                    P.dma("pool", lambda e, k=k, rows=rows, j=j: e.dma_start(out=wg[:, k, j * 1536:(j + 1) * 1536], in_=w_in[rows, C_G + j * 1536:C_G + (j + 1) * 1536]), writes=[d_w])
                P.dma("pool", lambda e, k=k, rows=rows: e.dma_start(out=wos[:, k, :], in_=wos_d[rows, :]), writes=[d_w])
                P.dma("pool", lambda e, k=k, rows=rows: e.dma_start(out=wout[:, k, :], in_=wout_d[rows, :]), writes=[d_w])
                P.dma("sp", lambda e, k=k, rows=rows: e.dma_start(out=wr[:, k, :], in_=wr_d[rows, :]), writes=[d_w])
            for k in range(4):
                rows = slice(k * 128, (k + 1) * 128)
                P.dma("pool", lambda e, k=k, rows=rows: e.dma_start(out=woa[:, k, :], in_=woa_d[rows, :]), writes=[d_w])
                P.dma("pool", lambda e, k=k, rows=rows: e.dma_start(out=wom[:, k, :], in_=wom_d[rows, :]), writes=[d_w])
            def norm_d(tj):
                xj = tj % 2
                P.op("act", lambda e: e.activation(out=junk[:], in_=xt[xj][:], func=AF.Square, accum_out=ssqn[:]), reads=[d_xt[xj]], writes=[d_junk, d_ssqn])
                P.op("act", lambda e: e.activation(out=rstdn[:], in_=ssqn[:], func=AF.Sqrt, bias=EPS, scale=1.0 / D), reads=[d_ssqn], writes=[d_rstdn])
                P.op("dve", lambda e: e.reciprocal(out=rstdn[:], in_=rstdn[:]), reads=[d_rstdn], writes=[d_rstdn])
                P.op("dve", lambda e: e.scalar_tensor_tensor(out=hb_[xj][:], in0=xt[xj][:], scalar=rstdn[:, 0:1], in1=gm[:], op0=ALU.mult, op1=ALU.mult),
                     reads=[d_xt[xj], d_rstdn, d_c], writes=[d_hb_[xj]])

            def loads(tj):
                xj = tj % 2
                rj = tj * 128
                P.dma("sp", lambda e: e.dma_start(out=xt[xj][:], in_=xown[rj:rj + 128, :]), writes=[d_xt[xj]])
                P.dma("sp", lambda e: e.dma_start(out=oat[xj][:], in_=oa_d[rj:rj + 128, :]), reads=[d_oad], writes=[d_oat[xj]])
                P.dma("sp", lambda e: e.dma_start(out=ost[xj][:], in_=os_d[rj:rj + 128, :]), reads=[d_osd], writes=[d_ost[xj]])

            for ti in range(ntiles):
                r0 = ti * 128
                hbuf = ti % 2
                xb = ti % 2
                if ti == 0:
                    loads(0)
                if ti + 1 < ntiles:
                    loads(ti + 1)
                if ti == 0:
                    norm_d(0)
                for k in range(8):
                    P.op("pe", lambda e, k=k, xb=xb: e.transpose(out=pT[:, k, :], in_=hb_[xb][:, k * 128:(k + 1) * 128], identity=idb[:]), reads=[d_hb_[xb], d_idb], writes=[d_pT])
                P.op("act", lambda e: e.copy(out=hT[:], in_=pT[:]), reads=[d_pT], writes=[d_hT])
                for k in range(8):
                    P.op("pe", lambda e, k=k: e.matmul(pG[0][:, :], lhsT=hT[:, k, :], rhs=wqm[:, k, :], start=(k == 0), stop=(k == 7)),
                         reads=[d_hT, d_w], writes=[d_pG[0]])
                head_norm(pG[0][:, :], d_pG[0], mqn, 4, qmb[:], d_qmb)
                for g6 in range(6):
                    bk = g6 % 3
                    for k in range(8):
                        P.op("pe", lambda e, g6=g6, k=k, bk=bk: e.matmul(pG[bk][:, :], lhsT=hT[:, k, :], rhs=wg[:, k, g6 * 512:(g6 + 1) * 512], start=(k == 0), stop=(k == 7)),
                             reads=[d_hT, d_w], writes=[d_pG[bk]])
                    P.op("act", lambda e, g6=g6, bk=bk: e.activation(out=gs[:, g6 * 512:(g6 + 1) * 512], in_=pG[bk][:, :], func=AF.Sigmoid),
                         reads=[d_pG[bk]], writes=[d_gs])
                for h in range(4):
                    P.op("pe", lambda e, h=h: e.transpose(out=pT[:, h, :], in_=qmb[:, h * 128:(h + 1) * 128], identity=idb[:]), reads=[d_qmb, d_idb], writes=[d_pT])
                P.op("act", lambda e: e.copy(out=qmT[:], in_=pT[:, 0:4, :]), reads=[d_pT], writes=[d_qmT])
                for h in range(4):
                    for mt in range(2):
                        bank = pms[h // 2]
                        c0 = ((h % 2) * 2 + mt) * 128
                        P.op("pe", lambda e, h=h, mt=mt, bank=bank, c0=c0: e.matmul(bank[:, c0:c0 + 128], lhsT=kmT[:, h, mt * 128:(mt + 1) * 128], rhs=qmT[:, h, :],
                                                                                  start=True, stop=True), reads=[d_kmT, d_qmT], writes=[d_pms[h // 2]])
                for hp in range(2):
                    P.op("act", lambda e, hp=hp: e.activation(out=pm[:, hp * 2:(hp + 1) * 2, :, :].rearrange("p a b c -> p (a b c)"), in_=pms[hp][:, :], func=AF.Exp,
                                                              scale=float(128 ** -0.5)), reads=[d_pms[hp]], writes=[d_pm])
                for h in range(4):
                    for mt in range(2):
                        P.op("pe", lambda e, h=h, mt=mt: e.matmul(pmo[:, h * 128:(h + 1) * 128], lhsT=vm[:, mt, h * 128:(h + 1) * 128], rhs=pm[:, h, mt, :],
                                                                 start=(mt == 0), stop=(mt == 1)), reads=[d_vm, d_pm], writes=[d_pmo])
                for mt in range(2):
                    P.op("pe", lambda e, mt=mt: e.matmul(pG[1][:, :].rearrange("p (h t) -> p h t", h=4), lhsT=onesb[:], rhs=pm[:, :, mt, :],
                                                        start=(mt == 0), stop=(mt == 1)), reads=[d_c, d_pm], writes=[d_pG[1]])
                P.op("dve", lambda e: e.reciprocal(out=rcp[:], in_=pG[1][:, :]), reads=[d_pG[1]], writes=[d_rcp])
                P.op("dve", lambda e: e.tensor_tensor(out=omT[:].rearrange("p h t -> p (h t)"), in0=pmo[:, :], in1=rcp[:], op=ALU.mult),
                     reads=[d_pmo, d_rcp], writes=[d_omT])
                for k in range(4):
                    P.op("pe", lambda e, xb=xb, k=k: e.transpose(out=pT[:, k, :], in_=oat[xb][:, k * 128:(k + 1) * 128], identity=idb[:]), reads=[d_oat[xb], d_idb], writes=[d_pT])
                P.op("act", lambda e: e.copy(out=oaT[:], in_=pT[:, 0:4, :]), reads=[d_pT], writes=[d_oaT])
                for k in range(8):
                    P.op("pe", lambda e, xb=xb, k=k: e.transpose(out=pT[:, k, :], in_=ost[xb][:, k * 128:(k + 1) * 128], identity=idb[:]), reads=[d_ost[xb], d_idb], writes=[d_pT])
                P.op("act", lambda e: e.copy(out=osT[:], in_=pT[:]), reads=[d_pT], writes=[d_osT])
                for hv in range(2):
                    cs_ = slice(hv * 512, (hv + 1) * 512)
                    for k in range(4):
                        P.op("pe", lambda e, k=k, cs_=cs_: e.matmul(pG[0][:, :], lhsT=oaT[:, k, :], rhs=woa[:, k, cs_], start=(k == 0), stop=(k == 3)),
                             reads=[d_oaT, d_w], writes=[d_pG[0]])
                    for k in range(8):
                        P.op("pe", lambda e, k=k, cs_=cs_: e.matmul(pG[1][:, :], lhsT=osT[:, k, :], rhs=wos[:, k, cs_], start=(k == 0), stop=(k == 7)),
                             reads=[d_osT, d_w], writes=[d_pG[1]])
                    for k in range(4):
                        P.op("pe", lambda e, k=k, cs_=cs_: e.matmul(pG[2][:, :], lhsT=omT[:, k, :], rhs=wom[:, k, cs_], start=(k == 0), stop=(k == 3)),
                             reads=[d_omT, d_w], writes=[d_pG[2]])
                    P.op("dve", lambda e, hv=hv: e.tensor_tensor(out=mg[:], in0=pG[0][:, :], in1=gs[:, hv * 512:(hv + 1) * 512], op=ALU.mult),
                         reads=[d_pG[0], d_gs], writes=[d_mg])
                    P.op("dve", lambda e, hv=hv: e.tensor_tensor(out=tt[:], in0=pG[1][:, :], in1=gs[:, 1024 + hv * 512:1024 + (hv + 1) * 512], op=ALU.mult),
                         reads=[d_pG[1], d_gs], writes=[d_tt])
                    P.op("pool", lambda e: e.tensor_tensor(out=mg[:], in0=mg[:], in1=tt[:], op=ALU.add), reads=[d_mg, d_tt], writes=[d_mg])
                    P.op("dve", lambda e, hv=hv: e.tensor_tensor(out=tt[:], in0=pG[2][:, :], in1=gs[:, 2048 + hv * 512:2048 + (hv + 1) * 512], op=ALU.mult),
                         reads=[d_pG[2], d_gs], writes=[d_tt])
                    P.op("pool", lambda e, cs_=cs_: e.tensor_tensor(out=mgb[:, cs_], in0=mg[:], in1=tt[:], op=ALU.add), reads=[d_mg, d_tt], writes=[d_mgb])
                for k in range(8):
                    P.op("pe", lambda e, k=k: e.transpose(out=pT[:, k, :], in_=mgb[:, k * 128:(k + 1) * 128], identity=idb[:]), reads=[d_mgb, d_idb], writes=[d_pT])
                P.op("act", lambda e: e.copy(out=mgT[:], in_=pT[:]), reads=[d_pT], writes=[d_mgT])
                for hv in range(2):
                    cs_ = slice(hv * 512, (hv + 1) * 512)
                    for k in range(8):
                        P.op("pe", lambda e, k=k, cs_=cs_, hv=hv: e.matmul(pG[hv][:, :], lhsT=mgT[:, k, :], rhs=wout[:, k, cs_], start=(k == 0), stop=(k == 7)),
                             reads=[d_mgT, d_w], writes=[d_pG[hv]])
                    P.op("dve", lambda e, xb=xb, cs_=cs_, hv=hv: e.tensor_tensor(out=x1[:, cs_], in0=pG[hv][:, :], in1=xt[xb][:, cs_], op=ALU.add),
                         reads=[d_pG[hv], d_xt[xb]], writes=[d_x1])
                P.dma("sp", lambda e, r0=r0: e.dma_start(out=x1_d[r0:r0 + 128, :], in_=x1[:]), reads=[d_x1], writes=[d_x1d])
                if ti + 1 < ntiles:
                    norm_d(ti + 1)
                if stage < 2:
                    continue
                P.op("act", lambda e: e.activation(out=junk[:], in_=x1[:], func=AF.Square, accum_out=ssq[:]), reads=[d_x1], writes=[d_junk, d_ssq])
                P.op("act", lambda e: e.activation(out=rstd[:], in_=ssq[:], func=AF.Sqrt, bias=EPS, scale=1.0 / D), reads=[d_ssq], writes=[d_rstd])
                P.op("dve", lambda e: e.reciprocal(out=rstd[:], in_=rstd[:]), reads=[d_rstd], writes=[d_rstd])
                P.op("dve", lambda e: e.scalar_tensor_tensor(out=h2f[:], in0=x1[:], scalar=rstd[:, 0:1], in1=gf[:], op0=ALU.mult, op1=ALU.mult),
                     reads=[d_x1, d_rstd, d_c], writes=[d_h2f])
                P.op("pool", lambda e, hbuf=hbuf: e.tensor_copy(out=h2b[hbuf][:], in_=h2f[:]), reads=[d_h2f], writes=[d_h2b[hbuf]])
                for half in range(2):
                    for k4 in range(4):
                        k = half * 4 + k4
                        P.op("pe", lambda e, k=k, k4=k4: e.transpose(out=pF[:, k4 * 128:(k4 + 1) * 128], in_=h2f[:, k * 128:(k + 1) * 128], identity=idf[:]),
                             reads=[d_h2f, d_idf], writes=[d_pF])
                    P.op("act", lambda e, half=half: e.copy(out=h2T[:, half * 4:(half + 1) * 4, :].rearrange("p a b -> p (a b)"), in_=pF[:, :]), reads=[d_pF], writes=[d_h2T])
                for k in range(8):
                    P.op("pe", lambda e, k=k: e.matmul(pF[:, 0:36], lhsT=h2T[:, k, :], rhs=wr[:, k, :], start=(k == 0), stop=(k == 7)), reads=[d_h2T, d_w], writes=[d_pF])
                P.op("act", lambda e: e.copy(out=lg[:], in_=pF[:, 0:36]), reads=[d_pF], writes=[d_lg])
                P.op("dve", lambda e: e.tensor_reduce(out=sm[:, 0:1], in_=lg[:, 0:4], axis=AX.X, op=ALU.max), reads=[d_lg], writes=[d_sm])
                P.op("dve", lambda e: e.tensor_scalar(out=sm[:, 1:2], in0=sm[:, 0:1], scalar1=-1.0, scalar2=None, op0=ALU.mult), reads=[d_sm], writes=[d_sm])
                P.op("act", lambda e: e.activation(out=sm[:, 8:12], in_=lg[:, 0:4], func=AF.Exp, bias=sm[:, 1:2], accum_out=sm[:, 2:3]), reads=[d_lg, d_sm], writes=[d_sm])
                P.op("dve", lambda e: e.reciprocal(out=sm[:, 3:4], in_=sm[:, 2:3]), reads=[d_sm], writes=[d_sm])
                P.op("dve", lambda e: e.tensor_scalar(out=goh[:], in0=lg[:, 0:4], scalar1=sm[:, 0:1], scalar2=None, op0=ALU.is_ge), reads=[d_lg, d_sm], writes=[d_goh])
                P.op("dve", lambda e: e.tensor_tensor(out=lsel[:].rearrange("p (g e) -> p g e", g=4), in0=lg[:, 4:36].rearrange("p (g e) -> p g e", g=4),
                                                      in1=goh[:].unsqueeze(2).to_broadcast([128, 4, 8]), op=ALU.mult), reads=[d_lg, d_goh], writes=[d_lsel])
                P.op("dve", lambda e: e.tensor_reduce(out=les[:], in_=lsel[:].rearrange("p (g e) -> p e g", g=4), axis=AX.X, op=ALU.add), reads=[d_lsel], writes=[d_les])
                P.op("dve", lambda e: e.max(out=t8[:], in_=les[:]), reads=[d_les], writes=[d_t8])
                P.op("dve", lambda e: e.tensor_tensor(out=sm[:, 4:5], in0=t8[:, 1:2], in1=t8[:, 0:1], op=ALU.subtract), reads=[d_t8, d_sm], writes=[d_sm])
                P.op("act", lambda e: e.activation(out=sm[:, 5:6], in_=sm[:, 4:5], func=AF.Exp), reads=[d_sm], writes=[d_sm])
                P.op("dve", lambda e: e.tensor_scalar(out=sm[:, 6:7], in0=sm[:, 5:6], scalar1=1.0, scalar2=None, op0=ALU.add), reads=[d_sm], writes=[d_sm])
                P.op("dve", lambda e: e.reciprocal(out=sm[:, 6:7], in_=sm[:, 6:7]), reads=[d_sm], writes=[d_sm])
                P.op("dve", lambda e: e.tensor_tensor(out=sm[:, 7:8], in0=sm[:, 5:6], in1=sm[:, 6:7], op=ALU.mult), reads=[d_sm], writes=[d_sm])
                P.op("dve", lambda e, ti=ti: e.tensor_scalar(out=combs[:, ti, :], in0=sm[:, 6:8], scalar1=sm[:, 3:4], scalar2=None, op0=ALU.mult), reads=[d_sm], writes=[d_combs])
                for j in range(2):
                    P.op("dve", lambda e, j=j: e.tensor_scalar(out=oh[:, j * 32:(j + 1) * 32], in0=lg[:, 4:36], scalar1=t8[:, j:j + 1], scalar2=None, op0=ALU.is_equal),
                         reads=[d_lg, d_t8], writes=[d_oh])
                    P.op("dve", lambda e, j=j: e.tensor_tensor(out=oh[:, j * 32:(j + 1) * 32].rearrange("p (g e) -> p g e", g=4), in0=oh[:, j * 32:(j + 1) * 32].rearrange("p (g e) -> p g e", g=4),
                                                               in1=goh[:].unsqueeze(2).to_broadcast([128, 4, 8]), op=ALU.mult), reads=[d_oh, d_goh], writes=[d_oh])
                P.op("dve", lambda e: e.tensor_tensor(out=ohs[:], in0=oh[:, 0:32], in1=oh[:, 32:64], op=ALU.add), reads=[d_oh], writes=[d_ohs])
                P.op("pe", lambda e: e.matmul(pF[:, 64:96], lhsT=tri[:], rhs=ohs[:], start=True, stop=True), reads=[d_c, d_ohs], writes=[d_pF])
                P.op("pe", lambda e: e.matmul(pF[:, 128:160], lhsT=onesf[:], rhs=ohs[:], start=True, stop=True), reads=[d_c, d_ohs], writes=[d_pF])
                P.op("dve", lambda e: e.tensor_tensor(out=posf[:], in0=pF[:, 64:96], in1=ohs[:], op=ALU.subtract), reads=[d_pF, d_ohs], writes=[d_posf])
                P.op("dve", lambda e: e.tensor_tensor(out=posf[:], in0=posf[:], in1=base[:], op=ALU.add), reads=[d_posf, d_base], writes=[d_posf])
                P.op("dve", lambda e: e.tensor_tensor(out=posf[:], in0=posf[:], in1=ecap[:], op=ALU.add), reads=[d_posf, d_c], writes=[d_posf])
                P.op("dve", lambda e: e.tensor_tensor(out=base[:], in0=base[:], in1=pF[:, 128:160], op=ALU.add), reads=[d_base, d_pF, d_posf], writes=[d_base])
                for j in range(2):
                    P.op("dve", lambda e, j=j: e.tensor_tensor(out=oh[:, j * 32:(j + 1) * 32], in0=oh[:, j * 32:(j + 1) * 32], in1=posf[:], op=ALU.mult), reads=[d_oh, d_posf], writes=[d_oh])
                    P.op("dve", lambda e, j=j: e.tensor_reduce(out=idf2[:, j:j + 1], in_=oh[:, j * 32:(j + 1) * 32], axis=AX.X, op=ALU.add), reads=[d_oh], writes=[d_idf2])
                P.op("dve", lambda e, ti=ti: e.tensor_copy(out=idxs[:, ti, :], in_=idf2[:]), reads=[d_idf2], writes=[d_idxs])
                for j in range(2):
                    P.dma("pool", lambda e, ti=ti, j=j, hbuf=hbuf: e.indirect_dma_start(out=xbuf[:, :], out_offset=bass.IndirectOffsetOnAxis(ap=idxs[:, ti, j:j + 1], axis=0),
                                                                                      in_=h2b[hbuf][:], in_offset=None),
                          reads=[d_h2b[hbuf], d_idxs], writes=[d_xbuf])

        if stage < 3:
            return
        if hasattr(P, 'barrier'):
            P.barrier()
        with contextlib.ExitStack() as st:
            def sb(name, shape, dt):
                return st.enter_context(nc.sbuf_tensor("d_" + name, shape, dt))

            def ps(name, shape, dt=F32):
                return st.enter_context(nc.psum_tensor(name, shape, dt))
            NW = 2
            wge = [sb("wge%d" % i, [128, 8, 512], BF16) for i in range(NW)]; d_wge = [Dep() for _ in range(NW)]
            wue = [sb("wue%d" % i, [128, 8, 512], BF16) for i in range(NW)]; d_wue = [Dep() for _ in range(NW)]
            wde = [sb("wde%d" % i, [128, 4, 1024], BF16) for i in range(NW)]; d_wde = [Dep() for _ in range(NW)]
            xe = [sb("xe%d" % i, [128, 3, D], BF16) for i in range(NW)]; d_xe = [Dep() for _ in range(NW)]
            xeT = sb("xeT", [128, 8, CAP], BF16); d_xeT = Dep()
            sg = sb("sg", [128, CAP], F32); d_sg = Dep()
            hTe = sb("hTe", [128, 4, CAP], BF16); d_hTe = Dep()
            ye = [sb("ye%d" % i, [128, 3, D], F32) for i in range(NW)]; d_ye = [Dep() for _ in range(NW)]
            pT = ps("pTe", [128, 8, 128], BF16); d_pT = Dep()
            pg_ = [ps("pge%d" % i, [128, 512], F32) for i in range(2)]; d_pg = [Dep() for _ in range(2)]
            pu_ = [ps("pue%d" % i, [128, 512], F32) for i in range(2)]; d_pu = [Dep() for _ in range(2)]
            py_ = [ps("pye%d" % i, [128, 512], F32) for i in range(2)]; d_py = [Dep() for _ in range(2)]
            for ie, ex in enumerate(experts):
                b = ie % NW
                P.dma("pool", lambda e, b=b, ex=ex: e.dma_start(out=wge[b][:], in_=wgate_d[ex].rearrange("(k p) f -> p k f", p=128)), writes=[d_wge[b]])
                P.dma("pool", lambda e, b=b, ex=ex: e.dma_start(out=wue[b][:], in_=wup_d[ex].rearrange("(k p) f -> p k f", p=128)), writes=[d_wue[b]])
                P.dma("pool", lambda e, b=b, ex=ex: e.dma_start(out=wde[b][:], in_=wdown_d[ex].rearrange("(k p) f -> p k f", p=128)), writes=[d_wde[b]])
                P.dma("sp", lambda e, b=b, ex=ex: e.dma_start(out=xe[b][:], in_=xbuf[ex * CAP:(ex + 1) * CAP, :].rearrange("(s p) d -> p s d", p=128)),
                      reads=[d_xbuf], writes=[d_xe[b]])
                for s in range(3):
                    for k in range(8):
                        P.op("pe", lambda e, b=b, s=s, k=k: e.transpose(out=pT[:, k, :], in_=xe[b][:, s, k * 128:(k + 1) * 128], identity=idb[:]),
                             reads=[d_xe[b], d_idb], writes=[d_pT])
                    P.op("act", lambda e, s=s: e.copy(out=xeT[:, :, s * 128:(s + 1) * 128], in_=pT[:]), reads=[d_pT], writes=[d_xeT])
                for ft in range(4):
                    pb = ft % 2
                    for k in range(8):
                        P.op("pe", lambda e, b=b, ft=ft, k=k, pb=pb: e.matmul(pg_[pb][:, 0:CAP], lhsT=wge[b][:, k, ft * 128:(ft + 1) * 128], rhs=xeT[:, k, :],
                                                                            start=(k == 0), stop=(k == 7)), reads=[d_wge[b], d_xeT], writes=[d_pg[pb]])
                    for k in range(8):
                        P.op("pe", lambda e, b=b, ft=ft, k=k, pb=pb: e.matmul(pu_[pb][:, 0:CAP], lhsT=wue[b][:, k, ft * 128:(ft + 1) * 128], rhs=xeT[:, k, :],
                                                                            start=(k == 0), stop=(k == 7)), reads=[d_wue[b], d_xeT], writes=[d_pu[pb]])
                    P.op("act", lambda e, pb=pb: e.activation(out=sg[:], in_=pg_[pb][:, 0:CAP], func=AF.Silu), reads=[d_pg[pb]], writes=[d_sg])
                    P.op("dve", lambda e, ft=ft, pb=pb: e.tensor_tensor(out=hTe[:, ft, :], in0=sg[:], in1=pu_[pb][:, 0:CAP], op=ALU.mult),
                         reads=[d_sg, d_pu[pb]], writes=[d_hTe])
                for s in range(3):
                    for hv in range(2):
                        for ft in range(4):
                            P.op("pe", lambda e, b=b, s=s, hv=hv, ft=ft: e.matmul(py_[hv][:, :], lhsT=hTe[:, ft, s * 128:(s + 1) * 128], rhs=wde[b][:, ft, hv * 512:(hv + 1) * 512],
                                                                                start=(ft == 0), stop=(ft == 3)), reads=[d_hTe, d_wde[b]], writes=[d_py[hv]])
                        P.op("act" if hv == 0 else "dve", (lambda e, b=b, s=s, hv=hv: e.copy(out=ye[b][:, s, hv * 512:(hv + 1) * 512], in_=py_[hv][:, :])) if hv == 0 else
                             (lambda e, b=b, s=s, hv=hv: e.tensor_copy(out=ye[b][:, s, hv * 512:(hv + 1) * 512], in_=py_[hv][:, :])),
                             reads=[d_py[hv]], writes=[d_ye[b]])
                P.dma("sp", lambda e, b=b, ex=ex: e.dma_start(out=ybuf[ex * CAP:(ex + 1) * CAP, :].rearrange("(s p) d -> p s d", p=128), in_=ye[b][:]),
                      reads=[d_ye[b]], writes=[d_ybuf])

        if hasattr(P, 'barrier'):
            P.barrier()
        with contextlib.ExitStack() as st:
            def sb(name, shape, dt):
                return st.enter_context(nc.sbuf_tensor("d_" + name, shape, dt))
            NF = 4
            x1t = [sb("x1t%d" % i, [128, D], F32) for i in range(NF)]; d_x1t = [Dep() for _ in range(NF)]
            y1 = [sb("y1t%d" % i, [128, D], F32) for i in range(NF)]; d_y1 = [Dep() for _ in range(NF)]
            y2 = [sb("y2t%d" % i, [128, D], F32) for i in range(NF)]; d_y2 = [Dep() for _ in range(NF)]
            ot = [sb("ot%d" % i, [128, D], F32) for i in range(NF)]; d_ot = [Dep() for _ in range(NF)]
            for ti in range(ntiles):
                b = ti % NF
                r0 = ti * 128
                P.dma("sp", lambda e, b=b, r0=r0: e.dma_start(out=x1t[b][:], in_=x1_d[r0:r0 + 128, :]), reads=[d_x1d], writes=[d_x1t[b]])
                P.dma("pool", lambda e, b=b, ti=ti: e.indirect_dma_start(out=y1[b][:], out_offset=None, in_=ybuf[:, :],
                                                                        in_offset=bass.IndirectOffsetOnAxis(ap=idxs[:, ti, 0:1], axis=0)),
                      reads=[d_ybuf, d_idxs], writes=[d_y1[b]])
                P.dma("pool", lambda e, b=b, ti=ti: e.indirect_dma_start(out=y2[b][:], out_offset=None, in_=ybuf[:, :],
                                                                        in_offset=bass.IndirectOffsetOnAxis(ap=idxs[:, ti, 1:2], axis=0)),
                      reads=[d_ybuf, d_idxs], writes=[d_y2[b]])
                P.op("dve", lambda e, b=b, ti=ti: e.scalar_tensor_tensor(out=ot[b][:], in0=y1[b][:], scalar=combs[:, ti, 0:1], in1=x1t[b][:], op0=ALU.mult, op1=ALU.add),
                     reads=[d_y1[b], d_combs, d_x1t[b]], writes=[d_ot[b]])
                P.op("dve", lambda e, b=b, ti=ti: e.scalar_tensor_tensor(out=ot[b][:], in0=y2[b][:], scalar=combs[:, ti, 1:2], in1=ot[b][:], op0=ALU.mult, op1=ALU.add),
                     reads=[d_y2[b], d_combs, d_ot[b]], writes=[d_ot[b]])
                P.dma("sp", lambda e, b=b, r0=r0: e.dma_start(out=out_d[r0:r0 + 128, :], in_=ot[b][:]), reads=[d_ot[b]], writes=[d_out])


def host_consts_d(z):
    tri = (np.arange(128)[:, None] <= np.arange(128)[None, :]).astype(np.float32)
    return dict(mqn=np.tile(z['mem_q_norm'], 4)[None, :], mkn=np.tile(z['mem_k_norm'], 4)[None, :],
                w_r=np.ascontiguousarray(np.concatenate([z['w_router_group'], z['w_router_expert']], axis=1)),
                trif=tri, ecap=(np.arange(32, dtype=np.float32) * CAP)[None, :], ident=np.eye(128, dtype=np.float32))


def build_full():
    nc = bass.Bass("TRN2", target_bir_lowering=False)
    P = Prog(nc)

    def di(name, shape, dt=F32):
        return nc.dram_tensor(name, shape, dt, kind="ExternalInput")
    xin = di("xin", [TP + T, D]); w_in = di("w_in", [D, IN_COLS]); g_mix = di("g_mix", [1, D])
    qkn = di("qkn", [2, 512]); cs = di("cs", [TP + T, 64]); ident_d = di("ident", [128, 128])
    e_d = di("e_d", [32, NK], BF16); vq_d = di("vq", [1, 1024]); nb_d = di("nb", [1, 1024]); oq_d = di("oq", [1, 1024])
    tri_d = di("tri", [128, 128], BF16)
    cwl_d = di("cwl", [128, 16, 4]); cbl_d = di("cbl", [128, 16]); dtb_d = di("dtb", [1, 16]); alog_d = di("alog", [1, 16])
    dsk_d = di("dsk", [1, 16]); ssdn_d = di("ssdn", [1, D]); tri2_d = di("tri2", [128, 2, 256]); sel_d = di("sel_d", [16, 16, 128])
    tb_d = di("tb_d", [128, 384]); pflag_d = di("pflag", [1, 1])
    mem_d = di("mem", [256, D]); gmem_d = di("g_mem", [1, D]); wmemkv = di("w_mem_kv", [D, 1024])
    mqn_d = di("mqn", [1, 512]); mkn_d = di("mkn", [1, 512])
    woa_d = di("w_o_moba", [512, D]); wos_d = di("w_o_ssd", [D, D]); wom_d = di("w_o_mem", [512, D]); wout_d = di("w_out", [D, D])
    gffn_d = di("g_ffn", [1, D]); wr_d = di("w_r", [D, 36])
    wgate_d = di("w_gate", [NE, D, 512]); wup_d = di("w_up", [NE, D, 512]); wdown_d = di("w_down", [NE, 512, D])
    trif_d = di("trif", [128, 128]); ecap_d = di("ecap", [1, 32])
    out_d = nc.dram_tensor("out", [T, D], F32, kind="ExternalOutput")
    kT_d = nc.dram_tensor("kT_d", [512, NK], BF16)
    v_d = nc.dram_tensor("v_d", [NK, 512], BF16)
    qT_d = nc.dram_tensor("qT_d", [512, T], BF16)
    oa_d = nc.dram_tensor("oa_d", [T, 512], BF16)
    os_d = nc.dram_tensor("os_d", [T, D], BF16)
    x1_d = nc.dram_tensor("x1_d", [T, D], F32)
    xbuf = nc.dram_tensor("xbuf", [NE * CAP, D], BF16)
    ybuf = nc.dram_tensor("ybuf", [NE * CAP, D], F32)

    with contextlib.ExitStack() as st0:
        idf = st0.enter_context(nc.sbuf_tensor("idf", [128, 128], F32)); d_idf = Dep()
        idb = st0.enter_context(nc.sbuf_tensor("idb", [128, 128], BF16)); d_idb = Dep()
        P.dma("sp", lambda e: e.dma_start(out=idf[:], in_=ident_d[:, :]), writes=[d_idf])
        P.op("dve", lambda e: e.tensor_copy(out=idb[:], in_=idf[:]), reads=[d_idf], writes=[d_idb])
        with contextlib.ExitStack() as st:
            phase_a(nc, P, st, xin, w_in, g_mix, qkn, cs, idb, d_idb, kT_d, v_d, qT_d)
        P.barrier()
        with contextlib.ExitStack() as st:
            oa = st.enter_context(nc.sbuf_tensor("s_oa", [128, 32, 512], BF16)); d_oa = Dep()
            phase_b(nc, P, st, kT_d, v_d, qT_d, e_d, vq_d, nb_d, oq_d, tri_d, idb, d_idb, oa, d_oa)
            P.dma("sp", lambda e: e.dma_start(out=oa_d[:, :].rearrange("(t p) c -> p t c", p=128), in_=oa[:]), reads=[d_oa])
        P.barrier()
        with contextlib.ExitStack() as st:
            phase_c(nc, P, st, xin, w_in, g_mix, cwl_d, cbl_d, dtb_d, alog_d, dsk_d, ssdn_d, tri2_d, sel_d, tb_d, pflag_d,
                    idb, d_idb, os_d, Dep())
        P.barrier()
        phase_def(nc, P, xin[TP:TP + T, :], w_in, g_mix, mem_d, gmem_d, wmemkv, mqn_d, mkn_d, woa_d, wos_d, wom_d, wout_d, gffn_d, wr_d,
                  wgate_d, wup_d, wdown_d, trif_d, ecap_d, oa_d, Dep(), os_d, Dep(), x1_d, xbuf, ybuf, out_d, idb, d_idb, idf, d_idf)
        P.emit()
    return nc


_NC_CACHE = {}


def kernel(x, mem, g_mix, w_in, moba_q_norm, moba_k_norm, conv_w, conv_b, dt_bias, a_log, d_skip, ssd_norm, g_mem, w_mem_kv,
           mem_q_norm, mem_k_norm, w_o_moba, w_o_ssd, w_o_mem, w_out, g_ffn, w_router_group, w_router_expert, w_gate, w_up, w_down):
    f32 = np.float32
    A = lambda a: np.ascontiguousarray(np.asarray(a, dtype=f32))
    x = A(x); mem = A(mem)
    z = dict(conv_w=A(conv_w), conv_b=A(conv_b), dt_bias=A(dt_bias), a_log=A(a_log), d_skip=A(d_skip), ssd_norm=A(ssd_norm),
             mem_q_norm=A(mem_q_norm), mem_k_norm=A(mem_k_norm), w_router_group=A(w_router_group), w_router_expert=A(w_router_expert))
    if "nc" not in _NC_CACHE:
        _NC_CACHE["nc"] = build_full()
    nc = _NC_CACHE["nc"]
    pos = np.arange(2 * T, dtype=f32)
    inv = (10000.0 ** (-np.arange(32, dtype=f32) / 32)).astype(f32)
    ang = pos[:, None] * inv[None, :]
    cs_full = np.concatenate([np.cos(ang), np.sin(ang)], axis=1).astype(f32)
    qkn = np.stack([np.tile(A(moba_q_norm), 8), np.tile(A(moba_k_norm), 8)]).astype(f32)
    shared = dict(w_in=A(w_in), g_mix=A(g_mix)[None, :], qkn=qkn, ident=np.eye(128, dtype=f32),
                  g_mem=A(g_mem)[None, :], w_mem_kv=A(w_mem_kv), w_o_moba=A(w_o_moba), w_o_ssd=A(w_o_ssd), w_o_mem=A(w_o_mem),
                  w_out=A(w_out), g_ffn=A(g_ffn)[None, :], w_gate=A(w_gate), w_up=A(w_up), w_down=A(w_down))
    shared.update(host_consts_d(z))
    in_maps = []
    for c in range(8):
        bb, hh = c // 2, c % 2
        if hh == 0:
            xin = np.concatenate([np.zeros((TP, D), f32), x[bb, :T]], 0)
            csc = np.concatenate([cs_full[:TP], cs_full[:T]], 0)
        else:
            xin = x[bb]
            csc = cs_full
        m = dict(shared)
        m.update(xin=np.ascontiguousarray(xin), cs=np.ascontiguousarray(csc), mem=mem[bb])
        m.update(host_consts(hh))
        m.update(host_consts_c(z, hh))
        in_maps.append(m)
    res = run_bass_kernel_spmd(nc, in_maps, core_ids=list(range(8)))
    out = np.empty((4, 2 * T, D), f32)
    for c in range(8):
        bb, hh = c // 2, c % 2
        out[bb, hh * T:(hh + 1) * T] = np.asarray(res.results[c]["out"], dtype=f32)
    return out
```

```python
import contextlib
import numpy as np
import ml_dtypes
import concourse.bass as bass
import concourse.mybir as mybir
from concourse.bass_utils import run_bass_kernel_spmd

F32 = mybir.dt.float32
BF16 = mybir.dt.bfloat16
I32 = mybir.dt.int32
U32 = mybir.dt.uint32
AF = mybir.ActivationFunctionType
ALU = mybir.AluOpType
AX = mybir.AxisListType

T = 4096
TP = 4096
NK = TP + T
D = 1024
EPS = 1e-6
IN_COLS = 8208
BIGG = 30000.0
MB = 1000.0
NEG = -30000.0
C_Z, C_X, C_DT = 1536, 2560, 4608
C_QM, C_G = 4624, 5136
CAP = 384
NE = 32


class Dep:
    __slots__ = ("w", "r")

    def __init__(self):
        self.w = None
        self.r = {}


class Op:
    __slots__ = ("eng", "fn", "deps", "is_dma", "sig", "sigval", "dsem", "dval", "prev")

    def __init__(self, eng, fn, is_dma):
        self.eng = eng
        self.fn = fn
        self.is_dma = is_dma
        self.deps = []
        self.sig = False
        self.sigval = 0
        self.dsem = None
        self.dval = 0
        self.prev = None


class Prog:
    ENGS = ("pe", "act", "dve", "pool", "sp")

    def __init__(self, nc, ndma_sems=12):
        self.nc = nc
        self.ops = {e: [] for e in self.ENGS}
        self.ndma = {e: 0 for e in self.ENGS}
        self.dma_last = {}
        self.ndma_sems = ndma_sems
        self.all_dma = []

    def _add(self, o, reads, writes):
        deps = {}
        raw = set()
        for t in reads:
            if t.w is not None:
                deps[id(t.w)] = t.w
                raw.add(id(t.w))
        for t in writes:
            if t.w is not None:
                deps[id(t.w)] = t.w
            for r in t.r.values():
                deps[id(r)] = r
        for t in reads:
            key = id(o) if o.is_dma else o.eng
            t.r[key] = o
        for t in writes:
            t.w = o
            t.r = {}
        dl = []
        for d in deps.values():
            if d is o:
                continue
            if (not d.is_dma) and (not o.is_dma) and d.eng == "pe" and o.eng == "pe":
                continue
            if (not d.is_dma) and (not o.is_dma) and d.eng == o.eng and id(d) not in raw:
                continue
            dl.append(d)
            if not d.is_dma:
                d.sig = True
        o.deps = dl
        self.ops[o.eng].append(o)
        return o

    def op(self, eng, fn, reads=(), writes=()):
        return self._add(Op(eng, fn, False), reads, writes)

    def dma(self, eng, fn, reads=(), writes=()):
        o = Op(eng, fn, True)
        n = self.ndma[eng]
        self.ndma[eng] += 1
        slot = (eng, n % self.ndma_sems)
        o.dsem = slot
        o.prev = self.dma_last.get(slot)
        o.dval = (o.prev.dval if o.prev else 0) + 16
        self.dma_last[slot] = o
        self.all_dma.append(o)
        return self._add(o, reads, writes)


    def barrier(self):
        lasts = []
        for e in self.ENGS:
            for o in reversed(self.ops[e]):
                if (not o.is_dma) and o.fn is not None:
                    o.sig = True
                    lasts.append(o)
                    break
        dmas = list(self.dma_last.values())
        for e in self.ENGS:
            o = Op(e, None, False)
            o.deps = [d for d in lasts if d.eng != e] + dmas
            self.ops[e].append(o)

    def emit(self):
        nc = self.nc
        import contextlib
        with contextlib.ExitStack() as st:
            esem = {e: st.enter_context(nc.semaphore("S_" + e)) for e in self.ENGS}
            dsem = {}
            for e in self.ENGS:
                if self.ndma[e]:
                    for i in range(min(self.ndma_sems, self.ndma[e])):
                        dsem[(e, i)] = st.enter_context(nc.semaphore("D_%s_%d" % (e, i)))
            for e in self.ENGS:
                c = 0
                for o in self.ops[e]:
                    if (not o.is_dma) and o.sig and o.fn is not None:
                        c += 1
                        o.sigval = c
            block = st.enter_context(nc.Block())
            handles = {"pe": block.tensor, "act": block.scalar, "dve": block.vector,
                       "pool": block.gpsimd, "sp": block.sync}

            def make(e):
                def body(eng):
                    known = {}

                    def wait(sem, key, val):
                        if known.get(key, 0) < val:
                            eng.wait_ge(sem, val)
                            known[key] = val
                    for o in self.ops[e]:
                        for d in o.deps:
                            if d.is_dma:
                                wait(dsem[d.dsem], d.dsem, d.dval)
                            else:
                                wait(esem[d.eng], d.eng, d.sigval)
                        if o.is_dma:
                            if o.prev is not None:
                                wait(dsem[o.dsem], o.dsem, o.prev.dval)
                            o.fn(eng).then_inc(dsem[o.dsem], 16)
                        elif o.fn is not None:
                            ins = o.fn(eng)
                            if o.sig:
                                ins.then_inc(esem[e], 1)
                    if e == "sp":
                        for slot, o in self.dma_last.items():
                            wait(dsem[slot], slot, o.dval)
                return body
            for e in self.ENGS:
                if self.ops[e] or e == "sp":
                    handles[e](make(e))


def phase_a(nc, P, st, xin, w_in, g_mix, qkn, cs, idb, d_idb, kT_d, v_d, qT_d):
    def sb(name, shape, dt):
        return st.enter_context(nc.sbuf_tensor("a_" + name, shape, dt))

    def ps(name, shape, dt=F32):
        return st.enter_context(nc.psum_tensor("a_" + name, shape, dt))
    wq = sb("wq", [128, 8, 1536], BF16); d_wq = Dep()
    gm = sb("gm", [128, D], F32); d_gm = Dep()
    gq = sb("gq", [128, 2, 512], F32); d_gq = Dep()
    NB = 2
    NX = 4
    xt = [sb("xt%d" % i, [128, D], F32) for i in range(NX)]; d_xt = [Dep() for _ in range(NX)]
    cst = [sb("cst%d" % i, [128, 64], F32) for i in range(NX)]; d_cst = [Dep() for _ in range(NX)]
    junk = sb("junk", [128, D], BF16); d_junk = Dep()
    ssq = sb("ssq", [128, 1], F32); d_ssq = Dep()
    rstd = sb("rstd", [128, 1], F32); d_rstd = Dep()
    hb_ = [sb("hb%d" % i, [128, D], BF16) for i in range(2)]; d_hb_ = [Dep(), Dep()]
    hT = [sb("hT%d" % i, [128, 8, 128], BF16) for i in range(NB)]; d_hT = [Dep() for _ in range(NB)]
    pT = ps("pT", [128, 8, 128], BF16); d_pT = Dep()
    pq = [[ps("pq%d_%d" % (j, i), [128, 512], F32) for i in range(3)] for j in range(2)]; d_pq = [[Dep() for _ in range(3)] for _ in range(2)]
    pkT = ps("pkT", [128, 4, 128], BF16); d_pkT = Dep()
    sq_ = [sb("sq%d" % i, [128, 512], F32) for i in range(2)]; d_sq_ = [Dep(), Dep()]
    hs_ = [sb("hs%d" % i, [128, 8], F32) for i in range(2)]; d_hs_ = [Dep(), Dep()]
    hr_ = [sb("hr%d" % i, [128, 8], F32) for i in range(2)]; d_hr_ = [Dep(), Dep()]
    qn_ = [sb("qn%d" % i, [128, 512], F32) for i in range(2)]; d_qn_ = [Dep(), Dep()]
    t1_ = [sb("t1%d" % i, [128, 256], F32) for i in range(2)]; d_t1_ = [Dep(), Dep()]
    t2_ = [sb("t2%d" % i, [128, 256], F32) for i in range(2)]; d_t2_ = [Dep(), Dep()]
    t3_ = [sb("t3%d" % i, [128, 256], F32) for i in range(2)]; d_t3_ = [Dep(), Dep()]
    t4_ = [sb("t4%d" % i, [128, 256], F32) for i in range(2)]; d_t4_ = [Dep(), Dep()]
    qr_ = [[sb("qr%d_%d" % (i, j), [128, 512], BF16) for j in range(2)] for i in range(2)]; d_qr_ = [[Dep(), Dep()] for _ in range(2)]
    kTs = [[sb("kTs%d_%d" % (j, i), [128, 4, 128], BF16) for i in range(NB)] for j in range(2)]; d_kTs = [[Dep() for _ in range(NB)] for _ in range(2)]
    vs = [sb("vs%d" % i, [128, 512], BF16) for i in range(NB)]; d_vs = [Dep() for _ in range(NB)]

    for k in range(8):
        P.dma("pool", lambda e, k=k: e.dma_start(out=wq[:, k, :], in_=w_in[k * 128:(k + 1) * 128, 0:1536]), writes=[d_wq])
    P.dma("sp", lambda e: e.dma_start(out=gm[:], in_=g_mix[0:1, :].partition_broadcast(128)), writes=[d_gm])
    for i in range(2):
        P.dma("sp", lambda e, i=i: e.dma_start(out=gq[:, i, :], in_=qkn[i:i + 1, :].partition_broadcast(128)), writes=[d_gq])
    P.op("dve", lambda e: e.tensor_scalar(out=gq[:, 0, :], in0=gq[:, 0, :], scalar1=0.125, scalar2=None, op0=ALU.mult),
         reads=[d_gq], writes=[d_gq])

    def qk_post_steps(src_ps, d_src, which, b, xs):
        sl = which
        sq, hs, hr, qn, t1, t2, t3, t4, qr = sq_[sl], hs_[sl], hr_[sl], qn_[sl], t1_[sl], t2_[sl], t3_[sl], t4_[sl], qr_[sl][b]
        d_sq, d_hs, d_hr, d_qn, d_t1, d_t2, d_t3, d_t4, d_qr = (d_sq_[sl], d_hs_[sl], d_hr_[sl], d_qn_[sl], d_t1_[sl], d_t2_[sl],
                                                               d_t3_[sl], d_t4_[sl], d_qr_[sl][b])
        q3 = qn[:].rearrange("p (h d) -> p h d", h=8)
        q1 = q3[:, :, 0:32]
        q2 = q3[:, :, 32:64]
        cosb = cst[xs][:, 0:32].unsqueeze(1).to_broadcast([128, 8, 32])
        sinb = cst[xs][:, 32:64].unsqueeze(1).to_broadcast([128, 8, 32])
        v3 = lambda t: t[:].rearrange("p (h d) -> p h d", h=8)
        r3 = qr[:].rearrange("p (h d) -> p h d", h=8)
        steps = [
            lambda: P.op("act", lambda e: e.activation(out=sq[:], in_=src_ps[:], func=AF.Square), reads=[d_src], writes=[d_sq]),
            lambda: P.op("dve", lambda e: e.tensor_reduce(out=hs[:], in_=sq[:].rearrange("p (h d) -> p h d", h=8), axis=AX.X, op=ALU.add),
                         reads=[d_sq], writes=[d_hs]),
            lambda: P.op("act", lambda e: e.activation(out=hr[:], in_=hs[:], func=AF.Sqrt, bias=EPS, scale=1.0 / 64), reads=[d_hs], writes=[d_hr]),
            lambda: P.op("dve", lambda e: e.reciprocal(out=hr[:], in_=hr[:]), reads=[d_hr], writes=[d_hr]),
            lambda: P.op("dve", lambda e: e.tensor_tensor(out=qn[:].rearrange("p (h d) -> p h d", h=8), in0=src_ps[:].rearrange("p (h d) -> p h d", h=8),
                                                          in1=hr[:].unsqueeze(2).to_broadcast([128, 8, 64]), op=ALU.mult),
                         reads=[d_src, d_hr], writes=[d_qn]),
            lambda: P.op("pool", lambda e: e.tensor_tensor(out=qn[:], in0=qn[:], in1=gq[:, which, :], op=ALU.mult), reads=[d_qn, d_gq], writes=[d_qn]),
            lambda: P.op("dve", lambda e: e.tensor_tensor(out=v3(t1), in0=q1, in1=cosb, op=ALU.mult), reads=[d_qn, d_cst[xs]], writes=[d_t1]),
            lambda: P.op("pool", lambda e: e.tensor_tensor(out=v3(t2), in0=q2, in1=sinb, op=ALU.mult), reads=[d_qn, d_cst[xs]], writes=[d_t2]),
            lambda: P.op("dve", lambda e: e.tensor_tensor(out=v3(t3), in0=q2, in1=cosb, op=ALU.mult), reads=[d_qn, d_cst[xs]], writes=[d_t3]),
            lambda: P.op("pool", lambda e: e.tensor_tensor(out=v3(t4), in0=q1, in1=sinb, op=ALU.mult), reads=[d_qn, d_cst[xs]], writes=[d_t4]),
            lambda: P.op("dve", lambda e: e.tensor_tensor(out=r3[:, :, 0:32], in0=v3(t1), in1=v3(t2), op=ALU.subtract), reads=[d_t1, d_t2], writes=[d_qr]),
            lambda: P.op("pool", lambda e: e.tensor_tensor(out=r3[:, :, 32:64], in0=v3(t3), in1=v3(t4), op=ALU.add), reads=[d_t3, d_t4], writes=[d_qr]),
        ]
        return steps

    def to_T_and_store(dst_dram, col0, b, sl):
        for c in range(4):
            P.op("pe", lambda e, c=c: e.transpose(out=pkT[:, c, :], in_=qr_[sl][b][:, c * 128:(c + 1) * 128], identity=idb[:]),
                 reads=[d_qr_[sl][b], d_idb], writes=[d_pkT])
        P.op("act", lambda e: e.copy(out=kTs[sl][b][:], in_=pkT[:]), reads=[d_pkT], writes=[d_kTs[sl][b]])
        P.dma("sp", lambda e: e.dma_start(out=dst_dram[:, col0:col0 + 128].rearrange("(c p) t -> p c t", p=128), in_=kTs[sl][b][:]),
              reads=[d_kTs[sl][b]])

    def load(ti):
        xs = ti % NX
        r0 = ti * 128
        P.dma("sp", lambda e: e.dma_start(out=xt[xs][:], in_=xin[r0:r0 + 128, :]), writes=[d_xt[xs]])
        P.dma("sp", lambda e: e.dma_start(out=cst[xs][:], in_=cs[r0:r0 + 128, :]), writes=[d_cst[xs]])

    def norm(ti):
        b = ti % NB
        xs = ti % NX
        hb = hb_[b]
        P.op("act", lambda e: e.activation(out=junk[:], in_=xt[xs][:], func=AF.Square, accum_out=ssq[:]), reads=[d_xt[xs]], writes=[d_junk, d_ssq])
        P.op("act", lambda e: e.activation(out=rstd[:], in_=ssq[:], func=AF.Sqrt, bias=EPS, scale=1.0 / D), reads=[d_ssq], writes=[d_rstd])
        P.op("dve", lambda e: e.reciprocal(out=rstd[:], in_=rstd[:]), reads=[d_rstd], writes=[d_rstd])
        P.op("dve", lambda e: e.scalar_tensor_tensor(out=hb[:], in0=xt[xs][:], scalar=rstd[:, 0:1], in1=gm[:], op0=ALU.mult, op1=ALU.mult),
             reads=[d_xt[xs], d_rstd, d_gm], writes=[d_hb_[b]])

    def front(ti):
        b = ti % NB
        xs = ti % NX
        own = ti >= 32
        hb = hb_[b]
        for k in range(8):
            P.op("pe", lambda e, k=k: e.transpose(out=pT[:, k, :], in_=hb[:, k * 128:(k + 1) * 128], identity=idb[:]), reads=[d_hb_[b], d_idb], writes=[d_pT])
        P.op("act", lambda e: e.copy(out=hT[b][:], in_=pT[:]), reads=[d_pT], writes=[d_hT[b]])
        groups = [0, 1, 2] if own else [1, 2]
        for g in groups:
            for k in range(8):
                P.op("pe", lambda e, g=g, k=k: e.matmul(pq[b][g][:], lhsT=hT[b][:, k, :], rhs=wq[:, k, g * 512:(g + 1) * 512], start=(k == 0), stop=(k == 7)),
                     reads=[d_hT[b], d_wq], writes=[d_pq[b][g]])

    def post(ti):
        b = ti % NB
        xs = ti % NX
        own = ti >= 32
        ks = qk_post_steps(pq[b][1], d_pq[b][1], 1, b, xs)
        qs = qk_post_steps(pq[b][0], d_pq[b][0], 0, b, xs) if own else []
        for i in range(len(ks)):
            ks[i]()
            if qs:
                qs[i]()
        P.op("act", lambda e: e.copy(out=vs[b][:], in_=pq[b][2][:]), reads=[d_pq[b][2]], writes=[d_vs[b]])

    def tail(ti):
        b = ti % NB
        own = ti >= 32
        r0 = ti * 128
        to_T_and_store(kT_d, r0, b, 1)
        if own:
            to_T_and_store(qT_d, r0 - TP, b, 0)
        P.dma("sp", lambda e: e.dma_start(out=v_d[r0:r0 + 128, :], in_=vs[b][:]), reads=[d_vs[b]])

    load(0)
    load(1)
    load(2)
    norm(0)
    norm(1)
    front(0)
    for ti in range(64):
        if ti + 3 < 64:
            load(ti + 3)
        if ti + 2 < 64:
            norm(ti + 2)
        if ti + 1 < 64:
            front(ti + 1)
        if ti >= 1:
            tail(ti - 1)
        post(ti)
    tail(63)


def phase_b(nc, P, st, kT_d, v_d, qT_d, e_d, vq_d, nb_d, oq_d, tri_d, idb, d_idb, oa, d_oa, heads=range(8), nblk=16):
    def sb(name, shape, dt):
        return st.enter_context(nc.sbuf_tensor("b_" + name, shape, dt))

    def ps(name, shape, dt=F32):
        return st.enter_context(nc.psum_tensor(name, shape, dt))
    NB = 2
    kaug = [sb("kaug%d" % i, [96, NK], BF16) for i in range(NB)]; d_kaug = [Dep() for _ in range(NB)]
    vaug = [sb("vaug%d" % i, [128, 64, 65], BF16) for i in range(NB)]; d_vaug = [Dep() for _ in range(NB)]
    qaug = [sb("qaug%d" % i, [96, T], BF16) for i in range(NB)]; d_qaug = [Dep() for _ in range(NB)]
    d_qmb = [Dep() for _ in range(NB)]
    vq = sb("vq_s", [128, 1024], F32); d_c = Dep()
    nb = sb("nb_s", [128, 1024], F32)
    oq = sb("oq_s", [128, 1024], F32)
    tri = sb("tri_s", [128, 128], BF16)
    ksum = sb("ksum", [64, 32], F32); d_ksum = Dep()
    kmean = sb("kmean", [64, 32], BF16); d_kmean = Dep()
    g1 = sb("g1", [128, 512], F32); d_g1 = Dep()
    top8 = sb("top8", [128, 16, 8], F32); d_top8 = Dep()
    sel = sb("sel", [128, 512], F32); d_sel = Dep()
    mbp = sb("mbp", [128, 16, 96], BF16); d_mbp = Dep()
    NPT = 3
    pt = [sb("pt%d" % i, [128, 512], BF16) for i in range(NPT)]; d_pt = [Dep() for _ in range(NPT)]
    rc = sb("rc", [128, 2], F32); d_rc = [Dep(), Dep()]
    pg = ps("pg", [128, 512], F32); d_pg = Dep()
    pmT = ps("pmT", [96, 8, 128], BF16); d_pmT = Dep()
    psS = [ps("psS%d" % i, [128, 512], F32) for i in range(NPT)]; d_psS = [Dep() for _ in range(NPT)]
    po = [ps("po%d" % i, [128, 65], F32) for i in range(2)]; d_po = [Dep() for _ in range(2)]

    for i in range(NB):
        P.dma("sp", lambda e, i=i: e.dma_start(out=kaug[i][64:96, :], in_=e_d[:, :]), writes=[d_kaug[i]])
        P.op("pool", lambda e, i=i: e.memset(vaug[i][:], 1.0), writes=[d_vaug[i]])
    P.dma("sp", lambda e: e.dma_start(out=vq[:], in_=vq_d[0:1, :].partition_broadcast(128)), writes=[d_c])
    P.dma("sp", lambda e: e.dma_start(out=nb[:], in_=nb_d[0:1, :].partition_broadcast(128)), writes=[d_c])
    P.dma("sp", lambda e: e.dma_start(out=oq[:], in_=oq_d[0:1, :].partition_broadcast(128)), writes=[d_c])
    P.dma("pool", lambda e: e.dma_start(out=tri[:], in_=tri_d[:, :]), writes=[d_c])
    P.op("pool", lambda e: e.memset(mbp[:], 0.0), writes=[d_mbp])

    for ih, h in enumerate(heads):
        b = ih % NB
        P.dma("sp", lambda e, b=b, h=h: e.dma_start(out=kaug[b][0:64, :], in_=kT_d[h * 64:(h + 1) * 64, :]), writes=[d_kaug[b]])
        P.dma("sp", lambda e, b=b, h=h: e.dma_start(out=vaug[b][:, :, 0:64],
                                                    in_=v_d[:, h * 64:(h + 1) * 64].rearrange("(t p) d -> p t d", p=128)),
              writes=[d_vaug[b]])
        P.dma("sp", lambda e, b=b, h=h: e.dma_start(out=qaug[b][0:64, :], in_=qT_d[h * 64:(h + 1) * 64, :]), writes=[d_qaug[b]])
        P.op("dve", lambda e, b=b: e.tensor_reduce(out=ksum[:], in_=kaug[b][0:64, :].rearrange("p (b k) -> p b k", k=256),
                                                   axis=AX.X, op=ALU.add), reads=[d_kaug[b]], writes=[d_ksum])
        P.op("dve", lambda e: e.tensor_scalar(out=kmean[:], in0=ksum[:], scalar1=1.0 / 256, scalar2=None, op0=ALU.mult),
             reads=[d_ksum], writes=[d_kmean])
        for half in range(2):
            for j in range(16):
                qt = half * 16 + j
                P.op("pe", lambda e, b=b, j=j, qt=qt: e.matmul(pg[:, j * 32:(j + 1) * 32], lhsT=qaug[b][0:64, qt * 128:(qt + 1) * 128],
                                                              rhs=kmean[:, :], start=True, stop=True),
                     reads=[d_qaug[b], d_kmean], writes=[d_pg])
            cs_ = slice(half * 512, (half + 1) * 512)
            P.op("dve", lambda e, cs_=cs_: e.tensor_tensor(out=g1[:], in0=pg[:], in1=vq[:, cs_], op=ALU.mult),
                 reads=[d_pg, d_c], writes=[d_g1])
            P.op("dve", lambda e, cs_=cs_: e.tensor_tensor(out=g1[:], in0=g1[:], in1=nb[:, cs_], op=ALU.add),
                 reads=[d_g1, d_c], writes=[d_g1])
            for j in range(16):
                P.op("dve", lambda e, j=j: e.max(out=top8[:, j, :], in_=g1[:, j * 32:(j + 1) * 32]), reads=[d_g1], writes=[d_top8])
            P.op("dve", lambda e: e.tensor_tensor(out=sel[:].rearrange("p (j b) -> p j b", b=32),
                                                  in0=g1[:].rearrange("p (j b) -> p j b", b=32),
                                                  in1=top8[:, :, 2:3].to_broadcast([128, 16, 32]), op=ALU.is_ge),
                 reads=[d_g1, d_top8], writes=[d_sel])
            P.op("dve", lambda e, cs_=cs_: e.tensor_tensor(out=sel[:], in0=sel[:], in1=vq[:, cs_], op=ALU.mult),
                 reads=[d_sel, d_c], writes=[d_sel])
            P.op("dve", lambda e, cs_=cs_: e.tensor_tensor(out=sel[:], in0=sel[:], in1=oq[:, cs_], op=ALU.add),
                 reads=[d_sel, d_c], writes=[d_sel])
            P.op("dve", lambda e: e.tensor_scalar(out=mbp[:, :, 64:96], in0=sel[:].rearrange("p (j b) -> p j b", b=32),
                                                  scalar1=1.0, scalar2=MB, op0=ALU.subtract, op1=ALU.mult),
                 reads=[d_sel], writes=[d_mbp])
            for grp in range(2):
                for j in range(8):
                    P.op("pe", lambda e, grp=grp, j=j: e.transpose(out=pmT[:, j, :], in_=mbp[:, grp * 8 + j, :], identity=idb[:]),
                         reads=[d_mbp, d_idb], writes=[d_pmT])
                c0 = (half * 16 + grp * 8) * 128
                P.op("act", lambda e, b=b, c0=c0: e.copy(out=qaug[b][64:96, c0:c0 + 1024], in_=pmT[64:96, :, :]),
                     reads=[d_pmT], writes=[d_qmb[b]])
        units = []
        for jb in range(nblk):
            ncommon = 32 + 2 * jb
            for u in range(ncommon // 2):
                units.append((jb, [2 * u, 2 * u + 1], False, u == 0))
            units.append((jb, [32 + 2 * jb, 32 + 2 * jb + 1], True, False))

        def emit_S(u, s, b=b):
            jb, kts, diag, first = u
            for j, kt in enumerate(kts):
                if diag and j == 1:
                    q0, nq, c0 = jb * 256 + 128, 128, 256
                else:
                    q0, nq, c0 = jb * 256, 256, j * 256
                P.op("pe", lambda e, s=s, kt=kt, q0=q0, nq=nq, c0=c0: e.matmul(psS[s][:, c0:c0 + nq], lhsT=kaug[b][0:96, kt * 128:(kt + 1) * 128],
                                                                             rhs=qaug[b][0:96, q0:q0 + nq], start=True, stop=True),
                     reads=[d_kaug[b], d_qaug[b], d_qmb[b]], writes=[d_psS[s]])

        def emit_rest(u, s, b=b, h=h):
            jb, kts, diag, first = u
            ncols = 384 if diag else 512
            P.op("act", lambda e, s=s, ncols=ncols: e.activation(out=pt[s][:, 0:ncols], in_=psS[s][:, 0:ncols], func=AF.Exp),
                 reads=[d_psS[s]], writes=[d_pt[s]])
            if diag:
                P.op("pool", lambda e, s=s: e.tensor_tensor(out=pt[s][:, 0:128], in0=pt[s][:, 0:128], in1=tri[:], op=ALU.mult),
                     reads=[d_pt[s], d_c], writes=[d_pt[s]])
                P.op("pool", lambda e, s=s: e.tensor_tensor(out=pt[s][:, 256:384], in0=pt[s][:, 256:384], in1=tri[:], op=ALU.mult),
                     reads=[d_pt[s], d_c], writes=[d_pt[s]])
            for j, kt in enumerate(kts):
                if diag and j == 1:
                    pv = [(1, 256, True)]
                elif diag:
                    pv = [(0, 0, True), (1, 128, False)]
                else:
                    pv = [(0, j * 256, False), (1, j * 256 + 128, False)]
                st_ = first and j == 0
                for qi, col0, last in pv:
                    P.op("pe", lambda e, s=s, kt=kt, qi=qi, col0=col0, st_=st_, last=last: e.matmul(
                        po[qi][:, :], lhsT=pt[s][:, col0:col0 + 128], rhs=vaug[b][:, kt, :], start=st_, stop=last),
                        reads=[d_pt[s], d_vaug[b]], writes=[d_po[qi]])
            if diag:
                for qi in range(2):
                    qt = jb * 2 + qi
                    P.op("dve", lambda e, qi=qi: e.reciprocal(out=rc[:, qi:qi + 1], in_=po[qi][:, 64:65]), reads=[d_po[qi]], writes=[d_rc[qi]])
                    P.op("dve", lambda e, qi=qi, qt=qt: e.tensor_scalar(out=oa[:, qt, h * 64:(h + 1) * 64], in0=po[qi][:, 0:64],
                                                                       scalar1=rc[:, qi:qi + 1], scalar2=None, op0=ALU.mult),
                         reads=[d_po[qi], d_rc[qi]], writes=[d_oa])
        SK = 2
        n = len(units)
        for i in range(n + SK):
            if i < n:
                emit_S(units[i], i % NPT)
            if i >= SK:
                emit_rest(units[i - SK], (i - SK) % NPT)


def host_consts(half):
    pv = 1.0 if half == 1 else 0.0
    V = np.zeros((32, 32), np.float32); O = np.zeros((32, 32), np.float32)
    for qt in range(32):
        jb = qt // 2
        V[qt, :16] = pv
        V[qt, 16:16 + jb] = 1.0
        O[qt, 16 + jb] = 1.0
    NBm = (V - 1.0) * BIGG
    E = np.zeros((32, NK), np.float32)
    for b in range(32):
        E[b, b * 256:(b + 1) * 256] = 1.0
    tri = (np.arange(128)[:, None] <= np.arange(128)[None, :]).astype(np.float32)
    return dict(vq=V.reshape(1, 1024), nb=NBm.reshape(1, 1024), oq=O.reshape(1, 1024),
                e_d=E.astype(ml_dtypes.bfloat16), tri=tri.astype(ml_dtypes.bfloat16))


def phase_c(nc, P, st, xin, w_in, g_mix, cwl_d, cbl_d, dtb_d, alog_d, dsk_d, ssdn_d, tri2_d, sel_d, tb_d, pflag_d,
            idb, d_idb, os_d, d_osd, chunks=range(32)):
    def sb(name, shape, dt):
        return st.enter_context(nc.sbuf_tensor("c_" + name, shape, dt))

    def ps(name, shape, dt=F32):
        return st.enter_context(nc.psum_tensor(name, shape, dt))
    wz = sb("wz", [128, 8, 1024], BF16); d_w = Dep()
    wx = sb("wx", [128, 8, 2048], BF16)
    wdt = sb("wdt", [128, 8, 16], BF16)
    gm = sb("gmc", [128, D], F32); d_c = Dep()
    cw = sb("cw", [128, 16, 4], F32)
    cb = sb("cb", [128, 16], F32)
    dtb = sb("dtb", [128, 2, 16], F32)
    Aneg = sb("Aneg", [128, 2, 16], F32); d_A = Dep()
    dsk = sb("dsk", [128, 16], F32)
    ssdn = sb("ssdn", [128, D], F32)
    tri2 = sb("tri2", [128, 2, 256], F32)
    onesf = sb("onesf", [128, 128], F32)
    sel = sb("selc", [16, 16, 128], F32)
    tb = sb("tbc", [128, 384], F32)
    pflag = sb("pflag", [128, 1], F32)
    xt_ = [sb("xtc%d" % i, [128, 2, D], F32) for i in range(2)]; d_xt_ = [Dep(), Dep()]
    junk = sb("junkc", [128, D], BF16); d_junk = Dep()
    ssq = sb("ssqc", [128, 2], F32); d_ssq = Dep()
    rstd = sb("rstdc", [128, 2], F32); d_rstd = Dep()
    hb_ = [sb("hbc%d" % i, [128, 2, D], BF16) for i in range(2)]; d_hb_ = [Dep(), Dep()]
    hT = sb("hTc", [128, 8, 256], BF16); d_hT = Dep()
    xraw = sb("xraw", [128, 16, 259], F32); d_xraw = Dep(); d_halo = Dep()
    acc = sb("acc", [128, 8, 256], F32); d_acc = [Dep() for _ in range(8)]
    xc = sb("xc", [128, 16, 256], BF16); d_xc = [Dep() for _ in range(16)]
    xtok = sb("xtok", [128, 2, 1024], BF16); d_xtok = Dep()
    btok = sb("btok", [128, 2, 512], BF16); d_btok = Dep()
    dtr = sb("dtr", [128, 2, 16], F32); d_dtr = Dep()
    dt = sb("dt", [128, 2, 16], F32); d_dt = Dep()
    aa = sb("aa", [128, 2, 16], F32); d_aa = Dep()
    acum = sb("acum", [128, 2, 16], F32); d_acum = Dep()
    nacum = sb("nacum", [128, 2, 16], F32); d_nacum = Dep()
    acumT = sb("acumT", [16, 256], F32); d_acumT = Dep()
    tot = sb("tot", [128, 16], F32); d_tot = Dep()
    cdec = sb("cdec", [128, 16], F32); d_cdec = Dep()
    dte = sb("dte", [128, 2, 16], F32); d_dte = Dep()
    eac = sb("eac", [128, 2, 16], F32); d_eac = Dep()
    w2 = sb("w2", [128, 2, 16], F32); d_w2 = Dep()
    xdt = sb("xdt", [128, 2, 1024], BF16); d_xdt = Dep()
    xdd = sb("xdd", [128, 2, 1024], BF16); d_xdd = Dep()
    zs = sb("zs", [128, 2, 1024], BF16); d_zs = Dep()
    cbT = sb("cbT", [128, 4, 384], F32); d_cbT = [Dep() for _ in range(4)]
    arg = [sb("arg%d" % i, [128, 384], F32) for i in range(2)]; d_arg = [Dep(), Dep()]
    Lt = [sb("Lt%d" % i, [128, 384], F32) for i in range(2)]; d_Lt = [Dep(), Dep()]
    Mt = [sb("Mt%d" % i, [128, 384], BF16) for i in range(2)]; d_Mt = [Dep() for _ in range(2)]
    stf = sb("stf", [128, 4, 256], F32); d_stf = Dep()
    stT = sb("stT", [128, 4, 256], BF16); d_stT = Dep()
    yt = [sb("yt%d" % i, [128, 256], F32) for i in range(2)]; d_yt = [Dep(), Dep()]
    y2 = [sb("y2%d" % i, [128, 256], F32) for i in range(2)]; d_y2 = [Dep(), Dep()]
    gss = [sb("gss%d" % i, [128, 1], F32) for i in range(2)]; d_gss = [Dep(), Dep()]
    grs = [sb("grs%d" % i, [128, 1], F32) for i in range(2)]; d_grs = [Dep(), Dep()]
    osb = sb("osb", [128, 2, 1024], BF16); d_osb = Dep()

    B = [ps("bk%d" % i, [128, 512], F32) for i in range(7)]
    d_B = [Dep() for _ in range(7)]
    d_B1h = [Dep(), Dep()]
    d_B5h = [Dep(), Dep()]
    pT = ps("pTc", [128, 8, 128], BF16); d_pT = Dep()

    for k in range(8):
        rows = slice(k * 128, (k + 1) * 128)
        P.dma("pool", lambda e, k=k, rows=rows: e.dma_start(out=wz[:, k, :], in_=w_in[rows, C_Z:C_Z + 1024]), writes=[d_w])
        P.dma("pool", lambda e, k=k, rows=rows: e.dma_start(out=wx[:, k, :], in_=w_in[rows, C_X:C_X + 2048]), writes=[d_w])
        P.dma("pool", lambda e, k=k, rows=rows: e.dma_start(out=wdt[:, k, :], in_=w_in[rows, C_DT:C_DT + 16]), writes=[d_w])
    P.dma("sp", lambda e: e.dma_start(out=gm[:], in_=g_mix[0:1, :].partition_broadcast(128)), writes=[d_c])
    P.dma("sp", lambda e: e.dma_start(out=cw[:], in_=cwl_d[:, :, :]), writes=[d_c])
    P.dma("sp", lambda e: e.dma_start(out=cb[:], in_=cbl_d[:, :]), writes=[d_c])
    for i in range(2):
        P.dma("sp", lambda e, i=i: e.dma_start(out=dtb[:, i, :], in_=dtb_d[0:1, :].partition_broadcast(128)), writes=[d_c])
        P.dma("sp", lambda e, i=i: e.dma_start(out=Aneg[:, i, :], in_=alog_d[0:1, :].partition_broadcast(128)), writes=[d_A])
    P.dma("sp", lambda e: e.dma_start(out=dsk[:], in_=dsk_d[0:1, :].partition_broadcast(128)), writes=[d_c])
    P.dma("sp", lambda e: e.dma_start(out=ssdn[:], in_=ssdn_d[0:1, :].partition_broadcast(128)), writes=[d_c])
    P.dma("sp", lambda e: e.dma_start(out=tri2[:], in_=tri2_d[:, :, :]), writes=[d_c])
    P.dma("sp", lambda e: e.dma_start(out=sel[:], in_=sel_d[:, :, :]), writes=[d_c])
    P.dma("sp", lambda e: e.dma_start(out=tb[:], in_=tb_d[:, :]), writes=[d_c])
    P.dma("sp", lambda e: e.dma_start(out=pflag[:], in_=pflag_d[0:1, :].partition_broadcast(128)), writes=[d_c])
    P.op("pool", lambda e: e.memset(onesf[:], 1.0), writes=[d_c])
    P.op("act", lambda e: e.activation(out=Aneg[:], in_=Aneg[:], func=AF.Exp), reads=[d_A], writes=[d_A])
    P.op("dve", lambda e: e.tensor_scalar(out=Aneg[:], in0=Aneg[:], scalar1=-1.0, scalar2=None, op0=ALU.mult), reads=[d_A], writes=[d_A])
    P.op("pool", lambda e: e.memset(xraw[:], 0.0), writes=[d_xraw, d_halo])
    P.op("pool", lambda e: e.memset(stf[:], 0.0), writes=[d_stf])
    P.op("pool", lambda e: e.memset(stT[:], 0.0), writes=[d_stT])

    h3 = lambda ap: ap.rearrange("p (h d) -> p h d", d=64)

    chunks = list(chunks)

    def s1a(cj, cc):
        pj = cj % 2
        rr = cc * 256
        P.dma("sp", lambda e: e.dma_start(out=xt_[pj][:], in_=xin[rr:rr + 256, :].rearrange("(t p) d -> p t d", p=128)), writes=[d_xt_[pj]])
        for it in range(2):
            P.op("act", lambda e, it=it: e.activation(out=junk[:], in_=xt_[pj][:, it, :], func=AF.Square, accum_out=ssq[:, it:it + 1]),
                 reads=[d_xt_[pj]], writes=[d_junk, d_ssq])
        P.op("act", lambda e: e.activation(out=rstd[:], in_=ssq[:], func=AF.Sqrt, bias=EPS, scale=1.0 / D), reads=[d_ssq], writes=[d_rstd])
        P.op("dve", lambda e: e.reciprocal(out=rstd[:], in_=rstd[:]), reads=[d_rstd], writes=[d_rstd])
        for it in range(2):
            P.op("dve", lambda e, it=it: e.scalar_tensor_tensor(out=hb_[pj][:, it, :], in0=xt_[pj][:, it, :], scalar=rstd[:, it:it + 1], in1=gm[:],
                                                                op0=ALU.mult, op1=ALU.mult),
                 reads=[d_xt_[pj], d_rstd, d_c], writes=[d_hb_[pj]])

    for ci, c in enumerate(chunks):
        own = c >= 16
        r0 = c * 256
        if ci == 0:
            s1a(0, c)
        pp = ci % 2
        for it in range(2):
            for k in range(8):
                P.op("pe", lambda e, it=it, k=k, pp=pp: e.transpose(out=pT[:, k, :], in_=hb_[pp][:, it, k * 128:(k + 1) * 128], identity=idb[:]),
                     reads=[d_hb_[pp], d_idb], writes=[d_pT])
            P.op("act", lambda e, it=it: e.copy(out=hT[:, :, it * 128:(it + 1) * 128], in_=pT[:]), reads=[d_pT], writes=[d_hT])
        for it in range(2):
            for k in range(8):
                P.op("pe", lambda e, it=it, k=k: e.matmul(B[3][:, it * 16:(it + 1) * 16], lhsT=hT[:, k, it * 128:(it + 1) * 128], rhs=wdt[:, k, :],
                                                         start=(k == 0), stop=(k == 7)), reads=[d_hT, d_w], writes=[d_B[3]])
        P.op("dve", lambda e: e.tensor_tensor(out=dtr[:].rearrange("p a b -> p (a b)"), in0=B[3][:, 0:32], in1=dtb[:].rearrange("p a b -> p (a b)"), op=ALU.add),
             reads=[d_B[3], d_c], writes=[d_dtr])
        P.op("act", lambda e: e.activation(out=dtr[:], in_=dtr[:], func=AF.Exp), reads=[d_dtr], writes=[d_dtr])
        P.op("act", lambda e: e.activation(out=dt[:], in_=dtr[:], func=AF.Ln, bias=1.0), reads=[d_dtr], writes=[d_dt])
        P.op("dve", lambda e: e.tensor_tensor(out=aa[:], in0=dt[:], in1=Aneg[:], op=ALU.mult), reads=[d_dt, d_A], writes=[d_aa])
        nfc = 16 if (own or c == 15) else 12
        for fc in range(nfc):
            bi = 1 if fc % 2 == 0 else 4
            for k in range(8):
                P.op("pe", lambda e, fc=fc, k=k, bi=bi: e.matmul(B[bi][:, 0:256], lhsT=wx[:, k, fc * 128:(fc + 1) * 128], rhs=hT[:, k, :],
                                                               start=(k == 0), stop=(k == 7)), reads=[d_hT, d_w], writes=[d_B[bi]])
            P.op("act", lambda e, fc=fc, bi=bi: e.copy(out=xraw[:, fc, 3:259], in_=B[bi][:, 0:256]),
                 reads=[d_B[bi]], writes=[d_xraw])
        for it in range(2):
            for jt in range(it + 1):
                P.op("pe", lambda e, it=it, jt=jt: e.matmul(B[3][:, 32 + it * 16:32 + (it + 1) * 16], lhsT=tri2[:, jt, it * 128:(it + 1) * 128],
                                                           rhs=aa[:, jt, :], start=(jt == 0), stop=(jt == it)),
                     reads=[d_aa, d_c], writes=[d_B[3]])
        for jt in range(2):
            P.op("pe", lambda e, jt=jt: e.matmul(B[3][:, 64:80], lhsT=onesf[:], rhs=aa[:, jt, :], start=(jt == 0), stop=(jt == 1)),
                 reads=[d_aa, d_c], writes=[d_B[3]])
        for jt in range(2):
            P.op("pe", lambda e, jt=jt: e.matmul(B[3][0:16, 128:384], lhsT=aa[:, jt, :], rhs=tri2[:, jt, :], start=(jt == 0), stop=(jt == 1)),
                 reads=[d_aa, d_c], writes=[d_B[3]])
        P.op("act", lambda e: e.copy(out=acum[:].rearrange("p a b -> p (a b)"), in_=B[3][:, 32:64]), reads=[d_B[3]], writes=[d_acum])
        P.op("dve", lambda e: e.tensor_scalar(out=nacum[:].rearrange("p a b -> p (a b)"), in0=B[3][:, 32:64], scalar1=-1.0, scalar2=None, op0=ALU.mult),
             reads=[d_B[3]], writes=[d_nacum])
        P.op("act", lambda e: e.copy(out=tot[:], in_=B[3][:, 64:80]), reads=[d_B[3]], writes=[d_tot])
        P.op("act", lambda e: e.copy(out=acumT[:], in_=B[3][0:16, 128:384]), reads=[d_B[3]], writes=[d_acumT])
        P.op("act", lambda e: e.activation(out=cdec[:], in_=tot[:], func=AF.Exp), reads=[d_tot], writes=[d_cdec])
        P.op("dve", lambda e: e.tensor_tensor(out=dte[:], in0=nacum[:], in1=tot[:].unsqueeze(1).to_broadcast([128, 2, 16]), op=ALU.add),
             reads=[d_nacum, d_tot], writes=[d_dte])
        P.op("act", lambda e: e.activation(out=dte[:], in_=dte[:], func=AF.Exp), reads=[d_dte], writes=[d_dte])
        P.op("act", lambda e: e.activation(out=eac[:], in_=acum[:], func=AF.Exp), reads=[d_acum], writes=[d_eac])
        P.op("dve", lambda e: e.tensor_tensor(out=w2[:], in0=dt[:], in1=dte[:], op=ALU.mult), reads=[d_dt, d_dte], writes=[d_w2])
        if own:
            for it in range(2):
                for hv in range(2):
                    zb = 2 if hv == 0 else 6
                    for k in range(8):
                        P.op("pe", lambda e, it=it, hv=hv, k=k, zb=zb: e.matmul(B[zb][:, :], lhsT=hT[:, k, it * 128:(it + 1) * 128], rhs=wz[:, k, hv * 512:(hv + 1) * 512],
                                                                              start=(k == 0), stop=(k == 7)), reads=[d_hT, d_w], writes=[d_B[zb]])
                    P.op("act", lambda e, it=it, hv=hv, zb=zb: e.activation(out=zs[:, it, hv * 512:(hv + 1) * 512], in_=B[zb][:, :], func=AF.Silu),
                         reads=[d_B[zb]], writes=[d_zs])
        for half in range(2):
            n8 = 8 if (half == 0 or nfc == 16) else 4
            for f8 in range(n8):
                fc = half * 8 + f8
                P.op("dve", lambda e, fc=fc, f8=f8: e.tensor_scalar(out=acc[:, f8, :], in0=xraw[:, fc, 0:256], scalar1=cw[:, fc, 0:1], scalar2=cb[:, fc:fc + 1],
                                                                   op0=ALU.mult, op1=ALU.add), reads=[d_xraw, d_halo, d_c], writes=[d_acc[f8]])
            for kk in range(1, 4):
                for f8 in range(n8):
                    fc = half * 8 + f8
                    P.op("dve", lambda e, fc=fc, f8=f8, kk=kk: e.scalar_tensor_tensor(out=acc[:, f8, :], in0=xraw[:, fc, kk:kk + 256], scalar=cw[:, fc, kk:kk + 1],
                                                                                     in1=acc[:, f8, :], op0=ALU.mult, op1=ALU.add),
                         reads=[d_xraw, d_halo, d_c, d_acc[f8]], writes=[d_acc[f8]])
            for f8 in range(n8):
                fc = half * 8 + f8
                P.op("act", lambda e, fc=fc, f8=f8: e.activation(out=xc[:, fc, :], in_=acc[:, f8, :], func=AF.Silu),
                     reads=[d_acc[f8]], writes=[d_xc[fc]])
        P.op("pool", lambda e: e.tensor_copy(out=xraw[:, :, 0:3], in_=xraw[:, :, 256:259]), reads=[d_xraw], writes=[d_halo])
        for it in range(2):
            for fc in range(8):
                P.op("pe", lambda e, it=it, fc=fc: e.transpose(out=pT[:, fc, :], in_=xc[:, fc, it * 128:(it + 1) * 128], identity=idb[:]),
                     reads=[d_xc[fc], d_idb], writes=[d_pT])
            P.op("act", lambda e, it=it: e.copy(out=xtok[:, it, :], in_=pT[:]), reads=[d_pT], writes=[d_xtok])
            for g in range(4):
                P.op("pe", lambda e, it=it, g=g: e.transpose(out=pT[:, g, :], in_=xc[:, 8 + g, it * 128:(it + 1) * 128], identity=idb[:]),
                     reads=[d_xc[8 + g], d_idb], writes=[d_pT])
            P.op("act", lambda e, it=it: e.copy(out=btok[:, it, :], in_=pT[:, 0:4, :]), reads=[d_pT], writes=[d_btok])
        for it in range(2):
            if own:
                P.op("dve", lambda e, it=it: e.tensor_tensor(out=h3(xdt[:, it, :]), in0=h3(xtok[:, it, :]),
                                                             in1=dt[:, it, :].unsqueeze(2).to_broadcast([128, 16, 64]), op=ALU.mult),
                     reads=[d_xtok, d_dt], writes=[d_xdt])
            P.op("pool", lambda e, it=it: e.tensor_tensor(out=h3(xdd[:, it, :]), in0=h3(xtok[:, it, :]),
                                                          in1=w2[:, it, :].unsqueeze(2).to_broadcast([128, 16, 64]), op=ALU.mult),
                 reads=[d_xtok, d_w2], writes=[d_xdd])
        if ci + 1 < len(chunks):
            s1a(ci + 1, chunks[ci + 1])
        if own:
            for g in range(4):
                P.op("pe", lambda e, g=g: e.matmul(B[4][:, 0:256], lhsT=xc[:, 8 + g, 0:128], rhs=xc[:, 12 + g, :], start=True, stop=True),
                     reads=[d_xc[8 + g], d_xc[12 + g]], writes=[d_B[4]])
                P.op("pe", lambda e, g=g: e.matmul(B[4][:, 256:384], lhsT=xc[:, 8 + g, 128:256], rhs=xc[:, 12 + g, 128:256], start=True, stop=True),
                     reads=[d_xc[8 + g], d_xc[12 + g]], writes=[d_B[4]])
                P.op("act", lambda e, g=g: e.copy(out=cbT[:, g, :], in_=B[4][:, 0:384]), reads=[d_B[4]], writes=[d_cbT[g]])
            for g in range(4):
                def stA(h, g=g):
                    mi = h % 2
                    bk = 5 if mi == 0 else 4
                    P.op("pe", lambda e: e.matmul(B[bk][:, 0:256], lhsT=sel[:, h, :], rhs=acumT[:, :], start=True, stop=True),
                         reads=[d_c, d_acumT], writes=[d_B[bk]])
                    P.op("dve", lambda e: e.scalar_tensor_tensor(out=arg[mi][:, 0:256], in0=B[bk][:, 0:256], scalar=nacum[:, 0, h:h + 1], in1=tb[:, 0:256],
                                                                 op0=ALU.add, op1=ALU.add), reads=[d_B[bk], d_nacum, d_c], writes=[d_arg[mi]])
                    P.op("dve", lambda e: e.scalar_tensor_tensor(out=arg[mi][:, 256:384], in0=B[bk][:, 128:256], scalar=nacum[:, 1, h:h + 1], in1=tb[:, 256:384],
                                                                 op0=ALU.add, op1=ALU.add), reads=[d_B[bk], d_nacum, d_c], writes=[d_arg[mi]])
                    P.op("act", lambda e: e.activation(out=Lt[mi][:], in_=arg[mi][:], func=AF.Exp), reads=[d_arg[mi]], writes=[d_Lt[mi]])
                    P.op("pool", lambda e: e.tensor_tensor(out=Mt[mi][:], in0=Lt[mi][:], in1=cbT[:, g, :], op=ALU.mult),
                         reads=[d_Lt[mi], d_cbT[g]], writes=[d_Mt[mi]])

                def stB(h, g=g):
                    mi = h % 2
                    r = h % 4
                    cs_ = slice(r * 64, (r + 1) * 64)
                    hs_ = slice(h * 64, (h + 1) * 64)
                    P.op("pe", lambda e: e.matmul(B[6][:, 0:256][:, cs_], lhsT=Mt[mi][:, 0:128], rhs=xdt[:, 0, hs_], start=True, stop=True),
                         reads=[d_Mt[mi], d_xdt], writes=[d_B[6]])
                    P.op("pe", lambda e: e.matmul(B[6][:, 256:512][:, cs_], lhsT=Mt[mi][:, 128:256], rhs=xdt[:, 0, hs_], start=True, stop=False),
                         reads=[d_Mt[mi], d_xdt], writes=[d_B[6]])
                    P.op("pe", lambda e: e.matmul(B[6][:, 256:512][:, cs_], lhsT=Mt[mi][:, 256:384], rhs=xdt[:, 1, hs_], start=False, stop=True),
                         reads=[d_Mt[mi], d_xdt], writes=[d_B[6]])
                hs4 = [g * 4 + r for r in range(4)]
                stA(hs4[0]); stA(hs4[1]); stB(hs4[0]); stA(hs4[2]); stB(hs4[1]); stA(hs4[3]); stB(hs4[2]); stB(hs4[3])
                gs_ = slice(g * 256, (g + 1) * 256)
                for it in range(2):
                    P.op("pe", lambda e, g=g, it=it: e.matmul(B[0][:, it * 256:(it + 1) * 256], lhsT=xc[:, 12 + g, it * 128:(it + 1) * 128], rhs=stT[:, g, :],
                                                             start=True, stop=True), reads=[d_xc[12 + g], d_stT], writes=[d_B[0]])
                v4 = lambda ap: ap.rearrange("p (r d) -> p r d", d=64)

                def ystep(step, it, g=g, gs_=gs_):
                    if step == 0:
                        P.op("dve", lambda e: e.tensor_tensor(out=v4(yt[it][:]), in0=v4(B[0][:, it * 256:(it + 1) * 256]),
                                                              in1=eac[:, it, g * 4:(g + 1) * 4].unsqueeze(2).to_broadcast([128, 4, 64]), op=ALU.mult),
                             reads=[d_B[0], d_eac], writes=[d_yt[it]])
                    elif step == 1:
                        P.op("dve", lambda e: e.tensor_tensor(out=yt[it][:], in0=yt[it][:], in1=B[6][:, it * 256:(it + 1) * 256], op=ALU.add),
                             reads=[d_yt[it], d_B[6]], writes=[d_yt[it]])
                    elif step == 2:
                        P.op("pool", lambda e: e.tensor_tensor(out=v4(y2[it][:]), in0=v4(xtok[:, it, gs_]),
                                                               in1=dsk[:, g * 4:(g + 1) * 4].unsqueeze(2).to_broadcast([128, 4, 64]), op=ALU.mult),
                             reads=[d_xtok, d_c], writes=[d_y2[it]])
                    elif step == 3:
                        P.op("dve", lambda e: e.tensor_tensor(out=yt[it][:], in0=yt[it][:], in1=y2[it][:], op=ALU.add), reads=[d_yt[it], d_y2[it]], writes=[d_yt[it]])
                    elif step == 4:
                        P.op("dve", lambda e: e.tensor_tensor(out=yt[it][:], in0=yt[it][:], in1=zs[:, it, gs_], op=ALU.mult), reads=[d_yt[it], d_zs], writes=[d_yt[it]])
                    elif step == 5:
                        P.op("act", lambda e: e.activation(out=y2[it][:], in_=yt[it][:], func=AF.Square, accum_out=gss[it][:]), reads=[d_yt[it], d_y2[it]], writes=[d_y2[it], d_gss[it]])
                    elif step == 6:
                        P.op("act", lambda e: e.activation(out=grs[it][:], in_=gss[it][:], func=AF.Sqrt, bias=EPS, scale=1.0 / 256), reads=[d_gss[it]], writes=[d_grs[it]])
                    elif step == 7:
                        P.op("dve", lambda e: e.reciprocal(out=grs[it][:], in_=grs[it][:]), reads=[d_grs[it]], writes=[d_grs[it]])
                    elif step == 8:
                        P.op("dve", lambda e: e.scalar_tensor_tensor(out=osb[:, it, gs_], in0=yt[it][:], scalar=grs[it][:, 0:1], in1=ssdn[:, gs_],
                                                                     op0=ALU.mult, op1=ALU.mult), reads=[d_yt[it], d_grs[it], d_c], writes=[d_osb])
                for step in range(9):
                    for it in range(2):
                        ystep(step, it)
            P.dma("sp", lambda e, r0=r0: e.dma_start(out=os_d[r0 - TP:r0 - TP + 256, :].rearrange("(t p) c -> p t c", p=128), in_=osb[:]),
                  reads=[d_osb], writes=[d_osd])
        for g in range(4):
            bank = B[2] if g < 2 else B[1]
            dbank = d_B[2] if g < 2 else d_B[1]
            cs_ = slice((g % 2) * 256, (g % 2 + 1) * 256)
            for jt in range(2):
                P.op("pe", lambda e, g=g, jt=jt, bank=bank, cs_=cs_: e.matmul(bank[:, cs_], lhsT=btok[:, jt, g * 128:(g + 1) * 128], rhs=xdd[:, jt, g * 256:(g + 1) * 256],
                                                                            start=(jt == 0), stop=(jt == 1)), reads=[d_btok, d_xdd], writes=[dbank])
        v16 = lambda ap: ap.rearrange("p (h d) -> p h d", d=64)
        P.op("dve", lambda e: e.tensor_tensor(out=v16(stf[:].rearrange("p g c -> p (g c)")), in0=v16(stf[:].rearrange("p g c -> p (g c)")),
                                              in1=cdec[:].unsqueeze(2).to_broadcast([128, 16, 64]), op=ALU.mult), reads=[d_stf, d_cdec], writes=[d_stf])
        P.op("dve", lambda e: e.tensor_tensor(out=stf[:, 0:2, :].rearrange("p g c -> p (g c)"), in0=stf[:, 0:2, :].rearrange("p g c -> p (g c)"), in1=B[2][:, :], op=ALU.add),
             reads=[d_stf, d_B[2]], writes=[d_stf])
        P.op("dve", lambda e: e.tensor_tensor(out=stf[:, 2:4, :].rearrange("p g c -> p (g c)"), in0=stf[:, 2:4, :].rearrange("p g c -> p (g c)"), in1=B[1][:, :], op=ALU.add),
             reads=[d_stf, d_B[1]], writes=[d_stf])
        if c == 15:
            P.op("dve", lambda e: e.tensor_scalar(out=stf[:], in0=stf[:], scalar1=pflag[:, 0:1], scalar2=None, op0=ALU.mult), reads=[d_stf, d_c], writes=[d_stf])
        P.op("act", lambda e: e.copy(out=stT[:], in_=stf[:]), reads=[d_stf], writes=[d_stT])


def host_consts_c(z, half):
    cwl = np.ascontiguousarray(z['conv_w'].reshape(4, 16, 128).transpose(2, 1, 0)).astype(np.float32)
    cbl = np.ascontiguousarray(z['conv_b'].reshape(16, 128).T).astype(np.float32)
    tri = (np.arange(128)[:, None] <= np.arange(128)[None, :]).astype(np.float32)
    tri2 = np.zeros((128, 2, 256), np.float32)
    tri2[:, 0, 0:128] = tri; tri2[:, 0, 128:256] = 1.0; tri2[:, 1, 128:256] = tri
    sel = np.zeros((16, 16, 128), np.float32)
    for h in range(16):
        sel[h, h, :] = 1.0
    tbias = np.where(tri > 0, 0.0, NEG).astype(np.float32)
    tb = np.zeros((128, 384), np.float32)
    tb[:, 0:128] = tbias; tb[:, 256:384] = tbias
    return dict(cwl=cwl, cbl=cbl, dtb=z['dt_bias'][None, :], alog=z['a_log'][None, :], dsk=z['d_skip'][None, :], ssdn=z['ssd_norm'][None, :],
                tri2=tri2, sel_d=sel, tb_d=tb, pflag=np.array([[1.0 if half == 1 else 0.0]], np.float32))


def phase_def(nc, P, xown, w_in, g_mix, mem_d, gmem_d, wmemkv, mqn_d, mkn_d, woa_d, wos_d, wom_d, wout_d, gffn_d, wr_d,
              wgate_d, wup_d, wdown_d, tri_d, ecap_d, oa_d, d_oad, os_d, d_osd, x1_d, xbuf, ybuf, out_d, idb, d_idb, idf, d_idf,
              ntiles=32, experts=range(32), stage=3):
    d_x1d = Dep(); d_xbuf = Dep(); d_ybuf = Dep(); d_out = Dep()
    with contextlib.ExitStack() as stp:
        def sbp(name, shape, dt):
            return stp.enter_context(nc.sbuf_tensor("d_" + name, shape, dt))
        combs = sbp("combs", [128, 32, 2], F32); d_combs = Dep()
        idxs = sbp("idxs", [128, 32, 2], I32); d_idxs = Dep()

        with contextlib.ExitStack() as st:
            def sb(name, shape, dt):
                return st.enter_context(nc.sbuf_tensor("d_" + name, shape, dt))

            def ps(name, shape, dt=F32):
                return st.enter_context(nc.psum_tensor(name, shape, dt))
            d_w = Dep(); d_c = Dep()
            gm = sb("gmd", [128, D], F32)
            gf = sb("gfd", [128, D], F32)
            mqn = sb("mqn", [128, 512], F32)
            mkn = sb("mkn", [128, 512], F32)
            tri = sb("trid", [128, 128], F32)
            onesf = sb("onesfd", [128, 128], F32)
            onesb = sb("onesbd", [128, 128], BF16)
            ecap = sb("ecap", [128, 32], F32)
            base = sb("base", [128, 32], F32); d_base = Dep()
            kmT = sb("kmT", [128, 4, 256], BF16); d_kmT = Dep()
            vm = sb("vm", [128, 2, 512], BF16); d_vm = Dep()

            xt = [sb("xtd%d" % i, [128, D], F32) for i in range(2)]; d_xt = [Dep(), Dep()]
            junk = sb("junkd", [128, D], BF16); d_junk = Dep()
            ssq = sb("ssqd", [128, 1], F32); d_ssq = Dep()
            rstd = sb("rstdd", [128, 1], F32); d_rstd = Dep()
            hb_ = [sb("hbd%d" % i, [128, D], BF16) for i in range(2)]; d_hb_ = [Dep(), Dep()]
            ssqn = sb("ssqn", [128, 1], F32); d_ssqn = Dep()
            rstdn = sb("rstdn", [128, 1], F32); d_rstdn = Dep()
            hT = sb("hTd", [128, 8, 128], BF16); d_hT = Dep()
            sq = sb("sqd", [128, 512], F32); d_sq = Dep()
            hs = sb("hsd", [128, 4], F32); d_hs = Dep()
            hr = sb("hrd", [128, 4], F32); d_hr = Dep()
            qmn = sb("qmn", [128, 512], F32); d_qmn = Dep()
            qmb = sb("qmb", [128, 512], BF16); d_qmb = Dep()
            qmT = sb("qmT", [128, 4, 128], BF16); d_qmT = Dep()
            pm = sb("pm", [128, 4, 2, 128], BF16); d_pm = Dep()
            rcp = sb("rcp", [128, 512], F32); d_rcp = Dep()
            omT = sb("omT", [128, 4, 128], BF16); d_omT = Dep()
            gs = sb("gs", [128, 3072], F32); d_gs = Dep()
            oat = [sb("oat%d" % i, [128, 512], BF16) for i in range(2)]; d_oat = [Dep(), Dep()]
            ost = [sb("ost%d" % i, [128, 1024], BF16) for i in range(2)]; d_ost = [Dep(), Dep()]
            oaT = sb("oaT", [128, 4, 128], BF16); d_oaT = Dep()
            osT = sb("osT", [128, 8, 128], BF16); d_osT = Dep()
            mg = sb("mg", [128, 512], F32); d_mg = Dep()
            tt = sb("tt", [128, 512], F32); d_tt = Dep()
            mgb = sb("mgb", [128, D], BF16); d_mgb = Dep()
            mgT = sb("mgT", [128, 8, 128], BF16); d_mgT = Dep()
            x1 = sb("x1", [128, D], F32); d_x1 = Dep()
            h2f = sb("h2f", [128, D], F32); d_h2f = Dep()
            h2b = [sb("h2b%d" % i, [128, D], BF16) for i in range(2)]; d_h2b = [Dep() for _ in range(2)]
            h2T = sb("h2T", [128, 8, 128], F32); d_h2T = Dep()
            lg = sb("lg", [128, 36], F32); d_lg = Dep()
            sm = sb("sm", [128, 64], F32); d_sm = Dep()
            goh = sb("goh", [128, 4], F32); d_goh = Dep()
            lsel = sb("lsel", [128, 32], F32); d_lsel = Dep()
            les = sb("les", [128, 8], F32); d_les = Dep()
            t8 = sb("t8", [128, 8], F32); d_t8 = Dep()
            oh = sb("oh", [128, 64], F32); d_oh = Dep()
            ohs = sb("ohs", [128, 32], F32); d_ohs = Dep()
            posf = sb("posf", [128, 32], F32); d_posf = Dep()
            idf2 = sb("idf2", [128, 2], F32); d_idf2 = Dep()

            pT = ps("pTd", [128, 8, 128], BF16); d_pT = Dep()
            pF = ps("pFd", [128, 512], F32); d_pF = Dep()
            pG = [ps("pGd%d" % i, [128, 512], F32) for i in range(3)]; d_pG = [Dep() for _ in range(3)]
            pms = [ps("pmsd%d" % i, [128, 512], F32) for i in range(2)]; d_pms = [Dep() for _ in range(2)]
            pmo = ps("pmod", [128, 512], F32); d_pmo = Dep()

            P.dma("sp", lambda e: e.dma_start(out=gm[:], in_=g_mix[0:1, :].partition_broadcast(128)), writes=[d_c])
            P.dma("sp", lambda e: e.dma_start(out=gf[:], in_=gffn_d[0:1, :].partition_broadcast(128)), writes=[d_c])
            P.dma("sp", lambda e: e.dma_start(out=mqn[:], in_=mqn_d[0:1, :].partition_broadcast(128)), writes=[d_c])
            P.dma("sp", lambda e: e.dma_start(out=mkn[:], in_=mkn_d[0:1, :].partition_broadcast(128)), writes=[d_c])
            P.dma("sp", lambda e: e.dma_start(out=tri[:], in_=tri_d[:, :]), writes=[d_c])
            P.dma("sp", lambda e: e.dma_start(out=ecap[:], in_=ecap_d[0:1, :].partition_broadcast(128)), writes=[d_c])
            P.op("pool", lambda e: e.memset(onesf[:], 1.0), writes=[d_c])
            P.op("pool", lambda e: e.memset(onesb[:], 1.0), writes=[d_c])
            P.op("pool", lambda e: e.memset(base[:], 0.0), writes=[d_base])

            def head_norm(src_ps, d_src, gain, nh, dst, d_dst):
                hd = 512 // nh
                v = lambda ap: ap.rearrange("p (h d) -> p h d", h=nh)
                P.op("act", lambda e: e.activation(out=sq[:], in_=src_ps, func=AF.Square), reads=[d_src], writes=[d_sq])
                P.op("dve", lambda e: e.tensor_reduce(out=hs[:, 0:nh], in_=v(sq[:]), axis=AX.X, op=ALU.add), reads=[d_sq], writes=[d_hs])
                P.op("act", lambda e: e.activation(out=hr[:, 0:nh], in_=hs[:, 0:nh], func=AF.Sqrt, bias=EPS, scale=1.0 / hd), reads=[d_hs], writes=[d_hr])
                P.op("dve", lambda e: e.reciprocal(out=hr[:, 0:nh], in_=hr[:, 0:nh]), reads=[d_hr], writes=[d_hr])
                P.op("dve", lambda e: e.tensor_tensor(out=v(qmn[:]), in0=v(src_ps), in1=hr[:, 0:nh].unsqueeze(2).to_broadcast([128, nh, hd]), op=ALU.mult),
                     reads=[d_src, d_hr], writes=[d_qmn])
                P.op("pool", lambda e: e.tensor_tensor(out=dst, in0=qmn[:], in1=gain[:], op=ALU.mult), reads=[d_qmn, d_c], writes=[d_dst])

            with contextlib.ExitStack() as stm:
                wkv = stm.enter_context(nc.sbuf_tensor("s_wkv", [128, 8, 1024], BF16)); d_wkv = Dep()
                mt_ = stm.enter_context(nc.sbuf_tensor("s_memt", [128, 2, D], F32)); d_mt = Dep()
                gme = stm.enter_context(nc.sbuf_tensor("s_gme", [128, D], F32)); d_gme = Dep()
                mhb = stm.enter_context(nc.sbuf_tensor("s_mhb", [128, 2, D], BF16)); d_mhb = Dep()
                mT = stm.enter_context(nc.sbuf_tensor("s_mT", [128, 8, 256], BF16)); d_mT = Dep()
                ss2 = stm.enter_context(nc.sbuf_tensor("s_ss2", [128, 2], F32)); d_ss2 = Dep()
                for k in range(8):
                    P.dma("pool", lambda e, k=k: e.dma_start(out=wkv[:, k, :], in_=wmemkv[k * 128:(k + 1) * 128, :]), writes=[d_wkv])
                P.dma("sp", lambda e: e.dma_start(out=mt_[:], in_=mem_d[:, :].rearrange("(t p) d -> p t d", p=128)), writes=[d_mt])
                P.dma("sp", lambda e: e.dma_start(out=gme[:], in_=gmem_d[0:1, :].partition_broadcast(128)), writes=[d_gme])
                for it in range(2):
                    P.op("act", lambda e, it=it: e.activation(out=junk[:], in_=mt_[:, it, :], func=AF.Square, accum_out=ss2[:, it:it + 1]),
                         reads=[d_mt], writes=[d_junk, d_ss2])
                P.op("act", lambda e: e.activation(out=ss2[:], in_=ss2[:], func=AF.Sqrt, bias=EPS, scale=1.0 / D), reads=[d_ss2], writes=[d_ss2])
                P.op("dve", lambda e: e.reciprocal(out=ss2[:], in_=ss2[:]), reads=[d_ss2], writes=[d_ss2])
                for it in range(2):
                    P.op("dve", lambda e, it=it: e.scalar_tensor_tensor(out=mhb[:, it, :], in0=mt_[:, it, :], scalar=ss2[:, it:it + 1], in1=gme[:],
                                                                        op0=ALU.mult, op1=ALU.mult), reads=[d_mt, d_ss2, d_gme], writes=[d_mhb])
                    for k in range(8):
                        P.op("pe", lambda e, it=it, k=k: e.transpose(out=pT[:, k, :], in_=mhb[:, it, k * 128:(k + 1) * 128], identity=idb[:]),
                             reads=[d_mhb, d_idb], writes=[d_pT])
                    P.op("act", lambda e, it=it: e.copy(out=mT[:, :, it * 128:(it + 1) * 128], in_=pT[:]), reads=[d_pT], writes=[d_mT])
                for it in range(2):
                    for hv in range(2):
                        for k in range(8):
                            P.op("pe", lambda e, it=it, hv=hv, k=k: e.matmul(pG[hv][:, :], lhsT=mT[:, k, it * 128:(it + 1) * 128], rhs=wkv[:, k, hv * 512:(hv + 1) * 512],
                                                                           start=(k == 0), stop=(k == 7)), reads=[d_mT, d_wkv], writes=[d_pG[hv]])
                    head_norm(pG[0][:, :], d_pG[0], mkn, 4, qmb[:], d_qmb)
                    for h in range(4):
                        P.op("pe", lambda e, h=h: e.transpose(out=pT[:, h, :], in_=qmb[:, h * 128:(h + 1) * 128], identity=idb[:]),
                             reads=[d_qmb, d_idb], writes=[d_pT])
                    P.op("act", lambda e, it=it: e.copy(out=kmT[:, :, it * 128:(it + 1) * 128], in_=pT[:, 0:4, :]), reads=[d_pT], writes=[d_kmT])
                    P.op("act", lambda e, it=it: e.copy(out=vm[:, it, :], in_=pG[1][:, :]), reads=[d_pG[1]], writes=[d_vm])

            wqm = sb("wqm", [128, 8, 512], BF16)
            wg = sb("wg", [128, 8, 3072], BF16)
            woa = sb("woa", [128, 4, 1024], BF16)
            wos = sb("wos", [128, 8, 1024], BF16)
            wom = sb("wom", [128, 4, 1024], BF16)
            wout = sb("wout", [128, 8, 1024], BF16)
            wr = sb("wr", [128, 8, 36], F32)
            for k in range(8):
                rows = slice(k * 128, (k + 1) * 128)
                P.dma("pool", lambda e, k=k, rows=rows: e.dma_start(out=wqm[:, k, :], in_=w_in[rows, C_QM:C_QM + 512]), writes=[d_w])
                for j in range(2):
                    P.dma("pool", lambda e, k=k, rows=rows, j=j: e.dma_start(out=wg[:, k, j * 1536:(j + 1) * 1536], in_=w_in[rows, C_G + j * 1536:C_G + (j + 1) * 1536]), writes=[d_w])
                P.dma("pool", lambda e, k=k, rows=rows: e.dma_start(out=wos[:, k, :], in_=wos_d[rows, :]), writes=[d_w])
                P.dma("pool", lambda e, k=k, rows=rows: e.dma_start(out=wout[:, k, :], in_=wout_d[rows, :]), writes=[d_w])
                P.dma("sp", lambda e, k=k, rows=rows: e.dma_start(out=wr[:, k, :], in_=wr_d[rows, :]), writes=[d_w])
            for k in range(4):
                rows = slice(k * 128, (k + 1) * 128)
                P.dma("pool", lambda e, k=k, rows=rows: e.dma_start(out=woa[:, k, :], in_=woa_d[rows, :]), writes=[d_w])
                P.dma("pool", lambda e, k=k, rows=rows: e.dma_start(out=wom[:, k, :], in_=wom_d[rows, :]), writes=[d_w])
            def norm_d(tj):
                xj = tj % 2
                P.op("act", lambda e: e.activation(out=junk[:], in_=xt[xj][:], func=AF.Square, accum_out=ssqn[:]), reads=[d_xt[xj]], writes=[d_junk, d_ssqn])
                P.op("act", lambda e: e.activation(out=rstdn[:], in_=ssqn[:], func=AF.Sqrt, bias=EPS, scale=1.0 / D), reads=[d_ssqn], writes=[d_rstdn])
                P.op("dve", lambda e: e.reciprocal(out=rstdn[:], in_=rstdn[:]), reads=[d_rstdn], writes=[d_rstdn])
                P.op("dve", lambda e: e.scalar_tensor_tensor(out=hb_[xj][:], in0=xt[xj][:], scalar=rstdn[:, 0:1], in1=gm[:], op0=ALU.mult, op1=ALU.mult),
                     reads=[d_xt[xj], d_rstdn, d_c], writes=[d_hb_[xj]])

            def loads(tj):
                xj = tj % 2
                rj = tj * 128
                P.dma("sp", lambda e: e.dma_start(out=xt[xj][:], in_=xown[rj:rj + 128, :]), writes=[d_xt[xj]])
                P.dma("sp", lambda e: e.dma_start(out=oat[xj][:], in_=oa_d[rj:rj + 128, :]), reads=[d_oad], writes=[d_oat[xj]])
                P.dma("sp", lambda e: e.dma_start(out=ost[xj][:], in_=os_d[rj:rj + 128, :]), reads=[d_osd], writes=[d_ost[xj]])

            for ti in range(ntiles):
                r0 = ti * 128
                hbuf = ti % 2
                xb = ti % 2
                if ti == 0:
                    loads(0)
                if ti + 1 < ntiles:
                    loads(ti + 1)
                if ti == 0:
                    norm_d(0)
                for k in range(8):
                    P.op("pe", lambda e, k=k, xb=xb: e.transpose(out=pT[:, k, :], in_=hb_[xb][:, k * 128:(k + 1) * 128], identity=idb[:]), reads=[d_hb_[xb], d_idb], writes=[d_pT])
                P.op("act", lambda e: e.copy(out=hT[:], in_=pT[:]), reads=[d_pT], writes=[d_hT])
                for k in range(8):
                    P.op("pe", lambda e, k=k: e.matmul(pG[0][:, :], lhsT=hT[:, k, :], rhs=wqm[:, k, :], start=(k == 0), stop=(k == 7)),
                         reads=[d_hT, d_w], writes=[d_pG[0]])
                head_norm(pG[0][:, :], d_pG[0], mqn, 4, qmb[:], d_qmb)
                for g6 in range(6):
                    bk = g6 % 3
                    for k in range(8):
                        P.op("pe", lambda e, g6=g6, k=k, bk=bk: e.matmul(pG[bk][:, :], lhsT=hT[:, k, :], rhs=wg[:, k, g6 * 512:(g6 + 1) * 512], start=(k == 0), stop=(k == 7)),
                             reads=[d_hT, d_w], writes=[d_pG[bk]])
                    P.op("act", lambda e, g6=g6, bk=bk: e.activation(out=gs[:, g6 * 512:(g6 + 1) * 512], in_=pG[bk][:, :], func=AF.Sigmoid),
                         reads=[d_pG[bk]], writes=[d_gs])
                for h in range(4):
                    P.op("pe", lambda e, h=h: e.transpose(out=pT[:, h, :], in_=qmb[:, h * 128:(h + 1) * 128], identity=idb[:]), reads=[d_qmb, d_idb], writes=[d_pT])
                P.op("act", lambda e: e.copy(out=qmT[:], in_=pT[:, 0:4, :]), reads=[d_pT], writes=[d_qmT])
                for h in range(4):
                    for mt in range(2):
                        bank = pms[h // 2]
                        c0 = ((h % 2) * 2 + mt) * 128
                        P.op("pe", lambda e, h=h, mt=mt, bank=bank, c0=c0: e.matmul(bank[:, c0:c0 + 128], lhsT=kmT[:, h, mt * 128:(mt + 1) * 128], rhs=qmT[:, h, :],
                                                                                  start=True, stop=True), reads=[d_kmT, d_qmT], writes=[d_pms[h // 2]])
                for hp in range(2):
                    P.op("act", lambda e, hp=hp: e.activation(out=pm[:, hp * 2:(hp + 1) * 2, :, :].rearrange("p a b c -> p (a b c)"), in_=pms[hp][:, :], func=AF.Exp,
                                                              scale=float(128 ** -0.5)), reads=[d_pms[hp]], writes=[d_pm])
                for h in range(4):
                    for mt in range(2):
                        P.op("pe", lambda e, h=h, mt=mt: e.matmul(pmo[:, h * 128:(h + 1) * 128], lhsT=vm[:, mt, h * 128:(h + 1) * 128], rhs=pm[:, h, mt, :],
                                                                 start=(mt == 0), stop=(mt == 1)), reads=[d_vm, d_pm], writes=[d_pmo])
                for mt in range(2):
                    P.op("pe", lambda e, mt=mt: e.matmul(pG[1][:, :].rearrange("p (h t) -> p h t", h=4), lhsT=onesb[:], rhs=pm[:, :, mt, :],
                                                        start=(mt == 0), stop=(mt == 1)), reads=[d_c, d_pm], writes=[d_pG[1]])
                P.op("dve", lambda e: e.reciprocal(out=rcp[:], in_=pG[1][:, :]), reads=[d_pG[1]], writes=[d_rcp])
                P.op("dve", lambda e: e.tensor_tensor(out=omT[:].rearrange("p h t -> p (h t)"), in0=pmo[:, :], in1=rcp[:], op=ALU.mult),
                     reads=[d_pmo, d_rcp], writes=[d_omT])
                for k in range(4):
                    P.op("pe", lambda e, xb=xb, k=k: e.transpose(out=pT[:, k, :], in_=oat[xb][:, k * 128:(k + 1) * 128], identity=idb[:]), reads=[d_oat[xb], d_idb], writes=[d_pT])
                P.op("act", lambda e: e.copy(out=oaT[:], in_=pT[:, 0:4, :]), reads=[d_pT], writes=[d_oaT])
                for k in range(8):
                    P.op("pe", lambda e, xb=xb, k=k: e.transpose(out=pT[:, k, :], in_=ost[xb][:, k * 128:(k + 1) * 128], identity=idb[:]), reads=[d_ost[xb], d_idb], writes=[d_pT])
                P.op("act", lambda e: e.copy(out=osT[:], in_=pT[:]), reads=[d_pT], writes=[d_osT])
                for hv in range(2):
                    cs_ = slice(hv * 512, (hv + 1) * 512)
                    for k in range(4):
                        P.op("pe", lambda e, k=k, cs_=cs_: e.matmul(pG[0][:, :], lhsT=oaT[:, k, :], rhs=woa[:, k, cs_], start=(k == 0), stop=(k == 3)),
                             reads=[d_oaT, d_w], writes=[d_pG[0]])
                    for k in range(8):
                        P.op("pe", lambda e, k=k, cs_=cs_: e.matmul(pG[1][:, :], lhsT=osT[:, k, :], rhs=wos[:, k, cs_], start=(k == 0), stop=(k == 7)),
                             reads=[d_osT, d_w], writes=[d_pG[1]])
                    for k in range(4):
                        P.op("pe", lambda e, k=k, cs_=cs_: e.matmul(pG[2][:, :], lhsT=omT[:, k, :], rhs=wom[:, k, cs_], start=(k == 0), stop=(k == 3)),
                             reads=[d_omT, d_w], writes=[d_pG[2]])
                    P.op("dve", lambda e, hv=hv: e.tensor_tensor(out=mg[:], in0=pG[0][:, :], in1=gs[:, hv * 512:(hv + 1) * 512], op=ALU.mult),
                         reads=[d_pG[0], d_gs], writes=[d_mg])
                    P.op("dve", lambda e, hv=hv: e.tensor_tensor(out=tt[:], in0=pG[1][:, :], in1=gs[:, 1024 + hv * 512:1024 + (hv + 1) * 512], op=ALU.mult),
                         reads=[d_pG[1], d_gs], writes=[d_tt])
                    P.op("pool", lambda e: e.tensor_tensor(out=mg[:], in0=mg[:], in1=tt[:], op=ALU.add), reads=[d_mg, d_tt], writes=[d_mg])
                    P.op("dve", lambda e, hv=hv: e.tensor_tensor(out=tt[:], in0=pG[2][:, :], in1=gs[:, 2048 + hv * 512:2048 + (hv + 1) * 512], op=ALU.mult),
                         reads=[d_pG[2], d_gs], writes=[d_tt])
                    P.op("pool", lambda e, cs_=cs_: e.tensor_tensor(out=mgb[:, cs_], in0=mg[:], in1=tt[:], op=ALU.add), reads=[d_mg, d_tt], writes=[d_mgb])
                for k in range(8):
                    P.op("pe", lambda e, k=k: e.transpose(out=pT[:, k, :], in_=mgb[:, k * 128:(k + 1) * 128], identity=idb[:]), reads=[d_mgb, d_idb], writes=[d_pT])
                P.op("act", lambda e: e.copy(out=mgT[:], in_=pT[:]), reads=[d_pT], writes=[d_mgT])
                for hv in range(2):
                    cs_ = slice(hv * 512, (hv + 1) * 512)
                    for k in range(8):
                        P.op("pe", lambda e, k=k, cs_=cs_, hv=hv: e.matmul(pG[hv][:, :], lhsT=mgT[:, k, :], rhs=wout[:, k, cs_], start=(k == 0), stop=(k == 7)),
                             reads=[d_mgT, d_w], writes=[d_pG[hv]])
                    P.op("dve", lambda e, xb=xb, cs_=cs_, hv=hv: e.tensor_tensor(out=x1[:, cs_], in0=pG[hv][:, :], in1=xt[xb][:, cs_], op=ALU.add),
                         reads=[d_pG[hv], d_xt[xb]], writes=[d_x1])
                P.dma("sp", lambda e, r0=r0: e.dma_start(out=x1_d[r0:r0 + 128, :], in_=x1[:]), reads=[d_x1], writes=[d_x1d])
                if ti + 1 < ntiles:
                    norm_d(ti + 1)
                if stage < 2:
                    continue
                P.op("act", lambda e: e.activation(out=junk[:], in_=x1[:], func=AF.Square, accum_out=ssq[:]), reads=[d_x1], writes=[d_junk, d_ssq])
                P.op("act", lambda e: e.activation(out=rstd[:], in_=ssq[:], func=AF.Sqrt, bias=EPS, scale=1.0 / D), reads=[d_ssq], writes=[d_rstd])
                P.op("dve", lambda e: e.reciprocal(out=rstd[:], in_=rstd[:]), reads=[d_rstd], writes=[d_rstd])
                P.op("dve", lambda e: e.scalar_tensor_tensor(out=h2f[:], in0=x1[:], scalar=rstd[:, 0:1], in1=gf[:], op0=ALU.mult, op1=ALU.mult),
                     reads=[d_x1, d_rstd, d_c], writes=[d_h2f])
                P.op("pool", lambda e, hbuf=hbuf: e.tensor_copy(out=h2b[hbuf][:], in_=h2f[:]), reads=[d_h2f], writes=[d_h2b[hbuf]])
                for half in range(2):
                    for k4 in range(4):
                        k = half * 4 + k4
                        P.op("pe", lambda e, k=k, k4=k4: e.transpose(out=pF[:, k4 * 128:(k4 + 1) * 128], in_=h2f[:, k * 128:(k + 1) * 128], identity=idf[:]),
                             reads=[d_h2f, d_idf], writes=[d_pF])
                    P.op("act", lambda e, half=half: e.copy(out=h2T[:, half * 4:(half + 1) * 4, :].rearrange("p a b -> p (a b)"), in_=pF[:, :]), reads=[d_pF], writes=[d_h2T])
                for k in range(8):
                    P.op("pe", lambda e, k=k: e.matmul(pF[:, 0:36], lhsT=h2T[:, k, :], rhs=wr[:, k, :], start=(k == 0), stop=(k == 7)), reads=[d_h2T, d_w], writes=[d_pF])
                P.op("act", lambda e: e.copy(out=lg[:], in_=pF[:, 0:36]), reads=[d_pF], writes=[d_lg])
                P.op("dve", lambda e: e.tensor_reduce(out=sm[:, 0:1], in_=lg[:, 0:4], axis=AX.X, op=ALU.max), reads=[d_lg], writes=[d_sm])
                P.op("dve", lambda e: e.tensor_scalar(out=sm[:, 1:2], in0=sm[:, 0:1], scalar1=-1.0, scalar2=None, op0=ALU.mult), reads=[d_sm], writes=[d_sm])
                P.op("act", lambda e: e.activation(out=sm[:, 8:12], in_=lg[:, 0:4], func=AF.Exp, bias=sm[:, 1:2], accum_out=sm[:, 2:3]), reads=[d_lg, d_sm], writes=[d_sm])
                P.op("dve", lambda e: e.reciprocal(out=sm[:, 3:4], in_=sm[:, 2:3]), reads=[d_sm], writes=[d_sm])
                P.op("dve", lambda e: e.tensor_scalar(out=goh[:], in0=lg[:, 0:4], scalar1=sm[:, 0:1], scalar2=None, op0=ALU.is_ge), reads=[d_lg, d_sm], writes=[d_goh])
                P.op("dve", lambda e: e.tensor_tensor(out=lsel[:].rearrange("p (g e) -> p g e", g=4), in0=lg[:, 4:36].rearrange("p (g e) -> p g e", g=4),
                                                      in1=goh[:].unsqueeze(2).to_broadcast([128, 4, 8]), op=ALU.mult), reads=[d_lg, d_goh], writes=[d_lsel])
                P.op("dve", lambda e: e.tensor_reduce(out=les[:], in_=lsel[:].rearrange("p (g e) -> p e g", g=4), axis=AX.X, op=ALU.add), reads=[d_lsel], writes=[d_les])
                P.op("dve", lambda e: e.max(out=t8[:], in_=les[:]), reads=[d_les], writes=[d_t8])
                P.op("dve", lambda e: e.tensor_tensor(out=sm[:, 4:5], in0=t8[:, 1:2], in1=t8[:, 0:1], op=ALU.subtract), reads=[d_t8, d_sm], writes=[d_sm])
                P.op("act", lambda e: e.activation(out=sm[:, 5:6], in_=sm[:, 4:5], func=AF.Exp), reads=[d_sm], writes=[d_sm])
                P.op("dve", lambda e: e.tensor_scalar(out=sm[:, 6:7], in0=sm[:, 5:6], scalar1=1.0, scalar2=None, op0=ALU.add), reads=[d_sm], writes=[d_sm])
                P.op("dve", lambda e: e.reciprocal(out=sm[:, 6:7], in_=sm[:, 6:7]), reads=[d_sm], writes=[d_sm])
                P.op("dve", lambda e: e.tensor_tensor(out=sm[:, 7:8], in0=sm[:, 5:6], in1=sm[:, 6:7], op=ALU.mult), reads=[d_sm], writes=[d_sm])
                P.op("dve", lambda e, ti=ti: e.tensor_scalar(out=combs[:, ti, :], in0=sm[:, 6:8], scalar1=sm[:, 3:4], scalar2=None, op0=ALU.mult), reads=[d_sm], writes=[d_combs])
                for j in range(2):
                    P.op("dve", lambda e, j=j: e.tensor_scalar(out=oh[:, j * 32:(j + 1) * 32], in0=lg[:, 4:36], scalar1=t8[:, j:j + 1], scalar2=None, op0=ALU.is_equal),
                         reads=[d_lg, d_t8], writes=[d_oh])
                    P.op("dve", lambda e, j=j: e.tensor_tensor(out=oh[:, j * 32:(j + 1) * 32].rearrange("p (g e) -> p g e", g=4), in0=oh[:, j * 32:(j + 1) * 32].rearrange("p (g e) -> p g e", g=4),
                                                               in1=goh[:].unsqueeze(2).to_broadcast([128, 4, 8]), op=ALU.mult), reads=[d_oh, d_goh], writes=[d_oh])
                P.op("dve", lambda e: e.tensor_tensor(out=ohs[:], in0=oh[:, 0:32], in1=oh[:, 32:64], op=ALU.add), reads=[d_oh], writes=[d_ohs])
                P.op("pe", lambda e: e.matmul(pF[:, 64:96], lhsT=tri[:], rhs=ohs[:], start=True, stop=True), reads=[d_c, d_ohs], writes=[d_pF])
                P.op("pe", lambda e: e.matmul(pF[:, 128:160], lhsT=onesf[:], rhs=ohs[:], start=True, stop=True), reads=[d_c, d_ohs], writes=[d_pF])
                P.op("dve", lambda e: e.tensor_tensor(out=posf[:], in0=pF[:, 64:96], in1=ohs[:], op=ALU.subtract), reads=[d_pF, d_ohs], writes=[d_posf])
                P.op("dve", lambda e: e.tensor_tensor(out=posf[:], in0=posf[:], in1=base[:], op=ALU.add), reads=[d_posf, d_base], writes=[d_posf])
                P.op("dve", lambda e: e.tensor_tensor(out=posf[:], in0=posf[:], in1=ecap[:], op=ALU.add), reads=[d_posf, d_c], writes=[d_posf])
                P.op("dve", lambda e: e.tensor_tensor(out=base[:], in0=base[:], in1=pF[:, 128:160], op=ALU.add), reads=[d_base, d_pF, d_posf], writes=[d_base])
                for j in range(2):
                    P.op("dve", lambda e, j=j: e.tensor_tensor(out=oh[:, j * 32:(j + 1) * 32], in0=oh[:, j * 32:(j + 1) * 32], in1=posf[:], op=ALU.mult), reads=[d_oh, d_posf], writes=[d_oh])
                    P.op("dve", lambda e, j=j: e.tensor_reduce(out=idf2[:, j:j + 1], in_=oh[:, j * 32:(j + 1) * 32], axis=AX.X, op=ALU.add), reads=[d_oh], writes=[d_idf2])
                P.op("dve", lambda e, ti=ti: e.tensor_copy(out=idxs[:, ti, :], in_=idf2[:]), reads=[d_idf2], writes=[d_idxs])
                for j in range(2):
                    P.dma("pool", lambda e, ti=ti, j=j, hbuf=hbuf: e.indirect_dma_start(out=xbuf[:, :], out_offset=bass.IndirectOffsetOnAxis(ap=idxs[:, ti, j:j + 1], axis=0),
                                                                                      in_=h2b[hbuf][:], in_offset=None),
                          reads=[d_h2b[hbuf], d_idxs], writes=[d_xbuf])

        if stage < 3:
            return
        if hasattr(P, 'barrier'):
            P.barrier()
        with contextlib.ExitStack() as st:
            def sb(name, shape, dt):
                return st.enter_context(nc.sbuf_tensor("d_" + name, shape, dt))

            def ps(name, shape, dt=F32):
                return st.enter_context(nc.psum_tensor(name, shape, dt))
            NW = 2
            wge = [sb("wge%d" % i, [128, 8, 512], BF16) for i in range(NW)]; d_wge = [Dep() for _ in range(NW)]
            wue = [sb("wue%d" % i, [128, 8, 512], BF16) for i in range(NW)]; d_wue = [Dep() for _ in range(NW)]
            wde = [sb("wde%d" % i, [128, 4, 1024], BF16) for i in range(NW)]; d_wde = [Dep() for _ in range(NW)]
            xe = [sb("xe%d" % i, [128, 3, D], BF16) for i in range(NW)]; d_xe = [Dep() for _ in range(NW)]
            xeT = sb("xeT", [128, 8, CAP], BF16); d_xeT = Dep()
            sg = sb("sg", [128, CAP], F32); d_sg = Dep()
            hTe = sb("hTe", [128, 4, CAP], BF16); d_hTe = Dep()
            ye = [sb("ye%d" % i, [128, 3, D], F32) for i in range(NW)]; d_ye = [Dep() for _ in range(NW)]
            pT = ps("pTe", [128, 8, 128], BF16); d_pT = Dep()
            pg_ = [ps("pge%d" % i, [128, 512], F32) for i in range(2)]; d_pg = [Dep() for _ in range(2)]
            pu_ = [ps("pue%d" % i, [128, 512], F32) for i in range(2)]; d_pu = [Dep() for _ in range(2)]
            py_ = [ps("pye%d" % i, [128, 512], F32) for i in range(2)]; d_py = [Dep() for _ in range(2)]
            for ie, ex in enumerate(experts):
                b = ie % NW
                P.dma("pool", lambda e, b=b, ex=ex: e.dma_start(out=wge[b][:], in_=wgate_d[ex].rearrange("(k p) f -> p k f", p=128)), writes=[d_wge[b]])
                P.dma("pool", lambda e, b=b, ex=ex: e.dma_start(out=wue[b][:], in_=wup_d[ex].rearrange("(k p) f -> p k f", p=128)), writes=[d_wue[b]])
                P.dma("pool", lambda e, b=b, ex=ex: e.dma_start(out=wde[b][:], in_=wdown_d[ex].rearrange("(k p) f -> p k f", p=128)), writes=[d_wde[b]])
                P.dma("sp", lambda e, b=b, ex=ex: e.dma_start(out=xe[b][:], in_=xbuf[ex * CAP:(ex + 1) * CAP, :].rearrange("(s p) d -> p s d", p=128)),
                      reads=[d_xbuf], writes=[d_xe[b]])
                for s in range(3):
                    for k in range(8):
                        P.op("pe", lambda e, b=b, s=s, k=k: e.transpose(out=pT[:, k, :], in_=xe[b][:, s, k * 128:(k + 1) * 128], identity=idb[:]),
                             reads=[d_xe[b], d_idb], writes=[d_pT])
                    P.op("act", lambda e, s=s: e.copy(out=xeT[:, :, s * 128:(s + 1) * 128], in_=pT[:]), reads=[d_pT], writes=[d_xeT])
                for ft in range(4):
                    pb = ft % 2
                    for k in range(8):
                        P.op("pe", lambda e, b=b, ft=ft, k=k, pb=pb: e.matmul(pg_[pb][:, 0:CAP], lhsT=wge[b][:, k, ft * 128:(ft + 1) * 128], rhs=xeT[:, k, :],
                                                                            start=(k == 0), stop=(k == 7)), reads=[d_wge[b], d_xeT], writes=[d_pg[pb]])
                    for k in range(8):
                        P.op("pe", lambda e, b=b, ft=ft, k=k, pb=pb: e.matmul(pu_[pb][:, 0:CAP], lhsT=wue[b][:, k, ft * 128:(ft + 1) * 128], rhs=xeT[:, k, :],
                                                                            start=(k == 0), stop=(k == 7)), reads=[d_wue[b], d_xeT], writes=[d_pu[pb]])
                    P.op("act", lambda e, pb=pb: e.activation(out=sg[:], in_=pg_[pb][:, 0:CAP], func=AF.Silu), reads=[d_pg[pb]], writes=[d_sg])
                    P.op("dve", lambda e, ft=ft, pb=pb: e.tensor_tensor(out=hTe[:, ft, :], in0=sg[:], in1=pu_[pb][:, 0:CAP], op=ALU.mult),
                         reads=[d_sg, d_pu[pb]], writes=[d_hTe])
                for s in range(3):
                    for hv in range(2):
                        for ft in range(4):
                            P.op("pe", lambda e, b=b, s=s, hv=hv, ft=ft: e.matmul(py_[hv][:, :], lhsT=hTe[:, ft, s * 128:(s + 1) * 128], rhs=wde[b][:, ft, hv * 512:(hv + 1) * 512],
                                                                                start=(ft == 0), stop=(ft == 3)), reads=[d_hTe, d_wde[b]], writes=[d_py[hv]])
                        P.op("act" if hv == 0 else "dve", (lambda e, b=b, s=s, hv=hv: e.copy(out=ye[b][:, s, hv * 512:(hv + 1) * 512], in_=py_[hv][:, :])) if hv == 0 else
                             (lambda e, b=b, s=s, hv=hv: e.tensor_copy(out=ye[b][:, s, hv * 512:(hv + 1) * 512], in_=py_[hv][:, :])),
                             reads=[d_py[hv]], writes=[d_ye[b]])
                P.dma("sp", lambda e, b=b, ex=ex: e.dma_start(out=ybuf[ex * CAP:(ex + 1) * CAP, :].rearrange("(s p) d -> p s d", p=128), in_=ye[b][:]),
                      reads=[d_ye[b]], writes=[d_ybuf])

        if hasattr(P, 'barrier'):
            P.barrier()
        with contextlib.ExitStack() as st:
            def sb(name, shape, dt):
                return st.enter_context(nc.sbuf_tensor("d_" + name, shape, dt))
            NF = 4
            x1t = [sb("x1t%d" % i, [128, D], F32) for i in range(NF)]; d_x1t = [Dep() for _ in range(NF)]
            y1 = [sb("y1t%d" % i, [128, D], F32) for i in range(NF)]; d_y1 = [Dep() for _ in range(NF)]
            y2 = [sb("y2t%d" % i, [128, D], F32) for i in range(NF)]; d_y2 = [Dep() for _ in range(NF)]
            ot = [sb("ot%d" % i, [128, D], F32) for i in range(NF)]; d_ot = [Dep() for _ in range(NF)]
            for ti in range(ntiles):
                b = ti % NF
                r0 = ti * 128
                P.dma("sp", lambda e, b=b, r0=r0: e.dma_start(out=x1t[b][:], in_=x1_d[r0:r0 + 128, :]), reads=[d_x1d], writes=[d_x1t[b]])
                P.dma("pool", lambda e, b=b, ti=ti: e.indirect_dma_start(out=y1[b][:], out_offset=None, in_=ybuf[:, :],
                                                                        in_offset=bass.IndirectOffsetOnAxis(ap=idxs[:, ti, 0:1], axis=0)),
                      reads=[d_ybuf, d_idxs], writes=[d_y1[b]])
                P.dma("pool", lambda e, b=b, ti=ti: e.indirect_dma_start(out=y2[b][:], out_offset=None, in_=ybuf[:, :],
                                                                        in_offset=bass.IndirectOffsetOnAxis(ap=idxs[:, ti, 1:2], axis=0)),
                      reads=[d_ybuf, d_idxs], writes=[d_y2[b]])
                P.op("dve", lambda e, b=b, ti=ti: e.scalar_tensor_tensor(out=ot[b][:], in0=y1[b][:], scalar=combs[:, ti, 0:1], in1=x1t[b][:], op0=ALU.mult, op1=ALU.add),
                     reads=[d_y1[b], d_combs, d_x1t[b]], writes=[d_ot[b]])
                P.op("dve", lambda e, b=b, ti=ti: e.scalar_tensor_tensor(out=ot[b][:], in0=y2[b][:], scalar=combs[:, ti, 1:2], in1=ot[b][:], op0=ALU.mult, op1=ALU.add),
                     reads=[d_y2[b], d_combs, d_ot[b]], writes=[d_ot[b]])
                P.dma("sp", lambda e, b=b, r0=r0: e.dma_start(out=out_d[r0:r0 + 128, :], in_=ot[b][:]), reads=[d_ot[b]], writes=[d_out])


def host_consts_d(z):
    tri = (np.arange(128)[:, None] <= np.arange(128)[None, :]).astype(np.float32)
    return dict(mqn=np.tile(z['mem_q_norm'], 4)[None, :], mkn=np.tile(z['mem_k_norm'], 4)[None, :],
                w_r=np.ascontiguousarray(np.concatenate([z['w_router_group'], z['w_router_expert']], axis=1)),
                trif=tri, ecap=(np.arange(32, dtype=np.float32) * CAP)[None, :], ident=np.eye(128, dtype=np.float32))


def build_full():
    nc = bass.Bass("TRN2", target_bir_lowering=False)
    P = Prog(nc)

    def di(name, shape, dt=F32):
        return nc.dram_tensor(name, shape, dt, kind="ExternalInput")
    xin = di("xin", [TP + T, D]); w_in = di("w_in", [D, IN_COLS]); g_mix = di("g_mix", [1, D])
    qkn = di("qkn", [2, 512]); cs = di("cs", [TP + T, 64]); ident_d = di("ident", [128, 128])
    e_d = di("e_d", [32, NK], BF16); vq_d = di("vq", [1, 1024]); nb_d = di("nb", [1, 1024]); oq_d = di("oq", [1, 1024])
    tri_d = di("tri", [128, 128], BF16)
    cwl_d = di("cwl", [128, 16, 4]); cbl_d = di("cbl", [128, 16]); dtb_d = di("dtb", [1, 16]); alog_d = di("alog", [1, 16])
    dsk_d = di("dsk", [1, 16]); ssdn_d = di("ssdn", [1, D]); tri2_d = di("tri2", [128, 2, 256]); sel_d = di("sel_d", [16, 16, 128])
    tb_d = di("tb_d", [128, 384]); pflag_d = di("pflag", [1, 1])
    mem_d = di("mem", [256, D]); gmem_d = di("g_mem", [1, D]); wmemkv = di("w_mem_kv", [D, 1024])
    mqn_d = di("mqn", [1, 512]); mkn_d = di("mkn", [1, 512])
    woa_d = di("w_o_moba", [512, D]); wos_d = di("w_o_ssd", [D, D]); wom_d = di("w_o_mem", [512, D]); wout_d = di("w_out", [D, D])
    gffn_d = di("g_ffn", [1, D]); wr_d = di("w_r", [D, 36])
    wgate_d = di("w_gate", [NE, D, 512]); wup_d = di("w_up", [NE, D, 512]); wdown_d = di("w_down", [NE, 512, D])
    trif_d = di("trif", [128, 128]); ecap_d = di("ecap", [1, 32])
    out_d = nc.dram_tensor("out", [T, D], F32, kind="ExternalOutput")
    kT_d = nc.dram_tensor("kT_d", [512, NK], BF16)
    v_d = nc.dram_tensor("v_d", [NK, 512], BF16)
    qT_d = nc.dram_tensor("qT_d", [512, T], BF16)
    oa_d = nc.dram_tensor("oa_d", [T, 512], BF16)
    os_d = nc.dram_tensor("os_d", [T, D], BF16)
    x1_d = nc.dram_tensor("x1_d", [T, D], F32)
    xbuf = nc.dram_tensor("xbuf", [NE * CAP, D], BF16)
    ybuf = nc.dram_tensor("ybuf", [NE * CAP, D], F32)

    with contextlib.ExitStack() as st0:
        idf = st0.enter_context(nc.sbuf_tensor("idf", [128, 128], F32)); d_idf = Dep()
        idb = st0.enter_context(nc.sbuf_tensor("idb", [128, 128], BF16)); d_idb = Dep()
        P.dma("sp", lambda e: e.dma_start(out=idf[:], in_=ident_d[:, :]), writes=[d_idf])
        P.op("dve", lambda e: e.tensor_copy(out=idb[:], in_=idf[:]), reads=[d_idf], writes=[d_idb])
        with contextlib.ExitStack() as st:
            phase_a(nc, P, st, xin, w_in, g_mix, qkn, cs, idb, d_idb, kT_d, v_d, qT_d)
        P.barrier()
        with contextlib.ExitStack() as st:
            oa = st.enter_context(nc.sbuf_tensor("s_oa", [128, 32, 512], BF16)); d_oa = Dep()
            phase_b(nc, P, st, kT_d, v_d, qT_d, e_d, vq_d, nb_d, oq_d, tri_d, idb, d_idb, oa, d_oa)
            P.dma("sp", lambda e: e.dma_start(out=oa_d[:, :].rearrange("(t p) c -> p t c", p=128), in_=oa[:]), reads=[d_oa])
        P.barrier()
        with contextlib.ExitStack() as st:
            phase_c(nc, P, st, xin, w_in, g_mix, cwl_d, cbl_d, dtb_d, alog_d, dsk_d, ssdn_d, tri2_d, sel_d, tb_d, pflag_d,
                    idb, d_idb, os_d, Dep())
        P.barrier()
        phase_def(nc, P, xin[TP:TP + T, :], w_in, g_mix, mem_d, gmem_d, wmemkv, mqn_d, mkn_d, woa_d, wos_d, wom_d, wout_d, gffn_d, wr_d,
                  wgate_d, wup_d, wdown_d, trif_d, ecap_d, oa_d, Dep(), os_d, Dep(), x1_d, xbuf, ybuf, out_d, idb, d_idb, idf, d_idf)
        P.emit()
    return nc


_NC_CACHE = {}


def kernel(x, mem, g_mix, w_in, moba_q_norm, moba_k_norm, conv_w, conv_b, dt_bias, a_log, d_skip, ssd_norm, g_mem, w_mem_kv,
           mem_q_norm, mem_k_norm, w_o_moba, w_o_ssd, w_o_mem, w_out, g_ffn, w_router_group, w_router_expert, w_gate, w_up, w_down):
    f32 = np.float32
    A = lambda a: np.ascontiguousarray(np.asarray(a, dtype=f32))
    x = A(x); mem = A(mem)
    z = dict(conv_w=A(conv_w), conv_b=A(conv_b), dt_bias=A(dt_bias), a_log=A(a_log), d_skip=A(d_skip), ssd_norm=A(ssd_norm),
             mem_q_norm=A(mem_q_norm), mem_k_norm=A(mem_k_norm), w_router_group=A(w_router_group), w_router_expert=A(w_router_expert))
    if "nc" not in _NC_CACHE:
        _NC_CACHE["nc"] = build_full()
    nc = _NC_CACHE["nc"]
    pos = np.arange(2 * T, dtype=f32)
    inv = (10000.0 ** (-np.arange(32, dtype=f32) / 32)).astype(f32)
    ang = pos[:, None] * inv[None, :]
    cs_full = np.concatenate([np.cos(ang), np.sin(ang)], axis=1).astype(f32)
    qkn = np.stack([np.tile(A(moba_q_norm), 8), np.tile(A(moba_k_norm), 8)]).astype(f32)
    shared = dict(w_in=A(w_in), g_mix=A(g_mix)[None, :], qkn=qkn, ident=np.eye(128, dtype=f32),
                  g_mem=A(g_mem)[None, :], w_mem_kv=A(w_mem_kv), w_o_moba=A(w_o_moba), w_o_ssd=A(w_o_ssd), w_o_mem=A(w_o_mem),
                  w_out=A(w_out), g_ffn=A(g_ffn)[None, :], w_gate=A(w_gate), w_up=A(w_up), w_down=A(w_down))
    shared.update(host_consts_d(z))
    in_maps = []
    for c in range(8):
        bb, hh = c // 2, c % 2
        if hh == 0:
            xin = np.concatenate([np.zeros((TP, D), f32), x[bb, :T]], 0)
            csc = np.concatenate([cs_full[:TP], cs_full[:T]], 0)
        else:
            xin = x[bb]
            csc = cs_full
        m = dict(shared)
        m.update(xin=np.ascontiguousarray(xin), cs=np.ascontiguousarray(csc), mem=mem[bb])
        m.update(host_consts(hh))
        m.update(host_consts_c(z, hh))
        in_maps.append(m)
    res = run_bass_kernel_spmd(nc, in_maps, core_ids=list(range(8)))
    out = np.empty((4, 2 * T, D), f32)
    for c in range(8):
        bb, hh = c // 2, c % 2
        out[bb, hh * T:(hh + 1) * T] = np.asarray(res.results[c]["out"], dtype=f32)
    return out
```

```python
import contextlib
import numpy as np
import ml_dtypes
import concourse.bass as bass
import concourse.mybir as mybir
from concourse.bass_utils import run_bass_kernel_spmd

F32 = mybir.dt.float32
BF16 = mybir.dt.bfloat16
I32 = mybir.dt.int32
U32 = mybir.dt.uint32
AF = mybir.ActivationFunctionType
ALU = mybir.AluOpType
AX = mybir.AxisListType

T = 4096
TP = 4096
NK = TP + T
D = 1024
EPS = 1e-6
IN_COLS = 8208
BIGG = 30000.0
MB = 1000.0
NEG = -30000.0
C_Z, C_X, C_DT = 1536, 2560, 4608
C_QM, C_G = 4624, 5136
CAP = 384
NE = 32


class Dep:
    __slots__ = ("w", "r")

    def __init__(self):
        self.w = None
        self.r = {}


class Op:
    __slots__ = ("eng", "fn", "deps", "is_dma", "sig", "sigval", "dsem", "dval", "prev")

    def __init__(self, eng, fn, is_dma):
        self.eng = eng
        self.fn = fn
        self.is_dma = is_dma
        self.deps = []
        self.sig = False
        self.sigval = 0
        self.dsem = None
        self.dval = 0
        self.prev = None


class Prog:
    ENGS = ("pe", "act", "dve", "pool", "sp")

    def __init__(self, nc, ndma_sems=12):
        self.nc = nc
        self.ops = {e: [] for e in self.ENGS}
        self.ndma = {e: 0 for e in self.ENGS}
        self.dma_last = {}
        self.ndma_sems = ndma_sems
        self.all_dma = []

    def _add(self, o, reads, writes):
        deps = {}
        raw = set()
        for t in reads:
            if t.w is not None:
                deps[id(t.w)] = t.w
                raw.add(id(t.w))
        for t in writes:
            if t.w is not None:
                deps[id(t.w)] = t.w
            for r in t.r.values():
                deps[id(r)] = r
        for t in reads:
            key = id(o) if o.is_dma else o.eng
            t.r[key] = o
        for t in writes:
            t.w = o
            t.r = {}
        dl = []
        for d in deps.values():
            if d is o:
                continue
            if (not d.is_dma) and (not o.is_dma) and d.eng == "pe" and o.eng == "pe":
                continue
            if (not d.is_dma) and (not o.is_dma) and d.eng == o.eng and id(d) not in raw:
                continue
            dl.append(d)
            if not d.is_dma:
                d.sig = True
        o.deps = dl
        self.ops[o.eng].append(o)
        return o

    def op(self, eng, fn, reads=(), writes=()):
        return self._add(Op(eng, fn, False), reads, writes)

    def dma(self, eng, fn, reads=(), writes=()):
        o = Op(eng, fn, True)
        n = self.ndma[eng]
        self.ndma[eng] += 1
        slot = (eng, n % self.ndma_sems)
        o.dsem = slot
        o.prev = self.dma_last.get(slot)
        o.dval = (o.prev.dval if o.prev else 0) + 16
        self.dma_last[slot] = o
        self.all_dma.append(o)
        return self._add(o, reads, writes)


    def barrier(self):
        lasts = []
        for e in self.ENGS:
            for o in reversed(self.ops[e]):
                if (not o.is_dma) and o.fn is not None:
                    o.sig = True
                    lasts.append(o)
                    break
        dmas = list(self.dma_last.values())
        for e in self.ENGS:
            o = Op(e, None, False)
            o.deps = [d for d in lasts if d.eng != e] + dmas
            self.ops[e].append(o)

    def emit(self):
        nc = self.nc
        import contextlib
        with contextlib.ExitStack() as st:
            esem = {e: st.enter_context(nc.semaphore("S_" + e)) for e in self.ENGS}
            dsem = {}
            for e in self.ENGS:
                if self.ndma[e]:
                    for i in range(min(self.ndma_sems, self.ndma[e])):
                        dsem[(e, i)] = st.enter_context(nc.semaphore("D_%s_%d" % (e, i)))
            for e in self.ENGS:
                c = 0
                for o in self.ops[e]:
                    if (not o.is_dma) and o.sig and o.fn is not None:
                        c += 1
                        o.sigval = c
            block = st.enter_context(nc.Block())
            handles = {"pe": block.tensor, "act": block.scalar, "dve": block.vector,
                       "pool": block.gpsimd, "sp": block.sync}

            def make(e):
                def body(eng):
                    known = {}

                    def wait(sem, key, val):
                        if known.get(key, 0) < val:
                            eng.wait_ge(sem, val)
                            known[key] = val
                    for o in self.ops[e]:
                        for d in o.deps:
                            if d.is_dma:
                                wait(dsem[d.dsem], d.dsem, d.dval)
                            else:
                                wait(esem[d.eng], d.eng, d.sigval)
                        if o.is_dma:
                            if o.prev is not None:
                                wait(dsem[o.dsem], o.dsem, o.prev.dval)
                            o.fn(eng).then_inc(dsem[o.dsem], 16)
                        elif o.fn is not None:
                            ins = o.fn(eng)
                            if o.sig:
                                ins.then_inc(esem[e], 1)
                    if e == "sp":
                        for slot, o in self.dma_last.items():
                            wait(dsem[slot], slot, o.dval)
                return body
            for e in self.ENGS:
                if self.ops[e] or e == "sp":
                    handles[e](make(e))


def phase_a(nc, P, st, xin, w_in, g_mix, qkn, cs, idb, d_idb, kT_d, v_d, qT_d):
    def sb(name, shape, dt):
        return st.enter_context(nc.sbuf_tensor("a_" + name, shape, dt))

    def ps(name, shape, dt=F32):
        return st.enter_context(nc.psum_tensor("a_" + name, shape, dt))
    wq = sb("wq", [128, 8, 1536], BF16); d_wq = Dep()
    gm = sb("gm", [128, D], F32); d_gm = Dep()
    gq = sb("gq", [128, 2, 512], F32); d_gq = Dep()
    NB = 2
    NX = 4
    xt = [sb("xt%d" % i, [128, D], F32) for i in range(NX)]; d_xt = [Dep() for _ in range(NX)]
    cst = [sb("cst%d" % i, [128, 64], F32) for i in range(NX)]; d_cst = [Dep() for _ in range(NX)]
    junk = sb("junk", [128, D], BF16); d_junk = Dep()
    ssq = sb("ssq", [128, 1], F32); d_ssq = Dep()
    rstd = sb("rstd", [128, 1], F32); d_rstd = Dep()
    hb_ = [sb("hb%d" % i, [128, D], BF16) for i in range(2)]; d_hb_ = [Dep(), Dep()]
    hT = [sb("hT%d" % i, [128, 8, 128], BF16) for i in range(NB)]; d_hT = [Dep() for _ in range(NB)]
    pT = ps("pT", [128, 8, 128], BF16); d_pT = Dep()
    pq = [[ps("pq%d_%d" % (j, i), [128, 512], F32) for i in range(3)] for j in range(2)]; d_pq = [[Dep() for _ in range(3)] for _ in range(2)]
    pkT = ps("pkT", [128, 4, 128], BF16); d_pkT = Dep()
    sq_ = [sb("sq%d" % i, [128, 512], F32) for i in range(2)]; d_sq_ = [Dep(), Dep()]
    hs_ = [sb("hs%d" % i, [128, 8], F32) for i in range(2)]; d_hs_ = [Dep(), Dep()]
    hr_ = [sb("hr%d" % i, [128, 8], F32) for i in range(2)]; d_hr_ = [Dep(), Dep()]
    qn_ = [sb("qn%d" % i, [128, 512], F32) for i in range(2)]; d_qn_ = [Dep(), Dep()]
    t1_ = [sb("t1%d" % i, [128, 256], F32) for i in range(2)]; d_t1_ = [Dep(), Dep()]
    t2_ = [sb("t2%d" % i, [128, 256], F32) for i in range(2)]; d_t2_ = [Dep(), Dep()]
    t3_ = [sb("t3%d" % i, [128, 256], F32) for i in range(2)]; d_t3_ = [Dep(), Dep()]
    t4_ = [sb("t4%d" % i, [128, 256], F32) for i in range(2)]; d_t4_ = [Dep(), Dep()]
    qr_ = [[sb("qr%d_%d" % (i, j), [128, 512], BF16) for j in range(2)] for i in range(2)]; d_qr_ = [[Dep(), Dep()] for _ in range(2)]
    kTs = [[sb("kTs%d_%d" % (j, i), [128, 4, 128], BF16) for i in range(NB)] for j in range(2)]; d_kTs = [[Dep() for _ in range(NB)] for _ in range(2)]
    vs = [sb("vs%d" % i, [128, 512], BF16) for i in range(NB)]; d_vs = [Dep() for _ in range(NB)]

    for k in range(8):
        P.dma("pool", lambda e, k=k: e.dma_start(out=wq[:, k, :], in_=w_in[k * 128:(k + 1) * 128, 0:1536]), writes=[d_wq])
    P.dma("sp", lambda e: e.dma_start(out=gm[:], in_=g_mix[0:1, :].partition_broadcast(128)), writes=[d_gm])
    for i in range(2):
        P.dma("sp", lambda e, i=i: e.dma_start(out=gq[:, i, :], in_=qkn[i:i + 1, :].partition_broadcast(128)), writes=[d_gq])
    P.op("dve", lambda e: e.tensor_scalar(out=gq[:, 0, :], in0=gq[:, 0, :], scalar1=0.125, scalar2=None, op0=ALU.mult),
         reads=[d_gq], writes=[d_gq])

    def qk_post_steps(src_ps, d_src, which, b, xs):
        sl = which
        sq, hs, hr, qn, t1, t2, t3, t4, qr = sq_[sl], hs_[sl], hr_[sl], qn_[sl], t1_[sl], t2_[sl], t3_[sl], t4_[sl], qr_[sl][b]
        d_sq, d_hs, d_hr, d_qn, d_t1, d_t2, d_t3, d_t4, d_qr = (d_sq_[sl], d_hs_[sl], d_hr_[sl], d_qn_[sl], d_t1_[sl], d_t2_[sl],
                                                               d_t3_[sl], d_t4_[sl], d_qr_[sl][b])
        q3 = qn[:].rearrange("p (h d) -> p h d", h=8)
        q1 = q3[:, :, 0:32]
        q2 = q3[:, :, 32:64]
        cosb = cst[xs][:, 0:32].unsqueeze(1).to_broadcast([128, 8, 32])
        sinb = cst[xs][:, 32:64].unsqueeze(1).to_broadcast([128, 8, 32])
        v3 = lambda t: t[:].rearrange("p (h d) -> p h d", h=8)
        r3 = qr[:].rearrange("p (h d) -> p h d", h=8)
        steps = [
            lambda: P.op("act", lambda e: e.activation(out=sq[:], in_=src_ps[:], func=AF.Square), reads=[d_src], writes=[d_sq]),
            lambda: P.op("dve", lambda e: e.tensor_reduce(out=hs[:], in_=sq[:].rearrange("p (h d) -> p h d", h=8), axis=AX.X, op=ALU.add),
                         reads=[d_sq], writes=[d_hs]),
            lambda: P.op("act", lambda e: e.activation(out=hr[:], in_=hs[:], func=AF.Sqrt, bias=EPS, scale=1.0 / 64), reads=[d_hs], writes=[d_hr]),
            lambda: P.op("dve", lambda e: e.reciprocal(out=hr[:], in_=hr[:]), reads=[d_hr], writes=[d_hr]),
            lambda: P.op("dve", lambda e: e.tensor_tensor(out=qn[:].rearrange("p (h d) -> p h d", h=8), in0=src_ps[:].rearrange("p (h d) -> p h d", h=8),
                                                          in1=hr[:].unsqueeze(2).to_broadcast([128, 8, 64]), op=ALU.mult),
                         reads=[d_src, d_hr], writes=[d_qn]),
            lambda: P.op("pool", lambda e: e.tensor_tensor(out=qn[:], in0=qn[:], in1=gq[:, which, :], op=ALU.mult), reads=[d_qn, d_gq], writes=[d_qn]),
            lambda: P.op("dve", lambda e: e.tensor_tensor(out=v3(t1), in0=q1, in1=cosb, op=ALU.mult), reads=[d_qn, d_cst[xs]], writes=[d_t1]),
            lambda: P.op("pool", lambda e: e.tensor_tensor(out=v3(t2), in0=q2, in1=sinb, op=ALU.mult), reads=[d_qn, d_cst[xs]], writes=[d_t2]),
            lambda: P.op("dve", lambda e: e.tensor_tensor(out=v3(t3), in0=q2, in1=cosb, op=ALU.mult), reads=[d_qn, d_cst[xs]], writes=[d_t3]),
            lambda: P.op("pool", lambda e: e.tensor_tensor(out=v3(t4), in0=q1, in1=sinb, op=ALU.mult), reads=[d_qn, d_cst[xs]], writes=[d_t4]),
            lambda: P.op("dve", lambda e: e.tensor_tensor(out=r3[:, :, 0:32], in0=v3(t1), in1=v3(t2), op=ALU.subtract), reads=[d_t1, d_t2], writes=[d_qr]),
            lambda: P.op("pool", lambda e: e.tensor_tensor(out=r3[:, :, 32:64], in0=v3(t3), in1=v3(t4), op=ALU.add), reads=[d_t3, d_t4], writes=[d_qr]),
        ]
        return steps

    def to_T_and_store(dst_dram, col0, b, sl):
        for c in range(4):
            P.op("pe", lambda e, c=c: e.transpose(out=pkT[:, c, :], in_=qr_[sl][b][:, c * 128:(c + 1) * 128], identity=idb[:]),
                 reads=[d_qr_[sl][b], d_idb], writes=[d_pkT])
        P.op("act", lambda e: e.copy(out=kTs[sl][b][:], in_=pkT[:]), reads=[d_pkT], writes=[d_kTs[sl][b]])
        P.dma("sp", lambda e: e.dma_start(out=dst_dram[:, col0:col0 + 128].rearrange("(c p) t -> p c t", p=128), in_=kTs[sl][b][:]),
              reads=[d_kTs[sl][b]])

    def load(ti):
        xs = ti % NX
        r0 = ti * 128
        P.dma("sp", lambda e: e.dma_start(out=xt[xs][:], in_=xin[r0:r0 + 128, :]), writes=[d_xt[xs]])
        P.dma("sp", lambda e: e.dma_start(out=cst[xs][:], in_=cs[r0:r0 + 128, :]), writes=[d_cst[xs]])

    def norm(ti):
        b = ti % NB
        xs = ti % NX
        hb = hb_[b]
        P.op("act", lambda e: e.activation(out=junk[:], in_=xt[xs][:], func=AF.Square, accum_out=ssq[:]), reads=[d_xt[xs]], writes=[d_junk, d_ssq])
        P.op("act", lambda e: e.activation(out=rstd[:], in_=ssq[:], func=AF.Sqrt, bias=EPS, scale=1.0 / D), reads=[d_ssq], writes=[d_rstd])
        P.op("dve", lambda e: e.reciprocal(out=rstd[:], in_=rstd[:]), reads=[d_rstd], writes=[d_rstd])
        P.op("dve", lambda e: e.scalar_tensor_tensor(out=hb[:], in0=xt[xs][:], scalar=rstd[:, 0:1], in1=gm[:], op0=ALU.mult, op1=ALU.mult),
             reads=[d_xt[xs], d_rstd, d_gm], writes=[d_hb_[b]])

    def front(ti):
        b = ti % NB
        xs = ti % NX
        own = ti >= 32
        hb = hb_[b]
        for k in range(8):
            P.op("pe", lambda e, k=k: e.transpose(out=pT[:, k, :], in_=hb[:, k * 128:(k + 1) * 128], identity=idb[:]), reads=[d_hb_[b], d_idb], writes=[d_pT])
        P.op("act", lambda e: e.copy(out=hT[b][:], in_=pT[:]), reads=[d_pT], writes=[d_hT[b]])
        groups = [0, 1, 2] if own else [1, 2]
        for g in groups:
            for k in range(8):
                P.op("pe", lambda e, g=g, k=k: e.matmul(pq[b][g][:], lhsT=hT[b][:, k, :], rhs=wq[:, k, g * 512:(g + 1) * 512], start=(k == 0), stop=(k == 7)),
                     reads=[d_hT[b], d_wq], writes=[d_pq[b][g]])

    def post(ti):
        b = ti % NB
        xs = ti % NX
        own = ti >= 32
        ks = qk_post_steps(pq[b][1], d_pq[b][1], 1, b, xs)
        qs = qk_post_steps(pq[b][0], d_pq[b][0], 0, b, xs) if own else []
        for i in range(len(ks)):
            ks[i]()
            if qs:
                qs[i]()
        P.op("act", lambda e: e.copy(out=vs[b][:], in_=pq[b][2][:]), reads=[d_pq[b][2]], writes=[d_vs[b]])

    def tail(ti):
        b = ti % NB
        own = ti >= 32
        r0 = ti * 128
        to_T_and_store(kT_d, r0, b, 1)
        if own:
            to_T_and_store(qT_d, r0 - TP, b, 0)
        P.dma("sp", lambda e: e.dma_start(out=v_d[r0:r0 + 128, :], in_=vs[b][:]), reads=[d_vs[b]])

    load(0)
    load(1)
    load(2)
    norm(0)
    norm(1)
    front(0)
    for ti in range(64):
        if ti + 3 < 64:
            load(ti + 3)
        if ti + 2 < 64:
            norm(ti + 2)
        if ti + 1 < 64:
            front(ti + 1)
        if ti >= 1:
            tail(ti - 1)
        post(ti)
    tail(63)


def phase_b(nc, P, st, kT_d, v_d, qT_d, e_d, vq_d, nb_d, oq_d, tri_d, idb, d_idb, oa, d_oa, heads=range(8), nblk=16):
    def sb(name, shape, dt):
        return st.enter_context(nc.sbuf_tensor("b_" + name, shape, dt))

    def ps(name, shape, dt=F32):
        return st.enter_context(nc.psum_tensor(name, shape, dt))
    NB = 2
    kaug = [sb("kaug%d" % i, [96, NK], BF16) for i in range(NB)]; d_kaug = [Dep() for _ in range(NB)]
    vaug = [sb("vaug%d" % i, [128, 64, 65], BF16) for i in range(NB)]; d_vaug = [Dep() for _ in range(NB)]
    qaug = [sb("qaug%d" % i, [96, T], BF16) for i in range(NB)]; d_qaug = [Dep() for _ in range(NB)]
    d_qmb = [Dep() for _ in range(NB)]
    vq = sb("vq_s", [128, 1024], F32); d_c = Dep()
    nb = sb("nb_s", [128, 1024], F32)
    oq = sb("oq_s", [128, 1024], F32)
    tri = sb("tri_s", [128, 128], BF16)
    ksum = sb("ksum", [64, 32], F32); d_ksum = Dep()
    kmean = sb("kmean", [64, 32], BF16); d_kmean = Dep()
    g1 = sb("g1", [128, 512], F32); d_g1 = Dep()
    top8 = sb("top8", [128, 16, 8], F32); d_top8 = Dep()
    sel = sb("sel", [128, 512], F32); d_sel = Dep()
    mbp = sb("mbp", [128, 16, 96], BF16); d_mbp = Dep()
    NPT = 3
    pt = [sb("pt%d" % i, [128, 512], BF16) for i in range(NPT)]; d_pt = [Dep() for _ in range(NPT)]
    rc = sb("rc", [128, 2], F32); d_rc = [Dep(), Dep()]
    pg = ps("pg", [128, 512], F32); d_pg = Dep()
    pmT = ps("pmT", [96, 8, 128], BF16); d_pmT = Dep()
    psS = [ps("psS%d" % i, [128, 512], F32) for i in range(NPT)]; d_psS = [Dep() for _ in range(NPT)]
    po = [ps("po%d" % i, [128, 65], F32) for i in range(2)]; d_po = [Dep() for _ in range(2)]

    for i in range(NB):
        P.dma("sp", lambda e, i=i: e.dma_start(out=kaug[i][64:96, :], in_=e_d[:, :]), writes=[d_kaug[i]])
        P.op("pool", lambda e, i=i: e.memset(vaug[i][:], 1.0), writes=[d_vaug[i]])
    P.dma("sp", lambda e: e.dma_start(out=vq[:], in_=vq_d[0:1, :].partition_broadcast(128)), writes=[d_c])
    P.dma("sp", lambda e: e.dma_start(out=nb[:], in_=nb_d[0:1, :].partition_broadcast(128)), writes=[d_c])
    P.dma("sp", lambda e: e.dma_start(out=oq[:], in_=oq_d[0:1, :].partition_broadcast(128)), writes=[d_c])
    P.dma("pool", lambda e: e.dma_start(out=tri[:], in_=tri_d[:, :]), writes=[d_c])
    P.op("pool", lambda e: e.memset(mbp[:], 0.0), writes=[d_mbp])

    for ih, h in enumerate(heads):
        b = ih % NB
        P.dma("sp", lambda e, b=b, h=h: e.dma_start(out=kaug[b][0:64, :], in_=kT_d[h * 64:(h + 1) * 64, :]), writes=[d_kaug[b]])
        P.dma("sp", lambda e, b=b, h=h: e.dma_start(out=vaug[b][:, :, 0:64],
                                                    in_=v_d[:, h * 64:(h + 1) * 64].rearrange("(t p) d -> p t d", p=128)),
              writes=[d_vaug[b]])
        P.dma("sp", lambda e, b=b, h=h: e.dma_start(out=qaug[b][0:64, :], in_=qT_d[h * 64:(h + 1) * 64, :]), writes=[d_qaug[b]])
        P.op("dve", lambda e, b=b: e.tensor_reduce(out=ksum[:], in_=kaug[b][0:64, :].rearrange("p (b k) -> p b k", k=256),
                                                   axis=AX.X, op=ALU.add), reads=[d_kaug[b]], writes=[d_ksum])
        P.op("dve", lambda e: e.tensor_scalar(out=kmean[:], in0=ksum[:], scalar1=1.0 / 256, scalar2=None, op0=ALU.mult),
             reads=[d_ksum], writes=[d_kmean])
        for half in range(2):
            for j in range(16):
                qt = half * 16 + j
                P.op("pe", lambda e, b=b, j=j, qt=qt: e.matmul(pg[:, j * 32:(j + 1) * 32], lhsT=qaug[b][0:64, qt * 128:(qt + 1) * 128],
                                                              rhs=kmean[:, :], start=True, stop=True),
                     reads=[d_qaug[b], d_kmean], writes=[d_pg])
            cs_ = slice(half * 512, (half + 1) * 512)
            P.op("dve", lambda e, cs_=cs_: e.tensor_tensor(out=g1[:], in0=pg[:], in1=vq[:, cs_], op=ALU.mult),
                 reads=[d_pg, d_c], writes=[d_g1])
            P.op("dve", lambda e, cs_=cs_: e.tensor_tensor(out=g1[:], in0=g1[:], in1=nb[:, cs_], op=ALU.add),
                 reads=[d_g1, d_c], writes=[d_g1])
            for j in range(16):
                P.op("dve", lambda e, j=j: e.max(out=top8[:, j, :], in_=g1[:, j * 32:(j + 1) * 32]), reads=[d_g1], writes=[d_top8])
            P.op("dve", lambda e: e.tensor_tensor(out=sel[:].rearrange("p (j b) -> p j b", b=32),
                                                  in0=g1[:].rearrange("p (j b) -> p j b", b=32),
                                                  in1=top8[:, :, 2:3].to_broadcast([128, 16, 32]), op=ALU.is_ge),
                 reads=[d_g1, d_top8], writes=[d_sel])
            P.op("dve", lambda e, cs_=cs_: e.tensor_tensor(out=sel[:], in0=sel[:], in1=vq[:, cs_], op=ALU.mult),
                 reads=[d_sel, d_c], writes=[d_sel])
            P.op("dve", lambda e, cs_=cs_: e.tensor_tensor(out=sel[:], in0=sel[:], in1=oq[:, cs_], op=ALU.add),
                 reads=[d_sel, d_c], writes=[d_sel])
            P.op("dve", lambda e: e.tensor_scalar(out=mbp[:, :, 64:96], in0=sel[:].rearrange("p (j b) -> p j b", b=32),
                                                  scalar1=1.0, scalar2=MB, op0=ALU.subtract, op1=ALU.mult),
                 reads=[d_sel], writes=[d_mbp])
            for grp in range(2):
                for j in range(8):
                    P.op("pe", lambda e, grp=grp, j=j: e.transpose(out=pmT[:, j, :], in_=mbp[:, grp * 8 + j, :], identity=idb[:]),
                         reads=[d_mbp, d_idb], writes=[d_pmT])
                c0 = (half * 16 + grp * 8) * 128
                P.op("act", lambda e, b=b, c0=c0: e.copy(out=qaug[b][64:96, c0:c0 + 1024], in_=pmT[64:96, :, :]),
                     reads=[d_pmT], writes=[d_qmb[b]])
        units = []
        for jb in range(nblk):
            ncommon = 32 + 2 * jb
            for u in range(ncommon // 2):
                units.append((jb, [2 * u, 2 * u + 1], False, u == 0))
            units.append((jb, [32 + 2 * jb, 32 + 2 * jb + 1], True, False))

        def emit_S(u, s, b=b):
            jb, kts, diag, first = u
            for j, kt in enumerate(kts):
                if diag and j == 1:
                    q0, nq, c0 = jb * 256 + 128, 128, 256
                else:
                    q0, nq, c0 = jb * 256, 256, j * 256
                P.op("pe", lambda e, s=s, kt=kt, q0=q0, nq=nq, c0=c0: e.matmul(psS[s][:, c0:c0 + nq], lhsT=kaug[b][0:96, kt * 128:(kt + 1) * 128],
                                                                             rhs=qaug[b][0:96, q0:q0 + nq], start=True, stop=True),
                     reads=[d_kaug[b], d_qaug[b], d_qmb[b]], writes=[d_psS[s]])

        def emit_rest(u, s, b=b, h=h):
            jb, kts, diag, first = u
            ncols = 384 if diag else 512
            P.op("act", lambda e, s=s, ncols=ncols: e.activation(out=pt[s][:, 0:ncols], in_=psS[s][:, 0:ncols], func=AF.Exp),
                 reads=[d_psS[s]], writes=[d_pt[s]])
            if diag:
                P.op("pool", lambda e, s=s: e.tensor_tensor(out=pt[s][:, 0:128], in0=pt[s][:, 0:128], in1=tri[:], op=ALU.mult),
                     reads=[d_pt[s], d_c], writes=[d_pt[s]])
                P.op("pool", lambda e, s=s: e.tensor_tensor(out=pt[s][:, 256:384], in0=pt[s][:, 256:384], in1=tri[:], op=ALU.mult),
                     reads=[d_pt[s], d_c], writes=[d_pt[s]])
            for j, kt in enumerate(kts):
                if diag and j == 1:
                    pv = [(1, 256, True)]
                elif diag:
                    pv = [(0, 0, True), (1, 128, False)]
                else:
                    pv = [(0, j * 256, False), (1, j * 256 + 128, False)]
                st_ = first and j == 0
                for qi, col0, last in pv:
                    P.op("pe", lambda e, s=s, kt=kt, qi=qi, col0=col0, st_=st_, last=last: e.matmul(
                        po[qi][:, :], lhsT=pt[s][:, col0:col0 + 128], rhs=vaug[b][:, kt, :], start=st_, stop=last),
                        reads=[d_pt[s], d_vaug[b]], writes=[d_po[qi]])
            if diag:
                for qi in range(2):
                    qt = jb * 2 + qi
                    P.op("dve", lambda e, qi=qi: e.reciprocal(out=rc[:, qi:qi + 1], in_=po[qi][:, 64:65]), reads=[d_po[qi]], writes=[d_rc[qi]])
                    P.op("dve", lambda e, qi=qi, qt=qt: e.tensor_scalar(out=oa[:, qt, h * 64:(h + 1) * 64], in0=po[qi][:, 0:64],
                                                                       scalar1=rc[:, qi:qi + 1], scalar2=None, op0=ALU.mult),
                         reads=[d_po[qi], d_rc[qi]], writes=[d_oa])
        SK = 2
        n = len(units)
        for i in range(n + SK):
            if i < n:
                emit_S(units[i], i % NPT)
            if i >= SK:
                emit_rest(units[i - SK], (i - SK) % NPT)


def host_consts(half):
    pv = 1.0 if half == 1 else 0.0
    V = np.zeros((32, 32), np.float32); O = np.zeros((32, 32), np.float32)
    for qt in range(32):
        jb = qt // 2
        V[qt, :16] = pv
        V[qt, 16:16 + jb] = 1.0
        O[qt, 16 + jb] = 1.0
    NBm = (V - 1.0) * BIGG
    E = np.zeros((32, NK), np.float32)
    for b in range(32):
        E[b, b * 256:(b + 1) * 256] = 1.0
    tri = (np.arange(128)[:, None] <= np.arange(128)[None, :]).astype(np.float32)
    return dict(vq=V.reshape(1, 1024), nb=NBm.reshape(1, 1024), oq=O.reshape(1, 1024),
                e_d=E.astype(ml_dtypes.bfloat16), tri=tri.astype(ml_dtypes.bfloat16))


def phase_c(nc, P, st, xin, w_in, g_mix, cwl_d, cbl_d, dtb_d, alog_d, dsk_d, ssdn_d, tri2_d, sel_d, tb_d, pflag_d,
            idb, d_idb, os_d, d_osd, chunks=range(32)):
    def sb(name, shape, dt):
        return st.enter_context(nc.sbuf_tensor("c_" + name, shape, dt))

    def ps(name, shape, dt=F32):
        return st.enter_context(nc.psum_tensor(name, shape, dt))
    wz = sb("wz", [128, 8, 1024], BF16); d_w = Dep()
    wx = sb("wx", [128, 8, 2048], BF16)
    wdt = sb("wdt", [128, 8, 16], BF16)
    gm = sb("gmc", [128, D], F32); d_c = Dep()
    cw = sb("cw", [128, 16, 4], F32)
    cb = sb("cb", [128, 16], F32)
    dtb = sb("dtb", [128, 2, 16], F32)
    Aneg = sb("Aneg", [128, 2, 16], F32); d_A = Dep()
    dsk = sb("dsk", [128, 16], F32)
    ssdn = sb("ssdn", [128, D], F32)
    tri2 = sb("tri2", [128, 2, 256], F32)
    onesf = sb("onesf", [128, 128], F32)
    sel = sb("selc", [16, 16, 128], F32)
    tb = sb("tbc", [128, 384], F32)
    pflag = sb("pflag", [128, 1], F32)
    xt_ = [sb("xtc%d" % i, [128, 2, D], F32) for i in range(2)]; d_xt_ = [Dep(), Dep()]
    junk = sb("junkc", [128, D], BF16); d_junk = Dep()
    ssq = sb("ssqc", [128, 2], F32); d_ssq = Dep()
    rstd = sb("rstdc", [128, 2], F32); d_rstd = Dep()
    hb_ = [sb("hbc%d" % i, [128, 2, D], BF16) for i in range(2)]; d_hb_ = [Dep(), Dep()]
    hT = sb("hTc", [128, 8, 256], BF16); d_hT = Dep()
    xraw = sb("xraw", [128, 16, 259], F32); d_xraw = Dep(); d_halo = Dep()
    acc = sb("acc", [128, 8, 256], F32); d_acc = [Dep() for _ in range(8)]
    xc = sb("xc", [128, 16, 256], BF16); d_xc = [Dep() for _ in range(16)]
    xtok = sb("xtok", [128, 2, 1024], BF16); d_xtok = Dep()
    btok = sb("btok", [128, 2, 512], BF16); d_btok = Dep()
    dtr = sb("dtr", [128, 2, 16], F32); d_dtr = Dep()
    dt = sb("dt", [128, 2, 16], F32); d_dt = Dep()
    aa = sb("aa", [128, 2, 16], F32); d_aa = Dep()
    acum = sb("acum", [128, 2, 16], F32); d_acum = Dep()
    nacum = sb("nacum", [128, 2, 16], F32); d_nacum = Dep()
    acumT = sb("acumT", [16, 256], F32); d_acumT = Dep()
    tot = sb("tot", [128, 16], F32); d_tot = Dep()
    cdec = sb("cdec", [128, 16], F32); d_cdec = Dep()
    dte = sb("dte", [128, 2, 16], F32); d_dte = Dep()
    eac = sb("eac", [128, 2, 16], F32); d_eac = Dep()
    w2 = sb("w2", [128, 2, 16], F32); d_w2 = Dep()
    xdt = sb("xdt", [128, 2, 1024], BF16); d_xdt = Dep()
    xdd = sb("xdd", [128, 2, 1024], BF16); d_xdd = Dep()
    zs = sb("zs", [128, 2, 1024], BF16); d_zs = Dep()
    cbT = sb("cbT", [128, 4, 384], F32); d_cbT = [Dep() for _ in range(4)]
    arg = [sb("arg%d" % i, [128, 384], F32) for i in range(2)]; d_arg = [Dep(), Dep()]
    Lt = [sb("Lt%d" % i, [128, 384], F32) for i in range(2)]; d_Lt = [Dep(), Dep()]
    Mt = [sb("Mt%d" % i, [128, 384], BF16) for i in range(2)]; d_Mt = [Dep() for _ in range(2)]
    stf = sb("stf", [128, 4, 256], F32); d_stf = Dep()
    stT = sb("stT", [128, 4, 256], BF16); d_stT = Dep()
    yt = [sb("yt%d" % i, [128, 256], F32) for i in range(2)]; d_yt = [Dep(), Dep()]
    y2 = [sb("y2%d" % i, [128, 256], F32) for i in range(2)]; d_y2 = [Dep(), Dep()]
    gss = [sb("gss%d" % i, [128, 1], F32) for i in range(2)]; d_gss = [Dep(), Dep()]
    grs = [sb("grs%d" % i, [128, 1], F32) for i in range(2)]; d_grs = [Dep(), Dep()]
    osb = sb("osb", [128, 2, 1024], BF16); d_osb = Dep()

    B = [ps("bk%d" % i, [128, 512], F32) for i in range(7)]
    d_B = [Dep() for _ in range(7)]
    d_B1h = [Dep(), Dep()]
    d_B5h = [Dep(), Dep()]
    pT = ps("pTc", [128, 8, 128], BF16); d_pT = Dep()

    for k in range(8):
        rows = slice(k * 128, (k + 1) * 128)
        P.dma("pool", lambda e, k=k, rows=rows: e.dma_start(out=wz[:, k, :], in_=w_in[rows, C_Z:C_Z + 1024]), writes=[d_w])
        P.dma("pool", lambda e, k=k, rows=rows: e.dma_start(out=wx[:, k, :], in_=w_in[rows, C_X:C_X + 2048]), writes=[d_w])
        P.dma("pool", lambda e, k=k, rows=rows: e.dma_start(out=wdt[:, k, :], in_=w_in[rows, C_DT:C_DT + 16]), writes=[d_w])
    P.dma("sp", lambda e: e.dma_start(out=gm[:], in_=g_mix[0:1, :].partition_broadcast(128)), writes=[d_c])
    P.dma("sp", lambda e: e.dma_start(out=cw[:], in_=cwl_d[:, :, :]), writes=[d_c])
    P.dma("sp", lambda e: e.dma_start(out=cb[:], in_=cbl_d[:, :]), writes=[d_c])
    for i in range(2):
        P.dma("sp", lambda e, i=i: e.dma_start(out=dtb[:, i, :], in_=dtb_d[0:1, :].partition_broadcast(128)), writes=[d_c])
        P.dma("sp", lambda e, i=i: e.dma_start(out=Aneg[:, i, :], in_=alog_d[0:1, :].partition_broadcast(128)), writes=[d_A])
    P.dma("sp", lambda e: e.dma_start(out=dsk[:], in_=dsk_d[0:1, :].partition_broadcast(128)), writes=[d_c])
    P.dma("sp", lambda e: e.dma_start(out=ssdn[:], in_=ssdn_d[0:1, :].partition_broadcast(128)), writes=[d_c])
    P.dma("sp", lambda e: e.dma_start(out=tri2[:], in_=tri2_d[:, :, :]), writes=[d_c])
    P.dma("sp", lambda e: e.dma_start(out=sel[:], in_=sel_d[:, :, :]), writes=[d_c])
    P.dma("sp", lambda e: e.dma_start(out=tb[:], in_=tb_d[:, :]), writes=[d_c])
    P.dma("sp", lambda e: e.dma_start(out=pflag[:], in_=pflag_d[0:1, :].partition_broadcast(128)), writes=[d_c])
    P.op("pool", lambda e: e.memset(onesf[:], 1.0), writes=[d_c])
    P.op("act", lambda e: e.activation(out=Aneg[:], in_=Aneg[:], func=AF.Exp), reads=[d_A], writes=[d_A])
    P.op("dve", lambda e: e.tensor_scalar(out=Aneg[:], in0=Aneg[:], scalar1=-1.0, scalar2=None, op0=ALU.mult), reads=[d_A], writes=[d_A])
    P.op("pool", lambda e: e.memset(xraw[:], 0.0), writes=[d_xraw, d_halo])
    P.op("pool", lambda e: e.memset(stf[:], 0.0), writes=[d_stf])
    P.op("pool", lambda e: e.memset(stT[:], 0.0), writes=[d_stT])

    h3 = lambda ap: ap.rearrange("p (h d) -> p h d", d=64)

    chunks = list(chunks)

    def s1a(cj, cc):
        pj = cj % 2
        rr = cc * 256
        P.dma("sp", lambda e: e.dma_start(out=xt_[pj][:], in_=xin[rr:rr + 256, :].rearrange("(t p) d -> p t d", p=128)), writes=[d_xt_[pj]])
        for it in range(2):
            P.op("act", lambda e, it=it: e.activation(out=junk[:], in_=xt_[pj][:, it, :], func=AF.Square, accum_out=ssq[:, it:it + 1]),
                 reads=[d_xt_[pj]], writes=[d_junk, d_ssq])
        P.op("act", lambda e: e.activation(out=rstd[:], in_=ssq[:], func=AF.Sqrt, bias=EPS, scale=1.0 / D), reads=[d_ssq], writes=[d_rstd])
        P.op("dve", lambda e: e.reciprocal(out=rstd[:], in_=rstd[:]), reads=[d_rstd], writes=[d_rstd])
        for it in range(2):
            P.op("dve", lambda e, it=it: e.scalar_tensor_tensor(out=hb_[pj][:, it, :], in0=xt_[pj][:, it, :], scalar=rstd[:, it:it + 1], in1=gm[:],
                                                                op0=ALU.mult, op1=ALU.mult),
                 reads=[d_xt_[pj], d_rstd, d_c], writes=[d_hb_[pj]])

    for ci, c in enumerate(chunks):
        own = c >= 16
        r0 = c * 256
        if ci == 0:
            s1a(0, c)
        pp = ci % 2
        for it in range(2):
            for k in range(8):
                P.op("pe", lambda e, it=it, k=k, pp=pp: e.transpose(out=pT[:, k, :], in_=hb_[pp][:, it, k * 128:(k + 1) * 128], identity=idb[:]),
                     reads=[d_hb_[pp], d_idb], writes=[d_pT])
            P.op("act", lambda e, it=it: e.copy(out=hT[:, :, it * 128:(it + 1) * 128], in_=pT[:]), reads=[d_pT], writes=[d_hT])
        for it in range(2):
            for k in range(8):
                P.op("pe", lambda e, it=it, k=k: e.matmul(B[3][:, it * 16:(it + 1) * 16], lhsT=hT[:, k, it * 128:(it + 1) * 128], rhs=wdt[:, k, :],
                                                         start=(k == 0), stop=(k == 7)), reads=[d_hT, d_w], writes=[d_B[3]])
        P.op("dve", lambda e: e.tensor_tensor(out=dtr[:].rearrange("p a b -> p (a b)"), in0=B[3][:, 0:32], in1=dtb[:].rearrange("p a b -> p (a b)"), op=ALU.add),
             reads=[d_B[3], d_c], writes=[d_dtr])
        P.op("act", lambda e: e.activation(out=dtr[:], in_=dtr[:], func=AF.Exp), reads=[d_dtr], writes=[d_dtr])
        P.op("act", lambda e: e.activation(out=dt[:], in_=dtr[:], func=AF.Ln, bias=1.0), reads=[d_dtr], writes=[d_dt])
        P.op("dve", lambda e: e.tensor_tensor(out=aa[:], in0=dt[:], in1=Aneg[:], op=ALU.mult), reads=[d_dt, d_A], writes=[d_aa])
        nfc = 16 if (own or c == 15) else 12
        for fc in range(nfc):
            bi = 1 if fc % 2 == 0 else 4
            for k in range(8):
                P.op("pe", lambda e, fc=fc, k=k, bi=bi: e.matmul(B[bi][:, 0:256], lhsT=wx[:, k, fc * 128:(fc + 1) * 128], rhs=hT[:, k, :],
                                                               start=(k == 0), stop=(k == 7)), reads=[d_hT, d_w], writes=[d_B[bi]])
            P.op("act", lambda e, fc=fc, bi=bi: e.copy(out=xraw[:, fc, 3:259], in_=B[bi][:, 0:256]),
                 reads=[d_B[bi]], writes=[d_xraw])
        for it in range(2):
            for jt in range(it + 1):
                P.op("pe", lambda e, it=it, jt=jt: e.matmul(B[3][:, 32 + it * 16:32 + (it + 1) * 16], lhsT=tri2[:, jt, it * 128:(it + 1) * 128],
                                                           rhs=aa[:, jt, :], start=(jt == 0), stop=(jt == it)),
                     reads=[d_aa, d_c], writes=[d_B[3]])
        for jt in range(2):
            P.op("pe", lambda e, jt=jt: e.matmul(B[3][:, 64:80], lhsT=onesf[:], rhs=aa[:, jt, :], start=(jt == 0), stop=(jt == 1)),
                 reads=[d_aa, d_c], writes=[d_B[3]])
        for jt in range(2):
            P.op("pe", lambda e, jt=jt: e.matmul(B[3][0:16, 128:384], lhsT=aa[:, jt, :], rhs=tri2[:, jt, :], start=(jt == 0), stop=(jt == 1)),
                 reads=[d_aa, d_c], writes=[d_B[3]])
        P.op("act", lambda e: e.copy(out=acum[:].rearrange("p a b -> p (a b)"), in_=B[3][:, 32:64]), reads=[d_B[3]], writes=[d_acum])
        P.op("dve", lambda e: e.tensor_scalar(out=nacum[:].rearrange("p a b -> p (a b)"), in0=B[3][:, 32:64], scalar1=-1.0, scalar2=None, op0=ALU.mult),
             reads=[d_B[3]], writes=[d_nacum])
        P.op("act", lambda e: e.copy(out=tot[:], in_=B[3][:, 64:80]), reads=[d_B[3]], writes=[d_tot])
        P.op("act", lambda e: e.copy(out=acumT[:], in_=B[3][0:16, 128:384]), reads=[d_B[3]], writes=[d_acumT])
        P.op("act", lambda e: e.activation(out=cdec[:], in_=tot[:], func=AF.Exp), reads=[d_tot], writes=[d_cdec])
        P.op("dve", lambda e: e.tensor_tensor(out=dte[:], in0=nacum[:], in1=tot[:].unsqueeze(1).to_broadcast([128, 2, 16]), op=ALU.add),
             reads=[d_nacum, d_tot], writes=[d_dte])
        P.op("act", lambda e: e.activation(out=dte[:], in_=dte[:], func=AF.Exp), reads=[d_dte], writes=[d_dte])
        P.op("act", lambda e: e.activation(out=eac[:], in_=acum[:], func=AF.Exp), reads=[d_acum], writes=[d_eac])
        P.op("dve", lambda e: e.tensor_tensor(out=w2[:], in0=dt[:], in1=dte[:], op=ALU.mult), reads=[d_dt, d_dte], writes=[d_w2])
        if own:
            for it in range(2):
                for hv in range(2):
                    zb = 2 if hv == 0 else 6
                    for k in range(8):
                        P.op("pe", lambda e, it=it, hv=hv, k=k, zb=zb: e.matmul(B[zb][:, :], lhsT=hT[:, k, it * 128:(it + 1) * 128], rhs=wz[:, k, hv * 512:(hv + 1) * 512],
                                                                              start=(k == 0), stop=(k == 7)), reads=[d_hT, d_w], writes=[d_B[zb]])
                    P.op("act", lambda e, it=it, hv=hv, zb=zb: e.activation(out=zs[:, it, hv * 512:(hv + 1) * 512], in_=B[zb][:, :], func=AF.Silu),
                         reads=[d_B[zb]], writes=[d_zs])
        for half in range(2):
            n8 = 8 if (half == 0 or nfc == 16) else 4
            for f8 in range(n8):
                fc = half * 8 + f8
                P.op("dve", lambda e, fc=fc, f8=f8: e.tensor_scalar(out=acc[:, f8, :], in0=xraw[:, fc, 0:256], scalar1=cw[:, fc, 0:1], scalar2=cb[:, fc:fc + 1],
                                                                   op0=ALU.mult, op1=ALU.add), reads=[d_xraw, d_halo, d_c], writes=[d_acc[f8]])
            for kk in range(1, 4):
                for f8 in range(n8):
                    fc = half * 8 + f8
                    P.op("dve", lambda e, fc=fc, f8=f8, kk=kk: e.scalar_tensor_tensor(out=acc[:, f8, :], in0=xraw[:, fc, kk:kk + 256], scalar=cw[:, fc, kk:kk + 1],
                                                                                     in1=acc[:, f8, :], op0=ALU.mult, op1=ALU.add),
                         reads=[d_xraw, d_halo, d_c, d_acc[f8]], writes=[d_acc[f8]])
            for f8 in range(n8):
                fc = half * 8 + f8
                P.op("act", lambda e, fc=fc, f8=f8: e.activation(out=xc[:, fc, :], in_=acc[:, f8, :], func=AF.Silu),
                     reads=[d_acc[f8]], writes=[d_xc[fc]])
        P.op("pool", lambda e: e.tensor_copy(out=xraw[:, :, 0:3], in_=xraw[:, :, 256:259]), reads=[d_xraw], writes=[d_halo])
        for it in range(2):
            for fc in range(8):
                P.op("pe", lambda e, it=it, fc=fc: e.transpose(out=pT[:, fc, :], in_=xc[:, fc, it * 128:(it + 1) * 128], identity=idb[:]),
                     reads=[d_xc[fc], d_idb], writes=[d_pT])
            P.op("act", lambda e, it=it: e.copy(out=xtok[:, it, :], in_=pT[:]), reads=[d_pT], writes=[d_xtok])
            for g in range(4):
                P.op("pe", lambda e, it=it, g=g: e.transpose(out=pT[:, g, :], in_=xc[:, 8 + g, it * 128:(it + 1) * 128], identity=idb[:]),
                     reads=[d_xc[8 + g], d_idb], writes=[d_pT])
            P.op("act", lambda e, it=it: e.copy(out=btok[:, it, :], in_=pT[:, 0:4, :]), reads=[d_pT], writes=[d_btok])
        for it in range(2):
            if own:
                P.op("dve", lambda e, it=it: e.tensor_tensor(out=h3(xdt[:, it, :]), in0=h3(xtok[:, it, :]),
                                                             in1=dt[:, it, :].unsqueeze(2).to_broadcast([128, 16, 64]), op=ALU.mult),
                     reads=[d_xtok, d_dt], writes=[d_xdt])
            P.op("pool", lambda e, it=it: e.tensor_tensor(out=h3(xdd[:, it, :]), in0=h3(xtok[:, it, :]),
                                                          in1=w2[:, it, :].unsqueeze(2).to_broadcast([128, 16, 64]), op=ALU.mult),
                 reads=[d_xtok, d_w2], writes=[d_xdd])
        if ci + 1 < len(chunks):
            s1a(ci + 1, chunks[ci + 1])
        if own:
            for g in range(4):
                P.op("pe", lambda e, g=g: e.matmul(B[4][:, 0:256], lhsT=xc[:, 8 + g, 0:128], rhs=xc[:, 12 + g, :], start=True, stop=True),
                     reads=[d_xc[8 + g], d_xc[12 + g]], writes=[d_B[4]])
                P.op("pe", lambda e, g=g: e.matmul(B[4][:, 256:384], lhsT=xc[:, 8 + g, 128:256], rhs=xc[:, 12 + g, 128:256], start=True, stop=True),
                     reads=[d_xc[8 + g], d_xc[12 + g]], writes=[d_B[4]])
                P.op("act", lambda e, g=g: e.copy(out=cbT[:, g, :], in_=B[4][:, 0:384]), reads=[d_B[4]], writes=[d_cbT[g]])
            for g in range(4):
                def stA(h, g=g):
                    mi = h % 2
                    bk = 5 if mi == 0 else 4
                    P.op("pe", lambda e: e.matmul(B[bk][:, 0:256], lhsT=sel[:, h, :], rhs=acumT[:, :], start=True, stop=True),
                         reads=[d_c, d_acumT], writes=[d_B[bk]])
                    P.op("dve", lambda e: e.scalar_tensor_tensor(out=arg[mi][:, 0:256], in0=B[bk][:, 0:256], scalar=nacum[:, 0, h:h + 1], in1=tb[:, 0:256],
                                                                 op0=ALU.add, op1=ALU.add), reads=[d_B[bk], d_nacum, d_c], writes=[d_arg[mi]])
                    P.op("dve", lambda e: e.scalar_tensor_tensor(out=arg[mi][:, 256:384], in0=B[bk][:, 128:256], scalar=nacum[:, 1, h:h + 1], in1=tb[:, 256:384],
                                                                 op0=ALU.add, op1=ALU.add), reads=[d_B[bk], d_nacum, d_c], writes=[d_arg[mi]])
                    P.op("act", lambda e: e.activation(out=Lt[mi][:], in_=arg[mi][:], func=AF.Exp), reads=[d_arg[mi]], writes=[d_Lt[mi]])
                    P.op("pool", lambda e: e.tensor_tensor(out=Mt[mi][:], in0=Lt[mi][:], in1=cbT[:, g, :], op=ALU.mult),
                         reads=[d_Lt[mi], d_cbT[g]], writes=[d_Mt[mi]])

                def stB(h, g=g):
                    mi = h % 2
                    r = h % 4
                    cs_ = slice(r * 64, (r + 1) * 64)
                    hs_ = slice(h * 64, (h + 1) * 64)
                    P.op("pe", lambda e: e.matmul(B[6][:, 0:256][:, cs_], lhsT=Mt[mi][:, 0:128], rhs=xdt[:, 0, hs_], start=True, stop=True),
                         reads=[d_Mt[mi], d_xdt], writes=[d_B[6]])
                    P.op("pe", lambda e: e.matmul(B[6][:, 256:512][:, cs_], lhsT=Mt[mi][:, 128:256], rhs=xdt[:, 0, hs_], start=True, stop=False),
                         reads=[d_Mt[mi], d_xdt], writes=[d_B[6]])
                    P.op("pe", lambda e: e.matmul(B[6][:, 256:512][:, cs_], lhsT=Mt[mi][:, 256:384], rhs=xdt[:, 1, hs_], start=False, stop=True),
                         reads=[d_Mt[mi], d_xdt], writes=[d_B[6]])
                hs4 = [g * 4 + r for r in range(4)]
                stA(hs4[0]); stA(hs4[1]); stB(hs4[0]); stA(hs4[2]); stB(hs4[1]); stA(hs4[3]); stB(hs4[2]); stB(hs4[3])
                gs_ = slice(g * 256, (g + 1) * 256)
                for it in range(2):
                    P.op("pe", lambda e, g=g, it=it: e.matmul(B[0][:, it * 256:(it + 1) * 256], lhsT=xc[:, 12 + g, it * 128:(it + 1) * 128], rhs=stT[:, g, :],
                                                             start=True, stop=True), reads=[d_xc[12 + g], d_stT], writes=[d_B[0]])
                v4 = lambda ap: ap.rearrange("p (r d) -> p r d", d=64)

                def ystep(step, it, g=g, gs_=gs_):
                    if step == 0:
                        P.op("dve", lambda e: e.tensor_tensor(out=v4(yt[it][:]), in0=v4(B[0][:, it * 256:(it + 1) * 256]),
                                                              in1=eac[:, it, g * 4:(g + 1) * 4].unsqueeze(2).to_broadcast([128, 4, 64]), op=ALU.mult),
                             reads=[d_B[0], d_eac], writes=[d_yt[it]])
                    elif step == 1:
                        P.op("dve", lambda e: e.tensor_tensor(out=yt[it][:], in0=yt[it][:], in1=B[6][:, it * 256:(it + 1) * 256], op=ALU.add),
                             reads=[d_yt[it], d_B[6]], writes=[d_yt[it]])
                    elif step == 2:
                        P.op("pool", lambda e: e.tensor_tensor(out=v4(y2[it][:]), in0=v4(xtok[:, it, gs_]),
                                                               in1=dsk[:, g * 4:(g + 1) * 4].unsqueeze(2).to_broadcast([128, 4, 64]), op=ALU.mult),
                             reads=[d_xtok, d_c], writes=[d_y2[it]])
                    elif step == 3:
                        P.op("dve", lambda e: e.tensor_tensor(out=yt[it][:], in0=yt[it][:], in1=y2[it][:], op=ALU.add), reads=[d_yt[it], d_y2[it]], writes=[d_yt[it]])
                    elif step == 4:
                        P.op("dve", lambda e: e.tensor_tensor(out=yt[it][:], in0=yt[it][:], in1=zs[:, it, gs_], op=ALU.mult), reads=[d_yt[it], d_zs], writes=[d_yt[it]])
                    elif step == 5:
                        P.op("act", lambda e: e.activation(out=y2[it][:], in_=yt[it][:], func=AF.Square, accum_out=gss[it][:]), reads=[d_yt[it], d_y2[it]], writes=[d_y2[it], d_gss[it]])
                    elif step == 6:
                        P.op("act", lambda e: e.activation(out=grs[it][:], in_=gss[it][:], func=AF.Sqrt, bias=EPS, scale=1.0 / 256), reads=[d_gss[it]], writes=[d_grs[it]])
                    elif step == 7:
                        P.op("dve", lambda e: e.reciprocal(out=grs[it][:], in_=grs[it][:]), reads=[d_grs[it]], writes=[d_grs[it]])
                    elif step == 8:
                        P.op("dve", lambda e: e.scalar_tensor_tensor(out=osb[:, it, gs_], in0=yt[it][:], scalar=grs[it][:, 0:1], in1=ssdn[:, gs_],
                                                                     op0=ALU.mult, op1=ALU.mult), reads=[d_yt[it], d_grs[it], d_c], writes=[d_osb])
                for step in range(9):
                    for it in range(2):
                        ystep(step, it)
            P.dma("sp", lambda e, r0=r0: e.dma_start(out=os_d[r0 - TP:r0 - TP + 256, :].rearrange("(t p) c -> p t c", p=128), in_=osb[:]),
                  reads=[d_osb], writes=[d_osd])
        for g in range(4):
            bank = B[2] if g < 2 else B[1]
            dbank = d_B[2] if g < 2 else d_B[1]
            cs_ = slice((g % 2) * 256, (g % 2 + 1) * 256)
            for jt in range(2):
                P.op("pe", lambda e, g=g, jt=jt, bank=bank, cs_=cs_: e.matmul(bank[:, cs_], lhsT=btok[:, jt, g * 128:(g + 1) * 128], rhs=xdd[:, jt, g * 256:(g + 1) * 256],
                                                                            start=(jt == 0), stop=(jt == 1)), reads=[d_btok, d_xdd], writes=[dbank])
        v16 = lambda ap: ap.rearrange("p (h d) -> p h d", d=64)
        P.op("dve", lambda e: e.tensor_tensor(out=v16(stf[:].rearrange("p g c -> p (g c)")), in0=v16(stf[:].rearrange("p g c -> p (g c)")),
                                              in1=cdec[:].unsqueeze(2).to_broadcast([128, 16, 64]), op=ALU.mult), reads=[d_stf, d_cdec], writes=[d_stf])
        P.op("dve", lambda e: e.tensor_tensor(out=stf[:, 0:2, :].rearrange("p g c -> p (g c)"), in0=stf[:, 0:2, :].rearrange("p g c -> p (g c)"), in1=B[2][:, :], op=ALU.add),
             reads=[d_stf, d_B[2]], writes=[d_stf])
        P.op("dve", lambda e: e.tensor_tensor(out=stf[:, 2:4, :].rearrange("p g c -> p (g c)"), in0=stf[:, 2:4, :].rearrange("p g c -> p (g c)"), in1=B[1][:, :], op=ALU.add),
             reads=[d_stf, d_B[1]], writes=[d_stf])
        if c == 15:
            P.op("dve", lambda e: e.tensor_scalar(out=stf[:], in0=stf[:], scalar1=pflag[:, 0:1], scalar2=None, op0=ALU.mult), reads=[d_stf, d_c], writes=[d_stf])
        P.op("act", lambda e: e.copy(out=stT[:], in_=stf[:]), reads=[d_stf], writes=[d_stT])


def host_consts_c(z, half):
    cwl = np.ascontiguousarray(z['conv_w'].reshape(4, 16, 128).transpose(2, 1, 0)).astype(np.float32)
    cbl = np.ascontiguousarray(z['conv_b'].reshape(16, 128).T).astype(np.float32)
    tri = (np.arange(128)[:, None] <= np.arange(128)[None, :]).astype(np.float32)
    tri2 = np.zeros((128, 2, 256), np.float32)
    tri2[:, 0, 0:128] = tri; tri2[:, 0, 128:256] = 1.0; tri2[:, 1, 128:256] = tri
    sel = np.zeros((16, 16, 128), np.float32)
    for h in range(16):
        sel[h, h, :] = 1.0
    tbias = np.where(tri > 0, 0.0, NEG).astype(np.float32)
    tb = np.zeros((128, 384), np.float32)
    tb[:, 0:128] = tbias; tb[:, 256:384] = tbias
    return dict(cwl=cwl, cbl=cbl, dtb=z['dt_bias'][None, :], alog=z['a_log'][None, :], dsk=z['d_skip'][None, :], ssdn=z['ssd_norm'][None, :],
                tri2=tri2, sel_d=sel, tb_d=tb, pflag=np.array([[1.0 if half == 1 else 0.0]], np.float32))


def phase_def(nc, P, xown, w_in, g_mix, mem_d, gmem_d, wmemkv, mqn_d, mkn_d, woa_d, wos_d, wom_d, wout_d, gffn_d, wr_d,
              wgate_d, wup_d, wdown_d, tri_d, ecap_d, oa_d, d_oad, os_d, d_osd, x1_d, xbuf, ybuf, out_d, idb, d_idb, idf, d_idf,
              ntiles=32, experts=range(32), stage=3):
    d_x1d = Dep(); d_xbuf = Dep(); d_ybuf = Dep(); d_out = Dep()
    with contextlib.ExitStack() as stp:
        def sbp(name, shape, dt):
            return stp.enter_context(nc.sbuf_tensor("d_" + name, shape, dt))
        combs = sbp("combs", [128, 32, 2], F32); d_combs = Dep()
        idxs = sbp("idxs", [128, 32, 2], I32); d_idxs = Dep()

        with contextlib.ExitStack() as st:
            def sb(name, shape, dt):
                return st.enter_context(nc.sbuf_tensor("d_" + name, shape, dt))

            def ps(name, shape, dt=F32):
                return st.enter_context(nc.psum_tensor(name, shape, dt))
            d_w = Dep(); d_c = Dep()
            gm = sb("gmd", [128, D], F32)
            gf = sb("gfd", [128, D], F32)
            mqn = sb("mqn", [128, 512], F32)
            mkn = sb("mkn", [128, 512], F32)
            tri = sb("trid", [128, 128], F32)
            onesf = sb("onesfd", [128, 128], F32)
            onesb = sb("onesbd", [128, 128], BF16)
            ecap = sb("ecap", [128, 32], F32)
            base = sb("base", [128, 32], F32); d_base = Dep()
            kmT = sb("kmT", [128, 4, 256], BF16); d_kmT = Dep()
            vm = sb("vm", [128, 2, 512], BF16); d_vm = Dep()

            xt = [sb("xtd%d" % i, [128, D], F32) for i in range(2)]; d_xt = [Dep(), Dep()]
            junk = sb("junkd", [128, D], BF16); d_junk = Dep()
            ssq = sb("ssqd", [128, 1], F32); d_ssq = Dep()
            rstd = sb("rstdd", [128, 1], F32); d_rstd = Dep()
            hb_ = [sb("hbd%d" % i, [128, D], BF16) for i in range(2)]; d_hb_ = [Dep(), Dep()]
            ssqn = sb("ssqn", [128, 1], F32); d_ssqn = Dep()
            rstdn = sb("rstdn", [128, 1], F32); d_rstdn = Dep()
            hT = sb("hTd", [128, 8, 128], BF16); d_hT = Dep()
            sq = sb("sqd", [128, 512], F32); d_sq = Dep()
            hs = sb("hsd", [128, 4], F32); d_hs = Dep()
            hr = sb("hrd", [128, 4], F32); d_hr = Dep()
            qmn = sb("qmn", [128, 512], F32); d_qmn = Dep()
            qmb = sb("qmb", [128, 512], BF16); d_qmb = Dep()
            qmT = sb("qmT", [128, 4, 128], BF16); d_qmT = Dep()
            pm = sb("pm", [128, 4, 2, 128], BF16); d_pm = Dep()
            rcp = sb("rcp", [128, 512], F32); d_rcp = Dep()
            omT = sb("omT", [128, 4, 128], BF16); d_omT = Dep()
            gs = sb("gs", [128, 3072], F32); d_gs = Dep()
            oat = [sb("oat%d" % i, [128, 512], BF16) for i in range(2)]; d_oat = [Dep(), Dep()]
            ost = [sb("ost%d" % i, [128, 1024], BF16) for i in range(2)]; d_ost = [Dep(), Dep()]
            oaT = sb("oaT", [128, 4, 128], BF16); d_oaT = Dep()
            osT = sb("osT", [128, 8, 128], BF16); d_osT = Dep()
            mg = sb("mg", [128, 512], F32); d_mg = Dep()
            tt = sb("tt", [128, 512], F32); d_tt = Dep()
            mgb = sb("mgb", [128, D], BF16); d_mgb = Dep()
            mgT = sb("mgT", [128, 8, 128], BF16); d_mgT = Dep()
            x1 = sb("x1", [128, D], F32); d_x1 = Dep()
            h2f = sb("h2f", [128, D], F32); d_h2f = Dep()
            h2b = [sb("h2b%d" % i, [128, D], BF16) for i in range(2)]; d_h2b = [Dep() for _ in range(2)]
            h2T = sb("h2T", [128, 8, 128], F32); d_h2T = Dep()
            lg = sb("lg", [128, 36], F32); d_lg = Dep()
            sm = sb("sm", [128, 64], F32); d_sm = Dep()
            goh = sb("goh", [128, 4], F32); d_goh = Dep()
            lsel = sb("lsel", [128, 32], F32); d_lsel = Dep()
            les = sb("les", [128, 8], F32); d_les = Dep()
            t8 = sb("t8", [128, 8], F32); d_t8 = Dep()
            oh = sb("oh", [128, 64], F32); d_oh = Dep()
            ohs = sb("ohs", [128, 32], F32); d_ohs = Dep()
            posf = sb("posf", [128, 32], F32); d_posf = Dep()
            idf2 = sb("idf2", [128, 2], F32); d_idf2 = Dep()

            pT = ps("pTd", [128, 8, 128], BF16); d_pT = Dep()
            pF = ps("pFd", [128, 512], F32); d_pF = Dep()
            pG = [ps("pGd%d" % i, [128, 512], F32) for i in range(3)]; d_pG = [Dep() for _ in range(3)]
            pms = [ps("pmsd%d" % i, [128, 512], F32) for i in range(2)]; d_pms = [Dep() for _ in range(2)]
            pmo = ps("pmod", [128, 512], F32); d_pmo = Dep()

            P.dma("sp", lambda e: e.dma_start(out=gm[:], in_=g_mix[0:1, :].partition_broadcast(128)), writes=[d_c])
            P.dma("sp", lambda e: e.dma_start(out=gf[:], in_=gffn_d[0:1, :].partition_broadcast(128)), writes=[d_c])
            P.dma("sp", lambda e: e.dma_start(out=mqn[:], in_=mqn_d[0:1, :].partition_broadcast(128)), writes=[d_c])
            P.dma("sp", lambda e: e.dma_start(out=mkn[:], in_=mkn_d[0:1, :].partition_broadcast(128)), writes=[d_c])
            P.dma("sp", lambda e: e.dma_start(out=tri[:], in_=tri_d[:, :]), writes=[d_c])
            P.dma("sp", lambda e: e.dma_start(out=ecap[:], in_=ecap_d[0:1, :].partition_broadcast(128)), writes=[d_c])
            P.op("pool", lambda e: e.memset(onesf[:], 1.0), writes=[d_c])
            P.op("pool", lambda e: e.memset(onesb[:], 1.0), writes=[d_c])
            P.op("pool", lambda e: e.memset(base[:], 0.0), writes=[d_base])

            def head_norm(src_ps, d_src, gain, nh, dst, d_dst):
                hd = 512 // nh
                v = lambda ap: ap.rearrange("p (h d) -> p h d", h=nh)
                P.op("act", lambda e: e.activation(out=sq[:], in_=src_ps, func=AF.Square), reads=[d_src], writes=[d_sq])
                P.op("dve", lambda e: e.tensor_reduce(out=hs[:, 0:nh], in_=v(sq[:]), axis=AX.X, op=ALU.add), reads=[d_sq], writes=[d_hs])
                P.op("act", lambda e: e.activation(out=hr[:, 0:nh], in_=hs[:, 0:nh], func=AF.Sqrt, bias=EPS, scale=1.0 / hd), reads=[d_hs], writes=[d_hr])
                P.op("dve", lambda e: e.reciprocal(out=hr[:, 0:nh], in_=hr[:, 0:nh]), reads=[d_hr], writes=[d_hr])
                P.op("dve", lambda e: e.tensor_tensor(out=v(qmn[:]), in0=v(src_ps), in1=hr[:, 0:nh].unsqueeze(2).to_broadcast([128, nh, hd]), op=ALU.mult),
                     reads=[d_src, d_hr], writes=[d_qmn])
                P.op("pool", lambda e: e.tensor_tensor(out=dst, in0=qmn[:], in1=gain[:], op=ALU.mult), reads=[d_qmn, d_c], writes=[d_dst])

            with contextlib.ExitStack() as stm:
                wkv = stm.enter_context(nc.sbuf_tensor("s_wkv", [128, 8, 1024], BF16)); d_wkv = Dep()
                mt_ = stm.enter_context(nc.sbuf_tensor("s_memt", [128, 2, D], F32)); d_mt = Dep()
                gme = stm.enter_context(nc.sbuf_tensor("s_gme", [128, D], F32)); d_gme = Dep()
                mhb = stm.enter_context(nc.sbuf_tensor("s_mhb", [128, 2, D], BF16)); d_mhb = Dep()
                mT = stm.enter_context(nc.sbuf_tensor("s_mT", [128, 8, 256], BF16)); d_mT = Dep()
                ss2 = stm.enter_context(nc.sbuf_tensor("s_ss2", [128, 2], F32)); d_ss2 = Dep()
                for k in range(8):
                    P.dma("pool", lambda e, k=k: e.dma_start(out=wkv[:, k, :], in_=wmemkv[k * 128:(k + 1) * 128, :]), writes=[d_wkv])
                P.dma("sp", lambda e: e.dma_start(out=mt_[:], in_=mem_d[:, :].rearrange("(t p) d -> p t d", p=128)), writes=[d_mt])
                P.dma("sp", lambda e: e.dma_start(out=gme[:], in_=gmem_d[0:1, :].partition_broadcast(128)), writes=[d_gme])
                for it in range(2):
                    P.op("act", lambda e, it=it: e.activation(out=junk[:], in_=mt_[:, it, :], func=AF.Square, accum_out=ss2[:, it:it + 1]),
                         reads=[d_mt], writes=[d_junk, d_ss2])
                P.op("act", lambda e: e.activation(out=ss2[:], in_=ss2[:], func=AF.Sqrt, bias=EPS, scale=1.0 / D), reads=[d_ss2], writes=[d_ss2])
                P.op("dve", lambda e: e.reciprocal(out=ss2[:], in_=ss2[:]), reads=[d_ss2], writes=[d_ss2])
                for it in range(2):
                    P.op("dve", lambda e, it=it: e.scalar_tensor_tensor(out=mhb[:, it, :], in0=mt_[:, it, :], scalar=ss2[:, it:it + 1], in1=gme[:],
                                                                        op0=ALU.mult, op1=ALU.mult), reads=[d_mt, d_ss2, d_gme], writes=[d_mhb])
                    for k in range(8):
                        P.op("pe", lambda e, it=it, k=k: e.transpose(out=pT[:, k, :], in_=mhb[:, it, k * 128:(k + 1) * 128], identity=idb[:]),
                             reads=[d_mhb, d_idb], writes=[d_pT])
                    P.op("act", lambda e, it=it: e.copy(out=mT[:, :, it * 128:(it + 1) * 128], in_=pT[:]), reads=[d_pT], writes=[d_mT])
                for it in range(2):
                    for hv in range(2):
                        for k in range(8):
                            P.op("pe", lambda e, it=it, hv=hv, k=k: e.matmul(pG[hv][:, :], lhsT=mT[:, k, it * 128:(it + 1) * 128], rhs=wkv[:, k, hv * 512:(hv + 1) * 512],
                                                                           start=(k == 0), stop=(k == 7)), reads=[d_mT, d_wkv], writes=[d_pG[hv]])
                    head_norm(pG[0][:, :], d_pG[0], mkn, 4, qmb[:], d_qmb)
                    for h in range(4):
                        P.op("pe", lambda e, h=h: e.transpose(out=pT[:, h, :], in_=qmb[:, h * 128:(h + 1) * 128], identity=idb[:]),
                             reads=[d_qmb, d_idb], writes=[d_pT])
                    P.op("act", lambda e, it=it: e.copy(out=kmT[:, :, it * 128:(it + 1) * 128], in_=pT[:, 0:4, :]), reads=[d_pT], writes=[d_kmT])
                    P.op("act", lambda e, it=it: e.copy(out=vm[:, it, :], in_=pG[1][:, :]), reads=[d_pG[1]], writes=[d_vm])

            wqm = sb("wqm", [128, 8, 512], BF16)
            wg = sb("wg", [128, 8, 3072], BF16)
            woa = sb("woa", [128, 4, 1024], BF16)
            wos = sb("wos", [128, 8, 1024], BF16)
            wom = sb("wom", [128, 4, 1024], BF16)
            wout = sb("wout", [128, 8, 1024], BF16)
            wr = sb("wr", [128, 8, 36], F32)
            for k in range(8):
                rows = slice(k * 128, (k + 1) * 128)
                P.dma("pool", lambda e, k=k, rows=rows: e.dma_start(out=wqm[:, k, :], in_=w_in[rows, C_QM:C_QM + 512]), writes=[d_w])
                for j in range(2):
                    P.dma("pool", lambda e, k=k, rows=rows, j=j: e.dma_start(out=wg[:, k, j * 1536:(j + 1) * 1536], in_=w_in[rows, C_G + j * 1536:C_G + (j + 1) * 1536]), writes=[d_w])
                P.dma("pool", lambda e, k=k, rows=rows: e.dma_start(out=wos[:, k, :], in_=wos_d[rows, :]), writes=[d_w])
                P.dma("pool", lambda e, k=k, rows=rows: e.dma_start(out=wout[:, k, :], in_=wout_d[rows, :]), writes=[d_w])
                P.dma("sp", lambda e, k=k, rows=rows: e.dma_start(out=wr[:, k, :], in_=wr_d[rows, :]), writes=[d_w])
            for k in range(4):
                rows = slice(k * 128, (k + 1) * 128)
                P.dma("pool", lambda e, k=k, rows=rows: e.dma_start(out=woa[:, k, :], in_=woa_d[rows, :]), writes=[d_w])
                P.dma("pool", lambda e, k=k, rows=rows: e.dma_start(out=wom[:, k, :], in_=wom_d[rows, :]), writes=[d_w])
            def norm_d(tj):
                xj = tj % 2
                P.op("act", lambda e: e.activation(out=junk[:], in_=xt[xj][:], func=AF.Square, accum_out=ssqn[:]), reads=[d_xt[xj]], writes=[d_junk, d_ssqn])
                P.op("act", lambda e: e.activation(out=rstdn[:], in_=ssqn[:], func=AF.Sqrt, bias=EPS, scale=1.0 / D), reads=[d_ssqn], writes=[d_rstdn])
                P.op("dve", lambda e: e.reciprocal(out=rstdn[:], in_=rstdn[:]), reads=[d_rstdn], writes=[d_rstdn])
                P.op("dve", lambda e: e.scalar_tensor_tensor(out=hb_[xj][:], in0=xt[xj][:], scalar=rstdn[:, 0:1], in1=gm[:], op0=ALU.mult, op1=ALU.mult),
                     reads=[d_xt[xj], d_rstdn, d_c], writes=[d_hb_[xj]])

            def loads(tj):
                xj = tj % 2
                rj = tj * 128
                P.dma("sp", lambda e: e.dma_start(out=xt[xj][:], in_=xown[rj:rj + 128, :]), writes=[d_xt[xj]])
                P.dma("sp", lambda e: e.dma_start(out=oat[xj][:], in_=oa_d[rj:rj + 128, :]), reads=[d_oad], writes=[d_oat[xj]])
                P.dma("sp", lambda e: e.dma_start(out=ost[xj][:], in_=os_d[rj:rj + 128, :]), reads=[d_osd], writes=[d_ost[xj]])

            for ti in range(ntiles):
                r0 = ti * 128
                hbuf = ti % 2
                xb = ti % 2
                if ti == 0:
                    loads(0)
                if ti + 1 < ntiles:
                    loads(ti + 1)
                if ti == 0:
                    norm_d(0)
                for k in range(8):
                    P.op("pe", lambda e, k=k, xb=xb: e.transpose(out=pT[:, k, :], in_=hb_[xb][:, k * 128:(k + 1) * 128], identity=idb[:]), reads=[d_hb_[xb], d_idb], writes=[d_pT])
                P.op("act", lambda e: e.copy(out=hT[:], in_=pT[:]), reads=[d_pT], writes=[d_hT])
                for k in range(8):
                    P.op("pe", lambda e, k=k: e.matmul(pG[0][:, :], lhsT=hT[:, k, :], rhs=wqm[:, k, :], start=(k == 0), stop=(k == 7)),
                         reads=[d_hT, d_w], writes=[d_pG[0]])
                head_norm(pG[0][:, :], d_pG[0], mqn, 4, qmb[:], d_qmb)
                for g6 in range(6):
                    bk = g6 % 3
                    for k in range(8):
                        P.op("pe", lambda e, g6=g6, k=k, bk=bk: e.matmul(pG[bk][:, :], lhsT=hT[:, k, :], rhs=wg[:, k, g6 * 512:(g6 + 1) * 512], start=(k == 0), stop=(k == 7)),
                             reads=[d_hT, d_w], writes=[d_pG[bk]])
                    P.op("act", lambda e, g6=g6, bk=bk: e.activation(out=gs[:, g6 * 512:(g6 + 1) * 512], in_=pG[bk][:, :], func=AF.Sigmoid),
                         reads=[d_pG[bk]], writes=[d_gs])
                for h in range(4):
                    P.op("pe", lambda e, h=h: e.transpose(out=pT[:, h, :], in_=qmb[:, h * 128:(h + 1) * 128], identity=idb[:]), reads=[d_qmb, d_idb], writes=[d_pT])
                P.op("act", lambda e: e.copy(out=qmT[:], in_=pT[:, 0:4, :]), reads=[d_pT], writes=[d_qmT])
                for h in range(4):
                    for mt in range(2):
                        bank = pms[h // 2]
                        c0 = ((h % 2) * 2 + mt) * 128
                        P.op("pe", lambda e, h=h, mt=mt, bank=bank, c0=c0: e.matmul(bank[:, c0:c0 + 128], lhsT=kmT[:, h, mt * 128:(mt + 1) * 128], rhs=qmT[:, h, :],
                                                                                  start=True, stop=True), reads=[d_kmT, d_qmT], writes=[d_pms[h // 2]])
                for hp in range(2):
                    P.op("act", lambda e, hp=hp: e.activation(out=pm[:, hp * 2:(hp + 1) * 2, :, :].rearrange("p a b c -> p (a b c)"), in_=pms[hp][:, :], func=AF.Exp,
                                                              scale=float(128 ** -0.5)), reads=[d_pms[hp]], writes=[d_pm])
                for h in range(4):
                    for mt in range(2):
                        P.op("pe", lambda e, h=h, mt=mt: e.matmul(pmo[:, h * 128:(h + 1) * 128], lhsT=vm[:, mt, h * 128:(h + 1) * 128], rhs=pm[:, h, mt, :],
                                                                 start=(mt == 0), stop=(mt == 1)), reads=[d_vm, d_pm], writes=[d_pmo])
                for mt in range(2):
                    P.op("pe", lambda e, mt=mt: e.matmul(pG[1][:, :].rearrange("p (h t) -> p h t", h=4), lhsT=onesb[:], rhs=pm[:, :, mt, :],
                                                        start=(mt == 0), stop=(mt == 1)), reads=[d_c, d_pm], writes=[d_pG[1]])
                P.op("dve", lambda e: e.reciprocal(out=rcp[:], in_=pG[1][:, :]), reads=[d_pG[1]], writes=[d_rcp])
                P.op("dve", lambda e: e.tensor_tensor(out=omT[:].rearrange("p h t -> p (h t)"), in0=pmo[:, :], in1=rcp[:], op=ALU.mult),
                     reads=[d_pmo, d_rcp], writes=[d_omT])
                for k in range(4):
                    P.op("pe", lambda e, xb=xb, k=k: e.transpose(out=pT[:, k, :], in_=oat[xb][:, k * 128:(k + 1) * 128], identity=idb[:]), reads=[d_oat[xb], d_idb], writes=[d_pT])
                P.op("act", lambda e: e.copy(out=oaT[:], in_=pT[:, 0:4, :]), reads=[d_pT], writes=[d_oaT])
                for k in range(8):
                    P.op("pe", lambda e, xb=xb, k=k: e.transpose(out=pT[:, k, :], in_=ost[xb][:, k * 128:(k + 1) * 128], identity=idb[:]), reads=[d_ost[xb], d_idb], writes=[d_pT])
                P.op("act", lambda e: e.copy(out=osT[:], in_=pT[:]), reads=[d_pT], writes=[d_osT])
                for hv in range(2):
                    cs_ = slice(hv * 512, (hv + 1) * 512)
                    for k in range(4):
                        P.op("pe", lambda e, k=k, cs_=cs_: e.matmul(pG[0][:, :], lhsT=oaT[:, k, :], rhs=woa[:, k, cs_], start=(k == 0), stop=(k == 3)),
                             reads=[d_oaT, d_w], writes=[d_pG[0]])
                    for k in range(8):
                        P.op("pe", lambda e, k=k, cs_=cs_: e.matmul(pG[1][:, :], lhsT=osT[:, k, :], rhs=wos[:, k, cs_], start=(k == 0), stop=(k == 7)),
                             reads=[d_osT, d_w], writes=[d_pG[1]])
                    for k in range(4):
                        P.op("pe", lambda e, k=k, cs_=cs_: e.matmul(pG[2][:, :], lhsT=omT[:, k, :], rhs=wom[:, k, cs_], start=(k == 0), stop=(k == 3)),
                             reads=[d_omT, d_w], writes=[d_pG[2]])
                    P.op("dve", lambda e, hv=hv: e.tensor_tensor(out=mg[:], in0=pG[0][:, :], in1=gs[:, hv * 512:(hv + 1) * 512], op=ALU.mult),
                         reads=[d_pG[0], d_gs], writes=[d_mg])
                    P.op("dve", lambda e, hv=hv: e.tensor_tensor(out=tt[:], in0=pG[1][:, :], in1=gs[:, 1024 + hv * 512:1024 + (hv + 1) * 512], op=ALU.mult),
                         reads=[d_pG[1], d_gs], writes=[d_tt])
                    P.op("pool", lambda e: e.tensor_tensor(out=mg[:], in0=mg[:], in1=tt[:], op=ALU.add), reads=[d_mg, d_tt], writes=[d_mg])
                    P.op("dve", lambda e, hv=hv: e.tensor_tensor(out=tt[:], in0=pG[2][:, :], in1=gs[:, 2048 + hv * 512:2048 + (hv + 1) * 512], op=ALU.mult),
                         reads=[d_pG[2], d_gs], writes=[d_tt])
                    P.op("pool", lambda e, cs_=cs_: e.tensor_tensor(out=mgb[:, cs_], in0=mg[:], in1=tt[:], op=ALU.add), reads=[d_mg, d_tt], writes=[d_mgb])
                for k in range(8):
                    P.op("pe", lambda e, k=k: e.transpose(out=pT[:, k, :], in_=mgb[:, k * 128:(k + 1) * 128], identity=idb[:]), reads=[d_mgb, d_idb], writes=[d_pT])
                P.op("act", lambda e: e.copy(out=mgT[:], in_=pT[:]), reads=[d_pT], writes=[d_mgT])
                for hv in range(2):
                    cs_ = slice(hv * 512, (hv + 1) * 512)
                    for k in range(8):
                        P.op("pe", lambda e, k=k, cs_=cs_, hv=hv: e.matmul(pG[hv][:, :], lhsT=mgT[:, k, :], rhs=wout[:, k, cs_], start=(k == 0), stop=(k == 7)),
                             reads=[d_mgT, d_w], writes=[d_pG[hv]])
                    P.op("dve", lambda e, xb=xb, cs_=cs_, hv=hv: e.tensor_tensor(out=x1[:, cs_], in0=pG[hv][:, :], in1=xt[xb][:, cs_], op=ALU.add),
                         reads=[d_pG[hv], d_xt[xb]], writes=[d_x1])
                P.dma("sp", lambda e, r0=r0: e.dma_start(out=x1_d[r0:r0 + 128, :], in_=x1[:]), reads=[d_x1], writes=[d_x1d])
                if ti + 1 < ntiles:
                    norm_d(ti + 1)
                if stage < 2:
                    continue
                P.op("act", lambda e: e.activation(out=junk[:], in_=x1[:], func=AF.Square, accum_out=ssq[:]), reads=[d_x1], writes=[d_junk, d_ssq])
                P.op("act", lambda e: e.activation(out=rstd[:], in_=ssq[:], func=AF.Sqrt, bias=EPS, scale=1.0 / D), reads=[d_ssq], writes=[d_rstd])
                P.op("dve", lambda e: e.reciprocal(out=rstd[:], in_=rstd[:]), reads=[d_rstd], writes=[d_rstd])
                P.op("dve", lambda e: e.scalar_tensor_tensor(out=h2f[:], in0=x1[:], scalar=rstd[:, 0:1], in1=gf[:], op0=ALU.mult, op1=ALU.mult),
                     reads=[d_x1, d_rstd, d_c], writes=[d_h2f])
                P.op("pool", lambda e, hbuf=hbuf: e.tensor_copy(out=h2b[hbuf][:], in_=h2f[:]), reads=[d_h2f], writes=[d_h2b[hbuf]])
                for half in range(2):
                    for k4 in range(4):
                        k = half * 4 + k4
                        P.op("pe", lambda e, k=k, k4=k4: e.transpose(out=pF[:, k4 * 128:(k4 + 1) * 128], in_=h2f[:, k * 128:(k + 1) * 128], identity=idf[:]),
                             reads=[d_h2f, d_idf], writes=[d_pF])
                    P.op("act", lambda e, half=half: e.copy(out=h2T[:, half * 4:(half + 1) * 4, :].rearrange("p a b -> p (a b)"), in_=pF[:, :]), reads=[d_pF], writes=[d_h2T])
                for k in range(8):
                    P.op("pe", lambda e, k=k: e.matmul(pF[:, 0:36], lhsT=h2T[:, k, :], rhs=wr[:, k, :], start=(k == 0), stop=(k == 7)), reads=[d_h2T, d_w], writes=[d_pF])
                P.op("act", lambda e: e.copy(out=lg[:], in_=pF[:, 0:36]), reads=[d_pF], writes=[d_lg])
                P.op("dve", lambda e: e.tensor_reduce(out=sm[:, 0:1], in_=lg[:, 0:4], axis=AX.X, op=ALU.max), reads=[d_lg], writes=[d_sm])
                P.op("dve", lambda e: e.tensor_scalar(out=sm[:, 1:2], in0=sm[:, 0:1], scalar1=-1.0, scalar2=None, op0=ALU.mult), reads=[d_sm], writes=[d_sm])
                P.op("act", lambda e: e.activation(out=sm[:, 8:12], in_=lg[:, 0:4], func=AF.Exp, bias=sm[:, 1:2], accum_out=sm[:, 2:3]), reads=[d_lg, d_sm], writes=[d_sm])
                P.op("dve", lambda e: e.reciprocal(out=sm[:, 3:4], in_=sm[:, 2:3]), reads=[d_sm], writes=[d_sm])
                P.op("dve", lambda e: e.tensor_scalar(out=goh[:], in0=lg[:, 0:4], scalar1=sm[:, 0:1], scalar2=None, op0=ALU.is_ge), reads=[d_lg, d_sm], writes=[d_goh])
                P.op("dve", lambda e: e.tensor_tensor(out=lsel[:].rearrange("p (g e) -> p g e", g=4), in0=lg[:, 4:36].rearrange("p (g e) -> p g e", g=4),
                                                      in1=goh[:].unsqueeze(2).to_broadcast([128, 4, 8]), op=ALU.mult), reads=[d_lg, d_goh], writes=[d_lsel])
                P.op("dve", lambda e: e.tensor_reduce(out=les[:], in_=lsel[:].rearrange("p (g e) -> p e g", g=4), axis=AX.X, op=ALU.add), reads=[d_lsel], writes=[d_les])
                P.op("dve", lambda e: e.max(out=t8[:], in_=les[:]), reads=[d_les], writes=[d_t8])
                P.op("dve", lambda e: e.tensor_tensor(out=sm[:, 4:5], in0=t8[:, 1:2], in1=t8[:, 0:1], op=ALU.subtract), reads=[d_t8, d_sm], writes=[d_sm])
                P.op("act", lambda e: e.activation(out=sm[:, 5:6], in_=sm[:, 4:5], func=AF.Exp), reads=[d_sm], writes=[d_sm])
                P.op("dve", lambda e: e.tensor_scalar(out=sm[:, 6:7], in0=sm[:, 5:6], scalar1=1.0, scalar2=None, op0=ALU.add), reads=[d_sm], writes=[d_sm])
                P.op("dve", lambda e: e.reciprocal(out=sm[:, 6:7], in_=sm[:, 6:7]), reads=[d_sm], writes=[d_sm])
                P.op("dve", lambda e: e.tensor_tensor(out=sm[:, 7:8], in0=sm[:, 5:6], in1=sm[:, 6:7], op=ALU.mult), reads=[d_sm], writes=[d_sm])
                P.op("dve", lambda e, ti=ti: e.tensor_scalar(out=combs[:, ti, :], in0=sm[:, 6:8], scalar1=sm[:, 3:4], scalar2=None, op0=ALU.mult), reads=[d_sm], writes=[d_combs])
                for j in range(2):
                    P.op("dve", lambda e, j=j: e.tensor_scalar(out=oh[:, j * 32:(j + 1) * 32], in0=lg[:, 4:36], scalar1=t8[:, j:j + 1], scalar2=None, op0=ALU.is_equal),
                         reads=[d_lg, d_t8], writes=[d_oh])
                    P.op("dve", lambda e, j=j: e.tensor_tensor(out=oh[:, j * 32:(j + 1) * 32].rearrange("p (g e) -> p g e", g=4), in0=oh[:, j * 32:(j + 1) * 32].rearrange("p (g e) -> p g e", g=4),
                                                               in1=goh[:].unsqueeze(2).to_broadcast([128, 4, 8]), op=ALU.mult), reads=[d_oh, d_goh], writes=[d_oh])
                P.op("dve", lambda e: e.tensor_tensor(out=ohs[:], in0=oh[:, 0:32], in1=oh[:, 32:64], op=ALU.add), reads=[d_oh], writes=[d_ohs])
                P.op("pe", lambda e: e.matmul(pF[:, 64:96], lhsT=tri[:], rhs=ohs[:], start=True, stop=True), reads=[d_c, d_ohs], writes=[d_pF])
                P.op("pe", lambda e: e.matmul(pF[:, 128:160], lhsT=onesf[:], rhs=ohs[:], start=True, stop=True), reads=[d_c, d_ohs], writes=[d_pF])
                P.op("dve", lambda e: e.tensor_tensor(out=posf[:], in0=pF[:, 64:96], in1=ohs[:], op=ALU.subtract), reads=[d_pF, d_ohs], writes=[d_posf])
                P.op("dve", lambda e: e.tensor_tensor(out=posf[:], in0=posf[:], in1=base[:], op=ALU.add), reads=[d_posf, d_base], writes=[d_posf])
                P.op("dve", lambda e: e.tensor_tensor(out=posf[:], in0=posf[:], in1=ecap[:], op=ALU.add), reads=[d_posf, d_c], writes=[d_posf])
                P.op("dve", lambda e: e.tensor_tensor(out=base[:], in0=base[:], in1=pF[:, 128:160], op=ALU.add), reads=[d_base, d_pF, d_posf], writes=[d_base])
                for j in range(2):
                    P.op("dve", lambda e, j=j: e.tensor_tensor(out=oh[:, j * 32:(j + 1) * 32], in0=oh[:, j * 32:(j + 1) * 32], in1=posf[:], op=ALU.mult), reads=[d_oh, d_posf], writes=[d_oh])
                    P.op("dve", lambda e, j=j: e.tensor_reduce(out=idf2[:, j:j + 1], in_=oh[:, j * 32:(j + 1) * 32], axis=AX.X, op=ALU.add), reads=[d_oh], writes=[d_idf2])
                P.op("dve", lambda e, ti=ti: e.tensor_copy(out=idxs[:, ti, :], in_=idf2[:]), reads=[d_idf2], writes=[d_idxs])
                for j in range(2):
                    P.dma("pool", lambda e, ti=ti, j=j, hbuf=hbuf: e.indirect_dma_start(out=xbuf[:, :], out_offset=bass.IndirectOffsetOnAxis(ap=idxs[:, ti, j:j + 1], axis=0),
                                                                                      in_=h2b[hbuf][:], in_offset=None),
                          reads=[d_h2b[hbuf], d_idxs], writes=[d_xbuf])

        if stage < 3:
            return
        if hasattr(P, 'barrier'):
            P.barrier()
        with contextlib.ExitStack() as st:
            def sb(name, shape, dt):
                return st.enter_context(nc.sbuf_tensor("d_" + name, shape, dt))

            def ps(name, shape, dt=F32):
                return st.enter_context(nc.psum_tensor(name, shape, dt))
            NW = 2
            wge = [sb("wge%d" % i, [128, 8, 512], BF16) for i in range(NW)]; d_wge = [Dep() for _ in range(NW)]
            wue = [sb("wue%d" % i, [128, 8, 512], BF16) for i in range(NW)]; d_wue = [Dep() for _ in range(NW)]
            wde = [sb("wde%d" % i, [128, 4, 1024], BF16) for i in range(NW)]; d_wde = [Dep() for _ in range(NW)]
            xe = [sb("xe%d" % i, [128, 3, D], BF16) for i in range(NW)]; d_xe = [Dep() for _ in range(NW)]
            xeT = sb("xeT", [128, 8, CAP], BF16); d_xeT = Dep()
            sg = sb("sg", [128, CAP], F32); d_sg = Dep()
            hTe = sb("hTe", [128, 4, CAP], BF16); d_hTe = Dep()
            ye = [sb("ye%d" % i, [128, 3, D], F32) for i in range(NW)]; d_ye = [Dep() for _ in range(NW)]
            pT = ps("pTe", [128, 8, 128], BF16); d_pT = Dep()
            pg_ = [ps("pge%d" % i, [128, 512], F32) for i in range(2)]; d_pg = [Dep() for _ in range(2)]
            pu_ = [ps("pue%d" % i, [128, 512], F32) for i in range(2)]; d_pu = [Dep() for _ in range(2)]
            py_ = [ps("pye%d" % i, [128, 512], F32) for i in range(2)]; d_py = [Dep() for _ in range(2)]
            experts = list(experts)

            def load_xe(je):
                bj = je % NW
                exj = experts[je]
                P.dma("sp", lambda e: e.dma_start(out=xe[bj][:], in_=xbuf[exj * CAP:(exj + 1) * CAP, :].rearrange("(s p) d -> p s d", p=128)),
                      reads=[d_xbuf], writes=[d_xe[bj]])

            for ie, ex in enumerate(experts):
                b = ie % NW
                P.dma("pool", lambda e, b=b, ex=ex: e.dma_start(out=wge[b][:], in_=wgate_d[ex].rearrange("(k p) f -> p k f", p=128)), writes=[d_wge[b]])
                P.dma("pool", lambda e, b=b, ex=ex: e.dma_start(out=wue[b][:], in_=wup_d[ex].rearrange("(k p) f -> p k f", p=128)), writes=[d_wue[b]])
                P.dma("pool", lambda e, b=b, ex=ex: e.dma_start(out=wde[b][:], in_=wdown_d[ex].rearrange("(k p) f -> p k f", p=128)), writes=[d_wde[b]])
                if ie == 0:
                    load_xe(0)
                if ie + 1 < len(experts):
                    load_xe(ie + 1)
                for s in range(3):
                    for k in range(8):
                        P.op("pe", lambda e, b=b, s=s, k=k: e.transpose(out=pT[:, k, :], in_=xe[b][:, s, k * 128:(k + 1) * 128], identity=idb[:]),
                             reads=[d_xe[b], d_idb], writes=[d_pT])
                    P.op("act", lambda e, s=s: e.copy(out=xeT[:, :, s * 128:(s + 1) * 128], in_=pT[:]), reads=[d_pT], writes=[d_xeT])
                for ft in range(4):
                    pb = ft % 2
                    for k in range(8):
                        P.op("pe", lambda e, b=b, ft=ft, k=k, pb=pb: e.matmul(pg_[pb][:, 0:CAP], lhsT=wge[b][:, k, ft * 128:(ft + 1) * 128], rhs=xeT[:, k, :],
                                                                            start=(k == 0), stop=(k == 7)), reads=[d_wge[b], d_xeT], writes=[d_pg[pb]])
                    for k in range(8):
                        P.op("pe", lambda e, b=b, ft=ft, k=k, pb=pb: e.matmul(pu_[pb][:, 0:CAP], lhsT=wue[b][:, k, ft * 128:(ft + 1) * 128], rhs=xeT[:, k, :],
                                                                            start=(k == 0), stop=(k == 7)), reads=[d_wue[b], d_xeT], writes=[d_pu[pb]])
                    P.op("act", lambda e, pb=pb: e.activation(out=sg[:], in_=pg_[pb][:, 0:CAP], func=AF.Silu), reads=[d_pg[pb]], writes=[d_sg])
                    P.op("dve", lambda e, ft=ft, pb=pb: e.tensor_tensor(out=hTe[:, ft, :], in0=sg[:], in1=pu_[pb][:, 0:CAP], op=ALU.mult),
                         reads=[d_sg, d_pu[pb]], writes=[d_hTe])
                for s in range(3):
                    for hv in range(2):
                        for ft in range(4):
                            P.op("pe", lambda e, b=b, s=s, hv=hv, ft=ft: e.matmul(py_[hv][:, :], lhsT=hTe[:, ft, s * 128:(s + 1) * 128], rhs=wde[b][:, ft, hv * 512:(hv + 1) * 512],
                                                                                start=(ft == 0), stop=(ft == 3)), reads=[d_hTe, d_wde[b]], writes=[d_py[hv]])
                        P.op("act" if hv == 0 else "dve", (lambda e, b=b, s=s, hv=hv: e.copy(out=ye[b][:, s, hv * 512:(hv + 1) * 512], in_=py_[hv][:, :])) if hv == 0 else
                             (lambda e, b=b, s=s, hv=hv: e.tensor_copy(out=ye[b][:, s, hv * 512:(hv + 1) * 512], in_=py_[hv][:, :])),
                             reads=[d_py[hv]], writes=[d_ye[b]])
                P.dma("sp", lambda e, b=b, ex=ex: e.dma_start(out=ybuf[ex * CAP:(ex + 1) * CAP, :].rearrange("(s p) d -> p s d", p=128), in_=ye[b][:]),
                      reads=[d_ye[b]], writes=[d_ybuf])

        if hasattr(P, 'barrier'):
            P.barrier()
        with contextlib.ExitStack() as st:
            def sb(name, shape, dt):
                return st.enter_context(nc.sbuf_tensor("d_" + name, shape, dt))
            NF = 4
            x1t = [sb("x1t%d" % i, [128, D], F32) for i in range(NF)]; d_x1t = [Dep() for _ in range(NF)]
            y1 = [sb("y1t%d" % i, [128, D], F32) for i in range(NF)]; d_y1 = [Dep() for _ in range(NF)]
            y2 = [sb("y2t%d" % i, [128, D], F32) for i in range(NF)]; d_y2 = [Dep() for _ in range(NF)]
            ot = [sb("ot%d" % i, [128, D], F32) for i in range(NF)]; d_ot = [Dep() for _ in range(NF)]
            for ti in range(ntiles):
                b = ti % NF
                r0 = ti * 128
                P.dma("sp", lambda e, b=b, r0=r0: e.dma_start(out=x1t[b][:], in_=x1_d[r0:r0 + 128, :]), reads=[d_x1d], writes=[d_x1t[b]])
                P.dma("pool", lambda e, b=b, ti=ti: e.indirect_dma_start(out=y1[b][:], out_offset=None, in_=ybuf[:, :],
                                                                        in_offset=bass.IndirectOffsetOnAxis(ap=idxs[:, ti, 0:1], axis=0)),
                      reads=[d_ybuf, d_idxs], writes=[d_y1[b]])
                P.dma("pool", lambda e, b=b, ti=ti: e.indirect_dma_start(out=y2[b][:], out_offset=None, in_=ybuf[:, :],
                                                                        in_offset=bass.IndirectOffsetOnAxis(ap=idxs[:, ti, 1:2], axis=0)),
                      reads=[d_ybuf, d_idxs], writes=[d_y2[b]])
                P.op("dve", lambda e, b=b, ti=ti: e.scalar_tensor_tensor(out=ot[b][:], in0=y1[b][:], scalar=combs[:, ti, 0:1], in1=x1t[b][:], op0=ALU.mult, op1=ALU.add),
                     reads=[d_y1[b], d_combs, d_x1t[b]], writes=[d_ot[b]])
                P.op("dve", lambda e, b=b, ti=ti: e.scalar_tensor_tensor(out=ot[b][:], in0=y2[b][:], scalar=combs[:, ti, 1:2], in1=ot[b][:], op0=ALU.mult, op1=ALU.add),
                     reads=[d_y2[b], d_combs, d_ot[b]], writes=[d_ot[b]])
                P.dma("sp", lambda e, b=b, r0=r0: e.dma_start(out=out_d[r0:r0 + 128, :], in_=ot[b][:]), reads=[d_ot[b]], writes=[d_out])


def host_consts_d(z):
    tri = (np.arange(128)[:, None] <= np.arange(128)[None, :]).astype(np.float32)
    return dict(mqn=np.tile(z['mem_q_norm'], 4)[None, :], mkn=np.tile(z['mem_k_norm'], 4)[None, :],
                w_r=np.ascontiguousarray(np.concatenate([z['w_router_group'], z['w_router_expert']], axis=1)),
                trif=tri, ecap=(np.arange(32, dtype=np.float32) * CAP)[None, :], ident=np.eye(128, dtype=np.float32))


def build_full():
    nc = bass.Bass("TRN2", target_bir_lowering=False)
    P = Prog(nc)

    def di(name, shape, dt=F32):
        return nc.dram_tensor(name, shape, dt, kind="ExternalInput")
    xin = di("xin", [TP + T, D]); w_in = di("w_in", [D, IN_COLS]); g_mix = di("g_mix", [1, D])
    qkn = di("qkn", [2, 512]); cs = di("cs", [TP + T, 64]); ident_d = di("ident", [128, 128])
    e_d = di("e_d", [32, NK], BF16); vq_d = di("vq", [1, 1024]); nb_d = di("nb", [1, 1024]); oq_d = di("oq", [1, 1024])
    tri_d = di("tri", [128, 128], BF16)
    cwl_d = di("cwl", [128, 16, 4]); cbl_d = di("cbl", [128, 16]); dtb_d = di("dtb", [1, 16]); alog_d = di("alog", [1, 16])
    dsk_d = di("dsk", [1, 16]); ssdn_d = di("ssdn", [1, D]); tri2_d = di("tri2", [128, 2, 256]); sel_d = di("sel_d", [16, 16, 128])
    tb_d = di("tb_d", [128, 384]); pflag_d = di("pflag", [1, 1])
    mem_d = di("mem", [256, D]); gmem_d = di("g_mem", [1, D]); wmemkv = di("w_mem_kv", [D, 1024])
    mqn_d = di("mqn", [1, 512]); mkn_d = di("mkn", [1, 512])
    woa_d = di("w_o_moba", [512, D]); wos_d = di("w_o_ssd", [D, D]); wom_d = di("w_o_mem", [512, D]); wout_d = di("w_out", [D, D])
    gffn_d = di("g_ffn", [1, D]); wr_d = di("w_r", [D, 36])
    wgate_d = di("w_gate", [NE, D, 512]); wup_d = di("w_up", [NE, D, 512]); wdown_d = di("w_down", [NE, 512, D])
    trif_d = di("trif", [128, 128]); ecap_d = di("ecap", [1, 32])
    out_d = nc.dram_tensor("out", [T, D], F32, kind="ExternalOutput")
    kT_d = nc.dram_tensor("kT_d", [512, NK], BF16)
    v_d = nc.dram_tensor("v_d", [NK, 512], BF16)
    qT_d = nc.dram_tensor("qT_d", [512, T], BF16)
    oa_d = nc.dram_tensor("oa_d", [T, 512], BF16)
    os_d = nc.dram_tensor("os_d", [T, D], BF16)
    x1_d = nc.dram_tensor("x1_d", [T, D], F32)
    xbuf = nc.dram_tensor("xbuf", [NE * CAP, D], BF16)
    ybuf = nc.dram_tensor("ybuf", [NE * CAP, D], F32)

    with contextlib.ExitStack() as st0:
        idf = st0.enter_context(nc.sbuf_tensor("idf", [128, 128], F32)); d_idf = Dep()
        idb = st0.enter_context(nc.sbuf_tensor("idb", [128, 128], BF16)); d_idb = Dep()
        P.dma("sp", lambda e: e.dma_start(out=idf[:], in_=ident_d[:, :]), writes=[d_idf])
        P.op("dve", lambda e: e.tensor_copy(out=idb[:], in_=idf[:]), reads=[d_idf], writes=[d_idb])
        with contextlib.ExitStack() as st:
            phase_a(nc, P, st, xin, w_in, g_mix, qkn, cs, idb, d_idb, kT_d, v_d, qT_d)
        P.barrier()
        with contextlib.ExitStack() as st:
            oa = st.enter_context(nc.sbuf_tensor("s_oa", [128, 32, 512], BF16)); d_oa = Dep()
            phase_b(nc, P, st, kT_d, v_d, qT_d, e_d, vq_d, nb_d, oq_d, tri_d, idb, d_idb, oa, d_oa)
            P.dma("sp", lambda e: e.dma_start(out=oa_d[:, :].rearrange("(t p) c -> p t c", p=128), in_=oa[:]), reads=[d_oa])
        P.barrier()
        with contextlib.ExitStack() as st:
            phase_c(nc, P, st, xin, w_in, g_mix, cwl_d, cbl_d, dtb_d, alog_d, dsk_d, ssdn_d, tri2_d, sel_d, tb_d, pflag_d,
                    idb, d_idb, os_d, Dep())
        P.barrier()
        phase_def(nc, P, xin[TP:TP + T, :], w_in, g_mix, mem_d, gmem_d, wmemkv, mqn_d, mkn_d, woa_d, wos_d, wom_d, wout_d, gffn_d, wr_d,
                  wgate_d, wup_d, wdown_d, trif_d, ecap_d, oa_d, Dep(), os_d, Dep(), x1_d, xbuf, ybuf, out_d, idb, d_idb, idf, d_idf)
        P.emit()
    return nc


_NC_CACHE = {}


def kernel(x, mem, g_mix, w_in, moba_q_norm, moba_k_norm, conv_w, conv_b, dt_bias, a_log, d_skip, ssd_norm, g_mem, w_mem_kv,
           mem_q_norm, mem_k_norm, w_o_moba, w_o_ssd, w_o_mem, w_out, g_ffn, w_router_group, w_router_expert, w_gate, w_up, w_down):
    f32 = np.float32
    A = lambda a: np.ascontiguousarray(np.asarray(a, dtype=f32))
    x = A(x); mem = A(mem)
    z = dict(conv_w=A(conv_w), conv_b=A(conv_b), dt_bias=A(dt_bias), a_log=A(a_log), d_skip=A(d_skip), ssd_norm=A(ssd_norm),
             mem_q_norm=A(mem_q_norm), mem_k_norm=A(mem_k_norm), w_router_group=A(w_router_group), w_router_expert=A(w_router_expert))
    if "nc" not in _NC_CACHE:
        _NC_CACHE["nc"] = build_full()
    nc = _NC_CACHE["nc"]
    pos = np.arange(2 * T, dtype=f32)
    inv = (10000.0 ** (-np.arange(32, dtype=f32) / 32)).astype(f32)
    ang = pos[:, None] * inv[None, :]
    cs_full = np.concatenate([np.cos(ang), np.sin(ang)], axis=1).astype(f32)
    qkn = np.stack([np.tile(A(moba_q_norm), 8), np.tile(A(moba_k_norm), 8)]).astype(f32)
    shared = dict(w_in=A(w_in), g_mix=A(g_mix)[None, :], qkn=qkn, ident=np.eye(128, dtype=f32),
                  g_mem=A(g_mem)[None, :], w_mem_kv=A(w_mem_kv), w_o_moba=A(w_o_moba), w_o_ssd=A(w_o_ssd), w_o_mem=A(w_o_mem),
                  w_out=A(w_out), g_ffn=A(g_ffn)[None, :], w_gate=A(w_gate), w_up=A(w_up), w_down=A(w_down))
    shared.update(host_consts_d(z))
    in_maps = []
    for c in range(8):
        bb, hh = c // 2, c % 2
        if hh == 0:
            xin = np.concatenate([np.zeros((TP, D), f32), x[bb, :T]], 0)
            csc = np.concatenate([cs_full[:TP], cs_full[:T]], 0)
        else:
            xin = x[bb]
            csc = cs_full
        m = dict(shared)
        m.update(xin=np.ascontiguousarray(xin), cs=np.ascontiguousarray(csc), mem=mem[bb])
        m.update(host_consts(hh))
        m.update(host_consts_c(z, hh))
        in_maps.append(m)
    res = run_bass_kernel_spmd(nc, in_maps, core_ids=list(range(8)))
    out = np.empty((4, 2 * T, D), f32)
    for c in range(8):
        bb, hh = c // 2, c % 2
        out[bb, hh * T:(hh + 1) * T] = np.asarray(res.results[c]["out"], dtype=f32)
    return out
```
